# Optimizing a Trainium2 kernel written in Bass

```python
import math
import jax
import jax.numpy as jnp
from jax import lax
import numpy as np

D_MODEL = 1024
BATCH = 2
SEQ = 8192
DEPTH = 1

CHUNK = 64
EPS = 1e-6
NEG_INF = -1e30
ATT_HEADS = 8
ATT_HEAD_DIM = 64
ATT_WIDTH = ATT_HEADS * ATT_HEAD_DIM
LEFT_CHUNKS = 8
BAND_CHUNKS = LEFT_CHUNKS + 1
BAND = BAND_CHUNKS * CHUNK
REL_MAX = 128
REL_BUCKETS = (CHUNK - 1) + REL_MAX + 1
SSM_WIDTH = D_MODEL - ATT_WIDTH
SSM_GROUP = 16
SSM_GROUPS = SSM_WIDTH // SSM_GROUP
SSM_STATE = 64
MIX_WIDTH = ATT_WIDTH + SSM_WIDTH
IN_WIDTH = 3 * ATT_WIDTH + SSM_WIDTH
MEM_LEN = 256
MEM_HEADS = 4
MEM_HEAD_DIM = D_MODEL // MEM_HEADS
PEER_HEADS = 8
PEER_KEYS = 128
PEER_EXPERTS = PEER_KEYS * PEER_KEYS
PEER_TOPK = 16
PEER_QDIM = 256
PEER_BLOCK = 128

kernel_name = 'hybrid_chunkattn_s5_peer_block'


def _rmsnorm(x, g):
    xf = x.astype(jnp.float32)
    y = xf * lax.rsqrt(jnp.mean(xf * xf, axis=-1, keepdims=True) + EPS)
    return (y * g.astype(jnp.float32)).astype(x.dtype)


def _band_bias(rel_bias):
    qi = jnp.arange(CHUNK)
    kj = jnp.arange(BAND)
    dist = LEFT_CHUNKS * CHUNK + qi[:, None] - kj[None, :]
    bucket = jnp.clip(dist, -(CHUNK - 1), REL_MAX) + (CHUNK - 1)
    return rel_bias[:, bucket]


def _chunked_attention(q, k, v, rel_bias):
    bsz, seq = q.shape[0], q.shape[1]
    nc = seq // CHUNK
    shp = (bsz, nc, CHUNK, ATT_HEADS, ATT_HEAD_DIM)
    qc = q.reshape(shp)
    pad = ((0, 0), (LEFT_CHUNKS, 0), (0, 0), (0, 0), (0, 0))
    kp = jnp.pad(k.reshape(shp), pad)
    vp = jnp.pad(v.reshape(shp), pad)
    idx = jnp.arange(nc)[:, None] + jnp.arange(BAND_CHUNKS)[None, :]
    band_shp = (bsz, nc, BAND, ATT_HEADS, ATT_HEAD_DIM)
    kb = kp[:, idx].reshape(band_shp)
    vb = vp[:, idx].reshape(band_shp)
    s = jnp.einsum('bnqhd,bnkhd->bhnqk', qc, kb).astype(jnp.float32) * (ATT_HEAD_DIM ** -0.5)
    s = s + _band_bias(rel_bias).astype(jnp.float32)[None, :, None]
    key_chunk = jnp.arange(nc)[:, None] - LEFT_CHUNKS + (jnp.arange(BAND) // CHUNK)[None, :]
    s = jnp.where((key_chunk >= 0)[None, None, :, None, :], s, NEG_INF)
    p = jax.nn.softmax(s, axis=-1).astype(v.dtype)
    o = jnp.einsum('bhnqk,bnkhd->bnqhd', p, vb)
    return o.reshape(bsz, seq, ATT_WIDTH)


def _complex_affine_combine(c1, c2):
    a1r, a1i, b1r, b1i = c1
    a2r, a2i, b2r, b2i = c2
    ar = a2r * a1r - a2i * a1i
    ai = a2r * a1i + a2i * a1r
    br = a2r * b1r - a2i * b1i + b2r
    bi = a2r * b1i + a2i * b1r + b2i
    return (ar, ai, br, bi)


def _s5_mixer(u, lam_re, lam_im, log_step, b_re, b_im, c_re, c_im, d_skip, w_glu, b_glu):
    bsz, seq = u.shape[0], u.shape[1]
    uf = u.astype(jnp.float32).reshape(bsz, seq, SSM_GROUPS, SSM_GROUP)
    step = jnp.exp(log_step.astype(jnp.float32))[:, None]
    lr = lam_re.astype(jnp.float32)
    li = lam_im.astype(jnp.float32)
    mag = jnp.exp(lr * step)
    ar = mag * jnp.cos(li * step)
    ai = mag * jnp.sin(li * step)
    zr = ar - 1.0
    den = lr * lr + li * li
    fr = (zr * lr + ai * li) / den
    fi = (ai * lr - zr * li) / den
    br = b_re.astype(jnp.float32)
    bi = b_im.astype(jnp.float32)
    bbr = fr[..., None] * br - fi[..., None] * bi
    bbi = fr[..., None] * bi + fi[..., None] * br
    bu_r = jnp.einsum('blgc,gpc->blgp', uf, bbr)
    bu_i = jnp.einsum('blgc,gpc->blgp', uf, bbi)
    a_r = jnp.broadcast_to(ar, bu_r.shape)
    a_i = jnp.broadcast_to(ai, bu_i.shape)
    _, _, xr, xi = lax.associative_scan(_complex_affine_combine, (a_r, a_i, bu_r, bu_i), axis=1)
    y = (jnp.einsum('blgp,gcp->blgc', xr, c_re.astype(jnp.float32))
         - jnp.einsum('blgp,gcp->blgc', xi, c_im.astype(jnp.float32))
         + d_skip.astype(jnp.float32) * uf)
    y = jax.nn.gelu(y.reshape(bsz, seq, SSM_WIDTH), approximate=False)
    y = y * jax.nn.sigmoid(y @ w_glu.astype(jnp.float32) + b_glu.astype(jnp.float32))
    return y.astype(u.dtype)


def _memory_attention(hn, memn, w_q, w_k, w_v, q_g, k_g, w_o):
    bsz, seq = hn.shape[0], hn.shape[1]
    m = memn.shape[1]
    q = _rmsnorm((hn @ w_q).reshape(bsz, seq, MEM_HEADS, MEM_HEAD_DIM), q_g)
    k = _rmsnorm((memn @ w_k).reshape(bsz, m, MEM_HEADS, MEM_HEAD_DIM), k_g)
    v = (memn @ w_v).reshape(bsz, m, MEM_HEADS, MEM_HEAD_DIM)
    s = jnp.einsum('blhd,bmhd->bhlm', q, k).astype(jnp.float32) * (MEM_HEAD_DIM ** -0.5)
    p = jax.nn.softmax(s, axis=-1).astype(v.dtype)
    o = jnp.einsum('bhlm,bmhd->blhd', p, v).reshape(bsz, seq, D_MODEL)
    return o @ w_o


def _peer_ffn(hn, w_q, sub_keys, u_tab, v_tab):
    bsz, seq, d = hn.shape
    tokens = hn.reshape(-1, PEER_BLOCK, d)

    def block(xb):
        q = (xb @ w_q).reshape(PEER_BLOCK, PEER_HEADS, 2, PEER_QDIM // 2)
        s = jnp.einsum('thsd,hsnd->thsn', q, sub_keys).astype(jnp.float32)
        sv, si = lax.top_k(s, PEER_TOPK)
        cand = (sv[:, :, 0, :, None] + sv[:, :, 1, None, :]).reshape(PEER_BLOCK, PEER_HEADS, -1)
        cidx = (si[:, :, 0, :, None] * PEER_KEYS + si[:, :, 1, None, :]).reshape(PEER_BLOCK, PEER_HEADS, -1)
        top, pos = lax.top_k(cand, PEER_TOPK)
        e = jnp.take_along_axis(cidx, pos, axis=-1)
        g = jax.nn.softmax(top, axis=-1)
        act = jax.nn.gelu(jnp.einsum('thkd,td->thk', u_tab[e], xb), approximate=False)
        w = (g * act).astype(xb.dtype)
        return jnp.einsum('thk,thkd->td', w, v_tab[e])

    return lax.map(block, tokens).reshape(bsz, seq, d)


def setup_inputs(seed: int = 0) -> dict:
    key = jax.random.key(seed)
    ks = iter(jax.random.split(key, 40))

    def nrm(shape, scale):
        return scale * jax.random.normal(next(ks), shape, jnp.float32)

    def gain(n):
        return 1.0 + nrm((DEPTH, n), 0.02)

    x = nrm((BATCH, SEQ, D_MODEL), 1.0)
    mem = nrm((BATCH, MEM_LEN, D_MODEL), 1.0)
    norm_mix_g = gain(D_MODEL)
    w_in = nrm((DEPTH, D_MODEL, IN_WIDTH), D_MODEL ** -0.5)
    att_q_g = gain(ATT_HEAD_DIM)
    att_k_g = gain(ATT_HEAD_DIM)
    rel_bias = nrm((DEPTH, ATT_HEADS, REL_BUCKETS), 0.5)
    ssm_lam_re = -0.5 + nrm((DEPTH, SSM_GROUPS, SSM_STATE), 0.01)
    ssm_lam_im = math.pi * jnp.arange(SSM_STATE, dtype=jnp.float32) + nrm((DEPTH, SSM_GROUPS, SSM_STATE), 0.01)
    ssm_log_step = jax.random.uniform(next(ks), (DEPTH, SSM_GROUPS), jnp.float32, math.log(1e-3), math.log(1e-1))
    ssm_b_re = nrm((DEPTH, SSM_GROUPS, SSM_STATE, SSM_GROUP), (2 * SSM_GROUP) ** -0.5)
    ssm_b_im = nrm((DEPTH, SSM_GROUPS, SSM_STATE, SSM_GROUP), (2 * SSM_GROUP) ** -0.5)
    ssm_c_re = nrm((DEPTH, SSM_GROUPS, SSM_GROUP, SSM_STATE), (2 * SSM_STATE) ** -0.5)
    ssm_c_im = nrm((DEPTH, SSM_GROUPS, SSM_GROUP, SSM_STATE), (2 * SSM_STATE) ** -0.5)
    ssm_d = nrm((DEPTH, SSM_GROUPS, SSM_GROUP), 1.0)
    ssm_w_glu = nrm((DEPTH, SSM_WIDTH, SSM_WIDTH), SSM_WIDTH ** -0.5)
    ssm_b_glu = nrm((DEPTH, SSM_WIDTH), 0.01)
    att_out_g = gain(ATT_WIDTH)
    ssm_out_g = gain(SSM_WIDTH)
    w_out = nrm((DEPTH, MIX_WIDTH, D_MODEL), MIX_WIDTH ** -0.5)
    norm_mem_g = gain(D_MODEL)
    norm_memkv_g = gain(D_MODEL)
    w_mem_q = nrm((DEPTH, D_MODEL, D_MODEL), D_MODEL ** -0.5)
    w_mem_k = nrm((DEPTH, D_MODEL, D_MODEL), D_MODEL ** -0.5)
    w_mem_v = nrm((DEPTH, D_MODEL, D_MODEL), D_MODEL ** -0.5)
    mem_q_g = gain(MEM_HEAD_DIM)
    mem_k_g = gain(MEM_HEAD_DIM)
    w_mem_o = nrm((DEPTH, D_MODEL, D_MODEL), D_MODEL ** -0.5)
    norm_peer_g = gain(D_MODEL)
    w_peer_q = nrm((DEPTH, D_MODEL, PEER_HEADS * PEER_QDIM), D_MODEL ** -0.5)
    peer_keys = nrm((DEPTH, PEER_HEADS, 2, PEER_KEYS, PEER_QDIM // 2), (PEER_QDIM // 2) ** -0.5)
    peer_u = nrm((DEPTH, PEER_EXPERTS, D_MODEL), D_MODEL ** -0.5)
    peer_v = nrm((DEPTH, PEER_EXPERTS, D_MODEL), PEER_HEADS ** -0.5)
    return {'x': x, 'mem': mem, 'norm_mix_g': norm_mix_g, 'w_in': w_in,
            'att_q_g': att_q_g, 'att_k_g': att_k_g, 'rel_bias': rel_bias,
            'ssm_lam_re': ssm_lam_re, 'ssm_lam_im': ssm_lam_im, 'ssm_log_step': ssm_log_step,
            'ssm_b_re': ssm_b_re, 'ssm_b_im': ssm_b_im, 'ssm_c_re': ssm_c_re, 'ssm_c_im': ssm_c_im,
            'ssm_d': ssm_d, 'ssm_w_glu': ssm_w_glu, 'ssm_b_glu': ssm_b_glu,
            'att_out_g': att_out_g, 'ssm_out_g': ssm_out_g, 'w_out': w_out,
            'norm_mem_g': norm_mem_g, 'norm_memkv_g': norm_memkv_g,
            'w_mem_q': w_mem_q, 'w_mem_k': w_mem_k, 'w_mem_v': w_mem_v,
            'mem_q_g': mem_q_g, 'mem_k_g': mem_k_g, 'w_mem_o': w_mem_o,
            'norm_peer_g': norm_peer_g, 'w_peer_q': w_peer_q, 'peer_keys': peer_keys,
            'peer_u': peer_u, 'peer_v': peer_v}


def reference(x, mem, norm_mix_g, w_in, att_q_g, att_k_g, rel_bias,
              ssm_lam_re, ssm_lam_im, ssm_log_step, ssm_b_re, ssm_b_im, ssm_c_re, ssm_c_im,
              ssm_d, ssm_w_glu, ssm_b_glu, att_out_g, ssm_out_g, w_out,
              norm_mem_g, norm_memkv_g, w_mem_q, w_mem_k, w_mem_v, mem_q_g, mem_k_g, w_mem_o,
              norm_peer_g, w_peer_q, peer_keys, peer_u, peer_v):
    bsz, seq = x.shape[0], x.shape[1]
    h = x
    for l in range(DEPTH):
        proj = _rmsnorm(h, norm_mix_g[l]) @ w_in[l]
        q = proj[..., :ATT_WIDTH].reshape(bsz, seq, ATT_HEADS, ATT_HEAD_DIM)
        k = proj[..., ATT_WIDTH:2 * ATT_WIDTH].reshape(bsz, seq, ATT_HEADS, ATT_HEAD_DIM)
        v = proj[..., 2 * ATT_WIDTH:3 * ATT_WIDTH].reshape(bsz, seq, ATT_HEADS, ATT_HEAD_DIM)
        u = proj[..., 3 * ATT_WIDTH:]
        att = _chunked_attention(_rmsnorm(q, att_q_g[l]), _rmsnorm(k, att_k_g[l]), v, rel_bias[l])
        ssm = _s5_mixer(u, ssm_lam_re[l], ssm_lam_im[l], ssm_log_step[l], ssm_b_re[l], ssm_b_im[l],
                        ssm_c_re[l], ssm_c_im[l], ssm_d[l], ssm_w_glu[l], ssm_b_glu[l])
        mixed = jnp.concatenate([_rmsnorm(att, att_out_g[l]), _rmsnorm(ssm, ssm_out_g[l])], axis=-1)
        h = h + mixed @ w_out[l]
        h = h + _memory_attention(_rmsnorm(h, norm_mem_g[l]), _rmsnorm(mem, norm_memkv_g[l]),
                                  w_mem_q[l], w_mem_k[l], w_mem_v[l], mem_q_g[l], mem_k_g[l], w_mem_o[l])
        h = h + _peer_ffn(_rmsnorm(h, norm_peer_g[l]), w_peer_q[l], peer_keys[l], peer_u[l], peer_v[l])
    return h
```

```python
from contextlib import ExitStack, contextmanager
import numpy as np
import concourse.bass as bass
import concourse.mybir as mybir
from concourse.bass_utils import run_bass_kernel_spmd

F32 = mybir.dt.float32
BF16 = mybir.dt.bfloat16
I32 = mybir.dt.int32
AF = mybir.ActivationFunctionType
ALU = mybir.AluOpType
AX = mybir.AxisListType

ENGS = ("pe", "act", "dve", "pool", "sp")
TWO_PI = 6.283185307179586
EPS = 1e-6
NEG = -30000.0


class Sched:
    def __init__(self, nc, es, n_dma_sems=32):
        self.nc = nc
        self.streams = {e: [] for e in ENGS}
        self.sem = {e: es.enter_context(nc.semaphore("s_" + e)) for e in ("pe", "act", "dve", "pool")}
        self.cnt = {e: 0 for e in ("pe", "act", "dve", "pool")}
        self.dsem = [es.enter_context(nc.semaphore("s_dma%d" % i)) for i in range(n_dma_sems)]
        self.dcnt = [0] * n_dma_sems
        self.dnext = 0
        self.n_sw = 4
        self.dnext_sw = 0
        self.waited = {}
        self.last_w = {}
        self.readers = {}
        self.n_ops = 0

    def _deps(self, eng, reads, writes):
        deps = []
        for k in reads:
            if k in self.last_w:
                deps.append(self.last_w[k])
        for k in writes:
            if k in self.last_w:
                deps.append(self.last_w[k])
            deps.extend(self.readers.get(k, ()))
        need = {}
        for (sk, val, peng) in deps:
            if peng == "pe" and eng == "pe":
                continue
            if self.waited.get((eng, sk), 0) >= val:
                continue
            if need.get(sk, 0) < val:
                need[sk] = val
        return need

    def _semobj(self, sk):
        return self.sem[sk] if isinstance(sk, str) else self.dsem[sk]

    def _emit_waits(self, eng, need):
        for sk, val in need.items():
            self.waited[(eng, sk)] = val
            so = self._semobj(sk)
            self.streams[eng].append(lambda e, so=so, val=val: e.wait_ge(so, val))

    def _record(self, tok, reads, writes):
        for k in writes:
            self.last_w[k] = tok
            self.readers[k] = []
        for k in reads:
            if k not in writes:
                self.readers.setdefault(k, []).append(tok)

    def op(self, eng, fn, reads=(), writes=()):
        need = self._deps(eng, reads, writes)
        self._emit_waits(eng, need)
        self.cnt[eng] += 1
        val = self.cnt[eng]
        so = self.sem[eng]
        self.streams[eng].append(lambda e, fn=fn, so=so: fn(e).then_inc(so, 1))
        self._record((eng, val, eng), reads, writes)
        self.n_ops += 1

    def dma(self, q, out, in_, reads=(), writes=(), **kw):
        nhw = len(self.dsem) - self.n_sw
        if q == "pool":
            i = nhw + self.dnext_sw
            self.dnext_sw = (self.dnext_sw + 1) % self.n_sw
        else:
            i = self.dnext
            self.dnext = (self.dnext + 1) % nhw
        need = self._deps(q, reads, writes)
        prev = 16 * self.dcnt[i]
        if prev and self.waited.get((q, i), 0) < prev:
            need[i] = max(need.get(i, 0), prev)
        self._emit_waits(q, need)
        self.dcnt[i] += 1
        val = 16 * self.dcnt[i]
        so = self.dsem[i]
        self.streams[q].append(
            lambda e, out=out, in_=in_, so=so, kw=kw: e.dma_start(out=out, in_=in_, **kw).then_inc(so, 16))
        self._record((i, val, "dma"), reads, writes)
        self.n_ops += 1

    def barrier(self):
        for eng in ENGS:
            need = {}
            for pe_ in ("pe", "act", "dve", "pool"):
                v = self.cnt[pe_]
                if v and self.waited.get((eng, pe_), 0) < v:
                    need[pe_] = v
            for i, c in enumerate(self.dcnt):
                if c and self.waited.get((eng, i), 0) < 16 * c:
                    need[i] = 16 * c
            self._emit_waits(eng, need)

    def wait_all(self, eng, keys):
        need = {}
        for k in keys:
            if k in self.last_w:
                sk, val, _ = self.last_w[k]
                if self.waited.get((eng, sk), 0) < val and need.get(sk, 0) < val:
                    need[sk] = val
        self._emit_waits(eng, need)

    def emit(self):
        if not any(self.streams[e] for e in ENGS):
            return
        streams = self.streams
        self.streams = {e: [] for e in ENGS}
        self._emit_block(streams)

    def _emit_block(self, streams):
        self_streams = streams
        with self.nc.Block() as block:
            @block.tensor
            def _(e):
                for f in self_streams["pe"]:
                    f(e)

            @block.scalar
            def _(e):
                for f in self_streams["act"]:
                    f(e)

            @block.vector
            def _(e):
                for f in self_streams["dve"]:
                    f(e)

            @block.gpsimd
            def _(e):
                for f in self_streams["pool"]:
                    f(e)

            @block.sync
            def _(e):
                for f in self_streams["sp"]:
                    f(e)


class Ctx:
    def __init__(self, nc, S):
        self.nc = nc
        self.S = S
        self.uid = 0

    def sb(self, es, name, shape, dt=F32):
        self.uid += 1
        return es.enter_context(self.nc.sbuf_tensor("%s_%d" % (name, self.uid), list(shape), dt))

    def ps(self, es, name, shape, dt=F32):
        self.uid += 1
        return es.enter_context(self.nc.psum_tensor("%s_%d" % (name, self.uid), list(shape), dt))


@contextmanager
def scope(C):
    with ExitStack() as es:
        yield es
        C.S.barrier()
        C.S.emit()


def bc(ap, shape):
    return ap.to_broadcast(list(shape))


def make_ident(C, es, name="ident"):
    S = C.S
    idf = C.sb(es, name + "f", [128, 128])
    idb = C.sb(es, name + "b", [128, 128], BF16)
    S.op("pool", lambda e: e.memset(idf[:], 1.0), writes=[name + "f"])
    S.op("pool", lambda e: e.affine_select(out=idf[:], in_=idf[:], pattern=[[-1, 128]], compare_op=ALU.is_equal,
                                           fill=0.0, base=0, channel_multiplier=1), reads=[name + "f"], writes=[name + "f"])
    S.op("dve", lambda e: e.tensor_copy(out=idb[:], in_=idf[:]), reads=[name + "f"], writes=[name + "b"])
    return idf, idb


def sincos(C, es, ang, n, tag):
    S = C.S
    outs = []
    for which, off in (("s", 64.0), ("c", 64.25)):
        k = tag + which
        y = C.sb(es, k + "y", [128, n]); yi = C.sb(es, k + "yi", [128, n], I32); yf = C.sb(es, k + "yf", [128, n])
        m = C.sb(es, k + "m", [128, n]); o = C.sb(es, k + "o", [128, n])
        S.op("dve", lambda e, y=y, off=off: e.tensor_scalar(out=y[:], in0=ang, scalar1=1.0 / TWO_PI, scalar2=off, op0=ALU.mult, op1=ALU.add),
             reads=[tag + "ang"], writes=[k + "y"])
        S.op("dve", lambda e, y=y, yi=yi: e.tensor_copy(out=yi[:], in_=y[:]), reads=[k + "y"], writes=[k + "yi"])
        S.op("dve", lambda e, yi=yi, yf=yf: e.tensor_copy(out=yf[:], in_=yi[:]), reads=[k + "yi"], writes=[k + "yf"])
        S.op("dve", lambda e, y=y, yf=yf: e.tensor_tensor(out=y[:], in0=y[:], in1=yf[:], op=ALU.subtract), reads=[k + "y", k + "yf"], writes=[k + "y"])
        S.op("dve", lambda e, y=y, m=m: e.tensor_scalar(out=m[:], in0=y[:], scalar1=0.5, scalar2=None, op0=ALU.is_gt), reads=[k + "y"], writes=[k + "m"])
        S.op("dve", lambda e, y=y, m=m: e.tensor_tensor(out=y[:], in0=y[:], in1=m[:], op=ALU.subtract), reads=[k + "y", k + "m"], writes=[k + "y"])
        S.op("act", lambda e, y=y, o=o: e.activation(out=o[:], in_=y[:], func=AF.Sin, scale=TWO_PI), reads=[k + "y"], writes=[k + "o"])
        outs.append((o, k + "o"))
    return outs


def s5_params(C, es_keep, D):
    S, nc = C.S, C.nc
    P = {}
    LT2 = C.sb(es_keep, "LT2", [128, 8, 8, 2, 128], BF16)
    WPr = C.sb(es_keep, "WPr", [128, 9, 16]); WPi = C.sb(es_keep, "WPi", [128, 9, 16]); WPn = C.sb(es_keep, "WPn", [128, 9, 16])
    P.update(LT2=LT2, WPr=WPr, WPi=WPi, WPn=WPn)
    with scope(C) as es:
        sb = lambda name, shape, dt=F32: C.sb(es, name, shape, dt)
        Mz2 = sb("Mz2", [128, 16, 8, 128], BF16)
        CA = sb("CA", [128, 32, 128], BF16); CAs = sb("CAs", [128, 32, 128], BF16)
        LR = sb("LR", [128, 32]); LI = sb("LI", [128, 32]); LS = sb("LS", [128, 32])
        SG = sb("SG", [128, 2]); NV = sb("NV", [128, 23])
        P1B = sb("P1B", [128, 32, 16]); P2B = sb("P2B", [128, 32, 16]); P1C = sb("P1C", [128, 32, 16]); P2C = sb("P2C", [128, 32, 16])
        DD = sb("DD", [128, 16, 16])
        for t, nm in ((LR, "s5_lr"), (LI, "s5_li"), (LS, "s5_ls"), (SG, "s5_sg"), (NV, "s5_nv")):
            S.dma("sp", t[:], D[nm], writes=[nm])
        S.dma("sp", DD[:].rearrange("p e c -> p (e c)"), D["s5_dd"], writes=["s5_dd"])
        for t, nm in ((P1B, "s5_p1b"), (P2B, "s5_p2b"), (P1C, "s5_p1c"), (P2C, "s5_p2c")):
            S.dma("sp", t[:].rearrange("p g c -> p (g c)"), D[nm], writes=[nm])
        idf, idb = make_ident(C, es, "pid")
        STEP = sb("STEP", [128, 32]); AA = sb("AA", [128, 32]); PH = sb("PH", [128, 32])
        S.op("act", lambda e: e.activation(out=STEP[:], in_=LS[:], func=AF.Exp), reads=["s5_ls"], writes=["STEP"])
        S.op("dve", lambda e: e.tensor_tensor(out=AA[:], in0=LR[:], in1=STEP[:], op=ALU.mult), reads=["s5_lr", "STEP"], writes=["AA"])
        S.op("dve", lambda e: e.tensor_tensor(out=PH[:], in0=LI[:], in1=STEP[:], op=ALU.mult), reads=["s5_li", "STEP"], writes=["PH"])
        EXPO = sb("EXPO", [128, 32, 23]); ANG = sb("ANG", [128, 32, 23]); MAG = sb("MAG", [128, 32, 23])
        nvb = bc(NV[:].unsqueeze(1), [128, 32, 23])
        S.op("dve", lambda e: e.tensor_tensor(out=EXPO[:], in0=bc(AA[:].unsqueeze(2), [128, 32, 23]), in1=nvb, op=ALU.mult), reads=["AA", "s5_nv"], writes=["EXPO"])
        S.op("dve", lambda e: e.tensor_tensor(out=ANG[:], in0=bc(PH[:].unsqueeze(2), [128, 32, 23]), in1=nvb, op=ALU.mult), reads=["PH", "s5_nv"], writes=["pwang"])
        S.op("act", lambda e: e.activation(out=MAG[:], in_=EXPO[:], func=AF.Exp), reads=["EXPO"], writes=["MAG"])
        (sn, snk), (cs, csk) = sincos(C, es, ANG[:].rearrange("p g n -> p (g n)"), 32 * 23, "pw")
        CR = sb("CR", [128, 32, 23]); CI = sb("CI", [128, 32, 23])
        S.op("dve", lambda e: e.tensor_tensor(out=CR[:].rearrange("p g n -> p (g n)"), in0=MAG[:].rearrange("p g n -> p (g n)"), in1=cs[:], op=ALU.mult), reads=["MAG", csk], writes=["CR"])
        S.op("dve", lambda e: e.tensor_tensor(out=CI[:].rearrange("p g n -> p (g n)"), in0=MAG[:].rearrange("p g n -> p (g n)"), in1=sn[:], op=ALU.mult), reads=["MAG", snk], writes=["CI"])
        zr = sb("zr", [128, 32]); den = sb("den", [128, 32]); t0 = sb("t0", [128, 32]); fr = sb("fr", [128, 32]); fi = sb("fi", [128, 32])
        S.op("dve", lambda e: e.tensor_scalar(out=zr[:], in0=CR[:, :, 8], scalar1=-1.0, scalar2=None, op0=ALU.add), reads=["CR"], writes=["zr"])
        S.op("dve", lambda e: e.tensor_tensor(out=den[:], in0=LR[:], in1=LR[:], op=ALU.mult), reads=["s5_lr"], writes=["den"])
        S.op("dve", lambda e: e.tensor_tensor(out=t0[:], in0=LI[:], in1=LI[:], op=ALU.mult), reads=["s5_li"], writes=["t0"])
        S.op("dve", lambda e: e.tensor_tensor(out=den[:], in0=den[:], in1=t0[:], op=ALU.add), reads=["den", "t0"], writes=["den"])
        S.op("dve", lambda e: e.reciprocal(out=den[:], in_=den[:]), reads=["den"], writes=["den"])
        S.op("dve", lambda e: e.tensor_tensor(out=fr[:], in0=zr[:], in1=LR[:], op=ALU.mult), reads=["zr", "s5_lr"], writes=["fr"])
        S.op("dve", lambda e: e.tensor_tensor(out=t0[:], in0=CI[:, :, 8], in1=LI[:], op=ALU.mult), reads=["CI", "s5_li", "den"], writes=["t0"])
        S.op("dve", lambda e: e.tensor_tensor(out=fr[:], in0=fr[:], in1=t0[:], op=ALU.add), reads=["fr", "t0"], writes=["fr"])
        S.op("dve", lambda e: e.tensor_tensor(out=fr[:], in0=fr[:], in1=den[:], op=ALU.mult), reads=["fr", "den"], writes=["fr"])
        S.op("dve", lambda e: e.tensor_tensor(out=fi[:], in0=CI[:, :, 8], in1=LR[:], op=ALU.mult), reads=["CI", "s5_lr"], writes=["fi"])
        S.op("dve", lambda e: e.tensor_tensor(out=t0[:], in0=zr[:], in1=LI[:], op=ALU.mult), reads=["zr", "s5_li", "fr"], writes=["t0"])
        S.op("dve", lambda e: e.tensor_tensor(out=fi[:], in0=fi[:], in1=t0[:], op=ALU.subtract), reads=["fi", "t0"], writes=["fi"])
        S.op("dve", lambda e: e.tensor_tensor(out=fi[:], in0=fi[:], in1=den[:], op=ALU.mult), reads=["fi", "den"], writes=["fi"])
        BB1 = sb("BB1", [128, 32, 16]); BB2 = sb("BB2", [128, 32, 16]); ta = sb("ta", [128, 32, 16]); tb = sb("tb", [128, 32, 16])
        frb = bc(fr[:].unsqueeze(2), [128, 32, 16]); fib = bc(fi[:].unsqueeze(2), [128, 32, 16])
        fl = lambda t: t[:].rearrange("p g c -> p (g c)")
        S.op("dve", lambda e: e.tensor_tensor(out=ta[:], in0=P1B[:], in1=frb, op=ALU.mult), reads=["s5_p1b", "fr"], writes=["ta"])
        S.op("dve", lambda e: e.tensor_tensor(out=tb[:], in0=P2B[:], in1=fib, op=ALU.mult), reads=["s5_p2b", "fi"], writes=["tb"])
        S.op("dve", lambda e: e.scalar_tensor_tensor(out=fl(BB1), in0=fl(tb), scalar=SG[:, 0:1], in1=fl(ta), op0=ALU.mult, op1=ALU.add), reads=["ta", "tb", "s5_sg"], writes=["BB1"])
        S.op("dve", lambda e: e.tensor_tensor(out=ta[:], in0=P2B[:], in1=frb, op=ALU.mult), reads=["s5_p2b", "fr", "BB1"], writes=["ta"])
        S.op("dve", lambda e: e.tensor_tensor(out=tb[:], in0=P1B[:], in1=fib, op=ALU.mult), reads=["s5_p1b", "fi", "BB1"], writes=["tb"])
        S.op("dve", lambda e: e.scalar_tensor_tensor(out=fl(BB2), in0=fl(tb), scalar=SG[:, 1:2], in1=fl(ta), op0=ALU.mult, op1=ALU.add), reads=["ta", "tb", "s5_sg"], writes=["BB2"])
        t5 = sb("t5", [128, 32, 16]); t6 = sb("t6", [128, 32, 16])
        Rm = sb("Rm", [128, 32, 8, 16])
        Q1 = sb("Q1", [128, 32, 16]); Q2 = sb("Q2", [128, 32, 16])
        S.op("dve", lambda e: e.tensor_scalar(out=fl(Q1), in0=fl(P1C), scalar1=SG[:, 1:2], scalar2=None, op0=ALU.mult), reads=["s5_p1c", "s5_sg"], writes=["Q1"])
        S.op("dve", lambda e: e.tensor_scalar(out=fl(Q2), in0=fl(P2C), scalar1=SG[:, 0:1], scalar2=None, op0=ALU.mult), reads=["s5_p2c", "s5_sg"], writes=["Q2"])
        CAv = CA[:].rearrange("p g (t c) -> p g t c", c=16); CAsv = CAs[:].rearrange("p g (t c) -> p g t c", c=16)
        for t in range(8):
            for (dst, dkey, a_, akey, b_, bkey, idx) in ((Rm[:, :, t, :], "Rm", Q1, "Q1", P2C, "s5_p2c", 7 + t),
                                                         (CAv[:, :, t, :], "CA", Q1, "Q1", P2C, "s5_p2c", 15 + t),
                                                         (CAsv[:, :, t, :], "CAs", Q2, "Q2", P1C, "s5_p1c", 15 + t)):
                S.op("dve", lambda e, a_=a_, idx=idx: e.tensor_tensor(out=t5[:], in0=a_[:], in1=bc(CR[:, :, idx:idx + 1], [128, 32, 16]), op=ALU.mult), reads=[akey, "CR"], writes=["t5"])
                S.op("dve", lambda e, b_=b_, idx=idx: e.tensor_tensor(out=t6[:], in0=b_[:], in1=bc(CI[:, :, idx:idx + 1], [128, 32, 16]), op=ALU.mult), reads=[bkey, "CI"], writes=["t6"])
                S.op("dve", lambda e, dst=dst: e.tensor_tensor(out=dst, in0=t5[:], in1=t6[:], op=ALU.subtract), reads=["t5", "t6"], writes=[dkey])
        Wr = sb("Wr", [128, 9, 32]); Wi = sb("Wi", [128, 9, 32]); sq = sb("sq", [128, 32])
        S.op("dve", lambda e: e.tensor_copy(out=Wr[:, 0, :], in_=CR[:, :, 15]), reads=["CR"], writes=["Wr"])
        S.op("dve", lambda e: e.tensor_copy(out=Wi[:, 0, :], in_=CI[:, :, 15]), reads=["CI"], writes=["Wi"])
        for j in range(8):
            S.op("dve", lambda e, j=j: e.tensor_tensor(out=Wr[:, j + 1, :], in0=Wr[:, j, :], in1=Wr[:, j, :], op=ALU.mult), reads=["Wr"], writes=["Wr"])
            S.op("dve", lambda e, j=j: e.tensor_tensor(out=sq[:], in0=Wi[:, j, :], in1=Wi[:, j, :], op=ALU.mult), reads=["Wi"], writes=["sq"])
            S.op("dve", lambda e, j=j: e.tensor_tensor(out=Wr[:, j + 1, :], in0=Wr[:, j + 1, :], in1=sq[:], op=ALU.subtract), reads=["Wr", "sq"], writes=["Wr"])
            S.op("dve", lambda e, j=j: e.scalar_tensor_tensor(out=Wi[:, j + 1, :], in0=Wr[:, j, :], scalar=2.0, in1=Wi[:, j, :], op0=ALU.mult, op1=ALU.mult), reads=["Wr", "Wi"], writes=["Wi"])
        Wrv = Wr[:].rearrange("p j (a r w) -> p j a r w", r=2, w=2); Wiv = Wi[:].rearrange("p j (a r w) -> p j a r w", r=2, w=2)
        for r in range(2):
            pr_ = slice(64 * r, 64 * r + 64)
            for j in range(9):
                S.op("dve", lambda e, r=r, pr_=pr_, j=j: e.tensor_copy(out=WPr[pr_, j, :].rearrange("p (a w) -> p a w", w=2), in_=Wrv[pr_, j, :, r, :]), reads=["Wr"], writes=["WPr"])
                S.op("dve", lambda e, r=r, pr_=pr_, j=j: e.tensor_copy(out=WPi[pr_, j, :].rearrange("p (a w) -> p a w", w=2), in_=Wiv[pr_, j, :, r, :]), reads=["Wi"], writes=["WPi"])
        S.op("dve", lambda e: e.tensor_scalar(out=WPn[:].rearrange("p j q -> p (j q)"), in0=WPi[:].rearrange("p j q -> p (j q)"), scalar1=-1.0, scalar2=None, op0=ALU.mult), reads=["WPi"], writes=["WPn"])
        E = [[sb("E%d%d" % (h_, r), [128, 128]) for r in range(2)] for h_ in range(2)]
        for h_ in range(2):
            for r in range(2):
                S.op("pool", lambda e, h_=h_, r=r: e.memset(E[h_][r][:], 0.0), writes=["E%d%d" % (h_, r)])
                S.op("pool", lambda e, h_=h_, r=r: e.tensor_copy(out=E[h_][r][64 * h_:64 * h_ + 64, 64 * r:64 * r + 64], in_=idf[64 * h_:64 * h_ + 64, 64 * h_:64 * h_ + 64]),
                     reads=["pidf", "E%d%d" % (h_, r)], writes=["E%d%d" % (h_, r)])
        Lz = [sb("Lz%d" % i, [128, 32, 64]) for i in range(2)]
        for i in range(2):
            S.op("pool", lambda e, i=i: e.memset(Lz[i][:].rearrange("p g c -> p (g c)"), 0.0), writes=["Lz%d" % i])
        S.op("pool", lambda e: e.memset(Mz2[:].rearrange("p a b c -> p (a b c)"), 0.0), writes=["Mz2"])
        t5v = t5[:].rearrange("p (a j) c -> p a j c", j=4); t6v = t6[:].rearrange("p (a j) c -> p a j c", j=4)
        with scope(C) as esp:
            PM = C.ps(esp, "PM", [128, 16, 128]); PL = C.ps(esp, "PL", [128, 8, 2, 128])
            for s in range(8):
                i = 7 - s; sl = s % 2; lk = "Lz%d" % sl
                Lzv = Lz[sl][:].rearrange("p (a j) c -> p a j c", j=4)
                S.op("dve", lambda e, i=i: e.tensor_tensor(out=t5[:], in0=BB1[:], in1=bc(CR[:, :, i:i + 1], [128, 32, 16]), op=ALU.mult), reads=["BB1", "CR"], writes=["t5"])
                S.op("dve", lambda e, i=i: e.tensor_tensor(out=t6[:], in0=BB2[:], in1=bc(CI[:, :, i:i + 1], [128, 32, 16]), op=ALU.mult), reads=["BB2", "CI"], writes=["t6"])
                for j4 in range(4):
                    S.op("dve", lambda e, j4=j4, Lzv=Lzv: e.scalar_tensor_tensor(out=Lzv[:, :, j4, 16 * j4:16 * j4 + 16], in0=t6v[:, :, j4, :], scalar=SG[:, 0:1], in1=t5v[:, :, j4, :], op0=ALU.mult, op1=ALU.add),
                         reads=["t5", "t6", "s5_sg"], writes=[lk])
                for g in range(32):
                    chc = g // 8; hb = (g % 8) // 4; j4 = g % 4; e_ = chc * 4 + j4
                    rows = slice(64 * hb, 64 * hb + 64)
                    S.op("pe", lambda e, g=g, sl=sl, e_=e_, rows=rows: e.matmul(PM[rows, e_, :], lhsT=Lz[sl][:, g, :], rhs=Rm[:, g, :, :].rearrange("p t c -> p (t c)"), start=True, stop=True),
                         reads=[lk, "Rm"], writes=["PM"])
                for pr in range(16):
                    a_ = pr // 2; wp = pr % 2; chc = a_ // 2; hb = a_ % 2; e2 = chc * 2 + wp
                    rows = slice(64 * hb, 64 * hb + 64)
                    for h_ in range(2):
                        for r in range(2):
                            g = 4 * a_ + 2 * r + wp
                            S.op("pe", lambda e, g=g, sl=sl, e2=e2, rows=rows, h_=h_, r=r: e.matmul(PL[rows, e2, h_, :], lhsT=Lz[sl][:, g, :], rhs=E[h_][r][:], start=(r == 0), stop=(r == 1)),
                                 reads=[lk, "E%d%d" % (h_, r)], writes=["PL"])
                S.op("dve", lambda e, s=s: e.tensor_copy(out=Mz2[:, :, s, 16 * s:128], in_=PM[:, :, 16 * s:128]), reads=["PM"], writes=["Mz2"])
                S.op("dve", lambda e, s=s: e.tensor_tensor(out=Mz2[:, :, s, 16 * s:16 * s + 16], in0=PM[:, :, 16 * s:16 * s + 16], in1=DD[:], op=ALU.add), reads=["PM", "s5_dd"], writes=["Mz2"])
                S.op("act", lambda e, s=s: e.copy(out=LT2[:, :, s, :, :], in_=PL[:]), reads=["PL"], writes=["LT2"])
        S.dma("sp", D["Mz_d"], Mz2[:].rearrange("p a b c -> p (a b c)"), reads=["Mz2"], writes=["Mz_d"])
        S.dma("sp", D["CA_d"][:, 0, :], CA[:].rearrange("p g c -> p (g c)"), reads=["CA"], writes=["CA_d"])
        S.dma("sp", D["CA_d"][:, 1, :], CAs[:].rearrange("p g c -> p (g c)"), reads=["CAs"], writes=["CA_d"])
    return P


def load_weight_bf16(C, es, es_tmp, name, src, rows_chunks, ncols, gcol=None, q="sp"):
    S = C.S
    W = C.sb(es, name, [128, rows_chunks, ncols], BF16)
    stg = [C.sb(es_tmp, name + "_stg%d" % i, [128, ncols]) for i in range(2)]
    srcv = src.rearrange("(c p) n -> c p n", p=128)
    for c in range(rows_chunks):
        st = stg[c % 2]; sk = name + "_stg%d" % (c % 2)
        S.dma(q, st[:], srcv[c], writes=[sk])
        eng = "dve" if c % 2 == 0 else "pool"
        if gcol is not None:
            S.op(eng, lambda e, st=st, c=c: e.tensor_scalar(out=W[:, c, :], in0=st[:], scalar1=gcol[0][:, c:c + 1], scalar2=None, op0=ALU.mult),
                 reads=[sk, gcol[1]], writes=[name])
        else:
            S.op(eng, lambda e, st=st, c=c: e.tensor_copy(out=W[:, c, :], in_=st[:]), reads=[sk], writes=[name])
    return W


def rms_rstd(C, x_ap, xkey, junk, jkey, ss, rs, skey, n):
    S = C.S
    S.op("act", lambda e: e.activation(out=junk, in_=x_ap, func=AF.Square, accum_out=ss), reads=[xkey], writes=[skey + "_ss"])
    S.op("act", lambda e: e.activation(out=rs, in_=ss, func=AF.Sqrt, scale=1.0 / n, bias=EPS), reads=[skey + "_ss"], writes=[skey + "_sq"])
    S.op("dve", lambda e: e.reciprocal(out=rs, in_=rs), reads=[skey + "_sq"], writes=[skey])


def transpose_chunks(C, src, skey, nch, pbank, pkey, dst, dkey, idb, evac="act"):
    S = C.S
    for c in range(nch):
        S.op("pe", lambda e, c=c: e.transpose(out=pbank[:, c, :], in_=src[:, c * 128:(c + 1) * 128], identity=idb[:]), reads=[skey, "identb"], writes=[pkey])
    if evac == "act":
        S.op("act", lambda e: e.copy(out=dst, in_=pbank), reads=[pkey], writes=[dkey])
    else:
        S.op(evac, lambda e: e.tensor_copy(out=dst, in_=pbank), reads=[pkey], writes=[dkey])


def stage_mixer(C, D, dbg=None, upto=9, prep=None):
    S, nc = C.S, C.nc
    dbg = dbg or {}
    with scope(C) as es1:
        P = s5_params(C, es1, D)
        idf = C.sb(es1, "identf", [128, 128]); idb = C.sb(es1, "identb", [128, 128], BF16)
        S.op("pool", lambda e: e.memset(idf[:], 1.0), writes=["identf"])
        S.op("pool", lambda e: e.affine_select(out=idf[:], in_=idf[:], pattern=[[-1, 128]], compare_op=ALU.is_equal, fill=0.0, base=0, channel_multiplier=1), reads=["identf"], writes=["identf"])
        S.op("dve", lambda e: e.tensor_copy(out=idb[:], in_=idf[:]), reads=["identf"], writes=["identb"])
        if "WPr" in dbg:
            for nm in ("WPr", "WPi"):
                S.dma("sp", dbg[nm], P[nm][:].rearrange("p a b -> p (a b)"), reads=[nm], writes=["o_" + nm])
            S.dma("pool", dbg["LT2"], P["LT2"][:].rearrange("p a b c d -> p (a b c d)"), reads=["LT2"], writes=["o_LT2"])
            S.dma("pool", dbg["Mz"], D["Mz_d"], reads=["Mz_d"], writes=["o_Mz"])
            S.dma("pool", dbg["CA"], D["CA_d"].rearrange("p a b -> p (a b)"), reads=["CA_d"], writes=["o_CA"])
        if upto < 1:
            return
        carry_r = C.sb(es1, "carry_r", [128, 16]); carry_i = C.sb(es1, "carry_i", [128, 16])
        S.op("pool", lambda e: e.memset(carry_r[:], 0.0), writes=["carry_r"])
        S.op("pool", lambda e: e.memset(carry_i[:], 0.0), writes=["carry_i"])
        TRE = C.sb(es1, "TRE", [128, 16, 257]); TIM = C.sb(es1, "TIM", [128, 16, 257])
        uT = C.sb(es1, "uT", [128, 4, 8, 256], BF16)
        with scope(C) as es2:
            _mixer_passes(C, es2, D, P, idb, carry_r, carry_i, TRE, TIM, uT, dbg)
        if "carry" in dbg:
            S.dma("sp", dbg["carry"][:, 0:16], carry_r[:], reads=["carry_r"], writes=["o_carry"])
            S.dma("sp", dbg["carry"][:, 16:32], carry_i[:], reads=["carry_i"], writes=["o_carry2"])
        if upto < 2:
            return
        with scope(C) as es3:
            ytm = C.sb(es3, "ytm", [128, 2, 8, 512], BF16)
            with scope(C) as es4:
                _s5_scan_out(C, es4, D, P, idb, TRE, TIM, uT, ytm, carry_r, carry_i, dbg)
            if upto < 3:
                return
            _s5_glu_out(C, es3, D, idb, ytm, dbg)
    if upto < 4:
        return
    with scope(C) as es5:
        idf = C.sb(es5, "identf", [128, 128]); idb = C.sb(es5, "identb", [128, 128], BF16)
        S.op("pool", lambda e: e.memset(idf[:], 1.0), reads=[], writes=["identf"])
        S.op("pool", lambda e: e.affine_select(out=idf[:], in_=idf[:], pattern=[[-1, 128]], compare_op=ALU.is_equal, fill=0.0, base=0, channel_multiplier=1), reads=["identf"], writes=["identf"])
        S.op("dve", lambda e: e.tensor_copy(out=idb[:], in_=idf[:]), reads=["identf"], writes=["identb"])
        _attention(C, es5, D, idb, dbg, prep)


def _mixer_passes(C, es, D, P, idb, carry_r, carry_i, TRE, TIM, uT, dbg):
    S = C.S
    sb = lambda name, shape, dt=F32: C.sb(es, name, shape, dt)
    gin = sb("gin", [128, 8])
    S.dma("sp", gin[:], D["g_mix"], writes=["gin"])
    with scope(C) as est:
        Wb = load_weight_bf16(C, es, est, "Wb", D["w_in"], 8, 2048, gcol=(gin, "gin"))
    gq = sb("gq", [128, 64]); gk = sb("gk", [128, 64]); hv = sb("hv", [128, 4])
    S.dma("sp", gq[:], D["att_q_g"].partition_broadcast(128), writes=["gq"])
    S.dma("sp", gk[:], D["att_k_g"].partition_broadcast(128), writes=["gk"])
    S.dma("sp", hv[:], D["hvalid"], writes=["hv"])
    S.op("dve", lambda e: e.tensor_scalar(out=gq[:], in0=gq[:], scalar1=0.125, scalar2=None, op0=ALU.mult), reads=["gq"], writes=["gq"])
    xt = [sb("xt%d" % i, [128, 1024]) for i in range(2)]
    junk = sb("junk", [128, 1024]); st = [sb("st%d" % i, [128, 4]) for i in range(2)]
    xn = [sb("xn%d" % i, [128, 1024], BF16) for i in range(2)]
    xnT = [sb("xnT%d" % i, [128, 8, 512], BF16) for i in range(2)]
    qkv = sb("qkv", [128, 3, 512]); sq = sb("sq2", [128, 512]); qst = sb("qst", [128, 4, 8])
    qn = sb("qn", [128, 2, 512], BF16)
    kTs = [sb("kTs%d" % i, [128, 4, 128], BF16) for i in range(2)]; qTs = [sb("qTs%d" % i, [128, 4, 128], BF16) for i in range(2)]
    Vs = [sb("Vs%d" % i, [128, 8, 65], BF16) for i in range(2)]
    tA = [(sb("tAr%d" % i, [128, 256]), sb("tAi%d" % i, [128, 256])) for i in range(2)]
    tB = [(sb("tBr%d" % i, [128, 128]), sb("tBi%d" % i, [128, 128])) for i in range(2)]
    REDr = sb("REDr", [128, 16]); REDi = sb("REDi", [128, 16]); c1 = sb("c1", [128, 16]); c2 = sb("c2", [128, 16]); c3 = sb("c3", [128, 16])
    with scope(C) as esp:
        bank = [C.ps(esp, "bk%d" % i, [128, 512]) for i in range(8)]
        xv = D["x_ext"].rearrange("(n p) d -> n p d", p=128)
        tile_ctr = 0
        for q in range(4):
            for blk in range(4):
                bslot = (q * 4 + blk) % 2
                xT = xnT[bslot]; xTk = "xnT%d" % bslot
                for tt in range(4):
                    n_tile = q * 16 + blk * 4 + tt
                    sl = tile_ctr % 2; tile_ctr += 1
                    x_ = xt[sl]; xk = "xt%d" % sl
                    S.dma("sp", x_[:], xv[n_tile], writes=[xk])
                    rms_rstd(C, x_[:], xk, junk[:], "junk", st[sl][:, 0:1], st[sl][:, 1:2], "st%d" % sl, 1024)
                    S.op("dve", lambda e, x_=x_, sl=sl: e.tensor_scalar(out=xn[sl][:], in0=x_[:], scalar1=st[sl][:, 1:2], scalar2=None, op0=ALU.mult),
                         reads=[xk, "st%d" % sl], writes=["xn%d" % sl])
                    pb = bank[0][:].bitcast(BF16).rearrange("p (c n) -> p c n", n=128)
                    transpose_chunks(C, xn[sl][:], "xn%d" % sl, 8, pb, "bk0", xT[:, :, tt * 128:(tt + 1) * 128], xTk, idb)
                for chc in range(4):
                    bi = 1 + (chc % 2); bkk = "bk%d" % bi
                    for dc in range(8):
                        S.op("pe", lambda e, chc=chc, dc=dc, bi=bi, xT=xT: e.matmul(bank[bi][:], lhsT=Wb[:, dc, 1536 + chc * 128:1536 + (chc + 1) * 128], rhs=xT[:, dc, :], start=(dc == 0), stop=(dc == 7)),
                             reads=["Wb", xTk], writes=[bkk])
                    eng = "act" if chc % 2 == 0 else "dve"
                    src = bank[bi][:].rearrange("p (k s) -> p s k", s=8)
                    dst = uT[:, chc, :, blk * 64:(blk + 1) * 64]
                    if eng == "act":
                        S.op("act", lambda e, src=src, dst=dst: e.copy(out=dst, in_=src), reads=[bkk], writes=["uT"])
                    else:
                        S.op("dve", lambda e, src=src, dst=dst: e.tensor_copy(out=dst, in_=src), reads=[bkk], writes=["uT"])
                need_kv = (q == 3) or (q == 2 and blk == 3)
                need_q = (q == 3)
                if need_kv:
                    for tt in range(4):
                        n_tile = q * 16 + blk * 4 + tt
                        kvt = n_tile - 44
                        sl = kvt % 2
                        projs = [(1, 512, 3), (2, 1024, 4)] + ([(0, 0, 5)] if need_q else [])
                        for (pi, c0, bi) in projs:
                            for dc in range(8):
                                S.op("pe", lambda e, dc=dc, bi=bi, c0=c0, tt=tt, xT=xT: e.matmul(bank[bi][:], lhsT=xT[:, dc, tt * 128:(tt + 1) * 128], rhs=Wb[:, dc, c0:c0 + 512], start=(dc == 0), stop=(dc == 7)),
                                     reads=["Wb", xTk], writes=["bk%d" % bi])
                        S.op("act", lambda e, sl=sl: e.copy(out=Vs[sl][:, :, 0:64], in_=bank[4][:].rearrange("p (h d) -> p h d", d=64)), reads=["bk4"], writes=["Vs%d" % sl])
                        if kvt < 4:
                            S.op("pool", lambda e, sl=sl, kvt=kvt: e.tensor_copy(out=Vs[sl][:, :, 64], in_=bc(hv[:, kvt:kvt + 1], [128, 8])), reads=["hv"], writes=["Vs%d" % sl])
                        else:
                            S.op("pool", lambda e, sl=sl: e.memset(Vs[sl][:, :, 64], 1.0), reads=[], writes=["Vs%d" % sl])
                        S.dma("sp", D["V_d"][kvt], Vs[sl][:].rearrange("p h d -> p (h d)"), reads=["Vs%d" % sl], writes=["V_d"])
                        for (pi, bi, gt, gkey, dstT, dkey, dram, ncol_t) in ([(1, 3, gk, "gk", kTs[sl], "kTs%d" % sl, D["kT_d"], kvt)] +
                                                                          ([(0, 5, gq, "gq", qTs[sl], "qTs%d" % sl, D["qT_d"], kvt - 4)] if need_q else [])):
                            qs = qkv[:, pi, :]; qk_ = "qkv%d" % pi
                            S.op("act", lambda e, qs=qs, bi=bi: e.copy(out=qs, in_=bank[bi][:]), reads=["bk%d" % bi], writes=[qk_])
                            S.op("pool", lambda e, qs=qs: e.tensor_tensor(out=sq[:], in0=qs, in1=qs, op=ALU.mult), reads=[qk_], writes=["sq2"])
                            S.op("dve", lambda e, pi=pi: e.tensor_reduce(out=qst[:, pi, :], in_=sq[:].rearrange("p (h d) -> p h d", d=64), axis=AX.X, op=ALU.add), reads=["sq2"], writes=["qst%d" % pi])
                            S.op("act", lambda e, pi=pi: e.activation(out=qst[:, 2 + pi, :], in_=qst[:, pi, :], func=AF.Sqrt, scale=1.0 / 64, bias=EPS), reads=["qst%d" % pi], writes=["qsq%d" % pi])
                            S.op("dve", lambda e, pi=pi: e.reciprocal(out=qst[:, 2 + pi, :], in_=qst[:, 2 + pi, :]), reads=["qsq%d" % pi], writes=["qrs%d" % pi])
                            S.op("dve", lambda e, qs=qs, pi=pi: e.tensor_tensor(out=qs.rearrange("p (h d) -> p h d", d=64), in0=qs.rearrange("p (h d) -> p h d", d=64),
                                                                        in1=bc(qst[:, 2 + pi, :].unsqueeze(2), [128, 8, 64]), op=ALU.mult), reads=[qk_, "qrs%d" % pi], writes=[qk_])
                            S.op("pool", lambda e, qs=qs, pi=pi, gt=gt: e.tensor_tensor(out=qn[:, pi, :].rearrange("p (h d) -> p h d", d=64), in0=qs.rearrange("p (h d) -> p h d", d=64),
                                                                               in1=bc(gt[:].unsqueeze(1), [128, 8, 64]), op=ALU.mult), reads=[qk_, gkey], writes=["qn%d" % pi])
                            pb = bank[6][:].bitcast(BF16).rearrange("p (c n) -> p c n", n=128)[:, 0:4, :]
                            transpose_chunks(C, qn[:, pi, :], "qn%d" % pi, 4, pb, "bk6", dstT[:], dkey, idb)
                            S.dma("sp", dram.rearrange("p (c n) -> p c n", c=4)[:, :, ncol_t * 128:(ncol_t + 1) * 128], dstT[:], reads=[dkey], writes=["qkT_d"])
            for pair in range(16):
                psl = pair % 2
                br = bank[1 + 2 * psl]; bim = bank[2 + 2 * psl]; brk = "bk%d" % (1 + 2 * psl); bik = "bk%d" % (2 + 2 * psl)
                a_ = pair // 2; wp = pair % 2; chc = a_ // 2; hb = a_ % 2; e2 = chc * 2 + wp
                rows = slice(64 * hb, 64 * hb + 64)
                for half, (bkt, bkk) in enumerate(((br, brk), (bim, bik))):
                    for s in range(8):
                        S.op("pe", lambda e, e2=e2, s=s, half=half, rows=rows, chc=chc, bkt=bkt: e.matmul(bkt[:, 0:256], lhsT=P["LT2"][rows, e2, s, half, :], rhs=uT[rows, chc, s, :], start=(s == 0), stop=(s == 7)),
                             reads=["LT2", "uT"], writes=[bkk])
                if q < 3:
                    ar_, ai_ = tA[psl]; ark, aik = "tAr%d" % psl, "tAi%d" % psl
                    b_r, b_i = tB[psl]; brk2, bik2 = "tBr%d" % psl, "tBi%d" % psl
                    S.op("act", lambda e, ar_=ar_, br=br: e.copy(out=ar_[:], in_=br[:, 0:256]), reads=[brk], writes=[ark])
                    S.op("act", lambda e, ai_=ai_, bim=bim: e.copy(out=ai_[:], in_=bim[:, 0:256]), reads=[bik], writes=[aik])
                    src = (ar_, ai_, ark, aik); dst = (b_r, b_i, brk2, bik2)
                    for j in range(8):
                        n = 256 >> j; h = n // 2
                        sr, si, srk, sik = src; dr, di, drk, dik = dst
                        wr = P["WPr"][:, j, pair:pair + 1]; wi = P["WPi"][:, j, pair:pair + 1]; wn = P["WPn"][:, j, pair:pair + 1]
                        if j == 7:
                            odr, odi, odrk, odik = REDr[:, pair:pair + 1], REDi[:, pair:pair + 1], "REDr", "REDi"
                        else:
                            odr, odi, odrk, odik = dr[:, 0:h], di[:, 0:h], drk, dik
                        S.op("dve", lambda e, odr=odr, sr=sr, wr=wr, n=n: e.scalar_tensor_tensor(out=odr, in0=sr[:, 0:n:2], scalar=wr, in1=sr[:, 1:n:2], op0=ALU.mult, op1=ALU.add), reads=[srk, "WPr"], writes=[odrk])
                        S.op("dve", lambda e, odr=odr, si=si, wn=wn, n=n: e.scalar_tensor_tensor(out=odr, in0=si[:, 0:n:2], scalar=wn, in1=odr, op0=ALU.mult, op1=ALU.add), reads=[sik, "WPn", odrk], writes=[odrk])
                        S.op("dve", lambda e, odi=odi, si=si, wr=wr, n=n: e.scalar_tensor_tensor(out=odi, in0=si[:, 0:n:2], scalar=wr, in1=si[:, 1:n:2], op0=ALU.mult, op1=ALU.add), reads=[sik, "WPr"], writes=[odik])
                        S.op("dve", lambda e, odi=odi, sr=sr, wi=wi, n=n: e.scalar_tensor_tensor(out=odi, in0=sr[:, 0:n:2], scalar=wi, in1=odi, op0=ALU.mult, op1=ALU.add), reads=[srk, "WPi", odik], writes=[odik])
                        src, dst = dst, src
                else:
                    S.op("act", lambda e, pair=pair, br=br: e.copy(out=TRE[:, pair, 1:257], in_=br[:, 0:256]), reads=[brk], writes=["TRE%d" % pair])
                    S.op("act", lambda e, pair=pair, bim=bim: e.copy(out=TIM[:, pair, 1:257], in_=bim[:, 0:256]), reads=[bik], writes=["TIM%d" % pair])
            if q < 3:
                w8r = P["WPr"][:, 8, :]; w8i = P["WPi"][:, 8, :]
                S.op("dve", lambda e: e.tensor_tensor(out=c1[:], in0=w8r, in1=carry_r[:], op=ALU.mult), reads=["WPr", "carry_r"], writes=["c1"])
                S.op("dve", lambda e: e.tensor_tensor(out=c2[:], in0=w8i, in1=carry_i[:], op=ALU.mult), reads=["WPi", "carry_i"], writes=["c2"])
                S.op("dve", lambda e: e.tensor_tensor(out=c1[:], in0=c1[:], in1=c2[:], op=ALU.subtract), reads=["c1", "c2"], writes=["c1"])
                S.op("dve", lambda e: e.tensor_tensor(out=c1[:], in0=c1[:], in1=REDr[:], op=ALU.add), reads=["c1", "REDr"], writes=["c1"])
                S.op("dve", lambda e: e.tensor_tensor(out=c2[:], in0=w8r, in1=carry_i[:], op=ALU.mult), reads=["WPr", "carry_i", "c1"], writes=["c2"])
                S.op("dve", lambda e: e.tensor_tensor(out=c3[:], in0=w8i, in1=carry_r[:], op=ALU.mult), reads=["WPi", "carry_r"], writes=["c3"])
                S.op("dve", lambda e: e.tensor_tensor(out=c2[:], in0=c2[:], in1=c3[:], op=ALU.add), reads=["c2", "c3"], writes=["c2"])
                S.op("dve", lambda e: e.tensor_tensor(out=carry_i[:], in0=c2[:], in1=REDi[:], op=ALU.add), reads=["c2", "REDi"], writes=["carry_i"])
                S.op("dve", lambda e: e.tensor_copy(out=carry_r[:], in_=c1[:]), reads=["c1"], writes=["carry_r"])


def _s5_scan_out(C, es, D, P, idb, TRE, TIM, uT, ytm, carry_r, carry_i, dbg):
    S = C.S
    sb = lambda name, shape, dt=F32: C.sb(es, name, shape, dt)
    Mz2 = sb("Mz2s", [128, 16, 8, 128], BF16); CAA = sb("CAA", [128, 2, 32, 128], BF16)
    S.dma("sp", Mz2[:].rearrange("p a b c -> p (a b c)"), D["Mz_d"], reads=["Mz_d"], writes=["Mz2s"])
    S.dma("sp", CAA[:].rearrange("p a g c -> p a (g c)"), D["CA_d"], reads=["CA_d"], writes=["CAA"])
    tmr = [sb("tmr%d" % i, [128, 256]) for i in range(2)]; tmi = [sb("tmi%d" % i, [128, 256]) for i in range(2)]
    Tbr = [sb("Tbr%d" % i, [128, 256], BF16) for i in range(2)]; Tbi = [sb("Tbi%d" % i, [128, 256], BF16) for i in range(2)]
    Yg = [sb("Yg%d" % i, [128, 256], BF16) for i in range(2)]
    ysum = [sb("ysum%d" % i, [128, 256]) for i in range(2)]
    import os
    CUT = int(os.environ.get("SCAN_CUT", "9"))
    with scope(C) as esp:
        bank = [C.ps(esp, "sbk%d" % i, [128, 512]) for i in range(6)]
        for pair in range(16):
            sl = pair % 2
            a_ = pair // 2; wp = pair % 2; chc = a_ // 2; hb = a_ % 2
            rows = slice(64 * hb, 64 * hb + 64)
            kr, ki = "TRE%d" % pair, "TIM%d" % pair
            S.op("pool", lambda e, pair=pair: e.tensor_copy(out=TRE[:, pair, 0:1], in_=carry_r[:, pair:pair + 1]), reads=["carry_r"], writes=[kr])
            S.op("pool", lambda e, pair=pair: e.tensor_copy(out=TIM[:, pair, 0:1], in_=carry_i[:, pair:pair + 1]), reads=["carry_i"], writes=[ki])
            if CUT < 1:
                continue
            for j in range(9):
                d = 1 << j; m = 257 - d
                wr = P["WPr"][:, j, pair:pair + 1]; wi = P["WPi"][:, j, pair:pair + 1]; wn = P["WPn"][:, j, pair:pair + 1]
                tr = tmr[sl][:, 0:m]; ti = tmi[sl][:, 0:m]; trk = "tmr%d" % sl; tik = "tmi%d" % sl
                S.op("dve", lambda e, tr=tr, pair=pair, m=m, wr=wr: e.tensor_scalar(out=tr, in0=TRE[:, pair, 0:m], scalar1=wr, scalar2=None, op0=ALU.mult), reads=[kr, "WPr"], writes=[trk])
                S.op("dve", lambda e, tr=tr, pair=pair, m=m, wn=wn: e.scalar_tensor_tensor(out=tr, in0=TIM[:, pair, 0:m], scalar=wn, in1=tr, op0=ALU.mult, op1=ALU.add), reads=[ki, "WPn", trk], writes=[trk])
                S.op("dve", lambda e, ti=ti, pair=pair, m=m, wr=wr: e.tensor_scalar(out=ti, in0=TIM[:, pair, 0:m], scalar1=wr, scalar2=None, op0=ALU.mult), reads=[ki, "WPr"], writes=[tik])
                S.op("dve", lambda e, ti=ti, pair=pair, m=m, wi=wi: e.scalar_tensor_tensor(out=ti, in0=TRE[:, pair, 0:m], scalar=wi, in1=ti, op0=ALU.mult, op1=ALU.add), reads=[kr, "WPi", tik], writes=[tik])
                S.op("pool", lambda e, tr=tr, pair=pair, d=d: e.tensor_tensor(out=TRE[:, pair, d:257], in0=TRE[:, pair, d:257], in1=tr, op=ALU.add), reads=[kr, trk], writes=[kr])
                S.op("pool", lambda e, ti=ti, pair=pair, d=d: e.tensor_tensor(out=TIM[:, pair, d:257], in0=TIM[:, pair, d:257], in1=ti, op=ALU.add), reads=[ki, tik], writes=[ki])
            if CUT < 2:
                continue
            S.op("act", lambda e, pair=pair, sl=sl: e.copy(out=Tbr[sl][:], in_=TRE[:, pair, 0:256]), reads=[kr], writes=["Tbr%d" % sl])
            S.op("act", lambda e, pair=pair, sl=sl: e.copy(out=Tbi[sl][:], in_=TIM[:, pair, 0:256]), reads=[ki], writes=["Tbi%d" % sl])
            for r in range(2):
                g = 4 * a_ + 2 * r + wp
                e_ = chc * 4 + (g % 4)
                pr = slice(64 * r, 64 * r + 64)
                yb = bank[r]; ybk = "sbk%d" % r
                for s in range(8):
                    S.op("pe", lambda e, e_=e_, s=s, rows=rows, chc=chc, yb=yb: e.matmul(yb[:, 0:256], lhsT=Mz2[rows, e_, s, :], rhs=uT[rows, chc, s, :], start=(s == 0), stop=(s == 7)),
                         reads=["Mz2s", "uT"], writes=[ybk])
                zb_ = bank[4 + r]; zbk = "sbk%d" % (4 + r)
                S.op("pe", lambda e, g=g, pr=pr, zb_=zb_, r=r, sl=sl: e.matmul(zb_[:, 0:256], lhsT=CAA[pr, r, g, :], rhs=Tbr[sl][pr, :], start=True, stop=False), reads=["CAA", "Tbr%d" % sl], writes=[zbk])
                S.op("pe", lambda e, g=g, pr=pr, zb_=zb_, r=r, sl=sl: e.matmul(zb_[:, 0:256], lhsT=CAA[pr, 1 - r, g, :], rhs=Tbi[sl][pr, :], start=False, stop=True), reads=["CAA", "Tbi%d" % sl], writes=[zbk])
                if CUT < 3:
                    continue
                S.op("act", lambda e, r=r, zb_=zb_: e.copy(out=ysum[r][:], in_=zb_[:, 0:256]), reads=[zbk], writes=["ysum%d" % r])
                S.op("dve", lambda e, r=r, yb=yb: e.tensor_tensor(out=ysum[r][:], in0=yb[:, 0:256], in1=ysum[r][:], op=ALU.add), reads=[ybk, "ysum%d" % r], writes=["ysum%d" % r])
                S.op("act", lambda e, r=r: e.activation(out=Yg[r][:], in_=ysum[r][:], func=AF.Gelu), reads=["ysum%d" % r], writes=["Yg%d" % r])
                if CUT < 4:
                    continue
                pT = bank[2 + r][:].bitcast(BF16).rearrange("p (c n) -> p c n", n=128)
                for kb in range(2):
                    S.op("pe", lambda e, r=r, kb=kb, pT=pT: e.transpose(out=pT[:, kb, :], in_=Yg[r][:, kb * 128:(kb + 1) * 128], identity=idb[:]), reads=["Yg%d" % r, "identb"], writes=["sbk%d" % (2 + r)])
                for kb in range(2):
                    S.op("dve", lambda e, g=g, kb=kb, pT=pT: e.tensor_copy(out=ytm[:, kb, :, 16 * g:16 * g + 16], in_=pT[:, kb, :].rearrange("p (t c) -> p t c", c=16)),
                         reads=["sbk%d" % (2 + r)], writes=["ytm"])


def _s5_glu_out(C, es, D, idb, ytm, dbg):
    S = C.S
    sb = lambda name, shape, dt=F32: C.sb(es, name, shape, dt)
    gso = sb("gso", [128, 4]); bgl = sb("bgl", [128, 512])
    S.dma("sp", gso[:], D["g_ssm_out"], writes=["gso"])
    S.dma("sp", bgl[:], D["b_glu"].partition_broadcast(128), writes=["bgl"])
    with scope(C) as est:
        Wg = load_weight_bf16(C, es, est, "Wg", D["w_glu"], 4, 512)
    with scope(C) as est:
        Wo = load_weight_bf16(C, es, est, "Wos", D["w_out"][512:1024, :], 4, 1024, gcol=(gso, "gso"))
    yT = sb("yT", [128, 4, 128], BF16); zb = sb("zb", [128, 512]); ssm = sb("ssm", [128, 512]); junk = sb("junk3", [128, 512])
    st = sb("st3", [128, 2]); sn = sb("sn", [128, 512], BF16); snT = sb("snT", [128, 4, 128], BF16)
    ho = [sb("ho%d" % i, [128, 1024]) for i in range(2)]
    hsv = D["hs_d"].rearrange("(k t) d -> t k d", t=8)
    with scope(C) as esp:
        bank = [C.ps(esp, "gbk%d" % i, [128, 512]) for i in range(5)]
        it = 0
        for kb in range(2):
            for t in range(8):
                sl = it % 2; it += 1
                y = ytm[:, kb, t, :]
                pT = bank[0][:].bitcast(BF16).rearrange("p (c n) -> p c n", n=128)[:, 0:4, :]
                transpose_chunks(C, y, "ytm", 4, pT, "gbk0", yT[:], "yT", idb)
                for c in range(4):
                    S.op("pe", lambda e, c=c: e.matmul(bank[1][:], lhsT=yT[:, c, :], rhs=Wg[:, c, :], start=(c == 0), stop=(c == 3)), reads=["yT", "Wg"], writes=["gbk1"])
                S.op("dve", lambda e: e.tensor_tensor(out=zb[:], in0=bank[1][:], in1=bgl[:], op=ALU.add), reads=["gbk1", "bgl"], writes=["zb"])
                S.op("act", lambda e: e.activation(out=zb[:], in_=zb[:], func=AF.Sigmoid), reads=["zb"], writes=["zb"])
                S.op("pool", lambda e, y=y: e.tensor_tensor(out=ssm[:], in0=y, in1=zb[:], op=ALU.mult), reads=["ytm", "zb"], writes=["ssm"])
                if "ssm" in dbg:
                    S.dma("sp", dbg["ssm"].rearrange("(k t) d -> t k d", t=8)[t, kb * 128:(kb + 1) * 128, :], ssm[:], reads=["ssm"], writes=["o_ssm"])
                rms_rstd(C, ssm[:], "ssm", junk[:], "junk3", st[:, 0:1], st[:, 1:2], "st3", 512)
                S.op("dve", lambda e: e.tensor_scalar(out=sn[:], in0=ssm[:], scalar1=st[:, 1:2], scalar2=None, op0=ALU.mult), reads=["ssm", "st3"], writes=["sn"])
                pT2 = bank[2][:].bitcast(BF16).rearrange("p (c n) -> p c n", n=128)[:, 0:4, :]
                transpose_chunks(C, sn[:], "sn", 4, pT2, "gbk2", snT[:], "snT", idb)
                for cb in range(2):
                    for c in range(4):
                        S.op("pe", lambda e, c=c, cb=cb: e.matmul(bank[3 + cb][:], lhsT=snT[:, c, :], rhs=Wo[:, c, cb * 512:(cb + 1) * 512], start=(c == 0), stop=(c == 3)), reads=["snT", "Wos"], writes=["gbk%d" % (3 + cb)])
                    S.op("act", lambda e, cb=cb, sl=sl: e.copy(out=ho[sl][:, cb * 512:(cb + 1) * 512], in_=bank[3 + cb][:]), reads=["gbk%d" % (3 + cb)], writes=["ho%d" % sl])
                S.dma("sp", hsv[t, kb * 128:(kb + 1) * 128, :], ho[sl][:], reads=["ho%d" % sl], writes=["hs_d"])


def _attention(C, es, D, idb, dbg, prep=None):
    S = C.S
    sb = lambda name, shape, dt=F32: C.sb(es, name, shape, dt)
    kT = sb("kT", [128, 4, 2560], BF16); qT = sb("qT", [128, 4, 2048], BF16); V = sb("Vall", [128, 20, 520], BF16)
    qZ = sb("qZ", [128, 8, 2048], BF16)
    S.dma("sp", kT[:].rearrange("p c n -> p (c n)"), D["kT_d"], reads=["qkT_d"], writes=["kT"])
    S.dma("sp", qT[:].rearrange("p c n -> p (c n)"), D["qT_d"], reads=["qkT_d"], writes=["qT"])
    S.dma("sp", V[:], D["V_d"].rearrange("t p n -> p t n"), reads=["V_d"], writes=["Vall"])
    S.op("pool", lambda e: e.memset(qZ[:].rearrange("p h n -> p (h n)"), 0.0), writes=["qZ"])
    for h in range(8):
        rws = slice(64 * (h % 2), 64 * (h % 2) + 64)
        S.op("dve" if h % 2 else "act", (lambda e, h=h, rws=rws: e.tensor_copy(out=qZ[rws, h, :], in_=qT[rws, h // 2, :])) if h % 2 else (lambda e, h=h, rws=rws: e.copy(out=qZ[rws, h, :], in_=qT[rws, h // 2, :])),
             reads=["qT", "qZ"], writes=["qZ"])
    BT = sb("BT", [128, 8, 5, 128], BF16)
    gao = sb("gao", [128, 4])
    S.dma("sp", gao[:], D["g_att_out"], writes=["gao"])
    with scope(C) as est:
        stg = C.sb(est, "btstg", [128, 640])
        for h in range(8):
            S.dma("sp", stg[:], D["bias_t"][:, h * 640:(h + 1) * 640], writes=["btstg"])
            S.op("dve", lambda e, h=h: e.tensor_copy(out=BT[:, h, :, :].rearrange("p j q -> p (j q)"), in_=stg[:]), reads=["btstg"], writes=["BT"])
    with scope(C) as est:
        Wo = load_weight_bf16(C, es, est, "Woa", D["w_out"][0:512, :], 4, 1024, gcol=(gao, "gao"))
    PT = [sb("PT%d" % i, [128, 5, 128], BF16) for i in range(2)]
    rd = sb("rd", [128, 8]); att = sb("att", [128, 8, 64]); junk = sb("junk4", [128, 512]); st = sb("st4", [128, 2])
    an = sb("an", [128, 512], BF16); anT = sb("anT", [128, 4, 128], BF16)
    xo = [sb("xo%d" % i, [128, 1024]) for i in range(2)]; hsl = [sb("hsl%d" % i, [128, 1024]) for i in range(2)]
    h1t = [sb("h1t%d" % i, [128, 1024]) for i in range(2)]
    xv = D["x_ext"].rearrange("(n p) d -> n p d", p=128)
    hsv = D["hs_d"].rearrange("(n p) d -> n p d", p=128)
    h1v = D["h1_d"].rearrange("(n p) d -> n p d", p=128)
    with scope(C) as esp:
        bank = [C.ps(esp, "abk%d" % i, [128, 512]) for i in range(7)]
        for qt in range(16):
            sl = qt % 2
            drip(prep, 2)
            S.dma("sp", xo[sl][:], xv[48 + qt], writes=["xo%d" % sl])
            S.dma("sp", hsl[sl][:], hsv[qt], reads=["hs_d"], writes=["hsl%d" % sl])
            for h in range(8):
                hp = h // 2; rows = slice(64 * (h % 2), 64 * (h % 2) + 64); ps_ = h % 2
                bA = bank[2 * ps_]; bB = bank[2 * ps_ + 1]; bAk = "abk%d" % (2 * ps_); bBk = "abk%d" % (2 * ps_ + 1)
                for j in range(5):
                    o = bA[:, j * 128:(j + 1) * 128] if j < 4 else bB[:, 0:128]
                    ok = bAk if j < 4 else bBk
                    S.op("pe", lambda e, o=o, h=h, hp=hp, j=j, qt=qt: e.matmul(o, lhsT=kT[:, hp, (qt + j) * 128:(qt + j + 1) * 128], rhs=qZ[:, h, qt * 128:(qt + 1) * 128], start=True, stop=False),
                         reads=["kT", "qZ"], writes=[ok])
                    S.op("pe", lambda e, o=o, h=h, j=j: e.matmul(o, lhsT=idb[:], rhs=BT[:, h, j, :], start=False, stop=True), reads=["identb", "BT"], writes=[ok])
                S.op("act", lambda e, ps_=ps_, bA=bA: e.activation(out=PT[ps_][:, 0:4, :].rearrange("p j q -> p (j q)"), in_=bA[:], func=AF.Exp), reads=[bAk], writes=["PT%d" % ps_])
                S.op("act", lambda e, ps_=ps_, bB=bB: e.activation(out=PT[ps_][:, 4, :], in_=bB[:, 0:128], func=AF.Exp), reads=[bBk], writes=["PT%d" % ps_])
                ob = bank[4 + h // 4]; obk = "abk%d" % (4 + h // 4)
                for j in range(5):
                    S.op("pe", lambda e, ob=ob, h=h, j=j, ps_=ps_, qt=qt: e.matmul(ob[:, (h % 4) * 65:(h % 4) * 65 + 65], lhsT=PT[ps_][:, j, :], rhs=V[:, qt + j, h * 65:(h + 1) * 65], start=(j == 0), stop=(j == 4)),
                         reads=["PT%d" % ps_, "Vall"], writes=[obk])
            for hb in range(2):
                ov = bank[4 + hb][:, 0:260].rearrange("p (h d) -> p h d", d=65)
                S.op("dve", lambda e, hb=hb, ov=ov: e.reciprocal(out=rd[:, hb * 4:(hb + 1) * 4], in_=ov[:, :, 64]), reads=["abk%d" % (4 + hb)], writes=["rd%d" % hb])
                S.op("dve", lambda e, hb=hb, ov=ov: e.tensor_tensor(out=att[:, hb * 4:(hb + 1) * 4, :], in0=ov[:, :, 0:64], in1=bc(rd[:, hb * 4:(hb + 1) * 4].unsqueeze(2), [128, 4, 64]), op=ALU.mult),
                     reads=["abk%d" % (4 + hb), "rd%d" % hb], writes=["att%d" % hb])
            attf = att[:].rearrange("p h d -> p (h d)")
            if "att" in dbg:
                S.dma("sp", dbg["att"].rearrange("(n p) d -> n p d", p=128)[qt], attf, reads=["att0", "att1"], writes=["o_att"])
            S.op("act", lambda e: e.activation(out=junk[:], in_=attf, func=AF.Square, accum_out=st[:, 0:1]), reads=["att0", "att1"], writes=["st4_ss"])
            S.op("act", lambda e: e.activation(out=st[:, 1:2], in_=st[:, 0:1], func=AF.Sqrt, scale=1.0 / 512, bias=EPS), reads=["st4_ss"], writes=["st4_sq"])
            S.op("dve", lambda e: e.reciprocal(out=st[:, 1:2], in_=st[:, 1:2]), reads=["st4_sq"], writes=["st4"])
            S.op("dve", lambda e: e.tensor_scalar(out=an[:], in0=attf, scalar1=st[:, 1:2], scalar2=None, op0=ALU.mult), reads=["att0", "att1", "st4"], writes=["an"])
            pT = bank[6][:].bitcast(BF16).rearrange("p (c n) -> p c n", n=128)[:, 0:4, :]
            transpose_chunks(C, an[:], "an", 4, pT, "abk6", anT[:], "anT", idb)
            for cb in range(2):
                for c in range(4):
                    S.op("pe", lambda e, c=c, cb=cb: e.matmul(bank[cb][:], lhsT=anT[:, c, :], rhs=Wo[:, c, cb * 512:(cb + 1) * 512], start=(c == 0), stop=(c == 3)), reads=["anT", "Woa"], writes=["abk%d" % cb])
                S.op("dve", lambda e, cb=cb, sl=sl: e.tensor_tensor(out=h1t[sl][:, cb * 512:(cb + 1) * 512], in0=bank[cb][:], in1=xo[sl][:, cb * 512:(cb + 1) * 512], op=ALU.add),
                     reads=["abk%d" % cb, "xo%d" % sl], writes=["h1t%d" % sl])
            S.op("pool", lambda e, sl=sl: e.tensor_tensor(out=h1t[sl][:], in0=h1t[sl][:], in1=hsl[sl][:], op=ALU.add), reads=["h1t%d" % sl, "hsl%d" % sl], writes=["h1t%d" % sl])
            S.dma("sp", h1v[qt], h1t[sl][:], reads=["h1t%d" % sl], writes=["h1_d"])


def _col(g, n):
    return np.ascontiguousarray(np.asarray(g, np.float32).reshape(n, 128).T)


def host_shared(inp):
    f = lambda k: np.asarray(inp[k], np.float32)[0]
    sh = {}
    sh["g_mix"] = _col(f("norm_mix_g"), 8)
    sh["w_in"] = np.ascontiguousarray(f("w_in"))
    sh["att_q_g"] = f("att_q_g").reshape(1, 64)
    sh["att_k_g"] = f("att_k_g").reshape(1, 64)
    rb = f("rel_bias")
    p = np.arange(128); j = np.arange(5); q = np.arange(128)
    kidx = j[:, None] * 128 + p[None, :]
    kc = kidx // 64; ki = kidx % 64
    qc = q // 64; qi = q % 64
    jb = kc[:, :, None] - qc[None, None, :]
    allowed = (jb >= 0) & (jb <= 8)
    kj = jb * 64 + ki[:, :, None]
    dist = 512 + qi[None, None, :] - kj
    bucket = np.clip(np.clip(dist, -63, 128) + 63, 0, 191)
    bt = np.where(allowed[None], rb[:, bucket], np.float32(NEG))
    sh["bias_t"] = np.ascontiguousarray(bt.transpose(2, 0, 1, 3).reshape(128, 8 * 5 * 128).astype(np.float32))
    dup = lambda a: np.ascontiguousarray(np.concatenate([a, a], 0).astype(np.float32))
    sh["s5_lr"] = dup(f("ssm_lam_re").T)
    sh["s5_li"] = dup(f("ssm_lam_im").T)
    sh["s5_ls"] = np.ascontiguousarray(np.broadcast_to(f("ssm_log_step")[None, :], (128, 32)).astype(np.float32))
    sg = np.ones((128, 2), np.float32); sg[:64, 0] = -1.0; sg[64:, 1] = -1.0
    sh["s5_sg"] = sg
    sh["s5_nv"] = np.ascontiguousarray(np.broadcast_to(np.arange(-7, 16, dtype=np.float32)[None, :], (128, 23)))
    bre = f("ssm_b_re").transpose(1, 0, 2).reshape(64, 512); bim = f("ssm_b_im").transpose(1, 0, 2).reshape(64, 512)
    cre = f("ssm_c_re").transpose(2, 0, 1).reshape(64, 512); cim = f("ssm_c_im").transpose(2, 0, 1).reshape(64, 512)
    sh["s5_p1b"] = np.ascontiguousarray(np.concatenate([bre, bim], 0)); sh["s5_p2b"] = np.ascontiguousarray(np.concatenate([bim, bre], 0))
    sh["s5_p1c"] = np.ascontiguousarray(np.concatenate([cre, cim], 0)); sh["s5_p2c"] = np.ascontiguousarray(np.concatenate([cim, cre], 0))
    dd = np.zeros((2, 4, 16, 4, 4, 16), np.float32)
    dsk = f("ssm_d")
    for g in range(32):
        chc = g // 8; hb = (g % 8) // 4; j4 = g % 4
        for c in range(16):
            dd[hb, j4, c, chc, j4, c] = dsk[g, c]
    sh["s5_dd"] = dd.reshape(128, 256)
    sh["g_ssm_out"] = _col(f("ssm_out_g"), 4)
    sh["g_att_out"] = _col(f("att_out_g"), 4)
    sh["b_glu"] = f("ssm_b_glu").reshape(1, 512)
    sh["w_glu"] = np.ascontiguousarray(f("ssm_w_glu"))
    sh["w_out"] = np.ascontiguousarray(f("w_out"))
    return sh


def host_core(inp, c):
    b, seg = c // 4, c % 4
    x = np.asarray(inp["x"], np.float32)
    xe = np.zeros((8192, 1024), np.float32)
    n = (seg + 1) * 2048
    xe[8192 - n:] = x[b, :n]
    hv = np.full((512,), 1.0 if seg > 0 else 0.0, np.float32)
    return {"x_ext": xe, "hvalid": np.ascontiguousarray(hv.reshape(4, 128).T)}


IN_SHAPES = {
    "x_ext": [8192, 1024], "hvalid": [128, 4], "g_mix": [128, 8], "w_in": [1024, 2048], "att_q_g": [1, 64], "att_k_g": [1, 64],
    "bias_t": [128, 5120], "s5_lr": [128, 32], "s5_li": [128, 32], "s5_ls": [128, 32], "s5_sg": [128, 2], "s5_nv": [128, 23],
    "s5_p1b": [128, 512], "s5_p2b": [128, 512], "s5_p1c": [128, 512], "s5_p2c": [128, 512], "s5_dd": [128, 256],
    "g_ssm_out": [128, 4], "g_att_out": [128, 4], "b_glu": [1, 512], "w_glu": [512, 512], "w_out": [1024, 1024],
}
IN_SHAPES_MEM = {"mem": [256, 1024], "g_mem": [128, 8], "g_memkv": [128, 8], "mem_q_g": [1, 256], "mem_k_g": [1, 256],
                 "w_mem_q": [1024, 1024], "w_mem_k": [1024, 1024], "w_mem_v": [1024, 1024], "w_mem_o": [1024, 1024]}
IN_SHAPES_PEER = {"g_peer": [1, 1024], "w_peer_q": [1024, 2048], "keysT": [128, 2048], "peer_uT": [1024, 16384], "peer_v": [16384, 1024]}
SCRATCH = {"kT_d": ([128, 4 * 2560], BF16), "qT_d": ([128, 4 * 2048], BF16), "V_d": ([20, 128, 520], BF16),
           "hs_d": ([2048, 1024], F32), "Mz_d": ([128, 16 * 8 * 128], BF16), "CA_d": ([128, 2, 4096], BF16)}
SCRATCH_PEER = {"uT_b": ([1024, 16384], BF16), "v_b": ([16384, 1024], BF16), "xnT_d": ([16, 128, 1024], BF16),
                "sc_d": ([16, 128, 2048], F32), "tau_d": ([16, 128, 8], F32)}


def host_shared_rest(inp):
    f = lambda k: np.asarray(inp[k], np.float32)[0]
    sh = {}
    sh["g_mem"] = _col(f("norm_mem_g"), 8); sh["g_memkv"] = _col(f("norm_memkv_g"), 8)
    sh["mem_q_g"] = f("mem_q_g").reshape(1, 256); sh["mem_k_g"] = f("mem_k_g").reshape(1, 256)
    for k in ("w_mem_q", "w_mem_k", "w_mem_v", "w_mem_o", "w_peer_q"):
        sh[k] = np.ascontiguousarray(f(k))
    sh["g_peer"] = f("norm_peer_g").reshape(1, 1024)
    sh["keysT"] = np.ascontiguousarray(f("peer_keys").transpose(3, 0, 1, 2).reshape(128, 2048))
    sh["peer_uT"] = np.ascontiguousarray(f("peer_u").T)
    sh["peer_v"] = np.ascontiguousarray(f("peer_v"))
    return sh


def build_program(stages=("mixer", "mem", "peer"), dbg_specs=None, upto=9):
    nc = bass.Bass("TRN2", target_bir_lowering=False)
    D = {}
    shapes = {}
    if "mixer" in stages:
        shapes.update(IN_SHAPES)
    if "mem" in stages:
        shapes.update(IN_SHAPES_MEM)
    if "peer" in stages:
        shapes.update(IN_SHAPES_PEER)
    for k, shp in shapes.items():
        D[k] = nc.dram_tensor(k, shp, F32, kind="ExternalInput").ap()
    scr = {}
    if "mixer" in stages:
        scr.update(SCRATCH)
    if "peer" in stages:
        scr.update(SCRATCH_PEER)
    for k, (shp, dt) in scr.items():
        D[k] = nc.dram_tensor(k, shp, dt).ap()
    chain = ["h1_d", "h2_d", "out"]
    first = {"mixer": None, "mem": "h1_d", "peer": "h2_d"}[stages[0]]
    last = {"mixer": "h1_d", "mem": "h2_d", "peer": "out"}[stages[-1]]
    for k in chain:
        if k == first:
            D[k] = nc.dram_tensor(k, [2048, 1024], F32, kind="ExternalInput").ap()
        elif k == last:
            D[k] = nc.dram_tensor(k, [2048, 1024], F32, kind="ExternalOutput").ap()
        else:
            D[k] = nc.dram_tensor(k, [2048, 1024], F32).ap()
    dbg = {}
    for k, shp in (dbg_specs or {}).items():
        dbg[k] = nc.dram_tensor("dbg_" + k, shp, F32, kind="ExternalOutput").ap()
    with ExitStack() as es:
        S = Sched(nc, es)
        C = Ctx(nc, S)
        prep = stage_peer_prep(C, D) if "peer" in stages else None
        if "mixer" in stages:
            stage_mixer(C, D, dbg, upto, prep=prep)
        if "mem" in stages:
            stage_mem(C, D, dbg, prep=prep)
        drip(prep, 1000)
        if "peer" in stages:
            stage_peer(C, D, dbg)
        S.barrier()
        S.emit()
    return nc, S


def _headnorm(C, src_ap, skey, nh, hd, sqt, sqk, stat, stk, gt, gkey, dst_ap, dkey):
    S = C.S
    sv = src_ap.rearrange("p (h d) -> p h d", d=hd)
    S.op("pool", lambda e: e.tensor_tensor(out=sqt, in0=src_ap, in1=src_ap, op=ALU.mult), reads=[skey], writes=[sqk])
    S.op("dve", lambda e: e.tensor_reduce(out=stat[:, 0:nh], in_=sqt.rearrange("p (h d) -> p h d", d=hd), axis=AX.X, op=ALU.add), reads=[sqk], writes=[stk + "a"])
    S.op("act", lambda e: e.activation(out=stat[:, nh:2 * nh], in_=stat[:, 0:nh], func=AF.Sqrt, scale=1.0 / hd, bias=EPS), reads=[stk + "a"], writes=[stk + "b"])
    S.op("dve", lambda e: e.reciprocal(out=stat[:, nh:2 * nh], in_=stat[:, nh:2 * nh]), reads=[stk + "b"], writes=[stk])
    S.op("dve", lambda e: e.tensor_tensor(out=sv, in0=sv, in1=bc(stat[:, nh:2 * nh].unsqueeze(2), [128, nh, hd]), op=ALU.mult), reads=[skey, stk], writes=[skey])
    S.op("pool", lambda e: e.tensor_tensor(out=dst_ap.rearrange("p (h d) -> p h d", d=hd), in0=sv, in1=bc(gt.unsqueeze(1), [128, nh, hd]), op=ALU.mult), reads=[skey, gkey], writes=[dkey])


def stage_mem(C, D, dbg=None, prep=None):
    S = C.S
    dbg = dbg or {}
    with scope(C) as es:
        sb = lambda name, shape, dt=F32: C.sb(es, name, shape, dt)
        idf, idb = make_ident(C, es, "ident")
        gm = sb("gm", [128, 8]); gkv = sb("gkv", [128, 8]); gq = sb("mgq", [128, 256]); gk = sb("mgk", [128, 256])
        S.dma("sp", gm[:], D["g_mem"], writes=["gm"]); S.dma("sp", gkv[:], D["g_memkv"], writes=["gkv"])
        S.dma("sp", gq[:], D["mem_q_g"].partition_broadcast(128), writes=["mgq"]); S.dma("sp", gk[:], D["mem_k_g"].partition_broadcast(128), writes=["mgk"])
        S.op("dve", lambda e: e.tensor_scalar(out=gq[:], in0=gq[:], scalar1=1.0 / 16, scalar2=None, op0=ALU.mult), reads=["mgq"], writes=["mgq"])
        kTm = sb("kTm", [128, 8, 256], BF16); Vm = sb("Vm", [128, 2, 4, 257], BF16)
        xt = [sb("mxt%d" % i, [128, 1024]) for i in range(2)]; st = sb("mst", [128, 2]); xn = sb("mxn", [128, 1024], BF16)
        xnT = sb("mxnT", [128, 8, 128], BF16); qf = sb("mqf", [128, 1024]); sq = sb("msq", [128, 1024]); qst = sb("mqst", [128, 8])
        qn = sb("mqn", [128, 1024], BF16); mjunk = sb("mjunk", [128, 1024], BF16)
        with scope(C) as esk:
            with scope(C) as est:
                Wk = load_weight_bf16(C, esk, est, "Wmk", D["w_mem_k"], 8, 1024, gcol=(gkv, "gkv"))
            with scope(C) as est:
                Wv = load_weight_bf16(C, esk, est, "Wmv", D["w_mem_v"], 8, 1024, gcol=(gkv, "gkv"))
            with scope(C) as esp:
                bank = [C.ps(esp, "mkb%d" % i, [128, 512]) for i in range(6)]
                mv = D["mem"].rearrange("(n p) d -> n p d", p=128)
                for mt in range(2):
                    x_ = xt[mt]; xk = "mxt%d" % mt
                    S.dma("sp", x_[:], mv[mt], writes=[xk])
                    rms_rstd(C, x_[:], xk, mjunk[:], "mjunk", st[:, 0:1], st[:, 1:2], "mst", 1024)
                    S.op("dve", lambda e, x_=x_: e.tensor_scalar(out=xn[:], in0=x_[:], scalar1=st[:, 1:2], scalar2=None, op0=ALU.mult), reads=[xk, "mst"], writes=["mxn"])
                    pb = bank[0][:].bitcast(BF16).rearrange("p (c n) -> p c n", n=128)
                    transpose_chunks(C, xn[:], "mxn", 8, pb, "mkb0", xnT[:], "mxnT", idb)
                    for (W, wk, b0) in ((Wk, "Wmk", 1), (Wv, "Wmv", 3)):
                        for cb in range(2):
                            for dc in range(8):
                                S.op("pe", lambda e, W=W, cb=cb, dc=dc, b0=b0: e.matmul(bank[b0 + cb][:], lhsT=xnT[:, dc, :], rhs=W[:, dc, cb * 512:(cb + 1) * 512], start=(dc == 0), stop=(dc == 7)),
                                     reads=["mxnT", wk], writes=["mkb%d" % (b0 + cb)])
                    for cb in range(2):
                        S.op("act", lambda e, cb=cb: e.copy(out=qf[:, cb * 512:(cb + 1) * 512], in_=bank[1 + cb][:]), reads=["mkb%d" % (1 + cb)], writes=["mqf"])
                        S.op("dve", lambda e, cb=cb, mt=mt: e.tensor_copy(out=Vm[:, mt, 2 * cb:2 * cb + 2, 0:256], in_=bank[3 + cb][:].rearrange("p (h d) -> p h d", d=256)), reads=["mkb%d" % (3 + cb)], writes=["Vm"])
                    S.op("pool", lambda e, mt=mt: e.memset(Vm[:, mt, :, 256], 1.0), reads=[], writes=["Vm"])
                    _headnorm(C, qf[:], "mqf", 4, 256, sq[:], "msq", qst, "mqst", gk[:], "mgk", qn[:], "mqn")
                    pb2 = bank[5][:].bitcast(BF16).rearrange("p (c n) -> p c n", n=128)
                    transpose_chunks(C, qn[:], "mqn", 8, pb2, "mkb5", kTm[:, :, mt * 128:(mt + 1) * 128], "kTm", idb)
        with scope(C) as est:
            Wq = load_weight_bf16(C, es, est, "Wmq", D["w_mem_q"], 8, 1024, gcol=(gm, "gm"))
        with scope(C) as est:
            Wo = load_weight_bf16(C, es, est, "Wmo", D["w_mem_o"], 8, 1024)
        qT = sb("mqT", [128, 8, 128], BF16); PT = [sb("mPT%d" % i, [128, 2, 128], BF16) for i in range(2)]
        rd = sb("mrd", [128, 4]); ob = sb("mob", [128, 1024], BF16); oT = sb("moT", [128, 8, 128], BF16)
        h2t = [sb("h2t%d" % i, [128, 1024]) for i in range(2)]
        hv = D["h1_d"].rearrange("(n p) d -> n p d", p=128); ov = D["h2_d"].rearrange("(n p) d -> n p d", p=128)
        with scope(C) as esp:
            bank = [C.ps(esp, "mb%d" % i, [128, 512]) for i in range(8)]
            for tt in range(16):
                sl = tt % 2
                drip(prep, 1)
                x_ = xt[sl]; xk = "mxt%d" % sl
                S.dma("sp", x_[:], hv[tt], reads=["h1_d"], writes=[xk])
                rms_rstd(C, x_[:], xk, mjunk[:], "mjunk", st[:, 0:1], st[:, 1:2], "mst", 1024)
                S.op("dve", lambda e, x_=x_: e.tensor_scalar(out=xn[:], in0=x_[:], scalar1=st[:, 1:2], scalar2=None, op0=ALU.mult), reads=[xk, "mst"], writes=["mxn"])
                pb = bank[0][:].bitcast(BF16).rearrange("p (c n) -> p c n", n=128)
                transpose_chunks(C, xn[:], "mxn", 8, pb, "mb0", xnT[:], "mxnT", idb)
                for cb in range(2):
                    for dc in range(8):
                        S.op("pe", lambda e, cb=cb, dc=dc: e.matmul(bank[1 + cb][:], lhsT=xnT[:, dc, :], rhs=Wq[:, dc, cb * 512:(cb + 1) * 512], start=(dc == 0), stop=(dc == 7)),
                             reads=["mxnT", "Wmq"], writes=["mb%d" % (1 + cb)])
                    S.op("act", lambda e, cb=cb: e.copy(out=qf[:, cb * 512:(cb + 1) * 512], in_=bank[1 + cb][:]), reads=["mb%d" % (1 + cb)], writes=["mqf"])
                _headnorm(C, qf[:], "mqf", 4, 256, sq[:], "msq", qst, "mqst", gq[:], "mgq", qn[:], "mqn")
                pb2 = bank[3][:].bitcast(BF16).rearrange("p (c n) -> p c n", n=128)
                transpose_chunks(C, qn[:], "mqn", 8, pb2, "mb3", qT[:], "mqT", idb)
                for h in range(4):
                    ps_ = h % 2
                    sbk = bank[4 + ps_]; sbkk = "mb%d" % (4 + ps_)
                    for mt in range(2):
                        for dh in range(2):
                            S.op("pe", lambda e, h=h, mt=mt, dh=dh, sbk=sbk: e.matmul(sbk[:, mt * 128:(mt + 1) * 128], lhsT=kTm[:, 2 * h + dh, mt * 128:(mt + 1) * 128], rhs=qT[:, 2 * h + dh, :], start=(dh == 0), stop=(dh == 1)),
                                 reads=["kTm", "mqT"], writes=[sbkk])
                    S.op("act", lambda e, ps_=ps_, sbk=sbk: e.activation(out=PT[ps_][:].rearrange("p m q -> p (m q)"), in_=sbk[:, 0:256], func=AF.Exp, bias=-8.0), reads=[sbkk], writes=["mPT%d" % ps_])
                    obk = bank[6 + ps_]; obkk = "mb%d" % (6 + ps_)
                    for mt in range(2):
                        S.op("pe", lambda e, h=h, mt=mt, ps_=ps_, obk=obk: e.matmul(obk[:, 0:257], lhsT=PT[ps_][:, mt, :], rhs=Vm[:, mt, h, :], start=(mt == 0), stop=(mt == 1)), reads=["mPT%d" % ps_, "Vm"], writes=[obkk])
                    S.op("dve", lambda e, h=h, obk=obk: e.reciprocal(out=rd[:, h:h + 1], in_=obk[:, 256:257]), reads=[obkk], writes=["mrd%d" % h])
                    S.op("dve", lambda e, h=h, obk=obk: e.tensor_scalar(out=ob[:, h * 256:(h + 1) * 256], in0=obk[:, 0:256], scalar1=rd[:, h:h + 1], scalar2=None, op0=ALU.mult), reads=[obkk, "mrd%d" % h], writes=["mob"])
                pb3 = bank[0][:].bitcast(BF16).rearrange("p (c n) -> p c n", n=128)
                transpose_chunks(C, ob[:], "mob", 8, pb3, "mb0", oT[:], "moT", idb)
                for cb in range(2):
                    for dc in range(8):
                        S.op("pe", lambda e, cb=cb, dc=dc: e.matmul(bank[1 + cb][:], lhsT=oT[:, dc, :], rhs=Wo[:, dc, cb * 512:(cb + 1) * 512], start=(dc == 0), stop=(dc == 7)),
                             reads=["moT", "Wmo"], writes=["mb%d" % (1 + cb)])
                    S.op("dve", lambda e, cb=cb, sl=sl, x_=x_: e.tensor_tensor(out=h2t[sl][:, cb * 512:(cb + 1) * 512], in0=bank[1 + cb][:], in1=x_[:, cb * 512:(cb + 1) * 512], op=ALU.add),
                         reads=["mb%d" % (1 + cb), xk], writes=["h2t%d" % sl])
                S.dma("sp", ov[tt], h2t[sl][:], reads=["h2t%d" % sl], writes=["h2_d"])


def stage_peer_prep(C, D):
    S = C.S
    uv = D["peer_uT"].rearrange("(c p) (a e) -> c p a e", p=128, e=2048)
    ub = D["uT_b"].rearrange("(c p) (a e) -> c p a e", p=128, e=2048)
    vv = D["peer_v"].rearrange("(c p) d -> c p d", p=512)
    vb = D["v_b"].rearrange("(c p) d -> c p d", p=512)

    def gen():
        for c in range(8):
            S.dma("pool", ub[c], uv[c], writes=["uT_b"])
            yield
        for c in range(32):
            S.dma("pool", vb[c], vv[c], writes=["v_b"])
            yield
    return gen()


def drip(g, n):
    if g is None:
        return
    for _ in range(n):
        try:
            next(g)
        except StopIteration:
            return


def _top16(C, src, skey, work, wkey, dst, dkey):
    S = C.S
    S.op("dve", lambda e: e.max(out=dst[:, 0:8], in_=src), reads=[skey], writes=[dkey])
    S.op("dve", lambda e: e.match_replace(out=work, in_to_replace=dst[:, 0:8], in_values=src, imm_value=-1e30), reads=[skey, dkey], writes=[wkey])
    S.op("dve", lambda e: e.max(out=dst[:, 8:16], in_=work), reads=[wkey], writes=[dkey])


def stage_peer(C, D, dbg=None):
    S = C.S
    dbg = dbg or {}
    hv = D["h2_d"].rearrange("(n p) d -> n p d", p=128)
    with scope(C) as es:
        sb = lambda name, shape, dt=F32: C.sb(es, name, shape, dt)
        idf, idb = make_ident(C, es, "ident")
        gp = sb("gpb", [128, 1024])
        S.dma("sp", gp[:], D["g_peer"].partition_broadcast(128), writes=["gpb"])
        with scope(C) as est:
            Wq = load_weight_bf16(C, es, est, "Wpq", D["w_peer_q"], 8, 2048)
        keyT = sb("keyT", [128, 16, 128], BF16)
        with scope(C) as est:
            kst = C.sb(est, "kst", [128, 2048])
            S.dma("sp", kst[:], D["keysT"], writes=["kst"])
            S.op("dve", lambda e: e.tensor_copy(out=keyT[:].rearrange("p a n -> p (a n)"), in_=kst[:]), reads=["kst"], writes=["keyT"])
        xt = [sb("pxt%d" % i, [128, 1024]) for i in range(2)]; st_ = [sb("pst%d" % i, [128, 2]) for i in range(2)]; junk = sb("pjunk", [128, 1024], BF16)
        xn_ = [sb("pxn%d" % i, [128, 1024], BF16) for i in range(2)]; xnT = [sb("pxnT%d" % i, [128, 8, 128], BF16) for i in range(2)]
        qb_ = [sb("pqb%d" % i, [128, 2048], BF16) for i in range(2)]; qTp_ = [sb("pqT%d" % i, [128, 16, 128], BF16) for i in range(2)]
        sc = [sb("psc%d" % i, [128, 16, 128]) for i in range(2)]; work_ = [sb("pwork%d" % i, [128, 256]) for i in range(2)]
        sv_ = [sb("psv%d" % i, [128, 16, 16]) for i in range(2)]; cand_ = [sb("pcand%d" % i, [128, 8, 256]) for i in range(2)]
        cex_ = [sb("pcex%d" % i, [128, 8, 256]) for i in range(2)]; ctop_ = [sb("pctop%d" % i, [128, 8, 16]) for i in range(2)]
        Z_ = [sb("pZ%d" % i, [128, 8]) for i in range(2)]; off_ = [sb("poff%d" % i, [128, 8]) for i in range(2)]; tau = [sb("ptau%d" % i, [128, 8]) for i in range(2)]
        cjunk_ = [sb("pcj%d" % i, [128, 256]) for i in range(2)]; offs_ = [sb("poffs%d" % i, [128, 16]) for i in range(2)]
        xTd = D["xnT_d"].rearrange("n p (c t) -> n p c t", t=128)
        with scope(C) as esp:
            bank = [C.ps(esp, "pab%d" % i, [128, 512]) for i in range(6)]

            def tile(tt):
                sl = tt % 2
                K = lambda nm: "%s%d" % (nm, sl)
                x_ = xt[sl]; xk = K("pxt"); st = st_[sl]; xn = xn_[sl]; qb = qb_[sl]; qTp = qTp_[sl]; work = work_[sl]
                sv = sv_[sl]; cand = cand_[sl]; cex = cex_[sl]; ctop = ctop_[sl]; Z = Z_[sl]; off = off_[sl]; cjunk = cjunk_[sl]; offs = offs_[sl]
                S.dma("sp", x_[:], hv[tt], reads=["h2_d"], writes=[xk])
                rms_rstd(C, x_[:], xk, junk[:], "pjunk", st[:, 0:1], st[:, 1:2], K("pst"), 1024)
                S.op("dve", lambda e: e.scalar_tensor_tensor(out=xn[:], in0=x_[:], scalar=st[:, 1:2], in1=gp[:], op0=ALU.mult, op1=ALU.mult), reads=[xk, K("pst"), "gpb"], writes=[K("pxn")])
                pb = bank[0][:].bitcast(BF16).rearrange("p (c n) -> p c n", n=128)
                transpose_chunks(C, xn[:], K("pxn"), 8, pb, "pab0", xnT[sl][:], K("pxnT"), idb)
                S.dma("sp", xTd[tt], xnT[sl][:], reads=[K("pxnT")], writes=["xnT_d"])
                yield
                for cb in range(4):
                    for dc in range(8):
                        S.op("pe", lambda e, cb=cb, dc=dc: e.matmul(bank[1 + cb][:], lhsT=xnT[sl][:, dc, :], rhs=Wq[:, dc, cb * 512:(cb + 1) * 512], start=(dc == 0), stop=(dc == 7)),
                             reads=[K("pxnT"), "Wpq"], writes=["pab%d" % (1 + cb)])
                    S.op("act", lambda e, cb=cb: e.copy(out=qb[:, cb * 512:(cb + 1) * 512], in_=bank[1 + cb][:]), reads=["pab%d" % (1 + cb)], writes=[K("pqb")])
                for half in range(2):
                    pbq = bank[5][:].bitcast(BF16).rearrange("p (c n) -> p c n", n=128)
                    transpose_chunks(C, qb[:, half * 1024:(half + 1) * 1024], K("pqb"), 8, pbq, "pab5", qTp[:, half * 8:(half + 1) * 8, :], K("pqT"), idb)
                for hh in range(16):
                    S.op("pe", lambda e, hh=hh: e.matmul(bank[1 + hh // 4][:, (hh % 4) * 128:(hh % 4 + 1) * 128], lhsT=qTp[:, hh, :], rhs=keyT[:, hh, :], start=True, stop=True),
                         reads=[K("pqT"), "keyT"], writes=["pab%d" % (1 + hh // 4)])
                scs = sc[sl]; sck = K("psc")
                for cb in range(4):
                    S.op("act", lambda e, cb=cb: e.copy(out=scs[:, cb * 4:(cb + 1) * 4, :].rearrange("p a n -> p (a n)"), in_=bank[1 + cb][:]), reads=["pab%d" % (1 + cb)], writes=[sck])
                yield
                for hh in range(16):
                    _top16(C, scs[:, hh, :], sck, work[:, 0:128], K("pwork"), sv[:, hh, :], K("psv"))
                    yield
                svv = sv[:].rearrange("p (h s) k -> p h s k", s=2)
                for h in range(8):
                    S.op("dve", lambda e, h=h: e.tensor_tensor(out=cand[:, h, :].rearrange("p (a b) -> p a b", b=16), in0=bc(svv[:, h, 0, :].unsqueeze(2), [128, 16, 16]),
                                                            in1=bc(svv[:, h, 1, :].unsqueeze(1), [128, 16, 16]), op=ALU.add), reads=[K("psv")], writes=[K("pcand") + "_%d" % h])
                    yield
                ck = [K("pcand") + "_%d" % h for h in range(8)]
                for h in range(8):
                    _top16(C, cand[:, h, :], ck[h], work[:], K("pwork"), ctop[:, h, :], K("pctop"))
                    yield
                S.op("dve", lambda e: e.tensor_tensor(out=cand[:], in0=cand[:], in1=bc(ctop[:, :, 0:1], [128, 8, 256]), op=ALU.subtract), reads=ck + [K("pctop")], writes=ck)
                S.op("act", lambda e: e.activation(out=cex[:].rearrange("p h n -> p (h n)"), in_=cand[:].rearrange("p h n -> p (h n)"), func=AF.Exp), reads=ck, writes=[K("pcex")])
                yield
                S.op("dve", lambda e: e.tensor_tensor(out=tau[sl][:], in0=ctop[:, :, 15], in1=ctop[:, :, 0], op=ALU.subtract), reads=[K("pctop")], writes=[K("ptau")])
                yield
                S.op("dve", lambda e: e.tensor_scalar(out=tau[sl][:], in0=tau[sl][:], scalar1=-1e-5, scalar2=None, op0=ALU.add), reads=[K("ptau")], writes=[K("ptau")])
                yield
                for h in range(8):
                    S.op("dve", lambda e, h=h: e.scalar_tensor_tensor(out=cjunk[:], in0=cand[:, h, :], scalar=tau[sl][:, h:h + 1], in1=cex[:, h, :], op0=ALU.is_ge, op1=ALU.mult, accum_out=Z[:, h:h + 1]),
                         reads=ck + [K("pcex"), K("ptau")], writes=[K("pZ") + "_%d" % h])
                    yield
                S.op("act", lambda e: e.activation(out=off[:], in_=Z[:], func=AF.Ln), reads=[K("pZ") + "_%d" % h for h in range(8)], writes=[K("poff")])
                yield
                S.op("dve", lambda e: e.tensor_tensor(out=tau[sl][:], in0=tau[sl][:], in1=off[:], op=ALU.subtract), reads=[K("ptau"), K("poff")], writes=[K("ptau")])
                S.op("act", lambda e: e.activation(out=tau[sl][:], in_=tau[sl][:], func=AF.Exp), reads=[K("ptau")], writes=[K("ptau")])
                yield
                S.op("dve", lambda e: e.tensor_scalar(out=tau[sl][:], in0=tau[sl][:], scalar1=0.99997, scalar2=None, op0=ALU.mult), reads=[K("ptau")], writes=[K("ptau")])
                offv = offs[:].rearrange("p (h s) -> p h s", s=2)
                S.op("dve", lambda e: e.tensor_copy(out=offv[:, :, 0], in_=svv[:, :, 0, 0]), reads=[K("psv")], writes=[K("poffs") + "a"])
                yield
                S.op("dve", lambda e: e.tensor_tensor(out=offv[:, :, 1], in0=svv[:, :, 1, 0], in1=off[:], op=ALU.add), reads=[K("psv"), K("poff")], writes=[K("poffs") + "b"])
                yield
                S.op("dve", lambda e: e.tensor_tensor(out=scs[:], in0=scs[:], in1=bc(offs[:].unsqueeze(2), [128, 16, 128]), op=ALU.subtract), reads=[sck, K("poffs") + "a", K("poffs") + "b"], writes=[sck])
                S.op("act", lambda e: e.activation(out=scs[:].rearrange("p a n -> p (a n)"), in_=scs[:].rearrange("p a n -> p (a n)"), func=AF.Exp), reads=[sck], writes=[sck])
                S.dma("sp", D["sc_d"][tt], scs[:].rearrange("p a n -> p (a n)"), reads=[sck], writes=["sc_d"])
                S.dma("sp", D["tau_d"][tt], tau[sl][:], reads=[K("ptau")], writes=["tau_d"])

            from itertools import zip_longest
            for t0 in range(0, 16, 2):
                for _ in zip_longest(tile(t0), tile(t0 + 1)):
                    pass
    with scope(C) as es:
        sb = lambda name, shape, dt=F32: C.sb(es, name, shape, dt)
        idf, idb = make_ident(C, es, "ident")
        xnT = sb("bxnT", [128, 4, 1024], BF16); sc = sb("bsc", [128, 4, 2048]); kap = sb("btau", [128, 4, 8])
        UT = [sb("UT%d" % i, [128, 8, 1024], BF16) for i in range(2)]; Vb = [sb("Vb%d" % i, [128, 8, 1024], BF16) for i in range(2)]
        acc = sb("pacc", [128, 4, 1024])
        NP = 4
        Pt = [sb("pP%d" % i, [128, 512]) for i in range(NP)]; Wh = [[sb("pWh%d_%d" % (i, h), [128, 512], BF16) for h in range(8)] for i in range(2)]
        G = [sb("pG%d" % i, [128, 512], BF16) for i in range(3)]; WA = [sb("pWA%d" % i, [128, 512], BF16) for i in range(2)]
        WAT = [sb("pWAT%d" % i, [128, 4, 128], BF16) for i in range(2)]
        h2t = [sb("ph2t%d" % i, [128, 1024]) for i in range(2)]
        uTv = D["uT_b"].rearrange("(c p) (b e) -> b p c e", p=128, e=1024)
        vbv = D["v_b"].rearrange("(b c p) d -> b p c d", p=128, c=8)
        ov = D["out"].rearrange("(n p) d -> n p d", p=128)
        with scope(C) as esp:
            bank = [C.ps(esp, "pbb%d" % i, [128, 512]) for i in range(7)]
            state = {"it": 0}

            def stage1a(u, tg, eb, sub, tt, es_):
                ub = u % 2
                xT = xnT[:, tt, :].rearrange("p (c t) -> p c t", t=128)
                scv = sc[:, tt, :].rearrange("p (h s n) -> p h s n", s=2, n=128)
                i0 = eb * 8 + sub * 4
                for dc in range(8):
                    S.op("pe", lambda e, dc=dc, xT=xT: e.matmul(bank[ub][:], lhsT=xT[:, dc, :], rhs=UT[es_][:, dc, sub * 512:(sub + 1) * 512], start=(dc == 0), stop=(dc == 7)),
                         reads=["bxnT", "UT%d" % es_], writes=["pbb%d" % ub])
                for h in range(8):
                    hs = state["it"] % NP; state["it"] += 1
                    if h >= 5:
                        S.op("pool", lambda e, h=h, hs=hs, scv=scv: e.tensor_tensor(out=Pt[hs][:].rearrange("p (i j) -> p i j", j=128), in0=bc(scv[:, h, 0, i0:i0 + 4].unsqueeze(2), [128, 4, 128]),
                                                                             in1=bc(scv[:, h, 1, :].unsqueeze(1), [128, 4, 128]), op=ALU.mult), reads=["bsc"], writes=["pP%d_%d" % (hs, il) for il in range(4)])
                    else:
                        for il in range(4):
                            S.op("act", lambda e, h=h, hs=hs, scv=scv, il=il: e.activation(out=Pt[hs][:, il * 128:(il + 1) * 128], in_=scv[:, h, 1, :], func=AF.Copy, scale=scv[:, h, 0, i0 + il:i0 + il + 1]),
                                 reads=["bsc"], writes=["pP%d_%d" % (hs, il)])
                    S.op("dve", lambda e, h=h, hs=hs: e.scalar_tensor_tensor(out=Wh[ub][h][:], in0=Pt[hs][:], scalar=kap[:, tt, h:h + 1], in1=Pt[hs][:], op0=ALU.is_ge, op1=ALU.mult),
                         reads=["pP%d_%d" % (hs, il) for il in range(4)] + ["btau"], writes=["pWh%d_%d" % (ub, h)])

            def stage1b(u, tg, eb, sub, tt, es_):
                ub = u % 2; gb = u % 3
                S.op("act", lambda e: e.activation(out=G[gb][:], in_=bank[ub][:], func=AF.Gelu), reads=["pbb%d" % ub], writes=["pG%d" % gb])
                for h in range(8):
                    S.op("pe", lambda e, h=h: e.matmul(bank[2 + ub][:], lhsT=idb[:], rhs=Wh[ub][h][:], start=(h == 0), stop=(h == 7)),
                         reads=["identb", "pWh%d_%d" % (ub, h)], writes=["pbb%d" % (2 + ub)])

            def stage2(u, tg, eb, sub, tt, es_):
                ub = u % 2; gb = u % 3
                first = (eb == 0)
                S.op("dve", lambda e: e.tensor_tensor(out=WA[ub][:], in0=bank[2 + ub][:], in1=G[gb][:], op=ALU.mult), reads=["pbb%d" % (2 + ub), "pG%d" % gb], writes=["pWA%d" % ub])
                pbt = bank[4][:].bitcast(BF16).rearrange("p (c n) -> p c n", n=128)[:, 0:4, :]
                transpose_chunks(C, WA[ub][:], "pWA%d" % ub, 4, pbt, "pbb4", WAT[ub][:], "pWAT%d" % ub, idb)
                for cb in range(2):
                    for ec in range(4):
                        S.op("pe", lambda e, cb=cb, ec=ec: e.matmul(bank[5 + cb][:], lhsT=WAT[ub][:, ec, :], rhs=Vb[es_][:, sub * 4 + ec, cb * 512:(cb + 1) * 512], start=(sub == 0 and ec == 0), stop=(sub == 1 and ec == 3)),
                             reads=["pWAT%d" % ub, "Vb%d" % es_], writes=["pbb%d" % (5 + cb)])

            def stage2b(u, tg, eb, sub, tt, es_):
                first = (eb == 0)
                if sub != 1:
                    return
                for cb in range(2):
                    if first:
                        S.op("dve", lambda e, cb=cb: e.tensor_copy(out=acc[:, tt, cb * 512:(cb + 1) * 512], in_=bank[5 + cb][:]), reads=["pbb%d" % (5 + cb)], writes=["pacc%d" % tt])
                    else:
                        S.op("dve", lambda e, cb=cb: e.tensor_tensor(out=acc[:, tt, cb * 512:(cb + 1) * 512], in0=bank[5 + cb][:], in1=acc[:, tt, cb * 512:(cb + 1) * 512], op=ALU.add),
                             reads=["pbb%d" % (5 + cb), "pacc%d" % tt], writes=["pacc%d" % tt])

            u = 0
            for tg in range(4):
                S.dma("sp", xnT[:], D["xnT_d"][tg * 4:(tg + 1) * 4].rearrange("n p f -> p n f"), reads=["xnT_d"], writes=["bxnT"])
                S.dma("sp", sc[:], D["sc_d"][tg * 4:(tg + 1) * 4].rearrange("n p f -> p n f"), reads=["sc_d"], writes=["bsc"])
                S.dma("sp", kap[:], D["tau_d"][tg * 4:(tg + 1) * 4].rearrange("n p f -> p n f"), reads=["tau_d"], writes=["btau"])
                units = []
                for eb in range(16):
                    es_ = (tg * 16 + eb) % 2
                    for tt in range(4):
                        for sub in range(2):
                            units.append((u, tg, eb, sub, tt, es_)); u += 1
                loaded = set()
                n = len(units)
                for k in range(n + 2):
                    for ebk in ([0] if k == 0 else []) + ([k // 8 + 1] if (k % 8 == 2 and k // 8 + 1 < 16) else []):
                        es_ = (tg * 16 + ebk) % 2
                        S.dma("sp", UT[es_][:], uTv[ebk], reads=["uT_b"], writes=["UT%d" % es_])
                        S.dma("sp", Vb[es_][:], vbv[ebk], reads=["v_b"], writes=["Vb%d" % es_])
                    if 0 <= k - 2 < n:
                        stage2(*units[k - 2])
                    if 0 <= k - 1 < n:
                        stage1b(*units[k - 1])
                    if k < n:
                        stage1a(*units[k])
                    if 0 <= k - 2 < n:
                        stage2b(*units[k - 2])
                for tt in range(4):
                    sl = tt % 2; n = tg * 4 + tt
                    S.dma("sp", h2t[sl][:], hv[n], reads=["h2_d"], writes=["ph2t%d" % sl])
                    S.op("pool", lambda e, sl=sl, tt=tt: e.tensor_tensor(out=h2t[sl][:], in0=h2t[sl][:], in1=acc[:, tt, :], op=ALU.add), reads=["ph2t%d" % sl, "pacc%d" % tt], writes=["ph2t%d" % sl])
                    S.dma("sp", ov[n], h2t[sl][:], reads=["ph2t%d" % sl], writes=["out"])


_PROG = {}


def kernel(**inputs):
    sh = host_shared(inputs)
    sh.update(host_shared_rest(inputs))
    if "nc" not in _PROG:
        _PROG["nc"] = build_program(("mixer", "mem", "peer"))[0]
    nc = _PROG["nc"]
    names = set(IN_SHAPES) | set(IN_SHAPES_MEM) | set(IN_SHAPES_PEER)
    mem = np.asarray(inputs["mem"], np.float32)
    maps = []
    for c in range(8):
        m = {k: v for k, v in sh.items() if k in names}
        m.update(host_core(inputs, c))
        m["mem"] = np.ascontiguousarray(mem[c // 4])
        maps.append(m)
    res = run_bass_kernel_spmd(nc, maps, core_ids=list(range(8)))
    out = np.zeros((2, 8192, 1024), np.float32)
    for c in range(8):
        out[c // 4, (c % 4) * 2048:(c % 4 + 1) * 2048] = res.results[c]["out"]
    return out
```

```python
from contextlib import ExitStack, contextmanager
import numpy as np
import concourse.bass as bass
import concourse.mybir as mybir
from concourse.bass_utils import run_bass_kernel_spmd

F32 = mybir.dt.float32
BF16 = mybir.dt.bfloat16
I32 = mybir.dt.int32
AF = mybir.ActivationFunctionType
ALU = mybir.AluOpType
AX = mybir.AxisListType

ENGS = ("pe", "act", "dve", "pool", "sp")
TWO_PI = 6.283185307179586
EPS = 1e-6
NEG = -30000.0


class Sched:
    def __init__(self, nc, es, n_dma_sems=32):
        self.nc = nc
        self.streams = {e: [] for e in ENGS}
        self.sem = {e: es.enter_context(nc.semaphore("s_" + e)) for e in ("pe", "act", "dve", "pool")}
        self.cnt = {e: 0 for e in ("pe", "act", "dve", "pool")}
        self.dsem = [es.enter_context(nc.semaphore("s_dma%d" % i)) for i in range(n_dma_sems)]
        self.dcnt = [0] * n_dma_sems
        self.dnext = 0
        self.n_sw = 4
        self.dnext_sw = 0
        self.waited = {}
        self.last_w = {}
        self.readers = {}
        self.n_ops = 0

    def _deps(self, eng, reads, writes):
        deps = []
        for k in reads:
            if k in self.last_w:
                deps.append(self.last_w[k])
        for k in writes:
            if k in self.last_w:
                deps.append(self.last_w[k])
            deps.extend(self.readers.get(k, ()))
        need = {}
        for (sk, val, peng) in deps:
            if peng == "pe" and eng == "pe":
                continue
            if self.waited.get((eng, sk), 0) >= val:
                continue
            if need.get(sk, 0) < val:
                need[sk] = val
        return need

    def _semobj(self, sk):
        return self.sem[sk] if isinstance(sk, str) else self.dsem[sk]

    def _emit_waits(self, eng, need):
        for sk, val in need.items():
            self.waited[(eng, sk)] = val
            so = self._semobj(sk)
            self.streams[eng].append(lambda e, so=so, val=val: e.wait_ge(so, val))

    def _record(self, tok, reads, writes):
        for k in writes:
            self.last_w[k] = tok
            self.readers[k] = []
        for k in reads:
            if k not in writes:
                self.readers.setdefault(k, []).append(tok)

    def op(self, eng, fn, reads=(), writes=()):
        need = self._deps(eng, reads, writes)
        self._emit_waits(eng, need)
        self.cnt[eng] += 1
        val = self.cnt[eng]
        so = self.sem[eng]
        self.streams[eng].append(lambda e, fn=fn, so=so: fn(e).then_inc(so, 1))
        self._record((eng, val, eng), reads, writes)
        self.n_ops += 1

    def dma(self, q, out, in_, reads=(), writes=(), **kw):
        nhw = len(self.dsem) - self.n_sw
        if q == "pool":
            i = nhw + self.dnext_sw
            self.dnext_sw = (self.dnext_sw + 1) % self.n_sw
        else:
            i = self.dnext
            self.dnext = (self.dnext + 1) % nhw
        need = self._deps(q, reads, writes)
        prev = 16 * self.dcnt[i]
        if prev and self.waited.get((q, i), 0) < prev:
            need[i] = max(need.get(i, 0), prev)
        self._emit_waits(q, need)
        self.dcnt[i] += 1
        val = 16 * self.dcnt[i]
        so = self.dsem[i]
        self.streams[q].append(
            lambda e, out=out, in_=in_, so=so, kw=kw: e.dma_start(out=out, in_=in_, **kw).then_inc(so, 16))
        self._record((i, val, "dma"), reads, writes)
        self.n_ops += 1

    def barrier(self):
        for eng in ENGS:
            need = {}
            for pe_ in ("pe", "act", "dve", "pool"):
                v = self.cnt[pe_]
                if v and self.waited.get((eng, pe_), 0) < v:
                    need[pe_] = v
            for i, c in enumerate(self.dcnt):
                if c and self.waited.get((eng, i), 0) < 16 * c:
                    need[i] = 16 * c
            self._emit_waits(eng, need)

    def wait_all(self, eng, keys):
        need = {}
        for k in keys:
            if k in self.last_w:
                sk, val, _ = self.last_w[k]
                if self.waited.get((eng, sk), 0) < val and need.get(sk, 0) < val:
                    need[sk] = val
        self._emit_waits(eng, need)

    def emit(self):
        if not any(self.streams[e] for e in ENGS):
            return
        streams = self.streams
        self.streams = {e: [] for e in ENGS}
        self._emit_block(streams)

    def _emit_block(self, streams):
        self_streams = streams
        with self.nc.Block() as block:
            @block.tensor
            def _(e):
                for f in self_streams["pe"]:
                    f(e)

            @block.scalar
            def _(e):
                for f in self_streams["act"]:
                    f(e)

            @block.vector
            def _(e):
                for f in self_streams["dve"]:
                    f(e)

            @block.gpsimd
            def _(e):
                for f in self_streams["pool"]:
                    f(e)

            @block.sync
            def _(e):
                for f in self_streams["sp"]:
                    f(e)


class Ctx:
    def __init__(self, nc, S):
        self.nc = nc
        self.S = S
        self.uid = 0

    def sb(self, es, name, shape, dt=F32):
        self.uid += 1
        return es.enter_context(self.nc.sbuf_tensor("%s_%d" % (name, self.uid), list(shape), dt))

    def ps(self, es, name, shape, dt=F32):
        self.uid += 1
        return es.enter_context(self.nc.psum_tensor("%s_%d" % (name, self.uid), list(shape), dt))


@contextmanager
def scope(C):
    with ExitStack() as es:
        yield es
        C.S.barrier()
        C.S.emit()


def bc(ap, shape):
    return ap.to_broadcast(list(shape))


def make_ident(C, es, name="ident"):
    S = C.S
    idf = C.sb(es, name + "f", [128, 128])
    idb = C.sb(es, name + "b", [128, 128], BF16)
    S.op("pool", lambda e: e.memset(idf[:], 1.0), writes=[name + "f"])
    S.op("pool", lambda e: e.affine_select(out=idf[:], in_=idf[:], pattern=[[-1, 128]], compare_op=ALU.is_equal,
                                           fill=0.0, base=0, channel_multiplier=1), reads=[name + "f"], writes=[name + "f"])
    S.op("dve", lambda e: e.tensor_copy(out=idb[:], in_=idf[:]), reads=[name + "f"], writes=[name + "b"])
    return idf, idb


def sincos(C, es, ang, n, tag):
    S = C.S
    outs = []
    for which, off in (("s", 64.0), ("c", 64.25)):
        k = tag + which
        y = C.sb(es, k + "y", [128, n]); yi = C.sb(es, k + "yi", [128, n], I32); yf = C.sb(es, k + "yf", [128, n])
        m = C.sb(es, k + "m", [128, n]); o = C.sb(es, k + "o", [128, n])
        S.op("dve", lambda e, y=y, off=off: e.tensor_scalar(out=y[:], in0=ang, scalar1=1.0 / TWO_PI, scalar2=off, op0=ALU.mult, op1=ALU.add),
             reads=[tag + "ang"], writes=[k + "y"])
        S.op("dve", lambda e, y=y, yi=yi: e.tensor_copy(out=yi[:], in_=y[:]), reads=[k + "y"], writes=[k + "yi"])
        S.op("dve", lambda e, yi=yi, yf=yf: e.tensor_copy(out=yf[:], in_=yi[:]), reads=[k + "yi"], writes=[k + "yf"])
        S.op("dve", lambda e, y=y, yf=yf: e.tensor_tensor(out=y[:], in0=y[:], in1=yf[:], op=ALU.subtract), reads=[k + "y", k + "yf"], writes=[k + "y"])
        S.op("dve", lambda e, y=y, m=m: e.tensor_scalar(out=m[:], in0=y[:], scalar1=0.5, scalar2=None, op0=ALU.is_gt), reads=[k + "y"], writes=[k + "m"])
        S.op("dve", lambda e, y=y, m=m: e.tensor_tensor(out=y[:], in0=y[:], in1=m[:], op=ALU.subtract), reads=[k + "y", k + "m"], writes=[k + "y"])
        S.op("act", lambda e, y=y, o=o: e.activation(out=o[:], in_=y[:], func=AF.Sin, scale=TWO_PI), reads=[k + "y"], writes=[k + "o"])
        outs.append((o, k + "o"))
    return outs


def s5_params(C, es_keep, D):
    S, nc = C.S, C.nc
    P = {}
    LT2 = C.sb(es_keep, "LT2", [128, 8, 8, 2, 128], BF16)
    WPr = C.sb(es_keep, "WPr", [128, 9, 16]); WPi = C.sb(es_keep, "WPi", [128, 9, 16]); WPn = C.sb(es_keep, "WPn", [128, 9, 16])
    P.update(LT2=LT2, WPr=WPr, WPi=WPi, WPn=WPn)
    with scope(C) as es:
        sb = lambda name, shape, dt=F32: C.sb(es, name, shape, dt)
        Mz2 = sb("Mz2", [128, 16, 8, 128], BF16)
        CA = sb("CA", [128, 32, 128], BF16); CAs = sb("CAs", [128, 32, 128], BF16)
        LR = sb("LR", [128, 32]); LI = sb("LI", [128, 32]); LS = sb("LS", [128, 32])
        SG = sb("SG", [128, 2]); NV = sb("NV", [128, 23])
        P1B = sb("P1B", [128, 32, 16]); P2B = sb("P2B", [128, 32, 16]); P1C = sb("P1C", [128, 32, 16]); P2C = sb("P2C", [128, 32, 16])
        DD = sb("DD", [128, 16, 16])
        for t, nm in ((LR, "s5_lr"), (LI, "s5_li"), (LS, "s5_ls"), (SG, "s5_sg"), (NV, "s5_nv")):
            S.dma("sp", t[:], D[nm], writes=[nm])
        S.dma("sp", DD[:].rearrange("p e c -> p (e c)"), D["s5_dd"], writes=["s5_dd"])
        for t, nm in ((P1B, "s5_p1b"), (P2B, "s5_p2b"), (P1C, "s5_p1c"), (P2C, "s5_p2c")):
            S.dma("sp", t[:].rearrange("p g c -> p (g c)"), D[nm], writes=[nm])
        idf, idb = make_ident(C, es, "pid")
        STEP = sb("STEP", [128, 32]); AA = sb("AA", [128, 32]); PH = sb("PH", [128, 32])
        S.op("act", lambda e: e.activation(out=STEP[:], in_=LS[:], func=AF.Exp), reads=["s5_ls"], writes=["STEP"])
        S.op("dve", lambda e: e.tensor_tensor(out=AA[:], in0=LR[:], in1=STEP[:], op=ALU.mult), reads=["s5_lr", "STEP"], writes=["AA"])
        S.op("dve", lambda e: e.tensor_tensor(out=PH[:], in0=LI[:], in1=STEP[:], op=ALU.mult), reads=["s5_li", "STEP"], writes=["PH"])
        EXPO = sb("EXPO", [128, 32, 23]); ANG = sb("ANG", [128, 32, 23]); MAG = sb("MAG", [128, 32, 23])
        nvb = bc(NV[:].unsqueeze(1), [128, 32, 23])
        S.op("dve", lambda e: e.tensor_tensor(out=EXPO[:], in0=bc(AA[:].unsqueeze(2), [128, 32, 23]), in1=nvb, op=ALU.mult), reads=["AA", "s5_nv"], writes=["EXPO"])
        S.op("dve", lambda e: e.tensor_tensor(out=ANG[:], in0=bc(PH[:].unsqueeze(2), [128, 32, 23]), in1=nvb, op=ALU.mult), reads=["PH", "s5_nv"], writes=["pwang"])
        S.op("act", lambda e: e.activation(out=MAG[:], in_=EXPO[:], func=AF.Exp), reads=["EXPO"], writes=["MAG"])
        (sn, snk), (cs, csk) = sincos(C, es, ANG[:].rearrange("p g n -> p (g n)"), 32 * 23, "pw")
        CR = sb("CR", [128, 32, 23]); CI = sb("CI", [128, 32, 23])
        S.op("dve", lambda e: e.tensor_tensor(out=CR[:].rearrange("p g n -> p (g n)"), in0=MAG[:].rearrange("p g n -> p (g n)"), in1=cs[:], op=ALU.mult), reads=["MAG", csk], writes=["CR"])
        S.op("dve", lambda e: e.tensor_tensor(out=CI[:].rearrange("p g n -> p (g n)"), in0=MAG[:].rearrange("p g n -> p (g n)"), in1=sn[:], op=ALU.mult), reads=["MAG", snk], writes=["CI"])
        zr = sb("zr", [128, 32]); den = sb("den", [128, 32]); t0 = sb("t0", [128, 32]); fr = sb("fr", [128, 32]); fi = sb("fi", [128, 32])
        S.op("dve", lambda e: e.tensor_scalar(out=zr[:], in0=CR[:, :, 8], scalar1=-1.0, scalar2=None, op0=ALU.add), reads=["CR"], writes=["zr"])
        S.op("dve", lambda e: e.tensor_tensor(out=den[:], in0=LR[:], in1=LR[:], op=ALU.mult), reads=["s5_lr"], writes=["den"])
        S.op("dve", lambda e: e.tensor_tensor(out=t0[:], in0=LI[:], in1=LI[:], op=ALU.mult), reads=["s5_li"], writes=["t0"])
        S.op("dve", lambda e: e.tensor_tensor(out=den[:], in0=den[:], in1=t0[:], op=ALU.add), reads=["den", "t0"], writes=["den"])
        S.op("dve", lambda e: e.reciprocal(out=den[:], in_=den[:]), reads=["den"], writes=["den"])
        S.op("dve", lambda e: e.tensor_tensor(out=fr[:], in0=zr[:], in1=LR[:], op=ALU.mult), reads=["zr", "s5_lr"], writes=["fr"])
        S.op("dve", lambda e: e.tensor_tensor(out=t0[:], in0=CI[:, :, 8], in1=LI[:], op=ALU.mult), reads=["CI", "s5_li", "den"], writes=["t0"])
        S.op("dve", lambda e: e.tensor_tensor(out=fr[:], in0=fr[:], in1=t0[:], op=ALU.add), reads=["fr", "t0"], writes=["fr"])
        S.op("dve", lambda e: e.tensor_tensor(out=fr[:], in0=fr[:], in1=den[:], op=ALU.mult), reads=["fr", "den"], writes=["fr"])
        S.op("dve", lambda e: e.tensor_tensor(out=fi[:], in0=CI[:, :, 8], in1=LR[:], op=ALU.mult), reads=["CI", "s5_lr"], writes=["fi"])
        S.op("dve", lambda e: e.tensor_tensor(out=t0[:], in0=zr[:], in1=LI[:], op=ALU.mult), reads=["zr", "s5_li", "fr"], writes=["t0"])
        S.op("dve", lambda e: e.tensor_tensor(out=fi[:], in0=fi[:], in1=t0[:], op=ALU.subtract), reads=["fi", "t0"], writes=["fi"])
        S.op("dve", lambda e: e.tensor_tensor(out=fi[:], in0=fi[:], in1=den[:], op=ALU.mult), reads=["fi", "den"], writes=["fi"])
        BB1 = sb("BB1", [128, 32, 16]); BB2 = sb("BB2", [128, 32, 16]); ta = sb("ta", [128, 32, 16]); tb = sb("tb", [128, 32, 16])
        frb = bc(fr[:].unsqueeze(2), [128, 32, 16]); fib = bc(fi[:].unsqueeze(2), [128, 32, 16])
        fl = lambda t: t[:].rearrange("p g c -> p (g c)")
        S.op("dve", lambda e: e.tensor_tensor(out=ta[:], in0=P1B[:], in1=frb, op=ALU.mult), reads=["s5_p1b", "fr"], writes=["ta"])
        S.op("dve", lambda e: e.tensor_tensor(out=tb[:], in0=P2B[:], in1=fib, op=ALU.mult), reads=["s5_p2b", "fi"], writes=["tb"])
        S.op("dve", lambda e: e.scalar_tensor_tensor(out=fl(BB1), in0=fl(tb), scalar=SG[:, 0:1], in1=fl(ta), op0=ALU.mult, op1=ALU.add), reads=["ta", "tb", "s5_sg"], writes=["BB1"])
        S.op("dve", lambda e: e.tensor_tensor(out=ta[:], in0=P2B[:], in1=frb, op=ALU.mult), reads=["s5_p2b", "fr", "BB1"], writes=["ta"])
        S.op("dve", lambda e: e.tensor_tensor(out=tb[:], in0=P1B[:], in1=fib, op=ALU.mult), reads=["s5_p1b", "fi", "BB1"], writes=["tb"])
        S.op("dve", lambda e: e.scalar_tensor_tensor(out=fl(BB2), in0=fl(tb), scalar=SG[:, 1:2], in1=fl(ta), op0=ALU.mult, op1=ALU.add), reads=["ta", "tb", "s5_sg"], writes=["BB2"])
        t5 = sb("t5", [128, 32, 16]); t6 = sb("t6", [128, 32, 16])
        Rm = sb("Rm", [128, 32, 8, 16])
        Q1 = sb("Q1", [128, 32, 16]); Q2 = sb("Q2", [128, 32, 16])
        S.op("dve", lambda e: e.tensor_scalar(out=fl(Q1), in0=fl(P1C), scalar1=SG[:, 1:2], scalar2=None, op0=ALU.mult), reads=["s5_p1c", "s5_sg"], writes=["Q1"])
        S.op("dve", lambda e: e.tensor_scalar(out=fl(Q2), in0=fl(P2C), scalar1=SG[:, 0:1], scalar2=None, op0=ALU.mult), reads=["s5_p2c", "s5_sg"], writes=["Q2"])
        CAv = CA[:].rearrange("p g (t c) -> p g t c", c=16); CAsv = CAs[:].rearrange("p g (t c) -> p g t c", c=16)
        for t in range(8):
            for (dst, dkey, a_, akey, b_, bkey, idx) in ((Rm[:, :, t, :], "Rm", Q1, "Q1", P2C, "s5_p2c", 7 + t),
                                                         (CAv[:, :, t, :], "CA", Q1, "Q1", P2C, "s5_p2c", 15 + t),
                                                         (CAsv[:, :, t, :], "CAs", Q2, "Q2", P1C, "s5_p1c", 15 + t)):
                S.op("dve", lambda e, a_=a_, idx=idx: e.tensor_tensor(out=t5[:], in0=a_[:], in1=bc(CR[:, :, idx:idx + 1], [128, 32, 16]), op=ALU.mult), reads=[akey, "CR"], writes=["t5"])
                S.op("dve", lambda e, b_=b_, idx=idx: e.tensor_tensor(out=t6[:], in0=b_[:], in1=bc(CI[:, :, idx:idx + 1], [128, 32, 16]), op=ALU.mult), reads=[bkey, "CI"], writes=["t6"])
                S.op("dve", lambda e, dst=dst: e.tensor_tensor(out=dst, in0=t5[:], in1=t6[:], op=ALU.subtract), reads=["t5", "t6"], writes=[dkey])
        Wr = sb("Wr", [128, 9, 32]); Wi = sb("Wi", [128, 9, 32]); sq = sb("sq", [128, 32])
        S.op("dve", lambda e: e.tensor_copy(out=Wr[:, 0, :], in_=CR[:, :, 15]), reads=["CR"], writes=["Wr"])
        S.op("dve", lambda e: e.tensor_copy(out=Wi[:, 0, :], in_=CI[:, :, 15]), reads=["CI"], writes=["Wi"])
        for j in range(8):
            S.op("dve", lambda e, j=j: e.tensor_tensor(out=Wr[:, j + 1, :], in0=Wr[:, j, :], in1=Wr[:, j, :], op=ALU.mult), reads=["Wr"], writes=["Wr"])
            S.op("dve", lambda e, j=j: e.tensor_tensor(out=sq[:], in0=Wi[:, j, :], in1=Wi[:, j, :], op=ALU.mult), reads=["Wi"], writes=["sq"])
            S.op("dve", lambda e, j=j: e.tensor_tensor(out=Wr[:, j + 1, :], in0=Wr[:, j + 1, :], in1=sq[:], op=ALU.subtract), reads=["Wr", "sq"], writes=["Wr"])
            S.op("dve", lambda e, j=j: e.scalar_tensor_tensor(out=Wi[:, j + 1, :], in0=Wr[:, j, :], scalar=2.0, in1=Wi[:, j, :], op0=ALU.mult, op1=ALU.mult), reads=["Wr", "Wi"], writes=["Wi"])
        Wrv = Wr[:].rearrange("p j (a r w) -> p j a r w", r=2, w=2); Wiv = Wi[:].rearrange("p j (a r w) -> p j a r w", r=2, w=2)
        for r in range(2):
            pr_ = slice(64 * r, 64 * r + 64)
            for j in range(9):
                S.op("dve", lambda e, r=r, pr_=pr_, j=j: e.tensor_copy(out=WPr[pr_, j, :].rearrange("p (a w) -> p a w", w=2), in_=Wrv[pr_, j, :, r, :]), reads=["Wr"], writes=["WPr"])
                S.op("dve", lambda e, r=r, pr_=pr_, j=j: e.tensor_copy(out=WPi[pr_, j, :].rearrange("p (a w) -> p a w", w=2), in_=Wiv[pr_, j, :, r, :]), reads=["Wi"], writes=["WPi"])
        S.op("dve", lambda e: e.tensor_scalar(out=WPn[:].rearrange("p j q -> p (j q)"), in0=WPi[:].rearrange("p j q -> p (j q)"), scalar1=-1.0, scalar2=None, op0=ALU.mult), reads=["WPi"], writes=["WPn"])
        E = [[sb("E%d%d" % (h_, r), [128, 128]) for r in range(2)] for h_ in range(2)]
        for h_ in range(2):
            for r in range(2):
                S.op("pool", lambda e, h_=h_, r=r: e.memset(E[h_][r][:], 0.0), writes=["E%d%d" % (h_, r)])
                S.op("pool", lambda e, h_=h_, r=r: e.tensor_copy(out=E[h_][r][64 * h_:64 * h_ + 64, 64 * r:64 * r + 64], in_=idf[64 * h_:64 * h_ + 64, 64 * h_:64 * h_ + 64]),
                     reads=["pidf", "E%d%d" % (h_, r)], writes=["E%d%d" % (h_, r)])
        Lz = [sb("Lz%d" % i, [128, 32, 64]) for i in range(2)]
        for i in range(2):
            S.op("pool", lambda e, i=i: e.memset(Lz[i][:].rearrange("p g c -> p (g c)"), 0.0), writes=["Lz%d" % i])
        S.op("pool", lambda e: e.memset(Mz2[:].rearrange("p a b c -> p (a b c)"), 0.0), writes=["Mz2"])
        t5v = t5[:].rearrange("p (a j) c -> p a j c", j=4); t6v = t6[:].rearrange("p (a j) c -> p a j c", j=4)
        with scope(C) as esp:
            PM = C.ps(esp, "PM", [128, 16, 128]); PL = C.ps(esp, "PL", [128, 8, 2, 128])
            for s in range(8):
                i = 7 - s; sl = s % 2; lk = "Lz%d" % sl
                Lzv = Lz[sl][:].rearrange("p (a j) c -> p a j c", j=4)
                S.op("dve", lambda e, i=i: e.tensor_tensor(out=t5[:], in0=BB1[:], in1=bc(CR[:, :, i:i + 1], [128, 32, 16]), op=ALU.mult), reads=["BB1", "CR"], writes=["t5"])
                S.op("dve", lambda e, i=i: e.tensor_tensor(out=t6[:], in0=BB2[:], in1=bc(CI[:, :, i:i + 1], [128, 32, 16]), op=ALU.mult), reads=["BB2", "CI"], writes=["t6"])
                for j4 in range(4):
                    S.op("dve", lambda e, j4=j4, Lzv=Lzv: e.scalar_tensor_tensor(out=Lzv[:, :, j4, 16 * j4:16 * j4 + 16], in0=t6v[:, :, j4, :], scalar=SG[:, 0:1], in1=t5v[:, :, j4, :], op0=ALU.mult, op1=ALU.add),
                         reads=["t5", "t6", "s5_sg"], writes=[lk])
                for g in range(32):
                    chc = g // 8; hb = (g % 8) // 4; j4 = g % 4; e_ = chc * 4 + j4
                    rows = slice(64 * hb, 64 * hb + 64)
                    S.op("pe", lambda e, g=g, sl=sl, e_=e_, rows=rows: e.matmul(PM[rows, e_, :], lhsT=Lz[sl][:, g, :], rhs=Rm[:, g, :, :].rearrange("p t c -> p (t c)"), start=True, stop=True),
                         reads=[lk, "Rm"], writes=["PM"])
                for pr in range(16):
                    a_ = pr // 2; wp = pr % 2; chc = a_ // 2; hb = a_ % 2; e2 = chc * 2 + wp
                    rows = slice(64 * hb, 64 * hb + 64)
                    for h_ in range(2):
                        for r in range(2):
                            g = 4 * a_ + 2 * r + wp
                            S.op("pe", lambda e, g=g, sl=sl, e2=e2, rows=rows, h_=h_, r=r: e.matmul(PL[rows, e2, h_, :], lhsT=Lz[sl][:, g, :], rhs=E[h_][r][:], start=(r == 0), stop=(r == 1)),
                                 reads=[lk, "E%d%d" % (h_, r)], writes=["PL"])
                S.op("dve", lambda e, s=s: e.tensor_copy(out=Mz2[:, :, s, 16 * s:128], in_=PM[:, :, 16 * s:128]), reads=["PM"], writes=["Mz2"])
                S.op("dve", lambda e, s=s: e.tensor_tensor(out=Mz2[:, :, s, 16 * s:16 * s + 16], in0=PM[:, :, 16 * s:16 * s + 16], in1=DD[:], op=ALU.add), reads=["PM", "s5_dd"], writes=["Mz2"])
                S.op("act", lambda e, s=s: e.copy(out=LT2[:, :, s, :, :], in_=PL[:]), reads=["PL"], writes=["LT2"])
        S.dma("sp", D["Mz_d"], Mz2[:].rearrange("p a b c -> p (a b c)"), reads=["Mz2"], writes=["Mz_d"])
        S.dma("sp", D["CA_d"][:, 0, :], CA[:].rearrange("p g c -> p (g c)"), reads=["CA"], writes=["CA_d"])
        S.dma("sp", D["CA_d"][:, 1, :], CAs[:].rearrange("p g c -> p (g c)"), reads=["CAs"], writes=["CA_d"])
    return P


def load_weight_bf16(C, es, es_tmp, name, src, rows_chunks, ncols, gcol=None, q="sp"):
    S = C.S
    W = C.sb(es, name, [128, rows_chunks, ncols], BF16)
    stg = [C.sb(es_tmp, name + "_stg%d" % i, [128, ncols]) for i in range(2)]
    srcv = src.rearrange("(c p) n -> c p n", p=128)
    for c in range(rows_chunks):
        st = stg[c % 2]; sk = name + "_stg%d" % (c % 2)
        S.dma(q, st[:], srcv[c], writes=[sk])
        eng = "dve" if c % 2 == 0 else "pool"
        if gcol is not None:
            S.op(eng, lambda e, st=st, c=c: e.tensor_scalar(out=W[:, c, :], in0=st[:], scalar1=gcol[0][:, c:c + 1], scalar2=None, op0=ALU.mult),
                 reads=[sk, gcol[1]], writes=[name])
        else:
            S.op(eng, lambda e, st=st, c=c: e.tensor_copy(out=W[:, c, :], in_=st[:]), reads=[sk], writes=[name])
    return W


def rms_rstd(C, x_ap, xkey, junk, jkey, ss, rs, skey, n):
    S = C.S
    S.op("act", lambda e: e.activation(out=junk, in_=x_ap, func=AF.Square, accum_out=ss), reads=[xkey], writes=[skey + "_ss"])
    S.op("act", lambda e: e.activation(out=rs, in_=ss, func=AF.Sqrt, scale=1.0 / n, bias=EPS), reads=[skey + "_ss"], writes=[skey + "_sq"])
    S.op("dve", lambda e: e.reciprocal(out=rs, in_=rs), reads=[skey + "_sq"], writes=[skey])


def transpose_chunks(C, src, skey, nch, pbank, pkey, dst, dkey, idb, evac="act"):
    S = C.S
    for c in range(nch):
        S.op("pe", lambda e, c=c: e.transpose(out=pbank[:, c, :], in_=src[:, c * 128:(c + 1) * 128], identity=idb[:]), reads=[skey, "identb"], writes=[pkey])
    if evac == "act":
        S.op("act", lambda e: e.copy(out=dst, in_=pbank), reads=[pkey], writes=[dkey])
    else:
        S.op(evac, lambda e: e.tensor_copy(out=dst, in_=pbank), reads=[pkey], writes=[dkey])


def stage_mixer(C, D, dbg=None, upto=9, prep=None):
    S, nc = C.S, C.nc
    dbg = dbg or {}
    with scope(C) as es1:
        P = s5_params(C, es1, D)
        idf = C.sb(es1, "identf", [128, 128]); idb = C.sb(es1, "identb", [128, 128], BF16)
        S.op("pool", lambda e: e.memset(idf[:], 1.0), writes=["identf"])
        S.op("pool", lambda e: e.affine_select(out=idf[:], in_=idf[:], pattern=[[-1, 128]], compare_op=ALU.is_equal, fill=0.0, base=0, channel_multiplier=1), reads=["identf"], writes=["identf"])
        S.op("dve", lambda e: e.tensor_copy(out=idb[:], in_=idf[:]), reads=["identf"], writes=["identb"])
        if "WPr" in dbg:
            for nm in ("WPr", "WPi"):
                S.dma("sp", dbg[nm], P[nm][:].rearrange("p a b -> p (a b)"), reads=[nm], writes=["o_" + nm])
            S.dma("pool", dbg["LT2"], P["LT2"][:].rearrange("p a b c d -> p (a b c d)"), reads=["LT2"], writes=["o_LT2"])
            S.dma("pool", dbg["Mz"], D["Mz_d"], reads=["Mz_d"], writes=["o_Mz"])
            S.dma("pool", dbg["CA"], D["CA_d"].rearrange("p a b -> p (a b)"), reads=["CA_d"], writes=["o_CA"])
        if upto < 1:
            return
        carry_r = C.sb(es1, "carry_r", [128, 16]); carry_i = C.sb(es1, "carry_i", [128, 16])
        S.op("pool", lambda e: e.memset(carry_r[:], 0.0), writes=["carry_r"])
        S.op("pool", lambda e: e.memset(carry_i[:], 0.0), writes=["carry_i"])
        TRE = C.sb(es1, "TRE", [128, 16, 257]); TIM = C.sb(es1, "TIM", [128, 16, 257])
        uT = C.sb(es1, "uT", [128, 4, 8, 256], BF16)
        with scope(C) as es2:
            _mixer_passes(C, es2, D, P, idb, carry_r, carry_i, TRE, TIM, uT, dbg)
        if "carry" in dbg:
            S.dma("sp", dbg["carry"][:, 0:16], carry_r[:], reads=["carry_r"], writes=["o_carry"])
            S.dma("sp", dbg["carry"][:, 16:32], carry_i[:], reads=["carry_i"], writes=["o_carry2"])
        if upto < 2:
            return
        with scope(C) as es3:
            ytm = C.sb(es3, "ytm", [128, 2, 8, 512], BF16)
            with scope(C) as es4:
                _s5_scan_out(C, es4, D, P, idb, TRE, TIM, uT, ytm, carry_r, carry_i, dbg)
            if upto < 3:
                return
            _s5_glu_out(C, es3, D, idb, ytm, dbg)
    if upto < 4:
        return
    with scope(C) as es5:
        idf = C.sb(es5, "identf", [128, 128]); idb = C.sb(es5, "identb", [128, 128], BF16)
        S.op("pool", lambda e: e.memset(idf[:], 1.0), reads=[], writes=["identf"])
        S.op("pool", lambda e: e.affine_select(out=idf[:], in_=idf[:], pattern=[[-1, 128]], compare_op=ALU.is_equal, fill=0.0, base=0, channel_multiplier=1), reads=["identf"], writes=["identf"])
        S.op("dve", lambda e: e.tensor_copy(out=idb[:], in_=idf[:]), reads=["identf"], writes=["identb"])
        _attention(C, es5, D, idb, dbg, prep)


def _mixer_passes(C, es, D, P, idb, carry_r, carry_i, TRE, TIM, uT, dbg):
    S = C.S
    sb = lambda name, shape, dt=F32: C.sb(es, name, shape, dt)
    gin = sb("gin", [128, 8])
    S.dma("sp", gin[:], D["g_mix"], writes=["gin"])
    with scope(C) as est:
        Wb = load_weight_bf16(C, es, est, "Wb", D["w_in"], 8, 2048, gcol=(gin, "gin"))
    gq = sb("gq", [128, 64]); gk = sb("gk", [128, 64]); hv = sb("hv", [128, 4])
    S.dma("sp", gq[:], D["att_q_g"].partition_broadcast(128), writes=["gq"])
    S.dma("sp", gk[:], D["att_k_g"].partition_broadcast(128), writes=["gk"])
    S.dma("sp", hv[:], D["hvalid"], writes=["hv"])
    S.op("dve", lambda e: e.tensor_scalar(out=gq[:], in0=gq[:], scalar1=0.125, scalar2=None, op0=ALU.mult), reads=["gq"], writes=["gq"])
    xt = [sb("xt%d" % i, [128, 1024]) for i in range(2)]
    junk = sb("junk", [128, 1024]); st = [sb("st%d" % i, [128, 4]) for i in range(2)]
    xn = [sb("xn%d" % i, [128, 1024], BF16) for i in range(2)]
    xnT = [sb("xnT%d" % i, [128, 8, 512], BF16) for i in range(2)]
    qkv = sb("qkv", [128, 3, 512]); sq = sb("sq2", [128, 512]); qst = sb("qst", [128, 4, 8])
    qn = sb("qn", [128, 2, 512], BF16)
    kTs = [sb("kTs%d" % i, [128, 4, 128], BF16) for i in range(2)]; qTs = [sb("qTs%d" % i, [128, 4, 128], BF16) for i in range(2)]
    Vs = [sb("Vs%d" % i, [128, 8, 65], BF16) for i in range(2)]
    tA = [(sb("tAr%d" % i, [128, 256]), sb("tAi%d" % i, [128, 256])) for i in range(2)]
    tB = [(sb("tBr%d" % i, [128, 128]), sb("tBi%d" % i, [128, 128])) for i in range(2)]
    REDr = sb("REDr", [128, 16]); REDi = sb("REDi", [128, 16]); c1 = sb("c1", [128, 16]); c2 = sb("c2", [128, 16]); c3 = sb("c3", [128, 16])
    with scope(C) as esp:
        bank = [C.ps(esp, "bk%d" % i, [128, 512]) for i in range(8)]
        xv = D["x_ext"].rearrange("(n p) d -> n p d", p=128)
        tile_ctr = 0
        for q in range(4):
            for blk in range(4):
                bslot = (q * 4 + blk) % 2
                xT = xnT[bslot]; xTk = "xnT%d" % bslot
                for tt in range(4):
                    n_tile = q * 16 + blk * 4 + tt
                    sl = tile_ctr % 2; tile_ctr += 1
                    x_ = xt[sl]; xk = "xt%d" % sl
                    S.dma("sp", x_[:], xv[n_tile], writes=[xk])
                    rms_rstd(C, x_[:], xk, junk[:], "junk", st[sl][:, 0:1], st[sl][:, 1:2], "st%d" % sl, 1024)
                    S.op("dve", lambda e, x_=x_, sl=sl: e.tensor_scalar(out=xn[sl][:], in0=x_[:], scalar1=st[sl][:, 1:2], scalar2=None, op0=ALU.mult),
                         reads=[xk, "st%d" % sl], writes=["xn%d" % sl])
                    pb = bank[0][:].bitcast(BF16).rearrange("p (c n) -> p c n", n=128)
                    transpose_chunks(C, xn[sl][:], "xn%d" % sl, 8, pb, "bk0", xT[:, :, tt * 128:(tt + 1) * 128], xTk, idb)
                for chc in range(4):
                    bi = 1 + (chc % 2); bkk = "bk%d" % bi
                    for dc in range(8):
                        S.op("pe", lambda e, chc=chc, dc=dc, bi=bi, xT=xT: e.matmul(bank[bi][:], lhsT=Wb[:, dc, 1536 + chc * 128:1536 + (chc + 1) * 128], rhs=xT[:, dc, :], start=(dc == 0), stop=(dc == 7)),
                             reads=["Wb", xTk], writes=[bkk])
                    eng = "act" if chc % 2 == 0 else "dve"
                    src = bank[bi][:].rearrange("p (k s) -> p s k", s=8)
                    dst = uT[:, chc, :, blk * 64:(blk + 1) * 64]
                    if eng == "act":
                        S.op("act", lambda e, src=src, dst=dst: e.copy(out=dst, in_=src), reads=[bkk], writes=["uT"])
                    else:
                        S.op("dve", lambda e, src=src, dst=dst: e.tensor_copy(out=dst, in_=src), reads=[bkk], writes=["uT"])
                need_kv = (q == 3) or (q == 2 and blk == 3)
                need_q = (q == 3)
                if need_kv:
                    for tt in range(4):
                        n_tile = q * 16 + blk * 4 + tt
                        kvt = n_tile - 44
                        sl = kvt % 2
                        projs = [(1, 512, 3), (2, 1024, 4)] + ([(0, 0, 5)] if need_q else [])
                        for (pi, c0, bi) in projs:
                            for dc in range(8):
                                S.op("pe", lambda e, dc=dc, bi=bi, c0=c0, tt=tt, xT=xT: e.matmul(bank[bi][:], lhsT=xT[:, dc, tt * 128:(tt + 1) * 128], rhs=Wb[:, dc, c0:c0 + 512], start=(dc == 0), stop=(dc == 7)),
                                     reads=["Wb", xTk], writes=["bk%d" % bi])
                        S.op("act", lambda e, sl=sl: e.copy(out=Vs[sl][:, :, 0:64], in_=bank[4][:].rearrange("p (h d) -> p h d", d=64)), reads=["bk4"], writes=["Vs%d" % sl])
                        if kvt < 4:
                            S.op("pool", lambda e, sl=sl, kvt=kvt: e.tensor_copy(out=Vs[sl][:, :, 64], in_=bc(hv[:, kvt:kvt + 1], [128, 8])), reads=["hv"], writes=["Vs%d" % sl])
                        else:
                            S.op("pool", lambda e, sl=sl: e.memset(Vs[sl][:, :, 64], 1.0), reads=[], writes=["Vs%d" % sl])
                        S.dma("sp", D["V_d"][kvt], Vs[sl][:].rearrange("p h d -> p (h d)"), reads=["Vs%d" % sl], writes=["V_d"])
                        for (pi, bi, gt, gkey, dstT, dkey, dram, ncol_t) in ([(1, 3, gk, "gk", kTs[sl], "kTs%d" % sl, D["kT_d"], kvt)] +
                                                                          ([(0, 5, gq, "gq", qTs[sl], "qTs%d" % sl, D["qT_d"], kvt - 4)] if need_q else [])):
                            qs = qkv[:, pi, :]; qk_ = "qkv%d" % pi
                            S.op("act", lambda e, qs=qs, bi=bi: e.copy(out=qs, in_=bank[bi][:]), reads=["bk%d" % bi], writes=[qk_])
                            S.op("pool", lambda e, qs=qs: e.tensor_tensor(out=sq[:], in0=qs, in1=qs, op=ALU.mult), reads=[qk_], writes=["sq2"])
                            S.op("dve", lambda e, pi=pi: e.tensor_reduce(out=qst[:, pi, :], in_=sq[:].rearrange("p (h d) -> p h d", d=64), axis=AX.X, op=ALU.add), reads=["sq2"], writes=["qst%d" % pi])
                            S.op("act", lambda e, pi=pi: e.activation(out=qst[:, 2 + pi, :], in_=qst[:, pi, :], func=AF.Sqrt, scale=1.0 / 64, bias=EPS), reads=["qst%d" % pi], writes=["qsq%d" % pi])
                            S.op("dve", lambda e, pi=pi: e.reciprocal(out=qst[:, 2 + pi, :], in_=qst[:, 2 + pi, :]), reads=["qsq%d" % pi], writes=["qrs%d" % pi])
                            S.op("dve", lambda e, qs=qs, pi=pi: e.tensor_tensor(out=qs.rearrange("p (h d) -> p h d", d=64), in0=qs.rearrange("p (h d) -> p h d", d=64),
                                                                        in1=bc(qst[:, 2 + pi, :].unsqueeze(2), [128, 8, 64]), op=ALU.mult), reads=[qk_, "qrs%d" % pi], writes=[qk_])
                            S.op("pool", lambda e, qs=qs, pi=pi, gt=gt: e.tensor_tensor(out=qn[:, pi, :].rearrange("p (h d) -> p h d", d=64), in0=qs.rearrange("p (h d) -> p h d", d=64),
                                                                               in1=bc(gt[:].unsqueeze(1), [128, 8, 64]), op=ALU.mult), reads=[qk_, gkey], writes=["qn%d" % pi])
                            pb = bank[6][:].bitcast(BF16).rearrange("p (c n) -> p c n", n=128)[:, 0:4, :]
                            transpose_chunks(C, qn[:, pi, :], "qn%d" % pi, 4, pb, "bk6", dstT[:], dkey, idb)
                            S.dma("sp", dram.rearrange("p (c n) -> p c n", c=4)[:, :, ncol_t * 128:(ncol_t + 1) * 128], dstT[:], reads=[dkey], writes=["qkT_d"])
            for pair in range(16):
                psl = pair % 2
                br = bank[1 + 2 * psl]; bim = bank[2 + 2 * psl]; brk = "bk%d" % (1 + 2 * psl); bik = "bk%d" % (2 + 2 * psl)
                a_ = pair // 2; wp = pair % 2; chc = a_ // 2; hb = a_ % 2; e2 = chc * 2 + wp
                rows = slice(64 * hb, 64 * hb + 64)
                for half, (bkt, bkk) in enumerate(((br, brk), (bim, bik))):
                    for s in range(8):
                        S.op("pe", lambda e, e2=e2, s=s, half=half, rows=rows, chc=chc, bkt=bkt: e.matmul(bkt[:, 0:256], lhsT=P["LT2"][rows, e2, s, half, :], rhs=uT[rows, chc, s, :], start=(s == 0), stop=(s == 7)),
                             reads=["LT2", "uT"], writes=[bkk])
                if q < 3:
                    ar_, ai_ = tA[psl]; ark, aik = "tAr%d" % psl, "tAi%d" % psl
                    b_r, b_i = tB[psl]; brk2, bik2 = "tBr%d" % psl, "tBi%d" % psl
                    S.op("act", lambda e, ar_=ar_, br=br: e.copy(out=ar_[:], in_=br[:, 0:256]), reads=[brk], writes=[ark])
                    S.op("act", lambda e, ai_=ai_, bim=bim: e.copy(out=ai_[:], in_=bim[:, 0:256]), reads=[bik], writes=[aik])
                    src = (ar_, ai_, ark, aik); dst = (b_r, b_i, brk2, bik2)
                    for j in range(8):
                        n = 256 >> j; h = n // 2
                        sr, si, srk, sik = src; dr, di, drk, dik = dst
                        wr = P["WPr"][:, j, pair:pair + 1]; wi = P["WPi"][:, j, pair:pair + 1]; wn = P["WPn"][:, j, pair:pair + 1]
                        if j == 7:
                            odr, odi, odrk, odik = REDr[:, pair:pair + 1], REDi[:, pair:pair + 1], "REDr", "REDi"
                        else:
                            odr, odi, odrk, odik = dr[:, 0:h], di[:, 0:h], drk, dik
                        S.op("dve", lambda e, odr=odr, sr=sr, wr=wr, n=n: e.scalar_tensor_tensor(out=odr, in0=sr[:, 0:n:2], scalar=wr, in1=sr[:, 1:n:2], op0=ALU.mult, op1=ALU.add), reads=[srk, "WPr"], writes=[odrk])
                        S.op("dve", lambda e, odr=odr, si=si, wn=wn, n=n: e.scalar_tensor_tensor(out=odr, in0=si[:, 0:n:2], scalar=wn, in1=odr, op0=ALU.mult, op1=ALU.add), reads=[sik, "WPn", odrk], writes=[odrk])
                        S.op("dve", lambda e, odi=odi, si=si, wr=wr, n=n: e.scalar_tensor_tensor(out=odi, in0=si[:, 0:n:2], scalar=wr, in1=si[:, 1:n:2], op0=ALU.mult, op1=ALU.add), reads=[sik, "WPr"], writes=[odik])
                        S.op("dve", lambda e, odi=odi, sr=sr, wi=wi, n=n: e.scalar_tensor_tensor(out=odi, in0=sr[:, 0:n:2], scalar=wi, in1=odi, op0=ALU.mult, op1=ALU.add), reads=[srk, "WPi", odik], writes=[odik])
                        src, dst = dst, src
                else:
                    S.op("act", lambda e, pair=pair, br=br: e.copy(out=TRE[:, pair, 1:257], in_=br[:, 0:256]), reads=[brk], writes=["TRE%d" % pair])
                    S.op("act", lambda e, pair=pair, bim=bim: e.copy(out=TIM[:, pair, 1:257], in_=bim[:, 0:256]), reads=[bik], writes=["TIM%d" % pair])
            if q < 3:
                w8r = P["WPr"][:, 8, :]; w8i = P["WPi"][:, 8, :]
                S.op("dve", lambda e: e.tensor_tensor(out=c1[:], in0=w8r, in1=carry_r[:], op=ALU.mult), reads=["WPr", "carry_r"], writes=["c1"])
                S.op("dve", lambda e: e.tensor_tensor(out=c2[:], in0=w8i, in1=carry_i[:], op=ALU.mult), reads=["WPi", "carry_i"], writes=["c2"])
                S.op("dve", lambda e: e.tensor_tensor(out=c1[:], in0=c1[:], in1=c2[:], op=ALU.subtract), reads=["c1", "c2"], writes=["c1"])
                S.op("dve", lambda e: e.tensor_tensor(out=c1[:], in0=c1[:], in1=REDr[:], op=ALU.add), reads=["c1", "REDr"], writes=["c1"])
                S.op("dve", lambda e: e.tensor_tensor(out=c2[:], in0=w8r, in1=carry_i[:], op=ALU.mult), reads=["WPr", "carry_i", "c1"], writes=["c2"])
                S.op("dve", lambda e: e.tensor_tensor(out=c3[:], in0=w8i, in1=carry_r[:], op=ALU.mult), reads=["WPi", "carry_r"], writes=["c3"])
                S.op("dve", lambda e: e.tensor_tensor(out=c2[:], in0=c2[:], in1=c3[:], op=ALU.add), reads=["c2", "c3"], writes=["c2"])
                S.op("dve", lambda e: e.tensor_tensor(out=carry_i[:], in0=c2[:], in1=REDi[:], op=ALU.add), reads=["c2", "REDi"], writes=["carry_i"])
                S.op("dve", lambda e: e.tensor_copy(out=carry_r[:], in_=c1[:]), reads=["c1"], writes=["carry_r"])


def _s5_scan_out(C, es, D, P, idb, TRE, TIM, uT, ytm, carry_r, carry_i, dbg):
    S = C.S
    sb = lambda name, shape, dt=F32: C.sb(es, name, shape, dt)
    Mz2 = sb("Mz2s", [128, 16, 8, 128], BF16); CAA = sb("CAA", [128, 2, 32, 128], BF16)
    S.dma("sp", Mz2[:].rearrange("p a b c -> p (a b c)"), D["Mz_d"], reads=["Mz_d"], writes=["Mz2s"])
    S.dma("sp", CAA[:].rearrange("p a g c -> p a (g c)"), D["CA_d"], reads=["CA_d"], writes=["CAA"])
    tmr = [sb("tmr%d" % i, [128, 256]) for i in range(2)]; tmi = [sb("tmi%d" % i, [128, 256]) for i in range(2)]
    Tbr = [sb("Tbr%d" % i, [128, 256], BF16) for i in range(2)]; Tbi = [sb("Tbi%d" % i, [128, 256], BF16) for i in range(2)]
    Yg = [sb("Yg%d" % i, [128, 256], BF16) for i in range(2)]
    ysum = [sb("ysum%d" % i, [128, 256]) for i in range(2)]
    import os
    CUT = int(os.environ.get("SCAN_CUT", "9"))
    with scope(C) as esp:
        bank = [C.ps(esp, "sbk%d" % i, [128, 512]) for i in range(6)]
        for pair in range(16):
            sl = pair % 2
            a_ = pair // 2; wp = pair % 2; chc = a_ // 2; hb = a_ % 2
            rows = slice(64 * hb, 64 * hb + 64)
            kr, ki = "TRE%d" % pair, "TIM%d" % pair
            S.op("pool", lambda e, pair=pair: e.tensor_copy(out=TRE[:, pair, 0:1], in_=carry_r[:, pair:pair + 1]), reads=["carry_r"], writes=[kr])
            S.op("pool", lambda e, pair=pair: e.tensor_copy(out=TIM[:, pair, 0:1], in_=carry_i[:, pair:pair + 1]), reads=["carry_i"], writes=[ki])
            if CUT < 1:
                continue
            for j in range(9):
                d = 1 << j; m = 257 - d
                wr = P["WPr"][:, j, pair:pair + 1]; wi = P["WPi"][:, j, pair:pair + 1]; wn = P["WPn"][:, j, pair:pair + 1]
                tr = tmr[sl][:, 0:m]; ti = tmi[sl][:, 0:m]; trk = "tmr%d" % sl; tik = "tmi%d" % sl
                S.op("dve", lambda e, tr=tr, pair=pair, m=m, wr=wr: e.tensor_scalar(out=tr, in0=TRE[:, pair, 0:m], scalar1=wr, scalar2=None, op0=ALU.mult), reads=[kr, "WPr"], writes=[trk])
                S.op("dve", lambda e, tr=tr, pair=pair, m=m, wn=wn: e.scalar_tensor_tensor(out=tr, in0=TIM[:, pair, 0:m], scalar=wn, in1=tr, op0=ALU.mult, op1=ALU.add), reads=[ki, "WPn", trk], writes=[trk])
                S.op("dve", lambda e, ti=ti, pair=pair, m=m, wr=wr: e.tensor_scalar(out=ti, in0=TIM[:, pair, 0:m], scalar1=wr, scalar2=None, op0=ALU.mult), reads=[ki, "WPr"], writes=[tik])
                S.op("dve", lambda e, ti=ti, pair=pair, m=m, wi=wi: e.scalar_tensor_tensor(out=ti, in0=TRE[:, pair, 0:m], scalar=wi, in1=ti, op0=ALU.mult, op1=ALU.add), reads=[kr, "WPi", tik], writes=[tik])
                S.op("pool", lambda e, tr=tr, pair=pair, d=d: e.tensor_tensor(out=TRE[:, pair, d:257], in0=TRE[:, pair, d:257], in1=tr, op=ALU.add), reads=[kr, trk], writes=[kr])
                S.op("pool", lambda e, ti=ti, pair=pair, d=d: e.tensor_tensor(out=TIM[:, pair, d:257], in0=TIM[:, pair, d:257], in1=ti, op=ALU.add), reads=[ki, tik], writes=[ki])
            if CUT < 2:
                continue
            S.op("act", lambda e, pair=pair, sl=sl: e.copy(out=Tbr[sl][:], in_=TRE[:, pair, 0:256]), reads=[kr], writes=["Tbr%d" % sl])
            S.op("act", lambda e, pair=pair, sl=sl: e.copy(out=Tbi[sl][:], in_=TIM[:, pair, 0:256]), reads=[ki], writes=["Tbi%d" % sl])
            for r in range(2):
                g = 4 * a_ + 2 * r + wp
                e_ = chc * 4 + (g % 4)
                pr = slice(64 * r, 64 * r + 64)
                yb = bank[r]; ybk = "sbk%d" % r
                for s in range(8):
                    S.op("pe", lambda e, e_=e_, s=s, rows=rows, chc=chc, yb=yb: e.matmul(yb[:, 0:256], lhsT=Mz2[rows, e_, s, :], rhs=uT[rows, chc, s, :], start=(s == 0), stop=(s == 7)),
                         reads=["Mz2s", "uT"], writes=[ybk])
                zb_ = bank[4 + r]; zbk = "sbk%d" % (4 + r)
                S.op("pe", lambda e, g=g, pr=pr, zb_=zb_, r=r, sl=sl: e.matmul(zb_[:, 0:256], lhsT=CAA[pr, r, g, :], rhs=Tbr[sl][pr, :], start=True, stop=False), reads=["CAA", "Tbr%d" % sl], writes=[zbk])
                S.op("pe", lambda e, g=g, pr=pr, zb_=zb_, r=r, sl=sl: e.matmul(zb_[:, 0:256], lhsT=CAA[pr, 1 - r, g, :], rhs=Tbi[sl][pr, :], start=False, stop=True), reads=["CAA", "Tbi%d" % sl], writes=[zbk])
                if CUT < 3:
                    continue
                S.op("act", lambda e, r=r, zb_=zb_: e.copy(out=ysum[r][:], in_=zb_[:, 0:256]), reads=[zbk], writes=["ysum%d" % r])
                S.op("dve", lambda e, r=r, yb=yb: e.tensor_tensor(out=ysum[r][:], in0=yb[:, 0:256], in1=ysum[r][:], op=ALU.add), reads=[ybk, "ysum%d" % r], writes=["ysum%d" % r])
                S.op("act", lambda e, r=r: e.activation(out=Yg[r][:], in_=ysum[r][:], func=AF.Gelu), reads=["ysum%d" % r], writes=["Yg%d" % r])
                if CUT < 4:
                    continue
                pT = bank[2 + r][:].bitcast(BF16).rearrange("p (c n) -> p c n", n=128)
                for kb in range(2):
                    S.op("pe", lambda e, r=r, kb=kb, pT=pT: e.transpose(out=pT[:, kb, :], in_=Yg[r][:, kb * 128:(kb + 1) * 128], identity=idb[:]), reads=["Yg%d" % r, "identb"], writes=["sbk%d" % (2 + r)])
                for kb in range(2):
                    S.op("dve", lambda e, g=g, kb=kb, pT=pT: e.tensor_copy(out=ytm[:, kb, :, 16 * g:16 * g + 16], in_=pT[:, kb, :].rearrange("p (t c) -> p t c", c=16)),
                         reads=["sbk%d" % (2 + r)], writes=["ytm"])


def _s5_glu_out(C, es, D, idb, ytm, dbg):
    S = C.S
    sb = lambda name, shape, dt=F32: C.sb(es, name, shape, dt)
    gso = sb("gso", [128, 4]); bgl = sb("bgl", [128, 512])
    S.dma("sp", gso[:], D["g_ssm_out"], writes=["gso"])
    S.dma("sp", bgl[:], D["b_glu"].partition_broadcast(128), writes=["bgl"])
    with scope(C) as est:
        Wg = load_weight_bf16(C, es, est, "Wg", D["w_glu"], 4, 512)
    with scope(C) as est:
        Wo = load_weight_bf16(C, es, est, "Wos", D["w_out"][512:1024, :], 4, 1024, gcol=(gso, "gso"))
    yT = sb("yT", [128, 4, 128], BF16); zb = sb("zb", [128, 512]); ssm = sb("ssm", [128, 512]); junk = sb("junk3", [128, 512])
    st = sb("st3", [128, 2]); sn = sb("sn", [128, 512], BF16); snT = sb("snT", [128, 4, 128], BF16)
    ho = [sb("ho%d" % i, [128, 1024]) for i in range(2)]
    hsv = D["hs_d"].rearrange("(k t) d -> t k d", t=8)
    with scope(C) as esp:
        bank = [C.ps(esp, "gbk%d" % i, [128, 512]) for i in range(5)]
        it = 0
        for kb in range(2):
            for t in range(8):
                sl = it % 2; it += 1
                y = ytm[:, kb, t, :]
                pT = bank[0][:].bitcast(BF16).rearrange("p (c n) -> p c n", n=128)[:, 0:4, :]
                transpose_chunks(C, y, "ytm", 4, pT, "gbk0", yT[:], "yT", idb)
                for c in range(4):
                    S.op("pe", lambda e, c=c: e.matmul(bank[1][:], lhsT=yT[:, c, :], rhs=Wg[:, c, :], start=(c == 0), stop=(c == 3)), reads=["yT", "Wg"], writes=["gbk1"])
                S.op("dve", lambda e: e.tensor_tensor(out=zb[:], in0=bank[1][:], in1=bgl[:], op=ALU.add), reads=["gbk1", "bgl"], writes=["zb"])
                S.op("act", lambda e: e.activation(out=zb[:], in_=zb[:], func=AF.Sigmoid), reads=["zb"], writes=["zb"])
                S.op("pool", lambda e, y=y: e.tensor_tensor(out=ssm[:], in0=y, in1=zb[:], op=ALU.mult), reads=["ytm", "zb"], writes=["ssm"])
                if "ssm" in dbg:
                    S.dma("sp", dbg["ssm"].rearrange("(k t) d -> t k d", t=8)[t, kb * 128:(kb + 1) * 128, :], ssm[:], reads=["ssm"], writes=["o_ssm"])
                rms_rstd(C, ssm[:], "ssm", junk[:], "junk3", st[:, 0:1], st[:, 1:2], "st3", 512)
                S.op("dve", lambda e: e.tensor_scalar(out=sn[:], in0=ssm[:], scalar1=st[:, 1:2], scalar2=None, op0=ALU.mult), reads=["ssm", "st3"], writes=["sn"])
                pT2 = bank[2][:].bitcast(BF16).rearrange("p (c n) -> p c n", n=128)[:, 0:4, :]
                transpose_chunks(C, sn[:], "sn", 4, pT2, "gbk2", snT[:], "snT", idb)
                for cb in range(2):
                    for c in range(4):
                        S.op("pe", lambda e, c=c, cb=cb: e.matmul(bank[3 + cb][:], lhsT=snT[:, c, :], rhs=Wo[:, c, cb * 512:(cb + 1) * 512], start=(c == 0), stop=(c == 3)), reads=["snT", "Wos"], writes=["gbk%d" % (3 + cb)])
                    S.op("act", lambda e, cb=cb, sl=sl: e.copy(out=ho[sl][:, cb * 512:(cb + 1) * 512], in_=bank[3 + cb][:]), reads=["gbk%d" % (3 + cb)], writes=["ho%d" % sl])
                S.dma("sp", hsv[t, kb * 128:(kb + 1) * 128, :], ho[sl][:], reads=["ho%d" % sl], writes=["hs_d"])


def _attention(C, es, D, idb, dbg, prep=None):
    S = C.S
    sb = lambda name, shape, dt=F32: C.sb(es, name, shape, dt)
    kT = sb("kT", [128, 4, 2560], BF16); qT = sb("qT", [128, 4, 2048], BF16); V = sb("Vall", [128, 20, 520], BF16)
    qZ = sb("qZ", [128, 8, 2048], BF16)
    S.dma("sp", kT[:].rearrange("p c n -> p (c n)"), D["kT_d"], reads=["qkT_d"], writes=["kT"])
    S.dma("sp", qT[:].rearrange("p c n -> p (c n)"), D["qT_d"], reads=["qkT_d"], writes=["qT"])
    S.dma("sp", V[:], D["V_d"].rearrange("t p n -> p t n"), reads=["V_d"], writes=["Vall"])
    S.op("pool", lambda e: e.memset(qZ[:].rearrange("p h n -> p (h n)"), 0.0), writes=["qZ"])
    for h in range(8):
        rws = slice(64 * (h % 2), 64 * (h % 2) + 64)
        S.op("dve" if h % 2 else "act", (lambda e, h=h, rws=rws: e.tensor_copy(out=qZ[rws, h, :], in_=qT[rws, h // 2, :])) if h % 2 else (lambda e, h=h, rws=rws: e.copy(out=qZ[rws, h, :], in_=qT[rws, h // 2, :])),
             reads=["qT", "qZ"], writes=["qZ"])
    BT = sb("BT", [128, 8, 5, 128], BF16)
    gao = sb("gao", [128, 4])
    S.dma("sp", gao[:], D["g_att_out"], writes=["gao"])
    with scope(C) as est:
        stg = C.sb(est, "btstg", [128, 640])
        for h in range(8):
            S.dma("sp", stg[:], D["bias_t"][:, h * 640:(h + 1) * 640], writes=["btstg"])
            S.op("dve", lambda e, h=h: e.tensor_copy(out=BT[:, h, :, :].rearrange("p j q -> p (j q)"), in_=stg[:]), reads=["btstg"], writes=["BT"])
    with scope(C) as est:
        Wo = load_weight_bf16(C, es, est, "Woa", D["w_out"][0:512, :], 4, 1024, gcol=(gao, "gao"))
    PT = [sb("PT%d" % i, [128, 5, 128], BF16) for i in range(2)]
    rd = sb("rd", [128, 8]); att = sb("att", [128, 8, 64]); junk = sb("junk4", [128, 512]); st = sb("st4", [128, 2])
    an = sb("an", [128, 512], BF16); anT = sb("anT", [128, 4, 128], BF16)
    xo = [sb("xo%d" % i, [128, 1024]) for i in range(2)]; hsl = [sb("hsl%d" % i, [128, 1024]) for i in range(2)]
    h1t = [sb("h1t%d" % i, [128, 1024]) for i in range(2)]
    xv = D["x_ext"].rearrange("(n p) d -> n p d", p=128)
    hsv = D["hs_d"].rearrange("(n p) d -> n p d", p=128)
    h1v = D["h1_d"].rearrange("(n p) d -> n p d", p=128)
    with scope(C) as esp:
        bank = [C.ps(esp, "abk%d" % i, [128, 512]) for i in range(7)]
        for qt in range(16):
            sl = qt % 2
            drip(prep, 2)
            S.dma("sp", xo[sl][:], xv[48 + qt], writes=["xo%d" % sl])
            S.dma("sp", hsl[sl][:], hsv[qt], reads=["hs_d"], writes=["hsl%d" % sl])
            for h in range(8):
                hp = h // 2; rows = slice(64 * (h % 2), 64 * (h % 2) + 64); ps_ = h % 2
                bA = bank[2 * ps_]; bB = bank[2 * ps_ + 1]; bAk = "abk%d" % (2 * ps_); bBk = "abk%d" % (2 * ps_ + 1)
                for j in range(5):
                    o = bA[:, j * 128:(j + 1) * 128] if j < 4 else bB[:, 0:128]
                    ok = bAk if j < 4 else bBk
                    S.op("pe", lambda e, o=o, h=h, hp=hp, j=j, qt=qt: e.matmul(o, lhsT=kT[:, hp, (qt + j) * 128:(qt + j + 1) * 128], rhs=qZ[:, h, qt * 128:(qt + 1) * 128], start=True, stop=False),
                         reads=["kT", "qZ"], writes=[ok])
                    S.op("pe", lambda e, o=o, h=h, j=j: e.matmul(o, lhsT=idb[:], rhs=BT[:, h, j, :], start=False, stop=True), reads=["identb", "BT"], writes=[ok])
                S.op("act", lambda e, ps_=ps_, bA=bA: e.activation(out=PT[ps_][:, 0:4, :].rearrange("p j q -> p (j q)"), in_=bA[:], func=AF.Exp), reads=[bAk], writes=["PT%d" % ps_])
                S.op("act", lambda e, ps_=ps_, bB=bB: e.activation(out=PT[ps_][:, 4, :], in_=bB[:, 0:128], func=AF.Exp), reads=[bBk], writes=["PT%d" % ps_])
                ob = bank[4 + h // 4]; obk = "abk%d" % (4 + h // 4)
                for j in range(5):
                    S.op("pe", lambda e, ob=ob, h=h, j=j, ps_=ps_, qt=qt: e.matmul(ob[:, (h % 4) * 65:(h % 4) * 65 + 65], lhsT=PT[ps_][:, j, :], rhs=V[:, qt + j, h * 65:(h + 1) * 65], start=(j == 0), stop=(j == 4)),
                         reads=["PT%d" % ps_, "Vall"], writes=[obk])
            for hb in range(2):
                ov = bank[4 + hb][:, 0:260].rearrange("p (h d) -> p h d", d=65)
                S.op("dve", lambda e, hb=hb, ov=ov: e.reciprocal(out=rd[:, hb * 4:(hb + 1) * 4], in_=ov[:, :, 64]), reads=["abk%d" % (4 + hb)], writes=["rd%d" % hb])
                S.op("dve", lambda e, hb=hb, ov=ov: e.tensor_tensor(out=att[:, hb * 4:(hb + 1) * 4, :], in0=ov[:, :, 0:64], in1=bc(rd[:, hb * 4:(hb + 1) * 4].unsqueeze(2), [128, 4, 64]), op=ALU.mult),
                     reads=["abk%d" % (4 + hb), "rd%d" % hb], writes=["att%d" % hb])
            attf = att[:].rearrange("p h d -> p (h d)")
            if "att" in dbg:
                S.dma("sp", dbg["att"].rearrange("(n p) d -> n p d", p=128)[qt], attf, reads=["att0", "att1"], writes=["o_att"])
            S.op("act", lambda e: e.activation(out=junk[:], in_=attf, func=AF.Square, accum_out=st[:, 0:1]), reads=["att0", "att1"], writes=["st4_ss"])
            S.op("act", lambda e: e.activation(out=st[:, 1:2], in_=st[:, 0:1], func=AF.Sqrt, scale=1.0 / 512, bias=EPS), reads=["st4_ss"], writes=["st4_sq"])
            S.op("dve", lambda e: e.reciprocal(out=st[:, 1:2], in_=st[:, 1:2]), reads=["st4_sq"], writes=["st4"])
            S.op("dve", lambda e: e.tensor_scalar(out=an[:], in0=attf, scalar1=st[:, 1:2], scalar2=None, op0=ALU.mult), reads=["att0", "att1", "st4"], writes=["an"])
            pT = bank[6][:].bitcast(BF16).rearrange("p (c n) -> p c n", n=128)[:, 0:4, :]
            transpose_chunks(C, an[:], "an", 4, pT, "abk6", anT[:], "anT", idb)
            for cb in range(2):
                for c in range(4):
                    S.op("pe", lambda e, c=c, cb=cb: e.matmul(bank[cb][:], lhsT=anT[:, c, :], rhs=Wo[:, c, cb * 512:(cb + 1) * 512], start=(c == 0), stop=(c == 3)), reads=["anT", "Woa"], writes=["abk%d" % cb])
                S.op("dve", lambda e, cb=cb, sl=sl: e.tensor_tensor(out=h1t[sl][:, cb * 512:(cb + 1) * 512], in0=bank[cb][:], in1=xo[sl][:, cb * 512:(cb + 1) * 512], op=ALU.add),
                     reads=["abk%d" % cb, "xo%d" % sl], writes=["h1t%d" % sl])
            S.op("pool", lambda e, sl=sl: e.tensor_tensor(out=h1t[sl][:], in0=h1t[sl][:], in1=hsl[sl][:], op=ALU.add), reads=["h1t%d" % sl, "hsl%d" % sl], writes=["h1t%d" % sl])
            S.dma("sp", h1v[qt], h1t[sl][:], reads=["h1t%d" % sl], writes=["h1_d"])


def _col(g, n):
    return np.ascontiguousarray(np.asarray(g, np.float32).reshape(n, 128).T)


def host_shared(inp):
    f = lambda k: np.asarray(inp[k], np.float32)[0]
    sh = {}
    sh["g_mix"] = _col(f("norm_mix_g"), 8)
    sh["w_in"] = np.ascontiguousarray(f("w_in"))
    sh["att_q_g"] = f("att_q_g").reshape(1, 64)
    sh["att_k_g"] = f("att_k_g").reshape(1, 64)
    rb = f("rel_bias")
    p = np.arange(128); j = np.arange(5); q = np.arange(128)
    kidx = j[:, None] * 128 + p[None, :]
    kc = kidx // 64; ki = kidx % 64
    qc = q // 64; qi = q % 64
    jb = kc[:, :, None] - qc[None, None, :]
    allowed = (jb >= 0) & (jb <= 8)
    kj = jb * 64 + ki[:, :, None]
    dist = 512 + qi[None, None, :] - kj
    bucket = np.clip(np.clip(dist, -63, 128) + 63, 0, 191)
    bt = np.where(allowed[None], rb[:, bucket], np.float32(NEG))
    sh["bias_t"] = np.ascontiguousarray(bt.transpose(2, 0, 1, 3).reshape(128, 8 * 5 * 128).astype(np.float32))
    dup = lambda a: np.ascontiguousarray(np.concatenate([a, a], 0).astype(np.float32))
    sh["s5_lr"] = dup(f("ssm_lam_re").T)
    sh["s5_li"] = dup(f("ssm_lam_im").T)
    sh["s5_ls"] = np.ascontiguousarray(np.broadcast_to(f("ssm_log_step")[None, :], (128, 32)).astype(np.float32))
    sg = np.ones((128, 2), np.float32); sg[:64, 0] = -1.0; sg[64:, 1] = -1.0
    sh["s5_sg"] = sg
    sh["s5_nv"] = np.ascontiguousarray(np.broadcast_to(np.arange(-7, 16, dtype=np.float32)[None, :], (128, 23)))
    bre = f("ssm_b_re").transpose(1, 0, 2).reshape(64, 512); bim = f("ssm_b_im").transpose(1, 0, 2).reshape(64, 512)
    cre = f("ssm_c_re").transpose(2, 0, 1).reshape(64, 512); cim = f("ssm_c_im").transpose(2, 0, 1).reshape(64, 512)
    sh["s5_p1b"] = np.ascontiguousarray(np.concatenate([bre, bim], 0)); sh["s5_p2b"] = np.ascontiguousarray(np.concatenate([bim, bre], 0))
    sh["s5_p1c"] = np.ascontiguousarray(np.concatenate([cre, cim], 0)); sh["s5_p2c"] = np.ascontiguousarray(np.concatenate([cim, cre], 0))
    dd = np.zeros((2, 4, 16, 4, 4, 16), np.float32)
    dsk = f("ssm_d")
    for g in range(32):
        chc = g // 8; hb = (g % 8) // 4; j4 = g % 4
        for c in range(16):
            dd[hb, j4, c, chc, j4, c] = dsk[g, c]
    sh["s5_dd"] = dd.reshape(128, 256)
    sh["g_ssm_out"] = _col(f("ssm_out_g"), 4)
    sh["g_att_out"] = _col(f("att_out_g"), 4)
    sh["b_glu"] = f("ssm_b_glu").reshape(1, 512)
    sh["w_glu"] = np.ascontiguousarray(f("ssm_w_glu"))
    sh["w_out"] = np.ascontiguousarray(f("w_out"))
    return sh


def host_core(inp, c):
    b, seg = c // 4, c % 4
    x = np.asarray(inp["x"], np.float32)
    xe = np.zeros((8192, 1024), np.float32)
    n = (seg + 1) * 2048
    xe[8192 - n:] = x[b, :n]
    hv = np.full((512,), 1.0 if seg > 0 else 0.0, np.float32)
    return {"x_ext": xe, "hvalid": np.ascontiguousarray(hv.reshape(4, 128).T)}


IN_SHAPES = {
    "x_ext": [8192, 1024], "hvalid": [128, 4], "g_mix": [128, 8], "w_in": [1024, 2048], "att_q_g": [1, 64], "att_k_g": [1, 64],
    "bias_t": [128, 5120], "s5_lr": [128, 32], "s5_li": [128, 32], "s5_ls": [128, 32], "s5_sg": [128, 2], "s5_nv": [128, 23],
    "s5_p1b": [128, 512], "s5_p2b": [128, 512], "s5_p1c": [128, 512], "s5_p2c": [128, 512], "s5_dd": [128, 256],
    "g_ssm_out": [128, 4], "g_att_out": [128, 4], "b_glu": [1, 512], "w_glu": [512, 512], "w_out": [1024, 1024],
}
IN_SHAPES_MEM = {"mem": [256, 1024], "g_mem": [128, 8], "g_memkv": [128, 8], "mem_q_g": [1, 256], "mem_k_g": [1, 256],
                 "w_mem_q": [1024, 1024], "w_mem_k": [1024, 1024], "w_mem_v": [1024, 1024], "w_mem_o": [1024, 1024]}
IN_SHAPES_PEER = {"g_peer": [1, 1024], "w_peer_q": [1024, 2048], "keysT": [128, 2048], "peer_uT": [1024, 16384], "peer_v": [16384, 1024]}
SCRATCH = {"kT_d": ([128, 4 * 2560], BF16), "qT_d": ([128, 4 * 2048], BF16), "V_d": ([20, 128, 520], BF16),
           "hs_d": ([2048, 1024], F32), "Mz_d": ([128, 16 * 8 * 128], BF16), "CA_d": ([128, 2, 4096], BF16)}
SCRATCH_PEER = {"uT_b": ([1024, 16384], BF16), "v_b": ([16384, 1024], BF16), "xnT_d": ([16, 128, 1024], BF16),
                "sc_d": ([16, 128, 2048], F32), "tau_d": ([16, 128, 8], F32)}


def host_shared_rest(inp):
    f = lambda k: np.asarray(inp[k], np.float32)[0]
    sh = {}
    sh["g_mem"] = _col(f("norm_mem_g"), 8); sh["g_memkv"] = _col(f("norm_memkv_g"), 8)
    sh["mem_q_g"] = f("mem_q_g").reshape(1, 256); sh["mem_k_g"] = f("mem_k_g").reshape(1, 256)
    for k in ("w_mem_q", "w_mem_k", "w_mem_v", "w_mem_o", "w_peer_q"):
        sh[k] = np.ascontiguousarray(f(k))
    sh["g_peer"] = f("norm_peer_g").reshape(1, 1024)
    sh["keysT"] = np.ascontiguousarray(f("peer_keys").transpose(3, 0, 1, 2).reshape(128, 2048))
    sh["peer_uT"] = np.ascontiguousarray(f("peer_u").T)
    sh["peer_v"] = np.ascontiguousarray(f("peer_v"))
    return sh


def build_program(stages=("mixer", "mem", "peer"), dbg_specs=None, upto=9):
    nc = bass.Bass("TRN2", target_bir_lowering=False)
    D = {}
    shapes = {}
    if "mixer" in stages:
        shapes.update(IN_SHAPES)
    if "mem" in stages:
        shapes.update(IN_SHAPES_MEM)
    if "peer" in stages:
        shapes.update(IN_SHAPES_PEER)
    for k, shp in shapes.items():
        D[k] = nc.dram_tensor(k, shp, F32, kind="ExternalInput").ap()
    scr = {}
    if "mixer" in stages:
        scr.update(SCRATCH)
    if "peer" in stages:
        scr.update(SCRATCH_PEER)
    for k, (shp, dt) in scr.items():
        D[k] = nc.dram_tensor(k, shp, dt).ap()
    chain = ["h1_d", "h2_d", "out"]
    first = {"mixer": None, "mem": "h1_d", "peer": "h2_d"}[stages[0]]
    last = {"mixer": "h1_d", "mem": "h2_d", "peer": "out"}[stages[-1]]
    for k in chain:
        if k == first:
            D[k] = nc.dram_tensor(k, [2048, 1024], F32, kind="ExternalInput").ap()
        elif k == last:
            D[k] = nc.dram_tensor(k, [2048, 1024], F32, kind="ExternalOutput").ap()
        else:
            D[k] = nc.dram_tensor(k, [2048, 1024], F32).ap()
    dbg = {}
    for k, shp in (dbg_specs or {}).items():
        dbg[k] = nc.dram_tensor("dbg_" + k, shp, F32, kind="ExternalOutput").ap()
    with ExitStack() as es:
        S = Sched(nc, es)
        C = Ctx(nc, S)
        prep = stage_peer_prep(C, D) if "peer" in stages else None
        if "mixer" in stages:
            stage_mixer(C, D, dbg, upto, prep=prep)
        if "mem" in stages:
            stage_mem(C, D, dbg, prep=prep)
        drip(prep, 1000)
        if "peer" in stages:
            stage_peer(C, D, dbg)
        S.barrier()
        S.emit()
    return nc, S


def _headnorm(C, src_ap, skey, nh, hd, sqt, sqk, stat, stk, gt, gkey, dst_ap, dkey):
    S = C.S
    sv = src_ap.rearrange("p (h d) -> p h d", d=hd)
    S.op("pool", lambda e: e.tensor_tensor(out=sqt, in0=src_ap, in1=src_ap, op=ALU.mult), reads=[skey], writes=[sqk])
    S.op("dve", lambda e: e.tensor_reduce(out=stat[:, 0:nh], in_=sqt.rearrange("p (h d) -> p h d", d=hd), axis=AX.X, op=ALU.add), reads=[sqk], writes=[stk + "a"])
    S.op("act", lambda e: e.activation(out=stat[:, nh:2 * nh], in_=stat[:, 0:nh], func=AF.Sqrt, scale=1.0 / hd, bias=EPS), reads=[stk + "a"], writes=[stk + "b"])
    S.op("dve", lambda e: e.reciprocal(out=stat[:, nh:2 * nh], in_=stat[:, nh:2 * nh]), reads=[stk + "b"], writes=[stk])
    S.op("dve", lambda e: e.tensor_tensor(out=sv, in0=sv, in1=bc(stat[:, nh:2 * nh].unsqueeze(2), [128, nh, hd]), op=ALU.mult), reads=[skey, stk], writes=[skey])
    S.op("pool", lambda e: e.tensor_tensor(out=dst_ap.rearrange("p (h d) -> p h d", d=hd), in0=sv, in1=bc(gt.unsqueeze(1), [128, nh, hd]), op=ALU.mult), reads=[skey, gkey], writes=[dkey])


def stage_mem(C, D, dbg=None, prep=None):
    S = C.S
    dbg = dbg or {}
    with scope(C) as es:
        sb = lambda name, shape, dt=F32: C.sb(es, name, shape, dt)
        idf, idb = make_ident(C, es, "ident")
        gm = sb("gm", [128, 8]); gkv = sb("gkv", [128, 8]); gq = sb("mgq", [128, 256]); gk = sb("mgk", [128, 256])
        S.dma("sp", gm[:], D["g_mem"], writes=["gm"]); S.dma("sp", gkv[:], D["g_memkv"], writes=["gkv"])
        S.dma("sp", gq[:], D["mem_q_g"].partition_broadcast(128), writes=["mgq"]); S.dma("sp", gk[:], D["mem_k_g"].partition_broadcast(128), writes=["mgk"])
        S.op("dve", lambda e: e.tensor_scalar(out=gq[:], in0=gq[:], scalar1=1.0 / 16, scalar2=None, op0=ALU.mult), reads=["mgq"], writes=["mgq"])
        kTm = sb("kTm", [128, 8, 256], BF16); Vm = sb("Vm", [128, 2, 4, 257], BF16)
        xt = [sb("mxt%d" % i, [128, 1024]) for i in range(2)]; st = sb("mst", [128, 2]); xn = sb("mxn", [128, 1024], BF16)
        xnT = sb("mxnT", [128, 8, 128], BF16); qf = sb("mqf", [128, 1024]); sq = sb("msq", [128, 1024]); qst = sb("mqst", [128, 8])
        qn = sb("mqn", [128, 1024], BF16); mjunk = sb("mjunk", [128, 1024], BF16)
        with scope(C) as esk:
            with scope(C) as est:
                Wk = load_weight_bf16(C, esk, est, "Wmk", D["w_mem_k"], 8, 1024, gcol=(gkv, "gkv"))
            with scope(C) as est:
                Wv = load_weight_bf16(C, esk, est, "Wmv", D["w_mem_v"], 8, 1024, gcol=(gkv, "gkv"))
            with scope(C) as esp:
                bank = [C.ps(esp, "mkb%d" % i, [128, 512]) for i in range(6)]
                mv = D["mem"].rearrange("(n p) d -> n p d", p=128)
                for mt in range(2):
                    x_ = xt[mt]; xk = "mxt%d" % mt
                    S.dma("sp", x_[:], mv[mt], writes=[xk])
                    rms_rstd(C, x_[:], xk, mjunk[:], "mjunk", st[:, 0:1], st[:, 1:2], "mst", 1024)
                    S.op("dve", lambda e, x_=x_: e.tensor_scalar(out=xn[:], in0=x_[:], scalar1=st[:, 1:2], scalar2=None, op0=ALU.mult), reads=[xk, "mst"], writes=["mxn"])
                    pb = bank[0][:].bitcast(BF16).rearrange("p (c n) -> p c n", n=128)
                    transpose_chunks(C, xn[:], "mxn", 8, pb, "mkb0", xnT[:], "mxnT", idb)
                    for (W, wk, b0) in ((Wk, "Wmk", 1), (Wv, "Wmv", 3)):
                        for cb in range(2):
                            for dc in range(8):
                                S.op("pe", lambda e, W=W, cb=cb, dc=dc, b0=b0: e.matmul(bank[b0 + cb][:], lhsT=xnT[:, dc, :], rhs=W[:, dc, cb * 512:(cb + 1) * 512], start=(dc == 0), stop=(dc == 7)),
                                     reads=["mxnT", wk], writes=["mkb%d" % (b0 + cb)])
                    for cb in range(2):
                        S.op("act", lambda e, cb=cb: e.copy(out=qf[:, cb * 512:(cb + 1) * 512], in_=bank[1 + cb][:]), reads=["mkb%d" % (1 + cb)], writes=["mqf"])
                        S.op("dve", lambda e, cb=cb, mt=mt: e.tensor_copy(out=Vm[:, mt, 2 * cb:2 * cb + 2, 0:256], in_=bank[3 + cb][:].rearrange("p (h d) -> p h d", d=256)), reads=["mkb%d" % (3 + cb)], writes=["Vm"])
                    S.op("pool", lambda e, mt=mt: e.memset(Vm[:, mt, :, 256], 1.0), reads=[], writes=["Vm"])
                    _headnorm(C, qf[:], "mqf", 4, 256, sq[:], "msq", qst, "mqst", gk[:], "mgk", qn[:], "mqn")
                    pb2 = bank[5][:].bitcast(BF16).rearrange("p (c n) -> p c n", n=128)
                    transpose_chunks(C, qn[:], "mqn", 8, pb2, "mkb5", kTm[:, :, mt * 128:(mt + 1) * 128], "kTm", idb)
        with scope(C) as est:
            Wq = load_weight_bf16(C, es, est, "Wmq", D["w_mem_q"], 8, 1024, gcol=(gm, "gm"))
        with scope(C) as est:
            Wo = load_weight_bf16(C, es, est, "Wmo", D["w_mem_o"], 8, 1024)
        qT = sb("mqT", [128, 8, 128], BF16); PT = [sb("mPT%d" % i, [128, 2, 128], BF16) for i in range(2)]
        rd = sb("mrd", [128, 4]); ob = sb("mob", [128, 1024], BF16); oT = sb("moT", [128, 8, 128], BF16)
        h2t = [sb("h2t%d" % i, [128, 1024]) for i in range(2)]
        hv = D["h1_d"].rearrange("(n p) d -> n p d", p=128); ov = D["h2_d"].rearrange("(n p) d -> n p d", p=128)
        with scope(C) as esp:
            bank = [C.ps(esp, "mb%d" % i, [128, 512]) for i in range(8)]
            for tt in range(16):
                sl = tt % 2
                drip(prep, 1)
                x_ = xt[sl]; xk = "mxt%d" % sl
                S.dma("sp", x_[:], hv[tt], reads=["h1_d"], writes=[xk])
                rms_rstd(C, x_[:], xk, mjunk[:], "mjunk", st[:, 0:1], st[:, 1:2], "mst", 1024)
                S.op("dve", lambda e, x_=x_: e.tensor_scalar(out=xn[:], in0=x_[:], scalar1=st[:, 1:2], scalar2=None, op0=ALU.mult), reads=[xk, "mst"], writes=["mxn"])
                pb = bank[0][:].bitcast(BF16).rearrange("p (c n) -> p c n", n=128)
                transpose_chunks(C, xn[:], "mxn", 8, pb, "mb0", xnT[:], "mxnT", idb)
                for cb in range(2):
                    for dc in range(8):
                        S.op("pe", lambda e, cb=cb, dc=dc: e.matmul(bank[1 + cb][:], lhsT=xnT[:, dc, :], rhs=Wq[:, dc, cb * 512:(cb + 1) * 512], start=(dc == 0), stop=(dc == 7)),
                             reads=["mxnT", "Wmq"], writes=["mb%d" % (1 + cb)])
                    S.op("act", lambda e, cb=cb: e.copy(out=qf[:, cb * 512:(cb + 1) * 512], in_=bank[1 + cb][:]), reads=["mb%d" % (1 + cb)], writes=["mqf"])
                _headnorm(C, qf[:], "mqf", 4, 256, sq[:], "msq", qst, "mqst", gq[:], "mgq", qn[:], "mqn")
                pb2 = bank[3][:].bitcast(BF16).rearrange("p (c n) -> p c n", n=128)
                transpose_chunks(C, qn[:], "mqn", 8, pb2, "mb3", qT[:], "mqT", idb)
                for h in range(4):
                    ps_ = h % 2
                    sbk = bank[4 + ps_]; sbkk = "mb%d" % (4 + ps_)
                    for mt in range(2):
                        for dh in range(2):
                            S.op("pe", lambda e, h=h, mt=mt, dh=dh, sbk=sbk: e.matmul(sbk[:, mt * 128:(mt + 1) * 128], lhsT=kTm[:, 2 * h + dh, mt * 128:(mt + 1) * 128], rhs=qT[:, 2 * h + dh, :], start=(dh == 0), stop=(dh == 1)),
                                 reads=["kTm", "mqT"], writes=[sbkk])
                    S.op("act", lambda e, ps_=ps_, sbk=sbk: e.activation(out=PT[ps_][:].rearrange("p m q -> p (m q)"), in_=sbk[:, 0:256], func=AF.Exp, bias=-8.0), reads=[sbkk], writes=["mPT%d" % ps_])
                    obk = bank[6 + ps_]; obkk = "mb%d" % (6 + ps_)
                    for mt in range(2):
                        S.op("pe", lambda e, h=h, mt=mt, ps_=ps_, obk=obk: e.matmul(obk[:, 0:257], lhsT=PT[ps_][:, mt, :], rhs=Vm[:, mt, h, :], start=(mt == 0), stop=(mt == 1)), reads=["mPT%d" % ps_, "Vm"], writes=[obkk])
                    S.op("dve", lambda e, h=h, obk=obk: e.reciprocal(out=rd[:, h:h + 1], in_=obk[:, 256:257]), reads=[obkk], writes=["mrd%d" % h])
                    S.op("dve", lambda e, h=h, obk=obk: e.tensor_scalar(out=ob[:, h * 256:(h + 1) * 256], in0=obk[:, 0:256], scalar1=rd[:, h:h + 1], scalar2=None, op0=ALU.mult), reads=[obkk, "mrd%d" % h], writes=["mob"])
                pb3 = bank[0][:].bitcast(BF16).rearrange("p (c n) -> p c n", n=128)
                transpose_chunks(C, ob[:], "mob", 8, pb3, "mb0", oT[:], "moT", idb)
                for cb in range(2):
                    for dc in range(8):
                        S.op("pe", lambda e, cb=cb, dc=dc: e.matmul(bank[1 + cb][:], lhsT=oT[:, dc, :], rhs=Wo[:, dc, cb * 512:(cb + 1) * 512], start=(dc == 0), stop=(dc == 7)),
                             reads=["moT", "Wmo"], writes=["mb%d" % (1 + cb)])
                    S.op("dve", lambda e, cb=cb, sl=sl, x_=x_: e.tensor_tensor(out=h2t[sl][:, cb * 512:(cb + 1) * 512], in0=bank[1 + cb][:], in1=x_[:, cb * 512:(cb + 1) * 512], op=ALU.add),
                         reads=["mb%d" % (1 + cb), xk], writes=["h2t%d" % sl])
                S.dma("sp", ov[tt], h2t[sl][:], reads=["h2t%d" % sl], writes=["h2_d"])


def stage_peer_prep(C, D):
    S = C.S
    uv = D["peer_uT"].rearrange("(c p) (a e) -> c p a e", p=128, e=2048)
    ub = D["uT_b"].rearrange("(c p) (a e) -> c p a e", p=128, e=2048)
    vv = D["peer_v"].rearrange("(c p) d -> c p d", p=512)
    vb = D["v_b"].rearrange("(c p) d -> c p d", p=512)

    def gen():
        for c in range(8):
            S.dma("pool", ub[c], uv[c], writes=["uT_b"])
            yield
        for c in range(32):
            S.dma("pool", vb[c], vv[c], writes=["v_b"])
            yield
    return gen()


def drip(g, n):
    if g is None:
        return
    for _ in range(n):
        try:
            next(g)
        except StopIteration:
            return


def _top16(C, src, skey, work, wkey, dst, dkey):
    S = C.S
    S.op("dve", lambda e: e.max(out=dst[:, 0:8], in_=src), reads=[skey], writes=[dkey])
    S.op("dve", lambda e: e.match_replace(out=work, in_to_replace=dst[:, 0:8], in_values=src, imm_value=-1e30), reads=[skey, dkey], writes=[wkey])
    S.op("dve", lambda e: e.max(out=dst[:, 8:16], in_=work), reads=[wkey], writes=[dkey])


def stage_peer(C, D, dbg=None):
    S = C.S
    dbg = dbg or {}
    hv = D["h2_d"].rearrange("(n p) d -> n p d", p=128)
    with scope(C) as es:
        sb = lambda name, shape, dt=F32: C.sb(es, name, shape, dt)
        idf, idb = make_ident(C, es, "ident")
        gp = sb("gpb", [128, 1024])
        S.dma("sp", gp[:], D["g_peer"].partition_broadcast(128), writes=["gpb"])
        with scope(C) as est:
            Wq = load_weight_bf16(C, es, est, "Wpq", D["w_peer_q"], 8, 2048)
        keyT = sb("keyT", [128, 16, 128], BF16)
        with scope(C) as est:
            kst = C.sb(est, "kst", [128, 2048])
            S.dma("sp", kst[:], D["keysT"], writes=["kst"])
            S.op("dve", lambda e: e.tensor_copy(out=keyT[:].rearrange("p a n -> p (a n)"), in_=kst[:]), reads=["kst"], writes=["keyT"])
        xt = [sb("pxt%d" % i, [128, 1024]) for i in range(2)]; st_ = [sb("pst%d" % i, [128, 2]) for i in range(2)]; junk = sb("pjunk", [128, 1024], BF16)
        xn_ = [sb("pxn%d" % i, [128, 1024], BF16) for i in range(2)]; xnT = [sb("pxnT%d" % i, [128, 8, 128], BF16) for i in range(2)]
        qb_ = [sb("pqb%d" % i, [128, 2048], BF16) for i in range(2)]; qTp_ = [sb("pqT%d" % i, [128, 16, 128], BF16) for i in range(2)]
        sc = [sb("psc%d" % i, [128, 16, 128]) for i in range(2)]; work_ = [sb("pwork%d" % i, [128, 256]) for i in range(2)]
        sv_ = [sb("psv%d" % i, [128, 16, 16]) for i in range(2)]; cand_ = [sb("pcand%d" % i, [128, 8, 256]) for i in range(2)]
        cex_ = [sb("pcex%d" % i, [128, 8, 256]) for i in range(2)]; ctop_ = [sb("pctop%d" % i, [128, 8, 16]) for i in range(2)]
        Z_ = [sb("pZ%d" % i, [128, 8]) for i in range(2)]; off_ = [sb("poff%d" % i, [128, 8]) for i in range(2)]; tau = [sb("ptau%d" % i, [128, 8]) for i in range(2)]
        cjunk_ = [sb("pcj%d" % i, [128, 256]) for i in range(2)]; offs_ = [sb("poffs%d" % i, [128, 16]) for i in range(2)]
        xTd = D["xnT_d"].rearrange("n p (c t) -> n p c t", t=128)
        with scope(C) as esp:
            bank = [C.ps(esp, "pab%d" % i, [128, 512]) for i in range(6)]

            def tile(tt):
                sl = tt % 2
                K = lambda nm: "%s%d" % (nm, sl)
                x_ = xt[sl]; xk = K("pxt"); st = st_[sl]; xn = xn_[sl]; qb = qb_[sl]; qTp = qTp_[sl]; work = work_[sl]
                sv = sv_[sl]; cand = cand_[sl]; cex = cex_[sl]; ctop = ctop_[sl]; Z = Z_[sl]; off = off_[sl]; cjunk = cjunk_[sl]; offs = offs_[sl]
                S.dma("sp", x_[:], hv[tt], reads=["h2_d"], writes=[xk])
                rms_rstd(C, x_[:], xk, junk[:], "pjunk", st[:, 0:1], st[:, 1:2], K("pst"), 1024)
                S.op("dve", lambda e: e.scalar_tensor_tensor(out=xn[:], in0=x_[:], scalar=st[:, 1:2], in1=gp[:], op0=ALU.mult, op1=ALU.mult), reads=[xk, K("pst"), "gpb"], writes=[K("pxn")])
                pb = bank[0][:].bitcast(BF16).rearrange("p (c n) -> p c n", n=128)
                transpose_chunks(C, xn[:], K("pxn"), 8, pb, "pab0", xnT[sl][:], K("pxnT"), idb)
                S.dma("sp", xTd[tt], xnT[sl][:], reads=[K("pxnT")], writes=["xnT_d"])
                yield
                for cb in range(4):
                    for dc in range(8):
                        S.op("pe", lambda e, cb=cb, dc=dc: e.matmul(bank[1 + cb][:], lhsT=xnT[sl][:, dc, :], rhs=Wq[:, dc, cb * 512:(cb + 1) * 512], start=(dc == 0), stop=(dc == 7)),
                             reads=[K("pxnT"), "Wpq"], writes=["pab%d" % (1 + cb)])
                    S.op("act", lambda e, cb=cb: e.copy(out=qb[:, cb * 512:(cb + 1) * 512], in_=bank[1 + cb][:]), reads=["pab%d" % (1 + cb)], writes=[K("pqb")])
                for half in range(2):
                    pbq = bank[5][:].bitcast(BF16).rearrange("p (c n) -> p c n", n=128)
                    transpose_chunks(C, qb[:, half * 1024:(half + 1) * 1024], K("pqb"), 8, pbq, "pab5", qTp[:, half * 8:(half + 1) * 8, :], K("pqT"), idb)
                for hh in range(16):
                    S.op("pe", lambda e, hh=hh: e.matmul(bank[1 + hh // 4][:, (hh % 4) * 128:(hh % 4 + 1) * 128], lhsT=qTp[:, hh, :], rhs=keyT[:, hh, :], start=True, stop=True),
                         reads=[K("pqT"), "keyT"], writes=["pab%d" % (1 + hh // 4)])
                scs = sc[sl]; sck = K("psc")
                for cb in range(4):
                    S.op("act", lambda e, cb=cb: e.copy(out=scs[:, cb * 4:(cb + 1) * 4, :].rearrange("p a n -> p (a n)"), in_=bank[1 + cb][:]), reads=["pab%d" % (1 + cb)], writes=[sck])
                yield
                for hh in range(16):
                    _top16(C, scs[:, hh, :], sck, work[:, 0:128], K("pwork"), sv[:, hh, :], K("psv"))
                    yield
                svv = sv[:].rearrange("p (h s) k -> p h s k", s=2)
                for h in range(8):
                    S.op("dve", lambda e, h=h: e.tensor_tensor(out=cand[:, h, :].rearrange("p (a b) -> p a b", b=16), in0=bc(svv[:, h, 0, :].unsqueeze(2), [128, 16, 16]),
                                                            in1=bc(svv[:, h, 1, :].unsqueeze(1), [128, 16, 16]), op=ALU.add), reads=[K("psv")], writes=[K("pcand") + "_%d" % h])
                    yield
                ck = [K("pcand") + "_%d" % h for h in range(8)]
                for h in range(8):
                    _top16(C, cand[:, h, :], ck[h], work[:], K("pwork"), ctop[:, h, :], K("pctop"))
                    yield
                S.op("dve", lambda e: e.tensor_tensor(out=cand[:], in0=cand[:], in1=bc(ctop[:, :, 0:1], [128, 8, 256]), op=ALU.subtract), reads=ck + [K("pctop")], writes=ck)
                S.op("act", lambda e: e.activation(out=cex[:].rearrange("p h n -> p (h n)"), in_=cand[:].rearrange("p h n -> p (h n)"), func=AF.Exp), reads=ck, writes=[K("pcex")])
                yield
                S.op("dve", lambda e: e.tensor_tensor(out=tau[sl][:], in0=ctop[:, :, 15], in1=ctop[:, :, 0], op=ALU.subtract), reads=[K("pctop")], writes=[K("ptau")])
                yield
                S.op("dve", lambda e: e.tensor_scalar(out=tau[sl][:], in0=tau[sl][:], scalar1=-1e-5, scalar2=None, op0=ALU.add), reads=[K("ptau")], writes=[K("ptau")])
                yield
                for h in range(8):
                    S.op("dve", lambda e, h=h: e.scalar_tensor_tensor(out=cjunk[:], in0=cand[:, h, :], scalar=tau[sl][:, h:h + 1], in1=cex[:, h, :], op0=ALU.is_ge, op1=ALU.mult, accum_out=Z[:, h:h + 1]),
                         reads=ck + [K("pcex"), K("ptau")], writes=[K("pZ") + "_%d" % h])
                    yield
                S.op("act", lambda e: e.activation(out=off[:], in_=Z[:], func=AF.Ln), reads=[K("pZ") + "_%d" % h for h in range(8)], writes=[K("poff")])
                yield
                S.op("dve", lambda e: e.tensor_tensor(out=tau[sl][:], in0=tau[sl][:], in1=off[:], op=ALU.subtract), reads=[K("ptau"), K("poff")], writes=[K("ptau")])
                S.op("act", lambda e: e.activation(out=tau[sl][:], in_=tau[sl][:], func=AF.Exp), reads=[K("ptau")], writes=[K("ptau")])
                yield
                S.op("dve", lambda e: e.tensor_scalar(out=tau[sl][:], in0=tau[sl][:], scalar1=0.99997, scalar2=None, op0=ALU.mult), reads=[K("ptau")], writes=[K("ptau")])
                offv = offs[:].rearrange("p (h s) -> p h s", s=2)
                S.op("dve", lambda e: e.tensor_copy(out=offv[:, :, 0], in_=svv[:, :, 0, 0]), reads=[K("psv")], writes=[K("poffs") + "a"])
                yield
                S.op("dve", lambda e: e.tensor_tensor(out=offv[:, :, 1], in0=svv[:, :, 1, 0], in1=off[:], op=ALU.add), reads=[K("psv"), K("poff")], writes=[K("poffs") + "b"])
                yield
                S.op("dve", lambda e: e.tensor_tensor(out=scs[:], in0=scs[:], in1=bc(offs[:].unsqueeze(2), [128, 16, 128]), op=ALU.subtract), reads=[sck, K("poffs") + "a", K("poffs") + "b"], writes=[sck])
                S.op("act", lambda e: e.activation(out=scs[:].rearrange("p a n -> p (a n)"), in_=scs[:].rearrange("p a n -> p (a n)"), func=AF.Exp), reads=[sck], writes=[sck])
                S.dma("sp", D["sc_d"][tt], scs[:].rearrange("p a n -> p (a n)"), reads=[sck], writes=["sc_d"])
                S.dma("sp", D["tau_d"][tt], tau[sl][:], reads=[K("ptau")], writes=["tau_d"])

            from itertools import zip_longest
            for t0 in range(0, 16, 2):
                for _ in zip_longest(tile(t0), tile(t0 + 1)):
                    pass
    with scope(C) as es:
        sb = lambda name, shape, dt=F32: C.sb(es, name, shape, dt)
        idf, idb = make_ident(C, es, "ident")
        xnT = sb("bxnT", [128, 4, 1024], BF16); sc = sb("bsc", [128, 4, 2048]); kap = sb("btau", [128, 4, 8])
        UT = [sb("UT%d" % i, [128, 8, 1024], BF16) for i in range(2)]; Vb = [sb("Vb%d" % i, [128, 8, 1024], BF16) for i in range(2)]
        acc = sb("pacc", [128, 4, 1024])
        NP = 12
        Pt = [sb("pP%d" % i, [128, 512]) for i in range(NP)]; Wh = [[sb("pWh%d_%d" % (i, h), [128, 512], BF16) for h in range(8)] for i in range(2)]
        G = [sb("pG%d" % i, [128, 512], BF16) for i in range(3)]; WA = [sb("pWA%d" % i, [128, 512], BF16) for i in range(2)]
        WAT = [sb("pWAT%d" % i, [128, 4, 128], BF16) for i in range(2)]
        h2t = [sb("ph2t%d" % i, [128, 1024]) for i in range(2)]
        uTv = D["uT_b"].rearrange("(c p) (b e) -> b p c e", p=128, e=1024)
        vbv = D["v_b"].rearrange("(b c p) d -> b p c d", p=128, c=8)
        ov = D["out"].rearrange("(n p) d -> n p d", p=128)
        with scope(C) as esp:
            bank = [C.ps(esp, "pbb%d" % i, [128, 512]) for i in range(7)]
            state = {"it": 0}

            def stage1a(u, tg, eb, sub, tt, es_):
                ub = u % 2
                xT = xnT[:, tt, :].rearrange("p (c t) -> p c t", t=128)
                scv = sc[:, tt, :].rearrange("p (h s n) -> p h s n", s=2, n=128)
                i0 = eb * 8 + sub * 4
                for dc in range(8):
                    S.op("pe", lambda e, dc=dc, xT=xT: e.matmul(bank[ub][:], lhsT=xT[:, dc, :], rhs=UT[es_][:, dc, sub * 512:(sub + 1) * 512], start=(dc == 0), stop=(dc == 7)),
                         reads=["bxnT", "UT%d" % es_], writes=["pbb%d" % ub])
                for h in range(8):
                    hs = state["it"] % NP; state["it"] += 1
                    if h >= 5:
                        S.op("pool", lambda e, h=h, hs=hs, scv=scv: e.tensor_tensor(out=Pt[hs][:].rearrange("p (i j) -> p i j", j=128), in0=bc(scv[:, h, 0, i0:i0 + 4].unsqueeze(2), [128, 4, 128]),
                                                                             in1=bc(scv[:, h, 1, :].unsqueeze(1), [128, 4, 128]), op=ALU.mult), reads=["bsc"], writes=["pP%d_%d" % (hs, il) for il in range(4)])
                    else:
                        for il in range(4):
                            S.op("act", lambda e, h=h, hs=hs, scv=scv, il=il: e.activation(out=Pt[hs][:, il * 128:(il + 1) * 128], in_=scv[:, h, 1, :], func=AF.Copy, scale=scv[:, h, 0, i0 + il:i0 + il + 1]),
                                 reads=["bsc"], writes=["pP%d_%d" % (hs, il)])
                    S.op("dve", lambda e, h=h, hs=hs: e.scalar_tensor_tensor(out=Wh[ub][h][:], in0=Pt[hs][:], scalar=kap[:, tt, h:h + 1], in1=Pt[hs][:], op0=ALU.is_ge, op1=ALU.mult),
                         reads=["pP%d_%d" % (hs, il) for il in range(4)] + ["btau"], writes=["pWh%d_%d" % (ub, h)])

            def stage1b(u, tg, eb, sub, tt, es_):
                ub = u % 2; gb = u % 3
                S.op("act", lambda e: e.activation(out=G[gb][:], in_=bank[ub][:], func=AF.Gelu), reads=["pbb%d" % ub], writes=["pG%d" % gb])
                for h in range(8):
                    S.op("pe", lambda e, h=h: e.matmul(bank[2 + ub][:], lhsT=idb[:], rhs=Wh[ub][h][:], start=(h == 0), stop=(h == 7)),
                         reads=["identb", "pWh%d_%d" % (ub, h)], writes=["pbb%d" % (2 + ub)])

            def stage2(u, tg, eb, sub, tt, es_):
                ub = u % 2; gb = u % 3
                first = (eb == 0)
                S.op("dve", lambda e: e.tensor_tensor(out=WA[ub][:], in0=bank[2 + ub][:], in1=G[gb][:], op=ALU.mult), reads=["pbb%d" % (2 + ub), "pG%d" % gb], writes=["pWA%d" % ub])
                pbt = bank[4][:].bitcast(BF16).rearrange("p (c n) -> p c n", n=128)[:, 0:4, :]
                transpose_chunks(C, WA[ub][:], "pWA%d" % ub, 4, pbt, "pbb4", WAT[ub][:], "pWAT%d" % ub, idb)
                for cb in range(2):
                    for ec in range(4):
                        S.op("pe", lambda e, cb=cb, ec=ec: e.matmul(bank[5 + cb][:], lhsT=WAT[ub][:, ec, :], rhs=Vb[es_][:, sub * 4 + ec, cb * 512:(cb + 1) * 512], start=(sub == 0 and ec == 0), stop=(sub == 1 and ec == 3)),
                             reads=["pWAT%d" % ub, "Vb%d" % es_], writes=["pbb%d" % (5 + cb)])
                    if sub == 1:
                        if first:
                            S.op("dve", lambda e, cb=cb: e.tensor_copy(out=acc[:, tt, cb * 512:(cb + 1) * 512], in_=bank[5 + cb][:]), reads=["pbb%d" % (5 + cb)], writes=["pacc%d" % tt])
                        else:
                            S.op("dve", lambda e, cb=cb: e.tensor_tensor(out=acc[:, tt, cb * 512:(cb + 1) * 512], in0=bank[5 + cb][:], in1=acc[:, tt, cb * 512:(cb + 1) * 512], op=ALU.add),
                                 reads=["pbb%d" % (5 + cb), "pacc%d" % tt], writes=["pacc%d" % tt])

            u = 0
            for tg in range(4):
                S.dma("sp", xnT[:], D["xnT_d"][tg * 4:(tg + 1) * 4].rearrange("n p f -> p n f"), reads=["xnT_d"], writes=["bxnT"])
                S.dma("sp", sc[:], D["sc_d"][tg * 4:(tg + 1) * 4].rearrange("n p f -> p n f"), reads=["sc_d"], writes=["bsc"])
                S.dma("sp", kap[:], D["tau_d"][tg * 4:(tg + 1) * 4].rearrange("n p f -> p n f"), reads=["tau_d"], writes=["btau"])
                units = []
                for eb in range(16):
                    es_ = (tg * 16 + eb) % 2
                    for tt in range(4):
                        for sub in range(2):
                            units.append((u, tg, eb, sub, tt, es_)); u += 1
                loaded = set()
                n = len(units)
                for k in range(n + 2):
                    for ebk in ([0] if k == 0 else []) + ([k // 8 + 1] if (k % 8 == 2 and k // 8 + 1 < 16) else []):
                        es_ = (tg * 16 + ebk) % 2
                        S.dma("sp", UT[es_][:], uTv[ebk], reads=["uT_b"], writes=["UT%d" % es_])
                        S.dma("sp", Vb[es_][:], vbv[ebk], reads=["v_b"], writes=["Vb%d" % es_])
                    if k < n:
                        stage1a(*units[k])
                    if 0 <= k - 1 < n:
                        stage1b(*units[k - 1])
                    if 0 <= k - 2 < n:
                        stage2(*units[k - 2])
                for tt in range(4):
                    sl = tt % 2; n = tg * 4 + tt
                    S.dma("sp", h2t[sl][:], hv[n], reads=["h2_d"], writes=["ph2t%d" % sl])
                    S.op("pool", lambda e, sl=sl, tt=tt: e.tensor_tensor(out=h2t[sl][:], in0=h2t[sl][:], in1=acc[:, tt, :], op=ALU.add), reads=["ph2t%d" % sl, "pacc%d" % tt], writes=["ph2t%d" % sl])
                    S.dma("sp", ov[n], h2t[sl][:], reads=["ph2t%d" % sl], writes=["out"])


_PROG = {}


def kernel(**inputs):
    sh = host_shared(inputs)
    sh.update(host_shared_rest(inputs))
    if "nc" not in _PROG:
        _PROG["nc"] = build_program(("mixer", "mem", "peer"))[0]
    nc = _PROG["nc"]
    names = set(IN_SHAPES) | set(IN_SHAPES_MEM) | set(IN_SHAPES_PEER)
    mem = np.asarray(inputs["mem"], np.float32)
    maps = []
    for c in range(8):
        m = {k: v for k, v in sh.items() if k in names}
        m.update(host_core(inputs, c))
        m["mem"] = np.ascontiguousarray(mem[c // 4])
        maps.append(m)
    res = run_bass_kernel_spmd(nc, maps, core_ids=list(range(8)))
    out = np.zeros((2, 8192, 1024), np.float32)
    for c in range(8):
        out[c // 4, (c % 4) * 2048:(c % 4 + 1) * 2048] = res.results[c]["out"]
    return out
```

```python
from contextlib import ExitStack, contextmanager
import numpy as np
import concourse.bass as bass
import concourse.mybir as mybir
from concourse.bass_utils import run_bass_kernel_spmd

F32 = mybir.dt.float32
BF16 = mybir.dt.bfloat16
I32 = mybir.dt.int32
AF = mybir.ActivationFunctionType
ALU = mybir.AluOpType
AX = mybir.AxisListType

ENGS = ("pe", "act", "dve", "pool", "sp")
TWO_PI = 6.283185307179586
EPS = 1e-6
NEG = -30000.0


class Sched:
    def __init__(self, nc, es, n_dma_sems=32):
        self.nc = nc
        self.streams = {e: [] for e in ENGS}
        self.sem = {e: es.enter_context(nc.semaphore("s_" + e)) for e in ("pe", "act", "dve", "pool")}
        self.cnt = {e: 0 for e in ("pe", "act", "dve", "pool")}
        self.dsem = [es.enter_context(nc.semaphore("s_dma%d" % i)) for i in range(n_dma_sems)]
        self.dcnt = [0] * n_dma_sems
        self.dnext = 0
        self.n_sw = 4
        self.dnext_sw = 0
        self.waited = {}
        self.last_w = {}
        self.readers = {}
        self.n_ops = 0

    def _deps(self, eng, reads, writes):
        deps = []
        for k in reads:
            if k in self.last_w:
                deps.append(self.last_w[k])
        for k in writes:
            if k in self.last_w:
                deps.append(self.last_w[k])
            deps.extend(self.readers.get(k, ()))
        need = {}
        for (sk, val, peng) in deps:
            if peng == "pe" and eng == "pe":
                continue
            if self.waited.get((eng, sk), 0) >= val:
                continue
            if need.get(sk, 0) < val:
                need[sk] = val
        return need

    def _semobj(self, sk):
        return self.sem[sk] if isinstance(sk, str) else self.dsem[sk]

    def _emit_waits(self, eng, need):
        for sk, val in need.items():
            self.waited[(eng, sk)] = val
            so = self._semobj(sk)
            self.streams[eng].append(lambda e, so=so, val=val: e.wait_ge(so, val))

    def _record(self, tok, reads, writes):
        for k in writes:
            self.last_w[k] = tok
            self.readers[k] = []
        for k in reads:
            if k not in writes:
                self.readers.setdefault(k, []).append(tok)

    def op(self, eng, fn, reads=(), writes=()):
        need = self._deps(eng, reads, writes)
        self._emit_waits(eng, need)
        self.cnt[eng] += 1
        val = self.cnt[eng]
        so = self.sem[eng]
        self.streams[eng].append(lambda e, fn=fn, so=so: fn(e).then_inc(so, 1))
        self._record((eng, val, eng), reads, writes)
        self.n_ops += 1

    def dma(self, q, out, in_, reads=(), writes=(), **kw):
        nhw = len(self.dsem) - self.n_sw
        if q == "pool":
            i = nhw + self.dnext_sw
            self.dnext_sw = (self.dnext_sw + 1) % self.n_sw
        else:
            i = self.dnext
            self.dnext = (self.dnext + 1) % nhw
        need = self._deps(q, reads, writes)
        prev = 16 * self.dcnt[i]
        if prev and self.waited.get((q, i), 0) < prev:
            need[i] = max(need.get(i, 0), prev)
        self._emit_waits(q, need)
        self.dcnt[i] += 1
        val = 16 * self.dcnt[i]
        so = self.dsem[i]
        self.streams[q].append(
            lambda e, out=out, in_=in_, so=so, kw=kw: e.dma_start(out=out, in_=in_, **kw).then_inc(so, 16))
        self._record((i, val, "dma"), reads, writes)
        self.n_ops += 1

    def barrier(self):
        for eng in ENGS:
            need = {}
            for pe_ in ("pe", "act", "dve", "pool"):
                v = self.cnt[pe_]
                if v and self.waited.get((eng, pe_), 0) < v:
                    need[pe_] = v
            for i, c in enumerate(self.dcnt):
                if c and self.waited.get((eng, i), 0) < 16 * c:
                    need[i] = 16 * c
            self._emit_waits(eng, need)

    def wait_all(self, eng, keys):
        need = {}
        for k in keys:
            if k in self.last_w:
                sk, val, _ = self.last_w[k]
                if self.waited.get((eng, sk), 0) < val and need.get(sk, 0) < val:
                    need[sk] = val
        self._emit_waits(eng, need)

    def emit(self):
        if not any(self.streams[e] for e in ENGS):
            return
        streams = self.streams
        self.streams = {e: [] for e in ENGS}
        self._emit_block(streams)

    def _emit_block(self, streams):
        self_streams = streams
        with self.nc.Block() as block:
            @block.tensor
            def _(e):
                for f in self_streams["pe"]:
                    f(e)

            @block.scalar
            def _(e):
                for f in self_streams["act"]:
                    f(e)

            @block.vector
            def _(e):
                for f in self_streams["dve"]:
                    f(e)

            @block.gpsimd
            def _(e):
                for f in self_streams["pool"]:
                    f(e)

            @block.sync
            def _(e):
                for f in self_streams["sp"]:
                    f(e)


class Ctx:
    def __init__(self, nc, S):
        self.nc = nc
        self.S = S
        self.uid = 0

    def sb(self, es, name, shape, dt=F32):
        self.uid += 1
        return es.enter_context(self.nc.sbuf_tensor("%s_%d" % (name, self.uid), list(shape), dt))

    def ps(self, es, name, shape, dt=F32):
        self.uid += 1
        return es.enter_context(self.nc.psum_tensor("%s_%d" % (name, self.uid), list(shape), dt))


@contextmanager
def scope(C):
    with ExitStack() as es:
        yield es
        C.S.barrier()
        C.S.emit()


def bc(ap, shape):
    return ap.to_broadcast(list(shape))


def make_ident(C, es, name="ident"):
    S = C.S
    idf = C.sb(es, name + "f", [128, 128])
    idb = C.sb(es, name + "b", [128, 128], BF16)
    S.op("pool", lambda e: e.memset(idf[:], 1.0), writes=[name + "f"])
    S.op("pool", lambda e: e.affine_select(out=idf[:], in_=idf[:], pattern=[[-1, 128]], compare_op=ALU.is_equal,
                                           fill=0.0, base=0, channel_multiplier=1), reads=[name + "f"], writes=[name + "f"])
    S.op("dve", lambda e: e.tensor_copy(out=idb[:], in_=idf[:]), reads=[name + "f"], writes=[name + "b"])
    return idf, idb


def sincos(C, es, ang, n, tag):
    S = C.S
    outs = []
    for which, off in (("s", 64.0), ("c", 64.25)):
        k = tag + which
        y = C.sb(es, k + "y", [128, n]); yi = C.sb(es, k + "yi", [128, n], I32); yf = C.sb(es, k + "yf", [128, n])
        m = C.sb(es, k + "m", [128, n]); o = C.sb(es, k + "o", [128, n])
        S.op("dve", lambda e, y=y, off=off: e.tensor_scalar(out=y[:], in0=ang, scalar1=1.0 / TWO_PI, scalar2=off, op0=ALU.mult, op1=ALU.add),
             reads=[tag + "ang"], writes=[k + "y"])
        S.op("dve", lambda e, y=y, yi=yi: e.tensor_copy(out=yi[:], in_=y[:]), reads=[k + "y"], writes=[k + "yi"])
        S.op("dve", lambda e, yi=yi, yf=yf: e.tensor_copy(out=yf[:], in_=yi[:]), reads=[k + "yi"], writes=[k + "yf"])
        S.op("dve", lambda e, y=y, yf=yf: e.tensor_tensor(out=y[:], in0=y[:], in1=yf[:], op=ALU.subtract), reads=[k + "y", k + "yf"], writes=[k + "y"])
        S.op("dve", lambda e, y=y, m=m: e.tensor_scalar(out=m[:], in0=y[:], scalar1=0.5, scalar2=None, op0=ALU.is_gt), reads=[k + "y"], writes=[k + "m"])
        S.op("dve", lambda e, y=y, m=m: e.tensor_tensor(out=y[:], in0=y[:], in1=m[:], op=ALU.subtract), reads=[k + "y", k + "m"], writes=[k + "y"])
        S.op("act", lambda e, y=y, o=o: e.activation(out=o[:], in_=y[:], func=AF.Sin, scale=TWO_PI), reads=[k + "y"], writes=[k + "o"])
        outs.append((o, k + "o"))
    return outs


def s5_params(C, es_keep, D):
    S, nc = C.S, C.nc
    P = {}
    LT2 = C.sb(es_keep, "LT2", [128, 8, 8, 2, 128], BF16)
    WPr = C.sb(es_keep, "WPr", [128, 9, 16]); WPi = C.sb(es_keep, "WPi", [128, 9, 16]); WPn = C.sb(es_keep, "WPn", [128, 9, 16])
    P.update(LT2=LT2, WPr=WPr, WPi=WPi, WPn=WPn)
    with scope(C) as es:
        sb = lambda name, shape, dt=F32: C.sb(es, name, shape, dt)
        Mz2 = sb("Mz2", [128, 16, 8, 128], BF16)
        CA = sb("CA", [128, 32, 128], BF16); CAs = sb("CAs", [128, 32, 128], BF16)
        LR = sb("LR", [128, 32]); LI = sb("LI", [128, 32]); LS = sb("LS", [128, 32])
        SG = sb("SG", [128, 2]); NV = sb("NV", [128, 23])
        P1B = sb("P1B", [128, 32, 16]); P2B = sb("P2B", [128, 32, 16]); P1C = sb("P1C", [128, 32, 16]); P2C = sb("P2C", [128, 32, 16])
        DD = sb("DD", [128, 16, 16])
        for t, nm in ((LR, "s5_lr"), (LI, "s5_li"), (LS, "s5_ls"), (SG, "s5_sg"), (NV, "s5_nv")):
            S.dma("sp", t[:], D[nm], writes=[nm])
        S.dma("sp", DD[:].rearrange("p e c -> p (e c)"), D["s5_dd"], writes=["s5_dd"])
        for t, nm in ((P1B, "s5_p1b"), (P2B, "s5_p2b"), (P1C, "s5_p1c"), (P2C, "s5_p2c")):
            S.dma("sp", t[:].rearrange("p g c -> p (g c)"), D[nm], writes=[nm])
        idf, idb = make_ident(C, es, "pid")
        STEP = sb("STEP", [128, 32]); AA = sb("AA", [128, 32]); PH = sb("PH", [128, 32])
        S.op("act", lambda e: e.activation(out=STEP[:], in_=LS[:], func=AF.Exp), reads=["s5_ls"], writes=["STEP"])
        S.op("dve", lambda e: e.tensor_tensor(out=AA[:], in0=LR[:], in1=STEP[:], op=ALU.mult), reads=["s5_lr", "STEP"], writes=["AA"])
        S.op("dve", lambda e: e.tensor_tensor(out=PH[:], in0=LI[:], in1=STEP[:], op=ALU.mult), reads=["s5_li", "STEP"], writes=["PH"])
        EXPO = sb("EXPO", [128, 32, 23]); ANG = sb("ANG", [128, 32, 23]); MAG = sb("MAG", [128, 32, 23])
        nvb = bc(NV[:].unsqueeze(1), [128, 32, 23])
        S.op("dve", lambda e: e.tensor_tensor(out=EXPO[:], in0=bc(AA[:].unsqueeze(2), [128, 32, 23]), in1=nvb, op=ALU.mult), reads=["AA", "s5_nv"], writes=["EXPO"])
        S.op("dve", lambda e: e.tensor_tensor(out=ANG[:], in0=bc(PH[:].unsqueeze(2), [128, 32, 23]), in1=nvb, op=ALU.mult), reads=["PH", "s5_nv"], writes=["pwang"])
        S.op("act", lambda e: e.activation(out=MAG[:], in_=EXPO[:], func=AF.Exp), reads=["EXPO"], writes=["MAG"])
        (sn, snk), (cs, csk) = sincos(C, es, ANG[:].rearrange("p g n -> p (g n)"), 32 * 23, "pw")
        CR = sb("CR", [128, 32, 23]); CI = sb("CI", [128, 32, 23])
        S.op("dve", lambda e: e.tensor_tensor(out=CR[:].rearrange("p g n -> p (g n)"), in0=MAG[:].rearrange("p g n -> p (g n)"), in1=cs[:], op=ALU.mult), reads=["MAG", csk], writes=["CR"])
        S.op("dve", lambda e: e.tensor_tensor(out=CI[:].rearrange("p g n -> p (g n)"), in0=MAG[:].rearrange("p g n -> p (g n)"), in1=sn[:], op=ALU.mult), reads=["MAG", snk], writes=["CI"])
        zr = sb("zr", [128, 32]); den = sb("den", [128, 32]); t0 = sb("t0", [128, 32]); fr = sb("fr", [128, 32]); fi = sb("fi", [128, 32])
        S.op("dve", lambda e: e.tensor_scalar(out=zr[:], in0=CR[:, :, 8], scalar1=-1.0, scalar2=None, op0=ALU.add), reads=["CR"], writes=["zr"])
        S.op("dve", lambda e: e.tensor_tensor(out=den[:], in0=LR[:], in1=LR[:], op=ALU.mult), reads=["s5_lr"], writes=["den"])
        S.op("dve", lambda e: e.tensor_tensor(out=t0[:], in0=LI[:], in1=LI[:], op=ALU.mult), reads=["s5_li"], writes=["t0"])
        S.op("dve", lambda e: e.tensor_tensor(out=den[:], in0=den[:], in1=t0[:], op=ALU.add), reads=["den", "t0"], writes=["den"])
        S.op("dve", lambda e: e.reciprocal(out=den[:], in_=den[:]), reads=["den"], writes=["den"])
        S.op("dve", lambda e: e.tensor_tensor(out=fr[:], in0=zr[:], in1=LR[:], op=ALU.mult), reads=["zr", "s5_lr"], writes=["fr"])
        S.op("dve", lambda e: e.tensor_tensor(out=t0[:], in0=CI[:, :, 8], in1=LI[:], op=ALU.mult), reads=["CI", "s5_li", "den"], writes=["t0"])
        S.op("dve", lambda e: e.tensor_tensor(out=fr[:], in0=fr[:], in1=t0[:], op=ALU.add), reads=["fr", "t0"], writes=["fr"])
        S.op("dve", lambda e: e.tensor_tensor(out=fr[:], in0=fr[:], in1=den[:], op=ALU.mult), reads=["fr", "den"], writes=["fr"])
        S.op("dve", lambda e: e.tensor_tensor(out=fi[:], in0=CI[:, :, 8], in1=LR[:], op=ALU.mult), reads=["CI", "s5_lr"], writes=["fi"])
        S.op("dve", lambda e: e.tensor_tensor(out=t0[:], in0=zr[:], in1=LI[:], op=ALU.mult), reads=["zr", "s5_li", "fr"], writes=["t0"])
        S.op("dve", lambda e: e.tensor_tensor(out=fi[:], in0=fi[:], in1=t0[:], op=ALU.subtract), reads=["fi", "t0"], writes=["fi"])
        S.op("dve", lambda e: e.tensor_tensor(out=fi[:], in0=fi[:], in1=den[:], op=ALU.mult), reads=["fi", "den"], writes=["fi"])
        BB1 = sb("BB1", [128, 32, 16]); BB2 = sb("BB2", [128, 32, 16]); ta = sb("ta", [128, 32, 16]); tb = sb("tb", [128, 32, 16])
        frb = bc(fr[:].unsqueeze(2), [128, 32, 16]); fib = bc(fi[:].unsqueeze(2), [128, 32, 16])
        fl = lambda t: t[:].rearrange("p g c -> p (g c)")
        S.op("dve", lambda e: e.tensor_tensor(out=ta[:], in0=P1B[:], in1=frb, op=ALU.mult), reads=["s5_p1b", "fr"], writes=["ta"])
        S.op("dve", lambda e: e.tensor_tensor(out=tb[:], in0=P2B[:], in1=fib, op=ALU.mult), reads=["s5_p2b", "fi"], writes=["tb"])
        S.op("dve", lambda e: e.scalar_tensor_tensor(out=fl(BB1), in0=fl(tb), scalar=SG[:, 0:1], in1=fl(ta), op0=ALU.mult, op1=ALU.add), reads=["ta", "tb", "s5_sg"], writes=["BB1"])
        S.op("dve", lambda e: e.tensor_tensor(out=ta[:], in0=P2B[:], in1=frb, op=ALU.mult), reads=["s5_p2b", "fr", "BB1"], writes=["ta"])
        S.op("dve", lambda e: e.tensor_tensor(out=tb[:], in0=P1B[:], in1=fib, op=ALU.mult), reads=["s5_p1b", "fi", "BB1"], writes=["tb"])
        S.op("dve", lambda e: e.scalar_tensor_tensor(out=fl(BB2), in0=fl(tb), scalar=SG[:, 1:2], in1=fl(ta), op0=ALU.mult, op1=ALU.add), reads=["ta", "tb", "s5_sg"], writes=["BB2"])
        t5 = sb("t5", [128, 32, 16]); t6 = sb("t6", [128, 32, 16])
        Rm = sb("Rm", [128, 32, 8, 16])
        Q1 = sb("Q1", [128, 32, 16]); Q2 = sb("Q2", [128, 32, 16])
        S.op("dve", lambda e: e.tensor_scalar(out=fl(Q1), in0=fl(P1C), scalar1=SG[:, 1:2], scalar2=None, op0=ALU.mult), reads=["s5_p1c", "s5_sg"], writes=["Q1"])
        S.op("dve", lambda e: e.tensor_scalar(out=fl(Q2), in0=fl(P2C), scalar1=SG[:, 0:1], scalar2=None, op0=ALU.mult), reads=["s5_p2c", "s5_sg"], writes=["Q2"])
        CAv = CA[:].rearrange("p g (t c) -> p g t c", c=16); CAsv = CAs[:].rearrange("p g (t c) -> p g t c", c=16)
        for t in range(8):
            for (dst, dkey, a_, akey, b_, bkey, idx) in ((Rm[:, :, t, :], "Rm", Q1, "Q1", P2C, "s5_p2c", 7 + t),
                                                         (CAv[:, :, t, :], "CA", Q1, "Q1", P2C, "s5_p2c", 15 + t),
                                                         (CAsv[:, :, t, :], "CAs", Q2, "Q2", P1C, "s5_p1c", 15 + t)):
                S.op("dve", lambda e, a_=a_, idx=idx: e.tensor_tensor(out=t5[:], in0=a_[:], in1=bc(CR[:, :, idx:idx + 1], [128, 32, 16]), op=ALU.mult), reads=[akey, "CR"], writes=["t5"])
                S.op("dve", lambda e, b_=b_, idx=idx: e.tensor_tensor(out=t6[:], in0=b_[:], in1=bc(CI[:, :, idx:idx + 1], [128, 32, 16]), op=ALU.mult), reads=[bkey, "CI"], writes=["t6"])
                S.op("dve", lambda e, dst=dst: e.tensor_tensor(out=dst, in0=t5[:], in1=t6[:], op=ALU.subtract), reads=["t5", "t6"], writes=[dkey])
        Wr = sb("Wr", [128, 9, 32]); Wi = sb("Wi", [128, 9, 32]); sq = sb("sq", [128, 32])
        S.op("dve", lambda e: e.tensor_copy(out=Wr[:, 0, :], in_=CR[:, :, 15]), reads=["CR"], writes=["Wr"])
        S.op("dve", lambda e: e.tensor_copy(out=Wi[:, 0, :], in_=CI[:, :, 15]), reads=["CI"], writes=["Wi"])
        for j in range(8):
            S.op("dve", lambda e, j=j: e.tensor_tensor(out=Wr[:, j + 1, :], in0=Wr[:, j, :], in1=Wr[:, j, :], op=ALU.mult), reads=["Wr"], writes=["Wr"])
            S.op("dve", lambda e, j=j: e.tensor_tensor(out=sq[:], in0=Wi[:, j, :], in1=Wi[:, j, :], op=ALU.mult), reads=["Wi"], writes=["sq"])
            S.op("dve", lambda e, j=j: e.tensor_tensor(out=Wr[:, j + 1, :], in0=Wr[:, j + 1, :], in1=sq[:], op=ALU.subtract), reads=["Wr", "sq"], writes=["Wr"])
            S.op("dve", lambda e, j=j: e.scalar_tensor_tensor(out=Wi[:, j + 1, :], in0=Wr[:, j, :], scalar=2.0, in1=Wi[:, j, :], op0=ALU.mult, op1=ALU.mult), reads=["Wr", "Wi"], writes=["Wi"])
        Wrv = Wr[:].rearrange("p j (a r w) -> p j a r w", r=2, w=2); Wiv = Wi[:].rearrange("p j (a r w) -> p j a r w", r=2, w=2)
        for r in range(2):
            pr_ = slice(64 * r, 64 * r + 64)
            for j in range(9):
                S.op("dve", lambda e, r=r, pr_=pr_, j=j: e.tensor_copy(out=WPr[pr_, j, :].rearrange("p (a w) -> p a w", w=2), in_=Wrv[pr_, j, :, r, :]), reads=["Wr"], writes=["WPr"])
                S.op("dve", lambda e, r=r, pr_=pr_, j=j: e.tensor_copy(out=WPi[pr_, j, :].rearrange("p (a w) -> p a w", w=2), in_=Wiv[pr_, j, :, r, :]), reads=["Wi"], writes=["WPi"])
        S.op("dve", lambda e: e.tensor_scalar(out=WPn[:].rearrange("p j q -> p (j q)"), in0=WPi[:].rearrange("p j q -> p (j q)"), scalar1=-1.0, scalar2=None, op0=ALU.mult), reads=["WPi"], writes=["WPn"])
        E = [[sb("E%d%d" % (h_, r), [128, 128]) for r in range(2)] for h_ in range(2)]
        for h_ in range(2):
            for r in range(2):
                S.op("pool", lambda e, h_=h_, r=r: e.memset(E[h_][r][:], 0.0), writes=["E%d%d" % (h_, r)])
                S.op("pool", lambda e, h_=h_, r=r: e.tensor_copy(out=E[h_][r][64 * h_:64 * h_ + 64, 64 * r:64 * r + 64], in_=idf[64 * h_:64 * h_ + 64, 64 * h_:64 * h_ + 64]),
                     reads=["pidf", "E%d%d" % (h_, r)], writes=["E%d%d" % (h_, r)])
        Lz = [sb("Lz%d" % i, [128, 32, 64]) for i in range(2)]
        for i in range(2):
            S.op("pool", lambda e, i=i: e.memset(Lz[i][:].rearrange("p g c -> p (g c)"), 0.0), writes=["Lz%d" % i])
        S.op("pool", lambda e: e.memset(Mz2[:].rearrange("p a b c -> p (a b c)"), 0.0), writes=["Mz2"])
        t5v = t5[:].rearrange("p (a j) c -> p a j c", j=4); t6v = t6[:].rearrange("p (a j) c -> p a j c", j=4)
        with scope(C) as esp:
            PM = C.ps(esp, "PM", [128, 16, 128]); PL = C.ps(esp, "PL", [128, 8, 2, 128])
            for s in range(8):
                i = 7 - s; sl = s % 2; lk = "Lz%d" % sl
                Lzv = Lz[sl][:].rearrange("p (a j) c -> p a j c", j=4)
                S.op("dve", lambda e, i=i: e.tensor_tensor(out=t5[:], in0=BB1[:], in1=bc(CR[:, :, i:i + 1], [128, 32, 16]), op=ALU.mult), reads=["BB1", "CR"], writes=["t5"])
                S.op("dve", lambda e, i=i: e.tensor_tensor(out=t6[:], in0=BB2[:], in1=bc(CI[:, :, i:i + 1], [128, 32, 16]), op=ALU.mult), reads=["BB2", "CI"], writes=["t6"])
                for j4 in range(4):
                    S.op("dve", lambda e, j4=j4, Lzv=Lzv: e.scalar_tensor_tensor(out=Lzv[:, :, j4, 16 * j4:16 * j4 + 16], in0=t6v[:, :, j4, :], scalar=SG[:, 0:1], in1=t5v[:, :, j4, :], op0=ALU.mult, op1=ALU.add),
                         reads=["t5", "t6", "s5_sg"], writes=[lk])
                for g in range(32):
                    chc = g // 8; hb = (g % 8) // 4; j4 = g % 4; e_ = chc * 4 + j4
                    rows = slice(64 * hb, 64 * hb + 64)
                    S.op("pe", lambda e, g=g, sl=sl, e_=e_, rows=rows: e.matmul(PM[rows, e_, :], lhsT=Lz[sl][:, g, :], rhs=Rm[:, g, :, :].rearrange("p t c -> p (t c)"), start=True, stop=True),
                         reads=[lk, "Rm"], writes=["PM"])
                for pr in range(16):
                    a_ = pr // 2; wp = pr % 2; chc = a_ // 2; hb = a_ % 2; e2 = chc * 2 + wp
                    rows = slice(64 * hb, 64 * hb + 64)
                    for h_ in range(2):
                        for r in range(2):
                            g = 4 * a_ + 2 * r + wp
                            S.op("pe", lambda e, g=g, sl=sl, e2=e2, rows=rows, h_=h_, r=r: e.matmul(PL[rows, e2, h_, :], lhsT=Lz[sl][:, g, :], rhs=E[h_][r][:], start=(r == 0), stop=(r == 1)),
                                 reads=[lk, "E%d%d" % (h_, r)], writes=["PL"])
                S.op("dve", lambda e, s=s: e.tensor_copy(out=Mz2[:, :, s, 16 * s:128], in_=PM[:, :, 16 * s:128]), reads=["PM"], writes=["Mz2"])
                S.op("dve", lambda e, s=s: e.tensor_tensor(out=Mz2[:, :, s, 16 * s:16 * s + 16], in0=PM[:, :, 16 * s:16 * s + 16], in1=DD[:], op=ALU.add), reads=["PM", "s5_dd"], writes=["Mz2"])
                S.op("act", lambda e, s=s: e.copy(out=LT2[:, :, s, :, :], in_=PL[:]), reads=["PL"], writes=["LT2"])
        S.dma("sp", D["Mz_d"], Mz2[:].rearrange("p a b c -> p (a b c)"), reads=["Mz2"], writes=["Mz_d"])
        S.dma("sp", D["CA_d"][:, 0, :], CA[:].rearrange("p g c -> p (g c)"), reads=["CA"], writes=["CA_d"])
        S.dma("sp", D["CA_d"][:, 1, :], CAs[:].rearrange("p g c -> p (g c)"), reads=["CAs"], writes=["CA_d"])
    return P


def load_weight_bf16(C, es, es_tmp, name, src, rows_chunks, ncols, gcol=None, q="sp"):
    S = C.S
    W = C.sb(es, name, [128, rows_chunks, ncols], BF16)
    stg = [C.sb(es_tmp, name + "_stg%d" % i, [128, ncols]) for i in range(2)]
    srcv = src.rearrange("(c p) n -> c p n", p=128)
    for c in range(rows_chunks):
        st = stg[c % 2]; sk = name + "_stg%d" % (c % 2)
        S.dma(q, st[:], srcv[c], writes=[sk])
        eng = "dve" if c % 2 == 0 else "pool"
        if gcol is not None:
            S.op(eng, lambda e, st=st, c=c: e.tensor_scalar(out=W[:, c, :], in0=st[:], scalar1=gcol[0][:, c:c + 1], scalar2=None, op0=ALU.mult),
                 reads=[sk, gcol[1]], writes=[name])
        else:
            S.op(eng, lambda e, st=st, c=c: e.tensor_copy(out=W[:, c, :], in_=st[:]), reads=[sk], writes=[name])
    return W


def rms_rstd(C, x_ap, xkey, junk, jkey, ss, rs, skey, n):
    S = C.S
    S.op("act", lambda e: e.activation(out=junk, in_=x_ap, func=AF.Square, accum_out=ss), reads=[xkey], writes=[skey + "_ss"])
    S.op("act", lambda e: e.activation(out=rs, in_=ss, func=AF.Sqrt, scale=1.0 / n, bias=EPS), reads=[skey + "_ss"], writes=[skey + "_sq"])
    S.op("dve", lambda e: e.reciprocal(out=rs, in_=rs), reads=[skey + "_sq"], writes=[skey])


def transpose_chunks(C, src, skey, nch, pbank, pkey, dst, dkey, idb, evac="act"):
    S = C.S
    for c in range(nch):
        S.op("pe", lambda e, c=c: e.transpose(out=pbank[:, c, :], in_=src[:, c * 128:(c + 1) * 128], identity=idb[:]), reads=[skey, "identb"], writes=[pkey])
    if evac == "act":
        S.op("act", lambda e: e.copy(out=dst, in_=pbank), reads=[pkey], writes=[dkey])
    else:
        S.op(evac, lambda e: e.tensor_copy(out=dst, in_=pbank), reads=[pkey], writes=[dkey])


def stage_mixer(C, D, dbg=None, upto=9, prep=None):
    S, nc = C.S, C.nc
    dbg = dbg or {}
    with scope(C) as es1:
        P = s5_params(C, es1, D)
        idf = C.sb(es1, "identf", [128, 128]); idb = C.sb(es1, "identb", [128, 128], BF16)
        S.op("pool", lambda e: e.memset(idf[:], 1.0), writes=["identf"])
        S.op("pool", lambda e: e.affine_select(out=idf[:], in_=idf[:], pattern=[[-1, 128]], compare_op=ALU.is_equal, fill=0.0, base=0, channel_multiplier=1), reads=["identf"], writes=["identf"])
        S.op("dve", lambda e: e.tensor_copy(out=idb[:], in_=idf[:]), reads=["identf"], writes=["identb"])
        if "WPr" in dbg:
            for nm in ("WPr", "WPi"):
                S.dma("sp", dbg[nm], P[nm][:].rearrange("p a b -> p (a b)"), reads=[nm], writes=["o_" + nm])
            S.dma("pool", dbg["LT2"], P["LT2"][:].rearrange("p a b c d -> p (a b c d)"), reads=["LT2"], writes=["o_LT2"])
            S.dma("pool", dbg["Mz"], D["Mz_d"], reads=["Mz_d"], writes=["o_Mz"])
            S.dma("pool", dbg["CA"], D["CA_d"].rearrange("p a b -> p (a b)"), reads=["CA_d"], writes=["o_CA"])
        if upto < 1:
            return
        carry_r = C.sb(es1, "carry_r", [128, 16]); carry_i = C.sb(es1, "carry_i", [128, 16])
        S.op("pool", lambda e: e.memset(carry_r[:], 0.0), writes=["carry_r"])
        S.op("pool", lambda e: e.memset(carry_i[:], 0.0), writes=["carry_i"])
        TRE = C.sb(es1, "TRE", [128, 16, 257]); TIM = C.sb(es1, "TIM", [128, 16, 257])
        uT = C.sb(es1, "uT", [128, 4, 8, 256], BF16)
        with scope(C) as es2:
            _mixer_passes(C, es2, D, P, idb, carry_r, carry_i, TRE, TIM, uT, dbg)
        if "carry" in dbg:
            S.dma("sp", dbg["carry"][:, 0:16], carry_r[:], reads=["carry_r"], writes=["o_carry"])
            S.dma("sp", dbg["carry"][:, 16:32], carry_i[:], reads=["carry_i"], writes=["o_carry2"])
        if upto < 2:
            return
        with scope(C) as es3:
            ytm = C.sb(es3, "ytm", [128, 2, 8, 512], BF16)
            with scope(C) as es4:
                _s5_scan_out(C, es4, D, P, idb, TRE, TIM, uT, ytm, carry_r, carry_i, dbg)
            if upto < 3:
                return
            _s5_glu_out(C, es3, D, idb, ytm, dbg)
    if upto < 4:
        return
    with scope(C) as es5:
        idf = C.sb(es5, "identf", [128, 128]); idb = C.sb(es5, "identb", [128, 128], BF16)
        S.op("pool", lambda e: e.memset(idf[:], 1.0), reads=[], writes=["identf"])
        S.op("pool", lambda e: e.affine_select(out=idf[:], in_=idf[:], pattern=[[-1, 128]], compare_op=ALU.is_equal, fill=0.0, base=0, channel_multiplier=1), reads=["identf"], writes=["identf"])
        S.op("dve", lambda e: e.tensor_copy(out=idb[:], in_=idf[:]), reads=["identf"], writes=["identb"])
        _attention(C, es5, D, idb, dbg, prep)


def _mixer_passes(C, es, D, P, idb, carry_r, carry_i, TRE, TIM, uT, dbg):
    S = C.S
    sb = lambda name, shape, dt=F32: C.sb(es, name, shape, dt)
    gin = sb("gin", [128, 8])
    S.dma("sp", gin[:], D["g_mix"], writes=["gin"])
    with scope(C) as est:
        Wb = load_weight_bf16(C, es, est, "Wb", D["w_in"], 8, 2048, gcol=(gin, "gin"))
    gq = sb("gq", [128, 64]); gk = sb("gk", [128, 64]); hv = sb("hv", [128, 4])
    S.dma("sp", gq[:], D["att_q_g"].partition_broadcast(128), writes=["gq"])
    S.dma("sp", gk[:], D["att_k_g"].partition_broadcast(128), writes=["gk"])
    S.dma("sp", hv[:], D["hvalid"], writes=["hv"])
    S.op("dve", lambda e: e.tensor_scalar(out=gq[:], in0=gq[:], scalar1=0.125, scalar2=None, op0=ALU.mult), reads=["gq"], writes=["gq"])
    xt = [sb("xt%d" % i, [128, 1024]) for i in range(2)]
    junk = sb("junk", [128, 1024]); st = [sb("st%d" % i, [128, 4]) for i in range(2)]
    xn = [sb("xn%d" % i, [128, 1024], BF16) for i in range(2)]
    xnT = [sb("xnT%d" % i, [128, 8, 512], BF16) for i in range(2)]
    qkv = sb("qkv", [128, 3, 512]); sq = sb("sq2", [128, 512]); qst = sb("qst", [128, 4, 8])
    qn = sb("qn", [128, 2, 512], BF16)
    kTs = [sb("kTs%d" % i, [128, 4, 128], BF16) for i in range(2)]; qTs = [sb("qTs%d" % i, [128, 4, 128], BF16) for i in range(2)]
    Vs = [sb("Vs%d" % i, [128, 8, 65], BF16) for i in range(2)]
    tA = [(sb("tAr%d" % i, [128, 256]), sb("tAi%d" % i, [128, 256])) for i in range(2)]
    tB = [(sb("tBr%d" % i, [128, 128]), sb("tBi%d" % i, [128, 128])) for i in range(2)]
    REDr = sb("REDr", [128, 16]); REDi = sb("REDi", [128, 16]); c1 = sb("c1", [128, 16]); c2 = sb("c2", [128, 16]); c3 = sb("c3", [128, 16])
    with scope(C) as esp:
        bank = [C.ps(esp, "bk%d" % i, [128, 512]) for i in range(8)]
        xv = D["x_ext"].rearrange("(n p) d -> n p d", p=128)
        tile_ctr = 0
        for q in range(4):
            for blk in range(4):
                bslot = (q * 4 + blk) % 2
                xT = xnT[bslot]; xTk = "xnT%d" % bslot
                for tt in range(4):
                    n_tile = q * 16 + blk * 4 + tt
                    sl = tile_ctr % 2; tile_ctr += 1
                    x_ = xt[sl]; xk = "xt%d" % sl
                    S.dma("sp", x_[:], xv[n_tile], writes=[xk])
                    rms_rstd(C, x_[:], xk, junk[:], "junk", st[sl][:, 0:1], st[sl][:, 1:2], "st%d" % sl, 1024)
                    S.op("dve", lambda e, x_=x_, sl=sl: e.tensor_scalar(out=xn[sl][:], in0=x_[:], scalar1=st[sl][:, 1:2], scalar2=None, op0=ALU.mult),
                         reads=[xk, "st%d" % sl], writes=["xn%d" % sl])
                    pb = bank[0][:].bitcast(BF16).rearrange("p (c n) -> p c n", n=128)
                    transpose_chunks(C, xn[sl][:], "xn%d" % sl, 8, pb, "bk0", xT[:, :, tt * 128:(tt + 1) * 128], xTk, idb)
                for chc in range(4):
                    bi = 1 + (chc % 2); bkk = "bk%d" % bi
                    for dc in range(8):
                        S.op("pe", lambda e, chc=chc, dc=dc, bi=bi, xT=xT: e.matmul(bank[bi][:], lhsT=Wb[:, dc, 1536 + chc * 128:1536 + (chc + 1) * 128], rhs=xT[:, dc, :], start=(dc == 0), stop=(dc == 7)),
                             reads=["Wb", xTk], writes=[bkk])
                    eng = "act" if chc % 2 == 0 else "dve"
                    src = bank[bi][:].rearrange("p (k s) -> p s k", s=8)
                    dst = uT[:, chc, :, blk * 64:(blk + 1) * 64]
                    if eng == "act":
                        S.op("act", lambda e, src=src, dst=dst: e.copy(out=dst, in_=src), reads=[bkk], writes=["uT"])
                    else:
                        S.op("dve", lambda e, src=src, dst=dst: e.tensor_copy(out=dst, in_=src), reads=[bkk], writes=["uT"])
                need_kv = (q == 3) or (q == 2 and blk == 3)
                need_q = (q == 3)
                if need_kv:
                    for tt in range(4):
                        n_tile = q * 16 + blk * 4 + tt
                        kvt = n_tile - 44
                        sl = kvt % 2
                        projs = [(1, 512, 3), (2, 1024, 4)] + ([(0, 0, 5)] if need_q else [])
                        for (pi, c0, bi) in projs:
                            for dc in range(8):
                                S.op("pe", lambda e, dc=dc, bi=bi, c0=c0, tt=tt, xT=xT: e.matmul(bank[bi][:], lhsT=xT[:, dc, tt * 128:(tt + 1) * 128], rhs=Wb[:, dc, c0:c0 + 512], start=(dc == 0), stop=(dc == 7)),
                                     reads=["Wb", xTk], writes=["bk%d" % bi])
                        S.op("act", lambda e, sl=sl: e.copy(out=Vs[sl][:, :, 0:64], in_=bank[4][:].rearrange("p (h d) -> p h d", d=64)), reads=["bk4"], writes=["Vs%d" % sl])
                        if kvt < 4:
                            S.op("pool", lambda e, sl=sl, kvt=kvt: e.tensor_copy(out=Vs[sl][:, :, 64], in_=bc(hv[:, kvt:kvt + 1], [128, 8])), reads=["hv"], writes=["Vs%d" % sl])
                        else:
                            S.op("pool", lambda e, sl=sl: e.memset(Vs[sl][:, :, 64], 1.0), reads=[], writes=["Vs%d" % sl])
                        S.dma("sp", D["V_d"][kvt], Vs[sl][:].rearrange("p h d -> p (h d)"), reads=["Vs%d" % sl], writes=["V_d"])
                        for (pi, bi, gt, gkey, dstT, dkey, dram, ncol_t) in ([(1, 3, gk, "gk", kTs[sl], "kTs%d" % sl, D["kT_d"], kvt)] +
                                                                          ([(0, 5, gq, "gq", qTs[sl], "qTs%d" % sl, D["qT_d"], kvt - 4)] if need_q else [])):
                            qs = qkv[:, pi, :]; qk_ = "qkv%d" % pi
                            S.op("act", lambda e, qs=qs, bi=bi: e.copy(out=qs, in_=bank[bi][:]), reads=["bk%d" % bi], writes=[qk_])
                            S.op("pool", lambda e, qs=qs: e.tensor_tensor(out=sq[:], in0=qs, in1=qs, op=ALU.mult), reads=[qk_], writes=["sq2"])
                            S.op("dve", lambda e, pi=pi: e.tensor_reduce(out=qst[:, pi, :], in_=sq[:].rearrange("p (h d) -> p h d", d=64), axis=AX.X, op=ALU.add), reads=["sq2"], writes=["qst%d" % pi])
                            S.op("act", lambda e, pi=pi: e.activation(out=qst[:, 2 + pi, :], in_=qst[:, pi, :], func=AF.Sqrt, scale=1.0 / 64, bias=EPS), reads=["qst%d" % pi], writes=["qsq%d" % pi])
                            S.op("dve", lambda e, pi=pi: e.reciprocal(out=qst[:, 2 + pi, :], in_=qst[:, 2 + pi, :]), reads=["qsq%d" % pi], writes=["qrs%d" % pi])
                            S.op("dve", lambda e, qs=qs, pi=pi: e.tensor_tensor(out=qs.rearrange("p (h d) -> p h d", d=64), in0=qs.rearrange("p (h d) -> p h d", d=64),
                                                                        in1=bc(qst[:, 2 + pi, :].unsqueeze(2), [128, 8, 64]), op=ALU.mult), reads=[qk_, "qrs%d" % pi], writes=[qk_])
                            S.op("pool", lambda e, qs=qs, pi=pi, gt=gt: e.tensor_tensor(out=qn[:, pi, :].rearrange("p (h d) -> p h d", d=64), in0=qs.rearrange("p (h d) -> p h d", d=64),
                                                                               in1=bc(gt[:].unsqueeze(1), [128, 8, 64]), op=ALU.mult), reads=[qk_, gkey], writes=["qn%d" % pi])
                            pb = bank[6][:].bitcast(BF16).rearrange("p (c n) -> p c n", n=128)[:, 0:4, :]
                            transpose_chunks(C, qn[:, pi, :], "qn%d" % pi, 4, pb, "bk6", dstT[:], dkey, idb)
                            S.dma("sp", dram.rearrange("p (c n) -> p c n", c=4)[:, :, ncol_t * 128:(ncol_t + 1) * 128], dstT[:], reads=[dkey], writes=["qkT_d"])
            for pair in range(16):
                psl = pair % 2
                br = bank[1 + 2 * psl]; bim = bank[2 + 2 * psl]; brk = "bk%d" % (1 + 2 * psl); bik = "bk%d" % (2 + 2 * psl)
                a_ = pair // 2; wp = pair % 2; chc = a_ // 2; hb = a_ % 2; e2 = chc * 2 + wp
                rows = slice(64 * hb, 64 * hb + 64)
                for half, (bkt, bkk) in enumerate(((br, brk), (bim, bik))):
                    for s in range(8):
                        S.op("pe", lambda e, e2=e2, s=s, half=half, rows=rows, chc=chc, bkt=bkt: e.matmul(bkt[:, 0:256], lhsT=P["LT2"][rows, e2, s, half, :], rhs=uT[rows, chc, s, :], start=(s == 0), stop=(s == 7)),
                             reads=["LT2", "uT"], writes=[bkk])
                if q < 3:
                    ar_, ai_ = tA[psl]; ark, aik = "tAr%d" % psl, "tAi%d" % psl
                    b_r, b_i = tB[psl]; brk2, bik2 = "tBr%d" % psl, "tBi%d" % psl
                    S.op("act", lambda e, ar_=ar_, br=br: e.copy(out=ar_[:], in_=br[:, 0:256]), reads=[brk], writes=[ark])
                    S.op("act", lambda e, ai_=ai_, bim=bim: e.copy(out=ai_[:], in_=bim[:, 0:256]), reads=[bik], writes=[aik])
                    src = (ar_, ai_, ark, aik); dst = (b_r, b_i, brk2, bik2)
                    for j in range(8):
                        n = 256 >> j; h = n // 2
                        sr, si, srk, sik = src; dr, di, drk, dik = dst
                        wr = P["WPr"][:, j, pair:pair + 1]; wi = P["WPi"][:, j, pair:pair + 1]; wn = P["WPn"][:, j, pair:pair + 1]
                        if j == 7:
                            odr, odi, odrk, odik = REDr[:, pair:pair + 1], REDi[:, pair:pair + 1], "REDr", "REDi"
                        else:
                            odr, odi, odrk, odik = dr[:, 0:h], di[:, 0:h], drk, dik
                        S.op("dve", lambda e, odr=odr, sr=sr, wr=wr, n=n: e.scalar_tensor_tensor(out=odr, in0=sr[:, 0:n:2], scalar=wr, in1=sr[:, 1:n:2], op0=ALU.mult, op1=ALU.add), reads=[srk, "WPr"], writes=[odrk])
                        S.op("dve", lambda e, odr=odr, si=si, wn=wn, n=n: e.scalar_tensor_tensor(out=odr, in0=si[:, 0:n:2], scalar=wn, in1=odr, op0=ALU.mult, op1=ALU.add), reads=[sik, "WPn", odrk], writes=[odrk])
                        S.op("dve", lambda e, odi=odi, si=si, wr=wr, n=n: e.scalar_tensor_tensor(out=odi, in0=si[:, 0:n:2], scalar=wr, in1=si[:, 1:n:2], op0=ALU.mult, op1=ALU.add), reads=[sik, "WPr"], writes=[odik])
                        S.op("dve", lambda e, odi=odi, sr=sr, wi=wi, n=n: e.scalar_tensor_tensor(out=odi, in0=sr[:, 0:n:2], scalar=wi, in1=odi, op0=ALU.mult, op1=ALU.add), reads=[srk, "WPi", odik], writes=[odik])
                        src, dst = dst, src
                else:
                    S.op("act", lambda e, pair=pair, br=br: e.copy(out=TRE[:, pair, 1:257], in_=br[:, 0:256]), reads=[brk], writes=["TRE%d" % pair])
                    S.op("act", lambda e, pair=pair, bim=bim: e.copy(out=TIM[:, pair, 1:257], in_=bim[:, 0:256]), reads=[bik], writes=["TIM%d" % pair])
            if q < 3:
                w8r = P["WPr"][:, 8, :]; w8i = P["WPi"][:, 8, :]
                S.op("dve", lambda e: e.tensor_tensor(out=c1[:], in0=w8r, in1=carry_r[:], op=ALU.mult), reads=["WPr", "carry_r"], writes=["c1"])
                S.op("dve", lambda e: e.tensor_tensor(out=c2[:], in0=w8i, in1=carry_i[:], op=ALU.mult), reads=["WPi", "carry_i"], writes=["c2"])
                S.op("dve", lambda e: e.tensor_tensor(out=c1[:], in0=c1[:], in1=c2[:], op=ALU.subtract), reads=["c1", "c2"], writes=["c1"])
                S.op("dve", lambda e: e.tensor_tensor(out=c1[:], in0=c1[:], in1=REDr[:], op=ALU.add), reads=["c1", "REDr"], writes=["c1"])
                S.op("dve", lambda e: e.tensor_tensor(out=c2[:], in0=w8r, in1=carry_i[:], op=ALU.mult), reads=["WPr", "carry_i", "c1"], writes=["c2"])
                S.op("dve", lambda e: e.tensor_tensor(out=c3[:], in0=w8i, in1=carry_r[:], op=ALU.mult), reads=["WPi", "carry_r"], writes=["c3"])
                S.op("dve", lambda e: e.tensor_tensor(out=c2[:], in0=c2[:], in1=c3[:], op=ALU.add), reads=["c2", "c3"], writes=["c2"])
                S.op("dve", lambda e: e.tensor_tensor(out=carry_i[:], in0=c2[:], in1=REDi[:], op=ALU.add), reads=["c2", "REDi"], writes=["carry_i"])
                S.op("dve", lambda e: e.tensor_copy(out=carry_r[:], in_=c1[:]), reads=["c1"], writes=["carry_r"])


def _s5_scan_out(C, es, D, P, idb, TRE, TIM, uT, ytm, carry_r, carry_i, dbg):
    S = C.S
    sb = lambda name, shape, dt=F32: C.sb(es, name, shape, dt)
    Mz2 = sb("Mz2s", [128, 16, 8, 128], BF16); CAA = sb("CAA", [128, 2, 32, 128], BF16)
    S.dma("sp", Mz2[:].rearrange("p a b c -> p (a b c)"), D["Mz_d"], reads=["Mz_d"], writes=["Mz2s"])
    S.dma("sp", CAA[:].rearrange("p a g c -> p a (g c)"), D["CA_d"], reads=["CA_d"], writes=["CAA"])
    tmr = [sb("tmr%d" % i, [128, 256]) for i in range(2)]; tmi = [sb("tmi%d" % i, [128, 256]) for i in range(2)]
    Tbr = [sb("Tbr%d" % i, [128, 256], BF16) for i in range(2)]; Tbi = [sb("Tbi%d" % i, [128, 256], BF16) for i in range(2)]
    Yg = [sb("Yg%d" % i, [128, 256], BF16) for i in range(2)]
    ysum = [sb("ysum%d" % i, [128, 256]) for i in range(2)]
    import os
    CUT = int(os.environ.get("SCAN_CUT", "9"))
    with scope(C) as esp:
        bank = [C.ps(esp, "sbk%d" % i, [128, 512]) for i in range(6)]
        for pair in range(16):
            sl = pair % 2
            a_ = pair // 2; wp = pair % 2; chc = a_ // 2; hb = a_ % 2
            rows = slice(64 * hb, 64 * hb + 64)
            kr, ki = "TRE%d" % pair, "TIM%d" % pair
            S.op("pool", lambda e, pair=pair: e.tensor_copy(out=TRE[:, pair, 0:1], in_=carry_r[:, pair:pair + 1]), reads=["carry_r"], writes=[kr])
            S.op("pool", lambda e, pair=pair: e.tensor_copy(out=TIM[:, pair, 0:1], in_=carry_i[:, pair:pair + 1]), reads=["carry_i"], writes=[ki])
            if CUT < 1:
                continue
            for j in range(9):
                d = 1 << j; m = 257 - d
                wr = P["WPr"][:, j, pair:pair + 1]; wi = P["WPi"][:, j, pair:pair + 1]; wn = P["WPn"][:, j, pair:pair + 1]
                tr = tmr[sl][:, 0:m]; ti = tmi[sl][:, 0:m]; trk = "tmr%d" % sl; tik = "tmi%d" % sl
                S.op("dve", lambda e, tr=tr, pair=pair, m=m, wr=wr: e.tensor_scalar(out=tr, in0=TRE[:, pair, 0:m], scalar1=wr, scalar2=None, op0=ALU.mult), reads=[kr, "WPr"], writes=[trk])
                S.op("dve", lambda e, tr=tr, pair=pair, m=m, wn=wn: e.scalar_tensor_tensor(out=tr, in0=TIM[:, pair, 0:m], scalar=wn, in1=tr, op0=ALU.mult, op1=ALU.add), reads=[ki, "WPn", trk], writes=[trk])
                S.op("dve", lambda e, ti=ti, pair=pair, m=m, wr=wr: e.tensor_scalar(out=ti, in0=TIM[:, pair, 0:m], scalar1=wr, scalar2=None, op0=ALU.mult), reads=[ki, "WPr"], writes=[tik])
                S.op("dve", lambda e, ti=ti, pair=pair, m=m, wi=wi: e.scalar_tensor_tensor(out=ti, in0=TRE[:, pair, 0:m], scalar=wi, in1=ti, op0=ALU.mult, op1=ALU.add), reads=[kr, "WPi", tik], writes=[tik])
                S.op("pool", lambda e, tr=tr, pair=pair, d=d: e.tensor_tensor(out=TRE[:, pair, d:257], in0=TRE[:, pair, d:257], in1=tr, op=ALU.add), reads=[kr, trk], writes=[kr])
                S.op("pool", lambda e, ti=ti, pair=pair, d=d: e.tensor_tensor(out=TIM[:, pair, d:257], in0=TIM[:, pair, d:257], in1=ti, op=ALU.add), reads=[ki, tik], writes=[ki])
            if CUT < 2:
                continue
            S.op("act", lambda e, pair=pair, sl=sl: e.copy(out=Tbr[sl][:], in_=TRE[:, pair, 0:256]), reads=[kr], writes=["Tbr%d" % sl])
            S.op("act", lambda e, pair=pair, sl=sl: e.copy(out=Tbi[sl][:], in_=TIM[:, pair, 0:256]), reads=[ki], writes=["Tbi%d" % sl])
            for r in range(2):
                g = 4 * a_ + 2 * r + wp
                e_ = chc * 4 + (g % 4)
                pr = slice(64 * r, 64 * r + 64)
                yb = bank[r]; ybk = "sbk%d" % r
                for s in range(8):
                    S.op("pe", lambda e, e_=e_, s=s, rows=rows, chc=chc, yb=yb: e.matmul(yb[:, 0:256], lhsT=Mz2[rows, e_, s, :], rhs=uT[rows, chc, s, :], start=(s == 0), stop=(s == 7)),
                         reads=["Mz2s", "uT"], writes=[ybk])
                zb_ = bank[4 + r]; zbk = "sbk%d" % (4 + r)
                S.op("pe", lambda e, g=g, pr=pr, zb_=zb_, r=r, sl=sl: e.matmul(zb_[:, 0:256], lhsT=CAA[pr, r, g, :], rhs=Tbr[sl][pr, :], start=True, stop=False), reads=["CAA", "Tbr%d" % sl], writes=[zbk])
                S.op("pe", lambda e, g=g, pr=pr, zb_=zb_, r=r, sl=sl: e.matmul(zb_[:, 0:256], lhsT=CAA[pr, 1 - r, g, :], rhs=Tbi[sl][pr, :], start=False, stop=True), reads=["CAA", "Tbi%d" % sl], writes=[zbk])
                if CUT < 3:
                    continue
                S.op("act", lambda e, r=r, zb_=zb_: e.copy(out=ysum[r][:], in_=zb_[:, 0:256]), reads=[zbk], writes=["ysum%d" % r])
                S.op("dve", lambda e, r=r, yb=yb: e.tensor_tensor(out=ysum[r][:], in0=yb[:, 0:256], in1=ysum[r][:], op=ALU.add), reads=[ybk, "ysum%d" % r], writes=["ysum%d" % r])
                S.op("act", lambda e, r=r: e.activation(out=Yg[r][:], in_=ysum[r][:], func=AF.Gelu), reads=["ysum%d" % r], writes=["Yg%d" % r])
                if CUT < 4:
                    continue
                pT = bank[2 + r][:].bitcast(BF16).rearrange("p (c n) -> p c n", n=128)
                for kb in range(2):
                    S.op("pe", lambda e, r=r, kb=kb, pT=pT: e.transpose(out=pT[:, kb, :], in_=Yg[r][:, kb * 128:(kb + 1) * 128], identity=idb[:]), reads=["Yg%d" % r, "identb"], writes=["sbk%d" % (2 + r)])
                for kb in range(2):
                    S.op("dve", lambda e, g=g, kb=kb, pT=pT: e.tensor_copy(out=ytm[:, kb, :, 16 * g:16 * g + 16], in_=pT[:, kb, :].rearrange("p (t c) -> p t c", c=16)),
                         reads=["sbk%d" % (2 + r)], writes=["ytm"])


def _s5_glu_out(C, es, D, idb, ytm, dbg):
    S = C.S
    sb = lambda name, shape, dt=F32: C.sb(es, name, shape, dt)
    gso = sb("gso", [128, 4]); bgl = sb("bgl", [128, 512])
    S.dma("sp", gso[:], D["g_ssm_out"], writes=["gso"])
    S.dma("sp", bgl[:], D["b_glu"].partition_broadcast(128), writes=["bgl"])
    with scope(C) as est:
        Wg = load_weight_bf16(C, es, est, "Wg", D["w_glu"], 4, 512)
    with scope(C) as est:
        Wo = load_weight_bf16(C, es, est, "Wos", D["w_out"][512:1024, :], 4, 1024, gcol=(gso, "gso"))
    yT = sb("yT", [128, 4, 128], BF16); zb = sb("zb", [128, 512]); ssm = sb("ssm", [128, 512]); junk = sb("junk3", [128, 512])
    st = sb("st3", [128, 2]); sn = sb("sn", [128, 512], BF16); snT = sb("snT", [128, 4, 128], BF16)
    ho = [sb("ho%d" % i, [128, 1024]) for i in range(2)]
    hsv = D["hs_d"].rearrange("(k t) d -> t k d", t=8)
    with scope(C) as esp:
        bank = [C.ps(esp, "gbk%d" % i, [128, 512]) for i in range(5)]
        it = 0
        for kb in range(2):
            for t in range(8):
                sl = it % 2; it += 1
                y = ytm[:, kb, t, :]
                pT = bank[0][:].bitcast(BF16).rearrange("p (c n) -> p c n", n=128)[:, 0:4, :]
                transpose_chunks(C, y, "ytm", 4, pT, "gbk0", yT[:], "yT", idb)
                for c in range(4):
                    S.op("pe", lambda e, c=c: e.matmul(bank[1][:], lhsT=yT[:, c, :], rhs=Wg[:, c, :], start=(c == 0), stop=(c == 3)), reads=["yT", "Wg"], writes=["gbk1"])
                S.op("dve", lambda e: e.tensor_tensor(out=zb[:], in0=bank[1][:], in1=bgl[:], op=ALU.add), reads=["gbk1", "bgl"], writes=["zb"])
                S.op("act", lambda e: e.activation(out=zb[:], in_=zb[:], func=AF.Sigmoid), reads=["zb"], writes=["zb"])
                S.op("pool", lambda e, y=y: e.tensor_tensor(out=ssm[:], in0=y, in1=zb[:], op=ALU.mult), reads=["ytm", "zb"], writes=["ssm"])
                if "ssm" in dbg:
                    S.dma("sp", dbg["ssm"].rearrange("(k t) d -> t k d", t=8)[t, kb * 128:(kb + 1) * 128, :], ssm[:], reads=["ssm"], writes=["o_ssm"])
                rms_rstd(C, ssm[:], "ssm", junk[:], "junk3", st[:, 0:1], st[:, 1:2], "st3", 512)
                S.op("dve", lambda e: e.tensor_scalar(out=sn[:], in0=ssm[:], scalar1=st[:, 1:2], scalar2=None, op0=ALU.mult), reads=["ssm", "st3"], writes=["sn"])
                pT2 = bank[2][:].bitcast(BF16).rearrange("p (c n) -> p c n", n=128)[:, 0:4, :]
                transpose_chunks(C, sn[:], "sn", 4, pT2, "gbk2", snT[:], "snT", idb)
                for cb in range(2):
                    for c in range(4):
                        S.op("pe", lambda e, c=c, cb=cb: e.matmul(bank[3 + cb][:], lhsT=snT[:, c, :], rhs=Wo[:, c, cb * 512:(cb + 1) * 512], start=(c == 0), stop=(c == 3)), reads=["snT", "Wos"], writes=["gbk%d" % (3 + cb)])
                    S.op("act", lambda e, cb=cb, sl=sl: e.copy(out=ho[sl][:, cb * 512:(cb + 1) * 512], in_=bank[3 + cb][:]), reads=["gbk%d" % (3 + cb)], writes=["ho%d" % sl])
                S.dma("sp", hsv[t, kb * 128:(kb + 1) * 128, :], ho[sl][:], reads=["ho%d" % sl], writes=["hs_d"])


def _attention(C, es, D, idb, dbg, prep=None):
    S = C.S
    sb = lambda name, shape, dt=F32: C.sb(es, name, shape, dt)
    kT = sb("kT", [128, 4, 2560], BF16); qT = sb("qT", [128, 4, 2048], BF16); V = sb("Vall", [128, 20, 520], BF16)
    qZ = sb("qZ", [128, 8, 2048], BF16)
    S.dma("sp", kT[:].rearrange("p c n -> p (c n)"), D["kT_d"], reads=["qkT_d"], writes=["kT"])
    S.dma("sp", qT[:].rearrange("p c n -> p (c n)"), D["qT_d"], reads=["qkT_d"], writes=["qT"])
    S.dma("sp", V[:], D["V_d"].rearrange("t p n -> p t n"), reads=["V_d"], writes=["Vall"])
    S.op("pool", lambda e: e.memset(qZ[:].rearrange("p h n -> p (h n)"), 0.0), writes=["qZ"])
    for h in range(8):
        rws = slice(64 * (h % 2), 64 * (h % 2) + 64)
        S.op("dve" if h % 2 else "act", (lambda e, h=h, rws=rws: e.tensor_copy(out=qZ[rws, h, :], in_=qT[rws, h // 2, :])) if h % 2 else (lambda e, h=h, rws=rws: e.copy(out=qZ[rws, h, :], in_=qT[rws, h // 2, :])),
             reads=["qT", "qZ"], writes=["qZ"])
    BT = sb("BT", [128, 8, 5, 128], BF16)
    gao = sb("gao", [128, 4])
    S.dma("sp", gao[:], D["g_att_out"], writes=["gao"])
    with scope(C) as est:
        stg = C.sb(est, "btstg", [128, 640])
        for h in range(8):
            S.dma("sp", stg[:], D["bias_t"][:, h * 640:(h + 1) * 640], writes=["btstg"])
            S.op("dve", lambda e, h=h: e.tensor_copy(out=BT[:, h, :, :].rearrange("p j q -> p (j q)"), in_=stg[:]), reads=["btstg"], writes=["BT"])
    with scope(C) as est:
        Wo = load_weight_bf16(C, es, est, "Woa", D["w_out"][0:512, :], 4, 1024, gcol=(gao, "gao"))
    PT = [sb("PT%d" % i, [128, 5, 128], BF16) for i in range(2)]
    rd = sb("rd", [128, 8]); att = sb("att", [128, 8, 64]); junk = sb("junk4", [128, 512]); st = sb("st4", [128, 2])
    an = sb("an", [128, 512], BF16); anT = sb("anT", [128, 4, 128], BF16)
    xo = [sb("xo%d" % i, [128, 1024]) for i in range(2)]; hsl = [sb("hsl%d" % i, [128, 1024]) for i in range(2)]
    h1t = [sb("h1t%d" % i, [128, 1024]) for i in range(2)]
    xv = D["x_ext"].rearrange("(n p) d -> n p d", p=128)
    hsv = D["hs_d"].rearrange("(n p) d -> n p d", p=128)
    h1v = D["h1_d"].rearrange("(n p) d -> n p d", p=128)
    with scope(C) as esp:
        bank = [C.ps(esp, "abk%d" % i, [128, 512]) for i in range(7)]
        for qt in range(16):
            sl = qt % 2
            drip(prep, 2)
            S.dma("sp", xo[sl][:], xv[48 + qt], writes=["xo%d" % sl])
            S.dma("sp", hsl[sl][:], hsv[qt], reads=["hs_d"], writes=["hsl%d" % sl])
            for h in range(8):
                hp = h // 2; rows = slice(64 * (h % 2), 64 * (h % 2) + 64); ps_ = h % 2
                bA = bank[2 * ps_]; bB = bank[2 * ps_ + 1]; bAk = "abk%d" % (2 * ps_); bBk = "abk%d" % (2 * ps_ + 1)
                for j in range(5):
                    o = bA[:, j * 128:(j + 1) * 128] if j < 4 else bB[:, 0:128]
                    ok = bAk if j < 4 else bBk
                    S.op("pe", lambda e, o=o, h=h, hp=hp, j=j, qt=qt: e.matmul(o, lhsT=kT[:, hp, (qt + j) * 128:(qt + j + 1) * 128], rhs=qZ[:, h, qt * 128:(qt + 1) * 128], start=True, stop=False),
                         reads=["kT", "qZ"], writes=[ok])
                    S.op("pe", lambda e, o=o, h=h, j=j: e.matmul(o, lhsT=idb[:], rhs=BT[:, h, j, :], start=False, stop=True), reads=["identb", "BT"], writes=[ok])
                S.op("act", lambda e, ps_=ps_, bA=bA: e.activation(out=PT[ps_][:, 0:4, :].rearrange("p j q -> p (j q)"), in_=bA[:], func=AF.Exp), reads=[bAk], writes=["PT%d" % ps_])
                S.op("act", lambda e, ps_=ps_, bB=bB: e.activation(out=PT[ps_][:, 4, :], in_=bB[:, 0:128], func=AF.Exp), reads=[bBk], writes=["PT%d" % ps_])
                ob = bank[4 + h // 4]; obk = "abk%d" % (4 + h // 4)
                for j in range(5):
                    S.op("pe", lambda e, ob=ob, h=h, j=j, ps_=ps_, qt=qt: e.matmul(ob[:, (h % 4) * 65:(h % 4) * 65 + 65], lhsT=PT[ps_][:, j, :], rhs=V[:, qt + j, h * 65:(h + 1) * 65], start=(j == 0), stop=(j == 4)),
                         reads=["PT%d" % ps_, "Vall"], writes=[obk])
            for hb in range(2):
                ov = bank[4 + hb][:, 0:260].rearrange("p (h d) -> p h d", d=65)
                S.op("dve", lambda e, hb=hb, ov=ov: e.reciprocal(out=rd[:, hb * 4:(hb + 1) * 4], in_=ov[:, :, 64]), reads=["abk%d" % (4 + hb)], writes=["rd%d" % hb])
                S.op("dve", lambda e, hb=hb, ov=ov: e.tensor_tensor(out=att[:, hb * 4:(hb + 1) * 4, :], in0=ov[:, :, 0:64], in1=bc(rd[:, hb * 4:(hb + 1) * 4].unsqueeze(2), [128, 4, 64]), op=ALU.mult),
                     reads=["abk%d" % (4 + hb), "rd%d" % hb], writes=["att%d" % hb])
            attf = att[:].rearrange("p h d -> p (h d)")
            if "att" in dbg:
                S.dma("sp", dbg["att"].rearrange("(n p) d -> n p d", p=128)[qt], attf, reads=["att0", "att1"], writes=["o_att"])
            S.op("act", lambda e: e.activation(out=junk[:], in_=attf, func=AF.Square, accum_out=st[:, 0:1]), reads=["att0", "att1"], writes=["st4_ss"])
            S.op("act", lambda e: e.activation(out=st[:, 1:2], in_=st[:, 0:1], func=AF.Sqrt, scale=1.0 / 512, bias=EPS), reads=["st4_ss"], writes=["st4_sq"])
            S.op("dve", lambda e: e.reciprocal(out=st[:, 1:2], in_=st[:, 1:2]), reads=["st4_sq"], writes=["st4"])
            S.op("dve", lambda e: e.tensor_scalar(out=an[:], in0=attf, scalar1=st[:, 1:2], scalar2=None, op0=ALU.mult), reads=["att0", "att1", "st4"], writes=["an"])
            pT = bank[6][:].bitcast(BF16).rearrange("p (c n) -> p c n", n=128)[:, 0:4, :]
            transpose_chunks(C, an[:], "an", 4, pT, "abk6", anT[:], "anT", idb)
            for cb in range(2):
                for c in range(4):
                    S.op("pe", lambda e, c=c, cb=cb: e.matmul(bank[cb][:], lhsT=anT[:, c, :], rhs=Wo[:, c, cb * 512:(cb + 1) * 512], start=(c == 0), stop=(c == 3)), reads=["anT", "Woa"], writes=["abk%d" % cb])
                S.op("dve", lambda e, cb=cb, sl=sl: e.tensor_tensor(out=h1t[sl][:, cb * 512:(cb + 1) * 512], in0=bank[cb][:], in1=xo[sl][:, cb * 512:(cb + 1) * 512], op=ALU.add),
                     reads=["abk%d" % cb, "xo%d" % sl], writes=["h1t%d" % sl])
            S.op("pool", lambda e, sl=sl: e.tensor_tensor(out=h1t[sl][:], in0=h1t[sl][:], in1=hsl[sl][:], op=ALU.add), reads=["h1t%d" % sl, "hsl%d" % sl], writes=["h1t%d" % sl])
            S.dma("sp", h1v[qt], h1t[sl][:], reads=["h1t%d" % sl], writes=["h1_d"])


def _col(g, n):
    return np.ascontiguousarray(np.asarray(g, np.float32).reshape(n, 128).T)


def host_shared(inp):
    f = lambda k: np.asarray(inp[k], np.float32)[0]
    sh = {}
    sh["g_mix"] = _col(f("norm_mix_g"), 8)
    sh["w_in"] = np.ascontiguousarray(f("w_in"))
    sh["att_q_g"] = f("att_q_g").reshape(1, 64)
    sh["att_k_g"] = f("att_k_g").reshape(1, 64)
    rb = f("rel_bias")
    p = np.arange(128); j = np.arange(5); q = np.arange(128)
    kidx = j[:, None] * 128 + p[None, :]
    kc = kidx // 64; ki = kidx % 64
    qc = q // 64; qi = q % 64
    jb = kc[:, :, None] - qc[None, None, :]
    allowed = (jb >= 0) & (jb <= 8)
    kj = jb * 64 + ki[:, :, None]
    dist = 512 + qi[None, None, :] - kj
    bucket = np.clip(np.clip(dist, -63, 128) + 63, 0, 191)
    bt = np.where(allowed[None], rb[:, bucket], np.float32(NEG))
    sh["bias_t"] = np.ascontiguousarray(bt.transpose(2, 0, 1, 3).reshape(128, 8 * 5 * 128).astype(np.float32))
    dup = lambda a: np.ascontiguousarray(np.concatenate([a, a], 0).astype(np.float32))
    sh["s5_lr"] = dup(f("ssm_lam_re").T)
    sh["s5_li"] = dup(f("ssm_lam_im").T)
    sh["s5_ls"] = np.ascontiguousarray(np.broadcast_to(f("ssm_log_step")[None, :], (128, 32)).astype(np.float32))
    sg = np.ones((128, 2), np.float32); sg[:64, 0] = -1.0; sg[64:, 1] = -1.0
    sh["s5_sg"] = sg
    sh["s5_nv"] = np.ascontiguousarray(np.broadcast_to(np.arange(-7, 16, dtype=np.float32)[None, :], (128, 23)))
    bre = f("ssm_b_re").transpose(1, 0, 2).reshape(64, 512); bim = f("ssm_b_im").transpose(1, 0, 2).reshape(64, 512)
    cre = f("ssm_c_re").transpose(2, 0, 1).reshape(64, 512); cim = f("ssm_c_im").transpose(2, 0, 1).reshape(64, 512)
    sh["s5_p1b"] = np.ascontiguousarray(np.concatenate([bre, bim], 0)); sh["s5_p2b"] = np.ascontiguousarray(np.concatenate([bim, bre], 0))
    sh["s5_p1c"] = np.ascontiguousarray(np.concatenate([cre, cim], 0)); sh["s5_p2c"] = np.ascontiguousarray(np.concatenate([cim, cre], 0))
    dd = np.zeros((2, 4, 16, 4, 4, 16), np.float32)
    dsk = f("ssm_d")
    for g in range(32):
        chc = g // 8; hb = (g % 8) // 4; j4 = g % 4
        for c in range(16):
            dd[hb, j4, c, chc, j4, c] = dsk[g, c]
    sh["s5_dd"] = dd.reshape(128, 256)
    sh["g_ssm_out"] = _col(f("ssm_out_g"), 4)
    sh["g_att_out"] = _col(f("att_out_g"), 4)
    sh["b_glu"] = f("ssm_b_glu").reshape(1, 512)
    sh["w_glu"] = np.ascontiguousarray(f("ssm_w_glu"))
    sh["w_out"] = np.ascontiguousarray(f("w_out"))
    return sh


def host_core(inp, c):
    b, seg = c // 4, c % 4
    x = np.asarray(inp["x"], np.float32)
    xe = np.zeros((8192, 1024), np.float32)
    n = (seg + 1) * 2048
    xe[8192 - n:] = x[b, :n]
    hv = np.full((512,), 1.0 if seg > 0 else 0.0, np.float32)
    return {"x_ext": xe, "hvalid": np.ascontiguousarray(hv.reshape(4, 128).T)}


IN_SHAPES = {
    "x_ext": [8192, 1024], "hvalid": [128, 4], "g_mix": [128, 8], "w_in": [1024, 2048], "att_q_g": [1, 64], "att_k_g": [1, 64],
    "bias_t": [128, 5120], "s5_lr": [128, 32], "s5_li": [128, 32], "s5_ls": [128, 32], "s5_sg": [128, 2], "s5_nv": [128, 23],
    "s5_p1b": [128, 512], "s5_p2b": [128, 512], "s5_p1c": [128, 512], "s5_p2c": [128, 512], "s5_dd": [128, 256],
    "g_ssm_out": [128, 4], "g_att_out": [128, 4], "b_glu": [1, 512], "w_glu": [512, 512], "w_out": [1024, 1024],
}
IN_SHAPES_MEM = {"mem": [256, 1024], "g_mem": [128, 8], "g_memkv": [128, 8], "mem_q_g": [1, 256], "mem_k_g": [1, 256],
                 "w_mem_q": [1024, 1024], "w_mem_k": [1024, 1024], "w_mem_v": [1024, 1024], "w_mem_o": [1024, 1024]}
IN_SHAPES_PEER = {"g_peer": [1, 1024], "w_peer_q": [1024, 2048], "keysT": [128, 2048], "peer_uT": [1024, 16384], "peer_v": [16384, 1024]}
SCRATCH = {"kT_d": ([128, 4 * 2560], BF16), "qT_d": ([128, 4 * 2048], BF16), "V_d": ([20, 128, 520], BF16),
           "hs_d": ([2048, 1024], F32), "Mz_d": ([128, 16 * 8 * 128], BF16), "CA_d": ([128, 2, 4096], BF16)}
SCRATCH_PEER = {"uT_b": ([1024, 16384], BF16), "v_b": ([16384, 1024], BF16), "xnT_d": ([16, 128, 1024], BF16),
                "sc_d": ([16, 128, 2048], F32), "tau_d": ([16, 128, 8], F32)}


def host_shared_rest(inp):
    f = lambda k: np.asarray(inp[k], np.float32)[0]
    sh = {}
    sh["g_mem"] = _col(f("norm_mem_g"), 8); sh["g_memkv"] = _col(f("norm_memkv_g"), 8)
    sh["mem_q_g"] = f("mem_q_g").reshape(1, 256); sh["mem_k_g"] = f("mem_k_g").reshape(1, 256)
    for k in ("w_mem_q", "w_mem_k", "w_mem_v", "w_mem_o", "w_peer_q"):
        sh[k] = np.ascontiguousarray(f(k))
    sh["g_peer"] = f("norm_peer_g").reshape(1, 1024)
    sh["keysT"] = np.ascontiguousarray(f("peer_keys").transpose(3, 0, 1, 2).reshape(128, 2048))
    sh["peer_uT"] = np.ascontiguousarray(f("peer_u").T)
    sh["peer_v"] = np.ascontiguousarray(f("peer_v"))
    return sh


def build_program(stages=("mixer", "mem", "peer"), dbg_specs=None, upto=9):
    nc = bass.Bass("TRN2", target_bir_lowering=False)
    D = {}
    shapes = {}
    if "mixer" in stages:
        shapes.update(IN_SHAPES)
    if "mem" in stages:
        shapes.update(IN_SHAPES_MEM)
    if "peer" in stages:
        shapes.update(IN_SHAPES_PEER)
    for k, shp in shapes.items():
        D[k] = nc.dram_tensor(k, shp, F32, kind="ExternalInput").ap()
    scr = {}
    if "mixer" in stages:
        scr.update(SCRATCH)
    if "peer" in stages:
        scr.update(SCRATCH_PEER)
    for k, (shp, dt) in scr.items():
        D[k] = nc.dram_tensor(k, shp, dt).ap()
    chain = ["h1_d", "h2_d", "out"]
    first = {"mixer": None, "mem": "h1_d", "peer": "h2_d"}[stages[0]]
    last = {"mixer": "h1_d", "mem": "h2_d", "peer": "out"}[stages[-1]]
    for k in chain:
        if k == first:
            D[k] = nc.dram_tensor(k, [2048, 1024], F32, kind="ExternalInput").ap()
        elif k == last:
            D[k] = nc.dram_tensor(k, [2048, 1024], F32, kind="ExternalOutput").ap()
        else:
            D[k] = nc.dram_tensor(k, [2048, 1024], F32).ap()
    dbg = {}
    for k, shp in (dbg_specs or {}).items():
        dbg[k] = nc.dram_tensor("dbg_" + k, shp, F32, kind="ExternalOutput").ap()
    with ExitStack() as es:
        S = Sched(nc, es)
        C = Ctx(nc, S)
        prep = stage_peer_prep(C, D) if "peer" in stages else None
        if "mixer" in stages:
            stage_mixer(C, D, dbg, upto, prep=prep)
        if "mem" in stages:
            stage_mem(C, D, dbg, prep=prep)
        drip(prep, 1000)
        if "peer" in stages:
            stage_peer(C, D, dbg)
        S.barrier()
        S.emit()
    return nc, S


def _headnorm(C, src_ap, skey, nh, hd, sqt, sqk, stat, stk, gt, gkey, dst_ap, dkey):
    S = C.S
    sv = src_ap.rearrange("p (h d) -> p h d", d=hd)
    S.op("pool", lambda e: e.tensor_tensor(out=sqt, in0=src_ap, in1=src_ap, op=ALU.mult), reads=[skey], writes=[sqk])
    S.op("dve", lambda e: e.tensor_reduce(out=stat[:, 0:nh], in_=sqt.rearrange("p (h d) -> p h d", d=hd), axis=AX.X, op=ALU.add), reads=[sqk], writes=[stk + "a"])
    S.op("act", lambda e: e.activation(out=stat[:, nh:2 * nh], in_=stat[:, 0:nh], func=AF.Sqrt, scale=1.0 / hd, bias=EPS), reads=[stk + "a"], writes=[stk + "b"])
    S.op("dve", lambda e: e.reciprocal(out=stat[:, nh:2 * nh], in_=stat[:, nh:2 * nh]), reads=[stk + "b"], writes=[stk])
    S.op("dve", lambda e: e.tensor_tensor(out=sv, in0=sv, in1=bc(stat[:, nh:2 * nh].unsqueeze(2), [128, nh, hd]), op=ALU.mult), reads=[skey, stk], writes=[skey])
    S.op("pool", lambda e: e.tensor_tensor(out=dst_ap.rearrange("p (h d) -> p h d", d=hd), in0=sv, in1=bc(gt.unsqueeze(1), [128, nh, hd]), op=ALU.mult), reads=[skey, gkey], writes=[dkey])


def stage_mem(C, D, dbg=None, prep=None):
    S = C.S
    dbg = dbg or {}
    with scope(C) as es:
        sb = lambda name, shape, dt=F32: C.sb(es, name, shape, dt)
        idf, idb = make_ident(C, es, "ident")
        gm = sb("gm", [128, 8]); gkv = sb("gkv", [128, 8]); gq = sb("mgq", [128, 256]); gk = sb("mgk", [128, 256])
        S.dma("sp", gm[:], D["g_mem"], writes=["gm"]); S.dma("sp", gkv[:], D["g_memkv"], writes=["gkv"])
        S.dma("sp", gq[:], D["mem_q_g"].partition_broadcast(128), writes=["mgq"]); S.dma("sp", gk[:], D["mem_k_g"].partition_broadcast(128), writes=["mgk"])
        S.op("dve", lambda e: e.tensor_scalar(out=gq[:], in0=gq[:], scalar1=1.0 / 16, scalar2=None, op0=ALU.mult), reads=["mgq"], writes=["mgq"])
        kTm = sb("kTm", [128, 8, 256], BF16); Vm = sb("Vm", [128, 2, 4, 257], BF16)
        xt = [sb("mxt%d" % i, [128, 1024]) for i in range(2)]; st = sb("mst", [128, 2]); xn = sb("mxn", [128, 1024], BF16)
        xnT = sb("mxnT", [128, 8, 128], BF16); qf = sb("mqf", [128, 1024]); sq = sb("msq", [128, 1024]); qst = sb("mqst", [128, 8])
        qn = sb("mqn", [128, 1024], BF16); mjunk = sb("mjunk", [128, 1024], BF16)
        with scope(C) as esk:
            with scope(C) as est:
                Wk = load_weight_bf16(C, esk, est, "Wmk", D["w_mem_k"], 8, 1024, gcol=(gkv, "gkv"))
            with scope(C) as est:
                Wv = load_weight_bf16(C, esk, est, "Wmv", D["w_mem_v"], 8, 1024, gcol=(gkv, "gkv"))
            with scope(C) as esp:
                bank = [C.ps(esp, "mkb%d" % i, [128, 512]) for i in range(6)]
                mv = D["mem"].rearrange("(n p) d -> n p d", p=128)
                for mt in range(2):
                    x_ = xt[mt]; xk = "mxt%d" % mt
                    S.dma("sp", x_[:], mv[mt], writes=[xk])
                    rms_rstd(C, x_[:], xk, mjunk[:], "mjunk", st[:, 0:1], st[:, 1:2], "mst", 1024)
                    S.op("dve", lambda e, x_=x_: e.tensor_scalar(out=xn[:], in0=x_[:], scalar1=st[:, 1:2], scalar2=None, op0=ALU.mult), reads=[xk, "mst"], writes=["mxn"])
                    pb = bank[0][:].bitcast(BF16).rearrange("p (c n) -> p c n", n=128)
                    transpose_chunks(C, xn[:], "mxn", 8, pb, "mkb0", xnT[:], "mxnT", idb)
                    for (W, wk, b0) in ((Wk, "Wmk", 1), (Wv, "Wmv", 3)):
                        for cb in range(2):
                            for dc in range(8):
                                S.op("pe", lambda e, W=W, cb=cb, dc=dc, b0=b0: e.matmul(bank[b0 + cb][:], lhsT=xnT[:, dc, :], rhs=W[:, dc, cb * 512:(cb + 1) * 512], start=(dc == 0), stop=(dc == 7)),
                                     reads=["mxnT", wk], writes=["mkb%d" % (b0 + cb)])
                    for cb in range(2):
                        S.op("act", lambda e, cb=cb: e.copy(out=qf[:, cb * 512:(cb + 1) * 512], in_=bank[1 + cb][:]), reads=["mkb%d" % (1 + cb)], writes=["mqf"])
                        S.op("dve", lambda e, cb=cb, mt=mt: e.tensor_copy(out=Vm[:, mt, 2 * cb:2 * cb + 2, 0:256], in_=bank[3 + cb][:].rearrange("p (h d) -> p h d", d=256)), reads=["mkb%d" % (3 + cb)], writes=["Vm"])
                    S.op("pool", lambda e, mt=mt: e.memset(Vm[:, mt, :, 256], 1.0), reads=[], writes=["Vm"])
                    _headnorm(C, qf[:], "mqf", 4, 256, sq[:], "msq", qst, "mqst", gk[:], "mgk", qn[:], "mqn")
                    pb2 = bank[5][:].bitcast(BF16).rearrange("p (c n) -> p c n", n=128)
                    transpose_chunks(C, qn[:], "mqn", 8, pb2, "mkb5", kTm[:, :, mt * 128:(mt + 1) * 128], "kTm", idb)
        with scope(C) as est:
            Wq = load_weight_bf16(C, es, est, "Wmq", D["w_mem_q"], 8, 1024, gcol=(gm, "gm"))
        with scope(C) as est:
            Wo = load_weight_bf16(C, es, est, "Wmo", D["w_mem_o"], 8, 1024)
        qT = sb("mqT", [128, 8, 128], BF16); PT = [sb("mPT%d" % i, [128, 2, 128], BF16) for i in range(2)]
        rd = sb("mrd", [128, 4]); ob = sb("mob", [128, 1024], BF16); oT = sb("moT", [128, 8, 128], BF16)
        h2t = [sb("h2t%d" % i, [128, 1024]) for i in range(2)]
        hv = D["h1_d"].rearrange("(n p) d -> n p d", p=128); ov = D["h2_d"].rearrange("(n p) d -> n p d", p=128)
        with scope(C) as esp:
            bank = [C.ps(esp, "mb%d" % i, [128, 512]) for i in range(8)]
            for tt in range(16):
                sl = tt % 2
                drip(prep, 1)
                x_ = xt[sl]; xk = "mxt%d" % sl
                S.dma("sp", x_[:], hv[tt], reads=["h1_d"], writes=[xk])
                rms_rstd(C, x_[:], xk, mjunk[:], "mjunk", st[:, 0:1], st[:, 1:2], "mst", 1024)
                S.op("dve", lambda e, x_=x_: e.tensor_scalar(out=xn[:], in0=x_[:], scalar1=st[:, 1:2], scalar2=None, op0=ALU.mult), reads=[xk, "mst"], writes=["mxn"])
                pb = bank[0][:].bitcast(BF16).rearrange("p (c n) -> p c n", n=128)
                transpose_chunks(C, xn[:], "mxn", 8, pb, "mb0", xnT[:], "mxnT", idb)
                for cb in range(2):
                    for dc in range(8):
                        S.op("pe", lambda e, cb=cb, dc=dc: e.matmul(bank[1 + cb][:], lhsT=xnT[:, dc, :], rhs=Wq[:, dc, cb * 512:(cb + 1) * 512], start=(dc == 0), stop=(dc == 7)),
                             reads=["mxnT", "Wmq"], writes=["mb%d" % (1 + cb)])
                    S.op("act", lambda e, cb=cb: e.copy(out=qf[:, cb * 512:(cb + 1) * 512], in_=bank[1 + cb][:]), reads=["mb%d" % (1 + cb)], writes=["mqf"])
                _headnorm(C, qf[:], "mqf", 4, 256, sq[:], "msq", qst, "mqst", gq[:], "mgq", qn[:], "mqn")
                pb2 = bank[3][:].bitcast(BF16).rearrange("p (c n) -> p c n", n=128)
                transpose_chunks(C, qn[:], "mqn", 8, pb2, "mb3", qT[:], "mqT", idb)
                for h in range(4):
                    ps_ = h % 2
                    sbk = bank[4 + ps_]; sbkk = "mb%d" % (4 + ps_)
                    for mt in range(2):
                        for dh in range(2):
                            S.op("pe", lambda e, h=h, mt=mt, dh=dh, sbk=sbk: e.matmul(sbk[:, mt * 128:(mt + 1) * 128], lhsT=kTm[:, 2 * h + dh, mt * 128:(mt + 1) * 128], rhs=qT[:, 2 * h + dh, :], start=(dh == 0), stop=(dh == 1)),
                                 reads=["kTm", "mqT"], writes=[sbkk])
                    S.op("act", lambda e, ps_=ps_, sbk=sbk: e.activation(out=PT[ps_][:].rearrange("p m q -> p (m q)"), in_=sbk[:, 0:256], func=AF.Exp, bias=-8.0), reads=[sbkk], writes=["mPT%d" % ps_])
                    obk = bank[6 + ps_]; obkk = "mb%d" % (6 + ps_)
                    for mt in range(2):
                        S.op("pe", lambda e, h=h, mt=mt, ps_=ps_, obk=obk: e.matmul(obk[:, 0:257], lhsT=PT[ps_][:, mt, :], rhs=Vm[:, mt, h, :], start=(mt == 0), stop=(mt == 1)), reads=["mPT%d" % ps_, "Vm"], writes=[obkk])
                    S.op("dve", lambda e, h=h, obk=obk: e.reciprocal(out=rd[:, h:h + 1], in_=obk[:, 256:257]), reads=[obkk], writes=["mrd%d" % h])
                    S.op("dve", lambda e, h=h, obk=obk: e.tensor_scalar(out=ob[:, h * 256:(h + 1) * 256], in0=obk[:, 0:256], scalar1=rd[:, h:h + 1], scalar2=None, op0=ALU.mult), reads=[obkk, "mrd%d" % h], writes=["mob"])
                pb3 = bank[0][:].bitcast(BF16).rearrange("p (c n) -> p c n", n=128)
                transpose_chunks(C, ob[:], "mob", 8, pb3, "mb0", oT[:], "moT", idb)
                for cb in range(2):
                    for dc in range(8):
                        S.op("pe", lambda e, cb=cb, dc=dc: e.matmul(bank[1 + cb][:], lhsT=oT[:, dc, :], rhs=Wo[:, dc, cb * 512:(cb + 1) * 512], start=(dc == 0), stop=(dc == 7)),
                             reads=["moT", "Wmo"], writes=["mb%d" % (1 + cb)])
                    S.op("dve", lambda e, cb=cb, sl=sl, x_=x_: e.tensor_tensor(out=h2t[sl][:, cb * 512:(cb + 1) * 512], in0=bank[1 + cb][:], in1=x_[:, cb * 512:(cb + 1) * 512], op=ALU.add),
                         reads=["mb%d" % (1 + cb), xk], writes=["h2t%d" % sl])
                S.dma("sp", ov[tt], h2t[sl][:], reads=["h2t%d" % sl], writes=["h2_d"])


def stage_peer_prep(C, D):
    S = C.S
    uv = D["peer_uT"].rearrange("(c p) (a e) -> c p a e", p=128, e=2048)
    ub = D["uT_b"].rearrange("(c p) (a e) -> c p a e", p=128, e=2048)
    vv = D["peer_v"].rearrange("(c p) d -> c p d", p=512)
    vb = D["v_b"].rearrange("(c p) d -> c p d", p=512)

    def gen():
        for c in range(8):
            S.dma("pool", ub[c], uv[c], writes=["uT_b"])
            yield
        for c in range(32):
            S.dma("pool", vb[c], vv[c], writes=["v_b"])
            yield
    return gen()


def drip(g, n):
    if g is None:
        return
    for _ in range(n):
        try:
            next(g)
        except StopIteration:
            return


def _top16(C, src, skey, work, wkey, dst, dkey):
    S = C.S
    S.op("dve", lambda e: e.max(out=dst[:, 0:8], in_=src), reads=[skey], writes=[dkey])
    S.op("dve", lambda e: e.match_replace(out=work, in_to_replace=dst[:, 0:8], in_values=src, imm_value=-1e30), reads=[skey, dkey], writes=[wkey])
    S.op("dve", lambda e: e.max(out=dst[:, 8:16], in_=work), reads=[wkey], writes=[dkey])


def stage_peer(C, D, dbg=None):
    S = C.S
    dbg = dbg or {}
    hv = D["h2_d"].rearrange("(n p) d -> n p d", p=128)
    with scope(C) as es:
        sb = lambda name, shape, dt=F32: C.sb(es, name, shape, dt)
        idf, idb = make_ident(C, es, "ident")
        gp = sb("gpb", [128, 1024])
        S.dma("sp", gp[:], D["g_peer"].partition_broadcast(128), writes=["gpb"])
        with scope(C) as est:
            Wq = load_weight_bf16(C, es, est, "Wpq", D["w_peer_q"], 8, 2048)
        keyT = sb("keyT", [128, 16, 128], BF16)
        with scope(C) as est:
            kst = C.sb(est, "kst", [128, 2048])
            S.dma("sp", kst[:], D["keysT"], writes=["kst"])
            S.op("dve", lambda e: e.tensor_copy(out=keyT[:].rearrange("p a n -> p (a n)"), in_=kst[:]), reads=["kst"], writes=["keyT"])
        xt = [sb("pxt%d" % i, [128, 1024]) for i in range(2)]; st_ = [sb("pst%d" % i, [128, 2]) for i in range(2)]; junk = sb("pjunk", [128, 1024], BF16)
        xn_ = [sb("pxn%d" % i, [128, 1024], BF16) for i in range(2)]; xnT = [sb("pxnT%d" % i, [128, 8, 128], BF16) for i in range(2)]
        qb_ = [sb("pqb%d" % i, [128, 2048], BF16) for i in range(2)]; qTp_ = [sb("pqT%d" % i, [128, 16, 128], BF16) for i in range(2)]
        sc = [sb("psc%d" % i, [128, 16, 128]) for i in range(2)]; work_ = [sb("pwork%d" % i, [128, 256]) for i in range(2)]
        sv_ = [sb("psv%d" % i, [128, 16, 16]) for i in range(2)]; cand_ = [sb("pcand%d" % i, [128, 8, 256]) for i in range(2)]
        cex_ = [sb("pcex%d" % i, [128, 8, 256]) for i in range(2)]; ctop_ = [sb("pctop%d" % i, [128, 8, 16]) for i in range(2)]
        Z_ = [sb("pZ%d" % i, [128, 8]) for i in range(2)]; off_ = [sb("poff%d" % i, [128, 8]) for i in range(2)]; tau = [sb("ptau%d" % i, [128, 8]) for i in range(2)]
        cjunk_ = [sb("pcj%d" % i, [128, 256]) for i in range(2)]; offs_ = [sb("poffs%d" % i, [128, 16]) for i in range(2)]
        xTd = D["xnT_d"].rearrange("n p (c t) -> n p c t", t=128)
        with scope(C) as esp:
            bank = [C.ps(esp, "pab%d" % i, [128, 512]) for i in range(6)]

            def tile(tt):
                sl = tt % 2
                K = lambda nm: "%s%d" % (nm, sl)
                x_ = xt[sl]; xk = K("pxt"); st = st_[sl]; xn = xn_[sl]; qb = qb_[sl]; qTp = qTp_[sl]; work = work_[sl]
                sv = sv_[sl]; cand = cand_[sl]; cex = cex_[sl]; ctop = ctop_[sl]; Z = Z_[sl]; off = off_[sl]; cjunk = cjunk_[sl]; offs = offs_[sl]
                S.dma("sp", x_[:], hv[tt], reads=["h2_d"], writes=[xk])
                rms_rstd(C, x_[:], xk, junk[:], "pjunk", st[:, 0:1], st[:, 1:2], K("pst"), 1024)
                S.op("dve", lambda e: e.scalar_tensor_tensor(out=xn[:], in0=x_[:], scalar=st[:, 1:2], in1=gp[:], op0=ALU.mult, op1=ALU.mult), reads=[xk, K("pst"), "gpb"], writes=[K("pxn")])
                pb = bank[0][:].bitcast(BF16).rearrange("p (c n) -> p c n", n=128)
                transpose_chunks(C, xn[:], K("pxn"), 8, pb, "pab0", xnT[sl][:], K("pxnT"), idb)
                S.dma("sp", xTd[tt], xnT[sl][:], reads=[K("pxnT")], writes=["xnT_d"])
                yield
                for cb in range(4):
                    for dc in range(8):
                        S.op("pe", lambda e, cb=cb, dc=dc: e.matmul(bank[1 + cb][:], lhsT=xnT[sl][:, dc, :], rhs=Wq[:, dc, cb * 512:(cb + 1) * 512], start=(dc == 0), stop=(dc == 7)),
                             reads=[K("pxnT"), "Wpq"], writes=["pab%d" % (1 + cb)])
                    S.op("act", lambda e, cb=cb: e.copy(out=qb[:, cb * 512:(cb + 1) * 512], in_=bank[1 + cb][:]), reads=["pab%d" % (1 + cb)], writes=[K("pqb")])
                for half in range(2):
                    pbq = bank[5][:].bitcast(BF16).rearrange("p (c n) -> p c n", n=128)
                    transpose_chunks(C, qb[:, half * 1024:(half + 1) * 1024], K("pqb"), 8, pbq, "pab5", qTp[:, half * 8:(half + 1) * 8, :], K("pqT"), idb)
                for hh in range(16):
                    S.op("pe", lambda e, hh=hh: e.matmul(bank[1 + hh // 4][:, (hh % 4) * 128:(hh % 4 + 1) * 128], lhsT=qTp[:, hh, :], rhs=keyT[:, hh, :], start=True, stop=True),
                         reads=[K("pqT"), "keyT"], writes=["pab%d" % (1 + hh // 4)])
                scs = sc[sl]; sck = K("psc")
                for cb in range(4):
                    S.op("act", lambda e, cb=cb: e.copy(out=scs[:, cb * 4:(cb + 1) * 4, :].rearrange("p a n -> p (a n)"), in_=bank[1 + cb][:]), reads=["pab%d" % (1 + cb)], writes=[sck])
                yield
                for hh in range(16):
                    _top16(C, scs[:, hh, :], sck, work[:, 0:128], K("pwork"), sv[:, hh, :], K("psv"))
                    yield
                svv = sv[:].rearrange("p (h s) k -> p h s k", s=2)
                for h in range(8):
                    S.op("dve", lambda e, h=h: e.tensor_tensor(out=cand[:, h, :].rearrange("p (a b) -> p a b", b=16), in0=bc(svv[:, h, 0, :].unsqueeze(2), [128, 16, 16]),
                                                            in1=bc(svv[:, h, 1, :].unsqueeze(1), [128, 16, 16]), op=ALU.add), reads=[K("psv")], writes=[K("pcand") + "_%d" % h])
                    yield
                ck = [K("pcand") + "_%d" % h for h in range(8)]
                for h in range(8):
                    _top16(C, cand[:, h, :], ck[h], work[:], K("pwork"), ctop[:, h, :], K("pctop"))
                    yield
                S.op("dve", lambda e: e.tensor_tensor(out=cand[:], in0=cand[:], in1=bc(ctop[:, :, 0:1], [128, 8, 256]), op=ALU.subtract), reads=ck + [K("pctop")], writes=ck)
                S.op("act", lambda e: e.activation(out=cex[:].rearrange("p h n -> p (h n)"), in_=cand[:].rearrange("p h n -> p (h n)"), func=AF.Exp), reads=ck, writes=[K("pcex")])
                yield
                S.op("dve", lambda e: e.tensor_tensor(out=tau[sl][:], in0=ctop[:, :, 15], in1=ctop[:, :, 0], op=ALU.subtract), reads=[K("pctop")], writes=[K("ptau")])
                yield
                S.op("dve", lambda e: e.tensor_scalar(out=tau[sl][:], in0=tau[sl][:], scalar1=-1e-5, scalar2=None, op0=ALU.add), reads=[K("ptau")], writes=[K("ptau")])
                yield
                for h in range(8):
                    S.op("dve", lambda e, h=h: e.scalar_tensor_tensor(out=cjunk[:], in0=cand[:, h, :], scalar=tau[sl][:, h:h + 1], in1=cex[:, h, :], op0=ALU.is_ge, op1=ALU.mult, accum_out=Z[:, h:h + 1]),
                         reads=ck + [K("pcex"), K("ptau")], writes=[K("pZ") + "_%d" % h])
                    yield
                S.op("act", lambda e: e.activation(out=off[:], in_=Z[:], func=AF.Ln), reads=[K("pZ") + "_%d" % h for h in range(8)], writes=[K("poff")])
                yield
                S.op("dve", lambda e: e.tensor_tensor(out=tau[sl][:], in0=tau[sl][:], in1=off[:], op=ALU.subtract), reads=[K("ptau"), K("poff")], writes=[K("ptau")])
                S.op("act", lambda e: e.activation(out=tau[sl][:], in_=tau[sl][:], func=AF.Exp), reads=[K("ptau")], writes=[K("ptau")])
                yield
                S.op("dve", lambda e: e.tensor_scalar(out=tau[sl][:], in0=tau[sl][:], scalar1=0.99997, scalar2=None, op0=ALU.mult), reads=[K("ptau")], writes=[K("ptau")])
                offv = offs[:].rearrange("p (h s) -> p h s", s=2)
                S.op("dve", lambda e: e.tensor_copy(out=offv[:, :, 0], in_=svv[:, :, 0, 0]), reads=[K("psv")], writes=[K("poffs") + "a"])
                yield
                S.op("dve", lambda e: e.tensor_tensor(out=offv[:, :, 1], in0=svv[:, :, 1, 0], in1=off[:], op=ALU.add), reads=[K("psv"), K("poff")], writes=[K("poffs") + "b"])
                yield
                S.op("dve", lambda e: e.tensor_tensor(out=scs[:], in0=scs[:], in1=bc(offs[:].unsqueeze(2), [128, 16, 128]), op=ALU.subtract), reads=[sck, K("poffs") + "a", K("poffs") + "b"], writes=[sck])
                S.op("act", lambda e: e.activation(out=scs[:].rearrange("p a n -> p (a n)"), in_=scs[:].rearrange("p a n -> p (a n)"), func=AF.Exp), reads=[sck], writes=[sck])
                S.dma("sp", D["sc_d"][tt], scs[:].rearrange("p a n -> p (a n)"), reads=[sck], writes=["sc_d"])
                S.dma("sp", D["tau_d"][tt], tau[sl][:], reads=[K("ptau")], writes=["tau_d"])

            from itertools import zip_longest
            for t0 in range(0, 16, 2):
                for _ in zip_longest(tile(t0), tile(t0 + 1)):
                    pass
    with scope(C) as es:
        sb = lambda name, shape, dt=F32: C.sb(es, name, shape, dt)
        idf, idb = make_ident(C, es, "ident")
        xnT = sb("bxnT", [128, 4, 1024], BF16); sc = sb("bsc", [128, 4, 2048]); kap = sb("btau", [128, 4, 8])
        UT = [sb("UT%d" % i, [128, 8, 1024], BF16) for i in range(2)]; Vb = [sb("Vb%d" % i, [128, 8, 1024], BF16) for i in range(2)]
        acc = sb("pacc", [128, 4, 1024])
        NP = 12
        Pt = [sb("pP%d" % i, [128, 512]) for i in range(NP)]; Wh = [[sb("pWh%d_%d" % (i, h), [128, 512], BF16) for h in range(8)] for i in range(2)]
        G = [sb("pG%d" % i, [128, 512], BF16) for i in range(3)]; WA = [sb("pWA%d" % i, [128, 512], BF16) for i in range(2)]
        WAT = [sb("pWAT%d" % i, [128, 4, 128], BF16) for i in range(2)]
        h2t = [sb("ph2t%d" % i, [128, 1024]) for i in range(2)]
        uTv = D["uT_b"].rearrange("(c p) (b e) -> b p c e", p=128, e=1024)
        vbv = D["v_b"].rearrange("(b c p) d -> b p c d", p=128, c=8)
        ov = D["out"].rearrange("(n p) d -> n p d", p=128)
        with scope(C) as esp:
            bank = [C.ps(esp, "pbb%d" % i, [128, 512]) for i in range(7)]
            state = {"it": 0}

            def stage1a(u, tg, eb, sub, tt, es_):
                ub = u % 2
                xT = xnT[:, tt, :].rearrange("p (c t) -> p c t", t=128)
                scv = sc[:, tt, :].rearrange("p (h s n) -> p h s n", s=2, n=128)
                i0 = eb * 8 + sub * 4
                for dc in range(8):
                    S.op("pe", lambda e, dc=dc, xT=xT: e.matmul(bank[ub][:], lhsT=xT[:, dc, :], rhs=UT[es_][:, dc, sub * 512:(sub + 1) * 512], start=(dc == 0), stop=(dc == 7)),
                         reads=["bxnT", "UT%d" % es_], writes=["pbb%d" % ub])
                for h in range(8):
                    hs = state["it"] % NP; state["it"] += 1
                    if h >= 5:
                        S.op("pool", lambda e, h=h, hs=hs, scv=scv: e.tensor_tensor(out=Pt[hs][:].rearrange("p (i j) -> p i j", j=128), in0=bc(scv[:, h, 0, i0:i0 + 4].unsqueeze(2), [128, 4, 128]),
                                                                             in1=bc(scv[:, h, 1, :].unsqueeze(1), [128, 4, 128]), op=ALU.mult), reads=["bsc"], writes=["pP%d_%d" % (hs, il) for il in range(4)])
                    else:
                        for il in range(4):
                            S.op("act", lambda e, h=h, hs=hs, scv=scv, il=il: e.activation(out=Pt[hs][:, il * 128:(il + 1) * 128], in_=scv[:, h, 1, :], func=AF.Copy, scale=scv[:, h, 0, i0 + il:i0 + il + 1]),
                                 reads=["bsc"], writes=["pP%d_%d" % (hs, il)])
                    S.op("dve", lambda e, h=h, hs=hs: e.scalar_tensor_tensor(out=Wh[ub][h][:], in0=Pt[hs][:], scalar=kap[:, tt, h:h + 1], in1=Pt[hs][:], op0=ALU.is_ge, op1=ALU.mult),
                         reads=["pP%d_%d" % (hs, il) for il in range(4)] + ["btau"], writes=["pWh%d_%d" % (ub, h)])

            def st_gelu(u, tg, eb, sub, tt, es_):
                ub = u % 2; gb = u % 3
                S.op("act", lambda e: e.activation(out=G[gb][:], in_=bank[ub][:], func=AF.Gelu), reads=["pbb%d" % ub], writes=["pG%d" % gb])

            def st_hs(u, tg, eb, sub, tt, es_):
                ub = u % 2
                for h in range(8):
                    S.op("pe", lambda e, h=h: e.matmul(bank[2 + ub][:], lhsT=idb[:], rhs=Wh[ub][h][:], start=(h == 0), stop=(h == 7)),
                         reads=["identb", "pWh%d_%d" % (ub, h)], writes=["pbb%d" % (2 + ub)])

            def st_wa(u, tg, eb, sub, tt, es_):
                ub = u % 2; gb = u % 3
                S.op("dve", lambda e: e.tensor_tensor(out=WA[ub][:], in0=bank[2 + ub][:], in1=G[gb][:], op=ALU.mult), reads=["pbb%d" % (2 + ub), "pG%d" % gb], writes=["pWA%d" % ub])

            def st_t(u, tg, eb, sub, tt, es_):
                ub = u % 2
                pbt = bank[4][:].bitcast(BF16).rearrange("p (c n) -> p c n", n=128)[:, 0:4, :]
                for c in range(4):
                    S.op("pe", lambda e, c=c: e.transpose(out=pbt[:, c, :], in_=WA[ub][:, c * 128:(c + 1) * 128], identity=idb[:]), reads=["pWA%d" % ub, "identb"], writes=["pbb4"])

            def st_watcopy(u, tg, eb, sub, tt, es_):
                ub = u % 2
                pbt = bank[4][:].bitcast(BF16).rearrange("p (c n) -> p c n", n=128)[:, 0:4, :]
                S.op("act", lambda e: e.copy(out=WAT[ub][:], in_=pbt), reads=["pbb4"], writes=["pWAT%d" % ub])

            def st_v(u, tg, eb, sub, tt, es_):
                ub = u % 2
                for cb in range(2):
                    for ec in range(4):
                        S.op("pe", lambda e, cb=cb, ec=ec: e.matmul(bank[5 + cb][:], lhsT=WAT[ub][:, ec, :], rhs=Vb[es_][:, sub * 4 + ec, cb * 512:(cb + 1) * 512], start=(sub == 0 and ec == 0), stop=(sub == 1 and ec == 3)),
                             reads=["pWAT%d" % ub, "Vb%d" % es_], writes=["pbb%d" % (5 + cb)])

            def st_acc(u, tg, eb, sub, tt, es_):
                if sub != 1:
                    return
                for cb in range(2):
                    if eb == 0:
                        S.op("dve", lambda e, cb=cb: e.tensor_copy(out=acc[:, tt, cb * 512:(cb + 1) * 512], in_=bank[5 + cb][:]), reads=["pbb%d" % (5 + cb)], writes=["pacc%d" % tt])
                    else:
                        S.op("dve", lambda e, cb=cb: e.tensor_tensor(out=acc[:, tt, cb * 512:(cb + 1) * 512], in0=bank[5 + cb][:], in1=acc[:, tt, cb * 512:(cb + 1) * 512], op=ALU.add),
                             reads=["pbb%d" % (5 + cb), "pacc%d" % tt], writes=["pacc%d" % tt])

            u = 0
            for tg in range(4):
                S.dma("sp", xnT[:], D["xnT_d"][tg * 4:(tg + 1) * 4].rearrange("n p f -> p n f"), reads=["xnT_d"], writes=["bxnT"])
                S.dma("sp", sc[:], D["sc_d"][tg * 4:(tg + 1) * 4].rearrange("n p f -> p n f"), reads=["sc_d"], writes=["bsc"])
                S.dma("sp", kap[:], D["tau_d"][tg * 4:(tg + 1) * 4].rearrange("n p f -> p n f"), reads=["tau_d"], writes=["btau"])
                units = []
                for eb in range(16):
                    es_ = (tg * 16 + eb) % 2
                    for tt in range(4):
                        for sub in range(2):
                            units.append((u, tg, eb, sub, tt, es_)); u += 1
                n = len(units)
                U = lambda j: units[j] if 0 <= j < n else None
                for k in range(n + 3):
                    for ebk in ([0] if k == 0 else []) + ([k // 8 + 1] if (k % 8 == 3 and k // 8 + 1 < 16) else []):
                        es_ = (tg * 16 + ebk) % 2
                        S.dma("sp", UT[es_][:], uTv[ebk], reads=["uT_b"], writes=["UT%d" % es_])
                        S.dma("sp", Vb[es_][:], vbv[ebk], reads=["v_b"], writes=["Vb%d" % es_])
                    if U(k - 2): st_wa(*U(k - 2))
                    if U(k - 3): st_v(*U(k - 3))
                    if U(k - 2): st_t(*U(k - 2))
                    if U(k - 1): st_gelu(*U(k - 1))
                    if U(k - 1): st_hs(*U(k - 1))
                    if U(k): stage1a(*U(k))
                    if U(k - 2): st_watcopy(*U(k - 2))
                    if U(k - 3): st_acc(*U(k - 3))
                for tt in range(4):
                    sl = tt % 2; n = tg * 4 + tt
                    S.dma("sp", h2t[sl][:], hv[n], reads=["h2_d"], writes=["ph2t%d" % sl])
                    S.op("pool", lambda e, sl=sl, tt=tt: e.tensor_tensor(out=h2t[sl][:], in0=h2t[sl][:], in1=acc[:, tt, :], op=ALU.add), reads=["ph2t%d" % sl, "pacc%d" % tt], writes=["ph2t%d" % sl])
                    S.dma("sp", ov[n], h2t[sl][:], reads=["ph2t%d" % sl], writes=["out"])


_PROG = {}


def kernel(**inputs):
    sh = host_shared(inputs)
    sh.update(host_shared_rest(inputs))
    if "nc" not in _PROG:
        _PROG["nc"] = build_program(("mixer", "mem", "peer"))[0]
    nc = _PROG["nc"]
    names = set(IN_SHAPES) | set(IN_SHAPES_MEM) | set(IN_SHAPES_PEER)
    mem = np.asarray(inputs["mem"], np.float32)
    maps = []
    for c in range(8):
        m = {k: v for k, v in sh.items() if k in names}
        m.update(host_core(inputs, c))
        m["mem"] = np.ascontiguousarray(mem[c // 4])
        maps.append(m)
    res = run_bass_kernel_spmd(nc, maps, core_ids=list(range(8)))
    out = np.zeros((2, 8192, 1024), np.float32)
    for c in range(8):
        out[c // 4, (c % 4) * 2048:(c % 4 + 1) * 2048] = res.results[c]["out"]
    return out
```

```python
from contextlib import ExitStack, contextmanager
import numpy as np
import concourse.bass as bass
import concourse.mybir as mybir
from concourse.bass_utils import run_bass_kernel_spmd

F32 = mybir.dt.float32
BF16 = mybir.dt.bfloat16
I32 = mybir.dt.int32
AF = mybir.ActivationFunctionType
ALU = mybir.AluOpType
AX = mybir.AxisListType

ENGS = ("pe", "act", "dve", "pool", "sp")
TWO_PI = 6.283185307179586
EPS = 1e-6
NEG = -30000.0


class Sched:
    def __init__(self, nc, es, n_dma_sems=32):
        self.nc = nc
        self.streams = {e: [] for e in ENGS}
        self.sem = {e: es.enter_context(nc.semaphore("s_" + e)) for e in ("pe", "act", "dve", "pool")}
        self.cnt = {e: 0 for e in ("pe", "act", "dve", "pool")}
        self.dsem = [es.enter_context(nc.semaphore("s_dma%d" % i)) for i in range(n_dma_sems)]
        self.dcnt = [0] * n_dma_sems
        self.dnext = 0
        self.n_sw = 4
        self.dnext_sw = 0
        self.waited = {}
        self.last_w = {}
        self.readers = {}
        self.n_ops = 0

    def _deps(self, eng, reads, writes):
        deps = []
        for k in reads:
            if k in self.last_w:
                deps.append(self.last_w[k])
        for k in writes:
            if k in self.last_w:
                deps.append(self.last_w[k])
            deps.extend(self.readers.get(k, ()))
        need = {}
        for (sk, val, peng) in deps:
            if peng == "pe" and eng == "pe":
                continue
            if self.waited.get((eng, sk), 0) >= val:
                continue
            if need.get(sk, 0) < val:
                need[sk] = val
        return need

    def _semobj(self, sk):
        return self.sem[sk] if isinstance(sk, str) else self.dsem[sk]

    def _emit_waits(self, eng, need):
        for sk, val in need.items():
            self.waited[(eng, sk)] = val
            so = self._semobj(sk)
            self.streams[eng].append(lambda e, so=so, val=val: e.wait_ge(so, val))

    def _record(self, tok, reads, writes):
        for k in writes:
            self.last_w[k] = tok
            self.readers[k] = []
        for k in reads:
            if k not in writes:
                self.readers.setdefault(k, []).append(tok)

    def op(self, eng, fn, reads=(), writes=()):
        need = self._deps(eng, reads, writes)
        self._emit_waits(eng, need)
        self.cnt[eng] += 1
        val = self.cnt[eng]
        so = self.sem[eng]
        self.streams[eng].append(lambda e, fn=fn, so=so: fn(e).then_inc(so, 1))
        self._record((eng, val, eng), reads, writes)
        self.n_ops += 1

    def dma(self, q, out, in_, reads=(), writes=(), **kw):
        nhw = len(self.dsem) - self.n_sw
        if q == "pool":
            i = nhw + self.dnext_sw
            self.dnext_sw = (self.dnext_sw + 1) % self.n_sw
        else:
            i = self.dnext
            self.dnext = (self.dnext + 1) % nhw
        need = self._deps(q, reads, writes)
        prev = 16 * self.dcnt[i]
        if prev and self.waited.get((q, i), 0) < prev:
            need[i] = max(need.get(i, 0), prev)
        self._emit_waits(q, need)
        self.dcnt[i] += 1
        val = 16 * self.dcnt[i]
        so = self.dsem[i]
        self.streams[q].append(
            lambda e, out=out, in_=in_, so=so, kw=kw: e.dma_start(out=out, in_=in_, **kw).then_inc(so, 16))
        self._record((i, val, "dma"), reads, writes)
        self.n_ops += 1

    def barrier(self):
        for eng in ENGS:
            need = {}
            for pe_ in ("pe", "act", "dve", "pool"):
                v = self.cnt[pe_]
                if v and self.waited.get((eng, pe_), 0) < v:
                    need[pe_] = v
            for i, c in enumerate(self.dcnt):
                if c and self.waited.get((eng, i), 0) < 16 * c:
                    need[i] = 16 * c
            self._emit_waits(eng, need)

    def wait_all(self, eng, keys):
        need = {}
        for k in keys:
            if k in self.last_w:
                sk, val, _ = self.last_w[k]
                if self.waited.get((eng, sk), 0) < val and need.get(sk, 0) < val:
                    need[sk] = val
        self._emit_waits(eng, need)

    def emit(self):
        if not any(self.streams[e] for e in ENGS):
            return
        streams = self.streams
        self.streams = {e: [] for e in ENGS}
        self._emit_block(streams)

    def _emit_block(self, streams):
        self_streams = streams
        with self.nc.Block() as block:
            @block.tensor
            def _(e):
                for f in self_streams["pe"]:
                    f(e)

            @block.scalar
            def _(e):
                for f in self_streams["act"]:
                    f(e)

            @block.vector
            def _(e):
                for f in self_streams["dve"]:
                    f(e)

            @block.gpsimd
            def _(e):
                for f in self_streams["pool"]:
                    f(e)

            @block.sync
            def _(e):
                for f in self_streams["sp"]:
                    f(e)


class Ctx:
    def __init__(self, nc, S):
        self.nc = nc
        self.S = S
        self.uid = 0

    def sb(self, es, name, shape, dt=F32):
        self.uid += 1
        return es.enter_context(self.nc.sbuf_tensor("%s_%d" % (name, self.uid), list(shape), dt))

    def ps(self, es, name, shape, dt=F32):
        self.uid += 1
        return es.enter_context(self.nc.psum_tensor("%s_%d" % (name, self.uid), list(shape), dt))


@contextmanager
def scope(C):
    with ExitStack() as es:
        yield es
        C.S.barrier()
        C.S.emit()


def bc(ap, shape):
    return ap.to_broadcast(list(shape))


def make_ident(C, es, name="ident"):
    S = C.S
    idf = C.sb(es, name + "f", [128, 128])
    idb = C.sb(es, name + "b", [128, 128], BF16)
    S.op("pool", lambda e: e.memset(idf[:], 1.0), writes=[name + "f"])
    S.op("pool", lambda e: e.affine_select(out=idf[:], in_=idf[:], pattern=[[-1, 128]], compare_op=ALU.is_equal,
                                           fill=0.0, base=0, channel_multiplier=1), reads=[name + "f"], writes=[name + "f"])
    S.op("dve", lambda e: e.tensor_copy(out=idb[:], in_=idf[:]), reads=[name + "f"], writes=[name + "b"])
    return idf, idb


def sincos(C, es, ang, n, tag):
    S = C.S
    outs = []
    for which, off in (("s", 64.0), ("c", 64.25)):
        k = tag + which
        y = C.sb(es, k + "y", [128, n]); yi = C.sb(es, k + "yi", [128, n], I32); yf = C.sb(es, k + "yf", [128, n])
        m = C.sb(es, k + "m", [128, n]); o = C.sb(es, k + "o", [128, n])
        S.op("dve", lambda e, y=y, off=off: e.tensor_scalar(out=y[:], in0=ang, scalar1=1.0 / TWO_PI, scalar2=off, op0=ALU.mult, op1=ALU.add),
             reads=[tag + "ang"], writes=[k + "y"])
        S.op("dve", lambda e, y=y, yi=yi: e.tensor_copy(out=yi[:], in_=y[:]), reads=[k + "y"], writes=[k + "yi"])
        S.op("dve", lambda e, yi=yi, yf=yf: e.tensor_copy(out=yf[:], in_=yi[:]), reads=[k + "yi"], writes=[k + "yf"])
        S.op("dve", lambda e, y=y, yf=yf: e.tensor_tensor(out=y[:], in0=y[:], in1=yf[:], op=ALU.subtract), reads=[k + "y", k + "yf"], writes=[k + "y"])
        S.op("dve", lambda e, y=y, m=m: e.tensor_scalar(out=m[:], in0=y[:], scalar1=0.5, scalar2=None, op0=ALU.is_gt), reads=[k + "y"], writes=[k + "m"])
        S.op("dve", lambda e, y=y, m=m: e.tensor_tensor(out=y[:], in0=y[:], in1=m[:], op=ALU.subtract), reads=[k + "y", k + "m"], writes=[k + "y"])
        S.op("act", lambda e, y=y, o=o: e.activation(out=o[:], in_=y[:], func=AF.Sin, scale=TWO_PI), reads=[k + "y"], writes=[k + "o"])
        outs.append((o, k + "o"))
    return outs


def s5_params(C, es_keep, D):
    S, nc = C.S, C.nc
    P = {}
    LT2 = C.sb(es_keep, "LT2", [128, 8, 8, 2, 128], BF16)
    WPr = C.sb(es_keep, "WPr", [128, 9, 16]); WPi = C.sb(es_keep, "WPi", [128, 9, 16]); WPn = C.sb(es_keep, "WPn", [128, 9, 16])
    P.update(LT2=LT2, WPr=WPr, WPi=WPi, WPn=WPn)
    with scope(C) as es:
        sb = lambda name, shape, dt=F32: C.sb(es, name, shape, dt)
        Mz2 = sb("Mz2", [128, 16, 8, 128], BF16)
        CA = sb("CA", [128, 32, 128], BF16); CAs = sb("CAs", [128, 32, 128], BF16)
        LR = sb("LR", [128, 32]); LI = sb("LI", [128, 32]); LS = sb("LS", [128, 32])
        SG = sb("SG", [128, 2]); NV = sb("NV", [128, 23])
        P1B = sb("P1B", [128, 32, 16]); P2B = sb("P2B", [128, 32, 16]); P1C = sb("P1C", [128, 32, 16]); P2C = sb("P2C", [128, 32, 16])
        DD = sb("DD", [128, 16, 16])
        for t, nm in ((LR, "s5_lr"), (LI, "s5_li"), (LS, "s5_ls"), (SG, "s5_sg"), (NV, "s5_nv")):
            S.dma("sp", t[:], D[nm], writes=[nm])
        S.dma("sp", DD[:].rearrange("p e c -> p (e c)"), D["s5_dd"], writes=["s5_dd"])
        for t, nm in ((P1B, "s5_p1b"), (P2B, "s5_p2b"), (P1C, "s5_p1c"), (P2C, "s5_p2c")):
            S.dma("sp", t[:].rearrange("p g c -> p (g c)"), D[nm], writes=[nm])
        idf, idb = make_ident(C, es, "pid")
        STEP = sb("STEP", [128, 32]); AA = sb("AA", [128, 32]); PH = sb("PH", [128, 32])
        S.op("act", lambda e: e.activation(out=STEP[:], in_=LS[:], func=AF.Exp), reads=["s5_ls"], writes=["STEP"])
        S.op("dve", lambda e: e.tensor_tensor(out=AA[:], in0=LR[:], in1=STEP[:], op=ALU.mult), reads=["s5_lr", "STEP"], writes=["AA"])
        S.op("dve", lambda e: e.tensor_tensor(out=PH[:], in0=LI[:], in1=STEP[:], op=ALU.mult), reads=["s5_li", "STEP"], writes=["PH"])
        EXPO = sb("EXPO", [128, 32, 23]); ANG = sb("ANG", [128, 32, 23]); MAG = sb("MAG", [128, 32, 23])
        nvb = bc(NV[:].unsqueeze(1), [128, 32, 23])
        S.op("dve", lambda e: e.tensor_tensor(out=EXPO[:], in0=bc(AA[:].unsqueeze(2), [128, 32, 23]), in1=nvb, op=ALU.mult), reads=["AA", "s5_nv"], writes=["EXPO"])
        S.op("dve", lambda e: e.tensor_tensor(out=ANG[:], in0=bc(PH[:].unsqueeze(2), [128, 32, 23]), in1=nvb, op=ALU.mult), reads=["PH", "s5_nv"], writes=["pwang"])
        S.op("act", lambda e: e.activation(out=MAG[:], in_=EXPO[:], func=AF.Exp), reads=["EXPO"], writes=["MAG"])
        (sn, snk), (cs, csk) = sincos(C, es, ANG[:].rearrange("p g n -> p (g n)"), 32 * 23, "pw")
        CR = sb("CR", [128, 32, 23]); CI = sb("CI", [128, 32, 23])
        S.op("dve", lambda e: e.tensor_tensor(out=CR[:].rearrange("p g n -> p (g n)"), in0=MAG[:].rearrange("p g n -> p (g n)"), in1=cs[:], op=ALU.mult), reads=["MAG", csk], writes=["CR"])
        S.op("dve", lambda e: e.tensor_tensor(out=CI[:].rearrange("p g n -> p (g n)"), in0=MAG[:].rearrange("p g n -> p (g n)"), in1=sn[:], op=ALU.mult), reads=["MAG", snk], writes=["CI"])
        zr = sb("zr", [128, 32]); den = sb("den", [128, 32]); t0 = sb("t0", [128, 32]); fr = sb("fr", [128, 32]); fi = sb("fi", [128, 32])
        S.op("dve", lambda e: e.tensor_scalar(out=zr[:], in0=CR[:, :, 8], scalar1=-1.0, scalar2=None, op0=ALU.add), reads=["CR"], writes=["zr"])
        S.op("dve", lambda e: e.tensor_tensor(out=den[:], in0=LR[:], in1=LR[:], op=ALU.mult), reads=["s5_lr"], writes=["den"])
        S.op("dve", lambda e: e.tensor_tensor(out=t0[:], in0=LI[:], in1=LI[:], op=ALU.mult), reads=["s5_li"], writes=["t0"])
        S.op("dve", lambda e: e.tensor_tensor(out=den[:], in0=den[:], in1=t0[:], op=ALU.add), reads=["den", "t0"], writes=["den"])
        S.op("dve", lambda e: e.reciprocal(out=den[:], in_=den[:]), reads=["den"], writes=["den"])
        S.op("dve", lambda e: e.tensor_tensor(out=fr[:], in0=zr[:], in1=LR[:], op=ALU.mult), reads=["zr", "s5_lr"], writes=["fr"])
        S.op("dve", lambda e: e.tensor_tensor(out=t0[:], in0=CI[:, :, 8], in1=LI[:], op=ALU.mult), reads=["CI", "s5_li", "den"], writes=["t0"])
        S.op("dve", lambda e: e.tensor_tensor(out=fr[:], in0=fr[:], in1=t0[:], op=ALU.add), reads=["fr", "t0"], writes=["fr"])
        S.op("dve", lambda e: e.tensor_tensor(out=fr[:], in0=fr[:], in1=den[:], op=ALU.mult), reads=["fr", "den"], writes=["fr"])
        S.op("dve", lambda e: e.tensor_tensor(out=fi[:], in0=CI[:, :, 8], in1=LR[:], op=ALU.mult), reads=["CI", "s5_lr"], writes=["fi"])
        S.op("dve", lambda e: e.tensor_tensor(out=t0[:], in0=zr[:], in1=LI[:], op=ALU.mult), reads=["zr", "s5_li", "fr"], writes=["t0"])
        S.op("dve", lambda e: e.tensor_tensor(out=fi[:], in0=fi[:], in1=t0[:], op=ALU.subtract), reads=["fi", "t0"], writes=["fi"])
        S.op("dve", lambda e: e.tensor_tensor(out=fi[:], in0=fi[:], in1=den[:], op=ALU.mult), reads=["fi", "den"], writes=["fi"])
        BB1 = sb("BB1", [128, 32, 16]); BB2 = sb("BB2", [128, 32, 16]); ta = sb("ta", [128, 32, 16]); tb = sb("tb", [128, 32, 16])
        frb = bc(fr[:].unsqueeze(2), [128, 32, 16]); fib = bc(fi[:].unsqueeze(2), [128, 32, 16])
        fl = lambda t: t[:].rearrange("p g c -> p (g c)")
        S.op("dve", lambda e: e.tensor_tensor(out=ta[:], in0=P1B[:], in1=frb, op=ALU.mult), reads=["s5_p1b", "fr"], writes=["ta"])
        S.op("dve", lambda e: e.tensor_tensor(out=tb[:], in0=P2B[:], in1=fib, op=ALU.mult), reads=["s5_p2b", "fi"], writes=["tb"])
        S.op("dve", lambda e: e.scalar_tensor_tensor(out=fl(BB1), in0=fl(tb), scalar=SG[:, 0:1], in1=fl(ta), op0=ALU.mult, op1=ALU.add), reads=["ta", "tb", "s5_sg"], writes=["BB1"])
        S.op("dve", lambda e: e.tensor_tensor(out=ta[:], in0=P2B[:], in1=frb, op=ALU.mult), reads=["s5_p2b", "fr", "BB1"], writes=["ta"])
        S.op("dve", lambda e: e.tensor_tensor(out=tb[:], in0=P1B[:], in1=fib, op=ALU.mult), reads=["s5_p1b", "fi", "BB1"], writes=["tb"])
        S.op("dve", lambda e: e.scalar_tensor_tensor(out=fl(BB2), in0=fl(tb), scalar=SG[:, 1:2], in1=fl(ta), op0=ALU.mult, op1=ALU.add), reads=["ta", "tb", "s5_sg"], writes=["BB2"])
        t5 = sb("t5", [128, 32, 16]); t6 = sb("t6", [128, 32, 16])
        Rm = sb("Rm", [128, 32, 8, 16])
        Q1 = sb("Q1", [128, 32, 16]); Q2 = sb("Q2", [128, 32, 16])
        S.op("dve", lambda e: e.tensor_scalar(out=fl(Q1), in0=fl(P1C), scalar1=SG[:, 1:2], scalar2=None, op0=ALU.mult), reads=["s5_p1c", "s5_sg"], writes=["Q1"])
        S.op("dve", lambda e: e.tensor_scalar(out=fl(Q2), in0=fl(P2C), scalar1=SG[:, 0:1], scalar2=None, op0=ALU.mult), reads=["s5_p2c", "s5_sg"], writes=["Q2"])
        CAv = CA[:].rearrange("p g (t c) -> p g t c", c=16); CAsv = CAs[:].rearrange("p g (t c) -> p g t c", c=16)
        for t in range(8):
            for (dst, dkey, a_, akey, b_, bkey, idx) in ((Rm[:, :, t, :], "Rm", Q1, "Q1", P2C, "s5_p2c", 7 + t),
                                                         (CAv[:, :, t, :], "CA", Q1, "Q1", P2C, "s5_p2c", 15 + t),
                                                         (CAsv[:, :, t, :], "CAs", Q2, "Q2", P1C, "s5_p1c", 15 + t)):
                S.op("dve", lambda e, a_=a_, idx=idx: e.tensor_tensor(out=t5[:], in0=a_[:], in1=bc(CR[:, :, idx:idx + 1], [128, 32, 16]), op=ALU.mult), reads=[akey, "CR"], writes=["t5"])
                S.op("dve", lambda e, b_=b_, idx=idx: e.tensor_tensor(out=t6[:], in0=b_[:], in1=bc(CI[:, :, idx:idx + 1], [128, 32, 16]), op=ALU.mult), reads=[bkey, "CI"], writes=["t6"])
                S.op("dve", lambda e, dst=dst: e.tensor_tensor(out=dst, in0=t5[:], in1=t6[:], op=ALU.subtract), reads=["t5", "t6"], writes=[dkey])
        Wr = sb("Wr", [128, 9, 32]); Wi = sb("Wi", [128, 9, 32]); sq = sb("sq", [128, 32])
        S.op("dve", lambda e: e.tensor_copy(out=Wr[:, 0, :], in_=CR[:, :, 15]), reads=["CR"], writes=["Wr"])
        S.op("dve", lambda e: e.tensor_copy(out=Wi[:, 0, :], in_=CI[:, :, 15]), reads=["CI"], writes=["Wi"])
        for j in range(8):
            S.op("dve", lambda e, j=j: e.tensor_tensor(out=Wr[:, j + 1, :], in0=Wr[:, j, :], in1=Wr[:, j, :], op=ALU.mult), reads=["Wr"], writes=["Wr"])
            S.op("dve", lambda e, j=j: e.tensor_tensor(out=sq[:], in0=Wi[:, j, :], in1=Wi[:, j, :], op=ALU.mult), reads=["Wi"], writes=["sq"])
            S.op("dve", lambda e, j=j: e.tensor_tensor(out=Wr[:, j + 1, :], in0=Wr[:, j + 1, :], in1=sq[:], op=ALU.subtract), reads=["Wr", "sq"], writes=["Wr"])
            S.op("dve", lambda e, j=j: e.scalar_tensor_tensor(out=Wi[:, j + 1, :], in0=Wr[:, j, :], scalar=2.0, in1=Wi[:, j, :], op0=ALU.mult, op1=ALU.mult), reads=["Wr", "Wi"], writes=["Wi"])
        Wrv = Wr[:].rearrange("p j (a r w) -> p j a r w", r=2, w=2); Wiv = Wi[:].rearrange("p j (a r w) -> p j a r w", r=2, w=2)
        for r in range(2):
            pr_ = slice(64 * r, 64 * r + 64)
            for j in range(9):
                S.op("dve", lambda e, r=r, pr_=pr_, j=j: e.tensor_copy(out=WPr[pr_, j, :].rearrange("p (a w) -> p a w", w=2), in_=Wrv[pr_, j, :, r, :]), reads=["Wr"], writes=["WPr"])
                S.op("dve", lambda e, r=r, pr_=pr_, j=j: e.tensor_copy(out=WPi[pr_, j, :].rearrange("p (a w) -> p a w", w=2), in_=Wiv[pr_, j, :, r, :]), reads=["Wi"], writes=["WPi"])
        S.op("dve", lambda e: e.tensor_scalar(out=WPn[:].rearrange("p j q -> p (j q)"), in0=WPi[:].rearrange("p j q -> p (j q)"), scalar1=-1.0, scalar2=None, op0=ALU.mult), reads=["WPi"], writes=["WPn"])
        E = [[sb("E%d%d" % (h_, r), [128, 128]) for r in range(2)] for h_ in range(2)]
        for h_ in range(2):
            for r in range(2):
                S.op("pool", lambda e, h_=h_, r=r: e.memset(E[h_][r][:], 0.0), writes=["E%d%d" % (h_, r)])
                S.op("pool", lambda e, h_=h_, r=r: e.tensor_copy(out=E[h_][r][64 * h_:64 * h_ + 64, 64 * r:64 * r + 64], in_=idf[64 * h_:64 * h_ + 64, 64 * h_:64 * h_ + 64]),
                     reads=["pidf", "E%d%d" % (h_, r)], writes=["E%d%d" % (h_, r)])
        Lz = [sb("Lz%d" % i, [128, 32, 64]) for i in range(2)]
        for i in range(2):
            S.op("pool", lambda e, i=i: e.memset(Lz[i][:].rearrange("p g c -> p (g c)"), 0.0), writes=["Lz%d" % i])
        S.op("pool", lambda e: e.memset(Mz2[:].rearrange("p a b c -> p (a b c)"), 0.0), writes=["Mz2"])
        t5v = t5[:].rearrange("p (a j) c -> p a j c", j=4); t6v = t6[:].rearrange("p (a j) c -> p a j c", j=4)
        with scope(C) as esp:
            PM = C.ps(esp, "PM", [128, 16, 128]); PL = C.ps(esp, "PL", [128, 8, 2, 128])
            for s in range(8):
                i = 7 - s; sl = s % 2; lk = "Lz%d" % sl
                Lzv = Lz[sl][:].rearrange("p (a j) c -> p a j c", j=4)
                S.op("dve", lambda e, i=i: e.tensor_tensor(out=t5[:], in0=BB1[:], in1=bc(CR[:, :, i:i + 1], [128, 32, 16]), op=ALU.mult), reads=["BB1", "CR"], writes=["t5"])
                S.op("dve", lambda e, i=i: e.tensor_tensor(out=t6[:], in0=BB2[:], in1=bc(CI[:, :, i:i + 1], [128, 32, 16]), op=ALU.mult), reads=["BB2", "CI"], writes=["t6"])
                for j4 in range(4):
                    S.op("dve", lambda e, j4=j4, Lzv=Lzv: e.scalar_tensor_tensor(out=Lzv[:, :, j4, 16 * j4:16 * j4 + 16], in0=t6v[:, :, j4, :], scalar=SG[:, 0:1], in1=t5v[:, :, j4, :], op0=ALU.mult, op1=ALU.add),
                         reads=["t5", "t6", "s5_sg"], writes=[lk])
                for g in range(32):
                    chc = g // 8; hb = (g % 8) // 4; j4 = g % 4; e_ = chc * 4 + j4
                    rows = slice(64 * hb, 64 * hb + 64)
                    S.op("pe", lambda e, g=g, sl=sl, e_=e_, rows=rows: e.matmul(PM[rows, e_, :], lhsT=Lz[sl][:, g, :], rhs=Rm[:, g, :, :].rearrange("p t c -> p (t c)"), start=True, stop=True),
                         reads=[lk, "Rm"], writes=["PM"])
                for pr in range(16):
                    a_ = pr // 2; wp = pr % 2; chc = a_ // 2; hb = a_ % 2; e2 = chc * 2 + wp
                    rows = slice(64 * hb, 64 * hb + 64)
                    for h_ in range(2):
                        for r in range(2):
                            g = 4 * a_ + 2 * r + wp
                            S.op("pe", lambda e, g=g, sl=sl, e2=e2, rows=rows, h_=h_, r=r: e.matmul(PL[rows, e2, h_, :], lhsT=Lz[sl][:, g, :], rhs=E[h_][r][:], start=(r == 0), stop=(r == 1)),
                                 reads=[lk, "E%d%d" % (h_, r)], writes=["PL"])
                S.op("dve", lambda e, s=s: e.tensor_copy(out=Mz2[:, :, s, 16 * s:128], in_=PM[:, :, 16 * s:128]), reads=["PM"], writes=["Mz2"])
                S.op("dve", lambda e, s=s: e.tensor_tensor(out=Mz2[:, :, s, 16 * s:16 * s + 16], in0=PM[:, :, 16 * s:16 * s + 16], in1=DD[:], op=ALU.add), reads=["PM", "s5_dd"], writes=["Mz2"])
                S.op("act", lambda e, s=s: e.copy(out=LT2[:, :, s, :, :], in_=PL[:]), reads=["PL"], writes=["LT2"])
        S.dma("sp", D["Mz_d"], Mz2[:].rearrange("p a b c -> p (a b c)"), reads=["Mz2"], writes=["Mz_d"])
        S.dma("sp", D["CA_d"][:, 0, :], CA[:].rearrange("p g c -> p (g c)"), reads=["CA"], writes=["CA_d"])
        S.dma("sp", D["CA_d"][:, 1, :], CAs[:].rearrange("p g c -> p (g c)"), reads=["CAs"], writes=["CA_d"])
    return P


def load_weight_bf16(C, es, es_tmp, name, src, rows_chunks, ncols, gcol=None, q="sp"):
    S = C.S
    W = C.sb(es, name, [128, rows_chunks, ncols], BF16)
    stg = [C.sb(es_tmp, name + "_stg%d" % i, [128, ncols]) for i in range(2)]
    srcv = src.rearrange("(c p) n -> c p n", p=128)
    for c in range(rows_chunks):
        st = stg[c % 2]; sk = name + "_stg%d" % (c % 2)
        S.dma(q, st[:], srcv[c], writes=[sk])
        eng = "dve" if c % 2 == 0 else "pool"
        if gcol is not None:
            S.op(eng, lambda e, st=st, c=c: e.tensor_scalar(out=W[:, c, :], in0=st[:], scalar1=gcol[0][:, c:c + 1], scalar2=None, op0=ALU.mult),
                 reads=[sk, gcol[1]], writes=[name])
        else:
            S.op(eng, lambda e, st=st, c=c: e.tensor_copy(out=W[:, c, :], in_=st[:]), reads=[sk], writes=[name])
    return W


def rms_rstd(C, x_ap, xkey, junk, jkey, ss, rs, skey, n):
    S = C.S
    S.op("act", lambda e: e.activation(out=junk, in_=x_ap, func=AF.Square, accum_out=ss), reads=[xkey], writes=[skey + "_ss"])
    S.op("act", lambda e: e.activation(out=rs, in_=ss, func=AF.Sqrt, scale=1.0 / n, bias=EPS), reads=[skey + "_ss"], writes=[skey + "_sq"])
    S.op("dve", lambda e: e.reciprocal(out=rs, in_=rs), reads=[skey + "_sq"], writes=[skey])


def transpose_chunks(C, src, skey, nch, pbank, pkey, dst, dkey, idb, evac="act"):
    S = C.S
    for c in range(nch):
        S.op("pe", lambda e, c=c: e.transpose(out=pbank[:, c, :], in_=src[:, c * 128:(c + 1) * 128], identity=idb[:]), reads=[skey, "identb"], writes=[pkey])
    if evac == "act":
        S.op("act", lambda e: e.copy(out=dst, in_=pbank), reads=[pkey], writes=[dkey])
    else:
        S.op(evac, lambda e: e.tensor_copy(out=dst, in_=pbank), reads=[pkey], writes=[dkey])


def stage_mixer(C, D, dbg=None, upto=9, prep=None):
    S, nc = C.S, C.nc
    dbg = dbg or {}
    with scope(C) as es1:
        P = s5_params(C, es1, D)
        idf = C.sb(es1, "identf", [128, 128]); idb = C.sb(es1, "identb", [128, 128], BF16)
        S.op("pool", lambda e: e.memset(idf[:], 1.0), writes=["identf"])
        S.op("pool", lambda e: e.affine_select(out=idf[:], in_=idf[:], pattern=[[-1, 128]], compare_op=ALU.is_equal, fill=0.0, base=0, channel_multiplier=1), reads=["identf"], writes=["identf"])
        S.op("dve", lambda e: e.tensor_copy(out=idb[:], in_=idf[:]), reads=["identf"], writes=["identb"])
        if "WPr" in dbg:
            for nm in ("WPr", "WPi"):
                S.dma("sp", dbg[nm], P[nm][:].rearrange("p a b -> p (a b)"), reads=[nm], writes=["o_" + nm])
            S.dma("pool", dbg["LT2"], P["LT2"][:].rearrange("p a b c d -> p (a b c d)"), reads=["LT2"], writes=["o_LT2"])
            S.dma("pool", dbg["Mz"], D["Mz_d"], reads=["Mz_d"], writes=["o_Mz"])
            S.dma("pool", dbg["CA"], D["CA_d"].rearrange("p a b -> p (a b)"), reads=["CA_d"], writes=["o_CA"])
        if upto < 1:
            return
        carry_r = C.sb(es1, "carry_r", [128, 16]); carry_i = C.sb(es1, "carry_i", [128, 16])
        S.op("pool", lambda e: e.memset(carry_r[:], 0.0), writes=["carry_r"])
        S.op("pool", lambda e: e.memset(carry_i[:], 0.0), writes=["carry_i"])
        TRE = C.sb(es1, "TRE", [128, 16, 257]); TIM = C.sb(es1, "TIM", [128, 16, 257])
        uT = C.sb(es1, "uT", [128, 4, 8, 256], BF16)
        with scope(C) as es2:
            _mixer_passes(C, es2, D, P, idb, carry_r, carry_i, TRE, TIM, uT, dbg)
        if "carry" in dbg:
            S.dma("sp", dbg["carry"][:, 0:16], carry_r[:], reads=["carry_r"], writes=["o_carry"])
            S.dma("sp", dbg["carry"][:, 16:32], carry_i[:], reads=["carry_i"], writes=["o_carry2"])
        if upto < 2:
            return
        with scope(C) as es3:
            ytm = C.sb(es3, "ytm", [128, 2, 8, 512], BF16)
            with scope(C) as es4:
                _s5_scan_out(C, es4, D, P, idb, TRE, TIM, uT, ytm, carry_r, carry_i, dbg)
            if upto < 3:
                return
            _s5_glu_out(C, es3, D, idb, ytm, dbg)
    if upto < 4:
        return
    with scope(C) as es5:
        idf = C.sb(es5, "identf", [128, 128]); idb = C.sb(es5, "identb", [128, 128], BF16)
        S.op("pool", lambda e: e.memset(idf[:], 1.0), reads=[], writes=["identf"])
        S.op("pool", lambda e: e.affine_select(out=idf[:], in_=idf[:], pattern=[[-1, 128]], compare_op=ALU.is_equal, fill=0.0, base=0, channel_multiplier=1), reads=["identf"], writes=["identf"])
        S.op("dve", lambda e: e.tensor_copy(out=idb[:], in_=idf[:]), reads=["identf"], writes=["identb"])
        _attention(C, es5, D, idb, dbg, prep)


def _mixer_passes(C, es, D, P, idb, carry_r, carry_i, TRE, TIM, uT, dbg):
    S = C.S
    sb = lambda name, shape, dt=F32: C.sb(es, name, shape, dt)
    gin = sb("gin", [128, 8])
    S.dma("sp", gin[:], D["g_mix"], writes=["gin"])
    with scope(C) as est:
        Wb = load_weight_bf16(C, es, est, "Wb", D["w_in"], 8, 2048, gcol=(gin, "gin"))
    gq = sb("gq", [128, 64]); gk = sb("gk", [128, 64]); hv = sb("hv", [128, 4])
    S.dma("sp", gq[:], D["att_q_g"].partition_broadcast(128), writes=["gq"])
    S.dma("sp", gk[:], D["att_k_g"].partition_broadcast(128), writes=["gk"])
    S.dma("sp", hv[:], D["hvalid"], writes=["hv"])
    S.op("dve", lambda e: e.tensor_scalar(out=gq[:], in0=gq[:], scalar1=0.125, scalar2=None, op0=ALU.mult), reads=["gq"], writes=["gq"])
    xt = [sb("xt%d" % i, [128, 1024]) for i in range(2)]
    junk = sb("junk", [128, 1024]); st = [sb("st%d" % i, [128, 4]) for i in range(2)]
    xn = [sb("xn%d" % i, [128, 1024], BF16) for i in range(2)]
    xnT = [sb("xnT%d" % i, [128, 8, 512], BF16) for i in range(2)]
    qkv = sb("qkv", [128, 3, 512]); sq = sb("sq2", [128, 512]); qst = sb("qst", [128, 4, 8])
    qn = sb("qn", [128, 2, 512], BF16)
    kTs = [sb("kTs%d" % i, [128, 4, 128], BF16) for i in range(2)]; qTs = [sb("qTs%d" % i, [128, 4, 128], BF16) for i in range(2)]
    Vs = [sb("Vs%d" % i, [128, 8, 65], BF16) for i in range(2)]
    tA = [(sb("tAr%d" % i, [128, 256]), sb("tAi%d" % i, [128, 256])) for i in range(2)]
    tB = [(sb("tBr%d" % i, [128, 128]), sb("tBi%d" % i, [128, 128])) for i in range(2)]
    REDr = sb("REDr", [128, 16]); REDi = sb("REDi", [128, 16]); c1 = sb("c1", [128, 16]); c2 = sb("c2", [128, 16]); c3 = sb("c3", [128, 16])
    with scope(C) as esp:
        bank = [C.ps(esp, "bk%d" % i, [128, 512]) for i in range(8)]
        xv = D["x_ext"].rearrange("(n p) d -> n p d", p=128)
        tile_ctr = 0
        for q in range(4):
            for blk in range(4):
                bslot = (q * 4 + blk) % 2
                xT = xnT[bslot]; xTk = "xnT%d" % bslot
                for tt in range(4):
                    n_tile = q * 16 + blk * 4 + tt
                    sl = tile_ctr % 2; tile_ctr += 1
                    x_ = xt[sl]; xk = "xt%d" % sl
                    S.dma("sp", x_[:], xv[n_tile], writes=[xk])
                    rms_rstd(C, x_[:], xk, junk[:], "junk", st[sl][:, 0:1], st[sl][:, 1:2], "st%d" % sl, 1024)
                    S.op("dve", lambda e, x_=x_, sl=sl: e.tensor_scalar(out=xn[sl][:], in0=x_[:], scalar1=st[sl][:, 1:2], scalar2=None, op0=ALU.mult),
                         reads=[xk, "st%d" % sl], writes=["xn%d" % sl])
                    tb_ = 0 if sl == 0 else 7
                    pb = bank[tb_][:].bitcast(BF16).rearrange("p (c n) -> p c n", n=128)
                    transpose_chunks(C, xn[sl][:], "xn%d" % sl, 8, pb, "bk%d" % tb_, xT[:, :, tt * 128:(tt + 1) * 128], xTk, idb, evac=("act" if sl == 0 else "dve"))
                for chc in range(4):
                    bi = 1 + (chc % 2); bkk = "bk%d" % bi
                    for dc in range(8):
                        S.op("pe", lambda e, chc=chc, dc=dc, bi=bi, xT=xT: e.matmul(bank[bi][:], lhsT=Wb[:, dc, 1536 + chc * 128:1536 + (chc + 1) * 128], rhs=xT[:, dc, :], start=(dc == 0), stop=(dc == 7)),
                             reads=["Wb", xTk], writes=[bkk])
                    eng = "act" if chc % 2 == 0 else "dve"
                    src = bank[bi][:].rearrange("p (k s) -> p s k", s=8)
                    dst = uT[:, chc, :, blk * 64:(blk + 1) * 64]
                    if eng == "act":
                        S.op("act", lambda e, src=src, dst=dst: e.copy(out=dst, in_=src), reads=[bkk], writes=["uT"])
                    else:
                        S.op("dve", lambda e, src=src, dst=dst: e.tensor_copy(out=dst, in_=src), reads=[bkk], writes=["uT"])
                need_kv = (q == 3) or (q == 2 and blk == 3)
                need_q = (q == 3)
                if need_kv:
                    for tt in range(4):
                        n_tile = q * 16 + blk * 4 + tt
                        kvt = n_tile - 44
                        sl = kvt % 2
                        projs = [(1, 512, 3), (2, 1024, 4)] + ([(0, 0, 5)] if need_q else [])
                        for (pi, c0, bi) in projs:
                            for dc in range(8):
                                S.op("pe", lambda e, dc=dc, bi=bi, c0=c0, tt=tt, xT=xT: e.matmul(bank[bi][:], lhsT=xT[:, dc, tt * 128:(tt + 1) * 128], rhs=Wb[:, dc, c0:c0 + 512], start=(dc == 0), stop=(dc == 7)),
                                     reads=["Wb", xTk], writes=["bk%d" % bi])
                        S.op("act", lambda e, sl=sl: e.copy(out=Vs[sl][:, :, 0:64], in_=bank[4][:].rearrange("p (h d) -> p h d", d=64)), reads=["bk4"], writes=["Vs%d" % sl])
                        if kvt < 4:
                            S.op("pool", lambda e, sl=sl, kvt=kvt: e.tensor_copy(out=Vs[sl][:, :, 64], in_=bc(hv[:, kvt:kvt + 1], [128, 8])), reads=["hv"], writes=["Vs%d" % sl])
                        else:
                            S.op("pool", lambda e, sl=sl: e.memset(Vs[sl][:, :, 64], 1.0), reads=[], writes=["Vs%d" % sl])
                        S.dma("sp", D["V_d"][kvt], Vs[sl][:].rearrange("p h d -> p (h d)"), reads=["Vs%d" % sl], writes=["V_d"])
                        for (pi, bi, gt, gkey, dstT, dkey, dram, ncol_t) in ([(1, 3, gk, "gk", kTs[sl], "kTs%d" % sl, D["kT_d"], kvt)] +
                                                                          ([(0, 5, gq, "gq", qTs[sl], "qTs%d" % sl, D["qT_d"], kvt - 4)] if need_q else [])):
                            qs = qkv[:, pi, :]; qk_ = "qkv%d" % pi
                            S.op("act", lambda e, qs=qs, bi=bi: e.copy(out=qs, in_=bank[bi][:]), reads=["bk%d" % bi], writes=[qk_])
                            S.op("pool", lambda e, qs=qs: e.tensor_tensor(out=sq[:], in0=qs, in1=qs, op=ALU.mult), reads=[qk_], writes=["sq2"])
                            S.op("dve", lambda e, pi=pi: e.tensor_reduce(out=qst[:, pi, :], in_=sq[:].rearrange("p (h d) -> p h d", d=64), axis=AX.X, op=ALU.add), reads=["sq2"], writes=["qst%d" % pi])
                            S.op("act", lambda e, pi=pi: e.activation(out=qst[:, 2 + pi, :], in_=qst[:, pi, :], func=AF.Sqrt, scale=1.0 / 64, bias=EPS), reads=["qst%d" % pi], writes=["qsq%d" % pi])
                            S.op("dve", lambda e, pi=pi: e.reciprocal(out=qst[:, 2 + pi, :], in_=qst[:, 2 + pi, :]), reads=["qsq%d" % pi], writes=["qrs%d" % pi])
                            S.op("dve", lambda e, qs=qs, pi=pi: e.tensor_tensor(out=qs.rearrange("p (h d) -> p h d", d=64), in0=qs.rearrange("p (h d) -> p h d", d=64),
                                                                        in1=bc(qst[:, 2 + pi, :].unsqueeze(2), [128, 8, 64]), op=ALU.mult), reads=[qk_, "qrs%d" % pi], writes=[qk_])
                            S.op("pool", lambda e, qs=qs, pi=pi, gt=gt: e.tensor_tensor(out=qn[:, pi, :].rearrange("p (h d) -> p h d", d=64), in0=qs.rearrange("p (h d) -> p h d", d=64),
                                                                               in1=bc(gt[:].unsqueeze(1), [128, 8, 64]), op=ALU.mult), reads=[qk_, gkey], writes=["qn%d" % pi])
                            pb = bank[6][:].bitcast(BF16).rearrange("p (c n) -> p c n", n=128)[:, 0:4, :]
                            transpose_chunks(C, qn[:, pi, :], "qn%d" % pi, 4, pb, "bk6", dstT[:], dkey, idb)
                            S.dma("sp", dram.rearrange("p (c n) -> p c n", c=4)[:, :, ncol_t * 128:(ncol_t + 1) * 128], dstT[:], reads=[dkey], writes=["qkT_d"])
            for pair in range(16):
                psl = pair % 2
                br = bank[1 + 2 * psl]; bim = bank[2 + 2 * psl]; brk = "bk%d" % (1 + 2 * psl); bik = "bk%d" % (2 + 2 * psl)
                a_ = pair // 2; wp = pair % 2; chc = a_ // 2; hb = a_ % 2; e2 = chc * 2 + wp
                rows = slice(64 * hb, 64 * hb + 64)
                for half, (bkt, bkk) in enumerate(((br, brk), (bim, bik))):
                    for s in range(8):
                        S.op("pe", lambda e, e2=e2, s=s, half=half, rows=rows, chc=chc, bkt=bkt: e.matmul(bkt[:, 0:256], lhsT=P["LT2"][rows, e2, s, half, :], rhs=uT[rows, chc, s, :], start=(s == 0), stop=(s == 7)),
                             reads=["LT2", "uT"], writes=[bkk])
                if q < 3:
                    ar_, ai_ = tA[psl]; ark, aik = "tAr%d" % psl, "tAi%d" % psl
                    b_r, b_i = tB[psl]; brk2, bik2 = "tBr%d" % psl, "tBi%d" % psl
                    S.op("act", lambda e, ar_=ar_, br=br: e.copy(out=ar_[:], in_=br[:, 0:256]), reads=[brk], writes=[ark])
                    S.op("act", lambda e, ai_=ai_, bim=bim: e.copy(out=ai_[:], in_=bim[:, 0:256]), reads=[bik], writes=[aik])
                    src = (ar_, ai_, ark, aik); dst = (b_r, b_i, brk2, bik2)
                    for j in range(8):
                        n = 256 >> j; h = n // 2
                        sr, si, srk, sik = src; dr, di, drk, dik = dst
                        wr = P["WPr"][:, j, pair:pair + 1]; wi = P["WPi"][:, j, pair:pair + 1]; wn = P["WPn"][:, j, pair:pair + 1]
                        if j == 7:
                            odr, odi, odrk, odik = REDr[:, pair:pair + 1], REDi[:, pair:pair + 1], "REDr", "REDi"
                        else:
                            odr, odi, odrk, odik = dr[:, 0:h], di[:, 0:h], drk, dik
                        S.op("dve", lambda e, odr=odr, sr=sr, wr=wr, n=n: e.scalar_tensor_tensor(out=odr, in0=sr[:, 0:n:2], scalar=wr, in1=sr[:, 1:n:2], op0=ALU.mult, op1=ALU.add), reads=[srk, "WPr"], writes=[odrk])
                        S.op("dve", lambda e, odr=odr, si=si, wn=wn, n=n: e.scalar_tensor_tensor(out=odr, in0=si[:, 0:n:2], scalar=wn, in1=odr, op0=ALU.mult, op1=ALU.add), reads=[sik, "WPn", odrk], writes=[odrk])
                        S.op("dve", lambda e, odi=odi, si=si, wr=wr, n=n: e.scalar_tensor_tensor(out=odi, in0=si[:, 0:n:2], scalar=wr, in1=si[:, 1:n:2], op0=ALU.mult, op1=ALU.add), reads=[sik, "WPr"], writes=[odik])
                        S.op("dve", lambda e, odi=odi, sr=sr, wi=wi, n=n: e.scalar_tensor_tensor(out=odi, in0=sr[:, 0:n:2], scalar=wi, in1=odi, op0=ALU.mult, op1=ALU.add), reads=[srk, "WPi", odik], writes=[odik])
                        src, dst = dst, src
                else:
                    S.op("act", lambda e, pair=pair, br=br: e.copy(out=TRE[:, pair, 1:257], in_=br[:, 0:256]), reads=[brk], writes=["TRE%d" % pair])
                    S.op("act", lambda e, pair=pair, bim=bim: e.copy(out=TIM[:, pair, 1:257], in_=bim[:, 0:256]), reads=[bik], writes=["TIM%d" % pair])
            if q < 3:
                w8r = P["WPr"][:, 8, :]; w8i = P["WPi"][:, 8, :]
                S.op("dve", lambda e: e.tensor_tensor(out=c1[:], in0=w8r, in1=carry_r[:], op=ALU.mult), reads=["WPr", "carry_r"], writes=["c1"])
                S.op("dve", lambda e: e.tensor_tensor(out=c2[:], in0=w8i, in1=carry_i[:], op=ALU.mult), reads=["WPi", "carry_i"], writes=["c2"])
                S.op("dve", lambda e: e.tensor_tensor(out=c1[:], in0=c1[:], in1=c2[:], op=ALU.subtract), reads=["c1", "c2"], writes=["c1"])
                S.op("dve", lambda e: e.tensor_tensor(out=c1[:], in0=c1[:], in1=REDr[:], op=ALU.add), reads=["c1", "REDr"], writes=["c1"])
                S.op("dve", lambda e: e.tensor_tensor(out=c2[:], in0=w8r, in1=carry_i[:], op=ALU.mult), reads=["WPr", "carry_i", "c1"], writes=["c2"])
                S.op("dve", lambda e: e.tensor_tensor(out=c3[:], in0=w8i, in1=carry_r[:], op=ALU.mult), reads=["WPi", "carry_r"], writes=["c3"])
                S.op("dve", lambda e: e.tensor_tensor(out=c2[:], in0=c2[:], in1=c3[:], op=ALU.add), reads=["c2", "c3"], writes=["c2"])
                S.op("dve", lambda e: e.tensor_tensor(out=carry_i[:], in0=c2[:], in1=REDi[:], op=ALU.add), reads=["c2", "REDi"], writes=["carry_i"])
                S.op("dve", lambda e: e.tensor_copy(out=carry_r[:], in_=c1[:]), reads=["c1"], writes=["carry_r"])


def _s5_scan_out(C, es, D, P, idb, TRE, TIM, uT, ytm, carry_r, carry_i, dbg):
    S = C.S
    sb = lambda name, shape, dt=F32: C.sb(es, name, shape, dt)
    Mz2 = sb("Mz2s", [128, 16, 8, 128], BF16); CAA = sb("CAA", [128, 2, 32, 128], BF16)
    S.dma("sp", Mz2[:].rearrange("p a b c -> p (a b c)"), D["Mz_d"], reads=["Mz_d"], writes=["Mz2s"])
    S.dma("sp", CAA[:].rearrange("p a g c -> p a (g c)"), D["CA_d"], reads=["CA_d"], writes=["CAA"])
    tmr = [sb("tmr%d" % i, [128, 256]) for i in range(2)]; tmi = [sb("tmi%d" % i, [128, 256]) for i in range(2)]
    Tbr = [sb("Tbr%d" % i, [128, 256], BF16) for i in range(2)]; Tbi = [sb("Tbi%d" % i, [128, 256], BF16) for i in range(2)]
    Yg = [sb("Yg%d" % i, [128, 256], BF16) for i in range(2)]
    ysum = [sb("ysum%d" % i, [128, 256]) for i in range(2)]
    import os
    CUT = int(os.environ.get("SCAN_CUT", "9"))
    with scope(C) as esp:
        bank = [C.ps(esp, "sbk%d" % i, [128, 512]) for i in range(6)]
        for pair in range(16):
            sl = pair % 2
            a_ = pair // 2; wp = pair % 2; chc = a_ // 2; hb = a_ % 2
            rows = slice(64 * hb, 64 * hb + 64)
            kr, ki = "TRE%d" % pair, "TIM%d" % pair
            S.op("pool", lambda e, pair=pair: e.tensor_copy(out=TRE[:, pair, 0:1], in_=carry_r[:, pair:pair + 1]), reads=["carry_r"], writes=[kr])
            S.op("pool", lambda e, pair=pair: e.tensor_copy(out=TIM[:, pair, 0:1], in_=carry_i[:, pair:pair + 1]), reads=["carry_i"], writes=[ki])
            if CUT < 1:
                continue
            for j in range(9):
                d = 1 << j; m = 257 - d
                wr = P["WPr"][:, j, pair:pair + 1]; wi = P["WPi"][:, j, pair:pair + 1]; wn = P["WPn"][:, j, pair:pair + 1]
                tr = tmr[sl][:, 0:m]; ti = tmi[sl][:, 0:m]; trk = "tmr%d" % sl; tik = "tmi%d" % sl
                S.op("dve", lambda e, tr=tr, pair=pair, m=m, wr=wr: e.tensor_scalar(out=tr, in0=TRE[:, pair, 0:m], scalar1=wr, scalar2=None, op0=ALU.mult), reads=[kr, "WPr"], writes=[trk])
                S.op("dve", lambda e, tr=tr, pair=pair, m=m, wn=wn: e.scalar_tensor_tensor(out=tr, in0=TIM[:, pair, 0:m], scalar=wn, in1=tr, op0=ALU.mult, op1=ALU.add), reads=[ki, "WPn", trk], writes=[trk])
                S.op("dve", lambda e, ti=ti, pair=pair, m=m, wr=wr: e.tensor_scalar(out=ti, in0=TIM[:, pair, 0:m], scalar1=wr, scalar2=None, op0=ALU.mult), reads=[ki, "WPr"], writes=[tik])
                S.op("dve", lambda e, ti=ti, pair=pair, m=m, wi=wi: e.scalar_tensor_tensor(out=ti, in0=TRE[:, pair, 0:m], scalar=wi, in1=ti, op0=ALU.mult, op1=ALU.add), reads=[kr, "WPi", tik], writes=[tik])
                S.op("pool", lambda e, tr=tr, pair=pair, d=d: e.tensor_tensor(out=TRE[:, pair, d:257], in0=TRE[:, pair, d:257], in1=tr, op=ALU.add), reads=[kr, trk], writes=[kr])
                S.op("pool", lambda e, ti=ti, pair=pair, d=d: e.tensor_tensor(out=TIM[:, pair, d:257], in0=TIM[:, pair, d:257], in1=ti, op=ALU.add), reads=[ki, tik], writes=[ki])
            if CUT < 2:
                continue
            S.op("act", lambda e, pair=pair, sl=sl: e.copy(out=Tbr[sl][:], in_=TRE[:, pair, 0:256]), reads=[kr], writes=["Tbr%d" % sl])
            S.op("act", lambda e, pair=pair, sl=sl: e.copy(out=Tbi[sl][:], in_=TIM[:, pair, 0:256]), reads=[ki], writes=["Tbi%d" % sl])
            for r in range(2):
                g = 4 * a_ + 2 * r + wp
                e_ = chc * 4 + (g % 4)
                pr = slice(64 * r, 64 * r + 64)
                yb = bank[r]; ybk = "sbk%d" % r
                for s in range(8):
                    S.op("pe", lambda e, e_=e_, s=s, rows=rows, chc=chc, yb=yb: e.matmul(yb[:, 0:256], lhsT=Mz2[rows, e_, s, :], rhs=uT[rows, chc, s, :], start=(s == 0), stop=(s == 7)),
                         reads=["Mz2s", "uT"], writes=[ybk])
                zb_ = bank[4 + r]; zbk = "sbk%d" % (4 + r)
                S.op("pe", lambda e, g=g, pr=pr, zb_=zb_, r=r, sl=sl: e.matmul(zb_[:, 0:256], lhsT=CAA[pr, r, g, :], rhs=Tbr[sl][pr, :], start=True, stop=False), reads=["CAA", "Tbr%d" % sl], writes=[zbk])
                S.op("pe", lambda e, g=g, pr=pr, zb_=zb_, r=r, sl=sl: e.matmul(zb_[:, 0:256], lhsT=CAA[pr, 1 - r, g, :], rhs=Tbi[sl][pr, :], start=False, stop=True), reads=["CAA", "Tbi%d" % sl], writes=[zbk])
                if CUT < 3:
                    continue
                S.op("act", lambda e, r=r, zb_=zb_: e.copy(out=ysum[r][:], in_=zb_[:, 0:256]), reads=[zbk], writes=["ysum%d" % r])
                S.op("dve", lambda e, r=r, yb=yb: e.tensor_tensor(out=ysum[r][:], in0=yb[:, 0:256], in1=ysum[r][:], op=ALU.add), reads=[ybk, "ysum%d" % r], writes=["ysum%d" % r])
                S.op("act", lambda e, r=r: e.activation(out=Yg[r][:], in_=ysum[r][:], func=AF.Gelu), reads=["ysum%d" % r], writes=["Yg%d" % r])
                if CUT < 4:
                    continue
                pT = bank[2 + r][:].bitcast(BF16).rearrange("p (c n) -> p c n", n=128)
                for kb in range(2):
                    S.op("pe", lambda e, r=r, kb=kb, pT=pT: e.transpose(out=pT[:, kb, :], in_=Yg[r][:, kb * 128:(kb + 1) * 128], identity=idb[:]), reads=["Yg%d" % r, "identb"], writes=["sbk%d" % (2 + r)])
                for kb in range(2):
                    S.op("dve", lambda e, g=g, kb=kb, pT=pT: e.tensor_copy(out=ytm[:, kb, :, 16 * g:16 * g + 16], in_=pT[:, kb, :].rearrange("p (t c) -> p t c", c=16)),
                         reads=["sbk%d" % (2 + r)], writes=["ytm"])


def _s5_glu_out(C, es, D, idb, ytm, dbg):
    S = C.S
    sb = lambda name, shape, dt=F32: C.sb(es, name, shape, dt)
    gso = sb("gso", [128, 4]); bgl = sb("bgl", [128, 512])
    S.dma("sp", gso[:], D["g_ssm_out"], writes=["gso"])
    S.dma("sp", bgl[:], D["b_glu"].partition_broadcast(128), writes=["bgl"])
    with scope(C) as est:
        Wg = load_weight_bf16(C, es, est, "Wg", D["w_glu"], 4, 512)
    with scope(C) as est:
        Wo = load_weight_bf16(C, es, est, "Wos", D["w_out"][512:1024, :], 4, 1024, gcol=(gso, "gso"))
    yT = sb("yT", [128, 4, 128], BF16); zb = sb("zb", [128, 512]); ssm = sb("ssm", [128, 512]); junk = sb("junk3", [128, 512])
    st = sb("st3", [128, 2]); sn = sb("sn", [128, 512], BF16); snT = sb("snT", [128, 4, 128], BF16)
    ho = [sb("ho%d" % i, [128, 1024]) for i in range(2)]
    hsv = D["hs_d"].rearrange("(k t) d -> t k d", t=8)
    with scope(C) as esp:
        bank = [C.ps(esp, "gbk%d" % i, [128, 512]) for i in range(5)]
        it = 0
        for kb in range(2):
            for t in range(8):
                sl = it % 2; it += 1
                y = ytm[:, kb, t, :]
                pT = bank[0][:].bitcast(BF16).rearrange("p (c n) -> p c n", n=128)[:, 0:4, :]
                transpose_chunks(C, y, "ytm", 4, pT, "gbk0", yT[:], "yT", idb)
                for c in range(4):
                    S.op("pe", lambda e, c=c: e.matmul(bank[1][:], lhsT=yT[:, c, :], rhs=Wg[:, c, :], start=(c == 0), stop=(c == 3)), reads=["yT", "Wg"], writes=["gbk1"])
                S.op("dve", lambda e: e.tensor_tensor(out=zb[:], in0=bank[1][:], in1=bgl[:], op=ALU.add), reads=["gbk1", "bgl"], writes=["zb"])
                S.op("act", lambda e: e.activation(out=zb[:], in_=zb[:], func=AF.Sigmoid), reads=["zb"], writes=["zb"])
                S.op("pool", lambda e, y=y: e.tensor_tensor(out=ssm[:], in0=y, in1=zb[:], op=ALU.mult), reads=["ytm", "zb"], writes=["ssm"])
                if "ssm" in dbg:
                    S.dma("sp", dbg["ssm"].rearrange("(k t) d -> t k d", t=8)[t, kb * 128:(kb + 1) * 128, :], ssm[:], reads=["ssm"], writes=["o_ssm"])
                rms_rstd(C, ssm[:], "ssm", junk[:], "junk3", st[:, 0:1], st[:, 1:2], "st3", 512)
                S.op("dve", lambda e: e.tensor_scalar(out=sn[:], in0=ssm[:], scalar1=st[:, 1:2], scalar2=None, op0=ALU.mult), reads=["ssm", "st3"], writes=["sn"])
                pT2 = bank[2][:].bitcast(BF16).rearrange("p (c n) -> p c n", n=128)[:, 0:4, :]
                transpose_chunks(C, sn[:], "sn", 4, pT2, "gbk2", snT[:], "snT", idb)
                for cb in range(2):
                    for c in range(4):
                        S.op("pe", lambda e, c=c, cb=cb: e.matmul(bank[3 + cb][:], lhsT=snT[:, c, :], rhs=Wo[:, c, cb * 512:(cb + 1) * 512], start=(c == 0), stop=(c == 3)), reads=["snT", "Wos"], writes=["gbk%d" % (3 + cb)])
                    S.op("act", lambda e, cb=cb, sl=sl: e.copy(out=ho[sl][:, cb * 512:(cb + 1) * 512], in_=bank[3 + cb][:]), reads=["gbk%d" % (3 + cb)], writes=["ho%d" % sl])
                S.dma("sp", hsv[t, kb * 128:(kb + 1) * 128, :], ho[sl][:], reads=["ho%d" % sl], writes=["hs_d"])


def _attention(C, es, D, idb, dbg, prep=None):
    S = C.S
    sb = lambda name, shape, dt=F32: C.sb(es, name, shape, dt)
    kT = sb("kT", [128, 4, 2560], BF16); qT = sb("qT", [128, 4, 2048], BF16); V = sb("Vall", [128, 20, 520], BF16)
    qZ = sb("qZ", [128, 8, 2048], BF16)
    S.dma("sp", kT[:].rearrange("p c n -> p (c n)"), D["kT_d"], reads=["qkT_d"], writes=["kT"])
    S.dma("sp", qT[:].rearrange("p c n -> p (c n)"), D["qT_d"], reads=["qkT_d"], writes=["qT"])
    S.dma("sp", V[:], D["V_d"].rearrange("t p n -> p t n"), reads=["V_d"], writes=["Vall"])
    S.op("pool", lambda e: e.memset(qZ[:].rearrange("p h n -> p (h n)"), 0.0), writes=["qZ"])
    for h in range(8):
        rws = slice(64 * (h % 2), 64 * (h % 2) + 64)
        S.op("dve" if h % 2 else "act", (lambda e, h=h, rws=rws: e.tensor_copy(out=qZ[rws, h, :], in_=qT[rws, h // 2, :])) if h % 2 else (lambda e, h=h, rws=rws: e.copy(out=qZ[rws, h, :], in_=qT[rws, h // 2, :])),
             reads=["qT", "qZ"], writes=["qZ"])
    BT = sb("BT", [128, 8, 5, 128], BF16)
    gao = sb("gao", [128, 4])
    S.dma("sp", gao[:], D["g_att_out"], writes=["gao"])
    with scope(C) as est:
        stg = C.sb(est, "btstg", [128, 640])
        for h in range(8):
            S.dma("sp", stg[:], D["bias_t"][:, h * 640:(h + 1) * 640], writes=["btstg"])
            S.op("dve", lambda e, h=h: e.tensor_copy(out=BT[:, h, :, :].rearrange("p j q -> p (j q)"), in_=stg[:]), reads=["btstg"], writes=["BT"])
    with scope(C) as est:
        Wo = load_weight_bf16(C, es, est, "Woa", D["w_out"][0:512, :], 4, 1024, gcol=(gao, "gao"))
    PT = [sb("PT%d" % i, [128, 5, 128], BF16) for i in range(2)]
    rd = sb("rd", [128, 8]); att = sb("att", [128, 8, 64]); junk = sb("junk4", [128, 512]); st = sb("st4", [128, 2])
    an = sb("an", [128, 512], BF16); anT = sb("anT", [128, 4, 128], BF16)
    xo = [sb("xo%d" % i, [128, 1024]) for i in range(2)]; hsl = [sb("hsl%d" % i, [128, 1024]) for i in range(2)]
    h1t = [sb("h1t%d" % i, [128, 1024]) for i in range(2)]
    xv = D["x_ext"].rearrange("(n p) d -> n p d", p=128)
    hsv = D["hs_d"].rearrange("(n p) d -> n p d", p=128)
    h1v = D["h1_d"].rearrange("(n p) d -> n p d", p=128)
    with scope(C) as esp:
        bank = [C.ps(esp, "abk%d" % i, [128, 512]) for i in range(7)]
        for qt in range(16):
            sl = qt % 2
            drip(prep, 2)
            S.dma("sp", xo[sl][:], xv[48 + qt], writes=["xo%d" % sl])
            S.dma("sp", hsl[sl][:], hsv[qt], reads=["hs_d"], writes=["hsl%d" % sl])
            for h in range(8):
                hp = h // 2; rows = slice(64 * (h % 2), 64 * (h % 2) + 64); ps_ = h % 2
                bA = bank[2 * ps_]; bB = bank[2 * ps_ + 1]; bAk = "abk%d" % (2 * ps_); bBk = "abk%d" % (2 * ps_ + 1)
                for j in range(5):
                    o = bA[:, j * 128:(j + 1) * 128] if j < 4 else bB[:, 0:128]
                    ok = bAk if j < 4 else bBk
                    S.op("pe", lambda e, o=o, h=h, hp=hp, j=j, qt=qt: e.matmul(o, lhsT=kT[:, hp, (qt + j) * 128:(qt + j + 1) * 128], rhs=qZ[:, h, qt * 128:(qt + 1) * 128], start=True, stop=False),
                         reads=["kT", "qZ"], writes=[ok])
                    S.op("pe", lambda e, o=o, h=h, j=j: e.matmul(o, lhsT=idb[:], rhs=BT[:, h, j, :], start=False, stop=True), reads=["identb", "BT"], writes=[ok])
                S.op("act", lambda e, ps_=ps_, bA=bA: e.activation(out=PT[ps_][:, 0:4, :].rearrange("p j q -> p (j q)"), in_=bA[:], func=AF.Exp), reads=[bAk], writes=["PT%d" % ps_])
                S.op("act", lambda e, ps_=ps_, bB=bB: e.activation(out=PT[ps_][:, 4, :], in_=bB[:, 0:128], func=AF.Exp), reads=[bBk], writes=["PT%d" % ps_])
                ob = bank[4 + h // 4]; obk = "abk%d" % (4 + h // 4)
                for j in range(5):
                    S.op("pe", lambda e, ob=ob, h=h, j=j, ps_=ps_, qt=qt: e.matmul(ob[:, (h % 4) * 65:(h % 4) * 65 + 65], lhsT=PT[ps_][:, j, :], rhs=V[:, qt + j, h * 65:(h + 1) * 65], start=(j == 0), stop=(j == 4)),
                         reads=["PT%d" % ps_, "Vall"], writes=[obk])
            for hb in range(2):
                ov = bank[4 + hb][:, 0:260].rearrange("p (h d) -> p h d", d=65)
                S.op("dve", lambda e, hb=hb, ov=ov: e.reciprocal(out=rd[:, hb * 4:(hb + 1) * 4], in_=ov[:, :, 64]), reads=["abk%d" % (4 + hb)], writes=["rd%d" % hb])
                S.op("dve", lambda e, hb=hb, ov=ov: e.tensor_tensor(out=att[:, hb * 4:(hb + 1) * 4, :], in0=ov[:, :, 0:64], in1=bc(rd[:, hb * 4:(hb + 1) * 4].unsqueeze(2), [128, 4, 64]), op=ALU.mult),
                     reads=["abk%d" % (4 + hb), "rd%d" % hb], writes=["att%d" % hb])
            attf = att[:].rearrange("p h d -> p (h d)")
            if "att" in dbg:
                S.dma("sp", dbg["att"].rearrange("(n p) d -> n p d", p=128)[qt], attf, reads=["att0", "att1"], writes=["o_att"])
            S.op("act", lambda e: e.activation(out=junk[:], in_=attf, func=AF.Square, accum_out=st[:, 0:1]), reads=["att0", "att1"], writes=["st4_ss"])
            S.op("act", lambda e: e.activation(out=st[:, 1:2], in_=st[:, 0:1], func=AF.Sqrt, scale=1.0 / 512, bias=EPS), reads=["st4_ss"], writes=["st4_sq"])
            S.op("dve", lambda e: e.reciprocal(out=st[:, 1:2], in_=st[:, 1:2]), reads=["st4_sq"], writes=["st4"])
            S.op("dve", lambda e: e.tensor_scalar(out=an[:], in0=attf, scalar1=st[:, 1:2], scalar2=None, op0=ALU.mult), reads=["att0", "att1", "st4"], writes=["an"])
            pT = bank[6][:].bitcast(BF16).rearrange("p (c n) -> p c n", n=128)[:, 0:4, :]
            transpose_chunks(C, an[:], "an", 4, pT, "abk6", anT[:], "anT", idb)
            for cb in range(2):
                for c in range(4):
                    S.op("pe", lambda e, c=c, cb=cb: e.matmul(bank[cb][:], lhsT=anT[:, c, :], rhs=Wo[:, c, cb * 512:(cb + 1) * 512], start=(c == 0), stop=(c == 3)), reads=["anT", "Woa"], writes=["abk%d" % cb])
                S.op("dve", lambda e, cb=cb, sl=sl: e.tensor_tensor(out=h1t[sl][:, cb * 512:(cb + 1) * 512], in0=bank[cb][:], in1=xo[sl][:, cb * 512:(cb + 1) * 512], op=ALU.add),
                     reads=["abk%d" % cb, "xo%d" % sl], writes=["h1t%d" % sl])
            S.op("pool", lambda e, sl=sl: e.tensor_tensor(out=h1t[sl][:], in0=h1t[sl][:], in1=hsl[sl][:], op=ALU.add), reads=["h1t%d" % sl, "hsl%d" % sl], writes=["h1t%d" % sl])
            S.dma("sp", h1v[qt], h1t[sl][:], reads=["h1t%d" % sl], writes=["h1_d"])


def _col(g, n):
    return np.ascontiguousarray(np.asarray(g, np.float32).reshape(n, 128).T)


def host_shared(inp):
    f = lambda k: np.asarray(inp[k], np.float32)[0]
    sh = {}
    sh["g_mix"] = _col(f("norm_mix_g"), 8)
    sh["w_in"] = np.ascontiguousarray(f("w_in"))
    sh["att_q_g"] = f("att_q_g").reshape(1, 64)
    sh["att_k_g"] = f("att_k_g").reshape(1, 64)
    rb = f("rel_bias")
    p = np.arange(128); j = np.arange(5); q = np.arange(128)
    kidx = j[:, None] * 128 + p[None, :]
    kc = kidx // 64; ki = kidx % 64
    qc = q // 64; qi = q % 64
    jb = kc[:, :, None] - qc[None, None, :]
    allowed = (jb >= 0) & (jb <= 8)
    kj = jb * 64 + ki[:, :, None]
    dist = 512 + qi[None, None, :] - kj
    bucket = np.clip(np.clip(dist, -63, 128) + 63, 0, 191)
    bt = np.where(allowed[None], rb[:, bucket], np.float32(NEG))
    sh["bias_t"] = np.ascontiguousarray(bt.transpose(2, 0, 1, 3).reshape(128, 8 * 5 * 128).astype(np.float32))
    dup = lambda a: np.ascontiguousarray(np.concatenate([a, a], 0).astype(np.float32))
    sh["s5_lr"] = dup(f("ssm_lam_re").T)
    sh["s5_li"] = dup(f("ssm_lam_im").T)
    sh["s5_ls"] = np.ascontiguousarray(np.broadcast_to(f("ssm_log_step")[None, :], (128, 32)).astype(np.float32))
    sg = np.ones((128, 2), np.float32); sg[:64, 0] = -1.0; sg[64:, 1] = -1.0
    sh["s5_sg"] = sg
    sh["s5_nv"] = np.ascontiguousarray(np.broadcast_to(np.arange(-7, 16, dtype=np.float32)[None, :], (128, 23)))
    bre = f("ssm_b_re").transpose(1, 0, 2).reshape(64, 512); bim = f("ssm_b_im").transpose(1, 0, 2).reshape(64, 512)
    cre = f("ssm_c_re").transpose(2, 0, 1).reshape(64, 512); cim = f("ssm_c_im").transpose(2, 0, 1).reshape(64, 512)
    sh["s5_p1b"] = np.ascontiguousarray(np.concatenate([bre, bim], 0)); sh["s5_p2b"] = np.ascontiguousarray(np.concatenate([bim, bre], 0))
    sh["s5_p1c"] = np.ascontiguousarray(np.concatenate([cre, cim], 0)); sh["s5_p2c"] = np.ascontiguousarray(np.concatenate([cim, cre], 0))
    dd = np.zeros((2, 4, 16, 4, 4, 16), np.float32)
    dsk = f("ssm_d")
    for g in range(32):
        chc = g // 8; hb = (g % 8) // 4; j4 = g % 4
        for c in range(16):
            dd[hb, j4, c, chc, j4, c] = dsk[g, c]
    sh["s5_dd"] = dd.reshape(128, 256)
    sh["g_ssm_out"] = _col(f("ssm_out_g"), 4)
    sh["g_att_out"] = _col(f("att_out_g"), 4)
    sh["b_glu"] = f("ssm_b_glu").reshape(1, 512)
    sh["w_glu"] = np.ascontiguousarray(f("ssm_w_glu"))
    sh["w_out"] = np.ascontiguousarray(f("w_out"))
    return sh


def host_core(inp, c):
    b, seg = c // 4, c % 4
    x = np.asarray(inp["x"], np.float32)
    xe = np.zeros((8192, 1024), np.float32)
    n = (seg + 1) * 2048
    xe[8192 - n:] = x[b, :n]
    hv = np.full((512,), 1.0 if seg > 0 else 0.0, np.float32)
    return {"x_ext": xe, "hvalid": np.ascontiguousarray(hv.reshape(4, 128).T)}


IN_SHAPES = {
    "x_ext": [8192, 1024], "hvalid": [128, 4], "g_mix": [128, 8], "w_in": [1024, 2048], "att_q_g": [1, 64], "att_k_g": [1, 64],
    "bias_t": [128, 5120], "s5_lr": [128, 32], "s5_li": [128, 32], "s5_ls": [128, 32], "s5_sg": [128, 2], "s5_nv": [128, 23],
    "s5_p1b": [128, 512], "s5_p2b": [128, 512], "s5_p1c": [128, 512], "s5_p2c": [128, 512], "s5_dd": [128, 256],
    "g_ssm_out": [128, 4], "g_att_out": [128, 4], "b_glu": [1, 512], "w_glu": [512, 512], "w_out": [1024, 1024],
}
IN_SHAPES_MEM = {"mem": [256, 1024], "g_mem": [128, 8], "g_memkv": [128, 8], "mem_q_g": [1, 256], "mem_k_g": [1, 256],
                 "w_mem_q": [1024, 1024], "w_mem_k": [1024, 1024], "w_mem_v": [1024, 1024], "w_mem_o": [1024, 1024]}
IN_SHAPES_PEER = {"g_peer": [1, 1024], "w_peer_q": [1024, 2048], "keysT": [128, 2048], "peer_uT": [1024, 16384], "peer_v": [16384, 1024]}
SCRATCH = {"kT_d": ([128, 4 * 2560], BF16), "qT_d": ([128, 4 * 2048], BF16), "V_d": ([20, 128, 520], BF16),
           "hs_d": ([2048, 1024], F32), "Mz_d": ([128, 16 * 8 * 128], BF16), "CA_d": ([128, 2, 4096], BF16)}
SCRATCH_PEER = {"uT_b": ([1024, 16384], BF16), "v_b": ([16384, 1024], BF16), "xnT_d": ([16, 128, 1024], BF16),
                "sc_d": ([16, 128, 2048], F32), "tau_d": ([16, 128, 8], F32)}


def host_shared_rest(inp):
    f = lambda k: np.asarray(inp[k], np.float32)[0]
    sh = {}
    sh["g_mem"] = _col(f("norm_mem_g"), 8); sh["g_memkv"] = _col(f("norm_memkv_g"), 8)
    sh["mem_q_g"] = f("mem_q_g").reshape(1, 256); sh["mem_k_g"] = f("mem_k_g").reshape(1, 256)
    for k in ("w_mem_q", "w_mem_k", "w_mem_v", "w_mem_o", "w_peer_q"):
        sh[k] = np.ascontiguousarray(f(k))
    sh["g_peer"] = f("norm_peer_g").reshape(1, 1024)
    sh["keysT"] = np.ascontiguousarray(f("peer_keys").transpose(3, 0, 1, 2).reshape(128, 2048))
    sh["peer_uT"] = np.ascontiguousarray(f("peer_u").T)
    sh["peer_v"] = np.ascontiguousarray(f("peer_v"))
    return sh


def build_program(stages=("mixer", "mem", "peer"), dbg_specs=None, upto=9):
    nc = bass.Bass("TRN2", target_bir_lowering=False)
    D = {}
    shapes = {}
    if "mixer" in stages:
        shapes.update(IN_SHAPES)
    if "mem" in stages:
        shapes.update(IN_SHAPES_MEM)
    if "peer" in stages:
        shapes.update(IN_SHAPES_PEER)
    for k, shp in shapes.items():
        D[k] = nc.dram_tensor(k, shp, F32, kind="ExternalInput").ap()
    scr = {}
    if "mixer" in stages:
        scr.update(SCRATCH)
    if "peer" in stages:
        scr.update(SCRATCH_PEER)
    for k, (shp, dt) in scr.items():
        D[k] = nc.dram_tensor(k, shp, dt).ap()
    chain = ["h1_d", "h2_d", "out"]
    first = {"mixer": None, "mem": "h1_d", "peer": "h2_d"}[stages[0]]
    last = {"mixer": "h1_d", "mem": "h2_d", "peer": "out"}[stages[-1]]
    for k in chain:
        if k == first:
            D[k] = nc.dram_tensor(k, [2048, 1024], F32, kind="ExternalInput").ap()
        elif k == last:
            D[k] = nc.dram_tensor(k, [2048, 1024], F32, kind="ExternalOutput").ap()
        else:
            D[k] = nc.dram_tensor(k, [2048, 1024], F32).ap()
    dbg = {}
    for k, shp in (dbg_specs or {}).items():
        dbg[k] = nc.dram_tensor("dbg_" + k, shp, F32, kind="ExternalOutput").ap()
    with ExitStack() as es:
        S = Sched(nc, es)
        C = Ctx(nc, S)
        prep = stage_peer_prep(C, D) if "peer" in stages else None
        if "mixer" in stages:
            stage_mixer(C, D, dbg, upto, prep=prep)
        if "mem" in stages:
            stage_mem(C, D, dbg, prep=prep)
        drip(prep, 1000)
        if "peer" in stages:
            stage_peer(C, D, dbg)
        S.barrier()
        S.emit()
    return nc, S


def _headnorm(C, src_ap, skey, nh, hd, sqt, sqk, stat, stk, gt, gkey, dst_ap, dkey):
    S = C.S
    sv = src_ap.rearrange("p (h d) -> p h d", d=hd)
    S.op("pool", lambda e: e.tensor_tensor(out=sqt, in0=src_ap, in1=src_ap, op=ALU.mult), reads=[skey], writes=[sqk])
    S.op("dve", lambda e: e.tensor_reduce(out=stat[:, 0:nh], in_=sqt.rearrange("p (h d) -> p h d", d=hd), axis=AX.X, op=ALU.add), reads=[sqk], writes=[stk + "a"])
    S.op("act", lambda e: e.activation(out=stat[:, nh:2 * nh], in_=stat[:, 0:nh], func=AF.Sqrt, scale=1.0 / hd, bias=EPS), reads=[stk + "a"], writes=[stk + "b"])
    S.op("dve", lambda e: e.reciprocal(out=stat[:, nh:2 * nh], in_=stat[:, nh:2 * nh]), reads=[stk + "b"], writes=[stk])
    S.op("dve", lambda e: e.tensor_tensor(out=sv, in0=sv, in1=bc(stat[:, nh:2 * nh].unsqueeze(2), [128, nh, hd]), op=ALU.mult), reads=[skey, stk], writes=[skey])
    S.op("pool", lambda e: e.tensor_tensor(out=dst_ap.rearrange("p (h d) -> p h d", d=hd), in0=sv, in1=bc(gt.unsqueeze(1), [128, nh, hd]), op=ALU.mult), reads=[skey, gkey], writes=[dkey])


def stage_mem(C, D, dbg=None, prep=None):
    S = C.S
    dbg = dbg or {}
    with scope(C) as es:
        sb = lambda name, shape, dt=F32: C.sb(es, name, shape, dt)
        idf, idb = make_ident(C, es, "ident")
        gm = sb("gm", [128, 8]); gkv = sb("gkv", [128, 8]); gq = sb("mgq", [128, 256]); gk = sb("mgk", [128, 256])
        S.dma("sp", gm[:], D["g_mem"], writes=["gm"]); S.dma("sp", gkv[:], D["g_memkv"], writes=["gkv"])
        S.dma("sp", gq[:], D["mem_q_g"].partition_broadcast(128), writes=["mgq"]); S.dma("sp", gk[:], D["mem_k_g"].partition_broadcast(128), writes=["mgk"])
        S.op("dve", lambda e: e.tensor_scalar(out=gq[:], in0=gq[:], scalar1=1.0 / 16, scalar2=None, op0=ALU.mult), reads=["mgq"], writes=["mgq"])
        kTm = sb("kTm", [128, 8, 256], BF16); Vm = sb("Vm", [128, 2, 4, 257], BF16)
        xt = [sb("mxt%d" % i, [128, 1024]) for i in range(2)]; st = sb("mst", [128, 2]); xn = sb("mxn", [128, 1024], BF16)
        xnT = sb("mxnT", [128, 8, 128], BF16); qf = sb("mqf", [128, 1024]); sq = sb("msq", [128, 1024]); qst = sb("mqst", [128, 8])
        qn = sb("mqn", [128, 1024], BF16); mjunk = sb("mjunk", [128, 1024], BF16)
        with scope(C) as esk:
            with scope(C) as est:
                Wk = load_weight_bf16(C, esk, est, "Wmk", D["w_mem_k"], 8, 1024, gcol=(gkv, "gkv"))
            with scope(C) as est:
                Wv = load_weight_bf16(C, esk, est, "Wmv", D["w_mem_v"], 8, 1024, gcol=(gkv, "gkv"))
            with scope(C) as esp:
                bank = [C.ps(esp, "mkb%d" % i, [128, 512]) for i in range(6)]
                mv = D["mem"].rearrange("(n p) d -> n p d", p=128)
                for mt in range(2):
                    x_ = xt[mt]; xk = "mxt%d" % mt
                    S.dma("sp", x_[:], mv[mt], writes=[xk])
                    rms_rstd(C, x_[:], xk, mjunk[:], "mjunk", st[:, 0:1], st[:, 1:2], "mst", 1024)
                    S.op("dve", lambda e, x_=x_: e.tensor_scalar(out=xn[:], in0=x_[:], scalar1=st[:, 1:2], scalar2=None, op0=ALU.mult), reads=[xk, "mst"], writes=["mxn"])
                    pb = bank[0][:].bitcast(BF16).rearrange("p (c n) -> p c n", n=128)
                    transpose_chunks(C, xn[:], "mxn", 8, pb, "mkb0", xnT[:], "mxnT", idb)
                    for (W, wk, b0) in ((Wk, "Wmk", 1), (Wv, "Wmv", 3)):
                        for cb in range(2):
                            for dc in range(8):
                                S.op("pe", lambda e, W=W, cb=cb, dc=dc, b0=b0: e.matmul(bank[b0 + cb][:], lhsT=xnT[:, dc, :], rhs=W[:, dc, cb * 512:(cb + 1) * 512], start=(dc == 0), stop=(dc == 7)),
                                     reads=["mxnT", wk], writes=["mkb%d" % (b0 + cb)])
                    for cb in range(2):
                        S.op("act", lambda e, cb=cb: e.copy(out=qf[:, cb * 512:(cb + 1) * 512], in_=bank[1 + cb][:]), reads=["mkb%d" % (1 + cb)], writes=["mqf"])
                        S.op("dve", lambda e, cb=cb, mt=mt: e.tensor_copy(out=Vm[:, mt, 2 * cb:2 * cb + 2, 0:256], in_=bank[3 + cb][:].rearrange("p (h d) -> p h d", d=256)), reads=["mkb%d" % (3 + cb)], writes=["Vm"])
                    S.op("pool", lambda e, mt=mt: e.memset(Vm[:, mt, :, 256], 1.0), reads=[], writes=["Vm"])
                    _headnorm(C, qf[:], "mqf", 4, 256, sq[:], "msq", qst, "mqst", gk[:], "mgk", qn[:], "mqn")
                    pb2 = bank[5][:].bitcast(BF16).rearrange("p (c n) -> p c n", n=128)
                    transpose_chunks(C, qn[:], "mqn", 8, pb2, "mkb5", kTm[:, :, mt * 128:(mt + 1) * 128], "kTm", idb)
        with scope(C) as est:
            Wq = load_weight_bf16(C, es, est, "Wmq", D["w_mem_q"], 8, 1024, gcol=(gm, "gm"))
        with scope(C) as est:
            Wo = load_weight_bf16(C, es, est, "Wmo", D["w_mem_o"], 8, 1024)
        qT = sb("mqT", [128, 8, 128], BF16); PT = [sb("mPT%d" % i, [128, 2, 128], BF16) for i in range(2)]
        rd = sb("mrd", [128, 4]); ob = sb("mob", [128, 1024], BF16); oT = sb("moT", [128, 8, 128], BF16)
        h2t = [sb("h2t%d" % i, [128, 1024]) for i in range(2)]
        hv = D["h1_d"].rearrange("(n p) d -> n p d", p=128); ov = D["h2_d"].rearrange("(n p) d -> n p d", p=128)
        with scope(C) as esp:
            bank = [C.ps(esp, "mb%d" % i, [128, 512]) for i in range(8)]
            for tt in range(16):
                sl = tt % 2
                drip(prep, 1)
                x_ = xt[sl]; xk = "mxt%d" % sl
                S.dma("sp", x_[:], hv[tt], reads=["h1_d"], writes=[xk])
                rms_rstd(C, x_[:], xk, mjunk[:], "mjunk", st[:, 0:1], st[:, 1:2], "mst", 1024)
                S.op("dve", lambda e, x_=x_: e.tensor_scalar(out=xn[:], in0=x_[:], scalar1=st[:, 1:2], scalar2=None, op0=ALU.mult), reads=[xk, "mst"], writes=["mxn"])
                pb = bank[0][:].bitcast(BF16).rearrange("p (c n) -> p c n", n=128)
                transpose_chunks(C, xn[:], "mxn", 8, pb, "mb0", xnT[:], "mxnT", idb)
                for cb in range(2):
                    for dc in range(8):
                        S.op("pe", lambda e, cb=cb, dc=dc: e.matmul(bank[1 + cb][:], lhsT=xnT[:, dc, :], rhs=Wq[:, dc, cb * 512:(cb + 1) * 512], start=(dc == 0), stop=(dc == 7)),
                             reads=["mxnT", "Wmq"], writes=["mb%d" % (1 + cb)])
                    S.op("act", lambda e, cb=cb: e.copy(out=qf[:, cb * 512:(cb + 1) * 512], in_=bank[1 + cb][:]), reads=["mb%d" % (1 + cb)], writes=["mqf"])
                _headnorm(C, qf[:], "mqf", 4, 256, sq[:], "msq", qst, "mqst", gq[:], "mgq", qn[:], "mqn")
                pb2 = bank[3][:].bitcast(BF16).rearrange("p (c n) -> p c n", n=128)
                transpose_chunks(C, qn[:], "mqn", 8, pb2, "mb3", qT[:], "mqT", idb)
                for h in range(4):
                    ps_ = h % 2
                    sbk = bank[4 + ps_]; sbkk = "mb%d" % (4 + ps_)
                    for mt in range(2):
                        for dh in range(2):
                            S.op("pe", lambda e, h=h, mt=mt, dh=dh, sbk=sbk: e.matmul(sbk[:, mt * 128:(mt + 1) * 128], lhsT=kTm[:, 2 * h + dh, mt * 128:(mt + 1) * 128], rhs=qT[:, 2 * h + dh, :], start=(dh == 0), stop=(dh == 1)),
                                 reads=["kTm", "mqT"], writes=[sbkk])
                    S.op("act", lambda e, ps_=ps_, sbk=sbk: e.activation(out=PT[ps_][:].rearrange("p m q -> p (m q)"), in_=sbk[:, 0:256], func=AF.Exp, bias=-8.0), reads=[sbkk], writes=["mPT%d" % ps_])
                    obk = bank[6 + ps_]; obkk = "mb%d" % (6 + ps_)
                    for mt in range(2):
                        S.op("pe", lambda e, h=h, mt=mt, ps_=ps_, obk=obk: e.matmul(obk[:, 0:257], lhsT=PT[ps_][:, mt, :], rhs=Vm[:, mt, h, :], start=(mt == 0), stop=(mt == 1)), reads=["mPT%d" % ps_, "Vm"], writes=[obkk])
                    S.op("dve", lambda e, h=h, obk=obk: e.reciprocal(out=rd[:, h:h + 1], in_=obk[:, 256:257]), reads=[obkk], writes=["mrd%d" % h])
                    S.op("dve", lambda e, h=h, obk=obk: e.tensor_scalar(out=ob[:, h * 256:(h + 1) * 256], in0=obk[:, 0:256], scalar1=rd[:, h:h + 1], scalar2=None, op0=ALU.mult), reads=[obkk, "mrd%d" % h], writes=["mob"])
                pb3 = bank[0][:].bitcast(BF16).rearrange("p (c n) -> p c n", n=128)
                transpose_chunks(C, ob[:], "mob", 8, pb3, "mb0", oT[:], "moT", idb)
                for cb in range(2):
                    for dc in range(8):
                        S.op("pe", lambda e, cb=cb, dc=dc: e.matmul(bank[1 + cb][:], lhsT=oT[:, dc, :], rhs=Wo[:, dc, cb * 512:(cb + 1) * 512], start=(dc == 0), stop=(dc == 7)),
                             reads=["moT", "Wmo"], writes=["mb%d" % (1 + cb)])
                    S.op("dve", lambda e, cb=cb, sl=sl, x_=x_: e.tensor_tensor(out=h2t[sl][:, cb * 512:(cb + 1) * 512], in0=bank[1 + cb][:], in1=x_[:, cb * 512:(cb + 1) * 512], op=ALU.add),
                         reads=["mb%d" % (1 + cb), xk], writes=["h2t%d" % sl])
                S.dma("sp", ov[tt], h2t[sl][:], reads=["h2t%d" % sl], writes=["h2_d"])


def stage_peer_prep(C, D):
    S = C.S
    uv = D["peer_uT"].rearrange("(c p) (a e) -> c p a e", p=128, e=2048)
    ub = D["uT_b"].rearrange("(c p) (a e) -> c p a e", p=128, e=2048)
    vv = D["peer_v"].rearrange("(c p) d -> c p d", p=512)
    vb = D["v_b"].rearrange("(c p) d -> c p d", p=512)

    def gen():
        for c in range(8):
            S.dma("pool", ub[c], uv[c], writes=["uT_b"])
            yield
        for c in range(32):
            S.dma("pool", vb[c], vv[c], writes=["v_b"])
            yield
    return gen()


def drip(g, n):
    if g is None:
        return
    for _ in range(n):
        try:
            next(g)
        except StopIteration:
            return


def _top16(C, src, skey, work, wkey, dst, dkey):
    S = C.S
    S.op("dve", lambda e: e.max(out=dst[:, 0:8], in_=src), reads=[skey], writes=[dkey])
    S.op("dve", lambda e: e.match_replace(out=work, in_to_replace=dst[:, 0:8], in_values=src, imm_value=-1e30), reads=[skey, dkey], writes=[wkey])
    S.op("dve", lambda e: e.max(out=dst[:, 8:16], in_=work), reads=[wkey], writes=[dkey])


def stage_peer(C, D, dbg=None):
    S = C.S
    dbg = dbg or {}
    hv = D["h2_d"].rearrange("(n p) d -> n p d", p=128)
    with scope(C) as es:
        sb = lambda name, shape, dt=F32: C.sb(es, name, shape, dt)
        idf, idb = make_ident(C, es, "ident")
        gp = sb("gpb", [128, 1024])
        S.dma("sp", gp[:], D["g_peer"].partition_broadcast(128), writes=["gpb"])
        with scope(C) as est:
            Wq = load_weight_bf16(C, es, est, "Wpq", D["w_peer_q"], 8, 2048)
        keyT = sb("keyT", [128, 16, 128], BF16)
        with scope(C) as est:
            kst = C.sb(est, "kst", [128, 2048])
            S.dma("sp", kst[:], D["keysT"], writes=["kst"])
            S.op("dve", lambda e: e.tensor_copy(out=keyT[:].rearrange("p a n -> p (a n)"), in_=kst[:]), reads=["kst"], writes=["keyT"])
        NS = 3
        xt = [sb("pxt%d" % i, [128, 1024]) for i in range(NS)]; st_ = [sb("pst%d" % i, [128, 2]) for i in range(NS)]; junk = sb("pjunk", [128, 1024], BF16)
        xn_ = [sb("pxn%d" % i, [128, 1024], BF16) for i in range(NS)]; xnT = [sb("pxnT%d" % i, [128, 8, 128], BF16) for i in range(NS)]
        qb_ = [sb("pqb%d" % i, [128, 2048], BF16) for i in range(NS)]; qTp_ = [sb("pqT%d" % i, [128, 16, 128], BF16) for i in range(NS)]
        sc = [sb("psc%d" % i, [128, 16, 128]) for i in range(NS)]; work_ = [sb("pwork%d" % i, [128, 256]) for i in range(NS)]
        sv_ = [sb("psv%d" % i, [128, 16, 16]) for i in range(NS)]; cand_ = [sb("pcand%d" % i, [128, 8, 256]) for i in range(NS)]
        cex_ = [sb("pcex%d" % i, [128, 8, 256]) for i in range(NS)]; ctop_ = [sb("pctop%d" % i, [128, 8, 16]) for i in range(NS)]
        Z_ = [sb("pZ%d" % i, [128, 8]) for i in range(NS)]; off_ = [sb("poff%d" % i, [128, 8]) for i in range(NS)]; tau = [sb("ptau%d" % i, [128, 8]) for i in range(NS)]
        cjunk_ = [sb("pcj%d" % i, [128, 256]) for i in range(NS)]; offs_ = [sb("poffs%d" % i, [128, 16]) for i in range(NS)]
        xTd = D["xnT_d"].rearrange("n p (c t) -> n p c t", t=128)
        with scope(C) as esp:
            bank = [C.ps(esp, "pab%d" % i, [128, 512]) for i in range(6)]

            def tile(tt):
                sl = tt % NS
                K = lambda nm: "%s%d" % (nm, sl)
                x_ = xt[sl]; xk = K("pxt"); st = st_[sl]; xn = xn_[sl]; qb = qb_[sl]; qTp = qTp_[sl]; work = work_[sl]
                sv = sv_[sl]; cand = cand_[sl]; cex = cex_[sl]; ctop = ctop_[sl]; Z = Z_[sl]; off = off_[sl]; cjunk = cjunk_[sl]; offs = offs_[sl]
                S.dma("sp", x_[:], hv[tt], reads=["h2_d"], writes=[xk])
                rms_rstd(C, x_[:], xk, junk[:], "pjunk", st[:, 0:1], st[:, 1:2], K("pst"), 1024)
                S.op("dve", lambda e: e.scalar_tensor_tensor(out=xn[:], in0=x_[:], scalar=st[:, 1:2], in1=gp[:], op0=ALU.mult, op1=ALU.mult), reads=[xk, K("pst"), "gpb"], writes=[K("pxn")])
                pb = bank[0][:].bitcast(BF16).rearrange("p (c n) -> p c n", n=128)
                transpose_chunks(C, xn[:], K("pxn"), 8, pb, "pab0", xnT[sl][:], K("pxnT"), idb)
                S.dma("sp", xTd[tt], xnT[sl][:], reads=[K("pxnT")], writes=["xnT_d"])
                yield
                for cb in range(4):
                    for dc in range(8):
                        S.op("pe", lambda e, cb=cb, dc=dc: e.matmul(bank[1 + cb][:], lhsT=xnT[sl][:, dc, :], rhs=Wq[:, dc, cb * 512:(cb + 1) * 512], start=(dc == 0), stop=(dc == 7)),
                             reads=[K("pxnT"), "Wpq"], writes=["pab%d" % (1 + cb)])
                    S.op("act", lambda e, cb=cb: e.copy(out=qb[:, cb * 512:(cb + 1) * 512], in_=bank[1 + cb][:]), reads=["pab%d" % (1 + cb)], writes=[K("pqb")])
                for half in range(2):
                    pbq = bank[5][:].bitcast(BF16).rearrange("p (c n) -> p c n", n=128)
                    transpose_chunks(C, qb[:, half * 1024:(half + 1) * 1024], K("pqb"), 8, pbq, "pab5", qTp[:, half * 8:(half + 1) * 8, :], K("pqT"), idb)
                for hh in range(16):
                    S.op("pe", lambda e, hh=hh: e.matmul(bank[1 + hh // 4][:, (hh % 4) * 128:(hh % 4 + 1) * 128], lhsT=qTp[:, hh, :], rhs=keyT[:, hh, :], start=True, stop=True),
                         reads=[K("pqT"), "keyT"], writes=["pab%d" % (1 + hh // 4)])
                scs = sc[sl]; sck = K("psc")
                for cb in range(4):
                    S.op("act", lambda e, cb=cb: e.copy(out=scs[:, cb * 4:(cb + 1) * 4, :].rearrange("p a n -> p (a n)"), in_=bank[1 + cb][:]), reads=["pab%d" % (1 + cb)], writes=[sck])
                yield
                for hh in range(16):
                    _top16(C, scs[:, hh, :], sck, work[:, 0:128], K("pwork"), sv[:, hh, :], K("psv"))
                    yield
                svv = sv[:].rearrange("p (h s) k -> p h s k", s=2)
                for h in range(8):
                    S.op("dve", lambda e, h=h: e.tensor_tensor(out=cand[:, h, :].rearrange("p (a b) -> p a b", b=16), in0=bc(svv[:, h, 0, :].unsqueeze(2), [128, 16, 16]),
                                                            in1=bc(svv[:, h, 1, :].unsqueeze(1), [128, 16, 16]), op=ALU.add), reads=[K("psv")], writes=[K("pcand") + "_%d" % h])
                    yield
                ck = [K("pcand") + "_%d" % h for h in range(8)]
                for h in range(8):
                    _top16(C, cand[:, h, :], ck[h], work[:], K("pwork"), ctop[:, h, :], K("pctop"))
                    yield
                S.op("dve", lambda e: e.tensor_tensor(out=cand[:], in0=cand[:], in1=bc(ctop[:, :, 0:1], [128, 8, 256]), op=ALU.subtract), reads=ck + [K("pctop")], writes=ck)
                S.op("act", lambda e: e.activation(out=cex[:].rearrange("p h n -> p (h n)"), in_=cand[:].rearrange("p h n -> p (h n)"), func=AF.Exp), reads=ck, writes=[K("pcex")])
                yield
                S.op("dve", lambda e: e.tensor_tensor(out=tau[sl][:], in0=ctop[:, :, 15], in1=ctop[:, :, 0], op=ALU.subtract), reads=[K("pctop")], writes=[K("ptau")])
                yield
                S.op("dve", lambda e: e.tensor_scalar(out=tau[sl][:], in0=tau[sl][:], scalar1=-1e-5, scalar2=None, op0=ALU.add), reads=[K("ptau")], writes=[K("ptau")])
                yield
                for h in range(8):
                    S.op("dve", lambda e, h=h: e.scalar_tensor_tensor(out=cjunk[:], in0=cand[:, h, :], scalar=tau[sl][:, h:h + 1], in1=cex[:, h, :], op0=ALU.is_ge, op1=ALU.mult, accum_out=Z[:, h:h + 1]),
                         reads=ck + [K("pcex"), K("ptau")], writes=[K("pZ") + "_%d" % h])
                    yield
                S.op("act", lambda e: e.activation(out=off[:], in_=Z[:], func=AF.Ln), reads=[K("pZ") + "_%d" % h for h in range(8)], writes=[K("poff")])
                yield
                S.op("dve", lambda e: e.tensor_tensor(out=tau[sl][:], in0=tau[sl][:], in1=off[:], op=ALU.subtract), reads=[K("ptau"), K("poff")], writes=[K("ptau")])
                S.op("act", lambda e: e.activation(out=tau[sl][:], in_=tau[sl][:], func=AF.Exp), reads=[K("ptau")], writes=[K("ptau")])
                yield
                S.op("dve", lambda e: e.tensor_scalar(out=tau[sl][:], in0=tau[sl][:], scalar1=0.99997, scalar2=None, op0=ALU.mult), reads=[K("ptau")], writes=[K("ptau")])
                offv = offs[:].rearrange("p (h s) -> p h s", s=2)
                S.op("dve", lambda e: e.tensor_copy(out=offv[:, :, 0], in_=svv[:, :, 0, 0]), reads=[K("psv")], writes=[K("poffs") + "a"])
                yield
                S.op("dve", lambda e: e.tensor_tensor(out=offv[:, :, 1], in0=svv[:, :, 1, 0], in1=off[:], op=ALU.add), reads=[K("psv"), K("poff")], writes=[K("poffs") + "b"])
                yield
                S.op("dve", lambda e: e.tensor_tensor(out=scs[:], in0=scs[:], in1=bc(offs[:].unsqueeze(2), [128, 16, 128]), op=ALU.subtract), reads=[sck, K("poffs") + "a", K("poffs") + "b"], writes=[sck])
                S.op("act", lambda e: e.activation(out=scs[:].rearrange("p a n -> p (a n)"), in_=scs[:].rearrange("p a n -> p (a n)"), func=AF.Exp), reads=[sck], writes=[sck])
                S.dma("sp", D["sc_d"][tt], scs[:].rearrange("p a n -> p (a n)"), reads=[sck], writes=["sc_d"])
                S.dma("sp", D["tau_d"][tt], tau[sl][:], reads=[K("ptau")], writes=["tau_d"])

            from itertools import zip_longest
            for t0 in range(0, 16, NS):
                for _ in zip_longest(*[tile(t0 + i) for i in range(NS) if t0 + i < 16]):
                    pass
    with scope(C) as es:
        sb = lambda name, shape, dt=F32: C.sb(es, name, shape, dt)
        idf, idb = make_ident(C, es, "ident")
        xnT = sb("bxnT", [128, 4, 1024], BF16); sc = sb("bsc", [128, 4, 2048]); kap = sb("btau", [128, 4, 8])
        UT = [sb("UT%d" % i, [128, 8, 1024], BF16) for i in range(2)]; Vb = [sb("Vb%d" % i, [128, 8, 1024], BF16) for i in range(2)]
        acc = sb("pacc", [128, 4, 1024])
        NP = 12
        Pt = [sb("pP%d" % i, [128, 512]) for i in range(NP)]; Wh = [[sb("pWh%d_%d" % (i, h), [128, 512], BF16) for h in range(8)] for i in range(2)]
        G = [sb("pG%d" % i, [128, 512], BF16) for i in range(3)]; WA = [sb("pWA%d" % i, [128, 512], BF16) for i in range(2)]
        WAT = [sb("pWAT%d" % i, [128, 4, 128], BF16) for i in range(2)]
        h2t = [sb("ph2t%d" % i, [128, 1024]) for i in range(2)]
        uTv = D["uT_b"].rearrange("(c p) (b e) -> b p c e", p=128, e=1024)
        vbv = D["v_b"].rearrange("(b c p) d -> b p c d", p=128, c=8)
        ov = D["out"].rearrange("(n p) d -> n p d", p=128)
        with scope(C) as esp:
            bank = [C.ps(esp, "pbb%d" % i, [128, 512]) for i in range(7)]
            state = {"it": 0}

            def stage1a(u, tg, eb, sub, tt, es_):
                ub = u % 2
                xT = xnT[:, tt, :].rearrange("p (c t) -> p c t", t=128)
                scv = sc[:, tt, :].rearrange("p (h s n) -> p h s n", s=2, n=128)
                i0 = eb * 8 + sub * 4
                for dc in range(8):
                    S.op("pe", lambda e, dc=dc, xT=xT: e.matmul(bank[ub][:], lhsT=xT[:, dc, :], rhs=UT[es_][:, dc, sub * 512:(sub + 1) * 512], start=(dc == 0), stop=(dc == 7)),
                         reads=["bxnT", "UT%d" % es_], writes=["pbb%d" % ub])
                for h in range(8):
                    hs = state["it"] % NP; state["it"] += 1
                    if h >= 5:
                        S.op("pool", lambda e, h=h, hs=hs, scv=scv: e.tensor_tensor(out=Pt[hs][:].rearrange("p (i j) -> p i j", j=128), in0=bc(scv[:, h, 0, i0:i0 + 4].unsqueeze(2), [128, 4, 128]),
                                                                             in1=bc(scv[:, h, 1, :].unsqueeze(1), [128, 4, 128]), op=ALU.mult), reads=["bsc"], writes=["pP%d_%d" % (hs, il) for il in range(4)])
                    else:
                        for il in range(4):
                            S.op("act", lambda e, h=h, hs=hs, scv=scv, il=il: e.activation(out=Pt[hs][:, il * 128:(il + 1) * 128], in_=scv[:, h, 1, :], func=AF.Copy, scale=scv[:, h, 0, i0 + il:i0 + il + 1]),
                                 reads=["bsc"], writes=["pP%d_%d" % (hs, il)])
                    S.op("dve", lambda e, h=h, hs=hs: e.scalar_tensor_tensor(out=Wh[ub][h][:], in0=Pt[hs][:], scalar=kap[:, tt, h:h + 1], in1=Pt[hs][:], op0=ALU.is_ge, op1=ALU.mult),
                         reads=["pP%d_%d" % (hs, il) for il in range(4)] + ["btau"], writes=["pWh%d_%d" % (ub, h)])

            def st_gelu(u, tg, eb, sub, tt, es_):
                ub = u % 2; gb = u % 3
                S.op("act", lambda e: e.activation(out=G[gb][:], in_=bank[ub][:], func=AF.Gelu), reads=["pbb%d" % ub], writes=["pG%d" % gb])

            def st_hs(u, tg, eb, sub, tt, es_):
                ub = u % 2
                for h in range(8):
                    S.op("pe", lambda e, h=h: e.matmul(bank[2 + ub][:], lhsT=idb[:], rhs=Wh[ub][h][:], start=(h == 0), stop=(h == 7)),
                         reads=["identb", "pWh%d_%d" % (ub, h)], writes=["pbb%d" % (2 + ub)])

            def st_wa(u, tg, eb, sub, tt, es_):
                ub = u % 2; gb = u % 3
                S.op("dve", lambda e: e.tensor_tensor(out=WA[ub][:], in0=bank[2 + ub][:], in1=G[gb][:], op=ALU.mult), reads=["pbb%d" % (2 + ub), "pG%d" % gb], writes=["pWA%d" % ub])

            def st_t(u, tg, eb, sub, tt, es_):
                ub = u % 2
                pbt = bank[4][:].bitcast(BF16).rearrange("p (c n) -> p c n", n=128)[:, 0:4, :]
                for c in range(4):
                    S.op("pe", lambda e, c=c: e.transpose(out=pbt[:, c, :], in_=WA[ub][:, c * 128:(c + 1) * 128], identity=idb[:]), reads=["pWA%d" % ub, "identb"], writes=["pbb4"])

            def st_watcopy(u, tg, eb, sub, tt, es_):
                ub = u % 2
                pbt = bank[4][:].bitcast(BF16).rearrange("p (c n) -> p c n", n=128)[:, 0:4, :]
                S.op("act", lambda e: e.copy(out=WAT[ub][:], in_=pbt), reads=["pbb4"], writes=["pWAT%d" % ub])

            def st_v(u, tg, eb, sub, tt, es_):
                ub = u % 2
                for cb in range(2):
                    for ec in range(4):
                        S.op("pe", lambda e, cb=cb, ec=ec: e.matmul(bank[5 + cb][:], lhsT=WAT[ub][:, ec, :], rhs=Vb[es_][:, sub * 4 + ec, cb * 512:(cb + 1) * 512], start=(sub == 0 and ec == 0), stop=(sub == 1 and ec == 3)),
                             reads=["pWAT%d" % ub, "Vb%d" % es_], writes=["pbb%d" % (5 + cb)])

            def st_acc(u, tg, eb, sub, tt, es_):
                if sub != 1:
                    return
                for cb in range(2):
                    if eb == 0:
                        S.op("dve", lambda e, cb=cb: e.tensor_copy(out=acc[:, tt, cb * 512:(cb + 1) * 512], in_=bank[5 + cb][:]), reads=["pbb%d" % (5 + cb)], writes=["pacc%d" % tt])
                    else:
                        S.op("dve", lambda e, cb=cb: e.tensor_tensor(out=acc[:, tt, cb * 512:(cb + 1) * 512], in0=bank[5 + cb][:], in1=acc[:, tt, cb * 512:(cb + 1) * 512], op=ALU.add),
                             reads=["pbb%d" % (5 + cb), "pacc%d" % tt], writes=["pacc%d" % tt])

            u = 0
            for tg in range(4):
                S.dma("sp", xnT[:], D["xnT_d"][tg * 4:(tg + 1) * 4].rearrange("n p f -> p n f"), reads=["xnT_d"], writes=["bxnT"])
                S.dma("sp", sc[:], D["sc_d"][tg * 4:(tg + 1) * 4].rearrange("n p f -> p n f"), reads=["sc_d"], writes=["bsc"])
                S.dma("sp", kap[:], D["tau_d"][tg * 4:(tg + 1) * 4].rearrange("n p f -> p n f"), reads=["tau_d"], writes=["btau"])
                units = []
                for eb in range(16):
                    es_ = (tg * 16 + eb) % 2
                    for tt in range(4):
                        for sub in range(2):
                            units.append((u, tg, eb, sub, tt, es_)); u += 1
                n = len(units)
                U = lambda j: units[j] if 0 <= j < n else None
                for k in range(n + 3):
                    for ebk in ([0] if k == 0 else []) + ([k // 8 + 1] if (k % 8 == 3 and k // 8 + 1 < 16) else []):
                        es_ = (tg * 16 + ebk) % 2
                        S.dma("sp", UT[es_][:], uTv[ebk], reads=["uT_b"], writes=["UT%d" % es_])
                        S.dma("sp", Vb[es_][:], vbv[ebk], reads=["v_b"], writes=["Vb%d" % es_])
                    if U(k - 2): st_wa(*U(k - 2))
                    if U(k - 3): st_v(*U(k - 3))
                    if U(k - 2): st_t(*U(k - 2))
                    if U(k - 1): st_gelu(*U(k - 1))
                    if U(k - 1): st_hs(*U(k - 1))
                    if U(k): stage1a(*U(k))
                    if U(k - 2): st_watcopy(*U(k - 2))
                    if U(k - 3): st_acc(*U(k - 3))
                for tt in range(4):
                    sl = tt % 2; n = tg * 4 + tt
                    S.dma("sp", h2t[sl][:], hv[n], reads=["h2_d"], writes=["ph2t%d" % sl])
                    S.op("pool", lambda e, sl=sl, tt=tt: e.tensor_tensor(out=h2t[sl][:], in0=h2t[sl][:], in1=acc[:, tt, :], op=ALU.add), reads=["ph2t%d" % sl, "pacc%d" % tt], writes=["ph2t%d" % sl])
                    S.dma("sp", ov[n], h2t[sl][:], reads=["ph2t%d" % sl], writes=["out"])


_PROG = {}


def kernel(**inputs):
    sh = host_shared(inputs)
    sh.update(host_shared_rest(inputs))
    if "nc" not in _PROG:
        _PROG["nc"] = build_program(("mixer", "mem", "peer"))[0]
    nc = _PROG["nc"]
    names = set(IN_SHAPES) | set(IN_SHAPES_MEM) | set(IN_SHAPES_PEER)
    mem = np.asarray(inputs["mem"], np.float32)
    maps = []
    for c in range(8):
        m = {k: v for k, v in sh.items() if k in names}
        m.update(host_core(inputs, c))
        m["mem"] = np.ascontiguousarray(mem[c // 4])
        maps.append(m)
    res = run_bass_kernel_spmd(nc, maps, core_ids=list(range(8)))
    out = np.zeros((2, 8192, 1024), np.float32)
    for c in range(8):
        out[c // 4, (c % 4) * 2048:(c % 4 + 1) * 2048] = res.results[c]["out"]
    return out
```

```python
from contextlib import ExitStack, contextmanager
import numpy as np
import concourse.bass as bass
import concourse.mybir as mybir
from concourse.bass_utils import run_bass_kernel_spmd

F32 = mybir.dt.float32
BF16 = mybir.dt.bfloat16
I32 = mybir.dt.int32
AF = mybir.ActivationFunctionType
ALU = mybir.AluOpType
AX = mybir.AxisListType

ENGS = ("pe", "act", "dve", "pool", "sp")
TWO_PI = 6.283185307179586
EPS = 1e-6
NEG = -30000.0


class Sched:
    def __init__(self, nc, es, n_dma_sems=32):
        self.nc = nc
        self.streams = {e: [] for e in ENGS}
        self.sem = {e: es.enter_context(nc.semaphore("s_" + e)) for e in ("pe", "act", "dve", "pool")}
        self.cnt = {e: 0 for e in ("pe", "act", "dve", "pool")}
        self.dsem = [es.enter_context(nc.semaphore("s_dma%d" % i)) for i in range(n_dma_sems)]
        self.dcnt = [0] * n_dma_sems
        self.dnext = 0
        self.n_sw = 4
        self.dnext_sw = 0
        self.waited = {}
        self.last_w = {}
        self.readers = {}
        self.n_ops = 0

    def _deps(self, eng, reads, writes):
        deps = []
        for k in reads:
            if k in self.last_w:
                deps.append(self.last_w[k])
        for k in writes:
            if k in self.last_w:
                deps.append(self.last_w[k])
            deps.extend(self.readers.get(k, ()))
        need = {}
        for (sk, val, peng) in deps:
            if peng == "pe" and eng == "pe":
                continue
            if self.waited.get((eng, sk), 0) >= val:
                continue
            if need.get(sk, 0) < val:
                need[sk] = val
        return need

    def _semobj(self, sk):
        return self.sem[sk] if isinstance(sk, str) else self.dsem[sk]

    def _emit_waits(self, eng, need):
        for sk, val in need.items():
            self.waited[(eng, sk)] = val
            so = self._semobj(sk)
            self.streams[eng].append(lambda e, so=so, val=val: e.wait_ge(so, val))

    def _record(self, tok, reads, writes):
        for k in writes:
            self.last_w[k] = tok
            self.readers[k] = []
        for k in reads:
            if k not in writes:
                self.readers.setdefault(k, []).append(tok)

    def op(self, eng, fn, reads=(), writes=()):
        need = self._deps(eng, reads, writes)
        self._emit_waits(eng, need)
        self.cnt[eng] += 1
        val = self.cnt[eng]
        so = self.sem[eng]
        self.streams[eng].append(lambda e, fn=fn, so=so: fn(e).then_inc(so, 1))
        self._record((eng, val, eng), reads, writes)
        self.n_ops += 1

    def dma(self, q, out, in_, reads=(), writes=(), **kw):
        nhw = len(self.dsem) - self.n_sw
        if q == "pool":
            i = nhw + self.dnext_sw
            self.dnext_sw = (self.dnext_sw + 1) % self.n_sw
        else:
            i = self.dnext
            self.dnext = (self.dnext + 1) % nhw
        need = self._deps(q, reads, writes)
        prev = 16 * self.dcnt[i]
        if prev and self.waited.get((q, i), 0) < prev:
            need[i] = max(need.get(i, 0), prev)
        self._emit_waits(q, need)
        self.dcnt[i] += 1
        val = 16 * self.dcnt[i]
        so = self.dsem[i]
        self.streams[q].append(
            lambda e, out=out, in_=in_, so=so, kw=kw: e.dma_start(out=out, in_=in_, **kw).then_inc(so, 16))
        self._record((i, val, "dma"), reads, writes)
        self.n_ops += 1

    def barrier(self):
        for eng in ENGS:
            need = {}
            for pe_ in ("pe", "act", "dve", "pool"):
                v = self.cnt[pe_]
                if v and self.waited.get((eng, pe_), 0) < v:
                    need[pe_] = v
            for i, c in enumerate(self.dcnt):
                if c and self.waited.get((eng, i), 0) < 16 * c:
                    need[i] = 16 * c
            self._emit_waits(eng, need)

    def wait_all(self, eng, keys):
        need = {}
        for k in keys:
            if k in self.last_w:
                sk, val, _ = self.last_w[k]
                if self.waited.get((eng, sk), 0) < val and need.get(sk, 0) < val:
                    need[sk] = val
        self._emit_waits(eng, need)

    def emit(self):
        if not any(self.streams[e] for e in ENGS):
            return
        streams = self.streams
        self.streams = {e: [] for e in ENGS}
        self._emit_block(streams)

    def _emit_block(self, streams):
        self_streams = streams
        with self.nc.Block() as block:
            @block.tensor
            def _(e):
                for f in self_streams["pe"]:
                    f(e)

            @block.scalar
            def _(e):
                for f in self_streams["act"]:
                    f(e)

            @block.vector
            def _(e):
                for f in self_streams["dve"]:
                    f(e)

            @block.gpsimd
            def _(e):
                for f in self_streams["pool"]:
                    f(e)

            @block.sync
            def _(e):
                for f in self_streams["sp"]:
                    f(e)


class Ctx:
    def __init__(self, nc, S):
        self.nc = nc
        self.S = S
        self.uid = 0

    def sb(self, es, name, shape, dt=F32):
        self.uid += 1
        return es.enter_context(self.nc.sbuf_tensor("%s_%d" % (name, self.uid), list(shape), dt))

    def ps(self, es, name, shape, dt=F32):
        self.uid += 1
        return es.enter_context(self.nc.psum_tensor("%s_%d" % (name, self.uid), list(shape), dt))


@contextmanager
def scope(C):
    with ExitStack() as es:
        yield es
        C.S.barrier()
        C.S.emit()


def bc(ap, shape):
    return ap.to_broadcast(list(shape))


def make_ident(C, es, name="ident"):
    S = C.S
    idf = C.sb(es, name + "f", [128, 128])
    idb = C.sb(es, name + "b", [128, 128], BF16)
    S.op("pool", lambda e: e.memset(idf[:], 1.0), writes=[name + "f"])
    S.op("pool", lambda e: e.affine_select(out=idf[:], in_=idf[:], pattern=[[-1, 128]], compare_op=ALU.is_equal,
                                           fill=0.0, base=0, channel_multiplier=1), reads=[name + "f"], writes=[name + "f"])
    S.op("dve", lambda e: e.tensor_copy(out=idb[:], in_=idf[:]), reads=[name + "f"], writes=[name + "b"])
    return idf, idb


def sincos(C, es, ang, n, tag):
    S = C.S
    outs = []
    for which, off in (("s", 64.0), ("c", 64.25)):
        k = tag + which
        y = C.sb(es, k + "y", [128, n]); yi = C.sb(es, k + "yi", [128, n], I32); yf = C.sb(es, k + "yf", [128, n])
        m = C.sb(es, k + "m", [128, n]); o = C.sb(es, k + "o", [128, n])
        S.op("dve", lambda e, y=y, off=off: e.tensor_scalar(out=y[:], in0=ang, scalar1=1.0 / TWO_PI, scalar2=off, op0=ALU.mult, op1=ALU.add),
             reads=[tag + "ang"], writes=[k + "y"])
        S.op("dve", lambda e, y=y, yi=yi: e.tensor_copy(out=yi[:], in_=y[:]), reads=[k + "y"], writes=[k + "yi"])
        S.op("dve", lambda e, yi=yi, yf=yf: e.tensor_copy(out=yf[:], in_=yi[:]), reads=[k + "yi"], writes=[k + "yf"])
        S.op("dve", lambda e, y=y, yf=yf: e.tensor_tensor(out=y[:], in0=y[:], in1=yf[:], op=ALU.subtract), reads=[k + "y", k + "yf"], writes=[k + "y"])
        S.op("dve", lambda e, y=y, m=m: e.tensor_scalar(out=m[:], in0=y[:], scalar1=0.5, scalar2=None, op0=ALU.is_gt), reads=[k + "y"], writes=[k + "m"])
        S.op("dve", lambda e, y=y, m=m: e.tensor_tensor(out=y[:], in0=y[:], in1=m[:], op=ALU.subtract), reads=[k + "y", k + "m"], writes=[k + "y"])
        S.op("act", lambda e, y=y, o=o: e.activation(out=o[:], in_=y[:], func=AF.Sin, scale=TWO_PI), reads=[k + "y"], writes=[k + "o"])
        outs.append((o, k + "o"))
    return outs


def s5_params(C, es_keep, D):
    S, nc = C.S, C.nc
    P = {}
    LT2 = C.sb(es_keep, "LT2", [128, 8, 8, 2, 128], BF16)
    WPr = C.sb(es_keep, "WPr", [128, 9, 16]); WPi = C.sb(es_keep, "WPi", [128, 9, 16]); WPn = C.sb(es_keep, "WPn", [128, 9, 16])
    P.update(LT2=LT2, WPr=WPr, WPi=WPi, WPn=WPn)
    with scope(C) as es:
        sb = lambda name, shape, dt=F32: C.sb(es, name, shape, dt)
        Mz2 = sb("Mz2", [128, 16, 8, 128], BF16)
        CA = sb("CA", [128, 32, 128], BF16); CAs = sb("CAs", [128, 32, 128], BF16)
        LR = sb("LR", [128, 32]); LI = sb("LI", [128, 32]); LS = sb("LS", [128, 32])
        SG = sb("SG", [128, 2]); NV = sb("NV", [128, 23])
        P1B = sb("P1B", [128, 32, 16]); P2B = sb("P2B", [128, 32, 16]); P1C = sb("P1C", [128, 32, 16]); P2C = sb("P2C", [128, 32, 16])
        DD = sb("DD", [128, 16, 16])
        for t, nm in ((LR, "s5_lr"), (LI, "s5_li"), (LS, "s5_ls"), (SG, "s5_sg"), (NV, "s5_nv")):
            S.dma("sp", t[:], D[nm], writes=[nm])
        S.dma("sp", DD[:].rearrange("p e c -> p (e c)"), D["s5_dd"], writes=["s5_dd"])
        for t, nm in ((P1B, "s5_p1b"), (P2B, "s5_p2b"), (P1C, "s5_p1c"), (P2C, "s5_p2c")):
            S.dma("sp", t[:].rearrange("p g c -> p (g c)"), D[nm], writes=[nm])
        idf, idb = make_ident(C, es, "pid")
        STEP = sb("STEP", [128, 32]); AA = sb("AA", [128, 32]); PH = sb("PH", [128, 32])
        S.op("act", lambda e: e.activation(out=STEP[:], in_=LS[:], func=AF.Exp), reads=["s5_ls"], writes=["STEP"])
        S.op("dve", lambda e: e.tensor_tensor(out=AA[:], in0=LR[:], in1=STEP[:], op=ALU.mult), reads=["s5_lr", "STEP"], writes=["AA"])
        S.op("dve", lambda e: e.tensor_tensor(out=PH[:], in0=LI[:], in1=STEP[:], op=ALU.mult), reads=["s5_li", "STEP"], writes=["PH"])
        EXPO = sb("EXPO", [128, 32, 23]); ANG = sb("ANG", [128, 32, 23]); MAG = sb("MAG", [128, 32, 23])
        nvb = bc(NV[:].unsqueeze(1), [128, 32, 23])
        S.op("dve", lambda e: e.tensor_tensor(out=EXPO[:], in0=bc(AA[:].unsqueeze(2), [128, 32, 23]), in1=nvb, op=ALU.mult), reads=["AA", "s5_nv"], writes=["EXPO"])
        S.op("dve", lambda e: e.tensor_tensor(out=ANG[:], in0=bc(PH[:].unsqueeze(2), [128, 32, 23]), in1=nvb, op=ALU.mult), reads=["PH", "s5_nv"], writes=["pwang"])
        S.op("act", lambda e: e.activation(out=MAG[:], in_=EXPO[:], func=AF.Exp), reads=["EXPO"], writes=["MAG"])
        (sn, snk), (cs, csk) = sincos(C, es, ANG[:].rearrange("p g n -> p (g n)"), 32 * 23, "pw")
        CR = sb("CR", [128, 32, 23]); CI = sb("CI", [128, 32, 23])
        S.op("dve", lambda e: e.tensor_tensor(out=CR[:].rearrange("p g n -> p (g n)"), in0=MAG[:].rearrange("p g n -> p (g n)"), in1=cs[:], op=ALU.mult), reads=["MAG", csk], writes=["CR"])
        S.op("dve", lambda e: e.tensor_tensor(out=CI[:].rearrange("p g n -> p (g n)"), in0=MAG[:].rearrange("p g n -> p (g n)"), in1=sn[:], op=ALU.mult), reads=["MAG", snk], writes=["CI"])
        zr = sb("zr", [128, 32]); den = sb("den", [128, 32]); t0 = sb("t0", [128, 32]); fr = sb("fr", [128, 32]); fi = sb("fi", [128, 32])
        S.op("dve", lambda e: e.tensor_scalar(out=zr[:], in0=CR[:, :, 8], scalar1=-1.0, scalar2=None, op0=ALU.add), reads=["CR"], writes=["zr"])
        S.op("dve", lambda e: e.tensor_tensor(out=den[:], in0=LR[:], in1=LR[:], op=ALU.mult), reads=["s5_lr"], writes=["den"])
        S.op("dve", lambda e: e.tensor_tensor(out=t0[:], in0=LI[:], in1=LI[:], op=ALU.mult), reads=["s5_li"], writes=["t0"])
        S.op("dve", lambda e: e.tensor_tensor(out=den[:], in0=den[:], in1=t0[:], op=ALU.add), reads=["den", "t0"], writes=["den"])
        S.op("dve", lambda e: e.reciprocal(out=den[:], in_=den[:]), reads=["den"], writes=["den"])
        S.op("dve", lambda e: e.tensor_tensor(out=fr[:], in0=zr[:], in1=LR[:], op=ALU.mult), reads=["zr", "s5_lr"], writes=["fr"])
        S.op("dve", lambda e: e.tensor_tensor(out=t0[:], in0=CI[:, :, 8], in1=LI[:], op=ALU.mult), reads=["CI", "s5_li", "den"], writes=["t0"])
        S.op("dve", lambda e: e.tensor_tensor(out=fr[:], in0=fr[:], in1=t0[:], op=ALU.add), reads=["fr", "t0"], writes=["fr"])
        S.op("dve", lambda e: e.tensor_tensor(out=fr[:], in0=fr[:], in1=den[:], op=ALU.mult), reads=["fr", "den"], writes=["fr"])
        S.op("dve", lambda e: e.tensor_tensor(out=fi[:], in0=CI[:, :, 8], in1=LR[:], op=ALU.mult), reads=["CI", "s5_lr"], writes=["fi"])
        S.op("dve", lambda e: e.tensor_tensor(out=t0[:], in0=zr[:], in1=LI[:], op=ALU.mult), reads=["zr", "s5_li", "fr"], writes=["t0"])
        S.op("dve", lambda e: e.tensor_tensor(out=fi[:], in0=fi[:], in1=t0[:], op=ALU.subtract), reads=["fi", "t0"], writes=["fi"])
        S.op("dve", lambda e: e.tensor_tensor(out=fi[:], in0=fi[:], in1=den[:], op=ALU.mult), reads=["fi", "den"], writes=["fi"])
        BB1 = sb("BB1", [128, 32, 16]); BB2 = sb("BB2", [128, 32, 16]); ta = sb("ta", [128, 32, 16]); tb = sb("tb", [128, 32, 16])
        frb = bc(fr[:].unsqueeze(2), [128, 32, 16]); fib = bc(fi[:].unsqueeze(2), [128, 32, 16])
        fl = lambda t: t[:].rearrange("p g c -> p (g c)")
        S.op("dve", lambda e: e.tensor_tensor(out=ta[:], in0=P1B[:], in1=frb, op=ALU.mult), reads=["s5_p1b", "fr"], writes=["ta"])
        S.op("dve", lambda e: e.tensor_tensor(out=tb[:], in0=P2B[:], in1=fib, op=ALU.mult), reads=["s5_p2b", "fi"], writes=["tb"])
        S.op("dve", lambda e: e.scalar_tensor_tensor(out=fl(BB1), in0=fl(tb), scalar=SG[:, 0:1], in1=fl(ta), op0=ALU.mult, op1=ALU.add), reads=["ta", "tb", "s5_sg"], writes=["BB1"])
        S.op("dve", lambda e: e.tensor_tensor(out=ta[:], in0=P2B[:], in1=frb, op=ALU.mult), reads=["s5_p2b", "fr", "BB1"], writes=["ta"])
        S.op("dve", lambda e: e.tensor_tensor(out=tb[:], in0=P1B[:], in1=fib, op=ALU.mult), reads=["s5_p1b", "fi", "BB1"], writes=["tb"])
        S.op("dve", lambda e: e.scalar_tensor_tensor(out=fl(BB2), in0=fl(tb), scalar=SG[:, 1:2], in1=fl(ta), op0=ALU.mult, op1=ALU.add), reads=["ta", "tb", "s5_sg"], writes=["BB2"])
        t5 = sb("t5", [128, 32, 16]); t6 = sb("t6", [128, 32, 16])
        Rm = sb("Rm", [128, 32, 8, 16])
        Q1 = sb("Q1", [128, 32, 16]); Q2 = sb("Q2", [128, 32, 16])
        S.op("dve", lambda e: e.tensor_scalar(out=fl(Q1), in0=fl(P1C), scalar1=SG[:, 1:2], scalar2=None, op0=ALU.mult), reads=["s5_p1c", "s5_sg"], writes=["Q1"])
        S.op("dve", lambda e: e.tensor_scalar(out=fl(Q2), in0=fl(P2C), scalar1=SG[:, 0:1], scalar2=None, op0=ALU.mult), reads=["s5_p2c", "s5_sg"], writes=["Q2"])
        CAv = CA[:].rearrange("p g (t c) -> p g t c", c=16); CAsv = CAs[:].rearrange("p g (t c) -> p g t c", c=16)
        for t in range(8):
            for (dst, dkey, a_, akey, b_, bkey, idx) in ((Rm[:, :, t, :], "Rm", Q1, "Q1", P2C, "s5_p2c", 7 + t),
                                                         (CAv[:, :, t, :], "CA", Q1, "Q1", P2C, "s5_p2c", 15 + t),
                                                         (CAsv[:, :, t, :], "CAs", Q2, "Q2", P1C, "s5_p1c", 15 + t)):
                S.op("dve", lambda e, a_=a_, idx=idx: e.tensor_tensor(out=t5[:], in0=a_[:], in1=bc(CR[:, :, idx:idx + 1], [128, 32, 16]), op=ALU.mult), reads=[akey, "CR"], writes=["t5"])
                S.op("dve", lambda e, b_=b_, idx=idx: e.tensor_tensor(out=t6[:], in0=b_[:], in1=bc(CI[:, :, idx:idx + 1], [128, 32, 16]), op=ALU.mult), reads=[bkey, "CI"], writes=["t6"])
                S.op("dve", lambda e, dst=dst: e.tensor_tensor(out=dst, in0=t5[:], in1=t6[:], op=ALU.subtract), reads=["t5", "t6"], writes=[dkey])
        Wr = sb("Wr", [128, 9, 32]); Wi = sb("Wi", [128, 9, 32]); sq = sb("sq", [128, 32])
        S.op("dve", lambda e: e.tensor_copy(out=Wr[:, 0, :], in_=CR[:, :, 15]), reads=["CR"], writes=["Wr"])
        S.op("dve", lambda e: e.tensor_copy(out=Wi[:, 0, :], in_=CI[:, :, 15]), reads=["CI"], writes=["Wi"])
        for j in range(8):
            S.op("dve", lambda e, j=j: e.tensor_tensor(out=Wr[:, j + 1, :], in0=Wr[:, j, :], in1=Wr[:, j, :], op=ALU.mult), reads=["Wr"], writes=["Wr"])
            S.op("dve", lambda e, j=j: e.tensor_tensor(out=sq[:], in0=Wi[:, j, :], in1=Wi[:, j, :], op=ALU.mult), reads=["Wi"], writes=["sq"])
            S.op("dve", lambda e, j=j: e.tensor_tensor(out=Wr[:, j + 1, :], in0=Wr[:, j + 1, :], in1=sq[:], op=ALU.subtract), reads=["Wr", "sq"], writes=["Wr"])
            S.op("dve", lambda e, j=j: e.scalar_tensor_tensor(out=Wi[:, j + 1, :], in0=Wr[:, j, :], scalar=2.0, in1=Wi[:, j, :], op0=ALU.mult, op1=ALU.mult), reads=["Wr", "Wi"], writes=["Wi"])
        Wrv = Wr[:].rearrange("p j (a r w) -> p j a r w", r=2, w=2); Wiv = Wi[:].rearrange("p j (a r w) -> p j a r w", r=2, w=2)
        for r in range(2):
            pr_ = slice(64 * r, 64 * r + 64)
            for j in range(9):
                S.op("dve", lambda e, r=r, pr_=pr_, j=j: e.tensor_copy(out=WPr[pr_, j, :].rearrange("p (a w) -> p a w", w=2), in_=Wrv[pr_, j, :, r, :]), reads=["Wr"], writes=["WPr"])
                S.op("dve", lambda e, r=r, pr_=pr_, j=j: e.tensor_copy(out=WPi[pr_, j, :].rearrange("p (a w) -> p a w", w=2), in_=Wiv[pr_, j, :, r, :]), reads=["Wi"], writes=["WPi"])
        S.op("dve", lambda e: e.tensor_scalar(out=WPn[:].rearrange("p j q -> p (j q)"), in0=WPi[:].rearrange("p j q -> p (j q)"), scalar1=-1.0, scalar2=None, op0=ALU.mult), reads=["WPi"], writes=["WPn"])
        E = [[sb("E%d%d" % (h_, r), [128, 128]) for r in range(2)] for h_ in range(2)]
        for h_ in range(2):
            for r in range(2):
                S.op("pool", lambda e, h_=h_, r=r: e.memset(E[h_][r][:], 0.0), writes=["E%d%d" % (h_, r)])
                S.op("pool", lambda e, h_=h_, r=r: e.tensor_copy(out=E[h_][r][64 * h_:64 * h_ + 64, 64 * r:64 * r + 64], in_=idf[64 * h_:64 * h_ + 64, 64 * h_:64 * h_ + 64]),
                     reads=["pidf", "E%d%d" % (h_, r)], writes=["E%d%d" % (h_, r)])
        Lz = [sb("Lz%d" % i, [128, 32, 64]) for i in range(2)]
        for i in range(2):
            S.op("pool", lambda e, i=i: e.memset(Lz[i][:].rearrange("p g c -> p (g c)"), 0.0), writes=["Lz%d" % i])
        S.op("pool", lambda e: e.memset(Mz2[:].rearrange("p a b c -> p (a b c)"), 0.0), writes=["Mz2"])
        t5v = t5[:].rearrange("p (a j) c -> p a j c", j=4); t6v = t6[:].rearrange("p (a j) c -> p a j c", j=4)
        with scope(C) as esp:
            PM = C.ps(esp, "PM", [128, 16, 128]); PL = C.ps(esp, "PL", [128, 8, 2, 128])
            for s in range(8):
                i = 7 - s; sl = s % 2; lk = "Lz%d" % sl
                Lzv = Lz[sl][:].rearrange("p (a j) c -> p a j c", j=4)
                S.op("dve", lambda e, i=i: e.tensor_tensor(out=t5[:], in0=BB1[:], in1=bc(CR[:, :, i:i + 1], [128, 32, 16]), op=ALU.mult), reads=["BB1", "CR"], writes=["t5"])
                S.op("dve", lambda e, i=i: e.tensor_tensor(out=t6[:], in0=BB2[:], in1=bc(CI[:, :, i:i + 1], [128, 32, 16]), op=ALU.mult), reads=["BB2", "CI"], writes=["t6"])
                for j4 in range(4):
                    S.op("dve", lambda e, j4=j4, Lzv=Lzv: e.scalar_tensor_tensor(out=Lzv[:, :, j4, 16 * j4:16 * j4 + 16], in0=t6v[:, :, j4, :], scalar=SG[:, 0:1], in1=t5v[:, :, j4, :], op0=ALU.mult, op1=ALU.add),
                         reads=["t5", "t6", "s5_sg"], writes=[lk])
                for g in range(32):
                    chc = g // 8; hb = (g % 8) // 4; j4 = g % 4; e_ = chc * 4 + j4
                    rows = slice(64 * hb, 64 * hb + 64)
                    S.op("pe", lambda e, g=g, sl=sl, e_=e_, rows=rows: e.matmul(PM[rows, e_, :], lhsT=Lz[sl][:, g, :], rhs=Rm[:, g, :, :].rearrange("p t c -> p (t c)"), start=True, stop=True),
                         reads=[lk, "Rm"], writes=["PM"])
                for pr in range(16):
                    a_ = pr // 2; wp = pr % 2; chc = a_ // 2; hb = a_ % 2; e2 = chc * 2 + wp
                    rows = slice(64 * hb, 64 * hb + 64)
                    for h_ in range(2):
                        for r in range(2):
                            g = 4 * a_ + 2 * r + wp
                            S.op("pe", lambda e, g=g, sl=sl, e2=e2, rows=rows, h_=h_, r=r: e.matmul(PL[rows, e2, h_, :], lhsT=Lz[sl][:, g, :], rhs=E[h_][r][:], start=(r == 0), stop=(r == 1)),
                                 reads=[lk, "E%d%d" % (h_, r)], writes=["PL"])
                S.op("dve", lambda e, s=s: e.tensor_copy(out=Mz2[:, :, s, 16 * s:128], in_=PM[:, :, 16 * s:128]), reads=["PM"], writes=["Mz2"])
                S.op("dve", lambda e, s=s: e.tensor_tensor(out=Mz2[:, :, s, 16 * s:16 * s + 16], in0=PM[:, :, 16 * s:16 * s + 16], in1=DD[:], op=ALU.add), reads=["PM", "s5_dd"], writes=["Mz2"])
                S.op("act", lambda e, s=s: e.copy(out=LT2[:, :, s, :, :], in_=PL[:]), reads=["PL"], writes=["LT2"])
        S.dma("sp", D["Mz_d"], Mz2[:].rearrange("p a b c -> p (a b c)"), reads=["Mz2"], writes=["Mz_d"])
        S.dma("sp", D["CA_d"][:, 0, :], CA[:].rearrange("p g c -> p (g c)"), reads=["CA"], writes=["CA_d"])
        S.dma("sp", D["CA_d"][:, 1, :], CAs[:].rearrange("p g c -> p (g c)"), reads=["CAs"], writes=["CA_d"])
    return P


def load_weight_bf16(C, es, es_tmp, name, src, rows_chunks, ncols, gcol=None, q="sp"):
    S = C.S
    W = C.sb(es, name, [128, rows_chunks, ncols], BF16)
    stg = [C.sb(es_tmp, name + "_stg%d" % i, [128, ncols]) for i in range(2)]
    srcv = src.rearrange("(c p) n -> c p n", p=128)
    for c in range(rows_chunks):
        st = stg[c % 2]; sk = name + "_stg%d" % (c % 2)
        S.dma(q, st[:], srcv[c], writes=[sk])
        eng = "dve" if c % 2 == 0 else "pool"
        if gcol is not None:
            S.op(eng, lambda e, st=st, c=c: e.tensor_scalar(out=W[:, c, :], in0=st[:], scalar1=gcol[0][:, c:c + 1], scalar2=None, op0=ALU.mult),
                 reads=[sk, gcol[1]], writes=[name])
        else:
            S.op(eng, lambda e, st=st, c=c: e.tensor_copy(out=W[:, c, :], in_=st[:]), reads=[sk], writes=[name])
    return W


def rms_rstd(C, x_ap, xkey, junk, jkey, ss, rs, skey, n):
    S = C.S
    S.op("act", lambda e: e.activation(out=junk, in_=x_ap, func=AF.Square, accum_out=ss), reads=[xkey], writes=[skey + "_ss"])
    S.op("act", lambda e: e.activation(out=rs, in_=ss, func=AF.Sqrt, scale=1.0 / n, bias=EPS), reads=[skey + "_ss"], writes=[skey + "_sq"])
    S.op("dve", lambda e: e.reciprocal(out=rs, in_=rs), reads=[skey + "_sq"], writes=[skey])


def transpose_chunks(C, src, skey, nch, pbank, pkey, dst, dkey, idb, evac="act"):
    S = C.S
    for c in range(nch):
        S.op("pe", lambda e, c=c: e.transpose(out=pbank[:, c, :], in_=src[:, c * 128:(c + 1) * 128], identity=idb[:]), reads=[skey, "identb"], writes=[pkey])
    if evac == "act":
        S.op("act", lambda e: e.copy(out=dst, in_=pbank), reads=[pkey], writes=[dkey])
    else:
        S.op(evac, lambda e: e.tensor_copy(out=dst, in_=pbank), reads=[pkey], writes=[dkey])


def stage_mixer(C, D, dbg=None, upto=9, prep=None):
    S, nc = C.S, C.nc
    dbg = dbg or {}
    with scope(C) as es1:
        P = s5_params(C, es1, D)
        idf = C.sb(es1, "identf", [128, 128]); idb = C.sb(es1, "identb", [128, 128], BF16)
        S.op("pool", lambda e: e.memset(idf[:], 1.0), writes=["identf"])
        S.op("pool", lambda e: e.affine_select(out=idf[:], in_=idf[:], pattern=[[-1, 128]], compare_op=ALU.is_equal, fill=0.0, base=0, channel_multiplier=1), reads=["identf"], writes=["identf"])
        S.op("dve", lambda e: e.tensor_copy(out=idb[:], in_=idf[:]), reads=["identf"], writes=["identb"])
        if "WPr" in dbg:
            for nm in ("WPr", "WPi"):
                S.dma("sp", dbg[nm], P[nm][:].rearrange("p a b -> p (a b)"), reads=[nm], writes=["o_" + nm])
            S.dma("pool", dbg["LT2"], P["LT2"][:].rearrange("p a b c d -> p (a b c d)"), reads=["LT2"], writes=["o_LT2"])
            S.dma("pool", dbg["Mz"], D["Mz_d"], reads=["Mz_d"], writes=["o_Mz"])
            S.dma("pool", dbg["CA"], D["CA_d"].rearrange("p a b -> p (a b)"), reads=["CA_d"], writes=["o_CA"])
        if upto < 1:
            return
        carry_r = C.sb(es1, "carry_r", [128, 16]); carry_i = C.sb(es1, "carry_i", [128, 16])
        S.op("pool", lambda e: e.memset(carry_r[:], 0.0), writes=["carry_r"])
        S.op("pool", lambda e: e.memset(carry_i[:], 0.0), writes=["carry_i"])
        TRE = C.sb(es1, "TRE", [128, 16, 257]); TIM = C.sb(es1, "TIM", [128, 16, 257])
        uT = C.sb(es1, "uT", [128, 4, 8, 256], BF16)
        with scope(C) as es2:
            _mixer_passes(C, es2, D, P, idb, carry_r, carry_i, TRE, TIM, uT, dbg)
        if "carry" in dbg:
            S.dma("sp", dbg["carry"][:, 0:16], carry_r[:], reads=["carry_r"], writes=["o_carry"])
            S.dma("sp", dbg["carry"][:, 16:32], carry_i[:], reads=["carry_i"], writes=["o_carry2"])
        if upto < 2:
            return
        with scope(C) as es3:
            ytm = C.sb(es3, "ytm", [128, 2, 8, 512], BF16)
            with scope(C) as es4:
                _s5_scan_out(C, es4, D, P, idb, TRE, TIM, uT, ytm, carry_r, carry_i, dbg)
            if upto < 3:
                return
            _s5_glu_out(C, es3, D, idb, ytm, dbg)
    if upto < 4:
        return
    with scope(C) as es5:
        idf = C.sb(es5, "identf", [128, 128]); idb = C.sb(es5, "identb", [128, 128], BF16)
        S.op("pool", lambda e: e.memset(idf[:], 1.0), reads=[], writes=["identf"])
        S.op("pool", lambda e: e.affine_select(out=idf[:], in_=idf[:], pattern=[[-1, 128]], compare_op=ALU.is_equal, fill=0.0, base=0, channel_multiplier=1), reads=["identf"], writes=["identf"])
        S.op("dve", lambda e: e.tensor_copy(out=idb[:], in_=idf[:]), reads=["identf"], writes=["identb"])
        _attention(C, es5, D, idb, dbg, prep)


def _mixer_passes(C, es, D, P, idb, carry_r, carry_i, TRE, TIM, uT, dbg):
    S = C.S
    sb = lambda name, shape, dt=F32: C.sb(es, name, shape, dt)
    gin = sb("gin", [128, 8])
    S.dma("sp", gin[:], D["g_mix"], writes=["gin"])
    with scope(C) as est:
        Wb = load_weight_bf16(C, es, est, "Wb", D["w_in"], 8, 2048, gcol=(gin, "gin"))
    gq = sb("gq", [128, 64]); gk = sb("gk", [128, 64]); hv = sb("hv", [128, 4])
    S.dma("sp", gq[:], D["att_q_g"].partition_broadcast(128), writes=["gq"])
    S.dma("sp", gk[:], D["att_k_g"].partition_broadcast(128), writes=["gk"])
    S.dma("sp", hv[:], D["hvalid"], writes=["hv"])
    S.op("dve", lambda e: e.tensor_scalar(out=gq[:], in0=gq[:], scalar1=0.125, scalar2=None, op0=ALU.mult), reads=["gq"], writes=["gq"])
    xt = [sb("xt%d" % i, [128, 1024]) for i in range(2)]
    junk = sb("junk", [128, 1024]); st = [sb("st%d" % i, [128, 4]) for i in range(2)]
    xn = [sb("xn%d" % i, [128, 1024], BF16) for i in range(2)]
    xnT = [sb("xnT%d" % i, [128, 8, 512], BF16) for i in range(2)]
    qkv = sb("qkv", [128, 3, 512]); sq = sb("sq2", [128, 512]); qst = sb("qst", [128, 4, 8])
    qn = sb("qn", [128, 2, 512], BF16)
    kTs = [sb("kTs%d" % i, [128, 4, 128], BF16) for i in range(2)]; qTs = [sb("qTs%d" % i, [128, 4, 128], BF16) for i in range(2)]
    Vs = [sb("Vs%d" % i, [128, 8, 65], BF16) for i in range(2)]
    tBr = sb("tBr", [128, 8, 128]); tBi = sb("tBi", [128, 8, 128]); tCr = sb("tCr", [128, 8, 64]); tCi = sb("tCi", [128, 8, 64])
    tt1 = sb("tt1", [128, 8, 128]); tt2 = sb("tt2", [128, 8, 128]); tt3 = sb("tt3", [128, 8, 128]); tt4 = sb("tt4", [128, 8, 128])
    REDr = sb("REDr", [128, 16]); REDi = sb("REDi", [128, 16]); c1 = sb("c1", [128, 16]); c2 = sb("c2", [128, 16]); c3 = sb("c3", [128, 16])
    with scope(C) as esp:
        bank = [C.ps(esp, "bk%d" % i, [128, 512]) for i in range(8)]
        xv = D["x_ext"].rearrange("(n p) d -> n p d", p=128)
        tile_ctr = 0
        for q in range(4):
            for blk in range(4):
                bslot = (q * 4 + blk) % 2
                xT = xnT[bslot]; xTk = "xnT%d" % bslot
                for tt in range(4):
                    n_tile = q * 16 + blk * 4 + tt
                    sl = tile_ctr % 2; tile_ctr += 1
                    x_ = xt[sl]; xk = "xt%d" % sl
                    S.dma("sp", x_[:], xv[n_tile], writes=[xk])
                    rms_rstd(C, x_[:], xk, junk[:], "junk", st[sl][:, 0:1], st[sl][:, 1:2], "st%d" % sl, 1024)
                    S.op("dve", lambda e, x_=x_, sl=sl: e.tensor_scalar(out=xn[sl][:], in0=x_[:], scalar1=st[sl][:, 1:2], scalar2=None, op0=ALU.mult),
                         reads=[xk, "st%d" % sl], writes=["xn%d" % sl])
                    tb_ = 0 if sl == 0 else 7
                    pb = bank[tb_][:].bitcast(BF16).rearrange("p (c n) -> p c n", n=128)
                    transpose_chunks(C, xn[sl][:], "xn%d" % sl, 8, pb, "bk%d" % tb_, xT[:, :, tt * 128:(tt + 1) * 128], xTk, idb, evac=("act" if sl == 0 else "dve"))
                for chc in range(4):
                    bi = 1 + (chc % 2); bkk = "bk%d" % bi
                    for dc in range(8):
                        S.op("pe", lambda e, chc=chc, dc=dc, bi=bi, xT=xT: e.matmul(bank[bi][:], lhsT=Wb[:, dc, 1536 + chc * 128:1536 + (chc + 1) * 128], rhs=xT[:, dc, :], start=(dc == 0), stop=(dc == 7)),
                             reads=["Wb", xTk], writes=[bkk])
                    eng = "act" if chc % 2 == 0 else "dve"
                    src = bank[bi][:].rearrange("p (k s) -> p s k", s=8)
                    dst = uT[:, chc, :, blk * 64:(blk + 1) * 64]
                    if eng == "act":
                        S.op("act", lambda e, src=src, dst=dst: e.copy(out=dst, in_=src), reads=[bkk], writes=["uT"])
                    else:
                        S.op("dve", lambda e, src=src, dst=dst: e.tensor_copy(out=dst, in_=src), reads=[bkk], writes=["uT"])
                need_kv = (q == 3) or (q == 2 and blk == 3)
                need_q = (q == 3)
                if need_kv:
                    for tt in range(4):
                        n_tile = q * 16 + blk * 4 + tt
                        kvt = n_tile - 44
                        sl = kvt % 2
                        projs = [(1, 512, 3), (2, 1024, 4)] + ([(0, 0, 5)] if need_q else [])
                        for (pi, c0, bi) in projs:
                            for dc in range(8):
                                S.op("pe", lambda e, dc=dc, bi=bi, c0=c0, tt=tt, xT=xT: e.matmul(bank[bi][:], lhsT=xT[:, dc, tt * 128:(tt + 1) * 128], rhs=Wb[:, dc, c0:c0 + 512], start=(dc == 0), stop=(dc == 7)),
                                     reads=["Wb", xTk], writes=["bk%d" % bi])
                        S.op("act", lambda e, sl=sl: e.copy(out=Vs[sl][:, :, 0:64], in_=bank[4][:].rearrange("p (h d) -> p h d", d=64)), reads=["bk4"], writes=["Vs%d" % sl])
                        if kvt < 4:
                            S.op("pool", lambda e, sl=sl, kvt=kvt: e.tensor_copy(out=Vs[sl][:, :, 64], in_=bc(hv[:, kvt:kvt + 1], [128, 8])), reads=["hv"], writes=["Vs%d" % sl])
                        else:
                            S.op("pool", lambda e, sl=sl: e.memset(Vs[sl][:, :, 64], 1.0), reads=[], writes=["Vs%d" % sl])
                        S.dma("sp", D["V_d"][kvt], Vs[sl][:].rearrange("p h d -> p (h d)"), reads=["Vs%d" % sl], writes=["V_d"])
                        for (pi, bi, gt, gkey, dstT, dkey, dram, ncol_t) in ([(1, 3, gk, "gk", kTs[sl], "kTs%d" % sl, D["kT_d"], kvt)] +
                                                                          ([(0, 5, gq, "gq", qTs[sl], "qTs%d" % sl, D["qT_d"], kvt - 4)] if need_q else [])):
                            qs = qkv[:, pi, :]; qk_ = "qkv%d" % pi
                            S.op("act", lambda e, qs=qs, bi=bi: e.copy(out=qs, in_=bank[bi][:]), reads=["bk%d" % bi], writes=[qk_])
                            S.op("pool", lambda e, qs=qs: e.tensor_tensor(out=sq[:], in0=qs, in1=qs, op=ALU.mult), reads=[qk_], writes=["sq2"])
                            S.op("dve", lambda e, pi=pi: e.tensor_reduce(out=qst[:, pi, :], in_=sq[:].rearrange("p (h d) -> p h d", d=64), axis=AX.X, op=ALU.add), reads=["sq2"], writes=["qst%d" % pi])
                            S.op("act", lambda e, pi=pi: e.activation(out=qst[:, 2 + pi, :], in_=qst[:, pi, :], func=AF.Sqrt, scale=1.0 / 64, bias=EPS), reads=["qst%d" % pi], writes=["qsq%d" % pi])
                            S.op("dve", lambda e, pi=pi: e.reciprocal(out=qst[:, 2 + pi, :], in_=qst[:, 2 + pi, :]), reads=["qsq%d" % pi], writes=["qrs%d" % pi])
                            S.op("dve", lambda e, qs=qs, pi=pi: e.tensor_tensor(out=qs.rearrange("p (h d) -> p h d", d=64), in0=qs.rearrange("p (h d) -> p h d", d=64),
                                                                        in1=bc(qst[:, 2 + pi, :].unsqueeze(2), [128, 8, 64]), op=ALU.mult), reads=[qk_, "qrs%d" % pi], writes=[qk_])
                            S.op("pool", lambda e, qs=qs, pi=pi, gt=gt: e.tensor_tensor(out=qn[:, pi, :].rearrange("p (h d) -> p h d", d=64), in0=qs.rearrange("p (h d) -> p h d", d=64),
                                                                               in1=bc(gt[:].unsqueeze(1), [128, 8, 64]), op=ALU.mult), reads=[qk_, gkey], writes=["qn%d" % pi])
                            pb = bank[6][:].bitcast(BF16).rearrange("p (c n) -> p c n", n=128)[:, 0:4, :]
                            transpose_chunks(C, qn[:, pi, :], "qn%d" % pi, 4, pb, "bk6", dstT[:], dkey, idb)
                            S.dma("sp", dram.rearrange("p (c n) -> p c n", c=4)[:, :, ncol_t * 128:(ncol_t + 1) * 128], dstT[:], reads=[dkey], writes=["qkT_d"])
            for pair in range(16):
                psl = pair % 2
                br = bank[1 + 2 * psl]; bim = bank[2 + 2 * psl]; brk = "bk%d" % (1 + 2 * psl); bik = "bk%d" % (2 + 2 * psl)
                a_ = pair // 2; wp = pair % 2; chc = a_ // 2; hb = a_ % 2; e2 = chc * 2 + wp
                rows = slice(64 * hb, 64 * hb + 64)
                for half, (bkt, bkk) in enumerate(((br, brk), (bim, bik))):
                    for s in range(8):
                        S.op("pe", lambda e, e2=e2, s=s, half=half, rows=rows, chc=chc, bkt=bkt: e.matmul(bkt[:, 0:256], lhsT=P["LT2"][rows, e2, s, half, :], rhs=uT[rows, chc, s, :], start=(s == 0), stop=(s == 7)),
                             reads=["LT2", "uT"], writes=[bkk])
                S.op("act", lambda e, pair=pair, br=br: e.copy(out=TRE[:, pair, 1:257], in_=br[:, 0:256]), reads=[brk], writes=["TRE%d" % pair])
                S.op("act", lambda e, pair=pair, bim=bim: e.copy(out=TIM[:, pair, 1:257], in_=bim[:, 0:256]), reads=[bik], writes=["TIM%d" % pair])
            if q < 3:
                for p0 in (0, 8):
                    kre = ["TRE%d" % p for p in range(p0, p0 + 8)]; kim = ["TIM%d" % p for p in range(p0, p0 + 8)]
                    src_r, src_i, srk, sik = TRE[:, p0:p0 + 8, 1:257], TIM[:, p0:p0 + 8, 1:257], kre, kim
                    bufs = [(tBr[:], tBi[:], ["tBr"], ["tBi"]), (tCr[:], tCi[:], ["tCr"], ["tCi"])]
                    for j in range(8):
                        n = 256 >> j; h = n // 2
                        wrb = bc(P["WPr"][:, j, p0:p0 + 8].unsqueeze(2), [128, 8, h]); wib = bc(P["WPi"][:, j, p0:p0 + 8].unsqueeze(2), [128, 8, h])
                        sre, sro = src_r[:, :, 0:n:2], src_r[:, :, 1:n:2]; sie, sio = src_i[:, :, 0:n:2], src_i[:, :, 1:n:2]
                        if j == 7:
                            dr, di, drk, dik = REDr[:, p0:p0 + 8].unsqueeze(2), REDi[:, p0:p0 + 8].unsqueeze(2), ["REDr%d" % p0], ["REDi%d" % p0]
                        else:
                            bb = bufs[j % 2]
                            dr, di, drk, dik = bb[0][:, :, 0:h], bb[1][:, :, 0:h], bb[2], bb[3]
                        t1v, t2v, t3v, t4v = tt1[:, :, 0:h], tt2[:, :, 0:h], tt3[:, :, 0:h], tt4[:, :, 0:h]
                        S.op("dve", lambda e, t1v=t1v, sre=sre, wrb=wrb: e.tensor_tensor(out=t1v, in0=sre, in1=wrb, op=ALU.mult), reads=srk + ["WPr"], writes=["tt1"])
                        S.op("pool", lambda e, t2v=t2v, sie=sie, wib=wib: e.tensor_tensor(out=t2v, in0=sie, in1=wib, op=ALU.mult), reads=sik + ["WPi"], writes=["tt2"])
                        S.op("pool", lambda e, t3v=t3v, sie=sie, wrb=wrb: e.tensor_tensor(out=t3v, in0=sie, in1=wrb, op=ALU.mult), reads=sik + ["WPr"], writes=["tt3"])
                        S.op("dve", lambda e, t4v=t4v, sre=sre, wib=wib: e.tensor_tensor(out=t4v, in0=sre, in1=wib, op=ALU.mult), reads=srk + ["WPi"], writes=["tt4"])
                        S.op("dve", lambda e, t1v=t1v, t2v=t2v: e.tensor_tensor(out=t1v, in0=t1v, in1=t2v, op=ALU.subtract), reads=["tt1", "tt2"], writes=["tt1"])
                        S.op("pool", lambda e, t3v=t3v, t4v=t4v: e.tensor_tensor(out=t3v, in0=t3v, in1=t4v, op=ALU.add), reads=["tt3", "tt4"], writes=["tt3"])
                        S.op("dve", lambda e, dr=dr, t1v=t1v, sro=sro: e.tensor_tensor(out=dr, in0=t1v, in1=sro, op=ALU.add), reads=["tt1"] + srk, writes=drk)
                        S.op("pool", lambda e, di=di, t3v=t3v, sio=sio: e.tensor_tensor(out=di, in0=t3v, in1=sio, op=ALU.add), reads=["tt3"] + sik, writes=dik)
                        if j < 7:
                            src_r, src_i, srk, sik = bb[0], bb[1], bb[2], bb[3]
            if q < 3:
                w8r = P["WPr"][:, 8, :]; w8i = P["WPi"][:, 8, :]
                S.op("dve", lambda e: e.tensor_tensor(out=c1[:], in0=w8r, in1=carry_r[:], op=ALU.mult), reads=["WPr", "carry_r"], writes=["c1"])
                S.op("dve", lambda e: e.tensor_tensor(out=c2[:], in0=w8i, in1=carry_i[:], op=ALU.mult), reads=["WPi", "carry_i"], writes=["c2"])
                S.op("dve", lambda e: e.tensor_tensor(out=c1[:], in0=c1[:], in1=c2[:], op=ALU.subtract), reads=["c1", "c2"], writes=["c1"])
                S.op("dve", lambda e: e.tensor_tensor(out=c1[:], in0=c1[:], in1=REDr[:], op=ALU.add), reads=["c1", "REDr0", "REDr8"], writes=["c1"])
                S.op("dve", lambda e: e.tensor_tensor(out=c2[:], in0=w8r, in1=carry_i[:], op=ALU.mult), reads=["WPr", "carry_i", "c1"], writes=["c2"])
                S.op("dve", lambda e: e.tensor_tensor(out=c3[:], in0=w8i, in1=carry_r[:], op=ALU.mult), reads=["WPi", "carry_r"], writes=["c3"])
                S.op("dve", lambda e: e.tensor_tensor(out=c2[:], in0=c2[:], in1=c3[:], op=ALU.add), reads=["c2", "c3"], writes=["c2"])
                S.op("dve", lambda e: e.tensor_tensor(out=carry_i[:], in0=c2[:], in1=REDi[:], op=ALU.add), reads=["c2", "REDi0", "REDi8"], writes=["carry_i"])
                S.op("dve", lambda e: e.tensor_copy(out=carry_r[:], in_=c1[:]), reads=["c1"], writes=["carry_r"])


def _s5_scan_out(C, es, D, P, idb, TRE, TIM, uT, ytm, carry_r, carry_i, dbg):
    S = C.S
    sb = lambda name, shape, dt=F32: C.sb(es, name, shape, dt)
    Mz2 = sb("Mz2s", [128, 16, 8, 128], BF16); CAA = sb("CAA", [128, 2, 32, 128], BF16)
    S.dma("sp", Mz2[:].rearrange("p a b c -> p (a b c)"), D["Mz_d"], reads=["Mz_d"], writes=["Mz2s"])
    S.dma("sp", CAA[:].rearrange("p a g c -> p a (g c)"), D["CA_d"], reads=["CA_d"], writes=["CAA"])
    hs1 = sb("hs1", [128, 8, 256]); hs2 = sb("hs2", [128, 8, 256]); hs3 = sb("hs3", [128, 8, 256]); hs4 = sb("hs4", [128, 8, 256])
    Tbr = [sb("Tbr%d" % i, [128, 256], BF16) for i in range(2)]; Tbi = [sb("Tbi%d" % i, [128, 256], BF16) for i in range(2)]
    Yg = [sb("Yg%d" % i, [128, 256], BF16) for i in range(2)]
    ysum = [sb("ysum%d" % i, [128, 256]) for i in range(2)]
    import os
    CUT = int(os.environ.get("SCAN_CUT", "9"))
    for p0 in (0, 8):
        kre = ["TRE%d" % p for p in range(p0, p0 + 8)]; kim = ["TIM%d" % p for p in range(p0, p0 + 8)]
        S.op("dve", lambda e, p0=p0: e.tensor_copy(out=TRE[:, p0:p0 + 8, 0:1], in_=carry_r[:, p0:p0 + 8].unsqueeze(2)), reads=["carry_r"], writes=kre)
        S.op("pool", lambda e, p0=p0: e.tensor_copy(out=TIM[:, p0:p0 + 8, 0:1], in_=carry_i[:, p0:p0 + 8].unsqueeze(2)), reads=["carry_i"], writes=kim)
        for j in range(9):
            d = 1 << j; m = 257 - d
            wrb = bc(P["WPr"][:, j, p0:p0 + 8].unsqueeze(2), [128, 8, m]); wib = bc(P["WPi"][:, j, p0:p0 + 8].unsqueeze(2), [128, 8, m])
            R0 = TRE[:, p0:p0 + 8, 0:m]; I0 = TIM[:, p0:p0 + 8, 0:m]; R1 = TRE[:, p0:p0 + 8, d:257]; I1 = TIM[:, p0:p0 + 8, d:257]
            h1, h2, h3, h4 = hs1[:, :, 0:m], hs2[:, :, 0:m], hs3[:, :, 0:m], hs4[:, :, 0:m]
            S.op("dve", lambda e, h1=h1, R0=R0, wrb=wrb: e.tensor_tensor(out=h1, in0=R0, in1=wrb, op=ALU.mult), reads=kre + ["WPr"], writes=["hs1"])
            S.op("pool", lambda e, h2=h2, I0=I0, wib=wib: e.tensor_tensor(out=h2, in0=I0, in1=wib, op=ALU.mult), reads=kim + ["WPi"], writes=["hs2"])
            S.op("pool", lambda e, h3=h3, I0=I0, wrb=wrb: e.tensor_tensor(out=h3, in0=I0, in1=wrb, op=ALU.mult), reads=kim + ["WPr"], writes=["hs3"])
            S.op("dve", lambda e, h4=h4, R0=R0, wib=wib: e.tensor_tensor(out=h4, in0=R0, in1=wib, op=ALU.mult), reads=kre + ["WPi"], writes=["hs4"])
            S.op("dve", lambda e, h1=h1, h2=h2: e.tensor_tensor(out=h1, in0=h1, in1=h2, op=ALU.subtract), reads=["hs1", "hs2"], writes=["hs1"])
            S.op("pool", lambda e, h3=h3, h4=h4: e.tensor_tensor(out=h3, in0=h3, in1=h4, op=ALU.add), reads=["hs3", "hs4"], writes=["hs3"])
            S.op("dve", lambda e, R1=R1, h1=h1: e.tensor_tensor(out=R1, in0=R1, in1=h1, op=ALU.add), reads=kre + ["hs1"], writes=kre)
            S.op("pool", lambda e, I1=I1, h3=h3: e.tensor_tensor(out=I1, in0=I1, in1=h3, op=ALU.add), reads=kim + ["hs3"], writes=kim)
    with scope(C) as esp:
        bank = [C.ps(esp, "sbk%d" % i, [128, 512]) for i in range(6)]
        for pair in range(16):
            sl = pair % 2
            kr, ki = "TRE%d" % pair, "TIM%d" % pair
            a_ = pair // 2; wp = pair % 2; chc = a_ // 2; hb = a_ % 2
            rows = slice(64 * hb, 64 * hb + 64)
            kr, ki = "TRE%d" % pair, "TIM%d" % pair
            if CUT < 2:
                continue
            S.op("act", lambda e, pair=pair, sl=sl: e.copy(out=Tbr[sl][:], in_=TRE[:, pair, 0:256]), reads=[kr], writes=["Tbr%d" % sl])
            S.op("act", lambda e, pair=pair, sl=sl: e.copy(out=Tbi[sl][:], in_=TIM[:, pair, 0:256]), reads=[ki], writes=["Tbi%d" % sl])
            for r in range(2):
                g = 4 * a_ + 2 * r + wp
                e_ = chc * 4 + (g % 4)
                pr = slice(64 * r, 64 * r + 64)
                yb = bank[r]; ybk = "sbk%d" % r
                for s in range(8):
                    S.op("pe", lambda e, e_=e_, s=s, rows=rows, chc=chc, yb=yb: e.matmul(yb[:, 0:256], lhsT=Mz2[rows, e_, s, :], rhs=uT[rows, chc, s, :], start=(s == 0), stop=(s == 7)),
                         reads=["Mz2s", "uT"], writes=[ybk])
                zb_ = bank[4 + r]; zbk = "sbk%d" % (4 + r)
                S.op("pe", lambda e, g=g, pr=pr, zb_=zb_, r=r, sl=sl: e.matmul(zb_[:, 0:256], lhsT=CAA[pr, r, g, :], rhs=Tbr[sl][pr, :], start=True, stop=False), reads=["CAA", "Tbr%d" % sl], writes=[zbk])
                S.op("pe", lambda e, g=g, pr=pr, zb_=zb_, r=r, sl=sl: e.matmul(zb_[:, 0:256], lhsT=CAA[pr, 1 - r, g, :], rhs=Tbi[sl][pr, :], start=False, stop=True), reads=["CAA", "Tbi%d" % sl], writes=[zbk])
                if CUT < 3:
                    continue
                S.op("act", lambda e, r=r, zb_=zb_: e.copy(out=ysum[r][:], in_=zb_[:, 0:256]), reads=[zbk], writes=["ysum%d" % r])
                S.op("dve", lambda e, r=r, yb=yb: e.tensor_tensor(out=ysum[r][:], in0=yb[:, 0:256], in1=ysum[r][:], op=ALU.add), reads=[ybk, "ysum%d" % r], writes=["ysum%d" % r])
                S.op("act", lambda e, r=r: e.activation(out=Yg[r][:], in_=ysum[r][:], func=AF.Gelu), reads=["ysum%d" % r], writes=["Yg%d" % r])
                if CUT < 4:
                    continue
                pT = bank[2 + r][:].bitcast(BF16).rearrange("p (c n) -> p c n", n=128)
                for kb in range(2):
                    S.op("pe", lambda e, r=r, kb=kb, pT=pT: e.transpose(out=pT[:, kb, :], in_=Yg[r][:, kb * 128:(kb + 1) * 128], identity=idb[:]), reads=["Yg%d" % r, "identb"], writes=["sbk%d" % (2 + r)])
                for kb in range(2):
                    S.op("dve", lambda e, g=g, kb=kb, pT=pT: e.tensor_copy(out=ytm[:, kb, :, 16 * g:16 * g + 16], in_=pT[:, kb, :].rearrange("p (t c) -> p t c", c=16)),
                         reads=["sbk%d" % (2 + r)], writes=["ytm"])


def _s5_glu_out(C, es, D, idb, ytm, dbg):
    S = C.S
    sb = lambda name, shape, dt=F32: C.sb(es, name, shape, dt)
    gso = sb("gso", [128, 4]); bgl = sb("bgl", [128, 512])
    S.dma("sp", gso[:], D["g_ssm_out"], writes=["gso"])
    S.dma("sp", bgl[:], D["b_glu"].partition_broadcast(128), writes=["bgl"])
    with scope(C) as est:
        Wg = load_weight_bf16(C, es, est, "Wg", D["w_glu"], 4, 512)
    with scope(C) as est:
        Wo = load_weight_bf16(C, es, est, "Wos", D["w_out"][512:1024, :], 4, 1024, gcol=(gso, "gso"))
    yT = sb("yT", [128, 4, 128], BF16); zb = sb("zb", [128, 512]); ssm = sb("ssm", [128, 512]); junk = sb("junk3", [128, 512])
    st = sb("st3", [128, 2]); sn = sb("sn", [128, 512], BF16); snT = sb("snT", [128, 4, 128], BF16)
    ho = [sb("ho%d" % i, [128, 1024]) for i in range(2)]
    hsv = D["hs_d"].rearrange("(k t) d -> t k d", t=8)
    with scope(C) as esp:
        bank = [C.ps(esp, "gbk%d" % i, [128, 512]) for i in range(5)]
        it = 0
        for kb in range(2):
            for t in range(8):
                sl = it % 2; it += 1
                y = ytm[:, kb, t, :]
                pT = bank[0][:].bitcast(BF16).rearrange("p (c n) -> p c n", n=128)[:, 0:4, :]
                transpose_chunks(C, y, "ytm", 4, pT, "gbk0", yT[:], "yT", idb)
                for c in range(4):
                    S.op("pe", lambda e, c=c: e.matmul(bank[1][:], lhsT=yT[:, c, :], rhs=Wg[:, c, :], start=(c == 0), stop=(c == 3)), reads=["yT", "Wg"], writes=["gbk1"])
                S.op("dve", lambda e: e.tensor_tensor(out=zb[:], in0=bank[1][:], in1=bgl[:], op=ALU.add), reads=["gbk1", "bgl"], writes=["zb"])
                S.op("act", lambda e: e.activation(out=zb[:], in_=zb[:], func=AF.Sigmoid), reads=["zb"], writes=["zb"])
                S.op("pool", lambda e, y=y: e.tensor_tensor(out=ssm[:], in0=y, in1=zb[:], op=ALU.mult), reads=["ytm", "zb"], writes=["ssm"])
                if "ssm" in dbg:
                    S.dma("sp", dbg["ssm"].rearrange("(k t) d -> t k d", t=8)[t, kb * 128:(kb + 1) * 128, :], ssm[:], reads=["ssm"], writes=["o_ssm"])
                rms_rstd(C, ssm[:], "ssm", junk[:], "junk3", st[:, 0:1], st[:, 1:2], "st3", 512)
                S.op("dve", lambda e: e.tensor_scalar(out=sn[:], in0=ssm[:], scalar1=st[:, 1:2], scalar2=None, op0=ALU.mult), reads=["ssm", "st3"], writes=["sn"])
                pT2 = bank[2][:].bitcast(BF16).rearrange("p (c n) -> p c n", n=128)[:, 0:4, :]
                transpose_chunks(C, sn[:], "sn", 4, pT2, "gbk2", snT[:], "snT", idb)
                for cb in range(2):
                    for c in range(4):
                        S.op("pe", lambda e, c=c, cb=cb: e.matmul(bank[3 + cb][:], lhsT=snT[:, c, :], rhs=Wo[:, c, cb * 512:(cb + 1) * 512], start=(c == 0), stop=(c == 3)), reads=["snT", "Wos"], writes=["gbk%d" % (3 + cb)])
                    S.op("act", lambda e, cb=cb, sl=sl: e.copy(out=ho[sl][:, cb * 512:(cb + 1) * 512], in_=bank[3 + cb][:]), reads=["gbk%d" % (3 + cb)], writes=["ho%d" % sl])
                S.dma("sp", hsv[t, kb * 128:(kb + 1) * 128, :], ho[sl][:], reads=["ho%d" % sl], writes=["hs_d"])


def _attention(C, es, D, idb, dbg, prep=None):
    S = C.S
    sb = lambda name, shape, dt=F32: C.sb(es, name, shape, dt)
    kT = sb("kT", [128, 4, 2560], BF16); qT = sb("qT", [128, 4, 2048], BF16); V = sb("Vall", [128, 20, 520], BF16)
    qZ = sb("qZ", [128, 8, 2048], BF16)
    S.dma("sp", kT[:].rearrange("p c n -> p (c n)"), D["kT_d"], reads=["qkT_d"], writes=["kT"])
    S.dma("sp", qT[:].rearrange("p c n -> p (c n)"), D["qT_d"], reads=["qkT_d"], writes=["qT"])
    S.dma("sp", V[:], D["V_d"].rearrange("t p n -> p t n"), reads=["V_d"], writes=["Vall"])
    S.op("pool", lambda e: e.memset(qZ[:].rearrange("p h n -> p (h n)"), 0.0), writes=["qZ"])
    for h in range(8):
        rws = slice(64 * (h % 2), 64 * (h % 2) + 64)
        S.op("dve" if h % 2 else "act", (lambda e, h=h, rws=rws: e.tensor_copy(out=qZ[rws, h, :], in_=qT[rws, h // 2, :])) if h % 2 else (lambda e, h=h, rws=rws: e.copy(out=qZ[rws, h, :], in_=qT[rws, h // 2, :])),
             reads=["qT", "qZ"], writes=["qZ"])
    BT = sb("BT", [128, 8, 5, 128], BF16)
    gao = sb("gao", [128, 4])
    S.dma("sp", gao[:], D["g_att_out"], writes=["gao"])
    with scope(C) as est:
        stg = C.sb(est, "btstg", [128, 640])
        for h in range(8):
            S.dma("sp", stg[:], D["bias_t"][:, h * 640:(h + 1) * 640], writes=["btstg"])
            S.op("dve", lambda e, h=h: e.tensor_copy(out=BT[:, h, :, :].rearrange("p j q -> p (j q)"), in_=stg[:]), reads=["btstg"], writes=["BT"])
    with scope(C) as est:
        Wo = load_weight_bf16(C, es, est, "Woa", D["w_out"][0:512, :], 4, 1024, gcol=(gao, "gao"))
    PT = [sb("PT%d" % i, [128, 5, 128], BF16) for i in range(2)]
    rd = sb("rd", [128, 8]); att = sb("att", [128, 8, 64]); junk = sb("junk4", [128, 512]); st = sb("st4", [128, 2])
    an = sb("an", [128, 512], BF16); anT = sb("anT", [128, 4, 128], BF16)
    xo = [sb("xo%d" % i, [128, 1024]) for i in range(2)]; hsl = [sb("hsl%d" % i, [128, 1024]) for i in range(2)]
    h1t = [sb("h1t%d" % i, [128, 1024]) for i in range(2)]
    xv = D["x_ext"].rearrange("(n p) d -> n p d", p=128)
    hsv = D["hs_d"].rearrange("(n p) d -> n p d", p=128)
    h1v = D["h1_d"].rearrange("(n p) d -> n p d", p=128)
    with scope(C) as esp:
        bank = [C.ps(esp, "abk%d" % i, [128, 512]) for i in range(7)]
        for qt in range(16):
            sl = qt % 2
            drip(prep, 2)
            S.dma("sp", xo[sl][:], xv[48 + qt], writes=["xo%d" % sl])
            S.dma("sp", hsl[sl][:], hsv[qt], reads=["hs_d"], writes=["hsl%d" % sl])
            for h in range(8):
                hp = h // 2; rows = slice(64 * (h % 2), 64 * (h % 2) + 64); ps_ = h % 2
                bA = bank[2 * ps_]; bB = bank[2 * ps_ + 1]; bAk = "abk%d" % (2 * ps_); bBk = "abk%d" % (2 * ps_ + 1)
                for j in range(5):
                    o = bA[:, j * 128:(j + 1) * 128] if j < 4 else bB[:, 0:128]
                    ok = bAk if j < 4 else bBk
                    S.op("pe", lambda e, o=o, h=h, hp=hp, j=j, qt=qt: e.matmul(o, lhsT=kT[:, hp, (qt + j) * 128:(qt + j + 1) * 128], rhs=qZ[:, h, qt * 128:(qt + 1) * 128], start=True, stop=False),
                         reads=["kT", "qZ"], writes=[ok])
                    S.op("pe", lambda e, o=o, h=h, j=j: e.matmul(o, lhsT=idb[:], rhs=BT[:, h, j, :], start=False, stop=True), reads=["identb", "BT"], writes=[ok])
                S.op("act", lambda e, ps_=ps_, bA=bA: e.activation(out=PT[ps_][:, 0:4, :].rearrange("p j q -> p (j q)"), in_=bA[:], func=AF.Exp), reads=[bAk], writes=["PT%d" % ps_])
                S.op("act", lambda e, ps_=ps_, bB=bB: e.activation(out=PT[ps_][:, 4, :], in_=bB[:, 0:128], func=AF.Exp), reads=[bBk], writes=["PT%d" % ps_])
                ob = bank[4 + h // 4]; obk = "abk%d" % (4 + h // 4)
                for j in range(5):
                    S.op("pe", lambda e, ob=ob, h=h, j=j, ps_=ps_, qt=qt: e.matmul(ob[:, (h % 4) * 65:(h % 4) * 65 + 65], lhsT=PT[ps_][:, j, :], rhs=V[:, qt + j, h * 65:(h + 1) * 65], start=(j == 0), stop=(j == 4)),
                         reads=["PT%d" % ps_, "Vall"], writes=[obk])
            for hb in range(2):
                ov = bank[4 + hb][:, 0:260].rearrange("p (h d) -> p h d", d=65)
                S.op("dve", lambda e, hb=hb, ov=ov: e.reciprocal(out=rd[:, hb * 4:(hb + 1) * 4], in_=ov[:, :, 64]), reads=["abk%d" % (4 + hb)], writes=["rd%d" % hb])
                S.op("dve", lambda e, hb=hb, ov=ov: e.tensor_tensor(out=att[:, hb * 4:(hb + 1) * 4, :], in0=ov[:, :, 0:64], in1=bc(rd[:, hb * 4:(hb + 1) * 4].unsqueeze(2), [128, 4, 64]), op=ALU.mult),
                     reads=["abk%d" % (4 + hb), "rd%d" % hb], writes=["att%d" % hb])
            attf = att[:].rearrange("p h d -> p (h d)")
            if "att" in dbg:
                S.dma("sp", dbg["att"].rearrange("(n p) d -> n p d", p=128)[qt], attf, reads=["att0", "att1"], writes=["o_att"])
            S.op("act", lambda e: e.activation(out=junk[:], in_=attf, func=AF.Square, accum_out=st[:, 0:1]), reads=["att0", "att1"], writes=["st4_ss"])
            S.op("act", lambda e: e.activation(out=st[:, 1:2], in_=st[:, 0:1], func=AF.Sqrt, scale=1.0 / 512, bias=EPS), reads=["st4_ss"], writes=["st4_sq"])
            S.op("dve", lambda e: e.reciprocal(out=st[:, 1:2], in_=st[:, 1:2]), reads=["st4_sq"], writes=["st4"])
            S.op("dve", lambda e: e.tensor_scalar(out=an[:], in0=attf, scalar1=st[:, 1:2], scalar2=None, op0=ALU.mult), reads=["att0", "att1", "st4"], writes=["an"])
            pT = bank[6][:].bitcast(BF16).rearrange("p (c n) -> p c n", n=128)[:, 0:4, :]
            transpose_chunks(C, an[:], "an", 4, pT, "abk6", anT[:], "anT", idb)
            for cb in range(2):
                for c in range(4):
                    S.op("pe", lambda e, c=c, cb=cb: e.matmul(bank[cb][:], lhsT=anT[:, c, :], rhs=Wo[:, c, cb * 512:(cb + 1) * 512], start=(c == 0), stop=(c == 3)), reads=["anT", "Woa"], writes=["abk%d" % cb])
                S.op("dve", lambda e, cb=cb, sl=sl: e.tensor_tensor(out=h1t[sl][:, cb * 512:(cb + 1) * 512], in0=bank[cb][:], in1=xo[sl][:, cb * 512:(cb + 1) * 512], op=ALU.add),
                     reads=["abk%d" % cb, "xo%d" % sl], writes=["h1t%d" % sl])
            S.op("pool", lambda e, sl=sl: e.tensor_tensor(out=h1t[sl][:], in0=h1t[sl][:], in1=hsl[sl][:], op=ALU.add), reads=["h1t%d" % sl, "hsl%d" % sl], writes=["h1t%d" % sl])
            S.dma("sp", h1v[qt], h1t[sl][:], reads=["h1t%d" % sl], writes=["h1_d"])


def _col(g, n):
    return np.ascontiguousarray(np.asarray(g, np.float32).reshape(n, 128).T)


def host_shared(inp):
    f = lambda k: np.asarray(inp[k], np.float32)[0]
    sh = {}
    sh["g_mix"] = _col(f("norm_mix_g"), 8)
    sh["w_in"] = np.ascontiguousarray(f("w_in"))
    sh["att_q_g"] = f("att_q_g").reshape(1, 64)
    sh["att_k_g"] = f("att_k_g").reshape(1, 64)
    rb = f("rel_bias")
    p = np.arange(128); j = np.arange(5); q = np.arange(128)
    kidx = j[:, None] * 128 + p[None, :]
    kc = kidx // 64; ki = kidx % 64
    qc = q // 64; qi = q % 64
    jb = kc[:, :, None] - qc[None, None, :]
    allowed = (jb >= 0) & (jb <= 8)
    kj = jb * 64 + ki[:, :, None]
    dist = 512 + qi[None, None, :] - kj
    bucket = np.clip(np.clip(dist, -63, 128) + 63, 0, 191)
    bt = np.where(allowed[None], rb[:, bucket], np.float32(NEG))
    sh["bias_t"] = np.ascontiguousarray(bt.transpose(2, 0, 1, 3).reshape(128, 8 * 5 * 128).astype(np.float32))
    dup = lambda a: np.ascontiguousarray(np.concatenate([a, a], 0).astype(np.float32))
    sh["s5_lr"] = dup(f("ssm_lam_re").T)
    sh["s5_li"] = dup(f("ssm_lam_im").T)
    sh["s5_ls"] = np.ascontiguousarray(np.broadcast_to(f("ssm_log_step")[None, :], (128, 32)).astype(np.float32))
    sg = np.ones((128, 2), np.float32); sg[:64, 0] = -1.0; sg[64:, 1] = -1.0
    sh["s5_sg"] = sg
    sh["s5_nv"] = np.ascontiguousarray(np.broadcast_to(np.arange(-7, 16, dtype=np.float32)[None, :], (128, 23)))
    bre = f("ssm_b_re").transpose(1, 0, 2).reshape(64, 512); bim = f("ssm_b_im").transpose(1, 0, 2).reshape(64, 512)
    cre = f("ssm_c_re").transpose(2, 0, 1).reshape(64, 512); cim = f("ssm_c_im").transpose(2, 0, 1).reshape(64, 512)
    sh["s5_p1b"] = np.ascontiguousarray(np.concatenate([bre, bim], 0)); sh["s5_p2b"] = np.ascontiguousarray(np.concatenate([bim, bre], 0))
    sh["s5_p1c"] = np.ascontiguousarray(np.concatenate([cre, cim], 0)); sh["s5_p2c"] = np.ascontiguousarray(np.concatenate([cim, cre], 0))
    dd = np.zeros((2, 4, 16, 4, 4, 16), np.float32)
    dsk = f("ssm_d")
    for g in range(32):
        chc = g // 8; hb = (g % 8) // 4; j4 = g % 4
        for c in range(16):
            dd[hb, j4, c, chc, j4, c] = dsk[g, c]
    sh["s5_dd"] = dd.reshape(128, 256)
    sh["g_ssm_out"] = _col(f("ssm_out_g"), 4)
    sh["g_att_out"] = _col(f("att_out_g"), 4)
    sh["b_glu"] = f("ssm_b_glu").reshape(1, 512)
    sh["w_glu"] = np.ascontiguousarray(f("ssm_w_glu"))
    sh["w_out"] = np.ascontiguousarray(f("w_out"))
    return sh


def host_core(inp, c):
    b, seg = c // 4, c % 4
    x = np.asarray(inp["x"], np.float32)
    xe = np.zeros((8192, 1024), np.float32)
    n = (seg + 1) * 2048
    xe[8192 - n:] = x[b, :n]
    hv = np.full((512,), 1.0 if seg > 0 else 0.0, np.float32)
    return {"x_ext": xe, "hvalid": np.ascontiguousarray(hv.reshape(4, 128).T)}


IN_SHAPES = {
    "x_ext": [8192, 1024], "hvalid": [128, 4], "g_mix": [128, 8], "w_in": [1024, 2048], "att_q_g": [1, 64], "att_k_g": [1, 64],
    "bias_t": [128, 5120], "s5_lr": [128, 32], "s5_li": [128, 32], "s5_ls": [128, 32], "s5_sg": [128, 2], "s5_nv": [128, 23],
    "s5_p1b": [128, 512], "s5_p2b": [128, 512], "s5_p1c": [128, 512], "s5_p2c": [128, 512], "s5_dd": [128, 256],
    "g_ssm_out": [128, 4], "g_att_out": [128, 4], "b_glu": [1, 512], "w_glu": [512, 512], "w_out": [1024, 1024],
}
IN_SHAPES_MEM = {"mem": [256, 1024], "g_mem": [128, 8], "g_memkv": [128, 8], "mem_q_g": [1, 256], "mem_k_g": [1, 256],
                 "w_mem_q": [1024, 1024], "w_mem_k": [1024, 1024], "w_mem_v": [1024, 1024], "w_mem_o": [1024, 1024]}
IN_SHAPES_PEER = {"g_peer": [1, 1024], "w_peer_q": [1024, 2048], "keysT": [128, 2048], "peer_uT": [1024, 16384], "peer_v": [16384, 1024]}
SCRATCH = {"kT_d": ([128, 4 * 2560], BF16), "qT_d": ([128, 4 * 2048], BF16), "V_d": ([20, 128, 520], BF16),
           "hs_d": ([2048, 1024], F32), "Mz_d": ([128, 16 * 8 * 128], BF16), "CA_d": ([128, 2, 4096], BF16)}
SCRATCH_PEER = {"uT_b": ([1024, 16384], BF16), "v_b": ([16384, 1024], BF16), "xnT_d": ([16, 128, 1024], BF16),
                "sc_d": ([16, 128, 2048], F32), "tau_d": ([16, 128, 8], F32)}


def host_shared_rest(inp):
    f = lambda k: np.asarray(inp[k], np.float32)[0]
    sh = {}
    sh["g_mem"] = _col(f("norm_mem_g"), 8); sh["g_memkv"] = _col(f("norm_memkv_g"), 8)
    sh["mem_q_g"] = f("mem_q_g").reshape(1, 256); sh["mem_k_g"] = f("mem_k_g").reshape(1, 256)
    for k in ("w_mem_q", "w_mem_k", "w_mem_v", "w_mem_o", "w_peer_q"):
        sh[k] = np.ascontiguousarray(f(k))
    sh["g_peer"] = f("norm_peer_g").reshape(1, 1024)
    sh["keysT"] = np.ascontiguousarray(f("peer_keys").transpose(3, 0, 1, 2).reshape(128, 2048))
    sh["peer_uT"] = np.ascontiguousarray(f("peer_u").T)
    sh["peer_v"] = np.ascontiguousarray(f("peer_v"))
    return sh


def build_program(stages=("mixer", "mem", "peer"), dbg_specs=None, upto=9):
    nc = bass.Bass("TRN2", target_bir_lowering=False)
    D = {}
    shapes = {}
    if "mixer" in stages:
        shapes.update(IN_SHAPES)
    if "mem" in stages:
        shapes.update(IN_SHAPES_MEM)
    if "peer" in stages:
        shapes.update(IN_SHAPES_PEER)
    for k, shp in shapes.items():
        D[k] = nc.dram_tensor(k, shp, F32, kind="ExternalInput").ap()
    scr = {}
    if "mixer" in stages:
        scr.update(SCRATCH)
    if "peer" in stages:
        scr.update(SCRATCH_PEER)
    for k, (shp, dt) in scr.items():
        D[k] = nc.dram_tensor(k, shp, dt).ap()
    chain = ["h1_d", "h2_d", "out"]
    first = {"mixer": None, "mem": "h1_d", "peer": "h2_d"}[stages[0]]
    last = {"mixer": "h1_d", "mem": "h2_d", "peer": "out"}[stages[-1]]
    for k in chain:
        if k == first:
            D[k] = nc.dram_tensor(k, [2048, 1024], F32, kind="ExternalInput").ap()
        elif k == last:
            D[k] = nc.dram_tensor(k, [2048, 1024], F32, kind="ExternalOutput").ap()
        else:
            D[k] = nc.dram_tensor(k, [2048, 1024], F32).ap()
    dbg = {}
    for k, shp in (dbg_specs or {}).items():
        dbg[k] = nc.dram_tensor("dbg_" + k, shp, F32, kind="ExternalOutput").ap()
    with ExitStack() as es:
        S = Sched(nc, es)
        C = Ctx(nc, S)
        prep = stage_peer_prep(C, D) if "peer" in stages else None
        if "mixer" in stages:
            stage_mixer(C, D, dbg, upto, prep=prep)
        if "mem" in stages:
            stage_mem(C, D, dbg, prep=prep)
        drip(prep, 1000)
        if "peer" in stages:
            stage_peer(C, D, dbg)
        S.barrier()
        S.emit()
    return nc, S


def _headnorm(C, src_ap, skey, nh, hd, sqt, sqk, stat, stk, gt, gkey, dst_ap, dkey):
    S = C.S
    sv = src_ap.rearrange("p (h d) -> p h d", d=hd)
    S.op("pool", lambda e: e.tensor_tensor(out=sqt, in0=src_ap, in1=src_ap, op=ALU.mult), reads=[skey], writes=[sqk])
    S.op("dve", lambda e: e.tensor_reduce(out=stat[:, 0:nh], in_=sqt.rearrange("p (h d) -> p h d", d=hd), axis=AX.X, op=ALU.add), reads=[sqk], writes=[stk + "a"])
    S.op("act", lambda e: e.activation(out=stat[:, nh:2 * nh], in_=stat[:, 0:nh], func=AF.Sqrt, scale=1.0 / hd, bias=EPS), reads=[stk + "a"], writes=[stk + "b"])
    S.op("dve", lambda e: e.reciprocal(out=stat[:, nh:2 * nh], in_=stat[:, nh:2 * nh]), reads=[stk + "b"], writes=[stk])
    S.op("dve", lambda e: e.tensor_tensor(out=sv, in0=sv, in1=bc(stat[:, nh:2 * nh].unsqueeze(2), [128, nh, hd]), op=ALU.mult), reads=[skey, stk], writes=[skey])
    S.op("pool", lambda e: e.tensor_tensor(out=dst_ap.rearrange("p (h d) -> p h d", d=hd), in0=sv, in1=bc(gt.unsqueeze(1), [128, nh, hd]), op=ALU.mult), reads=[skey, gkey], writes=[dkey])


def stage_mem(C, D, dbg=None, prep=None):
    S = C.S
    dbg = dbg or {}
    with scope(C) as es:
        sb = lambda name, shape, dt=F32: C.sb(es, name, shape, dt)
        idf, idb = make_ident(C, es, "ident")
        gm = sb("gm", [128, 8]); gkv = sb("gkv", [128, 8]); gq = sb("mgq", [128, 256]); gk = sb("mgk", [128, 256])
        S.dma("sp", gm[:], D["g_mem"], writes=["gm"]); S.dma("sp", gkv[:], D["g_memkv"], writes=["gkv"])
        S.dma("sp", gq[:], D["mem_q_g"].partition_broadcast(128), writes=["mgq"]); S.dma("sp", gk[:], D["mem_k_g"].partition_broadcast(128), writes=["mgk"])
        S.op("dve", lambda e: e.tensor_scalar(out=gq[:], in0=gq[:], scalar1=1.0 / 16, scalar2=None, op0=ALU.mult), reads=["mgq"], writes=["mgq"])
        kTm = sb("kTm", [128, 8, 256], BF16); Vm = sb("Vm", [128, 2, 4, 257], BF16)
        xt = [sb("mxt%d" % i, [128, 1024]) for i in range(2)]; st = sb("mst", [128, 2]); xn = sb("mxn", [128, 1024], BF16)
        xnT = sb("mxnT", [128, 8, 128], BF16); qf = sb("mqf", [128, 1024]); sq = sb("msq", [128, 1024]); qst = sb("mqst", [128, 8])
        qn = sb("mqn", [128, 1024], BF16); mjunk = sb("mjunk", [128, 1024], BF16)
        with scope(C) as esk:
            with scope(C) as est:
                Wk = load_weight_bf16(C, esk, est, "Wmk", D["w_mem_k"], 8, 1024, gcol=(gkv, "gkv"))
            with scope(C) as est:
                Wv = load_weight_bf16(C, esk, est, "Wmv", D["w_mem_v"], 8, 1024, gcol=(gkv, "gkv"))
            with scope(C) as esp:
                bank = [C.ps(esp, "mkb%d" % i, [128, 512]) for i in range(6)]
                mv = D["mem"].rearrange("(n p) d -> n p d", p=128)
                for mt in range(2):
                    x_ = xt[mt]; xk = "mxt%d" % mt
                    S.dma("sp", x_[:], mv[mt], writes=[xk])
                    rms_rstd(C, x_[:], xk, mjunk[:], "mjunk", st[:, 0:1], st[:, 1:2], "mst", 1024)
                    S.op("dve", lambda e, x_=x_: e.tensor_scalar(out=xn[:], in0=x_[:], scalar1=st[:, 1:2], scalar2=None, op0=ALU.mult), reads=[xk, "mst"], writes=["mxn"])
                    pb = bank[0][:].bitcast(BF16).rearrange("p (c n) -> p c n", n=128)
                    transpose_chunks(C, xn[:], "mxn", 8, pb, "mkb0", xnT[:], "mxnT", idb)
                    for (W, wk, b0) in ((Wk, "Wmk", 1), (Wv, "Wmv", 3)):
                        for cb in range(2):
                            for dc in range(8):
                                S.op("pe", lambda e, W=W, cb=cb, dc=dc, b0=b0: e.matmul(bank[b0 + cb][:], lhsT=xnT[:, dc, :], rhs=W[:, dc, cb * 512:(cb + 1) * 512], start=(dc == 0), stop=(dc == 7)),
                                     reads=["mxnT", wk], writes=["mkb%d" % (b0 + cb)])
                    for cb in range(2):
                        S.op("act", lambda e, cb=cb: e.copy(out=qf[:, cb * 512:(cb + 1) * 512], in_=bank[1 + cb][:]), reads=["mkb%d" % (1 + cb)], writes=["mqf"])
                        S.op("dve", lambda e, cb=cb, mt=mt: e.tensor_copy(out=Vm[:, mt, 2 * cb:2 * cb + 2, 0:256], in_=bank[3 + cb][:].rearrange("p (h d) -> p h d", d=256)), reads=["mkb%d" % (3 + cb)], writes=["Vm"])
                    S.op("pool", lambda e, mt=mt: e.memset(Vm[:, mt, :, 256], 1.0), reads=[], writes=["Vm"])
                    _headnorm(C, qf[:], "mqf", 4, 256, sq[:], "msq", qst, "mqst", gk[:], "mgk", qn[:], "mqn")
                    pb2 = bank[5][:].bitcast(BF16).rearrange("p (c n) -> p c n", n=128)
                    transpose_chunks(C, qn[:], "mqn", 8, pb2, "mkb5", kTm[:, :, mt * 128:(mt + 1) * 128], "kTm", idb)
        with scope(C) as est:
            Wq = load_weight_bf16(C, es, est, "Wmq", D["w_mem_q"], 8, 1024, gcol=(gm, "gm"))
        with scope(C) as est:
            Wo = load_weight_bf16(C, es, est, "Wmo", D["w_mem_o"], 8, 1024)
        qT = sb("mqT", [128, 8, 128], BF16); PT = [sb("mPT%d" % i, [128, 2, 128], BF16) for i in range(2)]
        rd = sb("mrd", [128, 4]); ob = sb("mob", [128, 1024], BF16); oT = sb("moT", [128, 8, 128], BF16)
        h2t = [sb("h2t%d" % i, [128, 1024]) for i in range(2)]
        hv = D["h1_d"].rearrange("(n p) d -> n p d", p=128); ov = D["h2_d"].rearrange("(n p) d -> n p d", p=128)
        with scope(C) as esp:
            bank = [C.ps(esp, "mb%d" % i, [128, 512]) for i in range(8)]
            for tt in range(16):
                sl = tt % 2
                drip(prep, 1)
                x_ = xt[sl]; xk = "mxt%d" % sl
                S.dma("sp", x_[:], hv[tt], reads=["h1_d"], writes=[xk])
                rms_rstd(C, x_[:], xk, mjunk[:], "mjunk", st[:, 0:1], st[:, 1:2], "mst", 1024)
                S.op("dve", lambda e, x_=x_: e.tensor_scalar(out=xn[:], in0=x_[:], scalar1=st[:, 1:2], scalar2=None, op0=ALU.mult), reads=[xk, "mst"], writes=["mxn"])
                pb = bank[0][:].bitcast(BF16).rearrange("p (c n) -> p c n", n=128)
                transpose_chunks(C, xn[:], "mxn", 8, pb, "mb0", xnT[:], "mxnT", idb)
                for cb in range(2):
                    for dc in range(8):
                        S.op("pe", lambda e, cb=cb, dc=dc: e.matmul(bank[1 + cb][:], lhsT=xnT[:, dc, :], rhs=Wq[:, dc, cb * 512:(cb + 1) * 512], start=(dc == 0), stop=(dc == 7)),
                             reads=["mxnT", "Wmq"], writes=["mb%d" % (1 + cb)])
                    S.op("act", lambda e, cb=cb: e.copy(out=qf[:, cb * 512:(cb + 1) * 512], in_=bank[1 + cb][:]), reads=["mb%d" % (1 + cb)], writes=["mqf"])
                _headnorm(C, qf[:], "mqf", 4, 256, sq[:], "msq", qst, "mqst", gq[:], "mgq", qn[:], "mqn")
                pb2 = bank[3][:].bitcast(BF16).rearrange("p (c n) -> p c n", n=128)
                transpose_chunks(C, qn[:], "mqn", 8, pb2, "mb3", qT[:], "mqT", idb)
                for h in range(4):
                    ps_ = h % 2
                    sbk = bank[4 + ps_]; sbkk = "mb%d" % (4 + ps_)
                    for mt in range(2):
                        for dh in range(2):
                            S.op("pe", lambda e, h=h, mt=mt, dh=dh, sbk=sbk: e.matmul(sbk[:, mt * 128:(mt + 1) * 128], lhsT=kTm[:, 2 * h + dh, mt * 128:(mt + 1) * 128], rhs=qT[:, 2 * h + dh, :], start=(dh == 0), stop=(dh == 1)),
                                 reads=["kTm", "mqT"], writes=[sbkk])
                    S.op("act", lambda e, ps_=ps_, sbk=sbk: e.activation(out=PT[ps_][:].rearrange("p m q -> p (m q)"), in_=sbk[:, 0:256], func=AF.Exp, bias=-8.0), reads=[sbkk], writes=["mPT%d" % ps_])
                    obk = bank[6 + ps_]; obkk = "mb%d" % (6 + ps_)
                    for mt in range(2):
                        S.op("pe", lambda e, h=h, mt=mt, ps_=ps_, obk=obk: e.matmul(obk[:, 0:257], lhsT=PT[ps_][:, mt, :], rhs=Vm[:, mt, h, :], start=(mt == 0), stop=(mt == 1)), reads=["mPT%d" % ps_, "Vm"], writes=[obkk])
                    S.op("dve", lambda e, h=h, obk=obk: e.reciprocal(out=rd[:, h:h + 1], in_=obk[:, 256:257]), reads=[obkk], writes=["mrd%d" % h])
                    S.op("dve", lambda e, h=h, obk=obk: e.tensor_scalar(out=ob[:, h * 256:(h + 1) * 256], in0=obk[:, 0:256], scalar1=rd[:, h:h + 1], scalar2=None, op0=ALU.mult), reads=[obkk, "mrd%d" % h], writes=["mob"])
                pb3 = bank[0][:].bitcast(BF16).rearrange("p (c n) -> p c n", n=128)
                transpose_chunks(C, ob[:], "mob", 8, pb3, "mb0", oT[:], "moT", idb)
                for cb in range(2):
                    for dc in range(8):
                        S.op("pe", lambda e, cb=cb, dc=dc: e.matmul(bank[1 + cb][:], lhsT=oT[:, dc, :], rhs=Wo[:, dc, cb * 512:(cb + 1) * 512], start=(dc == 0), stop=(dc == 7)),
                             reads=["moT", "Wmo"], writes=["mb%d" % (1 + cb)])
                    S.op("dve", lambda e, cb=cb, sl=sl, x_=x_: e.tensor_tensor(out=h2t[sl][:, cb * 512:(cb + 1) * 512], in0=bank[1 + cb][:], in1=x_[:, cb * 512:(cb + 1) * 512], op=ALU.add),
                         reads=["mb%d" % (1 + cb), xk], writes=["h2t%d" % sl])
                S.dma("sp", ov[tt], h2t[sl][:], reads=["h2t%d" % sl], writes=["h2_d"])


def stage_peer_prep(C, D):
    S = C.S
    uv = D["peer_uT"].rearrange("(c p) (a e) -> c p a e", p=128, e=2048)
    ub = D["uT_b"].rearrange("(c p) (a e) -> c p a e", p=128, e=2048)
    vv = D["peer_v"].rearrange("(c p) d -> c p d", p=512)
    vb = D["v_b"].rearrange("(c p) d -> c p d", p=512)

    def gen():
        for c in range(8):
            S.dma("pool", ub[c], uv[c], writes=["uT_b"])
            yield
        for c in range(32):
            S.dma("pool", vb[c], vv[c], writes=["v_b"])
            yield
    return gen()


def drip(g, n):
    if g is None:
        return
    for _ in range(n):
        try:
            next(g)
        except StopIteration:
            return


def _top16(C, src, skey, work, wkey, dst, dkey):
    S = C.S
    S.op("dve", lambda e: e.max(out=dst[:, 0:8], in_=src), reads=[skey], writes=[dkey])
    S.op("dve", lambda e: e.match_replace(out=work, in_to_replace=dst[:, 0:8], in_values=src, imm_value=-1e30), reads=[skey, dkey], writes=[wkey])
    S.op("dve", lambda e: e.max(out=dst[:, 8:16], in_=work), reads=[wkey], writes=[dkey])


def stage_peer(C, D, dbg=None):
    S = C.S
    dbg = dbg or {}
    hv = D["h2_d"].rearrange("(n p) d -> n p d", p=128)
    with scope(C) as es:
        sb = lambda name, shape, dt=F32: C.sb(es, name, shape, dt)
        idf, idb = make_ident(C, es, "ident")
        gp = sb("gpb", [128, 1024])
        S.dma("sp", gp[:], D["g_peer"].partition_broadcast(128), writes=["gpb"])
        with scope(C) as est:
            Wq = load_weight_bf16(C, es, est, "Wpq", D["w_peer_q"], 8, 2048)
        keyT = sb("keyT", [128, 16, 128], BF16)
        with scope(C) as est:
            kst = C.sb(est, "kst", [128, 2048])
            S.dma("sp", kst[:], D["keysT"], writes=["kst"])
            S.op("dve", lambda e: e.tensor_copy(out=keyT[:].rearrange("p a n -> p (a n)"), in_=kst[:]), reads=["kst"], writes=["keyT"])
        NS = 3
        xt = [sb("pxt%d" % i, [128, 1024]) for i in range(NS)]; st_ = [sb("pst%d" % i, [128, 2]) for i in range(NS)]; junk = sb("pjunk", [128, 1024], BF16)
        xn_ = [sb("pxn%d" % i, [128, 1024], BF16) for i in range(NS)]; xnT = [sb("pxnT%d" % i, [128, 8, 128], BF16) for i in range(NS)]
        qb_ = [sb("pqb%d" % i, [128, 2048], BF16) for i in range(NS)]; qTp_ = [sb("pqT%d" % i, [128, 16, 128], BF16) for i in range(NS)]
        sc = [sb("psc%d" % i, [128, 16, 128]) for i in range(NS)]; work_ = [sb("pwork%d" % i, [128, 256]) for i in range(NS)]
        sv_ = [sb("psv%d" % i, [128, 16, 16]) for i in range(NS)]; cand_ = [sb("pcand%d" % i, [128, 8, 256]) for i in range(NS)]
        cex_ = [sb("pcex%d" % i, [128, 8, 256]) for i in range(NS)]; ctop_ = [sb("pctop%d" % i, [128, 8, 16]) for i in range(NS)]
        Z_ = [sb("pZ%d" % i, [128, 8]) for i in range(NS)]; off_ = [sb("poff%d" % i, [128, 8]) for i in range(NS)]; tau = [sb("ptau%d" % i, [128, 8]) for i in range(NS)]
        cjunk_ = [sb("pcj%d" % i, [128, 256]) for i in range(NS)]; offs_ = [sb("poffs%d" % i, [128, 16]) for i in range(NS)]
        xTd = D["xnT_d"].rearrange("n p (c t) -> n p c t", t=128)
        with scope(C) as esp:
            bank = [C.ps(esp, "pab%d" % i, [128, 512]) for i in range(6)]

            def tile(tt):
                sl = tt % NS
                K = lambda nm: "%s%d" % (nm, sl)
                x_ = xt[sl]; xk = K("pxt"); st = st_[sl]; xn = xn_[sl]; qb = qb_[sl]; qTp = qTp_[sl]; work = work_[sl]
                sv = sv_[sl]; cand = cand_[sl]; cex = cex_[sl]; ctop = ctop_[sl]; Z = Z_[sl]; off = off_[sl]; cjunk = cjunk_[sl]; offs = offs_[sl]
                S.dma("sp", x_[:], hv[tt], reads=["h2_d"], writes=[xk])
                rms_rstd(C, x_[:], xk, junk[:], "pjunk", st[:, 0:1], st[:, 1:2], K("pst"), 1024)
                S.op("dve", lambda e: e.scalar_tensor_tensor(out=xn[:], in0=x_[:], scalar=st[:, 1:2], in1=gp[:], op0=ALU.mult, op1=ALU.mult), reads=[xk, K("pst"), "gpb"], writes=[K("pxn")])
                pb = bank[0][:].bitcast(BF16).rearrange("p (c n) -> p c n", n=128)
                transpose_chunks(C, xn[:], K("pxn"), 8, pb, "pab0", xnT[sl][:], K("pxnT"), idb)
                S.dma("sp", xTd[tt], xnT[sl][:], reads=[K("pxnT")], writes=["xnT_d"])
                yield
                for cb in range(4):
                    for dc in range(8):
                        S.op("pe", lambda e, cb=cb, dc=dc: e.matmul(bank[1 + cb][:], lhsT=xnT[sl][:, dc, :], rhs=Wq[:, dc, cb * 512:(cb + 1) * 512], start=(dc == 0), stop=(dc == 7)),
                             reads=[K("pxnT"), "Wpq"], writes=["pab%d" % (1 + cb)])
                    S.op("act", lambda e, cb=cb: e.copy(out=qb[:, cb * 512:(cb + 1) * 512], in_=bank[1 + cb][:]), reads=["pab%d" % (1 + cb)], writes=[K("pqb")])
                for half in range(2):
                    pbq = bank[5][:].bitcast(BF16).rearrange("p (c n) -> p c n", n=128)
                    transpose_chunks(C, qb[:, half * 1024:(half + 1) * 1024], K("pqb"), 8, pbq, "pab5", qTp[:, half * 8:(half + 1) * 8, :], K("pqT"), idb)
                for hh in range(16):
                    S.op("pe", lambda e, hh=hh: e.matmul(bank[1 + hh // 4][:, (hh % 4) * 128:(hh % 4 + 1) * 128], lhsT=qTp[:, hh, :], rhs=keyT[:, hh, :], start=True, stop=True),
                         reads=[K("pqT"), "keyT"], writes=["pab%d" % (1 + hh // 4)])
                scs = sc[sl]; sck = K("psc")
                for cb in range(4):
                    S.op("act", lambda e, cb=cb: e.copy(out=scs[:, cb * 4:(cb + 1) * 4, :].rearrange("p a n -> p (a n)"), in_=bank[1 + cb][:]), reads=["pab%d" % (1 + cb)], writes=[sck])
                yield
                for hh in range(16):
                    _top16(C, scs[:, hh, :], sck, work[:, 0:128], K("pwork"), sv[:, hh, :], K("psv"))
                    yield
                svv = sv[:].rearrange("p (h s) k -> p h s k", s=2)
                for h in range(8):
                    S.op("dve", lambda e, h=h: e.tensor_tensor(out=cand[:, h, :].rearrange("p (a b) -> p a b", b=16), in0=bc(svv[:, h, 0, :].unsqueeze(2), [128, 16, 16]),
                                                            in1=bc(svv[:, h, 1, :].unsqueeze(1), [128, 16, 16]), op=ALU.add), reads=[K("psv")], writes=[K("pcand") + "_%d" % h])
                    yield
                ck = [K("pcand") + "_%d" % h for h in range(8)]
                for h in range(8):
                    _top16(C, cand[:, h, :], ck[h], work[:], K("pwork"), ctop[:, h, :], K("pctop"))
                    yield
                S.op("dve", lambda e: e.tensor_tensor(out=cand[:], in0=cand[:], in1=bc(ctop[:, :, 0:1], [128, 8, 256]), op=ALU.subtract), reads=ck + [K("pctop")], writes=ck)
                S.op("act", lambda e: e.activation(out=cex[:].rearrange("p h n -> p (h n)"), in_=cand[:].rearrange("p h n -> p (h n)"), func=AF.Exp), reads=ck, writes=[K("pcex")])
                yield
                S.op("dve", lambda e: e.tensor_tensor(out=tau[sl][:], in0=ctop[:, :, 15], in1=ctop[:, :, 0], op=ALU.subtract), reads=[K("pctop")], writes=[K("ptau")])
                yield
                S.op("dve", lambda e: e.tensor_scalar(out=tau[sl][:], in0=tau[sl][:], scalar1=-1e-5, scalar2=None, op0=ALU.add), reads=[K("ptau")], writes=[K("ptau")])
                yield
                for h in range(8):
                    S.op("dve", lambda e, h=h: e.scalar_tensor_tensor(out=cjunk[:], in0=cand[:, h, :], scalar=tau[sl][:, h:h + 1], in1=cex[:, h, :], op0=ALU.is_ge, op1=ALU.mult, accum_out=Z[:, h:h + 1]),
                         reads=ck + [K("pcex"), K("ptau")], writes=[K("pZ") + "_%d" % h])
                    yield
                S.op("act", lambda e: e.activation(out=off[:], in_=Z[:], func=AF.Ln), reads=[K("pZ") + "_%d" % h for h in range(8)], writes=[K("poff")])
                yield
                S.op("dve", lambda e: e.tensor_tensor(out=tau[sl][:], in0=tau[sl][:], in1=off[:], op=ALU.subtract), reads=[K("ptau"), K("poff")], writes=[K("ptau")])
                S.op("act", lambda e: e.activation(out=tau[sl][:], in_=tau[sl][:], func=AF.Exp), reads=[K("ptau")], writes=[K("ptau")])
                yield
                S.op("dve", lambda e: e.tensor_scalar(out=tau[sl][:], in0=tau[sl][:], scalar1=0.99997, scalar2=None, op0=ALU.mult), reads=[K("ptau")], writes=[K("ptau")])
                offv = offs[:].rearrange("p (h s) -> p h s", s=2)
                S.op("dve", lambda e: e.tensor_copy(out=offv[:, :, 0], in_=svv[:, :, 0, 0]), reads=[K("psv")], writes=[K("poffs") + "a"])
                yield
                S.op("dve", lambda e: e.tensor_tensor(out=offv[:, :, 1], in0=svv[:, :, 1, 0], in1=off[:], op=ALU.add), reads=[K("psv"), K("poff")], writes=[K("poffs") + "b"])
                yield
                S.op("dve", lambda e: e.tensor_tensor(out=scs[:], in0=scs[:], in1=bc(offs[:].unsqueeze(2), [128, 16, 128]), op=ALU.subtract), reads=[sck, K("poffs") + "a", K("poffs") + "b"], writes=[sck])
                S.op("act", lambda e: e.activation(out=scs[:].rearrange("p a n -> p (a n)"), in_=scs[:].rearrange("p a n -> p (a n)"), func=AF.Exp), reads=[sck], writes=[sck])
                S.dma("sp", D["sc_d"][tt], scs[:].rearrange("p a n -> p (a n)"), reads=[sck], writes=["sc_d"])
                S.dma("sp", D["tau_d"][tt], tau[sl][:], reads=[K("ptau")], writes=["tau_d"])

            from itertools import zip_longest
            for t0 in range(0, 16, NS):
                for _ in zip_longest(*[tile(t0 + i) for i in range(NS) if t0 + i < 16]):
                    pass
    with scope(C) as es:
        sb = lambda name, shape, dt=F32: C.sb(es, name, shape, dt)
        idf, idb = make_ident(C, es, "ident")
        xnT = sb("bxnT", [128, 4, 1024], BF16); sc = sb("bsc", [128, 4, 2048]); kap = sb("btau", [128, 4, 8])
        UT = [sb("UT%d" % i, [128, 8, 1024], BF16) for i in range(2)]; Vb = [sb("Vb%d" % i, [128, 8, 1024], BF16) for i in range(2)]
        acc = sb("pacc", [128, 4, 1024])
        NP = 12
        Pt = [sb("pP%d" % i, [128, 512]) for i in range(NP)]; Wh = [[sb("pWh%d_%d" % (i, h), [128, 512], BF16) for h in range(8)] for i in range(2)]
        G = [sb("pG%d" % i, [128, 512], BF16) for i in range(3)]; WA = [sb("pWA%d" % i, [128, 512], BF16) for i in range(2)]
        WAT = [sb("pWAT%d" % i, [128, 4, 128], BF16) for i in range(2)]
        h2t = [sb("ph2t%d" % i, [128, 1024]) for i in range(2)]
        uTv = D["uT_b"].rearrange("(c p) (b e) -> b p c e", p=128, e=1024)
        vbv = D["v_b"].rearrange("(b c p) d -> b p c d", p=128, c=8)
        ov = D["out"].rearrange("(n p) d -> n p d", p=128)
        with scope(C) as esp:
            bank = [C.ps(esp, "pbb%d" % i, [128, 512]) for i in range(7)]
            state = {"it": 0}

            def stage1a(u, tg, eb, sub, tt, es_):
                ub = u % 2
                xT = xnT[:, tt, :].rearrange("p (c t) -> p c t", t=128)
                scv = sc[:, tt, :].rearrange("p (h s n) -> p h s n", s=2, n=128)
                i0 = eb * 8 + sub * 4
                for dc in range(8):
                    S.op("pe", lambda e, dc=dc, xT=xT: e.matmul(bank[ub][:], lhsT=xT[:, dc, :], rhs=UT[es_][:, dc, sub * 512:(sub + 1) * 512], start=(dc == 0), stop=(dc == 7)),
                         reads=["bxnT", "UT%d" % es_], writes=["pbb%d" % ub])
                for h in range(8):
                    hs = state["it"] % NP; state["it"] += 1
                    if h >= 5:
                        S.op("pool", lambda e, h=h, hs=hs, scv=scv: e.tensor_tensor(out=Pt[hs][:].rearrange("p (i j) -> p i j", j=128), in0=bc(scv[:, h, 0, i0:i0 + 4].unsqueeze(2), [128, 4, 128]),
                                                                             in1=bc(scv[:, h, 1, :].unsqueeze(1), [128, 4, 128]), op=ALU.mult), reads=["bsc"], writes=["pP%d_%d" % (hs, il) for il in range(4)])
                    else:
                        for il in range(4):
                            S.op("act", lambda e, h=h, hs=hs, scv=scv, il=il: e.activation(out=Pt[hs][:, il * 128:(il + 1) * 128], in_=scv[:, h, 1, :], func=AF.Copy, scale=scv[:, h, 0, i0 + il:i0 + il + 1]),
                                 reads=["bsc"], writes=["pP%d_%d" % (hs, il)])
                    S.op("dve", lambda e, h=h, hs=hs: e.scalar_tensor_tensor(out=Wh[ub][h][:], in0=Pt[hs][:], scalar=kap[:, tt, h:h + 1], in1=Pt[hs][:], op0=ALU.is_ge, op1=ALU.mult),
                         reads=["pP%d_%d" % (hs, il) for il in range(4)] + ["btau"], writes=["pWh%d_%d" % (ub, h)])

            def st_gelu(u, tg, eb, sub, tt, es_):
                ub = u % 2; gb = u % 3
                S.op("act", lambda e: e.activation(out=G[gb][:], in_=bank[ub][:], func=AF.Gelu), reads=["pbb%d" % ub], writes=["pG%d" % gb])

            def st_hs(u, tg, eb, sub, tt, es_):
                ub = u % 2
                for h in range(8):
                    S.op("pe", lambda e, h=h: e.matmul(bank[2 + ub][:], lhsT=idb[:], rhs=Wh[ub][h][:], start=(h == 0), stop=(h == 7)),
                         reads=["identb", "pWh%d_%d" % (ub, h)], writes=["pbb%d" % (2 + ub)])

            def st_wa(u, tg, eb, sub, tt, es_):
                ub = u % 2; gb = u % 3
                S.op("dve", lambda e: e.tensor_tensor(out=WA[ub][:], in0=bank[2 + ub][:], in1=G[gb][:], op=ALU.mult), reads=["pbb%d" % (2 + ub), "pG%d" % gb], writes=["pWA%d" % ub])

            def st_t(u, tg, eb, sub, tt, es_):
                ub = u % 2
                pbt = bank[4][:].bitcast(BF16).rearrange("p (c n) -> p c n", n=128)[:, 0:4, :]
                for c in range(4):
                    S.op("pe", lambda e, c=c: e.transpose(out=pbt[:, c, :], in_=WA[ub][:, c * 128:(c + 1) * 128], identity=idb[:]), reads=["pWA%d" % ub, "identb"], writes=["pbb4"])

            def st_watcopy(u, tg, eb, sub, tt, es_):
                ub = u % 2
                pbt = bank[4][:].bitcast(BF16).rearrange("p (c n) -> p c n", n=128)[:, 0:4, :]
                S.op("act", lambda e: e.copy(out=WAT[ub][:], in_=pbt), reads=["pbb4"], writes=["pWAT%d" % ub])

            def st_v(u, tg, eb, sub, tt, es_):
                ub = u % 2
                for cb in range(2):
                    for ec in range(4):
                        S.op("pe", lambda e, cb=cb, ec=ec: e.matmul(bank[5 + cb][:], lhsT=WAT[ub][:, ec, :], rhs=Vb[es_][:, sub * 4 + ec, cb * 512:(cb + 1) * 512], start=(sub == 0 and ec == 0), stop=(sub == 1 and ec == 3)),
                             reads=["pWAT%d" % ub, "Vb%d" % es_], writes=["pbb%d" % (5 + cb)])

            def st_acc(u, tg, eb, sub, tt, es_):
                if sub != 1:
                    return
                for cb in range(2):
                    if eb == 0:
                        S.op("dve", lambda e, cb=cb: e.tensor_copy(out=acc[:, tt, cb * 512:(cb + 1) * 512], in_=bank[5 + cb][:]), reads=["pbb%d" % (5 + cb)], writes=["pacc%d" % tt])
                    else:
                        S.op("dve", lambda e, cb=cb: e.tensor_tensor(out=acc[:, tt, cb * 512:(cb + 1) * 512], in0=bank[5 + cb][:], in1=acc[:, tt, cb * 512:(cb + 1) * 512], op=ALU.add),
                             reads=["pbb%d" % (5 + cb), "pacc%d" % tt], writes=["pacc%d" % tt])

            u = 0
            for tg in range(4):
                S.dma("sp", xnT[:], D["xnT_d"][tg * 4:(tg + 1) * 4].rearrange("n p f -> p n f"), reads=["xnT_d"], writes=["bxnT"])
                S.dma("sp", sc[:], D["sc_d"][tg * 4:(tg + 1) * 4].rearrange("n p f -> p n f"), reads=["sc_d"], writes=["bsc"])
                S.dma("sp", kap[:], D["tau_d"][tg * 4:(tg + 1) * 4].rearrange("n p f -> p n f"), reads=["tau_d"], writes=["btau"])
                units = []
                for eb in range(16):
                    es_ = (tg * 16 + eb) % 2
                    for tt in range(4):
                        for sub in range(2):
                            units.append((u, tg, eb, sub, tt, es_)); u += 1
                n = len(units)
                U = lambda j: units[j] if 0 <= j < n else None
                for k in range(n + 3):
                    for ebk in ([0] if k == 0 else []) + ([k // 8 + 1] if (k % 8 == 3 and k // 8 + 1 < 16) else []):
                        es_ = (tg * 16 + ebk) % 2
                        S.dma("sp", UT[es_][:], uTv[ebk], reads=["uT_b"], writes=["UT%d" % es_])
                        S.dma("sp", Vb[es_][:], vbv[ebk], reads=["v_b"], writes=["Vb%d" % es_])
                    if U(k - 2): st_wa(*U(k - 2))
                    if U(k - 3): st_v(*U(k - 3))
                    if U(k - 2): st_t(*U(k - 2))
                    if U(k - 1): st_gelu(*U(k - 1))
                    if U(k - 1): st_hs(*U(k - 1))
                    if U(k): stage1a(*U(k))
                    if U(k - 2): st_watcopy(*U(k - 2))
                    if U(k - 3): st_acc(*U(k - 3))
                for tt in range(4):
                    sl = tt % 2; n = tg * 4 + tt
                    S.dma("sp", h2t[sl][:], hv[n], reads=["h2_d"], writes=["ph2t%d" % sl])
                    S.op("pool", lambda e, sl=sl, tt=tt: e.tensor_tensor(out=h2t[sl][:], in0=h2t[sl][:], in1=acc[:, tt, :], op=ALU.add), reads=["ph2t%d" % sl, "pacc%d" % tt], writes=["ph2t%d" % sl])
                    S.dma("sp", ov[n], h2t[sl][:], reads=["ph2t%d" % sl], writes=["out"])


_PROG = {}


def kernel(**inputs):
    sh = host_shared(inputs)
    sh.update(host_shared_rest(inputs))
    if "nc" not in _PROG:
        _PROG["nc"] = build_program(("mixer", "mem", "peer"))[0]
    nc = _PROG["nc"]
    names = set(IN_SHAPES) | set(IN_SHAPES_MEM) | set(IN_SHAPES_PEER)
    mem = np.asarray(inputs["mem"], np.float32)
    maps = []
    for c in range(8):
        m = {k: v for k, v in sh.items() if k in names}
        m.update(host_core(inputs, c))
        m["mem"] = np.ascontiguousarray(mem[c // 4])
        maps.append(m)
    res = run_bass_kernel_spmd(nc, maps, core_ids=list(range(8)))
    out = np.zeros((2, 8192, 1024), np.float32)
    for c in range(8):
        out[c // 4, (c % 4) * 2048:(c % 4 + 1) * 2048] = res.results[c]["out"]
    return out
```

```python
from contextlib import ExitStack, contextmanager
import numpy as np
import concourse.bass as bass
import concourse.mybir as mybir
from concourse.bass_utils import run_bass_kernel_spmd

F32 = mybir.dt.float32
BF16 = mybir.dt.bfloat16
I32 = mybir.dt.int32
AF = mybir.ActivationFunctionType
ALU = mybir.AluOpType
AX = mybir.AxisListType

ENGS = ("pe", "act", "dve", "pool", "sp")
TWO_PI = 6.283185307179586
EPS = 1e-6
NEG = -30000.0


class Sched:
    def __init__(self, nc, es, n_dma_sems=32):
        self.nc = nc
        self.streams = {e: [] for e in ENGS}
        self.sem = {e: es.enter_context(nc.semaphore("s_" + e)) for e in ("pe", "act", "dve", "pool")}
        self.cnt = {e: 0 for e in ("pe", "act", "dve", "pool")}
        self.dsem = [es.enter_context(nc.semaphore("s_dma%d" % i)) for i in range(n_dma_sems)]
        self.dcnt = [0] * n_dma_sems
        self.dnext = 0
        self.n_sw = 4
        self.dnext_sw = 0
        self.waited = {}
        self.last_w = {}
        self.readers = {}
        self.n_ops = 0

    def _deps(self, eng, reads, writes):
        deps = []
        for k in reads:
            if k in self.last_w:
                deps.append(self.last_w[k])
        for k in writes:
            if k in self.last_w:
                deps.append(self.last_w[k])
            deps.extend(self.readers.get(k, ()))
        need = {}
        for (sk, val, peng) in deps:
            if peng == "pe" and eng == "pe":
                continue
            if self.waited.get((eng, sk), 0) >= val:
                continue
            if need.get(sk, 0) < val:
                need[sk] = val
        return need

    def _semobj(self, sk):
        return self.sem[sk] if isinstance(sk, str) else self.dsem[sk]

    def _emit_waits(self, eng, need):
        for sk, val in need.items():
            self.waited[(eng, sk)] = val
            so = self._semobj(sk)
            self.streams[eng].append(lambda e, so=so, val=val: e.wait_ge(so, val))

    def _record(self, tok, reads, writes):
        for k in writes:
            self.last_w[k] = tok
            self.readers[k] = []
        for k in reads:
            if k not in writes:
                self.readers.setdefault(k, []).append(tok)

    def op(self, eng, fn, reads=(), writes=()):
        need = self._deps(eng, reads, writes)
        self._emit_waits(eng, need)
        self.cnt[eng] += 1
        val = self.cnt[eng]
        so = self.sem[eng]
        self.streams[eng].append(lambda e, fn=fn, so=so: fn(e).then_inc(so, 1))
        self._record((eng, val, eng), reads, writes)
        self.n_ops += 1

    def dma(self, q, out, in_, reads=(), writes=(), **kw):
        nhw = len(self.dsem) - self.n_sw
        if q == "pool":
            i = nhw + self.dnext_sw
            self.dnext_sw = (self.dnext_sw + 1) % self.n_sw
        else:
            i = self.dnext
            self.dnext = (self.dnext + 1) % nhw
        need = self._deps(q, reads, writes)
        prev = 16 * self.dcnt[i]
        if prev and self.waited.get((q, i), 0) < prev:
            need[i] = max(need.get(i, 0), prev)
        self._emit_waits(q, need)
        self.dcnt[i] += 1
        val = 16 * self.dcnt[i]
        so = self.dsem[i]
        self.streams[q].append(
            lambda e, out=out, in_=in_, so=so, kw=kw: e.dma_start(out=out, in_=in_, **kw).then_inc(so, 16))
        self._record((i, val, "dma"), reads, writes)
        self.n_ops += 1

    def barrier(self):
        for eng in ENGS:
            need = {}
            for pe_ in ("pe", "act", "dve", "pool"):
                v = self.cnt[pe_]
                if v and self.waited.get((eng, pe_), 0) < v:
                    need[pe_] = v
            for i, c in enumerate(self.dcnt):
                if c and self.waited.get((eng, i), 0) < 16 * c:
                    need[i] = 16 * c
            self._emit_waits(eng, need)

    def wait_all(self, eng, keys):
        need = {}
        for k in keys:
            if k in self.last_w:
                sk, val, _ = self.last_w[k]
                if self.waited.get((eng, sk), 0) < val and need.get(sk, 0) < val:
                    need[sk] = val
        self._emit_waits(eng, need)

    def emit(self):
        if not any(self.streams[e] for e in ENGS):
            return
        streams = self.streams
        self.streams = {e: [] for e in ENGS}
        self._emit_block(streams)

    def _emit_block(self, streams):
        self_streams = streams
        with self.nc.Block() as block:
            @block.tensor
            def _(e):
                for f in self_streams["pe"]:
                    f(e)

            @block.scalar
            def _(e):
                for f in self_streams["act"]:
                    f(e)

            @block.vector
            def _(e):
                for f in self_streams["dve"]:
                    f(e)

            @block.gpsimd
            def _(e):
                for f in self_streams["pool"]:
                    f(e)

            @block.sync
            def _(e):
                for f in self_streams["sp"]:
                    f(e)


class Ctx:
    def __init__(self, nc, S):
        self.nc = nc
        self.S = S
        self.uid = 0

    def sb(self, es, name, shape, dt=F32):
        self.uid += 1
        return es.enter_context(self.nc.sbuf_tensor("%s_%d" % (name, self.uid), list(shape), dt))

    def ps(self, es, name, shape, dt=F32):
        self.uid += 1
        return es.enter_context(self.nc.psum_tensor("%s_%d" % (name, self.uid), list(shape), dt))


@contextmanager
def scope(C):
    with ExitStack() as es:
        yield es
        C.S.barrier()
        C.S.emit()


def bc(ap, shape):
    return ap.to_broadcast(list(shape))


def make_ident(C, es, name="ident"):
    S = C.S
    idf = C.sb(es, name + "f", [128, 128])
    idb = C.sb(es, name + "b", [128, 128], BF16)
    S.op("pool", lambda e: e.memset(idf[:], 1.0), writes=[name + "f"])
    S.op("pool", lambda e: e.affine_select(out=idf[:], in_=idf[:], pattern=[[-1, 128]], compare_op=ALU.is_equal,
                                           fill=0.0, base=0, channel_multiplier=1), reads=[name + "f"], writes=[name + "f"])
    S.op("dve", lambda e: e.tensor_copy(out=idb[:], in_=idf[:]), reads=[name + "f"], writes=[name + "b"])
    return idf, idb


def sincos(C, es, ang, n, tag):
    S = C.S
    outs = []
    for which, off in (("s", 64.0), ("c", 64.25)):
        k = tag + which
        y = C.sb(es, k + "y", [128, n]); yi = C.sb(es, k + "yi", [128, n], I32); yf = C.sb(es, k + "yf", [128, n])
        m = C.sb(es, k + "m", [128, n]); o = C.sb(es, k + "o", [128, n])
        S.op("dve", lambda e, y=y, off=off: e.tensor_scalar(out=y[:], in0=ang, scalar1=1.0 / TWO_PI, scalar2=off, op0=ALU.mult, op1=ALU.add),
             reads=[tag + "ang"], writes=[k + "y"])
        S.op("dve", lambda e, y=y, yi=yi: e.tensor_copy(out=yi[:], in_=y[:]), reads=[k + "y"], writes=[k + "yi"])
        S.op("dve", lambda e, yi=yi, yf=yf: e.tensor_copy(out=yf[:], in_=yi[:]), reads=[k + "yi"], writes=[k + "yf"])
        S.op("dve", lambda e, y=y, yf=yf: e.tensor_tensor(out=y[:], in0=y[:], in1=yf[:], op=ALU.subtract), reads=[k + "y", k + "yf"], writes=[k + "y"])
        S.op("dve", lambda e, y=y, m=m: e.tensor_scalar(out=m[:], in0=y[:], scalar1=0.5, scalar2=None, op0=ALU.is_gt), reads=[k + "y"], writes=[k + "m"])
        S.op("dve", lambda e, y=y, m=m: e.tensor_tensor(out=y[:], in0=y[:], in1=m[:], op=ALU.subtract), reads=[k + "y", k + "m"], writes=[k + "y"])
        S.op("act", lambda e, y=y, o=o: e.activation(out=o[:], in_=y[:], func=AF.Sin, scale=TWO_PI), reads=[k + "y"], writes=[k + "o"])
        outs.append((o, k + "o"))
    return outs


def s5_params(C, es_keep, D):
    S, nc = C.S, C.nc
    P = {}
    LT2 = C.sb(es_keep, "LT2", [128, 8, 8, 2, 128], BF16)
    WPr = C.sb(es_keep, "WPr", [128, 9, 16]); WPi = C.sb(es_keep, "WPi", [128, 9, 16]); WPn = C.sb(es_keep, "WPn", [128, 9, 16])
    P.update(LT2=LT2, WPr=WPr, WPi=WPi, WPn=WPn)
    with scope(C) as es:
        sb = lambda name, shape, dt=F32: C.sb(es, name, shape, dt)
        Mz2 = sb("Mz2", [128, 16, 8, 128], BF16)
        CA = sb("CA", [128, 32, 128], BF16); CAs = sb("CAs", [128, 32, 128], BF16)
        LR = sb("LR", [128, 32]); LI = sb("LI", [128, 32]); LS = sb("LS", [128, 32])
        SG = sb("SG", [128, 2]); NV = sb("NV", [128, 23])
        P1B = sb("P1B", [128, 32, 16]); P2B = sb("P2B", [128, 32, 16]); P1C = sb("P1C", [128, 32, 16]); P2C = sb("P2C", [128, 32, 16])
        DD = sb("DD", [128, 16, 16])
        for t, nm in ((LR, "s5_lr"), (LI, "s5_li"), (LS, "s5_ls"), (SG, "s5_sg"), (NV, "s5_nv")):
            S.dma("sp", t[:], D[nm], writes=[nm])
        S.dma("sp", DD[:].rearrange("p e c -> p (e c)"), D["s5_dd"], writes=["s5_dd"])
        for t, nm in ((P1B, "s5_p1b"), (P2B, "s5_p2b"), (P1C, "s5_p1c"), (P2C, "s5_p2c")):
            S.dma("sp", t[:].rearrange("p g c -> p (g c)"), D[nm], writes=[nm])
        idf, idb = make_ident(C, es, "pid")
        STEP = sb("STEP", [128, 32]); AA = sb("AA", [128, 32]); PH = sb("PH", [128, 32])
        S.op("act", lambda e: e.activation(out=STEP[:], in_=LS[:], func=AF.Exp), reads=["s5_ls"], writes=["STEP"])
        S.op("dve", lambda e: e.tensor_tensor(out=AA[:], in0=LR[:], in1=STEP[:], op=ALU.mult), reads=["s5_lr", "STEP"], writes=["AA"])
        S.op("dve", lambda e: e.tensor_tensor(out=PH[:], in0=LI[:], in1=STEP[:], op=ALU.mult), reads=["s5_li", "STEP"], writes=["PH"])
        EXPO = sb("EXPO", [128, 32, 23]); ANG = sb("ANG", [128, 32, 23]); MAG = sb("MAG", [128, 32, 23])
        nvb = bc(NV[:].unsqueeze(1), [128, 32, 23])
        S.op("dve", lambda e: e.tensor_tensor(out=EXPO[:], in0=bc(AA[:].unsqueeze(2), [128, 32, 23]), in1=nvb, op=ALU.mult), reads=["AA", "s5_nv"], writes=["EXPO"])
        S.op("dve", lambda e: e.tensor_tensor(out=ANG[:], in0=bc(PH[:].unsqueeze(2), [128, 32, 23]), in1=nvb, op=ALU.mult), reads=["PH", "s5_nv"], writes=["pwang"])
        S.op("act", lambda e: e.activation(out=MAG[:], in_=EXPO[:], func=AF.Exp), reads=["EXPO"], writes=["MAG"])
        (sn, snk), (cs, csk) = sincos(C, es, ANG[:].rearrange("p g n -> p (g n)"), 32 * 23, "pw")
        CR = sb("CR", [128, 32, 23]); CI = sb("CI", [128, 32, 23])
        S.op("dve", lambda e: e.tensor_tensor(out=CR[:].rearrange("p g n -> p (g n)"), in0=MAG[:].rearrange("p g n -> p (g n)"), in1=cs[:], op=ALU.mult), reads=["MAG", csk], writes=["CR"])
        S.op("dve", lambda e: e.tensor_tensor(out=CI[:].rearrange("p g n -> p (g n)"), in0=MAG[:].rearrange("p g n -> p (g n)"), in1=sn[:], op=ALU.mult), reads=["MAG", snk], writes=["CI"])
        zr = sb("zr", [128, 32]); den = sb("den", [128, 32]); t0 = sb("t0", [128, 32]); fr = sb("fr", [128, 32]); fi = sb("fi", [128, 32])
        S.op("dve", lambda e: e.tensor_scalar(out=zr[:], in0=CR[:, :, 8], scalar1=-1.0, scalar2=None, op0=ALU.add), reads=["CR"], writes=["zr"])
        S.op("dve", lambda e: e.tensor_tensor(out=den[:], in0=LR[:], in1=LR[:], op=ALU.mult), reads=["s5_lr"], writes=["den"])
        S.op("dve", lambda e: e.tensor_tensor(out=t0[:], in0=LI[:], in1=LI[:], op=ALU.mult), reads=["s5_li"], writes=["t0"])
        S.op("dve", lambda e: e.tensor_tensor(out=den[:], in0=den[:], in1=t0[:], op=ALU.add), reads=["den", "t0"], writes=["den"])
        S.op("dve", lambda e: e.reciprocal(out=den[:], in_=den[:]), reads=["den"], writes=["den"])
        S.op("dve", lambda e: e.tensor_tensor(out=fr[:], in0=zr[:], in1=LR[:], op=ALU.mult), reads=["zr", "s5_lr"], writes=["fr"])
        S.op("dve", lambda e: e.tensor_tensor(out=t0[:], in0=CI[:, :, 8], in1=LI[:], op=ALU.mult), reads=["CI", "s5_li", "den"], writes=["t0"])
        S.op("dve", lambda e: e.tensor_tensor(out=fr[:], in0=fr[:], in1=t0[:], op=ALU.add), reads=["fr", "t0"], writes=["fr"])
        S.op("dve", lambda e: e.tensor_tensor(out=fr[:], in0=fr[:], in1=den[:], op=ALU.mult), reads=["fr", "den"], writes=["fr"])
        S.op("dve", lambda e: e.tensor_tensor(out=fi[:], in0=CI[:, :, 8], in1=LR[:], op=ALU.mult), reads=["CI", "s5_lr"], writes=["fi"])
        S.op("dve", lambda e: e.tensor_tensor(out=t0[:], in0=zr[:], in1=LI[:], op=ALU.mult), reads=["zr", "s5_li", "fr"], writes=["t0"])
        S.op("dve", lambda e: e.tensor_tensor(out=fi[:], in0=fi[:], in1=t0[:], op=ALU.subtract), reads=["fi", "t0"], writes=["fi"])
        S.op("dve", lambda e: e.tensor_tensor(out=fi[:], in0=fi[:], in1=den[:], op=ALU.mult), reads=["fi", "den"], writes=["fi"])
        BB1 = sb("BB1", [128, 32, 16]); BB2 = sb("BB2", [128, 32, 16]); ta = sb("ta", [128, 32, 16]); tb = sb("tb", [128, 32, 16])
        frb = bc(fr[:].unsqueeze(2), [128, 32, 16]); fib = bc(fi[:].unsqueeze(2), [128, 32, 16])
        fl = lambda t: t[:].rearrange("p g c -> p (g c)")
        S.op("dve", lambda e: e.tensor_tensor(out=ta[:], in0=P1B[:], in1=frb, op=ALU.mult), reads=["s5_p1b", "fr"], writes=["ta"])
        S.op("dve", lambda e: e.tensor_tensor(out=tb[:], in0=P2B[:], in1=fib, op=ALU.mult), reads=["s5_p2b", "fi"], writes=["tb"])
        S.op("dve", lambda e: e.scalar_tensor_tensor(out=fl(BB1), in0=fl(tb), scalar=SG[:, 0:1], in1=fl(ta), op0=ALU.mult, op1=ALU.add), reads=["ta", "tb", "s5_sg"], writes=["BB1"])
        S.op("dve", lambda e: e.tensor_tensor(out=ta[:], in0=P2B[:], in1=frb, op=ALU.mult), reads=["s5_p2b", "fr", "BB1"], writes=["ta"])
        S.op("dve", lambda e: e.tensor_tensor(out=tb[:], in0=P1B[:], in1=fib, op=ALU.mult), reads=["s5_p1b", "fi", "BB1"], writes=["tb"])
        S.op("dve", lambda e: e.scalar_tensor_tensor(out=fl(BB2), in0=fl(tb), scalar=SG[:, 1:2], in1=fl(ta), op0=ALU.mult, op1=ALU.add), reads=["ta", "tb", "s5_sg"], writes=["BB2"])
        t5 = sb("t5", [128, 32, 16]); t6 = sb("t6", [128, 32, 16])
        Rm = sb("Rm", [128, 32, 8, 16])
        Q1 = sb("Q1", [128, 32, 16]); Q2 = sb("Q2", [128, 32, 16])
        S.op("dve", lambda e: e.tensor_scalar(out=fl(Q1), in0=fl(P1C), scalar1=SG[:, 1:2], scalar2=None, op0=ALU.mult), reads=["s5_p1c", "s5_sg"], writes=["Q1"])
        S.op("dve", lambda e: e.tensor_scalar(out=fl(Q2), in0=fl(P2C), scalar1=SG[:, 0:1], scalar2=None, op0=ALU.mult), reads=["s5_p2c", "s5_sg"], writes=["Q2"])
        CAv = CA[:].rearrange("p g (t c) -> p g t c", c=16); CAsv = CAs[:].rearrange("p g (t c) -> p g t c", c=16)
        for t in range(8):
            for (dst, dkey, a_, akey, b_, bkey, idx) in ((Rm[:, :, t, :], "Rm", Q1, "Q1", P2C, "s5_p2c", 7 + t),
                                                         (CAv[:, :, t, :], "CA", Q1, "Q1", P2C, "s5_p2c", 15 + t),
                                                         (CAsv[:, :, t, :], "CAs", Q2, "Q2", P1C, "s5_p1c", 15 + t)):
                S.op("dve", lambda e, a_=a_, idx=idx: e.tensor_tensor(out=t5[:], in0=a_[:], in1=bc(CR[:, :, idx:idx + 1], [128, 32, 16]), op=ALU.mult), reads=[akey, "CR"], writes=["t5"])
                S.op("dve", lambda e, b_=b_, idx=idx: e.tensor_tensor(out=t6[:], in0=b_[:], in1=bc(CI[:, :, idx:idx + 1], [128, 32, 16]), op=ALU.mult), reads=[bkey, "CI"], writes=["t6"])
                S.op("dve", lambda e, dst=dst: e.tensor_tensor(out=dst, in0=t5[:], in1=t6[:], op=ALU.subtract), reads=["t5", "t6"], writes=[dkey])
        Wr = sb("Wr", [128, 9, 32]); Wi = sb("Wi", [128, 9, 32]); sq = sb("sq", [128, 32])
        S.op("dve", lambda e: e.tensor_copy(out=Wr[:, 0, :], in_=CR[:, :, 15]), reads=["CR"], writes=["Wr"])
        S.op("dve", lambda e: e.tensor_copy(out=Wi[:, 0, :], in_=CI[:, :, 15]), reads=["CI"], writes=["Wi"])
        for j in range(8):
            S.op("dve", lambda e, j=j: e.tensor_tensor(out=Wr[:, j + 1, :], in0=Wr[:, j, :], in1=Wr[:, j, :], op=ALU.mult), reads=["Wr"], writes=["Wr"])
            S.op("dve", lambda e, j=j: e.tensor_tensor(out=sq[:], in0=Wi[:, j, :], in1=Wi[:, j, :], op=ALU.mult), reads=["Wi"], writes=["sq"])
            S.op("dve", lambda e, j=j: e.tensor_tensor(out=Wr[:, j + 1, :], in0=Wr[:, j + 1, :], in1=sq[:], op=ALU.subtract), reads=["Wr", "sq"], writes=["Wr"])
            S.op("dve", lambda e, j=j: e.scalar_tensor_tensor(out=Wi[:, j + 1, :], in0=Wr[:, j, :], scalar=2.0, in1=Wi[:, j, :], op0=ALU.mult, op1=ALU.mult), reads=["Wr", "Wi"], writes=["Wi"])
        Wrv = Wr[:].rearrange("p j (a r w) -> p j a r w", r=2, w=2); Wiv = Wi[:].rearrange("p j (a r w) -> p j a r w", r=2, w=2)
        for r in range(2):
            pr_ = slice(64 * r, 64 * r + 64)
            for j in range(9):
                S.op("dve", lambda e, r=r, pr_=pr_, j=j: e.tensor_copy(out=WPr[pr_, j, :].rearrange("p (a w) -> p a w", w=2), in_=Wrv[pr_, j, :, r, :]), reads=["Wr"], writes=["WPr"])
                S.op("dve", lambda e, r=r, pr_=pr_, j=j: e.tensor_copy(out=WPi[pr_, j, :].rearrange("p (a w) -> p a w", w=2), in_=Wiv[pr_, j, :, r, :]), reads=["Wi"], writes=["WPi"])
        S.op("dve", lambda e: e.tensor_scalar(out=WPn[:].rearrange("p j q -> p (j q)"), in0=WPi[:].rearrange("p j q -> p (j q)"), scalar1=-1.0, scalar2=None, op0=ALU.mult), reads=["WPi"], writes=["WPn"])
        E = [[sb("E%d%d" % (h_, r), [128, 128]) for r in range(2)] for h_ in range(2)]
        for h_ in range(2):
            for r in range(2):
                S.op("pool", lambda e, h_=h_, r=r: e.memset(E[h_][r][:], 0.0), writes=["E%d%d" % (h_, r)])
                S.op("pool", lambda e, h_=h_, r=r: e.tensor_copy(out=E[h_][r][64 * h_:64 * h_ + 64, 64 * r:64 * r + 64], in_=idf[64 * h_:64 * h_ + 64, 64 * h_:64 * h_ + 64]),
                     reads=["pidf", "E%d%d" % (h_, r)], writes=["E%d%d" % (h_, r)])
        Lz = [sb("Lz%d" % i, [128, 32, 64]) for i in range(2)]
        for i in range(2):
            S.op("pool", lambda e, i=i: e.memset(Lz[i][:].rearrange("p g c -> p (g c)"), 0.0), writes=["Lz%d" % i])
        S.op("pool", lambda e: e.memset(Mz2[:].rearrange("p a b c -> p (a b c)"), 0.0), writes=["Mz2"])
        t5v = t5[:].rearrange("p (a j) c -> p a j c", j=4); t6v = t6[:].rearrange("p (a j) c -> p a j c", j=4)
        with scope(C) as esp:
            PM = C.ps(esp, "PM", [128, 16, 128]); PL = C.ps(esp, "PL", [128, 8, 2, 128])
            for s in range(8):
                i = 7 - s; sl = s % 2; lk = "Lz%d" % sl
                Lzv = Lz[sl][:].rearrange("p (a j) c -> p a j c", j=4)
                S.op("dve", lambda e, i=i: e.tensor_tensor(out=t5[:], in0=BB1[:], in1=bc(CR[:, :, i:i + 1], [128, 32, 16]), op=ALU.mult), reads=["BB1", "CR"], writes=["t5"])
                S.op("dve", lambda e, i=i: e.tensor_tensor(out=t6[:], in0=BB2[:], in1=bc(CI[:, :, i:i + 1], [128, 32, 16]), op=ALU.mult), reads=["BB2", "CI"], writes=["t6"])
                for j4 in range(4):
                    S.op("dve", lambda e, j4=j4, Lzv=Lzv: e.scalar_tensor_tensor(out=Lzv[:, :, j4, 16 * j4:16 * j4 + 16], in0=t6v[:, :, j4, :], scalar=SG[:, 0:1], in1=t5v[:, :, j4, :], op0=ALU.mult, op1=ALU.add),
                         reads=["t5", "t6", "s5_sg"], writes=[lk])
                for g in range(32):
                    chc = g // 8; hb = (g % 8) // 4; j4 = g % 4; e_ = chc * 4 + j4
                    rows = slice(64 * hb, 64 * hb + 64)
                    S.op("pe", lambda e, g=g, sl=sl, e_=e_, rows=rows: e.matmul(PM[rows, e_, :], lhsT=Lz[sl][:, g, :], rhs=Rm[:, g, :, :].rearrange("p t c -> p (t c)"), start=True, stop=True),
                         reads=[lk, "Rm"], writes=["PM"])
                for pr in range(16):
                    a_ = pr // 2; wp = pr % 2; chc = a_ // 2; hb = a_ % 2; e2 = chc * 2 + wp
                    rows = slice(64 * hb, 64 * hb + 64)
                    for h_ in range(2):
                        for r in range(2):
                            g = 4 * a_ + 2 * r + wp
                            S.op("pe", lambda e, g=g, sl=sl, e2=e2, rows=rows, h_=h_, r=r: e.matmul(PL[rows, e2, h_, :], lhsT=Lz[sl][:, g, :], rhs=E[h_][r][:], start=(r == 0), stop=(r == 1)),
                                 reads=[lk, "E%d%d" % (h_, r)], writes=["PL"])
                S.op("dve", lambda e, s=s: e.tensor_copy(out=Mz2[:, :, s, 16 * s:128], in_=PM[:, :, 16 * s:128]), reads=["PM"], writes=["Mz2"])
                S.op("dve", lambda e, s=s: e.tensor_tensor(out=Mz2[:, :, s, 16 * s:16 * s + 16], in0=PM[:, :, 16 * s:16 * s + 16], in1=DD[:], op=ALU.add), reads=["PM", "s5_dd"], writes=["Mz2"])
                S.op("act", lambda e, s=s: e.copy(out=LT2[:, :, s, :, :], in_=PL[:]), reads=["PL"], writes=["LT2"])
        S.dma("sp", D["Mz_d"], Mz2[:].rearrange("p a b c -> p (a b c)"), reads=["Mz2"], writes=["Mz_d"])
        S.dma("sp", D["CA_d"][:, 0, :], CA[:].rearrange("p g c -> p (g c)"), reads=["CA"], writes=["CA_d"])
        S.dma("sp", D["CA_d"][:, 1, :], CAs[:].rearrange("p g c -> p (g c)"), reads=["CAs"], writes=["CA_d"])
    return P


def load_weight_bf16(C, es, es_tmp, name, src, rows_chunks, ncols, gcol=None, q="sp"):
    S = C.S
    W = C.sb(es, name, [128, rows_chunks, ncols], BF16)
    stg = [C.sb(es_tmp, name + "_stg%d" % i, [128, ncols]) for i in range(2)]
    srcv = src.rearrange("(c p) n -> c p n", p=128)
    for c in range(rows_chunks):
        st = stg[c % 2]; sk = name + "_stg%d" % (c % 2)
        S.dma(q, st[:], srcv[c], writes=[sk])
        eng = "dve" if c % 2 == 0 else "pool"
        if gcol is not None:
            S.op(eng, lambda e, st=st, c=c: e.tensor_scalar(out=W[:, c, :], in0=st[:], scalar1=gcol[0][:, c:c + 1], scalar2=None, op0=ALU.mult),
                 reads=[sk, gcol[1]], writes=[name])
        else:
            S.op(eng, lambda e, st=st, c=c: e.tensor_copy(out=W[:, c, :], in_=st[:]), reads=[sk], writes=[name])
    return W


def rms_rstd(C, x_ap, xkey, junk, jkey, ss, rs, skey, n):
    S = C.S
    S.op("act", lambda e: e.activation(out=junk, in_=x_ap, func=AF.Square, accum_out=ss), reads=[xkey], writes=[skey + "_ss"])
    S.op("act", lambda e: e.activation(out=rs, in_=ss, func=AF.Sqrt, scale=1.0 / n, bias=EPS), reads=[skey + "_ss"], writes=[skey + "_sq"])
    S.op("dve", lambda e: e.reciprocal(out=rs, in_=rs), reads=[skey + "_sq"], writes=[skey])


def transpose_chunks(C, src, skey, nch, pbank, pkey, dst, dkey, idb, evac="act"):
    S = C.S
    for c in range(nch):
        S.op("pe", lambda e, c=c: e.transpose(out=pbank[:, c, :], in_=src[:, c * 128:(c + 1) * 128], identity=idb[:]), reads=[skey, "identb"], writes=[pkey])
    if evac == "act":
        S.op("act", lambda e: e.copy(out=dst, in_=pbank), reads=[pkey], writes=[dkey])
    else:
        S.op(evac, lambda e: e.tensor_copy(out=dst, in_=pbank), reads=[pkey], writes=[dkey])


def stage_mixer(C, D, dbg=None, upto=9, prep=None):
    S, nc = C.S, C.nc
    dbg = dbg or {}
    with scope(C) as es1:
        P = s5_params(C, es1, D)
        idf = C.sb(es1, "identf", [128, 128]); idb = C.sb(es1, "identb", [128, 128], BF16)
        S.op("pool", lambda e: e.memset(idf[:], 1.0), writes=["identf"])
        S.op("pool", lambda e: e.affine_select(out=idf[:], in_=idf[:], pattern=[[-1, 128]], compare_op=ALU.is_equal, fill=0.0, base=0, channel_multiplier=1), reads=["identf"], writes=["identf"])
        S.op("dve", lambda e: e.tensor_copy(out=idb[:], in_=idf[:]), reads=["identf"], writes=["identb"])
        if "WPr" in dbg:
            for nm in ("WPr", "WPi"):
                S.dma("sp", dbg[nm], P[nm][:].rearrange("p a b -> p (a b)"), reads=[nm], writes=["o_" + nm])
            S.dma("pool", dbg["LT2"], P["LT2"][:].rearrange("p a b c d -> p (a b c d)"), reads=["LT2"], writes=["o_LT2"])
            S.dma("pool", dbg["Mz"], D["Mz_d"], reads=["Mz_d"], writes=["o_Mz"])
            S.dma("pool", dbg["CA"], D["CA_d"].rearrange("p a b -> p (a b)"), reads=["CA_d"], writes=["o_CA"])
        if upto < 1:
            return
        carry_r = C.sb(es1, "carry_r", [128, 16]); carry_i = C.sb(es1, "carry_i", [128, 16])
        S.op("pool", lambda e: e.memset(carry_r[:], 0.0), writes=["carry_r"])
        S.op("pool", lambda e: e.memset(carry_i[:], 0.0), writes=["carry_i"])
        TRE = C.sb(es1, "TRE", [128, 16, 257]); TIM = C.sb(es1, "TIM", [128, 16, 257])
        uT = C.sb(es1, "uT", [128, 4, 8, 256], BF16)
        with scope(C) as es2:
            _mixer_passes(C, es2, D, P, idb, carry_r, carry_i, TRE, TIM, uT, dbg)
        if "carry" in dbg:
            S.dma("sp", dbg["carry"][:, 0:16], carry_r[:], reads=["carry_r"], writes=["o_carry"])
            S.dma("sp", dbg["carry"][:, 16:32], carry_i[:], reads=["carry_i"], writes=["o_carry2"])
        if upto < 2:
            return
        with scope(C) as es3:
            ytm = C.sb(es3, "ytm", [128, 2, 8, 512], BF16)
            with scope(C) as es4:
                _s5_scan_out(C, es4, D, P, idb, TRE, TIM, uT, ytm, carry_r, carry_i, dbg)
            if upto < 3:
                return
            _s5_glu_out(C, es3, D, idb, ytm, dbg)
    if upto < 4:
        return
    with scope(C) as es5:
        idf = C.sb(es5, "identf", [128, 128]); idb = C.sb(es5, "identb", [128, 128], BF16)
        S.op("pool", lambda e: e.memset(idf[:], 1.0), reads=[], writes=["identf"])
        S.op("pool", lambda e: e.affine_select(out=idf[:], in_=idf[:], pattern=[[-1, 128]], compare_op=ALU.is_equal, fill=0.0, base=0, channel_multiplier=1), reads=["identf"], writes=["identf"])
        S.op("dve", lambda e: e.tensor_copy(out=idb[:], in_=idf[:]), reads=["identf"], writes=["identb"])
        _attention(C, es5, D, idb, dbg, prep)


def _mixer_passes(C, es, D, P, idb, carry_r, carry_i, TRE, TIM, uT, dbg):
    S = C.S
    sb = lambda name, shape, dt=F32: C.sb(es, name, shape, dt)
    gin = sb("gin", [128, 8])
    S.dma("sp", gin[:], D["g_mix"], writes=["gin"])
    with scope(C) as est:
        Wb = load_weight_bf16(C, es, est, "Wb", D["w_in"], 8, 2048, gcol=(gin, "gin"))
    gq = sb("gq", [128, 64]); gk = sb("gk", [128, 64]); hv = sb("hv", [128, 4])
    S.dma("sp", gq[:], D["att_q_g"].partition_broadcast(128), writes=["gq"])
    S.dma("sp", gk[:], D["att_k_g"].partition_broadcast(128), writes=["gk"])
    S.dma("sp", hv[:], D["hvalid"], writes=["hv"])
    S.op("dve", lambda e: e.tensor_scalar(out=gq[:], in0=gq[:], scalar1=0.125, scalar2=None, op0=ALU.mult), reads=["gq"], writes=["gq"])
    xt = [sb("xt%d" % i, [128, 1024]) for i in range(2)]
    junk = sb("junk", [128, 1024]); st = [sb("st%d" % i, [128, 4]) for i in range(2)]
    xn = [sb("xn%d" % i, [128, 1024], BF16) for i in range(2)]
    xnT = [sb("xnT%d" % i, [128, 8, 512], BF16) for i in range(2)]
    qkv = sb("qkv", [128, 3, 512]); sq = sb("sq2", [128, 512]); qst = sb("qst", [128, 4, 8])
    qn = sb("qn", [128, 2, 512], BF16)
    kTs = [sb("kTs%d" % i, [128, 4, 128], BF16) for i in range(2)]; qTs = [sb("qTs%d" % i, [128, 4, 128], BF16) for i in range(2)]
    Vs = [sb("Vs%d" % i, [128, 8, 65], BF16) for i in range(2)]
    tBr = sb("tBr", [128, 8, 128]); tBi = sb("tBi", [128, 8, 128]); tCr = sb("tCr", [128, 8, 64]); tCi = sb("tCi", [128, 8, 64])
    tt1 = sb("tt1", [128, 8, 128]); tt2 = sb("tt2", [128, 8, 128]); tt3 = sb("tt3", [128, 8, 128]); tt4 = sb("tt4", [128, 8, 128])
    REDr = sb("REDr", [128, 16]); REDi = sb("REDi", [128, 16]); c1 = sb("c1", [128, 16]); c2 = sb("c2", [128, 16]); c3 = sb("c3", [128, 16])
    with scope(C) as esp:
        bank = [C.ps(esp, "bk%d" % i, [128, 512]) for i in range(8)]
        xv = D["x_ext"].rearrange("(n p) d -> n p d", p=128)
        tile_ctr = 0
        for q in range(4):
            for blk in range(4):
                bslot = (q * 4 + blk) % 2
                xT = xnT[bslot]; xTk = "xnT%d" % bslot
                for tt in range(4):
                    n_tile = q * 16 + blk * 4 + tt
                    sl = tile_ctr % 2; tile_ctr += 1
                    x_ = xt[sl]; xk = "xt%d" % sl
                    S.dma("sp", x_[:], xv[n_tile], writes=[xk])
                    rms_rstd(C, x_[:], xk, junk[:], "junk", st[sl][:, 0:1], st[sl][:, 1:2], "st%d" % sl, 1024)
                    S.op("dve", lambda e, x_=x_, sl=sl: e.tensor_scalar(out=xn[sl][:], in0=x_[:], scalar1=st[sl][:, 1:2], scalar2=None, op0=ALU.mult),
                         reads=[xk, "st%d" % sl], writes=["xn%d" % sl])
                    tb_ = 0 if sl == 0 else 7
                    pb = bank[tb_][:].bitcast(BF16).rearrange("p (c n) -> p c n", n=128)
                    transpose_chunks(C, xn[sl][:], "xn%d" % sl, 8, pb, "bk%d" % tb_, xT[:, :, tt * 128:(tt + 1) * 128], xTk, idb, evac=("act" if sl == 0 else "dve"))
                for chc in range(4):
                    bi = 1 + (chc % 2); bkk = "bk%d" % bi
                    for dc in range(8):
                        S.op("pe", lambda e, chc=chc, dc=dc, bi=bi, xT=xT: e.matmul(bank[bi][:], lhsT=Wb[:, dc, 1536 + chc * 128:1536 + (chc + 1) * 128], rhs=xT[:, dc, :], start=(dc == 0), stop=(dc == 7)),
                             reads=["Wb", xTk], writes=[bkk])
                    eng = "act" if chc % 2 == 0 else "dve"
                    src = bank[bi][:].rearrange("p (k s) -> p s k", s=8)
                    dst = uT[:, chc, :, blk * 64:(blk + 1) * 64]
                    if eng == "act":
                        S.op("act", lambda e, src=src, dst=dst: e.copy(out=dst, in_=src), reads=[bkk], writes=["uT"])
                    else:
                        S.op("dve", lambda e, src=src, dst=dst: e.tensor_copy(out=dst, in_=src), reads=[bkk], writes=["uT"])
                need_kv = (q == 3) or (q == 2 and blk == 3)
                need_q = (q == 3)
                if need_kv:
                    for tt in range(4):
                        n_tile = q * 16 + blk * 4 + tt
                        kvt = n_tile - 44
                        sl = kvt % 2
                        projs = [(1, 512, 3), (2, 1024, 4)] + ([(0, 0, 5)] if need_q else [])
                        for (pi, c0, bi) in projs:
                            for dc in range(8):
                                S.op("pe", lambda e, dc=dc, bi=bi, c0=c0, tt=tt, xT=xT: e.matmul(bank[bi][:], lhsT=xT[:, dc, tt * 128:(tt + 1) * 128], rhs=Wb[:, dc, c0:c0 + 512], start=(dc == 0), stop=(dc == 7)),
                                     reads=["Wb", xTk], writes=["bk%d" % bi])
                        S.op("act", lambda e, sl=sl: e.copy(out=Vs[sl][:, :, 0:64], in_=bank[4][:].rearrange("p (h d) -> p h d", d=64)), reads=["bk4"], writes=["Vs%d" % sl])
                        if kvt < 4:
                            S.op("pool", lambda e, sl=sl, kvt=kvt: e.tensor_copy(out=Vs[sl][:, :, 64], in_=bc(hv[:, kvt:kvt + 1], [128, 8])), reads=["hv"], writes=["Vs%d" % sl])
                        else:
                            S.op("pool", lambda e, sl=sl: e.memset(Vs[sl][:, :, 64], 1.0), reads=[], writes=["Vs%d" % sl])
                        S.dma("sp", D["V_d"][kvt], Vs[sl][:].rearrange("p h d -> p (h d)"), reads=["Vs%d" % sl], writes=["V_d"])
                        for (pi, bi, gt, gkey, dstT, dkey, dram, ncol_t) in ([(1, 3, gk, "gk", kTs[sl], "kTs%d" % sl, D["kT_d"], kvt)] +
                                                                          ([(0, 5, gq, "gq", qTs[sl], "qTs%d" % sl, D["qT_d"], kvt - 4)] if need_q else [])):
                            qs = qkv[:, pi, :]; qk_ = "qkv%d" % pi
                            S.op("act", lambda e, qs=qs, bi=bi: e.copy(out=qs, in_=bank[bi][:]), reads=["bk%d" % bi], writes=[qk_])
                            S.op("pool", lambda e, qs=qs: e.tensor_tensor(out=sq[:], in0=qs, in1=qs, op=ALU.mult), reads=[qk_], writes=["sq2"])
                            S.op("dve", lambda e, pi=pi: e.tensor_reduce(out=qst[:, pi, :], in_=sq[:].rearrange("p (h d) -> p h d", d=64), axis=AX.X, op=ALU.add), reads=["sq2"], writes=["qst%d" % pi])
                            S.op("act", lambda e, pi=pi: e.activation(out=qst[:, 2 + pi, :], in_=qst[:, pi, :], func=AF.Sqrt, scale=1.0 / 64, bias=EPS), reads=["qst%d" % pi], writes=["qsq%d" % pi])
                            S.op("dve", lambda e, pi=pi: e.reciprocal(out=qst[:, 2 + pi, :], in_=qst[:, 2 + pi, :]), reads=["qsq%d" % pi], writes=["qrs%d" % pi])
                            S.op("dve", lambda e, qs=qs, pi=pi: e.tensor_tensor(out=qs.rearrange("p (h d) -> p h d", d=64), in0=qs.rearrange("p (h d) -> p h d", d=64),
                                                                        in1=bc(qst[:, 2 + pi, :].unsqueeze(2), [128, 8, 64]), op=ALU.mult), reads=[qk_, "qrs%d" % pi], writes=[qk_])
                            S.op("pool", lambda e, qs=qs, pi=pi, gt=gt: e.tensor_tensor(out=qn[:, pi, :].rearrange("p (h d) -> p h d", d=64), in0=qs.rearrange("p (h d) -> p h d", d=64),
                                                                               in1=bc(gt[:].unsqueeze(1), [128, 8, 64]), op=ALU.mult), reads=[qk_, gkey], writes=["qn%d" % pi])
                            pb = bank[6][:].bitcast(BF16).rearrange("p (c n) -> p c n", n=128)[:, 0:4, :]
                            transpose_chunks(C, qn[:, pi, :], "qn%d" % pi, 4, pb, "bk6", dstT[:], dkey, idb)
                            S.dma("sp", dram.rearrange("p (c n) -> p c n", c=4)[:, :, ncol_t * 128:(ncol_t + 1) * 128], dstT[:], reads=[dkey], writes=["qkT_d"])
            for pair in range(16):
                psl = pair % 2
                br = bank[1 + 2 * psl]; bim = bank[2 + 2 * psl]; brk = "bk%d" % (1 + 2 * psl); bik = "bk%d" % (2 + 2 * psl)
                a_ = pair // 2; wp = pair % 2; chc = a_ // 2; hb = a_ % 2; e2 = chc * 2 + wp
                rows = slice(64 * hb, 64 * hb + 64)
                for half, (bkt, bkk) in enumerate(((br, brk), (bim, bik))):
                    for s in range(8):
                        S.op("pe", lambda e, e2=e2, s=s, half=half, rows=rows, chc=chc, bkt=bkt: e.matmul(bkt[:, 0:256], lhsT=P["LT2"][rows, e2, s, half, :], rhs=uT[rows, chc, s, :], start=(s == 0), stop=(s == 7)),
                             reads=["LT2", "uT"], writes=[bkk])
                S.op("act", lambda e, pair=pair, br=br: e.copy(out=TRE[:, pair, 1:257], in_=br[:, 0:256]), reads=[brk], writes=["TRE%d" % pair])
                S.op("act", lambda e, pair=pair, bim=bim: e.copy(out=TIM[:, pair, 1:257], in_=bim[:, 0:256]), reads=[bik], writes=["TIM%d" % pair])
            if q < 3:
                for p0 in (0, 8):
                    kre = ["TRE%d" % p for p in range(p0, p0 + 8)]; kim = ["TIM%d" % p for p in range(p0, p0 + 8)]
                    src_r, src_i, srk, sik = TRE[:, p0:p0 + 8, 1:257], TIM[:, p0:p0 + 8, 1:257], kre, kim
                    bufs = [(tBr[:], tBi[:], ["tBr"], ["tBi"]), (tCr[:], tCi[:], ["tCr"], ["tCi"])]
                    for j in range(8):
                        n = 256 >> j; h = n // 2
                        wrb = bc(P["WPr"][:, j, p0:p0 + 8].unsqueeze(2), [128, 8, h]); wib = bc(P["WPi"][:, j, p0:p0 + 8].unsqueeze(2), [128, 8, h])
                        sre, sro = src_r[:, :, 0:n:2], src_r[:, :, 1:n:2]; sie, sio = src_i[:, :, 0:n:2], src_i[:, :, 1:n:2]
                        if j == 7:
                            dr, di, drk, dik = REDr[:, p0:p0 + 8].unsqueeze(2), REDi[:, p0:p0 + 8].unsqueeze(2), ["REDr%d" % p0], ["REDi%d" % p0]
                        else:
                            bb = bufs[j % 2]
                            dr, di, drk, dik = bb[0][:, :, 0:h], bb[1][:, :, 0:h], bb[2], bb[3]
                        t1v, t2v, t3v, t4v = tt1[:, :, 0:h], tt2[:, :, 0:h], tt3[:, :, 0:h], tt4[:, :, 0:h]
                        S.op("dve", lambda e, t1v=t1v, sre=sre, wrb=wrb: e.tensor_tensor(out=t1v, in0=sre, in1=wrb, op=ALU.mult), reads=srk + ["WPr"], writes=["tt1"])
                        S.op("pool", lambda e, t2v=t2v, sie=sie, wib=wib: e.tensor_tensor(out=t2v, in0=sie, in1=wib, op=ALU.mult), reads=sik + ["WPi"], writes=["tt2"])
                        S.op("pool", lambda e, t3v=t3v, sie=sie, wrb=wrb: e.tensor_tensor(out=t3v, in0=sie, in1=wrb, op=ALU.mult), reads=sik + ["WPr"], writes=["tt3"])
                        S.op("dve", lambda e, t4v=t4v, sre=sre, wib=wib: e.tensor_tensor(out=t4v, in0=sre, in1=wib, op=ALU.mult), reads=srk + ["WPi"], writes=["tt4"])
                        S.op("dve", lambda e, t1v=t1v, t2v=t2v: e.tensor_tensor(out=t1v, in0=t1v, in1=t2v, op=ALU.subtract), reads=["tt1", "tt2"], writes=["tt1"])
                        S.op("pool", lambda e, t3v=t3v, t4v=t4v: e.tensor_tensor(out=t3v, in0=t3v, in1=t4v, op=ALU.add), reads=["tt3", "tt4"], writes=["tt3"])
                        S.op("dve", lambda e, dr=dr, t1v=t1v, sro=sro: e.tensor_tensor(out=dr, in0=t1v, in1=sro, op=ALU.add), reads=["tt1"] + srk, writes=drk)
                        S.op("pool", lambda e, di=di, t3v=t3v, sio=sio: e.tensor_tensor(out=di, in0=t3v, in1=sio, op=ALU.add), reads=["tt3"] + sik, writes=dik)
                        if j < 7:
                            src_r, src_i, srk, sik = bb[0], bb[1], bb[2], bb[3]
            if q < 3:
                w8r = P["WPr"][:, 8, :]; w8i = P["WPi"][:, 8, :]
                S.op("dve", lambda e: e.tensor_tensor(out=c1[:], in0=w8r, in1=carry_r[:], op=ALU.mult), reads=["WPr", "carry_r"], writes=["c1"])
                S.op("dve", lambda e: e.tensor_tensor(out=c2[:], in0=w8i, in1=carry_i[:], op=ALU.mult), reads=["WPi", "carry_i"], writes=["c2"])
                S.op("dve", lambda e: e.tensor_tensor(out=c1[:], in0=c1[:], in1=c2[:], op=ALU.subtract), reads=["c1", "c2"], writes=["c1"])
                S.op("dve", lambda e: e.tensor_tensor(out=c1[:], in0=c1[:], in1=REDr[:], op=ALU.add), reads=["c1", "REDr0", "REDr8"], writes=["c1"])
                S.op("dve", lambda e: e.tensor_tensor(out=c2[:], in0=w8r, in1=carry_i[:], op=ALU.mult), reads=["WPr", "carry_i", "c1"], writes=["c2"])
                S.op("dve", lambda e: e.tensor_tensor(out=c3[:], in0=w8i, in1=carry_r[:], op=ALU.mult), reads=["WPi", "carry_r"], writes=["c3"])
                S.op("dve", lambda e: e.tensor_tensor(out=c2[:], in0=c2[:], in1=c3[:], op=ALU.add), reads=["c2", "c3"], writes=["c2"])
                S.op("dve", lambda e: e.tensor_tensor(out=carry_i[:], in0=c2[:], in1=REDi[:], op=ALU.add), reads=["c2", "REDi0", "REDi8"], writes=["carry_i"])
                S.op("dve", lambda e: e.tensor_copy(out=carry_r[:], in_=c1[:]), reads=["c1"], writes=["carry_r"])


def _s5_scan_out(C, es, D, P, idb, TRE, TIM, uT, ytm, carry_r, carry_i, dbg):
    S = C.S
    sb = lambda name, shape, dt=F32: C.sb(es, name, shape, dt)
    Mz2 = sb("Mz2s", [128, 16, 8, 128], BF16); CAA = sb("CAA", [128, 2, 32, 128], BF16)
    S.dma("sp", Mz2[:].rearrange("p a b c -> p (a b c)"), D["Mz_d"], reads=["Mz_d"], writes=["Mz2s"])
    S.dma("sp", CAA[:].rearrange("p a g c -> p a (g c)"), D["CA_d"], reads=["CA_d"], writes=["CAA"])
    hs1 = sb("hs1", [128, 8, 256]); hs2 = sb("hs2", [128, 8, 256]); hs3 = sb("hs3", [128, 8, 256]); hs4 = sb("hs4", [128, 8, 256])
    Tbr = [sb("Tbr%d" % i, [128, 256], BF16) for i in range(2)]; Tbi = [sb("Tbi%d" % i, [128, 256], BF16) for i in range(2)]
    Yg = [sb("Yg%d" % i, [128, 256], BF16) for i in range(2)]
    ysum = [sb("ysum%d" % i, [128, 256]) for i in range(2)]
    import os
    CUT = int(os.environ.get("SCAN_CUT", "9"))
    for p0 in (0, 8):
        kre = ["TRE%d" % p for p in range(p0, p0 + 8)]; kim = ["TIM%d" % p for p in range(p0, p0 + 8)]
        S.op("dve", lambda e, p0=p0: e.tensor_copy(out=TRE[:, p0:p0 + 8, 0:1], in_=carry_r[:, p0:p0 + 8].unsqueeze(2)), reads=["carry_r"], writes=kre)
        S.op("pool", lambda e, p0=p0: e.tensor_copy(out=TIM[:, p0:p0 + 8, 0:1], in_=carry_i[:, p0:p0 + 8].unsqueeze(2)), reads=["carry_i"], writes=kim)
        for j in range(9):
            d = 1 << j; m = 257 - d
            wrb = bc(P["WPr"][:, j, p0:p0 + 8].unsqueeze(2), [128, 8, m]); wib = bc(P["WPi"][:, j, p0:p0 + 8].unsqueeze(2), [128, 8, m])
            R0 = TRE[:, p0:p0 + 8, 0:m]; I0 = TIM[:, p0:p0 + 8, 0:m]; R1 = TRE[:, p0:p0 + 8, d:257]; I1 = TIM[:, p0:p0 + 8, d:257]
            h1, h2, h3, h4 = hs1[:, :, 0:m], hs2[:, :, 0:m], hs3[:, :, 0:m], hs4[:, :, 0:m]
            S.op("dve", lambda e, h1=h1, R0=R0, wrb=wrb: e.tensor_tensor(out=h1, in0=R0, in1=wrb, op=ALU.mult), reads=kre + ["WPr"], writes=["hs1"])
            S.op("pool", lambda e, h2=h2, I0=I0, wib=wib: e.tensor_tensor(out=h2, in0=I0, in1=wib, op=ALU.mult), reads=kim + ["WPi"], writes=["hs2"])
            S.op("pool", lambda e, h3=h3, I0=I0, wrb=wrb: e.tensor_tensor(out=h3, in0=I0, in1=wrb, op=ALU.mult), reads=kim + ["WPr"], writes=["hs3"])
            S.op("dve", lambda e, h4=h4, R0=R0, wib=wib: e.tensor_tensor(out=h4, in0=R0, in1=wib, op=ALU.mult), reads=kre + ["WPi"], writes=["hs4"])
            S.op("dve", lambda e, h1=h1, h2=h2: e.tensor_tensor(out=h1, in0=h1, in1=h2, op=ALU.subtract), reads=["hs1", "hs2"], writes=["hs1"])
            S.op("pool", lambda e, h3=h3, h4=h4: e.tensor_tensor(out=h3, in0=h3, in1=h4, op=ALU.add), reads=["hs3", "hs4"], writes=["hs3"])
            S.op("dve", lambda e, R1=R1, h1=h1: e.tensor_tensor(out=R1, in0=R1, in1=h1, op=ALU.add), reads=kre + ["hs1"], writes=kre)
            S.op("pool", lambda e, I1=I1, h3=h3: e.tensor_tensor(out=I1, in0=I1, in1=h3, op=ALU.add), reads=kim + ["hs3"], writes=kim)
    with scope(C) as esp:
        bank = [C.ps(esp, "sbk%d" % i, [128, 512]) for i in range(6)]
        for pair in range(16):
            sl = pair % 2
            kr, ki = "TRE%d" % pair, "TIM%d" % pair
            a_ = pair // 2; wp = pair % 2; chc = a_ // 2; hb = a_ % 2
            rows = slice(64 * hb, 64 * hb + 64)
            kr, ki = "TRE%d" % pair, "TIM%d" % pair
            if CUT < 2:
                continue
            S.op("act", lambda e, pair=pair, sl=sl: e.copy(out=Tbr[sl][:], in_=TRE[:, pair, 0:256]), reads=[kr], writes=["Tbr%d" % sl])
            S.op("act", lambda e, pair=pair, sl=sl: e.copy(out=Tbi[sl][:], in_=TIM[:, pair, 0:256]), reads=[ki], writes=["Tbi%d" % sl])
            for r in range(2):
                g = 4 * a_ + 2 * r + wp
                e_ = chc * 4 + (g % 4)
                pr = slice(64 * r, 64 * r + 64)
                yb = bank[r]; ybk = "sbk%d" % r
                for s in range(8):
                    S.op("pe", lambda e, e_=e_, s=s, rows=rows, chc=chc, yb=yb: e.matmul(yb[:, 0:256], lhsT=Mz2[rows, e_, s, :], rhs=uT[rows, chc, s, :], start=(s == 0), stop=(s == 7)),
                         reads=["Mz2s", "uT"], writes=[ybk])
                zb_ = bank[4 + r]; zbk = "sbk%d" % (4 + r)
                S.op("pe", lambda e, g=g, pr=pr, zb_=zb_, r=r, sl=sl: e.matmul(zb_[:, 0:256], lhsT=CAA[pr, r, g, :], rhs=Tbr[sl][pr, :], start=True, stop=False), reads=["CAA", "Tbr%d" % sl], writes=[zbk])
                S.op("pe", lambda e, g=g, pr=pr, zb_=zb_, r=r, sl=sl: e.matmul(zb_[:, 0:256], lhsT=CAA[pr, 1 - r, g, :], rhs=Tbi[sl][pr, :], start=False, stop=True), reads=["CAA", "Tbi%d" % sl], writes=[zbk])
                if CUT < 3:
                    continue
                S.op("act", lambda e, r=r, zb_=zb_: e.copy(out=ysum[r][:], in_=zb_[:, 0:256]), reads=[zbk], writes=["ysum%d" % r])
                S.op("dve", lambda e, r=r, yb=yb: e.tensor_tensor(out=ysum[r][:], in0=yb[:, 0:256], in1=ysum[r][:], op=ALU.add), reads=[ybk, "ysum%d" % r], writes=["ysum%d" % r])
                S.op("act", lambda e, r=r: e.activation(out=Yg[r][:], in_=ysum[r][:], func=AF.Gelu), reads=["ysum%d" % r], writes=["Yg%d" % r])
                if CUT < 4:
                    continue
                pT = bank[2 + r][:].bitcast(BF16).rearrange("p (c n) -> p c n", n=128)
                for kb in range(2):
                    S.op("pe", lambda e, r=r, kb=kb, pT=pT: e.transpose(out=pT[:, kb, :], in_=Yg[r][:, kb * 128:(kb + 1) * 128], identity=idb[:]), reads=["Yg%d" % r, "identb"], writes=["sbk%d" % (2 + r)])
                for kb in range(2):
                    S.op("dve", lambda e, g=g, kb=kb, pT=pT: e.tensor_copy(out=ytm[:, kb, :, 16 * g:16 * g + 16], in_=pT[:, kb, :].rearrange("p (t c) -> p t c", c=16)),
                         reads=["sbk%d" % (2 + r)], writes=["ytm"])


def _s5_glu_out(C, es, D, idb, ytm, dbg):
    S = C.S
    sb = lambda name, shape, dt=F32: C.sb(es, name, shape, dt)
    gso = sb("gso", [128, 4]); bgl = sb("bgl", [128, 512])
    S.dma("sp", gso[:], D["g_ssm_out"], writes=["gso"])
    S.dma("sp", bgl[:], D["b_glu"].partition_broadcast(128), writes=["bgl"])
    with scope(C) as est:
        Wg = load_weight_bf16(C, es, est, "Wg", D["w_glu"], 4, 512)
    with scope(C) as est:
        Wo = load_weight_bf16(C, es, est, "Wos", D["w_out"][512:1024, :], 4, 1024, gcol=(gso, "gso"))
    yT = sb("yT", [128, 4, 128], BF16); zb = sb("zb", [128, 512]); ssm = sb("ssm", [128, 512]); junk = sb("junk3", [128, 512])
    st = sb("st3", [128, 2]); sn = sb("sn", [128, 512], BF16); snT = sb("snT", [128, 4, 128], BF16)
    ho = [sb("ho%d" % i, [128, 1024]) for i in range(2)]
    hsv = D["hs_d"].rearrange("(k t) d -> t k d", t=8)
    with scope(C) as esp:
        bank = [C.ps(esp, "gbk%d" % i, [128, 512]) for i in range(5)]
        it = 0
        for kb in range(2):
            for t in range(8):
                sl = it % 2; it += 1
                y = ytm[:, kb, t, :]
                pT = bank[0][:].bitcast(BF16).rearrange("p (c n) -> p c n", n=128)[:, 0:4, :]
                transpose_chunks(C, y, "ytm", 4, pT, "gbk0", yT[:], "yT", idb)
                for c in range(4):
                    S.op("pe", lambda e, c=c: e.matmul(bank[1][:], lhsT=yT[:, c, :], rhs=Wg[:, c, :], start=(c == 0), stop=(c == 3)), reads=["yT", "Wg"], writes=["gbk1"])
                S.op("dve", lambda e: e.tensor_tensor(out=zb[:], in0=bank[1][:], in1=bgl[:], op=ALU.add), reads=["gbk1", "bgl"], writes=["zb"])
                S.op("act", lambda e: e.activation(out=zb[:], in_=zb[:], func=AF.Sigmoid), reads=["zb"], writes=["zb"])
                S.op("pool", lambda e, y=y: e.tensor_tensor(out=ssm[:], in0=y, in1=zb[:], op=ALU.mult), reads=["ytm", "zb"], writes=["ssm"])
                if "ssm" in dbg:
                    S.dma("sp", dbg["ssm"].rearrange("(k t) d -> t k d", t=8)[t, kb * 128:(kb + 1) * 128, :], ssm[:], reads=["ssm"], writes=["o_ssm"])
                rms_rstd(C, ssm[:], "ssm", junk[:], "junk3", st[:, 0:1], st[:, 1:2], "st3", 512)
                S.op("dve", lambda e: e.tensor_scalar(out=sn[:], in0=ssm[:], scalar1=st[:, 1:2], scalar2=None, op0=ALU.mult), reads=["ssm", "st3"], writes=["sn"])
                pT2 = bank[2][:].bitcast(BF16).rearrange("p (c n) -> p c n", n=128)[:, 0:4, :]
                transpose_chunks(C, sn[:], "sn", 4, pT2, "gbk2", snT[:], "snT", idb)
                for cb in range(2):
                    for c in range(4):
                        S.op("pe", lambda e, c=c, cb=cb: e.matmul(bank[3 + cb][:], lhsT=snT[:, c, :], rhs=Wo[:, c, cb * 512:(cb + 1) * 512], start=(c == 0), stop=(c == 3)), reads=["snT", "Wos"], writes=["gbk%d" % (3 + cb)])
                    S.op("act", lambda e, cb=cb, sl=sl: e.copy(out=ho[sl][:, cb * 512:(cb + 1) * 512], in_=bank[3 + cb][:]), reads=["gbk%d" % (3 + cb)], writes=["ho%d" % sl])
                S.dma("sp", hsv[t, kb * 128:(kb + 1) * 128, :], ho[sl][:], reads=["ho%d" % sl], writes=["hs_d"])


def _attention(C, es, D, idb, dbg, prep=None):
    S = C.S
    sb = lambda name, shape, dt=F32: C.sb(es, name, shape, dt)
    kT = sb("kT", [128, 4, 2560], BF16); qT = sb("qT", [128, 4, 2048], BF16); V = sb("Vall", [128, 20, 520], BF16)
    qZ = sb("qZ", [128, 8, 2048], BF16)
    S.dma("sp", kT[:].rearrange("p c n -> p (c n)"), D["kT_d"], reads=["qkT_d"], writes=["kT"])
    S.dma("sp", qT[:].rearrange("p c n -> p (c n)"), D["qT_d"], reads=["qkT_d"], writes=["qT"])
    S.dma("sp", V[:], D["V_d"].rearrange("t p n -> p t n"), reads=["V_d"], writes=["Vall"])
    S.op("pool", lambda e: e.memset(qZ[:].rearrange("p h n -> p (h n)"), 0.0), writes=["qZ"])
    for h in range(8):
        rws = slice(64 * (h % 2), 64 * (h % 2) + 64)
        S.op("dve" if h % 2 else "act", (lambda e, h=h, rws=rws: e.tensor_copy(out=qZ[rws, h, :], in_=qT[rws, h // 2, :])) if h % 2 else (lambda e, h=h, rws=rws: e.copy(out=qZ[rws, h, :], in_=qT[rws, h // 2, :])),
             reads=["qT", "qZ"], writes=["qZ"])
    BT = sb("BT", [128, 8, 5, 128], BF16)
    gao = sb("gao", [128, 4])
    S.dma("sp", gao[:], D["g_att_out"], writes=["gao"])
    with scope(C) as est:
        stg = C.sb(est, "btstg", [128, 640])
        for h in range(8):
            S.dma("sp", stg[:], D["bias_t"][:, h * 640:(h + 1) * 640], writes=["btstg"])
            S.op("dve", lambda e, h=h: e.tensor_copy(out=BT[:, h, :, :].rearrange("p j q -> p (j q)"), in_=stg[:]), reads=["btstg"], writes=["BT"])
    with scope(C) as est:
        Wo = load_weight_bf16(C, es, est, "Woa", D["w_out"][0:512, :], 4, 1024, gcol=(gao, "gao"))
    PT = [sb("PT%d" % i, [128, 5, 128], BF16) for i in range(2)]
    rd = sb("rd", [128, 8]); att = sb("att", [128, 8, 64]); junk = sb("junk4", [128, 512]); st = sb("st4", [128, 2])
    an = sb("an", [128, 512], BF16); anT = sb("anT", [128, 4, 128], BF16)
    xo = [sb("xo%d" % i, [128, 1024]) for i in range(2)]; hsl = [sb("hsl%d" % i, [128, 1024]) for i in range(2)]
    h1t = [sb("h1t%d" % i, [128, 1024]) for i in range(2)]
    xv = D["x_ext"].rearrange("(n p) d -> n p d", p=128)
    hsv = D["hs_d"].rearrange("(n p) d -> n p d", p=128)
    h1v = D["h1_d"].rearrange("(n p) d -> n p d", p=128)
    with scope(C) as esp:
        bank = [C.ps(esp, "abk%d" % i, [128, 512]) for i in range(7)]
        for qt in range(16):
            sl = qt % 2
            drip(prep, 2)
            S.dma("sp", xo[sl][:], xv[48 + qt], writes=["xo%d" % sl])
            S.dma("sp", hsl[sl][:], hsv[qt], reads=["hs_d"], writes=["hsl%d" % sl])
            for h in range(8):
                hp = h // 2; rows = slice(64 * (h % 2), 64 * (h % 2) + 64); ps_ = h % 2
                bA = bank[2 * ps_]; bB = bank[2 * ps_ + 1]; bAk = "abk%d" % (2 * ps_); bBk = "abk%d" % (2 * ps_ + 1)
                for j in range(5):
                    o = bA[:, j * 128:(j + 1) * 128] if j < 4 else bB[:, 0:128]
                    ok = bAk if j < 4 else bBk
                    S.op("pe", lambda e, o=o, h=h, hp=hp, j=j, qt=qt: e.matmul(o, lhsT=kT[:, hp, (qt + j) * 128:(qt + j + 1) * 128], rhs=qZ[:, h, qt * 128:(qt + 1) * 128], start=True, stop=False),
                         reads=["kT", "qZ"], writes=[ok])
                    S.op("pe", lambda e, o=o, h=h, j=j: e.matmul(o, lhsT=idb[:], rhs=BT[:, h, j, :], start=False, stop=True), reads=["identb", "BT"], writes=[ok])
                S.op("act", lambda e, ps_=ps_, bA=bA: e.activation(out=PT[ps_][:, 0:4, :].rearrange("p j q -> p (j q)"), in_=bA[:], func=AF.Exp), reads=[bAk], writes=["PT%d" % ps_])
                S.op("act", lambda e, ps_=ps_, bB=bB: e.activation(out=PT[ps_][:, 4, :], in_=bB[:, 0:128], func=AF.Exp), reads=[bBk], writes=["PT%d" % ps_])
                ob = bank[4 + h // 4]; obk = "abk%d" % (4 + h // 4)
                for j in range(5):
                    S.op("pe", lambda e, ob=ob, h=h, j=j, ps_=ps_, qt=qt: e.matmul(ob[:, (h % 4) * 65:(h % 4) * 65 + 65], lhsT=PT[ps_][:, j, :], rhs=V[:, qt + j, h * 65:(h + 1) * 65], start=(j == 0), stop=(j == 4)),
                         reads=["PT%d" % ps_, "Vall"], writes=[obk])
            for hb in range(2):
                ov = bank[4 + hb][:, 0:260].rearrange("p (h d) -> p h d", d=65)
                S.op("dve", lambda e, hb=hb, ov=ov: e.reciprocal(out=rd[:, hb * 4:(hb + 1) * 4], in_=ov[:, :, 64]), reads=["abk%d" % (4 + hb)], writes=["rd%d" % hb])
                S.op("dve", lambda e, hb=hb, ov=ov: e.tensor_tensor(out=att[:, hb * 4:(hb + 1) * 4, :], in0=ov[:, :, 0:64], in1=bc(rd[:, hb * 4:(hb + 1) * 4].unsqueeze(2), [128, 4, 64]), op=ALU.mult),
                     reads=["abk%d" % (4 + hb), "rd%d" % hb], writes=["att%d" % hb])
            attf = att[:].rearrange("p h d -> p (h d)")
            if "att" in dbg:
                S.dma("sp", dbg["att"].rearrange("(n p) d -> n p d", p=128)[qt], attf, reads=["att0", "att1"], writes=["o_att"])
            S.op("act", lambda e: e.activation(out=junk[:], in_=attf, func=AF.Square, accum_out=st[:, 0:1]), reads=["att0", "att1"], writes=["st4_ss"])
            S.op("act", lambda e: e.activation(out=st[:, 1:2], in_=st[:, 0:1], func=AF.Sqrt, scale=1.0 / 512, bias=EPS), reads=["st4_ss"], writes=["st4_sq"])
            S.op("dve", lambda e: e.reciprocal(out=st[:, 1:2], in_=st[:, 1:2]), reads=["st4_sq"], writes=["st4"])
            S.op("dve", lambda e: e.tensor_scalar(out=an[:], in0=attf, scalar1=st[:, 1:2], scalar2=None, op0=ALU.mult), reads=["att0", "att1", "st4"], writes=["an"])
            pT = bank[6][:].bitcast(BF16).rearrange("p (c n) -> p c n", n=128)[:, 0:4, :]
            transpose_chunks(C, an[:], "an", 4, pT, "abk6", anT[:], "anT", idb)
            for cb in range(2):
                for c in range(4):
                    S.op("pe", lambda e, c=c, cb=cb: e.matmul(bank[cb][:], lhsT=anT[:, c, :], rhs=Wo[:, c, cb * 512:(cb + 1) * 512], start=(c == 0), stop=(c == 3)), reads=["anT", "Woa"], writes=["abk%d" % cb])
                S.op("dve", lambda e, cb=cb, sl=sl: e.tensor_tensor(out=h1t[sl][:, cb * 512:(cb + 1) * 512], in0=bank[cb][:], in1=xo[sl][:, cb * 512:(cb + 1) * 512], op=ALU.add),
                     reads=["abk%d" % cb, "xo%d" % sl], writes=["h1t%d" % sl])
            S.op("pool", lambda e, sl=sl: e.tensor_tensor(out=h1t[sl][:], in0=h1t[sl][:], in1=hsl[sl][:], op=ALU.add), reads=["h1t%d" % sl, "hsl%d" % sl], writes=["h1t%d" % sl])
            S.dma("sp", h1v[qt], h1t[sl][:], reads=["h1t%d" % sl], writes=["h1_d"])


def _col(g, n):
    return np.ascontiguousarray(np.asarray(g, np.float32).reshape(n, 128).T)


def host_shared(inp):
    f = lambda k: np.asarray(inp[k], np.float32)[0]
    sh = {}
    sh["g_mix"] = _col(f("norm_mix_g"), 8)
    sh["w_in"] = np.ascontiguousarray(f("w_in"))
    sh["att_q_g"] = f("att_q_g").reshape(1, 64)
    sh["att_k_g"] = f("att_k_g").reshape(1, 64)
    rb = f("rel_bias")
    p = np.arange(128); j = np.arange(5); q = np.arange(128)
    kidx = j[:, None] * 128 + p[None, :]
    kc = kidx // 64; ki = kidx % 64
    qc = q // 64; qi = q % 64
    jb = kc[:, :, None] - qc[None, None, :]
    allowed = (jb >= 0) & (jb <= 8)
    kj = jb * 64 + ki[:, :, None]
    dist = 512 + qi[None, None, :] - kj
    bucket = np.clip(np.clip(dist, -63, 128) + 63, 0, 191)
    bt = np.where(allowed[None], rb[:, bucket], np.float32(NEG))
    sh["bias_t"] = np.ascontiguousarray(bt.transpose(2, 0, 1, 3).reshape(128, 8 * 5 * 128).astype(np.float32))
    dup = lambda a: np.ascontiguousarray(np.concatenate([a, a], 0).astype(np.float32))
    sh["s5_lr"] = dup(f("ssm_lam_re").T)
    sh["s5_li"] = dup(f("ssm_lam_im").T)
    sh["s5_ls"] = np.ascontiguousarray(np.broadcast_to(f("ssm_log_step")[None, :], (128, 32)).astype(np.float32))
    sg = np.ones((128, 2), np.float32); sg[:64, 0] = -1.0; sg[64:, 1] = -1.0
    sh["s5_sg"] = sg
    sh["s5_nv"] = np.ascontiguousarray(np.broadcast_to(np.arange(-7, 16, dtype=np.float32)[None, :], (128, 23)))
    bre = f("ssm_b_re").transpose(1, 0, 2).reshape(64, 512); bim = f("ssm_b_im").transpose(1, 0, 2).reshape(64, 512)
    cre = f("ssm_c_re").transpose(2, 0, 1).reshape(64, 512); cim = f("ssm_c_im").transpose(2, 0, 1).reshape(64, 512)
    sh["s5_p1b"] = np.ascontiguousarray(np.concatenate([bre, bim], 0)); sh["s5_p2b"] = np.ascontiguousarray(np.concatenate([bim, bre], 0))
    sh["s5_p1c"] = np.ascontiguousarray(np.concatenate([cre, cim], 0)); sh["s5_p2c"] = np.ascontiguousarray(np.concatenate([cim, cre], 0))
    dd = np.zeros((2, 4, 16, 4, 4, 16), np.float32)
    dsk = f("ssm_d")
    for g in range(32):
        chc = g // 8; hb = (g % 8) // 4; j4 = g % 4
        for c in range(16):
            dd[hb, j4, c, chc, j4, c] = dsk[g, c]
    sh["s5_dd"] = dd.reshape(128, 256)
    sh["g_ssm_out"] = _col(f("ssm_out_g"), 4)
    sh["g_att_out"] = _col(f("att_out_g"), 4)
    sh["b_glu"] = f("ssm_b_glu").reshape(1, 512)
    sh["w_glu"] = np.ascontiguousarray(f("ssm_w_glu"))
    sh["w_out"] = np.ascontiguousarray(f("w_out"))
    return sh


def host_core(inp, c):
    b, seg = c // 4, c % 4
    x = np.asarray(inp["x"], np.float32)
    xe = np.zeros((8192, 1024), np.float32)
    n = (seg + 1) * 2048
    xe[8192 - n:] = x[b, :n]
    hv = np.full((512,), 1.0 if seg > 0 else 0.0, np.float32)
    return {"x_ext": xe, "hvalid": np.ascontiguousarray(hv.reshape(4, 128).T)}


IN_SHAPES = {
    "x_ext": [8192, 1024], "hvalid": [128, 4], "g_mix": [128, 8], "w_in": [1024, 2048], "att_q_g": [1, 64], "att_k_g": [1, 64],
    "bias_t": [128, 5120], "s5_lr": [128, 32], "s5_li": [128, 32], "s5_ls": [128, 32], "s5_sg": [128, 2], "s5_nv": [128, 23],
    "s5_p1b": [128, 512], "s5_p2b": [128, 512], "s5_p1c": [128, 512], "s5_p2c": [128, 512], "s5_dd": [128, 256],
    "g_ssm_out": [128, 4], "g_att_out": [128, 4], "b_glu": [1, 512], "w_glu": [512, 512], "w_out": [1024, 1024],
}
IN_SHAPES_MEM = {"mem": [256, 1024], "g_mem": [128, 8], "g_memkv": [128, 8], "mem_q_g": [1, 256], "mem_k_g": [1, 256],
                 "w_mem_q": [1024, 1024], "w_mem_k": [1024, 1024], "w_mem_v": [1024, 1024], "w_mem_o": [1024, 1024]}
IN_SHAPES_PEER = {"g_peer": [1, 1024], "w_peer_q": [1024, 2048], "keysT": [128, 2048], "peer_uT": [1024, 16384], "peer_v": [16384, 1024]}
SCRATCH = {"kT_d": ([128, 4 * 2560], BF16), "qT_d": ([128, 4 * 2048], BF16), "V_d": ([20, 128, 520], BF16),
           "hs_d": ([2048, 1024], F32), "Mz_d": ([128, 16 * 8 * 128], BF16), "CA_d": ([128, 2, 4096], BF16)}
SCRATCH_PEER = {"uT_b": ([1024, 16384], BF16), "v_b": ([16384, 1024], BF16), "xnT_d": ([16, 128, 1024], BF16),
                "sc_d": ([16, 128, 2048], F32), "tau_d": ([16, 128, 8], F32)}


def host_shared_rest(inp):
    f = lambda k: np.asarray(inp[k], np.float32)[0]
    sh = {}
    sh["g_mem"] = _col(f("norm_mem_g"), 8); sh["g_memkv"] = _col(f("norm_memkv_g"), 8)
    sh["mem_q_g"] = f("mem_q_g").reshape(1, 256); sh["mem_k_g"] = f("mem_k_g").reshape(1, 256)
    for k in ("w_mem_q", "w_mem_k", "w_mem_v", "w_mem_o", "w_peer_q"):
        sh[k] = np.ascontiguousarray(f(k))
    sh["g_peer"] = f("norm_peer_g").reshape(1, 1024)
    sh["keysT"] = np.ascontiguousarray(f("peer_keys").transpose(3, 0, 1, 2).reshape(128, 2048))
    sh["peer_uT"] = np.ascontiguousarray(f("peer_u").T)
    sh["peer_v"] = np.ascontiguousarray(f("peer_v"))
    return sh


def build_program(stages=("mixer", "mem", "peer"), dbg_specs=None, upto=9):
    nc = bass.Bass("TRN2", target_bir_lowering=False)
    D = {}
    shapes = {}
    if "mixer" in stages:
        shapes.update(IN_SHAPES)
    if "mem" in stages:
        shapes.update(IN_SHAPES_MEM)
    if "peer" in stages:
        shapes.update(IN_SHAPES_PEER)
    for k, shp in shapes.items():
        D[k] = nc.dram_tensor(k, shp, F32, kind="ExternalInput").ap()
    scr = {}
    if "mixer" in stages:
        scr.update(SCRATCH)
    if "peer" in stages:
        scr.update(SCRATCH_PEER)
    for k, (shp, dt) in scr.items():
        D[k] = nc.dram_tensor(k, shp, dt).ap()
    chain = ["h1_d", "h2_d", "out"]
    first = {"mixer": None, "mem": "h1_d", "peer": "h2_d"}[stages[0]]
    last = {"mixer": "h1_d", "mem": "h2_d", "peer": "out"}[stages[-1]]
    for k in chain:
        if k == first:
            D[k] = nc.dram_tensor(k, [2048, 1024], F32, kind="ExternalInput").ap()
        elif k == last:
            D[k] = nc.dram_tensor(k, [2048, 1024], F32, kind="ExternalOutput").ap()
        else:
            D[k] = nc.dram_tensor(k, [2048, 1024], F32).ap()
    dbg = {}
    for k, shp in (dbg_specs or {}).items():
        dbg[k] = nc.dram_tensor("dbg_" + k, shp, F32, kind="ExternalOutput").ap()
    with ExitStack() as es:
        S = Sched(nc, es)
        C = Ctx(nc, S)
        prep = stage_peer_prep(C, D) if "peer" in stages else None
        if "mixer" in stages:
            stage_mixer(C, D, dbg, upto, prep=prep)
        if "mem" in stages:
            stage_mem(C, D, dbg, prep=prep)
        drip(prep, 1000)
        if "peer" in stages:
            stage_peer(C, D, dbg)
        S.barrier()
        S.emit()
    return nc, S


def _headnorm(C, src_ap, skey, nh, hd, sqt, sqk, stat, stk, gt, gkey, dst_ap, dkey):
    S = C.S
    sv = src_ap.rearrange("p (h d) -> p h d", d=hd)
    S.op("pool", lambda e: e.tensor_tensor(out=sqt, in0=src_ap, in1=src_ap, op=ALU.mult), reads=[skey], writes=[sqk])
    S.op("dve", lambda e: e.tensor_reduce(out=stat[:, 0:nh], in_=sqt.rearrange("p (h d) -> p h d", d=hd), axis=AX.X, op=ALU.add), reads=[sqk], writes=[stk + "a"])
    S.op("act", lambda e: e.activation(out=stat[:, nh:2 * nh], in_=stat[:, 0:nh], func=AF.Sqrt, scale=1.0 / hd, bias=EPS), reads=[stk + "a"], writes=[stk + "b"])
    S.op("dve", lambda e: e.reciprocal(out=stat[:, nh:2 * nh], in_=stat[:, nh:2 * nh]), reads=[stk + "b"], writes=[stk])
    S.op("dve", lambda e: e.tensor_tensor(out=sv, in0=sv, in1=bc(stat[:, nh:2 * nh].unsqueeze(2), [128, nh, hd]), op=ALU.mult), reads=[skey, stk], writes=[skey])
    S.op("pool", lambda e: e.tensor_tensor(out=dst_ap.rearrange("p (h d) -> p h d", d=hd), in0=sv, in1=bc(gt.unsqueeze(1), [128, nh, hd]), op=ALU.mult), reads=[skey, gkey], writes=[dkey])


def stage_mem(C, D, dbg=None, prep=None):
    S = C.S
    dbg = dbg or {}
    with scope(C) as es:
        sb = lambda name, shape, dt=F32: C.sb(es, name, shape, dt)
        idf, idb = make_ident(C, es, "ident")
        gm = sb("gm", [128, 8]); gkv = sb("gkv", [128, 8]); gq = sb("mgq", [128, 256]); gk = sb("mgk", [128, 256])
        S.dma("sp", gm[:], D["g_mem"], writes=["gm"]); S.dma("sp", gkv[:], D["g_memkv"], writes=["gkv"])
        S.dma("sp", gq[:], D["mem_q_g"].partition_broadcast(128), writes=["mgq"]); S.dma("sp", gk[:], D["mem_k_g"].partition_broadcast(128), writes=["mgk"])
        S.op("dve", lambda e: e.tensor_scalar(out=gq[:], in0=gq[:], scalar1=1.0 / 16, scalar2=None, op0=ALU.mult), reads=["mgq"], writes=["mgq"])
        kTm = sb("kTm", [128, 8, 256], BF16); Vm = sb("Vm", [128, 2, 4, 257], BF16)
        xt = [sb("mxt%d" % i, [128, 1024]) for i in range(2)]; st = sb("mst", [128, 2]); xn = sb("mxn", [128, 1024], BF16)
        xnT = sb("mxnT", [128, 8, 128], BF16); qf = sb("mqf", [128, 1024]); sq = sb("msq", [128, 1024]); qst = sb("mqst", [128, 8])
        qn = sb("mqn", [128, 1024], BF16); mjunk = sb("mjunk", [128, 1024], BF16)
        with scope(C) as esk:
            with scope(C) as est:
                Wk = load_weight_bf16(C, esk, est, "Wmk", D["w_mem_k"], 8, 1024, gcol=(gkv, "gkv"))
            with scope(C) as est:
                Wv = load_weight_bf16(C, esk, est, "Wmv", D["w_mem_v"], 8, 1024, gcol=(gkv, "gkv"))
            with scope(C) as esp:
                bank = [C.ps(esp, "mkb%d" % i, [128, 512]) for i in range(6)]
                mv = D["mem"].rearrange("(n p) d -> n p d", p=128)
                for mt in range(2):
                    x_ = xt[mt]; xk = "mxt%d" % mt
                    S.dma("sp", x_[:], mv[mt], writes=[xk])
                    rms_rstd(C, x_[:], xk, mjunk[:], "mjunk", st[:, 0:1], st[:, 1:2], "mst", 1024)
                    S.op("dve", lambda e, x_=x_: e.tensor_scalar(out=xn[:], in0=x_[:], scalar1=st[:, 1:2], scalar2=None, op0=ALU.mult), reads=[xk, "mst"], writes=["mxn"])
                    pb = bank[0][:].bitcast(BF16).rearrange("p (c n) -> p c n", n=128)
                    transpose_chunks(C, xn[:], "mxn", 8, pb, "mkb0", xnT[:], "mxnT", idb)
                    for (W, wk, b0) in ((Wk, "Wmk", 1), (Wv, "Wmv", 3)):
                        for cb in range(2):
                            for dc in range(8):
                                S.op("pe", lambda e, W=W, cb=cb, dc=dc, b0=b0: e.matmul(bank[b0 + cb][:], lhsT=xnT[:, dc, :], rhs=W[:, dc, cb * 512:(cb + 1) * 512], start=(dc == 0), stop=(dc == 7)),
                                     reads=["mxnT", wk], writes=["mkb%d" % (b0 + cb)])
                    for cb in range(2):
                        S.op("act", lambda e, cb=cb: e.copy(out=qf[:, cb * 512:(cb + 1) * 512], in_=bank[1 + cb][:]), reads=["mkb%d" % (1 + cb)], writes=["mqf"])
                        S.op("dve", lambda e, cb=cb, mt=mt: e.tensor_copy(out=Vm[:, mt, 2 * cb:2 * cb + 2, 0:256], in_=bank[3 + cb][:].rearrange("p (h d) -> p h d", d=256)), reads=["mkb%d" % (3 + cb)], writes=["Vm"])
                    S.op("pool", lambda e, mt=mt: e.memset(Vm[:, mt, :, 256], 1.0), reads=[], writes=["Vm"])
                    _headnorm(C, qf[:], "mqf", 4, 256, sq[:], "msq", qst, "mqst", gk[:], "mgk", qn[:], "mqn")
                    pb2 = bank[5][:].bitcast(BF16).rearrange("p (c n) -> p c n", n=128)
                    transpose_chunks(C, qn[:], "mqn", 8, pb2, "mkb5", kTm[:, :, mt * 128:(mt + 1) * 128], "kTm", idb)
        with scope(C) as est:
            Wq = load_weight_bf16(C, es, est, "Wmq", D["w_mem_q"], 8, 1024, gcol=(gm, "gm"))
        with scope(C) as est:
            Wo = load_weight_bf16(C, es, est, "Wmo", D["w_mem_o"], 8, 1024)
        qT = sb("mqT", [128, 8, 128], BF16); PT = [sb("mPT%d" % i, [128, 2, 128], BF16) for i in range(2)]
        rd = sb("mrd", [128, 4]); ob = sb("mob", [128, 1024], BF16); oT = sb("moT", [128, 8, 128], BF16)
        h2t = [sb("h2t%d" % i, [128, 1024]) for i in range(2)]
        hv = D["h1_d"].rearrange("(n p) d -> n p d", p=128); ov = D["h2_d"].rearrange("(n p) d -> n p d", p=128)
        with scope(C) as esp:
            bank = [C.ps(esp, "mb%d" % i, [128, 512]) for i in range(8)]
            for tt in range(16):
                sl = tt % 2
                drip(prep, 1)
                x_ = xt[sl]; xk = "mxt%d" % sl
                S.dma("sp", x_[:], hv[tt], reads=["h1_d"], writes=[xk])
                rms_rstd(C, x_[:], xk, mjunk[:], "mjunk", st[:, 0:1], st[:, 1:2], "mst", 1024)
                S.op("dve", lambda e, x_=x_: e.tensor_scalar(out=xn[:], in0=x_[:], scalar1=st[:, 1:2], scalar2=None, op0=ALU.mult), reads=[xk, "mst"], writes=["mxn"])
                pb = bank[0][:].bitcast(BF16).rearrange("p (c n) -> p c n", n=128)
                transpose_chunks(C, xn[:], "mxn", 8, pb, "mb0", xnT[:], "mxnT", idb)
                for cb in range(2):
                    for dc in range(8):
                        S.op("pe", lambda e, cb=cb, dc=dc: e.matmul(bank[1 + cb][:], lhsT=xnT[:, dc, :], rhs=Wq[:, dc, cb * 512:(cb + 1) * 512], start=(dc == 0), stop=(dc == 7)),
                             reads=["mxnT", "Wmq"], writes=["mb%d" % (1 + cb)])
                    S.op("act", lambda e, cb=cb: e.copy(out=qf[:, cb * 512:(cb + 1) * 512], in_=bank[1 + cb][:]), reads=["mb%d" % (1 + cb)], writes=["mqf"])
                _headnorm(C, qf[:], "mqf", 4, 256, sq[:], "msq", qst, "mqst", gq[:], "mgq", qn[:], "mqn")
                pb2 = bank[3][:].bitcast(BF16).rearrange("p (c n) -> p c n", n=128)
                transpose_chunks(C, qn[:], "mqn", 8, pb2, "mb3", qT[:], "mqT", idb)
                for h in range(4):
                    ps_ = h % 2
                    sbk = bank[4 + ps_]; sbkk = "mb%d" % (4 + ps_)
                    for mt in range(2):
                        for dh in range(2):
                            S.op("pe", lambda e, h=h, mt=mt, dh=dh, sbk=sbk: e.matmul(sbk[:, mt * 128:(mt + 1) * 128], lhsT=kTm[:, 2 * h + dh, mt * 128:(mt + 1) * 128], rhs=qT[:, 2 * h + dh, :], start=(dh == 0), stop=(dh == 1)),
                                 reads=["kTm", "mqT"], writes=[sbkk])
                    S.op("act", lambda e, ps_=ps_, sbk=sbk: e.activation(out=PT[ps_][:].rearrange("p m q -> p (m q)"), in_=sbk[:, 0:256], func=AF.Exp, bias=-8.0), reads=[sbkk], writes=["mPT%d" % ps_])
                    obk = bank[6 + ps_]; obkk = "mb%d" % (6 + ps_)
                    for mt in range(2):
                        S.op("pe", lambda e, h=h, mt=mt, ps_=ps_, obk=obk: e.matmul(obk[:, 0:257], lhsT=PT[ps_][:, mt, :], rhs=Vm[:, mt, h, :], start=(mt == 0), stop=(mt == 1)), reads=["mPT%d" % ps_, "Vm"], writes=[obkk])
                    S.op("dve", lambda e, h=h, obk=obk: e.reciprocal(out=rd[:, h:h + 1], in_=obk[:, 256:257]), reads=[obkk], writes=["mrd%d" % h])
                    S.op("dve", lambda e, h=h, obk=obk: e.tensor_scalar(out=ob[:, h * 256:(h + 1) * 256], in0=obk[:, 0:256], scalar1=rd[:, h:h + 1], scalar2=None, op0=ALU.mult), reads=[obkk, "mrd%d" % h], writes=["mob"])
                pb3 = bank[0][:].bitcast(BF16).rearrange("p (c n) -> p c n", n=128)
                transpose_chunks(C, ob[:], "mob", 8, pb3, "mb0", oT[:], "moT", idb)
                for cb in range(2):
                    for dc in range(8):
                        S.op("pe", lambda e, cb=cb, dc=dc: e.matmul(bank[1 + cb][:], lhsT=oT[:, dc, :], rhs=Wo[:, dc, cb * 512:(cb + 1) * 512], start=(dc == 0), stop=(dc == 7)),
                             reads=["moT", "Wmo"], writes=["mb%d" % (1 + cb)])
                    S.op("dve", lambda e, cb=cb, sl=sl, x_=x_: e.tensor_tensor(out=h2t[sl][:, cb * 512:(cb + 1) * 512], in0=bank[1 + cb][:], in1=x_[:, cb * 512:(cb + 1) * 512], op=ALU.add),
                         reads=["mb%d" % (1 + cb), xk], writes=["h2t%d" % sl])
                S.dma("sp", ov[tt], h2t[sl][:], reads=["h2t%d" % sl], writes=["h2_d"])


def stage_peer_prep(C, D):
    S = C.S
    uv = D["peer_uT"].rearrange("(c p) (a e) -> c p a e", p=128, e=2048)
    ub = D["uT_b"].rearrange("(c p) (a e) -> c p a e", p=128, e=2048)
    vv = D["peer_v"].rearrange("(c p) d -> c p d", p=512)
    vb = D["v_b"].rearrange("(c p) d -> c p d", p=512)

    def gen():
        for c in range(8):
            S.dma("pool", ub[c], uv[c], writes=["uT_b"])
            yield
        for c in range(32):
            S.dma("pool", vb[c], vv[c], writes=["v_b"])
            yield
    return gen()


def drip(g, n):
    if g is None:
        return
    for _ in range(n):
        try:
            next(g)
        except StopIteration:
            return


def _top16(C, src, skey, work, wkey, dst, dkey):
    S = C.S
    S.op("dve", lambda e: e.max(out=dst[:, 0:8], in_=src), reads=[skey], writes=[dkey])
    S.op("dve", lambda e: e.match_replace(out=work, in_to_replace=dst[:, 0:8], in_values=src, imm_value=-1e30), reads=[skey, dkey], writes=[wkey])
    S.op("dve", lambda e: e.max(out=dst[:, 8:16], in_=work), reads=[wkey], writes=[dkey])


def stage_peer(C, D, dbg=None):
    S = C.S
    dbg = dbg or {}
    hv = D["h2_d"].rearrange("(n p) d -> n p d", p=128)
    with scope(C) as es:
        sb = lambda name, shape, dt=F32: C.sb(es, name, shape, dt)
        idf, idb = make_ident(C, es, "ident")
        gp = sb("gpb", [128, 1024])
        S.dma("sp", gp[:], D["g_peer"].partition_broadcast(128), writes=["gpb"])
        with scope(C) as est:
            Wq = load_weight_bf16(C, es, est, "Wpq", D["w_peer_q"], 8, 2048)
        keyT = sb("keyT", [128, 16, 128], BF16)
        with scope(C) as est:
            kst = C.sb(est, "kst", [128, 2048])
            S.dma("sp", kst[:], D["keysT"], writes=["kst"])
            S.op("dve", lambda e: e.tensor_copy(out=keyT[:].rearrange("p a n -> p (a n)"), in_=kst[:]), reads=["kst"], writes=["keyT"])
        NS = 3
        xt = [sb("pxt%d" % i, [128, 1024]) for i in range(NS)]; st_ = [sb("pst%d" % i, [128, 2]) for i in range(NS)]; junk = sb("pjunk", [128, 1024], BF16)
        xn_ = [sb("pxn%d" % i, [128, 1024], BF16) for i in range(NS)]; xnT = [sb("pxnT%d" % i, [128, 8, 128], BF16) for i in range(NS)]
        qb_ = [sb("pqb%d" % i, [128, 2048], BF16) for i in range(NS)]; qTp_ = [sb("pqT%d" % i, [128, 16, 128], BF16) for i in range(NS)]
        sc = [sb("psc%d" % i, [128, 16, 128]) for i in range(NS)]; work_ = [sb("pwork%d" % i, [128, 256]) for i in range(NS)]
        sv_ = [sb("psv%d" % i, [128, 16, 16]) for i in range(NS)]; cand_ = [sb("pcand%d" % i, [128, 8, 256]) for i in range(NS)]
        cex_ = [sb("pcex%d" % i, [128, 8, 256]) for i in range(NS)]; ctop_ = [sb("pctop%d" % i, [128, 8, 16]) for i in range(NS)]
        Z_ = [sb("pZ%d" % i, [128, 8]) for i in range(NS)]; off_ = [sb("poff%d" % i, [128, 8]) for i in range(NS)]; tau = [sb("ptau%d" % i, [128, 8]) for i in range(NS)]
        cjunk_ = [[sb("pcj%d_%d" % (i, h), [128, 256], BF16) for h in range(8)] for i in range(NS)]; offs_ = [sb("poffs%d" % i, [128, 16]) for i in range(NS)]
        xTd = D["xnT_d"].rearrange("n p (c t) -> n p c t", t=128)
        with scope(C) as esp:
            bank = [C.ps(esp, "pab%d" % i, [128, 512]) for i in range(6)]

            def tile(tt):
                sl = tt % NS
                K = lambda nm: "%s%d" % (nm, sl)
                x_ = xt[sl]; xk = K("pxt"); st = st_[sl]; xn = xn_[sl]; qb = qb_[sl]; qTp = qTp_[sl]; work = work_[sl]
                sv = sv_[sl]; cand = cand_[sl]; cex = cex_[sl]; ctop = ctop_[sl]; Z = Z_[sl]; off = off_[sl]; cjunk = cjunk_[sl]; offs = offs_[sl]
                S.dma("sp", x_[:], hv[tt], reads=["h2_d"], writes=[xk])
                rms_rstd(C, x_[:], xk, junk[:], "pjunk", st[:, 0:1], st[:, 1:2], K("pst"), 1024)
                S.op("dve", lambda e: e.scalar_tensor_tensor(out=xn[:], in0=x_[:], scalar=st[:, 1:2], in1=gp[:], op0=ALU.mult, op1=ALU.mult), reads=[xk, K("pst"), "gpb"], writes=[K("pxn")])
                pb = bank[0][:].bitcast(BF16).rearrange("p (c n) -> p c n", n=128)
                transpose_chunks(C, xn[:], K("pxn"), 8, pb, "pab0", xnT[sl][:], K("pxnT"), idb)
                S.dma("sp", xTd[tt], xnT[sl][:], reads=[K("pxnT")], writes=["xnT_d"])
                yield
                for cb in range(4):
                    for dc in range(8):
                        S.op("pe", lambda e, cb=cb, dc=dc: e.matmul(bank[1 + cb][:], lhsT=xnT[sl][:, dc, :], rhs=Wq[:, dc, cb * 512:(cb + 1) * 512], start=(dc == 0), stop=(dc == 7)),
                             reads=[K("pxnT"), "Wpq"], writes=["pab%d" % (1 + cb)])
                    S.op("act", lambda e, cb=cb: e.copy(out=qb[:, cb * 512:(cb + 1) * 512], in_=bank[1 + cb][:]), reads=["pab%d" % (1 + cb)], writes=[K("pqb")])
                for half in range(2):
                    pbq = bank[5][:].bitcast(BF16).rearrange("p (c n) -> p c n", n=128)
                    transpose_chunks(C, qb[:, half * 1024:(half + 1) * 1024], K("pqb"), 8, pbq, "pab5", qTp[:, half * 8:(half + 1) * 8, :], K("pqT"), idb)
                for hh in range(16):
                    S.op("pe", lambda e, hh=hh: e.matmul(bank[1 + hh // 4][:, (hh % 4) * 128:(hh % 4 + 1) * 128], lhsT=qTp[:, hh, :], rhs=keyT[:, hh, :], start=True, stop=True),
                         reads=[K("pqT"), "keyT"], writes=["pab%d" % (1 + hh // 4)])
                scs = sc[sl]; sck = K("psc")
                for cb in range(4):
                    S.op("act", lambda e, cb=cb: e.copy(out=scs[:, cb * 4:(cb + 1) * 4, :].rearrange("p a n -> p (a n)"), in_=bank[1 + cb][:]), reads=["pab%d" % (1 + cb)], writes=[sck])
                yield
                for hh in range(16):
                    _top16(C, scs[:, hh, :], sck, work[:, 0:128], K("pwork"), sv[:, hh, :], K("psv"))
                    yield
                svv = sv[:].rearrange("p (h s) k -> p h s k", s=2)
                for h in range(8):
                    S.op("dve", lambda e, h=h: e.tensor_tensor(out=cand[:, h, :].rearrange("p (a b) -> p a b", b=16), in0=bc(svv[:, h, 0, :].unsqueeze(2), [128, 16, 16]),
                                                            in1=bc(svv[:, h, 1, :].unsqueeze(1), [128, 16, 16]), op=ALU.add), reads=[K("psv")], writes=[K("pcand") + "_%d" % h])
                    yield
                ck = [K("pcand") + "_%d" % h for h in range(8)]
                for h in range(8):
                    _top16(C, cand[:, h, :], ck[h], work[:], K("pwork"), ctop[:, h, :], K("pctop"))
                    yield
                S.op("dve", lambda e: e.tensor_tensor(out=cand[:], in0=cand[:], in1=bc(ctop[:, :, 0:1], [128, 8, 256]), op=ALU.subtract), reads=ck + [K("pctop")], writes=ck)
                S.op("act", lambda e: e.activation(out=cex[:].rearrange("p h n -> p (h n)"), in_=cand[:].rearrange("p h n -> p (h n)"), func=AF.Exp), reads=ck, writes=[K("pcex")])
                yield
                S.op("dve", lambda e: e.tensor_tensor(out=tau[sl][:], in0=ctop[:, :, 15], in1=ctop[:, :, 0], op=ALU.subtract), reads=[K("pctop")], writes=[K("ptau")])
                yield
                S.op("dve", lambda e: e.tensor_scalar(out=tau[sl][:], in0=tau[sl][:], scalar1=-1e-5, scalar2=None, op0=ALU.add), reads=[K("ptau")], writes=[K("ptau")])
                yield
                for h in range(8):
                    S.op("dve", lambda e, h=h: e.scalar_tensor_tensor(out=cjunk[h][:], in0=cand[:, h, :], scalar=tau[sl][:, h:h + 1], in1=cex[:, h, :], op0=ALU.is_ge, op1=ALU.mult, accum_out=Z[:, h:h + 1]),
                         reads=ck + [K("pcex"), K("ptau")], writes=[K("pZ") + "_%d" % h, K("pcj") + "_%d" % h])
                    yield
                S.op("act", lambda e: e.activation(out=off[:], in_=Z[:], func=AF.Ln), reads=[K("pZ") + "_%d" % h for h in range(8)], writes=[K("poff")])
                yield
                S.op("dve", lambda e: e.tensor_tensor(out=tau[sl][:], in0=tau[sl][:], in1=off[:], op=ALU.subtract), reads=[K("ptau"), K("poff")], writes=[K("ptau")])
                S.op("act", lambda e: e.activation(out=tau[sl][:], in_=tau[sl][:], func=AF.Exp), reads=[K("ptau")], writes=[K("ptau")])
                yield
                S.op("dve", lambda e: e.tensor_scalar(out=tau[sl][:], in0=tau[sl][:], scalar1=0.99997, scalar2=None, op0=ALU.mult), reads=[K("ptau")], writes=[K("ptau")])
                offv = offs[:].rearrange("p (h s) -> p h s", s=2)
                S.op("dve", lambda e: e.tensor_copy(out=offv[:, :, 0], in_=svv[:, :, 0, 0]), reads=[K("psv")], writes=[K("poffs") + "a"])
                yield
                S.op("dve", lambda e: e.tensor_tensor(out=offv[:, :, 1], in0=svv[:, :, 1, 0], in1=off[:], op=ALU.add), reads=[K("psv"), K("poff")], writes=[K("poffs") + "b"])
                yield
                S.op("dve", lambda e: e.tensor_tensor(out=scs[:], in0=scs[:], in1=bc(offs[:].unsqueeze(2), [128, 16, 128]), op=ALU.subtract), reads=[sck, K("poffs") + "a", K("poffs") + "b"], writes=[sck])
                S.op("act", lambda e: e.activation(out=scs[:].rearrange("p a n -> p (a n)"), in_=scs[:].rearrange("p a n -> p (a n)"), func=AF.Exp), reads=[sck], writes=[sck])
                S.dma("sp", D["sc_d"][tt], scs[:].rearrange("p a n -> p (a n)"), reads=[sck], writes=["sc_d"])
                S.dma("sp", D["tau_d"][tt], tau[sl][:], reads=[K("ptau")], writes=["tau_d"])

            from itertools import zip_longest
            for t0 in range(0, 16, NS):
                for _ in zip_longest(*[tile(t0 + i) for i in range(NS) if t0 + i < 16]):
                    pass
    with scope(C) as es:
        sb = lambda name, shape, dt=F32: C.sb(es, name, shape, dt)
        idf, idb = make_ident(C, es, "ident")
        xnT = sb("bxnT", [128, 4, 1024], BF16); sc = sb("bsc", [128, 4, 2048]); kap = sb("btau", [128, 4, 8])
        UT = [sb("UT%d" % i, [128, 8, 1024], BF16) for i in range(2)]; Vb = [sb("Vb%d" % i, [128, 8, 1024], BF16) for i in range(2)]
        acc = sb("pacc", [128, 4, 1024])
        NP = 12
        Pt = [sb("pP%d" % i, [128, 512]) for i in range(NP)]; Wh = [[sb("pWh%d_%d" % (i, h), [128, 512], BF16) for h in range(8)] for i in range(2)]
        G = [sb("pG%d" % i, [128, 512], BF16) for i in range(3)]; WA = [sb("pWA%d" % i, [128, 512], BF16) for i in range(2)]
        WAT = [sb("pWAT%d" % i, [128, 4, 128], BF16) for i in range(2)]
        h2t = [sb("ph2t%d" % i, [128, 1024]) for i in range(2)]
        uTv = D["uT_b"].rearrange("(c p) (b e) -> b p c e", p=128, e=1024)
        vbv = D["v_b"].rearrange("(b c p) d -> b p c d", p=128, c=8)
        ov = D["out"].rearrange("(n p) d -> n p d", p=128)
        with scope(C) as esp:
            bank = [C.ps(esp, "pbb%d" % i, [128, 512]) for i in range(7)]
            state = {"it": 0}

            def stage1a(u, tg, eb, sub, tt, es_):
                ub = u % 2
                xT = xnT[:, tt, :].rearrange("p (c t) -> p c t", t=128)
                scv = sc[:, tt, :].rearrange("p (h s n) -> p h s n", s=2, n=128)
                i0 = eb * 8 + sub * 4
                for dc in range(8):
                    S.op("pe", lambda e, dc=dc, xT=xT: e.matmul(bank[ub][:], lhsT=xT[:, dc, :], rhs=UT[es_][:, dc, sub * 512:(sub + 1) * 512], start=(dc == 0), stop=(dc == 7)),
                         reads=["bxnT", "UT%d" % es_], writes=["pbb%d" % ub])
                for h in range(8):
                    hs = state["it"] % NP; state["it"] += 1
                    if h >= 5:
                        S.op("pool", lambda e, h=h, hs=hs, scv=scv: e.tensor_tensor(out=Pt[hs][:].rearrange("p (i j) -> p i j", j=128), in0=bc(scv[:, h, 0, i0:i0 + 4].unsqueeze(2), [128, 4, 128]),
                                                                             in1=bc(scv[:, h, 1, :].unsqueeze(1), [128, 4, 128]), op=ALU.mult), reads=["bsc"], writes=["pP%d_%d" % (hs, il) for il in range(4)])
                    else:
                        for il in range(4):
                            S.op("act", lambda e, h=h, hs=hs, scv=scv, il=il: e.activation(out=Pt[hs][:, il * 128:(il + 1) * 128], in_=scv[:, h, 1, :], func=AF.Copy, scale=scv[:, h, 0, i0 + il:i0 + il + 1]),
                                 reads=["bsc"], writes=["pP%d_%d" % (hs, il)])
                    S.op("dve", lambda e, h=h, hs=hs: e.scalar_tensor_tensor(out=Wh[ub][h][:], in0=Pt[hs][:], scalar=kap[:, tt, h:h + 1], in1=Pt[hs][:], op0=ALU.is_ge, op1=ALU.mult),
                         reads=["pP%d_%d" % (hs, il) for il in range(4)] + ["btau"], writes=["pWh%d_%d" % (ub, h)])

            def st_gelu(u, tg, eb, sub, tt, es_):
                ub = u % 2; gb = u % 3
                S.op("act", lambda e: e.activation(out=G[gb][:], in_=bank[ub][:], func=AF.Gelu), reads=["pbb%d" % ub], writes=["pG%d" % gb])

            def st_hs(u, tg, eb, sub, tt, es_):
                ub = u % 2
                for h in range(8):
                    S.op("pe", lambda e, h=h: e.matmul(bank[2 + ub][:], lhsT=idb[:], rhs=Wh[ub][h][:], start=(h == 0), stop=(h == 7)),
                         reads=["identb", "pWh%d_%d" % (ub, h)], writes=["pbb%d" % (2 + ub)])

            def st_wa(u, tg, eb, sub, tt, es_):
                ub = u % 2; gb = u % 3
                S.op("dve", lambda e: e.tensor_tensor(out=WA[ub][:], in0=bank[2 + ub][:], in1=G[gb][:], op=ALU.mult), reads=["pbb%d" % (2 + ub), "pG%d" % gb], writes=["pWA%d" % ub])

            def st_t(u, tg, eb, sub, tt, es_):
                ub = u % 2
                pbt = bank[4][:].bitcast(BF16).rearrange("p (c n) -> p c n", n=128)[:, 0:4, :]
                for c in range(4):
                    S.op("pe", lambda e, c=c: e.transpose(out=pbt[:, c, :], in_=WA[ub][:, c * 128:(c + 1) * 128], identity=idb[:]), reads=["pWA%d" % ub, "identb"], writes=["pbb4"])

            def st_watcopy(u, tg, eb, sub, tt, es_):
                ub = u % 2
                pbt = bank[4][:].bitcast(BF16).rearrange("p (c n) -> p c n", n=128)[:, 0:4, :]
                S.op("act", lambda e: e.copy(out=WAT[ub][:], in_=pbt), reads=["pbb4"], writes=["pWAT%d" % ub])

            def st_v(u, tg, eb, sub, tt, es_):
                ub = u % 2
                for cb in range(2):
                    for ec in range(4):
                        S.op("pe", lambda e, cb=cb, ec=ec: e.matmul(bank[5 + cb][:], lhsT=WAT[ub][:, ec, :], rhs=Vb[es_][:, sub * 4 + ec, cb * 512:(cb + 1) * 512], start=(sub == 0 and ec == 0), stop=(sub == 1 and ec == 3)),
                             reads=["pWAT%d" % ub, "Vb%d" % es_], writes=["pbb%d" % (5 + cb)])

            def st_acc(u, tg, eb, sub, tt, es_):
                if sub != 1:
                    return
                for cb in range(2):
                    if eb == 0:
                        S.op("dve", lambda e, cb=cb: e.tensor_copy(out=acc[:, tt, cb * 512:(cb + 1) * 512], in_=bank[5 + cb][:]), reads=["pbb%d" % (5 + cb)], writes=["pacc%d" % tt])
                    else:
                        S.op("dve", lambda e, cb=cb: e.tensor_tensor(out=acc[:, tt, cb * 512:(cb + 1) * 512], in0=bank[5 + cb][:], in1=acc[:, tt, cb * 512:(cb + 1) * 512], op=ALU.add),
                             reads=["pbb%d" % (5 + cb), "pacc%d" % tt], writes=["pacc%d" % tt])

            u = 0
            for tg in range(4):
                S.dma("sp", xnT[:], D["xnT_d"][tg * 4:(tg + 1) * 4].rearrange("n p f -> p n f"), reads=["xnT_d"], writes=["bxnT"])
                S.dma("sp", sc[:], D["sc_d"][tg * 4:(tg + 1) * 4].rearrange("n p f -> p n f"), reads=["sc_d"], writes=["bsc"])
                S.dma("sp", kap[:], D["tau_d"][tg * 4:(tg + 1) * 4].rearrange("n p f -> p n f"), reads=["tau_d"], writes=["btau"])
                units = []
                for eb in range(16):
                    es_ = (tg * 16 + eb) % 2
                    for tt in range(4):
                        for sub in range(2):
                            units.append((u, tg, eb, sub, tt, es_)); u += 1
                n = len(units)
                U = lambda j: units[j] if 0 <= j < n else None
                for k in range(n + 3):
                    for ebk in ([0] if k == 0 else []) + ([k // 8 + 1] if (k % 8 == 3 and k // 8 + 1 < 16) else []):
                        es_ = (tg * 16 + ebk) % 2
                        S.dma("sp", UT[es_][:], uTv[ebk], reads=["uT_b"], writes=["UT%d" % es_])
                        S.dma("sp", Vb[es_][:], vbv[ebk], reads=["v_b"], writes=["Vb%d" % es_])
                    if U(k - 2): st_wa(*U(k - 2))
                    if U(k - 3): st_v(*U(k - 3))
                    if U(k - 2): st_t(*U(k - 2))
                    if U(k - 1): st_gelu(*U(k - 1))
                    if U(k - 1): st_hs(*U(k - 1))
                    if U(k): stage1a(*U(k))
                    if U(k - 2): st_watcopy(*U(k - 2))
                    if U(k - 3): st_acc(*U(k - 3))
                for tt in range(4):
                    sl = tt % 2; n = tg * 4 + tt
                    S.dma("sp", h2t[sl][:], hv[n], reads=["h2_d"], writes=["ph2t%d" % sl])
                    S.op("pool", lambda e, sl=sl, tt=tt: e.tensor_tensor(out=h2t[sl][:], in0=h2t[sl][:], in1=acc[:, tt, :], op=ALU.add), reads=["ph2t%d" % sl, "pacc%d" % tt], writes=["ph2t%d" % sl])
                    S.dma("sp", ov[n], h2t[sl][:], reads=["ph2t%d" % sl], writes=["out"])


_PROG = {}


def kernel(**inputs):
    sh = host_shared(inputs)
    sh.update(host_shared_rest(inputs))
    if "nc" not in _PROG:
        _PROG["nc"] = build_program(("mixer", "mem", "peer"))[0]
    nc = _PROG["nc"]
    names = set(IN_SHAPES) | set(IN_SHAPES_MEM) | set(IN_SHAPES_PEER)
    mem = np.asarray(inputs["mem"], np.float32)
    maps = []
    for c in range(8):
        m = {k: v for k, v in sh.items() if k in names}
        m.update(host_core(inputs, c))
        m["mem"] = np.ascontiguousarray(mem[c // 4])
        maps.append(m)
    res = run_bass_kernel_spmd(nc, maps, core_ids=list(range(8)))
    out = np.zeros((2, 8192, 1024), np.float32)
    for c in range(8):
        out[c // 4, (c % 4) * 2048:(c % 4 + 1) * 2048] = res.results[c]["out"]
    return out
```

```python
from contextlib import ExitStack, contextmanager
import numpy as np
import concourse.bass as bass
import concourse.mybir as mybir
from concourse.bass_utils import run_bass_kernel_spmd

F32 = mybir.dt.float32
BF16 = mybir.dt.bfloat16
I32 = mybir.dt.int32
AF = mybir.ActivationFunctionType
ALU = mybir.AluOpType
AX = mybir.AxisListType

ENGS = ("pe", "act", "dve", "pool", "sp")
TWO_PI = 6.283185307179586
EPS = 1e-6
NEG = -30000.0


class Sched:
    def __init__(self, nc, es, n_dma_sems=32):
        self.nc = nc
        self.streams = {e: [] for e in ENGS}
        self.sem = {e: es.enter_context(nc.semaphore("s_" + e)) for e in ("pe", "act", "dve", "pool")}
        self.cnt = {e: 0 for e in ("pe", "act", "dve", "pool")}
        self.dsem = [es.enter_context(nc.semaphore("s_dma%d" % i)) for i in range(n_dma_sems)]
        self.dcnt = [0] * n_dma_sems
        self.dnext = 0
        self.n_sw = 4
        self.dnext_sw = 0
        self.waited = {}
        self.last_w = {}
        self.readers = {}
        self.n_ops = 0

    def _deps(self, eng, reads, writes):
        deps = []
        for k in reads:
            if k in self.last_w:
                deps.append(self.last_w[k])
        for k in writes:
            if k in self.last_w:
                deps.append(self.last_w[k])
            deps.extend(self.readers.get(k, ()))
        need = {}
        for (sk, val, peng) in deps:
            if peng == "pe" and eng == "pe":
                continue
            if self.waited.get((eng, sk), 0) >= val:
                continue
            if need.get(sk, 0) < val:
                need[sk] = val
        return need

    def _semobj(self, sk):
        return self.sem[sk] if isinstance(sk, str) else self.dsem[sk]

    def _emit_waits(self, eng, need):
        for sk, val in need.items():
            self.waited[(eng, sk)] = val
            so = self._semobj(sk)
            self.streams[eng].append(lambda e, so=so, val=val: e.wait_ge(so, val))

    def _record(self, tok, reads, writes):
        for k in writes:
            self.last_w[k] = tok
            self.readers[k] = []
        for k in reads:
            if k not in writes:
                self.readers.setdefault(k, []).append(tok)

    def op(self, eng, fn, reads=(), writes=()):
        need = self._deps(eng, reads, writes)
        self._emit_waits(eng, need)
        self.cnt[eng] += 1
        val = self.cnt[eng]
        so = self.sem[eng]
        self.streams[eng].append(lambda e, fn=fn, so=so: fn(e).then_inc(so, 1))
        self._record((eng, val, eng), reads, writes)
        self.n_ops += 1

    def dma(self, q, out, in_, reads=(), writes=(), **kw):
        nhw = len(self.dsem) - self.n_sw
        if q == "pool":
            i = nhw + self.dnext_sw
            self.dnext_sw = (self.dnext_sw + 1) % self.n_sw
        else:
            i = self.dnext
            self.dnext = (self.dnext + 1) % nhw
        need = self._deps(q, reads, writes)
        prev = 16 * self.dcnt[i]
        if prev and self.waited.get((q, i), 0) < prev:
            need[i] = max(need.get(i, 0), prev)
        self._emit_waits(q, need)
        self.dcnt[i] += 1
        val = 16 * self.dcnt[i]
        so = self.dsem[i]
        self.streams[q].append(
            lambda e, out=out, in_=in_, so=so, kw=kw: e.dma_start(out=out, in_=in_, **kw).then_inc(so, 16))
        self._record((i, val, "dma"), reads, writes)
        self.n_ops += 1

    def barrier(self):
        for eng in ENGS:
            need = {}
            for pe_ in ("pe", "act", "dve", "pool"):
                v = self.cnt[pe_]
                if v and self.waited.get((eng, pe_), 0) < v:
                    need[pe_] = v
            for i, c in enumerate(self.dcnt):
                if c and self.waited.get((eng, i), 0) < 16 * c:
                    need[i] = 16 * c
            self._emit_waits(eng, need)

    def wait_all(self, eng, keys):
        need = {}
        for k in keys:
            if k in self.last_w:
                sk, val, _ = self.last_w[k]
                if self.waited.get((eng, sk), 0) < val and need.get(sk, 0) < val:
                    need[sk] = val
        self._emit_waits(eng, need)

    def emit(self):
        if not any(self.streams[e] for e in ENGS):
            return
        streams = self.streams
        self.streams = {e: [] for e in ENGS}
        self._emit_block(streams)

    def _emit_block(self, streams):
        self_streams = streams
        with self.nc.Block() as block:
            @block.tensor
            def _(e):
                for f in self_streams["pe"]:
                    f(e)

            @block.scalar
            def _(e):
                for f in self_streams["act"]:
                    f(e)

            @block.vector
            def _(e):
                for f in self_streams["dve"]:
                    f(e)

            @block.gpsimd
            def _(e):
                for f in self_streams["pool"]:
                    f(e)

            @block.sync
            def _(e):
                for f in self_streams["sp"]:
                    f(e)


class Ctx:
    def __init__(self, nc, S):
        self.nc = nc
        self.S = S
        self.uid = 0

    def sb(self, es, name, shape, dt=F32):
        self.uid += 1
        return es.enter_context(self.nc.sbuf_tensor("%s_%d" % (name, self.uid), list(shape), dt))

    def ps(self, es, name, shape, dt=F32):
        self.uid += 1
        return es.enter_context(self.nc.psum_tensor("%s_%d" % (name, self.uid), list(shape), dt))


@contextmanager
def scope(C):
    with ExitStack() as es:
        yield es
        C.S.barrier()
        C.S.emit()


def bc(ap, shape):
    return ap.to_broadcast(list(shape))


def make_ident(C, es, name="ident"):
    S = C.S
    idf = C.sb(es, name + "f", [128, 128])
    idb = C.sb(es, name + "b", [128, 128], BF16)
    S.op("pool", lambda e: e.memset(idf[:], 1.0), writes=[name + "f"])
    S.op("pool", lambda e: e.affine_select(out=idf[:], in_=idf[:], pattern=[[-1, 128]], compare_op=ALU.is_equal,
                                           fill=0.0, base=0, channel_multiplier=1), reads=[name + "f"], writes=[name + "f"])
    S.op("dve", lambda e: e.tensor_copy(out=idb[:], in_=idf[:]), reads=[name + "f"], writes=[name + "b"])
    return idf, idb


def sincos(C, es, ang, n, tag):
    S = C.S
    outs = []
    for which, off in (("s", 64.0), ("c", 64.25)):
        k = tag + which
        y = C.sb(es, k + "y", [128, n]); yi = C.sb(es, k + "yi", [128, n], I32); yf = C.sb(es, k + "yf", [128, n])
        m = C.sb(es, k + "m", [128, n]); o = C.sb(es, k + "o", [128, n])
        S.op("dve", lambda e, y=y, off=off: e.tensor_scalar(out=y[:], in0=ang, scalar1=1.0 / TWO_PI, scalar2=off, op0=ALU.mult, op1=ALU.add),
             reads=[tag + "ang"], writes=[k + "y"])
        S.op("dve", lambda e, y=y, yi=yi: e.tensor_copy(out=yi[:], in_=y[:]), reads=[k + "y"], writes=[k + "yi"])
        S.op("dve", lambda e, yi=yi, yf=yf: e.tensor_copy(out=yf[:], in_=yi[:]), reads=[k + "yi"], writes=[k + "yf"])
        S.op("dve", lambda e, y=y, yf=yf: e.tensor_tensor(out=y[:], in0=y[:], in1=yf[:], op=ALU.subtract), reads=[k + "y", k + "yf"], writes=[k + "y"])
        S.op("dve", lambda e, y=y, m=m: e.tensor_scalar(out=m[:], in0=y[:], scalar1=0.5, scalar2=None, op0=ALU.is_gt), reads=[k + "y"], writes=[k + "m"])
        S.op("dve", lambda e, y=y, m=m: e.tensor_tensor(out=y[:], in0=y[:], in1=m[:], op=ALU.subtract), reads=[k + "y", k + "m"], writes=[k + "y"])
        S.op("act", lambda e, y=y, o=o: e.activation(out=o[:], in_=y[:], func=AF.Sin, scale=TWO_PI), reads=[k + "y"], writes=[k + "o"])
        outs.append((o, k + "o"))
    return outs


def s5_params(C, es_keep, D):
    S, nc = C.S, C.nc
    P = {}
    LT2 = C.sb(es_keep, "LT2", [128, 8, 8, 2, 128], BF16)
    WPr = C.sb(es_keep, "WPr", [128, 9, 16]); WPi = C.sb(es_keep, "WPi", [128, 9, 16]); WPn = C.sb(es_keep, "WPn", [128, 9, 16])
    P.update(LT2=LT2, WPr=WPr, WPi=WPi, WPn=WPn)
    with scope(C) as es:
        sb = lambda name, shape, dt=F32: C.sb(es, name, shape, dt)
        Mz2 = sb("Mz2", [128, 16, 8, 128], BF16)
        CA = sb("CA", [128, 32, 128], BF16); CAs = sb("CAs", [128, 32, 128], BF16)
        LR = sb("LR", [128, 32]); LI = sb("LI", [128, 32]); LS = sb("LS", [128, 32])
        SG = sb("SG", [128, 2]); NV = sb("NV", [128, 23])
        P1B = sb("P1B", [128, 32, 16]); P2B = sb("P2B", [128, 32, 16]); P1C = sb("P1C", [128, 32, 16]); P2C = sb("P2C", [128, 32, 16])
        DD = sb("DD", [128, 16, 16])
        for t, nm in ((LR, "s5_lr"), (LI, "s5_li"), (LS, "s5_ls"), (SG, "s5_sg"), (NV, "s5_nv")):
            S.dma("sp", t[:], D[nm], writes=[nm])
        S.dma("sp", DD[:].rearrange("p e c -> p (e c)"), D["s5_dd"], writes=["s5_dd"])
        for t, nm in ((P1B, "s5_p1b"), (P2B, "s5_p2b"), (P1C, "s5_p1c"), (P2C, "s5_p2c")):
            S.dma("sp", t[:].rearrange("p g c -> p (g c)"), D[nm], writes=[nm])
        idf, idb = make_ident(C, es, "pid")
        STEP = sb("STEP", [128, 32]); AA = sb("AA", [128, 32]); PH = sb("PH", [128, 32])
        S.op("act", lambda e: e.activation(out=STEP[:], in_=LS[:], func=AF.Exp), reads=["s5_ls"], writes=["STEP"])
        S.op("dve", lambda e: e.tensor_tensor(out=AA[:], in0=LR[:], in1=STEP[:], op=ALU.mult), reads=["s5_lr", "STEP"], writes=["AA"])
        S.op("dve", lambda e: e.tensor_tensor(out=PH[:], in0=LI[:], in1=STEP[:], op=ALU.mult), reads=["s5_li", "STEP"], writes=["PH"])
        EXPO = sb("EXPO", [128, 32, 23]); ANG = sb("ANG", [128, 32, 23]); MAG = sb("MAG", [128, 32, 23])
        nvb = bc(NV[:].unsqueeze(1), [128, 32, 23])
        S.op("dve", lambda e: e.tensor_tensor(out=EXPO[:], in0=bc(AA[:].unsqueeze(2), [128, 32, 23]), in1=nvb, op=ALU.mult), reads=["AA", "s5_nv"], writes=["EXPO"])
        S.op("dve", lambda e: e.tensor_tensor(out=ANG[:], in0=bc(PH[:].unsqueeze(2), [128, 32, 23]), in1=nvb, op=ALU.mult), reads=["PH", "s5_nv"], writes=["pwang"])
        S.op("act", lambda e: e.activation(out=MAG[:], in_=EXPO[:], func=AF.Exp), reads=["EXPO"], writes=["MAG"])
        (sn, snk), (cs, csk) = sincos(C, es, ANG[:].rearrange("p g n -> p (g n)"), 32 * 23, "pw")
        CR = sb("CR", [128, 32, 23]); CI = sb("CI", [128, 32, 23])
        S.op("dve", lambda e: e.tensor_tensor(out=CR[:].rearrange("p g n -> p (g n)"), in0=MAG[:].rearrange("p g n -> p (g n)"), in1=cs[:], op=ALU.mult), reads=["MAG", csk], writes=["CR"])
        S.op("dve", lambda e: e.tensor_tensor(out=CI[:].rearrange("p g n -> p (g n)"), in0=MAG[:].rearrange("p g n -> p (g n)"), in1=sn[:], op=ALU.mult), reads=["MAG", snk], writes=["CI"])
        zr = sb("zr", [128, 32]); den = sb("den", [128, 32]); t0 = sb("t0", [128, 32]); fr = sb("fr", [128, 32]); fi = sb("fi", [128, 32])
        S.op("dve", lambda e: e.tensor_scalar(out=zr[:], in0=CR[:, :, 8], scalar1=-1.0, scalar2=None, op0=ALU.add), reads=["CR"], writes=["zr"])
        S.op("dve", lambda e: e.tensor_tensor(out=den[:], in0=LR[:], in1=LR[:], op=ALU.mult), reads=["s5_lr"], writes=["den"])
        S.op("dve", lambda e: e.tensor_tensor(out=t0[:], in0=LI[:], in1=LI[:], op=ALU.mult), reads=["s5_li"], writes=["t0"])
        S.op("dve", lambda e: e.tensor_tensor(out=den[:], in0=den[:], in1=t0[:], op=ALU.add), reads=["den", "t0"], writes=["den"])
        S.op("dve", lambda e: e.reciprocal(out=den[:], in_=den[:]), reads=["den"], writes=["den"])
        S.op("dve", lambda e: e.tensor_tensor(out=fr[:], in0=zr[:], in1=LR[:], op=ALU.mult), reads=["zr", "s5_lr"], writes=["fr"])
        S.op("dve", lambda e: e.tensor_tensor(out=t0[:], in0=CI[:, :, 8], in1=LI[:], op=ALU.mult), reads=["CI", "s5_li", "den"], writes=["t0"])
        S.op("dve", lambda e: e.tensor_tensor(out=fr[:], in0=fr[:], in1=t0[:], op=ALU.add), reads=["fr", "t0"], writes=["fr"])
        S.op("dve", lambda e: e.tensor_tensor(out=fr[:], in0=fr[:], in1=den[:], op=ALU.mult), reads=["fr", "den"], writes=["fr"])
        S.op("dve", lambda e: e.tensor_tensor(out=fi[:], in0=CI[:, :, 8], in1=LR[:], op=ALU.mult), reads=["CI", "s5_lr"], writes=["fi"])
        S.op("dve", lambda e: e.tensor_tensor(out=t0[:], in0=zr[:], in1=LI[:], op=ALU.mult), reads=["zr", "s5_li", "fr"], writes=["t0"])
        S.op("dve", lambda e: e.tensor_tensor(out=fi[:], in0=fi[:], in1=t0[:], op=ALU.subtract), reads=["fi", "t0"], writes=["fi"])
        S.op("dve", lambda e: e.tensor_tensor(out=fi[:], in0=fi[:], in1=den[:], op=ALU.mult), reads=["fi", "den"], writes=["fi"])
        BB1 = sb("BB1", [128, 32, 16]); BB2 = sb("BB2", [128, 32, 16]); ta = sb("ta", [128, 32, 16]); tb = sb("tb", [128, 32, 16])
        frb = bc(fr[:].unsqueeze(2), [128, 32, 16]); fib = bc(fi[:].unsqueeze(2), [128, 32, 16])
        fl = lambda t: t[:].rearrange("p g c -> p (g c)")
        S.op("dve", lambda e: e.tensor_tensor(out=ta[:], in0=P1B[:], in1=frb, op=ALU.mult), reads=["s5_p1b", "fr"], writes=["ta"])
        S.op("dve", lambda e: e.tensor_tensor(out=tb[:], in0=P2B[:], in1=fib, op=ALU.mult), reads=["s5_p2b", "fi"], writes=["tb"])
        S.op("dve", lambda e: e.scalar_tensor_tensor(out=fl(BB1), in0=fl(tb), scalar=SG[:, 0:1], in1=fl(ta), op0=ALU.mult, op1=ALU.add), reads=["ta", "tb", "s5_sg"], writes=["BB1"])
        S.op("dve", lambda e: e.tensor_tensor(out=ta[:], in0=P2B[:], in1=frb, op=ALU.mult), reads=["s5_p2b", "fr", "BB1"], writes=["ta"])
        S.op("dve", lambda e: e.tensor_tensor(out=tb[:], in0=P1B[:], in1=fib, op=ALU.mult), reads=["s5_p1b", "fi", "BB1"], writes=["tb"])
        S.op("dve", lambda e: e.scalar_tensor_tensor(out=fl(BB2), in0=fl(tb), scalar=SG[:, 1:2], in1=fl(ta), op0=ALU.mult, op1=ALU.add), reads=["ta", "tb", "s5_sg"], writes=["BB2"])
        t5 = sb("t5", [128, 32, 16]); t6 = sb("t6", [128, 32, 16])
        Rm = sb("Rm", [128, 32, 8, 16])
        Q1 = sb("Q1", [128, 32, 16]); Q2 = sb("Q2", [128, 32, 16])
        S.op("dve", lambda e: e.tensor_scalar(out=fl(Q1), in0=fl(P1C), scalar1=SG[:, 1:2], scalar2=None, op0=ALU.mult), reads=["s5_p1c", "s5_sg"], writes=["Q1"])
        S.op("dve", lambda e: e.tensor_scalar(out=fl(Q2), in0=fl(P2C), scalar1=SG[:, 0:1], scalar2=None, op0=ALU.mult), reads=["s5_p2c", "s5_sg"], writes=["Q2"])
        CAv = CA[:].rearrange("p g (t c) -> p g t c", c=16); CAsv = CAs[:].rearrange("p g (t c) -> p g t c", c=16)
        for t in range(8):
            for (dst, dkey, a_, akey, b_, bkey, idx) in ((Rm[:, :, t, :], "Rm", Q1, "Q1", P2C, "s5_p2c", 7 + t),
                                                         (CAv[:, :, t, :], "CA", Q1, "Q1", P2C, "s5_p2c", 15 + t),
                                                         (CAsv[:, :, t, :], "CAs", Q2, "Q2", P1C, "s5_p1c", 15 + t)):
                S.op("dve", lambda e, a_=a_, idx=idx: e.tensor_tensor(out=t5[:], in0=a_[:], in1=bc(CR[:, :, idx:idx + 1], [128, 32, 16]), op=ALU.mult), reads=[akey, "CR"], writes=["t5"])
                S.op("dve", lambda e, b_=b_, idx=idx: e.tensor_tensor(out=t6[:], in0=b_[:], in1=bc(CI[:, :, idx:idx + 1], [128, 32, 16]), op=ALU.mult), reads=[bkey, "CI"], writes=["t6"])
                S.op("dve", lambda e, dst=dst: e.tensor_tensor(out=dst, in0=t5[:], in1=t6[:], op=ALU.subtract), reads=["t5", "t6"], writes=[dkey])
        Wr = sb("Wr", [128, 9, 32]); Wi = sb("Wi", [128, 9, 32]); sq = sb("sq", [128, 32])
        S.op("dve", lambda e: e.tensor_copy(out=Wr[:, 0, :], in_=CR[:, :, 15]), reads=["CR"], writes=["Wr"])
        S.op("dve", lambda e: e.tensor_copy(out=Wi[:, 0, :], in_=CI[:, :, 15]), reads=["CI"], writes=["Wi"])
        for j in range(8):
            S.op("dve", lambda e, j=j: e.tensor_tensor(out=Wr[:, j + 1, :], in0=Wr[:, j, :], in1=Wr[:, j, :], op=ALU.mult), reads=["Wr"], writes=["Wr"])
            S.op("dve", lambda e, j=j: e.tensor_tensor(out=sq[:], in0=Wi[:, j, :], in1=Wi[:, j, :], op=ALU.mult), reads=["Wi"], writes=["sq"])
            S.op("dve", lambda e, j=j: e.tensor_tensor(out=Wr[:, j + 1, :], in0=Wr[:, j + 1, :], in1=sq[:], op=ALU.subtract), reads=["Wr", "sq"], writes=["Wr"])
            S.op("dve", lambda e, j=j: e.scalar_tensor_tensor(out=Wi[:, j + 1, :], in0=Wr[:, j, :], scalar=2.0, in1=Wi[:, j, :], op0=ALU.mult, op1=ALU.mult), reads=["Wr", "Wi"], writes=["Wi"])
        Wrv = Wr[:].rearrange("p j (a r w) -> p j a r w", r=2, w=2); Wiv = Wi[:].rearrange("p j (a r w) -> p j a r w", r=2, w=2)
        for r in range(2):
            pr_ = slice(64 * r, 64 * r + 64)
            for j in range(9):
                S.op("dve", lambda e, r=r, pr_=pr_, j=j: e.tensor_copy(out=WPr[pr_, j, :].rearrange("p (a w) -> p a w", w=2), in_=Wrv[pr_, j, :, r, :]), reads=["Wr"], writes=["WPr"])
                S.op("dve", lambda e, r=r, pr_=pr_, j=j: e.tensor_copy(out=WPi[pr_, j, :].rearrange("p (a w) -> p a w", w=2), in_=Wiv[pr_, j, :, r, :]), reads=["Wi"], writes=["WPi"])
        S.op("dve", lambda e: e.tensor_scalar(out=WPn[:].rearrange("p j q -> p (j q)"), in0=WPi[:].rearrange("p j q -> p (j q)"), scalar1=-1.0, scalar2=None, op0=ALU.mult), reads=["WPi"], writes=["WPn"])
        E = [[sb("E%d%d" % (h_, r), [128, 128]) for r in range(2)] for h_ in range(2)]
        for h_ in range(2):
            for r in range(2):
                S.op("pool", lambda e, h_=h_, r=r: e.memset(E[h_][r][:], 0.0), writes=["E%d%d" % (h_, r)])
                S.op("pool", lambda e, h_=h_, r=r: e.tensor_copy(out=E[h_][r][64 * h_:64 * h_ + 64, 64 * r:64 * r + 64], in_=idf[64 * h_:64 * h_ + 64, 64 * h_:64 * h_ + 64]),
                     reads=["pidf", "E%d%d" % (h_, r)], writes=["E%d%d" % (h_, r)])
        Lz = [sb("Lz%d" % i, [128, 32, 64]) for i in range(2)]
        for i in range(2):
            S.op("pool", lambda e, i=i: e.memset(Lz[i][:].rearrange("p g c -> p (g c)"), 0.0), writes=["Lz%d" % i])
        S.op("pool", lambda e: e.memset(Mz2[:].rearrange("p a b c -> p (a b c)"), 0.0), writes=["Mz2"])
        t5v = t5[:].rearrange("p (a j) c -> p a j c", j=4); t6v = t6[:].rearrange("p (a j) c -> p a j c", j=4)
        with scope(C) as esp:
            PM = C.ps(esp, "PM", [128, 16, 128]); PL = C.ps(esp, "PL", [128, 8, 2, 128])
            for s in range(8):
                i = 7 - s; sl = s % 2; lk = "Lz%d" % sl
                Lzv = Lz[sl][:].rearrange("p (a j) c -> p a j c", j=4)
                S.op("dve", lambda e, i=i: e.tensor_tensor(out=t5[:], in0=BB1[:], in1=bc(CR[:, :, i:i + 1], [128, 32, 16]), op=ALU.mult), reads=["BB1", "CR"], writes=["t5"])
                S.op("dve", lambda e, i=i: e.tensor_tensor(out=t6[:], in0=BB2[:], in1=bc(CI[:, :, i:i + 1], [128, 32, 16]), op=ALU.mult), reads=["BB2", "CI"], writes=["t6"])
                for j4 in range(4):
                    S.op("dve", lambda e, j4=j4, Lzv=Lzv: e.scalar_tensor_tensor(out=Lzv[:, :, j4, 16 * j4:16 * j4 + 16], in0=t6v[:, :, j4, :], scalar=SG[:, 0:1], in1=t5v[:, :, j4, :], op0=ALU.mult, op1=ALU.add),
                         reads=["t5", "t6", "s5_sg"], writes=[lk])
                for g in range(32):
                    chc = g // 8; hb = (g % 8) // 4; j4 = g % 4; e_ = chc * 4 + j4
                    rows = slice(64 * hb, 64 * hb + 64)
                    S.op("pe", lambda e, g=g, sl=sl, e_=e_, rows=rows: e.matmul(PM[rows, e_, :], lhsT=Lz[sl][:, g, :], rhs=Rm[:, g, :, :].rearrange("p t c -> p (t c)"), start=True, stop=True),
                         reads=[lk, "Rm"], writes=["PM"])
                for pr in range(16):
                    a_ = pr // 2; wp = pr % 2; chc = a_ // 2; hb = a_ % 2; e2 = chc * 2 + wp
                    rows = slice(64 * hb, 64 * hb + 64)
                    for h_ in range(2):
                        for r in range(2):
                            g = 4 * a_ + 2 * r + wp
                            S.op("pe", lambda e, g=g, sl=sl, e2=e2, rows=rows, h_=h_, r=r: e.matmul(PL[rows, e2, h_, :], lhsT=Lz[sl][:, g, :], rhs=E[h_][r][:], start=(r == 0), stop=(r == 1)),
                                 reads=[lk, "E%d%d" % (h_, r)], writes=["PL"])
                S.op("dve", lambda e, s=s: e.tensor_copy(out=Mz2[:, :, s, 16 * s:128], in_=PM[:, :, 16 * s:128]), reads=["PM"], writes=["Mz2"])
                S.op("dve", lambda e, s=s: e.tensor_tensor(out=Mz2[:, :, s, 16 * s:16 * s + 16], in0=PM[:, :, 16 * s:16 * s + 16], in1=DD[:], op=ALU.add), reads=["PM", "s5_dd"], writes=["Mz2"])
                S.op("act", lambda e, s=s: e.copy(out=LT2[:, :, s, :, :], in_=PL[:]), reads=["PL"], writes=["LT2"])
        S.dma("sp", D["Mz_d"], Mz2[:].rearrange("p a b c -> p (a b c)"), reads=["Mz2"], writes=["Mz_d"])
        S.dma("sp", D["CA_d"][:, 0, :], CA[:].rearrange("p g c -> p (g c)"), reads=["CA"], writes=["CA_d"])
        S.dma("sp", D["CA_d"][:, 1, :], CAs[:].rearrange("p g c -> p (g c)"), reads=["CAs"], writes=["CA_d"])
    return P


def load_weight_bf16(C, es, es_tmp, name, src, rows_chunks, ncols, gcol=None, q="sp"):
    S = C.S
    W = C.sb(es, name, [128, rows_chunks, ncols], BF16)
    stg = [C.sb(es_tmp, name + "_stg%d" % i, [128, ncols]) for i in range(2)]
    srcv = src.rearrange("(c p) n -> c p n", p=128)
    for c in range(rows_chunks):
        st = stg[c % 2]; sk = name + "_stg%d" % (c % 2)
        S.dma(q, st[:], srcv[c], writes=[sk])
        eng = "dve" if c % 2 == 0 else "pool"
        if gcol is not None:
            S.op(eng, lambda e, st=st, c=c: e.tensor_scalar(out=W[:, c, :], in0=st[:], scalar1=gcol[0][:, c:c + 1], scalar2=None, op0=ALU.mult),
                 reads=[sk, gcol[1]], writes=[name])
        else:
            S.op(eng, lambda e, st=st, c=c: e.tensor_copy(out=W[:, c, :], in_=st[:]), reads=[sk], writes=[name])
    return W


def rms_rstd(C, x_ap, xkey, junk, jkey, ss, rs, skey, n):
    S = C.S
    S.op("act", lambda e: e.activation(out=junk, in_=x_ap, func=AF.Square, accum_out=ss), reads=[xkey], writes=[skey + "_ss"])
    S.op("act", lambda e: e.activation(out=rs, in_=ss, func=AF.Sqrt, scale=1.0 / n, bias=EPS), reads=[skey + "_ss"], writes=[skey + "_sq"])
    S.op("dve", lambda e: e.reciprocal(out=rs, in_=rs), reads=[skey + "_sq"], writes=[skey])


def transpose_chunks(C, src, skey, nch, pbank, pkey, dst, dkey, idb, evac="act"):
    S = C.S
    for c in range(nch):
        S.op("pe", lambda e, c=c: e.transpose(out=pbank[:, c, :], in_=src[:, c * 128:(c + 1) * 128], identity=idb[:]), reads=[skey, "identb"], writes=[pkey])
    if evac == "act":
        S.op("act", lambda e: e.copy(out=dst, in_=pbank), reads=[pkey], writes=[dkey])
    else:
        S.op(evac, lambda e: e.tensor_copy(out=dst, in_=pbank), reads=[pkey], writes=[dkey])


def stage_mixer(C, D, dbg=None, upto=9, prep=None):
    S, nc = C.S, C.nc
    dbg = dbg or {}
    with scope(C) as es1:
        P = s5_params(C, es1, D)
        idf = C.sb(es1, "identf", [128, 128]); idb = C.sb(es1, "identb", [128, 128], BF16)
        S.op("pool", lambda e: e.memset(idf[:], 1.0), writes=["identf"])
        S.op("pool", lambda e: e.affine_select(out=idf[:], in_=idf[:], pattern=[[-1, 128]], compare_op=ALU.is_equal, fill=0.0, base=0, channel_multiplier=1), reads=["identf"], writes=["identf"])
        S.op("dve", lambda e: e.tensor_copy(out=idb[:], in_=idf[:]), reads=["identf"], writes=["identb"])
        if "WPr" in dbg:
            for nm in ("WPr", "WPi"):
                S.dma("sp", dbg[nm], P[nm][:].rearrange("p a b -> p (a b)"), reads=[nm], writes=["o_" + nm])
            S.dma("pool", dbg["LT2"], P["LT2"][:].rearrange("p a b c d -> p (a b c d)"), reads=["LT2"], writes=["o_LT2"])
            S.dma("pool", dbg["Mz"], D["Mz_d"], reads=["Mz_d"], writes=["o_Mz"])
            S.dma("pool", dbg["CA"], D["CA_d"].rearrange("p a b -> p (a b)"), reads=["CA_d"], writes=["o_CA"])
        if upto < 1:
            return
        carry_r = C.sb(es1, "carry_r", [128, 16]); carry_i = C.sb(es1, "carry_i", [128, 16])
        S.op("pool", lambda e: e.memset(carry_r[:], 0.0), writes=["carry_r"])
        S.op("pool", lambda e: e.memset(carry_i[:], 0.0), writes=["carry_i"])
        TRE = C.sb(es1, "TRE", [128, 16, 257]); TIM = C.sb(es1, "TIM", [128, 16, 257])
        uT = C.sb(es1, "uT", [128, 4, 8, 256], BF16)
        with scope(C) as es2:
            _mixer_passes(C, es2, D, P, idb, carry_r, carry_i, TRE, TIM, uT, dbg)
        if "carry" in dbg:
            S.dma("sp", dbg["carry"][:, 0:16], carry_r[:], reads=["carry_r"], writes=["o_carry"])
            S.dma("sp", dbg["carry"][:, 16:32], carry_i[:], reads=["carry_i"], writes=["o_carry2"])
        if upto < 2:
            return
        with scope(C) as es3:
            ytm = C.sb(es3, "ytm", [128, 2, 8, 512], BF16)
            with scope(C) as es4:
                _s5_scan_out(C, es4, D, P, idb, TRE, TIM, uT, ytm, carry_r, carry_i, dbg)
            if upto < 3:
                return
            _s5_glu_out(C, es3, D, idb, ytm, dbg)
    if upto < 4:
        return
    with scope(C) as es5:
        idf = C.sb(es5, "identf", [128, 128]); idb = C.sb(es5, "identb", [128, 128], BF16)
        S.op("pool", lambda e: e.memset(idf[:], 1.0), reads=[], writes=["identf"])
        S.op("pool", lambda e: e.affine_select(out=idf[:], in_=idf[:], pattern=[[-1, 128]], compare_op=ALU.is_equal, fill=0.0, base=0, channel_multiplier=1), reads=["identf"], writes=["identf"])
        S.op("dve", lambda e: e.tensor_copy(out=idb[:], in_=idf[:]), reads=["identf"], writes=["identb"])
        _attention(C, es5, D, idb, dbg, prep)


def _mixer_passes(C, es, D, P, idb, carry_r, carry_i, TRE, TIM, uT, dbg):
    S = C.S
    sb = lambda name, shape, dt=F32: C.sb(es, name, shape, dt)
    gin = sb("gin", [128, 8])
    S.dma("sp", gin[:], D["g_mix"], writes=["gin"])
    with scope(C) as est:
        Wb = load_weight_bf16(C, es, est, "Wb", D["w_in"], 8, 2048, gcol=(gin, "gin"))
    gq = sb("gq", [128, 64]); gk = sb("gk", [128, 64]); hv = sb("hv", [128, 4])
    S.dma("sp", gq[:], D["att_q_g"].partition_broadcast(128), writes=["gq"])
    S.dma("sp", gk[:], D["att_k_g"].partition_broadcast(128), writes=["gk"])
    S.dma("sp", hv[:], D["hvalid"], writes=["hv"])
    S.op("dve", lambda e: e.tensor_scalar(out=gq[:], in0=gq[:], scalar1=0.125, scalar2=None, op0=ALU.mult), reads=["gq"], writes=["gq"])
    xt = [sb("xt%d" % i, [128, 1024]) for i in range(2)]
    junk = sb("junk", [128, 1024]); st = [sb("st%d" % i, [128, 4]) for i in range(2)]
    xn = [sb("xn%d" % i, [128, 1024], BF16) for i in range(2)]
    xnT = [sb("xnT%d" % i, [128, 8, 512], BF16) for i in range(2)]
    qkv = sb("qkv", [128, 3, 512]); sq = sb("sq2", [128, 512]); qst = sb("qst", [128, 4, 8])
    qn = sb("qn", [128, 2, 512], BF16)
    kTs = [sb("kTs%d" % i, [128, 4, 128], BF16) for i in range(2)]; qTs = [sb("qTs%d" % i, [128, 4, 128], BF16) for i in range(2)]
    Vs = [sb("Vs%d" % i, [128, 8, 65], BF16) for i in range(2)]
    tBr = sb("tBr", [128, 8, 128]); tBi = sb("tBi", [128, 8, 128]); tCr = sb("tCr", [128, 8, 64]); tCi = sb("tCi", [128, 8, 64])
    tt1 = sb("tt1", [128, 8, 128]); tt2 = sb("tt2", [128, 8, 128]); tt3 = sb("tt3", [128, 8, 128]); tt4 = sb("tt4", [128, 8, 128])
    REDr = sb("REDr", [128, 16]); REDi = sb("REDi", [128, 16]); c1 = sb("c1", [128, 16]); c2 = sb("c2", [128, 16]); c3 = sb("c3", [128, 16])
    with scope(C) as esp:
        bank = [C.ps(esp, "bk%d" % i, [128, 512]) for i in range(8)]
        xv = D["x_ext"].rearrange("(n p) d -> n p d", p=128)
        tile_ctr = 0
        for q in range(4):
            for blk in range(4):
                bslot = (q * 4 + blk) % 2
                xT = xnT[bslot]; xTk = "xnT%d" % bslot
                for tt in range(4):
                    n_tile = q * 16 + blk * 4 + tt
                    sl = tile_ctr % 2; tile_ctr += 1
                    x_ = xt[sl]; xk = "xt%d" % sl
                    S.dma("sp", x_[:], xv[n_tile], writes=[xk])
                    rms_rstd(C, x_[:], xk, junk[:], "junk", st[sl][:, 0:1], st[sl][:, 1:2], "st%d" % sl, 1024)
                    S.op("dve", lambda e, x_=x_, sl=sl: e.tensor_scalar(out=xn[sl][:], in0=x_[:], scalar1=st[sl][:, 1:2], scalar2=None, op0=ALU.mult),
                         reads=[xk, "st%d" % sl], writes=["xn%d" % sl])
                    tb_ = 0 if sl == 0 else 7
                    pb = bank[tb_][:].bitcast(BF16).rearrange("p (c n) -> p c n", n=128)
                    transpose_chunks(C, xn[sl][:], "xn%d" % sl, 8, pb, "bk%d" % tb_, xT[:, :, tt * 128:(tt + 1) * 128], xTk, idb, evac=("act" if sl == 0 else "dve"))
                for chc in range(4):
                    bi = 1 + (chc % 2); bkk = "bk%d" % bi
                    for dc in range(8):
                        S.op("pe", lambda e, chc=chc, dc=dc, bi=bi, xT=xT: e.matmul(bank[bi][:], lhsT=Wb[:, dc, 1536 + chc * 128:1536 + (chc + 1) * 128], rhs=xT[:, dc, :], start=(dc == 0), stop=(dc == 7)),
                             reads=["Wb", xTk], writes=[bkk])
                    eng = "act" if chc % 2 == 0 else "dve"
                    src = bank[bi][:].rearrange("p (k s) -> p s k", s=8)
                    dst = uT[:, chc, :, blk * 64:(blk + 1) * 64]
                    if eng == "act":
                        S.op("act", lambda e, src=src, dst=dst: e.copy(out=dst, in_=src), reads=[bkk], writes=["uT"])
                    else:
                        S.op("dve", lambda e, src=src, dst=dst: e.tensor_copy(out=dst, in_=src), reads=[bkk], writes=["uT"])
                need_kv = (q == 3) or (q == 2 and blk == 3)
                need_q = (q == 3)
                if need_kv:
                    for tt in range(4):
                        n_tile = q * 16 + blk * 4 + tt
                        kvt = n_tile - 44
                        sl = kvt % 2
                        projs = [(1, 512, 3), (2, 1024, 4)] + ([(0, 0, 5)] if need_q else [])
                        for (pi, c0, bi) in projs:
                            for dc in range(8):
                                S.op("pe", lambda e, dc=dc, bi=bi, c0=c0, tt=tt, xT=xT: e.matmul(bank[bi][:], lhsT=xT[:, dc, tt * 128:(tt + 1) * 128], rhs=Wb[:, dc, c0:c0 + 512], start=(dc == 0), stop=(dc == 7)),
                                     reads=["Wb", xTk], writes=["bk%d" % bi])
                        S.op("act", lambda e, sl=sl: e.copy(out=Vs[sl][:, :, 0:64], in_=bank[4][:].rearrange("p (h d) -> p h d", d=64)), reads=["bk4"], writes=["Vs%d" % sl])
                        if kvt < 4:
                            S.op("pool", lambda e, sl=sl, kvt=kvt: e.tensor_copy(out=Vs[sl][:, :, 64], in_=bc(hv[:, kvt:kvt + 1], [128, 8])), reads=["hv"], writes=["Vs%d" % sl])
                        else:
                            S.op("pool", lambda e, sl=sl: e.memset(Vs[sl][:, :, 64], 1.0), reads=[], writes=["Vs%d" % sl])
                        S.dma("sp", D["V_d"][kvt], Vs[sl][:].rearrange("p h d -> p (h d)"), reads=["Vs%d" % sl], writes=["V_d"])
                        for (pi, bi, gt, gkey, dstT, dkey, dram, ncol_t) in ([(1, 3, gk, "gk", kTs[sl], "kTs%d" % sl, D["kT_d"], kvt)] +
                                                                          ([(0, 5, gq, "gq", qTs[sl], "qTs%d" % sl, D["qT_d"], kvt - 4)] if need_q else [])):
                            qs = qkv[:, pi, :]; qk_ = "qkv%d" % pi
                            S.op("act", lambda e, qs=qs, bi=bi: e.copy(out=qs, in_=bank[bi][:]), reads=["bk%d" % bi], writes=[qk_])
                            S.op("pool", lambda e, qs=qs: e.tensor_tensor(out=sq[:], in0=qs, in1=qs, op=ALU.mult), reads=[qk_], writes=["sq2"])
                            S.op("dve", lambda e, pi=pi: e.tensor_reduce(out=qst[:, pi, :], in_=sq[:].rearrange("p (h d) -> p h d", d=64), axis=AX.X, op=ALU.add), reads=["sq2"], writes=["qst%d" % pi])
                            S.op("act", lambda e, pi=pi: e.activation(out=qst[:, 2 + pi, :], in_=qst[:, pi, :], func=AF.Sqrt, scale=1.0 / 64, bias=EPS), reads=["qst%d" % pi], writes=["qsq%d" % pi])
                            S.op("dve", lambda e, pi=pi: e.reciprocal(out=qst[:, 2 + pi, :], in_=qst[:, 2 + pi, :]), reads=["qsq%d" % pi], writes=["qrs%d" % pi])
                            S.op("dve", lambda e, qs=qs, pi=pi: e.tensor_tensor(out=qs.rearrange("p (h d) -> p h d", d=64), in0=qs.rearrange("p (h d) -> p h d", d=64),
                                                                        in1=bc(qst[:, 2 + pi, :].unsqueeze(2), [128, 8, 64]), op=ALU.mult), reads=[qk_, "qrs%d" % pi], writes=[qk_])
                            S.op("pool", lambda e, qs=qs, pi=pi, gt=gt: e.tensor_tensor(out=qn[:, pi, :].rearrange("p (h d) -> p h d", d=64), in0=qs.rearrange("p (h d) -> p h d", d=64),
                                                                               in1=bc(gt[:].unsqueeze(1), [128, 8, 64]), op=ALU.mult), reads=[qk_, gkey], writes=["qn%d" % pi])
                            pb = bank[6][:].bitcast(BF16).rearrange("p (c n) -> p c n", n=128)[:, 0:4, :]
                            transpose_chunks(C, qn[:, pi, :], "qn%d" % pi, 4, pb, "bk6", dstT[:], dkey, idb)
                            S.dma("sp", dram.rearrange("p (c n) -> p c n", c=4)[:, :, ncol_t * 128:(ncol_t + 1) * 128], dstT[:], reads=[dkey], writes=["qkT_d"])
            for pair in range(16):
                psl = pair % 2
                br = bank[1 + 2 * psl]; bim = bank[2 + 2 * psl]; brk = "bk%d" % (1 + 2 * psl); bik = "bk%d" % (2 + 2 * psl)
                a_ = pair // 2; wp = pair % 2; chc = a_ // 2; hb = a_ % 2; e2 = chc * 2 + wp
                rows = slice(64 * hb, 64 * hb + 64)
                for half, (bkt, bkk) in enumerate(((br, brk), (bim, bik))):
                    for s in range(8):
                        S.op("pe", lambda e, e2=e2, s=s, half=half, rows=rows, chc=chc, bkt=bkt: e.matmul(bkt[:, 0:256], lhsT=P["LT2"][rows, e2, s, half, :], rhs=uT[rows, chc, s, :], start=(s == 0), stop=(s == 7)),
                             reads=["LT2", "uT"], writes=[bkk])
                S.op("act", lambda e, pair=pair, br=br: e.copy(out=TRE[:, pair, 1:257], in_=br[:, 0:256]), reads=[brk], writes=["TRE%d" % pair])
                S.op("act", lambda e, pair=pair, bim=bim: e.copy(out=TIM[:, pair, 1:257], in_=bim[:, 0:256]), reads=[bik], writes=["TIM%d" % pair])
            if q < 3:
                for p0 in (0, 8):
                    kre = ["TRE%d" % p for p in range(p0, p0 + 8)]; kim = ["TIM%d" % p for p in range(p0, p0 + 8)]
                    src_r, src_i, srk, sik = TRE[:, p0:p0 + 8, 1:257], TIM[:, p0:p0 + 8, 1:257], kre, kim
                    bufs = [(tBr[:], tBi[:], ["tBr"], ["tBi"]), (tCr[:], tCi[:], ["tCr"], ["tCi"])]
                    for j in range(8):
                        n = 256 >> j; h = n // 2
                        wrb = bc(P["WPr"][:, j, p0:p0 + 8].unsqueeze(2), [128, 8, h]); wib = bc(P["WPi"][:, j, p0:p0 + 8].unsqueeze(2), [128, 8, h])
                        sre, sro = src_r[:, :, 0:n:2], src_r[:, :, 1:n:2]; sie, sio = src_i[:, :, 0:n:2], src_i[:, :, 1:n:2]
                        if j == 7:
                            dr, di, drk, dik = REDr[:, p0:p0 + 8].unsqueeze(2), REDi[:, p0:p0 + 8].unsqueeze(2), ["REDr%d" % p0], ["REDi%d" % p0]
                        else:
                            bb = bufs[j % 2]
                            dr, di, drk, dik = bb[0][:, :, 0:h], bb[1][:, :, 0:h], bb[2], bb[3]
                        t1v, t2v, t3v, t4v = tt1[:, :, 0:h], tt2[:, :, 0:h], tt3[:, :, 0:h], tt4[:, :, 0:h]
                        S.op("dve", lambda e, t1v=t1v, sre=sre, wrb=wrb: e.tensor_tensor(out=t1v, in0=sre, in1=wrb, op=ALU.mult), reads=srk + ["WPr"], writes=["tt1"])
                        S.op("pool", lambda e, t2v=t2v, sie=sie, wib=wib: e.tensor_tensor(out=t2v, in0=sie, in1=wib, op=ALU.mult), reads=sik + ["WPi"], writes=["tt2"])
                        S.op("pool", lambda e, t3v=t3v, sie=sie, wrb=wrb: e.tensor_tensor(out=t3v, in0=sie, in1=wrb, op=ALU.mult), reads=sik + ["WPr"], writes=["tt3"])
                        S.op("dve", lambda e, t4v=t4v, sre=sre, wib=wib: e.tensor_tensor(out=t4v, in0=sre, in1=wib, op=ALU.mult), reads=srk + ["WPi"], writes=["tt4"])
                        S.op("dve", lambda e, t1v=t1v, t2v=t2v: e.tensor_tensor(out=t1v, in0=t1v, in1=t2v, op=ALU.subtract), reads=["tt1", "tt2"], writes=["tt1"])
                        S.op("pool", lambda e, t3v=t3v, t4v=t4v: e.tensor_tensor(out=t3v, in0=t3v, in1=t4v, op=ALU.add), reads=["tt3", "tt4"], writes=["tt3"])
                        S.op("dve", lambda e, dr=dr, t1v=t1v, sro=sro: e.tensor_tensor(out=dr, in0=t1v, in1=sro, op=ALU.add), reads=["tt1"] + srk, writes=drk)
                        S.op("pool", lambda e, di=di, t3v=t3v, sio=sio: e.tensor_tensor(out=di, in0=t3v, in1=sio, op=ALU.add), reads=["tt3"] + sik, writes=dik)
                        if j < 7:
                            src_r, src_i, srk, sik = bb[0], bb[1], bb[2], bb[3]
            if q < 3:
                w8r = P["WPr"][:, 8, :]; w8i = P["WPi"][:, 8, :]
                S.op("dve", lambda e: e.tensor_tensor(out=c1[:], in0=w8r, in1=carry_r[:], op=ALU.mult), reads=["WPr", "carry_r"], writes=["c1"])
                S.op("dve", lambda e: e.tensor_tensor(out=c2[:], in0=w8i, in1=carry_i[:], op=ALU.mult), reads=["WPi", "carry_i"], writes=["c2"])
                S.op("dve", lambda e: e.tensor_tensor(out=c1[:], in0=c1[:], in1=c2[:], op=ALU.subtract), reads=["c1", "c2"], writes=["c1"])
                S.op("dve", lambda e: e.tensor_tensor(out=c1[:], in0=c1[:], in1=REDr[:], op=ALU.add), reads=["c1", "REDr0", "REDr8"], writes=["c1"])
                S.op("dve", lambda e: e.tensor_tensor(out=c2[:], in0=w8r, in1=carry_i[:], op=ALU.mult), reads=["WPr", "carry_i", "c1"], writes=["c2"])
                S.op("dve", lambda e: e.tensor_tensor(out=c3[:], in0=w8i, in1=carry_r[:], op=ALU.mult), reads=["WPi", "carry_r"], writes=["c3"])
                S.op("dve", lambda e: e.tensor_tensor(out=c2[:], in0=c2[:], in1=c3[:], op=ALU.add), reads=["c2", "c3"], writes=["c2"])
                S.op("dve", lambda e: e.tensor_tensor(out=carry_i[:], in0=c2[:], in1=REDi[:], op=ALU.add), reads=["c2", "REDi0", "REDi8"], writes=["carry_i"])
                S.op("dve", lambda e: e.tensor_copy(out=carry_r[:], in_=c1[:]), reads=["c1"], writes=["carry_r"])


def _s5_scan_out(C, es, D, P, idb, TRE, TIM, uT, ytm, carry_r, carry_i, dbg):
    S = C.S
    sb = lambda name, shape, dt=F32: C.sb(es, name, shape, dt)
    Mz2 = sb("Mz2s", [128, 16, 8, 128], BF16); CAA = sb("CAA", [128, 2, 32, 128], BF16)
    S.dma("sp", Mz2[:].rearrange("p a b c -> p (a b c)"), D["Mz_d"], reads=["Mz_d"], writes=["Mz2s"])
    S.dma("sp", CAA[:].rearrange("p a g c -> p a (g c)"), D["CA_d"], reads=["CA_d"], writes=["CAA"])
    hs1 = sb("hs1", [128, 8, 256]); hs2 = sb("hs2", [128, 8, 256]); hs3 = sb("hs3", [128, 8, 256]); hs4 = sb("hs4", [128, 8, 256])
    Tbr = [sb("Tbr%d" % i, [128, 256], BF16) for i in range(2)]; Tbi = [sb("Tbi%d" % i, [128, 256], BF16) for i in range(2)]
    Yg = [sb("Yg%d" % i, [128, 256], BF16) for i in range(2)]
    ysum = [sb("ysum%d" % i, [128, 256]) for i in range(2)]
    import os
    CUT = int(os.environ.get("SCAN_CUT", "9"))
    for p0 in (0, 8):
        kre = ["TRE%d" % p for p in range(p0, p0 + 8)]; kim = ["TIM%d" % p for p in range(p0, p0 + 8)]
        S.op("dve", lambda e, p0=p0: e.tensor_copy(out=TRE[:, p0:p0 + 8, 0:1], in_=carry_r[:, p0:p0 + 8].unsqueeze(2)), reads=["carry_r"], writes=kre)
        S.op("pool", lambda e, p0=p0: e.tensor_copy(out=TIM[:, p0:p0 + 8, 0:1], in_=carry_i[:, p0:p0 + 8].unsqueeze(2)), reads=["carry_i"], writes=kim)
        for j in range(9):
            d = 1 << j; m = 257 - d
            wrb = bc(P["WPr"][:, j, p0:p0 + 8].unsqueeze(2), [128, 8, m]); wib = bc(P["WPi"][:, j, p0:p0 + 8].unsqueeze(2), [128, 8, m])
            R0 = TRE[:, p0:p0 + 8, 0:m]; I0 = TIM[:, p0:p0 + 8, 0:m]; R1 = TRE[:, p0:p0 + 8, d:257]; I1 = TIM[:, p0:p0 + 8, d:257]
            h1, h2, h3, h4 = hs1[:, :, 0:m], hs2[:, :, 0:m], hs3[:, :, 0:m], hs4[:, :, 0:m]
            S.op("dve", lambda e, h1=h1, R0=R0, wrb=wrb: e.tensor_tensor(out=h1, in0=R0, in1=wrb, op=ALU.mult), reads=kre + ["WPr"], writes=["hs1"])
            S.op("pool", lambda e, h2=h2, I0=I0, wib=wib: e.tensor_tensor(out=h2, in0=I0, in1=wib, op=ALU.mult), reads=kim + ["WPi"], writes=["hs2"])
            S.op("pool", lambda e, h3=h3, I0=I0, wrb=wrb: e.tensor_tensor(out=h3, in0=I0, in1=wrb, op=ALU.mult), reads=kim + ["WPr"], writes=["hs3"])
            S.op("dve", lambda e, h4=h4, R0=R0, wib=wib: e.tensor_tensor(out=h4, in0=R0, in1=wib, op=ALU.mult), reads=kre + ["WPi"], writes=["hs4"])
            S.op("dve", lambda e, h1=h1, h2=h2: e.tensor_tensor(out=h1, in0=h1, in1=h2, op=ALU.subtract), reads=["hs1", "hs2"], writes=["hs1"])
            S.op("pool", lambda e, h3=h3, h4=h4: e.tensor_tensor(out=h3, in0=h3, in1=h4, op=ALU.add), reads=["hs3", "hs4"], writes=["hs3"])
            S.op("dve", lambda e, R1=R1, h1=h1: e.tensor_tensor(out=R1, in0=R1, in1=h1, op=ALU.add), reads=kre + ["hs1"], writes=kre)
            S.op("pool", lambda e, I1=I1, h3=h3: e.tensor_tensor(out=I1, in0=I1, in1=h3, op=ALU.add), reads=kim + ["hs3"], writes=kim)
    with scope(C) as esp:
        bank = [C.ps(esp, "sbk%d" % i, [128, 512]) for i in range(6)]
        for pair in range(16):
            sl = pair % 2
            kr, ki = "TRE%d" % pair, "TIM%d" % pair
            a_ = pair // 2; wp = pair % 2; chc = a_ // 2; hb = a_ % 2
            rows = slice(64 * hb, 64 * hb + 64)
            kr, ki = "TRE%d" % pair, "TIM%d" % pair
            if CUT < 2:
                continue
            S.op("act", lambda e, pair=pair, sl=sl: e.copy(out=Tbr[sl][:], in_=TRE[:, pair, 0:256]), reads=[kr], writes=["Tbr%d" % sl])
            S.op("act", lambda e, pair=pair, sl=sl: e.copy(out=Tbi[sl][:], in_=TIM[:, pair, 0:256]), reads=[ki], writes=["Tbi%d" % sl])
            for r in range(2):
                g = 4 * a_ + 2 * r + wp
                e_ = chc * 4 + (g % 4)
                pr = slice(64 * r, 64 * r + 64)
                yb = bank[r]; ybk = "sbk%d" % r
                for s in range(8):
                    S.op("pe", lambda e, e_=e_, s=s, rows=rows, chc=chc, yb=yb: e.matmul(yb[:, 0:256], lhsT=Mz2[rows, e_, s, :], rhs=uT[rows, chc, s, :], start=(s == 0), stop=(s == 7)),
                         reads=["Mz2s", "uT"], writes=[ybk])
                zb_ = bank[4 + r]; zbk = "sbk%d" % (4 + r)
                S.op("pe", lambda e, g=g, pr=pr, zb_=zb_, r=r, sl=sl: e.matmul(zb_[:, 0:256], lhsT=CAA[pr, r, g, :], rhs=Tbr[sl][pr, :], start=True, stop=False), reads=["CAA", "Tbr%d" % sl], writes=[zbk])
                S.op("pe", lambda e, g=g, pr=pr, zb_=zb_, r=r, sl=sl: e.matmul(zb_[:, 0:256], lhsT=CAA[pr, 1 - r, g, :], rhs=Tbi[sl][pr, :], start=False, stop=True), reads=["CAA", "Tbi%d" % sl], writes=[zbk])
                if CUT < 3:
                    continue
                S.op("act", lambda e, r=r, zb_=zb_: e.copy(out=ysum[r][:], in_=zb_[:, 0:256]), reads=[zbk], writes=["ysum%d" % r])
                S.op("dve", lambda e, r=r, yb=yb: e.tensor_tensor(out=ysum[r][:], in0=yb[:, 0:256], in1=ysum[r][:], op=ALU.add), reads=[ybk, "ysum%d" % r], writes=["ysum%d" % r])
                S.op("act", lambda e, r=r: e.activation(out=Yg[r][:], in_=ysum[r][:], func=AF.Gelu), reads=["ysum%d" % r], writes=["Yg%d" % r])
                if CUT < 4:
                    continue
                pT = bank[2 + r][:].bitcast(BF16).rearrange("p (c n) -> p c n", n=128)
                for kb in range(2):
                    S.op("pe", lambda e, r=r, kb=kb, pT=pT: e.transpose(out=pT[:, kb, :], in_=Yg[r][:, kb * 128:(kb + 1) * 128], identity=idb[:]), reads=["Yg%d" % r, "identb"], writes=["sbk%d" % (2 + r)])
                for kb in range(2):
                    S.op("dve", lambda e, g=g, kb=kb, pT=pT: e.tensor_copy(out=ytm[:, kb, :, 16 * g:16 * g + 16], in_=pT[:, kb, :].rearrange("p (t c) -> p t c", c=16)),
                         reads=["sbk%d" % (2 + r)], writes=["ytm"])


def _s5_glu_out(C, es, D, idb, ytm, dbg):
    S = C.S
    sb = lambda name, shape, dt=F32: C.sb(es, name, shape, dt)
    gso = sb("gso", [128, 4]); bgl = sb("bgl", [128, 512])
    S.dma("sp", gso[:], D["g_ssm_out"], writes=["gso"])
    S.dma("sp", bgl[:], D["b_glu"].partition_broadcast(128), writes=["bgl"])
    with scope(C) as est:
        Wg = load_weight_bf16(C, es, est, "Wg", D["w_glu"], 4, 512)
    with scope(C) as est:
        Wo = load_weight_bf16(C, es, est, "Wos", D["w_out"][512:1024, :], 4, 1024, gcol=(gso, "gso"))
    yT = sb("yT", [128, 4, 128], BF16); zb = sb("zb", [128, 512]); ssm = sb("ssm", [128, 512]); junk = sb("junk3", [128, 512])
    st = sb("st3", [128, 2]); sn = sb("sn", [128, 512], BF16); snT = sb("snT", [128, 4, 128], BF16)
    ho = [sb("ho%d" % i, [128, 1024]) for i in range(2)]
    hsv = D["hs_d"].rearrange("(k t) d -> t k d", t=8)
    with scope(C) as esp:
        bank = [C.ps(esp, "gbk%d" % i, [128, 512]) for i in range(5)]
        it = 0
        for kb in range(2):
            for t in range(8):
                sl = it % 2; it += 1
                y = ytm[:, kb, t, :]
                pT = bank[0][:].bitcast(BF16).rearrange("p (c n) -> p c n", n=128)[:, 0:4, :]
                transpose_chunks(C, y, "ytm", 4, pT, "gbk0", yT[:], "yT", idb)
                for c in range(4):
                    S.op("pe", lambda e, c=c: e.matmul(bank[1][:], lhsT=yT[:, c, :], rhs=Wg[:, c, :], start=(c == 0), stop=(c == 3)), reads=["yT", "Wg"], writes=["gbk1"])
                S.op("dve", lambda e: e.tensor_tensor(out=zb[:], in0=bank[1][:], in1=bgl[:], op=ALU.add), reads=["gbk1", "bgl"], writes=["zb"])
                S.op("act", lambda e: e.activation(out=zb[:], in_=zb[:], func=AF.Sigmoid), reads=["zb"], writes=["zb"])
                S.op("pool", lambda e, y=y: e.tensor_tensor(out=ssm[:], in0=y, in1=zb[:], op=ALU.mult), reads=["ytm", "zb"], writes=["ssm"])
                if "ssm" in dbg:
                    S.dma("sp", dbg["ssm"].rearrange("(k t) d -> t k d", t=8)[t, kb * 128:(kb + 1) * 128, :], ssm[:], reads=["ssm"], writes=["o_ssm"])
                rms_rstd(C, ssm[:], "ssm", junk[:], "junk3", st[:, 0:1], st[:, 1:2], "st3", 512)
                S.op("dve", lambda e: e.tensor_scalar(out=sn[:], in0=ssm[:], scalar1=st[:, 1:2], scalar2=None, op0=ALU.mult), reads=["ssm", "st3"], writes=["sn"])
                pT2 = bank[2][:].bitcast(BF16).rearrange("p (c n) -> p c n", n=128)[:, 0:4, :]
                transpose_chunks(C, sn[:], "sn", 4, pT2, "gbk2", snT[:], "snT", idb)
                for cb in range(2):
                    for c in range(4):
                        S.op("pe", lambda e, c=c, cb=cb: e.matmul(bank[3 + cb][:], lhsT=snT[:, c, :], rhs=Wo[:, c, cb * 512:(cb + 1) * 512], start=(c == 0), stop=(c == 3)), reads=["snT", "Wos"], writes=["gbk%d" % (3 + cb)])
                    S.op("act", lambda e, cb=cb, sl=sl: e.copy(out=ho[sl][:, cb * 512:(cb + 1) * 512], in_=bank[3 + cb][:]), reads=["gbk%d" % (3 + cb)], writes=["ho%d" % sl])
                S.dma("sp", hsv[t, kb * 128:(kb + 1) * 128, :], ho[sl][:], reads=["ho%d" % sl], writes=["hs_d"])


def _attention(C, es, D, idb, dbg, prep=None):
    S = C.S
    sb = lambda name, shape, dt=F32: C.sb(es, name, shape, dt)
    kT = sb("kT", [128, 4, 2560], BF16); qT = sb("qT", [128, 4, 2048], BF16); V = sb("Vall", [128, 20, 520], BF16)
    qZ = sb("qZ", [128, 8, 2048], BF16)
    S.dma("sp", kT[:].rearrange("p c n -> p (c n)"), D["kT_d"], reads=["qkT_d"], writes=["kT"])
    S.dma("sp", qT[:].rearrange("p c n -> p (c n)"), D["qT_d"], reads=["qkT_d"], writes=["qT"])
    S.dma("sp", V[:], D["V_d"].rearrange("t p n -> p t n"), reads=["V_d"], writes=["Vall"])
    S.op("pool", lambda e: e.memset(qZ[:].rearrange("p h n -> p (h n)"), 0.0), writes=["qZ"])
    for h in range(8):
        rws = slice(64 * (h % 2), 64 * (h % 2) + 64)
        S.op("dve" if h % 2 else "act", (lambda e, h=h, rws=rws: e.tensor_copy(out=qZ[rws, h, :], in_=qT[rws, h // 2, :])) if h % 2 else (lambda e, h=h, rws=rws: e.copy(out=qZ[rws, h, :], in_=qT[rws, h // 2, :])),
             reads=["qT", "qZ"], writes=["qZ"])
    BT = sb("BT", [128, 8, 5, 128], BF16)
    gao = sb("gao", [128, 4])
    S.dma("sp", gao[:], D["g_att_out"], writes=["gao"])
    with scope(C) as est:
        stg = C.sb(est, "btstg", [128, 640])
        for h in range(8):
            S.dma("sp", stg[:], D["bias_t"][:, h * 640:(h + 1) * 640], writes=["btstg"])
            S.op("dve", lambda e, h=h: e.tensor_copy(out=BT[:, h, :, :].rearrange("p j q -> p (j q)"), in_=stg[:]), reads=["btstg"], writes=["BT"])
    with scope(C) as est:
        Wo = load_weight_bf16(C, es, est, "Woa", D["w_out"][0:512, :], 4, 1024, gcol=(gao, "gao"))
    PT = [sb("PT%d" % i, [128, 5, 128], BF16) for i in range(2)]
    rd = sb("rd", [128, 8]); att = sb("att", [128, 8, 64]); junk = sb("junk4", [128, 512]); st = sb("st4", [128, 2])
    an = sb("an", [128, 512], BF16); anT = sb("anT", [128, 4, 128], BF16)
    xo = [sb("xo%d" % i, [128, 1024]) for i in range(2)]; hsl = [sb("hsl%d" % i, [128, 1024]) for i in range(2)]
    h1t = [sb("h1t%d" % i, [128, 1024]) for i in range(2)]
    xv = D["x_ext"].rearrange("(n p) d -> n p d", p=128)
    hsv = D["hs_d"].rearrange("(n p) d -> n p d", p=128)
    h1v = D["h1_d"].rearrange("(n p) d -> n p d", p=128)
    with scope(C) as esp:
        bank = [C.ps(esp, "abk%d" % i, [128, 512]) for i in range(7)]
        for qt in range(16):
            sl = qt % 2
            drip(prep, 2)
            S.dma("sp", xo[sl][:], xv[48 + qt], writes=["xo%d" % sl])
            S.dma("sp", hsl[sl][:], hsv[qt], reads=["hs_d"], writes=["hsl%d" % sl])
            for h in range(8):
                hp = h // 2; rows = slice(64 * (h % 2), 64 * (h % 2) + 64); ps_ = h % 2
                bA = bank[2 * ps_]; bB = bank[2 * ps_ + 1]; bAk = "abk%d" % (2 * ps_); bBk = "abk%d" % (2 * ps_ + 1)
                for j in range(5):
                    o = bA[:, j * 128:(j + 1) * 128] if j < 4 else bB[:, 0:128]
                    ok = bAk if j < 4 else bBk
                    S.op("pe", lambda e, o=o, h=h, hp=hp, j=j, qt=qt: e.matmul(o, lhsT=kT[:, hp, (qt + j) * 128:(qt + j + 1) * 128], rhs=qZ[:, h, qt * 128:(qt + 1) * 128], start=True, stop=False),
                         reads=["kT", "qZ"], writes=[ok])
                    S.op("pe", lambda e, o=o, h=h, j=j: e.matmul(o, lhsT=idb[:], rhs=BT[:, h, j, :], start=False, stop=True), reads=["identb", "BT"], writes=[ok])
                S.op("act", lambda e, ps_=ps_, bA=bA: e.activation(out=PT[ps_][:, 0:4, :].rearrange("p j q -> p (j q)"), in_=bA[:], func=AF.Exp), reads=[bAk], writes=["PT%d" % ps_])
                S.op("act", lambda e, ps_=ps_, bB=bB: e.activation(out=PT[ps_][:, 4, :], in_=bB[:, 0:128], func=AF.Exp), reads=[bBk], writes=["PT%d" % ps_])
                ob = bank[4 + h // 4]; obk = "abk%d" % (4 + h // 4)
                for j in range(5):
                    S.op("pe", lambda e, ob=ob, h=h, j=j, ps_=ps_, qt=qt: e.matmul(ob[:, (h % 4) * 65:(h % 4) * 65 + 65], lhsT=PT[ps_][:, j, :], rhs=V[:, qt + j, h * 65:(h + 1) * 65], start=(j == 0), stop=(j == 4)),
                         reads=["PT%d" % ps_, "Vall"], writes=[obk])
            for hb in range(2):
                ov = bank[4 + hb][:, 0:260].rearrange("p (h d) -> p h d", d=65)
                S.op("dve", lambda e, hb=hb, ov=ov: e.reciprocal(out=rd[:, hb * 4:(hb + 1) * 4], in_=ov[:, :, 64]), reads=["abk%d" % (4 + hb)], writes=["rd%d" % hb])
                S.op("dve", lambda e, hb=hb, ov=ov: e.tensor_tensor(out=att[:, hb * 4:(hb + 1) * 4, :], in0=ov[:, :, 0:64], in1=bc(rd[:, hb * 4:(hb + 1) * 4].unsqueeze(2), [128, 4, 64]), op=ALU.mult),
                     reads=["abk%d" % (4 + hb), "rd%d" % hb], writes=["att%d" % hb])
            attf = att[:].rearrange("p h d -> p (h d)")
            if "att" in dbg:
                S.dma("sp", dbg["att"].rearrange("(n p) d -> n p d", p=128)[qt], attf, reads=["att0", "att1"], writes=["o_att"])
            S.op("act", lambda e: e.activation(out=junk[:], in_=attf, func=AF.Square, accum_out=st[:, 0:1]), reads=["att0", "att1"], writes=["st4_ss"])
            S.op("act", lambda e: e.activation(out=st[:, 1:2], in_=st[:, 0:1], func=AF.Sqrt, scale=1.0 / 512, bias=EPS), reads=["st4_ss"], writes=["st4_sq"])
            S.op("dve", lambda e: e.reciprocal(out=st[:, 1:2], in_=st[:, 1:2]), reads=["st4_sq"], writes=["st4"])
            S.op("dve", lambda e: e.tensor_scalar(out=an[:], in0=attf, scalar1=st[:, 1:2], scalar2=None, op0=ALU.mult), reads=["att0", "att1", "st4"], writes=["an"])
            pT = bank[6][:].bitcast(BF16).rearrange("p (c n) -> p c n", n=128)[:, 0:4, :]
            transpose_chunks(C, an[:], "an", 4, pT, "abk6", anT[:], "anT", idb)
            for cb in range(2):
                for c in range(4):
                    S.op("pe", lambda e, c=c, cb=cb: e.matmul(bank[cb][:], lhsT=anT[:, c, :], rhs=Wo[:, c, cb * 512:(cb + 1) * 512], start=(c == 0), stop=(c == 3)), reads=["anT", "Woa"], writes=["abk%d" % cb])
                S.op("dve", lambda e, cb=cb, sl=sl: e.tensor_tensor(out=h1t[sl][:, cb * 512:(cb + 1) * 512], in0=bank[cb][:], in1=xo[sl][:, cb * 512:(cb + 1) * 512], op=ALU.add),
                     reads=["abk%d" % cb, "xo%d" % sl], writes=["h1t%d" % sl])
            S.op("pool", lambda e, sl=sl: e.tensor_tensor(out=h1t[sl][:], in0=h1t[sl][:], in1=hsl[sl][:], op=ALU.add), reads=["h1t%d" % sl, "hsl%d" % sl], writes=["h1t%d" % sl])
            S.dma("sp", h1v[qt], h1t[sl][:], reads=["h1t%d" % sl], writes=["h1_d"])


def _col(g, n):
    return np.ascontiguousarray(np.asarray(g, np.float32).reshape(n, 128).T)


def host_shared(inp):
    f = lambda k: np.asarray(inp[k], np.float32)[0]
    sh = {}
    sh["g_mix"] = _col(f("norm_mix_g"), 8)
    sh["w_in"] = np.ascontiguousarray(f("w_in"))
    sh["att_q_g"] = f("att_q_g").reshape(1, 64)
    sh["att_k_g"] = f("att_k_g").reshape(1, 64)
    rb = f("rel_bias")
    p = np.arange(128); j = np.arange(5); q = np.arange(128)
    kidx = j[:, None] * 128 + p[None, :]
    kc = kidx // 64; ki = kidx % 64
    qc = q // 64; qi = q % 64
    jb = kc[:, :, None] - qc[None, None, :]
    allowed = (jb >= 0) & (jb <= 8)
    kj = jb * 64 + ki[:, :, None]
    dist = 512 + qi[None, None, :] - kj
    bucket = np.clip(np.clip(dist, -63, 128) + 63, 0, 191)
    bt = np.where(allowed[None], rb[:, bucket], np.float32(NEG))
    sh["bias_t"] = np.ascontiguousarray(bt.transpose(2, 0, 1, 3).reshape(128, 8 * 5 * 128).astype(np.float32))
    dup = lambda a: np.ascontiguousarray(np.concatenate([a, a], 0).astype(np.float32))
    sh["s5_lr"] = dup(f("ssm_lam_re").T)
    sh["s5_li"] = dup(f("ssm_lam_im").T)
    sh["s5_ls"] = np.ascontiguousarray(np.broadcast_to(f("ssm_log_step")[None, :], (128, 32)).astype(np.float32))
    sg = np.ones((128, 2), np.float32); sg[:64, 0] = -1.0; sg[64:, 1] = -1.0
    sh["s5_sg"] = sg
    sh["s5_nv"] = np.ascontiguousarray(np.broadcast_to(np.arange(-7, 16, dtype=np.float32)[None, :], (128, 23)))
    bre = f("ssm_b_re").transpose(1, 0, 2).reshape(64, 512); bim = f("ssm_b_im").transpose(1, 0, 2).reshape(64, 512)
    cre = f("ssm_c_re").transpose(2, 0, 1).reshape(64, 512); cim = f("ssm_c_im").transpose(2, 0, 1).reshape(64, 512)
    sh["s5_p1b"] = np.ascontiguousarray(np.concatenate([bre, bim], 0)); sh["s5_p2b"] = np.ascontiguousarray(np.concatenate([bim, bre], 0))
    sh["s5_p1c"] = np.ascontiguousarray(np.concatenate([cre, cim], 0)); sh["s5_p2c"] = np.ascontiguousarray(np.concatenate([cim, cre], 0))
    dd = np.zeros((2, 4, 16, 4, 4, 16), np.float32)
    dsk = f("ssm_d")
    for g in range(32):
        chc = g // 8; hb = (g % 8) // 4; j4 = g % 4
        for c in range(16):
            dd[hb, j4, c, chc, j4, c] = dsk[g, c]
    sh["s5_dd"] = dd.reshape(128, 256)
    sh["g_ssm_out"] = _col(f("ssm_out_g"), 4)
    sh["g_att_out"] = _col(f("att_out_g"), 4)
    sh["b_glu"] = f("ssm_b_glu").reshape(1, 512)
    sh["w_glu"] = np.ascontiguousarray(f("ssm_w_glu"))
    sh["w_out"] = np.ascontiguousarray(f("w_out"))
    return sh


def host_core(inp, c):
    b, seg = c // 4, c % 4
    x = np.asarray(inp["x"], np.float32)
    xe = np.zeros((8192, 1024), np.float32)
    n = (seg + 1) * 2048
    xe[8192 - n:] = x[b, :n]
    hv = np.full((512,), 1.0 if seg > 0 else 0.0, np.float32)
    return {"x_ext": xe, "hvalid": np.ascontiguousarray(hv.reshape(4, 128).T)}


IN_SHAPES = {
    "x_ext": [8192, 1024], "hvalid": [128, 4], "g_mix": [128, 8], "w_in": [1024, 2048], "att_q_g": [1, 64], "att_k_g": [1, 64],
    "bias_t": [128, 5120], "s5_lr": [128, 32], "s5_li": [128, 32], "s5_ls": [128, 32], "s5_sg": [128, 2], "s5_nv": [128, 23],
    "s5_p1b": [128, 512], "s5_p2b": [128, 512], "s5_p1c": [128, 512], "s5_p2c": [128, 512], "s5_dd": [128, 256],
    "g_ssm_out": [128, 4], "g_att_out": [128, 4], "b_glu": [1, 512], "w_glu": [512, 512], "w_out": [1024, 1024],
}
IN_SHAPES_MEM = {"mem": [256, 1024], "g_mem": [128, 8], "g_memkv": [128, 8], "mem_q_g": [1, 256], "mem_k_g": [1, 256],
                 "w_mem_q": [1024, 1024], "w_mem_k": [1024, 1024], "w_mem_v": [1024, 1024], "w_mem_o": [1024, 1024]}
IN_SHAPES_PEER = {"g_peer": [1, 1024], "w_peer_q": [1024, 2048], "keysT": [128, 2048], "peer_uT": [1024, 16384], "peer_v": [16384, 1024]}
SCRATCH = {"kT_d": ([128, 4 * 2560], BF16), "qT_d": ([128, 4 * 2048], BF16), "V_d": ([20, 128, 520], BF16),
           "hs_d": ([2048, 1024], F32), "Mz_d": ([128, 16 * 8 * 128], BF16), "CA_d": ([128, 2, 4096], BF16)}
SCRATCH_PEER = {"uT_b": ([1024, 16384], BF16), "v_b": ([16384, 1024], BF16), "xnT_d": ([16, 128, 1024], BF16),
                "sc_d": ([16, 128, 2048], F32), "tau_d": ([16, 128, 8], F32)}


def host_shared_rest(inp):
    f = lambda k: np.asarray(inp[k], np.float32)[0]
    sh = {}
    sh["g_mem"] = _col(f("norm_mem_g"), 8); sh["g_memkv"] = _col(f("norm_memkv_g"), 8)
    sh["mem_q_g"] = f("mem_q_g").reshape(1, 256); sh["mem_k_g"] = f("mem_k_g").reshape(1, 256)
    for k in ("w_mem_q", "w_mem_k", "w_mem_v", "w_mem_o", "w_peer_q"):
        sh[k] = np.ascontiguousarray(f(k))
    sh["g_peer"] = f("norm_peer_g").reshape(1, 1024)
    sh["keysT"] = np.ascontiguousarray(f("peer_keys").transpose(3, 0, 1, 2).reshape(128, 2048))
    sh["peer_uT"] = np.ascontiguousarray(f("peer_u").T)
    sh["peer_v"] = np.ascontiguousarray(f("peer_v"))
    return sh


def build_program(stages=("mixer", "mem", "peer"), dbg_specs=None, upto=9):
    nc = bass.Bass("TRN2", target_bir_lowering=False)
    D = {}
    shapes = {}
    if "mixer" in stages:
        shapes.update(IN_SHAPES)
    if "mem" in stages:
        shapes.update(IN_SHAPES_MEM)
    if "peer" in stages:
        shapes.update(IN_SHAPES_PEER)
    for k, shp in shapes.items():
        D[k] = nc.dram_tensor(k, shp, F32, kind="ExternalInput").ap()
    scr = {}
    if "mixer" in stages:
        scr.update(SCRATCH)
    if "peer" in stages:
        scr.update(SCRATCH_PEER)
    for k, (shp, dt) in scr.items():
        D[k] = nc.dram_tensor(k, shp, dt).ap()
    chain = ["h1_d", "h2_d", "out"]
    first = {"mixer": None, "mem": "h1_d", "peer": "h2_d"}[stages[0]]
    last = {"mixer": "h1_d", "mem": "h2_d", "peer": "out"}[stages[-1]]
    for k in chain:
        if k == first:
            D[k] = nc.dram_tensor(k, [2048, 1024], F32, kind="ExternalInput").ap()
        elif k == last:
            D[k] = nc.dram_tensor(k, [2048, 1024], F32, kind="ExternalOutput").ap()
        else:
            D[k] = nc.dram_tensor(k, [2048, 1024], F32).ap()
    dbg = {}
    for k, shp in (dbg_specs or {}).items():
        dbg[k] = nc.dram_tensor("dbg_" + k, shp, F32, kind="ExternalOutput").ap()
    with ExitStack() as es:
        S = Sched(nc, es)
        C = Ctx(nc, S)
        prep = stage_peer_prep(C, D) if "peer" in stages else None
        if "mixer" in stages:
            stage_mixer(C, D, dbg, upto, prep=prep)
        if "mem" in stages:
            stage_mem(C, D, dbg, prep=prep)
        drip(prep, 1000)
        if "peer" in stages:
            stage_peer(C, D, dbg)
        S.barrier()
        S.emit()
    return nc, S


def _headnorm(C, src_ap, skey, nh, hd, sqt, sqk, stat, stk, gt, gkey, dst_ap, dkey):
    S = C.S
    sv = src_ap.rearrange("p (h d) -> p h d", d=hd)
    S.op("pool", lambda e: e.tensor_tensor(out=sqt, in0=src_ap, in1=src_ap, op=ALU.mult), reads=[skey], writes=[sqk])
    S.op("dve", lambda e: e.tensor_reduce(out=stat[:, 0:nh], in_=sqt.rearrange("p (h d) -> p h d", d=hd), axis=AX.X, op=ALU.add), reads=[sqk], writes=[stk + "a"])
    S.op("act", lambda e: e.activation(out=stat[:, nh:2 * nh], in_=stat[:, 0:nh], func=AF.Sqrt, scale=1.0 / hd, bias=EPS), reads=[stk + "a"], writes=[stk + "b"])
    S.op("dve", lambda e: e.reciprocal(out=stat[:, nh:2 * nh], in_=stat[:, nh:2 * nh]), reads=[stk + "b"], writes=[stk])
    S.op("dve", lambda e: e.tensor_tensor(out=sv, in0=sv, in1=bc(stat[:, nh:2 * nh].unsqueeze(2), [128, nh, hd]), op=ALU.mult), reads=[skey, stk], writes=[skey])
    S.op("pool", lambda e: e.tensor_tensor(out=dst_ap.rearrange("p (h d) -> p h d", d=hd), in0=sv, in1=bc(gt.unsqueeze(1), [128, nh, hd]), op=ALU.mult), reads=[skey, gkey], writes=[dkey])


def stage_mem(C, D, dbg=None, prep=None):
    S = C.S
    dbg = dbg or {}
    with scope(C) as es:
        sb = lambda name, shape, dt=F32: C.sb(es, name, shape, dt)
        idf, idb = make_ident(C, es, "ident")
        gm = sb("gm", [128, 8]); gkv = sb("gkv", [128, 8]); gq = sb("mgq", [128, 256]); gk = sb("mgk", [128, 256])
        S.dma("sp", gm[:], D["g_mem"], writes=["gm"]); S.dma("sp", gkv[:], D["g_memkv"], writes=["gkv"])
        S.dma("sp", gq[:], D["mem_q_g"].partition_broadcast(128), writes=["mgq"]); S.dma("sp", gk[:], D["mem_k_g"].partition_broadcast(128), writes=["mgk"])
        S.op("dve", lambda e: e.tensor_scalar(out=gq[:], in0=gq[:], scalar1=1.0 / 16, scalar2=None, op0=ALU.mult), reads=["mgq"], writes=["mgq"])
        kTm = sb("kTm", [128, 8, 256], BF16); Vm = sb("Vm", [128, 2, 4, 257], BF16)
        xt = [sb("mxt%d" % i, [128, 1024]) for i in range(2)]; st = sb("mst", [128, 2]); xn = sb("mxn", [128, 1024], BF16)
        xnT = sb("mxnT", [128, 8, 128], BF16); qf = sb("mqf", [128, 1024]); sq = sb("msq", [128, 1024]); qst = sb("mqst", [128, 8])
        qn = sb("mqn", [128, 1024], BF16); mjunk = sb("mjunk", [128, 1024], BF16)
        with scope(C) as esk:
            with scope(C) as est:
                Wk = load_weight_bf16(C, esk, est, "Wmk", D["w_mem_k"], 8, 1024, gcol=(gkv, "gkv"))
            with scope(C) as est:
                Wv = load_weight_bf16(C, esk, est, "Wmv", D["w_mem_v"], 8, 1024, gcol=(gkv, "gkv"))
            with scope(C) as esp:
                bank = [C.ps(esp, "mkb%d" % i, [128, 512]) for i in range(6)]
                mv = D["mem"].rearrange("(n p) d -> n p d", p=128)
                for mt in range(2):
                    x_ = xt[mt]; xk = "mxt%d" % mt
                    S.dma("sp", x_[:], mv[mt], writes=[xk])
                    rms_rstd(C, x_[:], xk, mjunk[:], "mjunk", st[:, 0:1], st[:, 1:2], "mst", 1024)
                    S.op("dve", lambda e, x_=x_: e.tensor_scalar(out=xn[:], in0=x_[:], scalar1=st[:, 1:2], scalar2=None, op0=ALU.mult), reads=[xk, "mst"], writes=["mxn"])
                    pb = bank[0][:].bitcast(BF16).rearrange("p (c n) -> p c n", n=128)
                    transpose_chunks(C, xn[:], "mxn", 8, pb, "mkb0", xnT[:], "mxnT", idb)
                    for (W, wk, b0) in ((Wk, "Wmk", 1), (Wv, "Wmv", 3)):
                        for cb in range(2):
                            for dc in range(8):
                                S.op("pe", lambda e, W=W, cb=cb, dc=dc, b0=b0: e.matmul(bank[b0 + cb][:], lhsT=xnT[:, dc, :], rhs=W[:, dc, cb * 512:(cb + 1) * 512], start=(dc == 0), stop=(dc == 7)),
                                     reads=["mxnT", wk], writes=["mkb%d" % (b0 + cb)])
                    for cb in range(2):
                        S.op("act", lambda e, cb=cb: e.copy(out=qf[:, cb * 512:(cb + 1) * 512], in_=bank[1 + cb][:]), reads=["mkb%d" % (1 + cb)], writes=["mqf"])
                        S.op("dve", lambda e, cb=cb, mt=mt: e.tensor_copy(out=Vm[:, mt, 2 * cb:2 * cb + 2, 0:256], in_=bank[3 + cb][:].rearrange("p (h d) -> p h d", d=256)), reads=["mkb%d" % (3 + cb)], writes=["Vm"])
                    S.op("pool", lambda e, mt=mt: e.memset(Vm[:, mt, :, 256], 1.0), reads=[], writes=["Vm"])
                    _headnorm(C, qf[:], "mqf", 4, 256, sq[:], "msq", qst, "mqst", gk[:], "mgk", qn[:], "mqn")
                    pb2 = bank[5][:].bitcast(BF16).rearrange("p (c n) -> p c n", n=128)
                    transpose_chunks(C, qn[:], "mqn", 8, pb2, "mkb5", kTm[:, :, mt * 128:(mt + 1) * 128], "kTm", idb)
        with scope(C) as est:
            Wq = load_weight_bf16(C, es, est, "Wmq", D["w_mem_q"], 8, 1024, gcol=(gm, "gm"))
        with scope(C) as est:
            Wo = load_weight_bf16(C, es, est, "Wmo", D["w_mem_o"], 8, 1024)
        NM = 2
        xn2 = [sb("mxn_%d" % i, [128, 1024], BF16) for i in range(NM)]; xnT2 = [sb("mxnT_%d" % i, [128, 8, 128], BF16) for i in range(NM)]
        qf2 = [sb("mqf_%d" % i, [128, 1024]) for i in range(NM)]; sq2 = [sb("msq_%d" % i, [128, 1024]) for i in range(NM)]; qst2 = [sb("mqst_%d" % i, [128, 8]) for i in range(NM)]
        qn2 = [sb("mqn_%d" % i, [128, 1024], BF16) for i in range(NM)]; st2 = [sb("mst_%d" % i, [128, 2]) for i in range(NM)]
        qT2 = [sb("mqT%d" % i, [128, 8, 128], BF16) for i in range(NM)]; PT = [sb("mPT%d" % i, [128, 2, 128], BF16) for i in range(2)]
        rd2 = [sb("mrd%d" % i, [128, 4]) for i in range(NM)]; ob2 = [sb("mob%d" % i, [128, 1024], BF16) for i in range(NM)]; oT2 = [sb("moT%d" % i, [128, 8, 128], BF16) for i in range(NM)]
        h2t = [sb("h2t%d" % i, [128, 1024]) for i in range(2)]
        hv = D["h1_d"].rearrange("(n p) d -> n p d", p=128); ov = D["h2_d"].rearrange("(n p) d -> n p d", p=128)
        with scope(C) as esp:
            bank = [C.ps(esp, "mb%d" % i, [128, 512]) for i in range(8)]

            def mtile(tt):
                sl = tt % NM
                K = lambda nm: "%s_%d" % (nm, sl)
                xn = xn2[sl]; xnT = xnT2[sl]; qf = qf2[sl]; sq = sq2[sl]; qst = qst2[sl]; qn = qn2[sl]; st = st2[sl]; qT = qT2[sl]; rd = rd2[sl]; ob = ob2[sl]; oT = oT2[sl]
                drip(prep, 1)
                x_ = xt[sl]; xk = "mxt%d" % sl
                S.dma("sp", x_[:], hv[tt], reads=["h1_d"], writes=[xk])
                rms_rstd(C, x_[:], xk, mjunk[:], "mjunk", st[:, 0:1], st[:, 1:2], K("mst"), 1024)
                S.op("dve", lambda e: e.tensor_scalar(out=xn[:], in0=x_[:], scalar1=st[:, 1:2], scalar2=None, op0=ALU.mult), reads=[xk, K("mst")], writes=[K("mxn")])
                pb = bank[0][:].bitcast(BF16).rearrange("p (c n) -> p c n", n=128)
                transpose_chunks(C, xn[:], K("mxn"), 8, pb, "mb0", xnT[:], K("mxnT"), idb)
                yield
                for cb in range(2):
                    for dc in range(8):
                        S.op("pe", lambda e, cb=cb, dc=dc: e.matmul(bank[1 + cb][:], lhsT=xnT[:, dc, :], rhs=Wq[:, dc, cb * 512:(cb + 1) * 512], start=(dc == 0), stop=(dc == 7)),
                             reads=[K("mxnT"), "Wmq"], writes=["mb%d" % (1 + cb)])
                    S.op("act", lambda e, cb=cb: e.copy(out=qf[:, cb * 512:(cb + 1) * 512], in_=bank[1 + cb][:]), reads=["mb%d" % (1 + cb)], writes=[K("mqf")])
                yield
                _headnorm(C, qf[:], K("mqf"), 4, 256, sq[:], K("msq"), qst, K("mqst"), gq[:], "mgq", qn[:], K("mqn"))
                pb2 = bank[3][:].bitcast(BF16).rearrange("p (c n) -> p c n", n=128)
                transpose_chunks(C, qn[:], K("mqn"), 8, pb2, "mb3", qT[:], K("mqT"), idb)
                yield
                for h in range(4):
                    ps_ = h % 2
                    sbk = bank[4 + ps_]; sbkk = "mb%d" % (4 + ps_)
                    for mt in range(2):
                        for dh in range(2):
                            S.op("pe", lambda e, h=h, mt=mt, dh=dh, sbk=sbk: e.matmul(sbk[:, mt * 128:(mt + 1) * 128], lhsT=kTm[:, 2 * h + dh, mt * 128:(mt + 1) * 128], rhs=qT[:, 2 * h + dh, :], start=(dh == 0), stop=(dh == 1)),
                                 reads=["kTm", K("mqT")], writes=[sbkk])
                    S.op("act", lambda e, ps_=ps_, sbk=sbk: e.activation(out=PT[ps_][:].rearrange("p m q -> p (m q)"), in_=sbk[:, 0:256], func=AF.Exp, bias=-8.0), reads=[sbkk], writes=["mPT%d" % ps_])
                    obk = bank[6 + ps_]; obkk = "mb%d" % (6 + ps_)
                    for mt in range(2):
                        S.op("pe", lambda e, h=h, mt=mt, ps_=ps_, obk=obk: e.matmul(obk[:, 0:257], lhsT=PT[ps_][:, mt, :], rhs=Vm[:, mt, h, :], start=(mt == 0), stop=(mt == 1)), reads=["mPT%d" % ps_, "Vm"], writes=[obkk])
                    S.op("dve", lambda e, h=h, obk=obk: e.reciprocal(out=rd[:, h:h + 1], in_=obk[:, 256:257]), reads=[obkk], writes=[K("mrd") + "_%d" % h])
                    S.op("dve", lambda e, h=h, obk=obk: e.tensor_scalar(out=ob[:, h * 256:(h + 1) * 256], in0=obk[:, 0:256], scalar1=rd[:, h:h + 1], scalar2=None, op0=ALU.mult), reads=[obkk, K("mrd") + "_%d" % h], writes=[K("mob")])
                    yield
                pb3 = bank[0][:].bitcast(BF16).rearrange("p (c n) -> p c n", n=128)
                transpose_chunks(C, ob[:], K("mob"), 8, pb3, "mb0", oT[:], K("moT"), idb)
                yield
                for cb in range(2):
                    for dc in range(8):
                        S.op("pe", lambda e, cb=cb, dc=dc: e.matmul(bank[1 + cb][:], lhsT=oT[:, dc, :], rhs=Wo[:, dc, cb * 512:(cb + 1) * 512], start=(dc == 0), stop=(dc == 7)),
                             reads=[K("moT"), "Wmo"], writes=["mb%d" % (1 + cb)])
                    S.op("dve", lambda e, cb=cb: e.tensor_tensor(out=h2t[sl][:, cb * 512:(cb + 1) * 512], in0=bank[1 + cb][:], in1=x_[:, cb * 512:(cb + 1) * 512], op=ALU.add),
                         reads=["mb%d" % (1 + cb), xk], writes=["h2t%d" % sl])
                S.dma("sp", ov[tt], h2t[sl][:], reads=["h2t%d" % sl], writes=["h2_d"])

            from itertools import zip_longest
            gens = []
            for t0 in range(0, 16, NM):
                for _ in zip_longest(*[mtile(t0 + i) for i in range(NM)]):
                    pass


def stage_peer_prep(C, D):
    S = C.S
    uv = D["peer_uT"].rearrange("(c p) (a e) -> c p a e", p=128, e=2048)
    ub = D["uT_b"].rearrange("(c p) (a e) -> c p a e", p=128, e=2048)
    vv = D["peer_v"].rearrange("(c p) d -> c p d", p=512)
    vb = D["v_b"].rearrange("(c p) d -> c p d", p=512)

    def gen():
        for c in range(8):
            S.dma("pool", ub[c], uv[c], writes=["uT_b"])
            yield
        for c in range(32):
            S.dma("pool", vb[c], vv[c], writes=["v_b"])
            yield
    return gen()


def drip(g, n):
    if g is None:
        return
    for _ in range(n):
        try:
            next(g)
        except StopIteration:
            return


def _top16(C, src, skey, work, wkey, dst, dkey):
    S = C.S
    S.op("dve", lambda e: e.max(out=dst[:, 0:8], in_=src), reads=[skey], writes=[dkey])
    S.op("dve", lambda e: e.match_replace(out=work, in_to_replace=dst[:, 0:8], in_values=src, imm_value=-1e30), reads=[skey, dkey], writes=[wkey])
    S.op("dve", lambda e: e.max(out=dst[:, 8:16], in_=work), reads=[wkey], writes=[dkey])


def stage_peer(C, D, dbg=None):
    S = C.S
    dbg = dbg or {}
    hv = D["h2_d"].rearrange("(n p) d -> n p d", p=128)
    with scope(C) as es:
        sb = lambda name, shape, dt=F32: C.sb(es, name, shape, dt)
        idf, idb = make_ident(C, es, "ident")
        gp = sb("gpb", [128, 1024])
        S.dma("sp", gp[:], D["g_peer"].partition_broadcast(128), writes=["gpb"])
        with scope(C) as est:
            Wq = load_weight_bf16(C, es, est, "Wpq", D["w_peer_q"], 8, 2048)
        keyT = sb("keyT", [128, 16, 128], BF16)
        with scope(C) as est:
            kst = C.sb(est, "kst", [128, 2048])
            S.dma("sp", kst[:], D["keysT"], writes=["kst"])
            S.op("dve", lambda e: e.tensor_copy(out=keyT[:].rearrange("p a n -> p (a n)"), in_=kst[:]), reads=["kst"], writes=["keyT"])
        NS = 3
        xt = [sb("pxt%d" % i, [128, 1024]) for i in range(NS)]; st_ = [sb("pst%d" % i, [128, 2]) for i in range(NS)]; junk = sb("pjunk", [128, 1024], BF16)
        xn_ = [sb("pxn%d" % i, [128, 1024], BF16) for i in range(NS)]; xnT = [sb("pxnT%d" % i, [128, 8, 128], BF16) for i in range(NS)]
        qb_ = [sb("pqb%d" % i, [128, 2048], BF16) for i in range(NS)]; qTp_ = [sb("pqT%d" % i, [128, 16, 128], BF16) for i in range(NS)]
        sc = [sb("psc%d" % i, [128, 16, 128]) for i in range(NS)]; work_ = [sb("pwork%d" % i, [128, 256]) for i in range(NS)]
        sv_ = [sb("psv%d" % i, [128, 16, 16]) for i in range(NS)]; cand_ = [sb("pcand%d" % i, [128, 8, 256]) for i in range(NS)]
        cex_ = [sb("pcex%d" % i, [128, 8, 256]) for i in range(NS)]; ctop_ = [sb("pctop%d" % i, [128, 8, 16]) for i in range(NS)]
        Z_ = [sb("pZ%d" % i, [128, 8]) for i in range(NS)]; off_ = [sb("poff%d" % i, [128, 8]) for i in range(NS)]; tau = [sb("ptau%d" % i, [128, 8]) for i in range(NS)]
        cjunk_ = [[sb("pcj%d_%d" % (i, h), [128, 256], BF16) for h in range(8)] for i in range(NS)]; offs_ = [sb("poffs%d" % i, [128, 16]) for i in range(NS)]
        xTd = D["xnT_d"].rearrange("n p (c t) -> n p c t", t=128)
        with scope(C) as esp:
            bank = [C.ps(esp, "pab%d" % i, [128, 512]) for i in range(6)]

            def tile(tt):
                sl = tt % NS
                K = lambda nm: "%s%d" % (nm, sl)
                x_ = xt[sl]; xk = K("pxt"); st = st_[sl]; xn = xn_[sl]; qb = qb_[sl]; qTp = qTp_[sl]; work = work_[sl]
                sv = sv_[sl]; cand = cand_[sl]; cex = cex_[sl]; ctop = ctop_[sl]; Z = Z_[sl]; off = off_[sl]; cjunk = cjunk_[sl]; offs = offs_[sl]
                S.dma("sp", x_[:], hv[tt], reads=["h2_d"], writes=[xk])
                rms_rstd(C, x_[:], xk, junk[:], "pjunk", st[:, 0:1], st[:, 1:2], K("pst"), 1024)
                S.op("dve", lambda e: e.scalar_tensor_tensor(out=xn[:], in0=x_[:], scalar=st[:, 1:2], in1=gp[:], op0=ALU.mult, op1=ALU.mult), reads=[xk, K("pst"), "gpb"], writes=[K("pxn")])
                pb = bank[0][:].bitcast(BF16).rearrange("p (c n) -> p c n", n=128)
                transpose_chunks(C, xn[:], K("pxn"), 8, pb, "pab0", xnT[sl][:], K("pxnT"), idb)
                S.dma("sp", xTd[tt], xnT[sl][:], reads=[K("pxnT")], writes=["xnT_d"])
                yield
                for cb in range(4):
                    for dc in range(8):
                        S.op("pe", lambda e, cb=cb, dc=dc: e.matmul(bank[1 + cb][:], lhsT=xnT[sl][:, dc, :], rhs=Wq[:, dc, cb * 512:(cb + 1) * 512], start=(dc == 0), stop=(dc == 7)),
                             reads=[K("pxnT"), "Wpq"], writes=["pab%d" % (1 + cb)])
                    S.op("act", lambda e, cb=cb: e.copy(out=qb[:, cb * 512:(cb + 1) * 512], in_=bank[1 + cb][:]), reads=["pab%d" % (1 + cb)], writes=[K("pqb")])
                for half in range(2):
                    pbq = bank[5][:].bitcast(BF16).rearrange("p (c n) -> p c n", n=128)
                    transpose_chunks(C, qb[:, half * 1024:(half + 1) * 1024], K("pqb"), 8, pbq, "pab5", qTp[:, half * 8:(half + 1) * 8, :], K("pqT"), idb)
                for hh in range(16):
                    S.op("pe", lambda e, hh=hh: e.matmul(bank[1 + hh // 4][:, (hh % 4) * 128:(hh % 4 + 1) * 128], lhsT=qTp[:, hh, :], rhs=keyT[:, hh, :], start=True, stop=True),
                         reads=[K("pqT"), "keyT"], writes=["pab%d" % (1 + hh // 4)])
                scs = sc[sl]; sck = K("psc")
                for cb in range(4):
                    S.op("act", lambda e, cb=cb: e.copy(out=scs[:, cb * 4:(cb + 1) * 4, :].rearrange("p a n -> p (a n)"), in_=bank[1 + cb][:]), reads=["pab%d" % (1 + cb)], writes=[sck])
                yield
                for hh in range(16):
                    _top16(C, scs[:, hh, :], sck, work[:, 0:128], K("pwork"), sv[:, hh, :], K("psv"))
                    yield
                svv = sv[:].rearrange("p (h s) k -> p h s k", s=2)
                for h in range(8):
                    S.op("dve", lambda e, h=h: e.tensor_tensor(out=cand[:, h, :].rearrange("p (a b) -> p a b", b=16), in0=bc(svv[:, h, 0, :].unsqueeze(2), [128, 16, 16]),
                                                            in1=bc(svv[:, h, 1, :].unsqueeze(1), [128, 16, 16]), op=ALU.add), reads=[K("psv")], writes=[K("pcand") + "_%d" % h])
                    yield
                ck = [K("pcand") + "_%d" % h for h in range(8)]
                for h in range(8):
                    _top16(C, cand[:, h, :], ck[h], work[:], K("pwork"), ctop[:, h, :], K("pctop"))
                    yield
                S.op("dve", lambda e: e.tensor_tensor(out=cand[:], in0=cand[:], in1=bc(ctop[:, :, 0:1], [128, 8, 256]), op=ALU.subtract), reads=ck + [K("pctop")], writes=ck)
                S.op("act", lambda e: e.activation(out=cex[:].rearrange("p h n -> p (h n)"), in_=cand[:].rearrange("p h n -> p (h n)"), func=AF.Exp), reads=ck, writes=[K("pcex")])
                yield
                S.op("dve", lambda e: e.tensor_tensor(out=tau[sl][:], in0=ctop[:, :, 15], in1=ctop[:, :, 0], op=ALU.subtract), reads=[K("pctop")], writes=[K("ptau")])
                yield
                S.op("dve", lambda e: e.tensor_scalar(out=tau[sl][:], in0=tau[sl][:], scalar1=-1e-5, scalar2=None, op0=ALU.add), reads=[K("ptau")], writes=[K("ptau")])
                yield
                for h in range(8):
                    S.op("dve", lambda e, h=h: e.scalar_tensor_tensor(out=cjunk[h][:], in0=cand[:, h, :], scalar=tau[sl][:, h:h + 1], in1=cex[:, h, :], op0=ALU.is_ge, op1=ALU.mult, accum_out=Z[:, h:h + 1]),
                         reads=ck + [K("pcex"), K("ptau")], writes=[K("pZ") + "_%d" % h, K("pcj") + "_%d" % h])
                    yield
                S.op("act", lambda e: e.activation(out=off[:], in_=Z[:], func=AF.Ln), reads=[K("pZ") + "_%d" % h for h in range(8)], writes=[K("poff")])
                yield
                S.op("dve", lambda e: e.tensor_tensor(out=tau[sl][:], in0=tau[sl][:], in1=off[:], op=ALU.subtract), reads=[K("ptau"), K("poff")], writes=[K("ptau")])
                S.op("act", lambda e: e.activation(out=tau[sl][:], in_=tau[sl][:], func=AF.Exp), reads=[K("ptau")], writes=[K("ptau")])
                yield
                S.op("dve", lambda e: e.tensor_scalar(out=tau[sl][:], in0=tau[sl][:], scalar1=0.99997, scalar2=None, op0=ALU.mult), reads=[K("ptau")], writes=[K("ptau")])
                offv = offs[:].rearrange("p (h s) -> p h s", s=2)
                S.op("dve", lambda e: e.tensor_copy(out=offv[:, :, 0], in_=svv[:, :, 0, 0]), reads=[K("psv")], writes=[K("poffs") + "a"])
                yield
                S.op("dve", lambda e: e.tensor_tensor(out=offv[:, :, 1], in0=svv[:, :, 1, 0], in1=off[:], op=ALU.add), reads=[K("psv"), K("poff")], writes=[K("poffs") + "b"])
                yield
                S.op("dve", lambda e: e.tensor_tensor(out=scs[:], in0=scs[:], in1=bc(offs[:].unsqueeze(2), [128, 16, 128]), op=ALU.subtract), reads=[sck, K("poffs") + "a", K("poffs") + "b"], writes=[sck])
                S.op("act", lambda e: e.activation(out=scs[:].rearrange("p a n -> p (a n)"), in_=scs[:].rearrange("p a n -> p (a n)"), func=AF.Exp), reads=[sck], writes=[sck])
                S.dma("sp", D["sc_d"][tt], scs[:].rearrange("p a n -> p (a n)"), reads=[sck], writes=["sc_d"])
                S.dma("sp", D["tau_d"][tt], tau[sl][:], reads=[K("ptau")], writes=["tau_d"])

            from itertools import zip_longest
            for t0 in range(0, 16, NS):
                for _ in zip_longest(*[tile(t0 + i) for i in range(NS) if t0 + i < 16]):
                    pass
    with scope(C) as es:
        sb = lambda name, shape, dt=F32: C.sb(es, name, shape, dt)
        idf, idb = make_ident(C, es, "ident")
        xnT = sb("bxnT", [128, 4, 1024], BF16); sc = sb("bsc", [128, 4, 2048]); kap = sb("btau", [128, 4, 8])
        UT = [sb("UT%d" % i, [128, 8, 1024], BF16) for i in range(2)]; Vb = [sb("Vb%d" % i, [128, 8, 1024], BF16) for i in range(2)]
        acc = sb("pacc", [128, 4, 1024])
        NP = 12
        Pt = [sb("pP%d" % i, [128, 512]) for i in range(NP)]; Wh = [[sb("pWh%d_%d" % (i, h), [128, 512], BF16) for h in range(8)] for i in range(2)]
        G = [sb("pG%d" % i, [128, 512], BF16) for i in range(3)]; WA = [sb("pWA%d" % i, [128, 512], BF16) for i in range(2)]
        WAT = [sb("pWAT%d" % i, [128, 4, 128], BF16) for i in range(2)]
        h2t = [sb("ph2t%d" % i, [128, 1024]) for i in range(2)]
        uTv = D["uT_b"].rearrange("(c p) (b e) -> b p c e", p=128, e=1024)
        vbv = D["v_b"].rearrange("(b c p) d -> b p c d", p=128, c=8)
        ov = D["out"].rearrange("(n p) d -> n p d", p=128)
        with scope(C) as esp:
            bank = [C.ps(esp, "pbb%d" % i, [128, 512]) for i in range(7)]
            state = {"it": 0}

            def stage1a(u, tg, eb, sub, tt, es_):
                ub = u % 2
                xT = xnT[:, tt, :].rearrange("p (c t) -> p c t", t=128)
                scv = sc[:, tt, :].rearrange("p (h s n) -> p h s n", s=2, n=128)
                i0 = eb * 8 + sub * 4
                for dc in range(8):
                    S.op("pe", lambda e, dc=dc, xT=xT: e.matmul(bank[ub][:], lhsT=xT[:, dc, :], rhs=UT[es_][:, dc, sub * 512:(sub + 1) * 512], start=(dc == 0), stop=(dc == 7)),
                         reads=["bxnT", "UT%d" % es_], writes=["pbb%d" % ub])
                for h in range(8):
                    hs = state["it"] % NP; state["it"] += 1
                    if h >= 5:
                        S.op("pool", lambda e, h=h, hs=hs, scv=scv: e.tensor_tensor(out=Pt[hs][:].rearrange("p (i j) -> p i j", j=128), in0=bc(scv[:, h, 0, i0:i0 + 4].unsqueeze(2), [128, 4, 128]),
                                                                             in1=bc(scv[:, h, 1, :].unsqueeze(1), [128, 4, 128]), op=ALU.mult), reads=["bsc"], writes=["pP%d_%d" % (hs, il) for il in range(4)])
                    else:
                        for il in range(4):
                            S.op("act", lambda e, h=h, hs=hs, scv=scv, il=il: e.activation(out=Pt[hs][:, il * 128:(il + 1) * 128], in_=scv[:, h, 1, :], func=AF.Copy, scale=scv[:, h, 0, i0 + il:i0 + il + 1]),
                                 reads=["bsc"], writes=["pP%d_%d" % (hs, il)])
                    S.op("dve", lambda e, h=h, hs=hs: e.scalar_tensor_tensor(out=Wh[ub][h][:], in0=Pt[hs][:], scalar=kap[:, tt, h:h + 1], in1=Pt[hs][:], op0=ALU.is_ge, op1=ALU.mult),
                         reads=["pP%d_%d" % (hs, il) for il in range(4)] + ["btau"], writes=["pWh%d_%d" % (ub, h)])

            def st_gelu(u, tg, eb, sub, tt, es_):
                ub = u % 2; gb = u % 3
                S.op("act", lambda e: e.activation(out=G[gb][:], in_=bank[ub][:], func=AF.Gelu), reads=["pbb%d" % ub], writes=["pG%d" % gb])

            def st_hs(u, tg, eb, sub, tt, es_):
                ub = u % 2
                for h in range(8):
                    S.op("pe", lambda e, h=h: e.matmul(bank[2 + ub][:], lhsT=idb[:], rhs=Wh[ub][h][:], start=(h == 0), stop=(h == 7)),
                         reads=["identb", "pWh%d_%d" % (ub, h)], writes=["pbb%d" % (2 + ub)])

            def st_wa(u, tg, eb, sub, tt, es_):
                ub = u % 2; gb = u % 3
                S.op("dve", lambda e: e.tensor_tensor(out=WA[ub][:], in0=bank[2 + ub][:], in1=G[gb][:], op=ALU.mult), reads=["pbb%d" % (2 + ub), "pG%d" % gb], writes=["pWA%d" % ub])

            def st_t(u, tg, eb, sub, tt, es_):
                ub = u % 2
                pbt = bank[4][:].bitcast(BF16).rearrange("p (c n) -> p c n", n=128)[:, 0:4, :]
                for c in range(4):
                    S.op("pe", lambda e, c=c: e.transpose(out=pbt[:, c, :], in_=WA[ub][:, c * 128:(c + 1) * 128], identity=idb[:]), reads=["pWA%d" % ub, "identb"], writes=["pbb4"])

            def st_watcopy(u, tg, eb, sub, tt, es_):
                ub = u % 2
                pbt = bank[4][:].bitcast(BF16).rearrange("p (c n) -> p c n", n=128)[:, 0:4, :]
                S.op("act", lambda e: e.copy(out=WAT[ub][:], in_=pbt), reads=["pbb4"], writes=["pWAT%d" % ub])

            def st_v(u, tg, eb, sub, tt, es_):
                ub = u % 2
                for cb in range(2):
                    for ec in range(4):
                        S.op("pe", lambda e, cb=cb, ec=ec: e.matmul(bank[5 + cb][:], lhsT=WAT[ub][:, ec, :], rhs=Vb[es_][:, sub * 4 + ec, cb * 512:(cb + 1) * 512], start=(sub == 0 and ec == 0), stop=(sub == 1 and ec == 3)),
                             reads=["pWAT%d" % ub, "Vb%d" % es_], writes=["pbb%d" % (5 + cb)])

            def st_acc(u, tg, eb, sub, tt, es_):
                if sub != 1:
                    return
                for cb in range(2):
                    if eb == 0:
                        S.op("dve", lambda e, cb=cb: e.tensor_copy(out=acc[:, tt, cb * 512:(cb + 1) * 512], in_=bank[5 + cb][:]), reads=["pbb%d" % (5 + cb)], writes=["pacc%d" % tt])
                    else:
                        S.op("dve", lambda e, cb=cb: e.tensor_tensor(out=acc[:, tt, cb * 512:(cb + 1) * 512], in0=bank[5 + cb][:], in1=acc[:, tt, cb * 512:(cb + 1) * 512], op=ALU.add),
                             reads=["pbb%d" % (5 + cb), "pacc%d" % tt], writes=["pacc%d" % tt])

            u = 0
            for tg in range(4):
                S.dma("sp", xnT[:], D["xnT_d"][tg * 4:(tg + 1) * 4].rearrange("n p f -> p n f"), reads=["xnT_d"], writes=["bxnT"])
                S.dma("sp", sc[:], D["sc_d"][tg * 4:(tg + 1) * 4].rearrange("n p f -> p n f"), reads=["sc_d"], writes=["bsc"])
                S.dma("sp", kap[:], D["tau_d"][tg * 4:(tg + 1) * 4].rearrange("n p f -> p n f"), reads=["tau_d"], writes=["btau"])
                units = []
                for eb in range(16):
                    es_ = (tg * 16 + eb) % 2
                    for tt in range(4):
                        for sub in range(2):
                            units.append((u, tg, eb, sub, tt, es_)); u += 1
                n = len(units)
                U = lambda j: units[j] if 0 <= j < n else None
                for k in range(n + 3):
                    for ebk in ([0] if k == 0 else []) + ([k // 8 + 1] if (k % 8 == 3 and k // 8 + 1 < 16) else []):
                        es_ = (tg * 16 + ebk) % 2
                        S.dma("sp", UT[es_][:], uTv[ebk], reads=["uT_b"], writes=["UT%d" % es_])
                        S.dma("sp", Vb[es_][:], vbv[ebk], reads=["v_b"], writes=["Vb%d" % es_])
                    if U(k - 2): st_wa(*U(k - 2))
                    if U(k - 3): st_v(*U(k - 3))
                    if U(k - 2): st_t(*U(k - 2))
                    if U(k - 1): st_gelu(*U(k - 1))
                    if U(k - 1): st_hs(*U(k - 1))
                    if U(k): stage1a(*U(k))
                    if U(k - 2): st_watcopy(*U(k - 2))
                    if U(k - 3): st_acc(*U(k - 3))
                for tt in range(4):
                    sl = tt % 2; n = tg * 4 + tt
                    S.dma("sp", h2t[sl][:], hv[n], reads=["h2_d"], writes=["ph2t%d" % sl])
                    S.op("pool", lambda e, sl=sl, tt=tt: e.tensor_tensor(out=h2t[sl][:], in0=h2t[sl][:], in1=acc[:, tt, :], op=ALU.add), reads=["ph2t%d" % sl, "pacc%d" % tt], writes=["ph2t%d" % sl])
                    S.dma("sp", ov[n], h2t[sl][:], reads=["ph2t%d" % sl], writes=["out"])


_PROG = {}


def kernel(**inputs):
    sh = host_shared(inputs)
    sh.update(host_shared_rest(inputs))
    if "nc" not in _PROG:
        _PROG["nc"] = build_program(("mixer", "mem", "peer"))[0]
    nc = _PROG["nc"]
    names = set(IN_SHAPES) | set(IN_SHAPES_MEM) | set(IN_SHAPES_PEER)
    mem = np.asarray(inputs["mem"], np.float32)
    maps = []
    for c in range(8):
        m = {k: v for k, v in sh.items() if k in names}
        m.update(host_core(inputs, c))
        m["mem"] = np.ascontiguousarray(mem[c // 4])
        maps.append(m)
    res = run_bass_kernel_spmd(nc, maps, core_ids=list(range(8)))
    out = np.zeros((2, 8192, 1024), np.float32)
    for c in range(8):
        out[c // 4, (c % 4) * 2048:(c % 4 + 1) * 2048] = res.results[c]["out"]
    return out
```

```python
from contextlib import ExitStack, contextmanager
import numpy as np
import concourse.bass as bass
import concourse.mybir as mybir
from concourse.bass_utils import run_bass_kernel_spmd

F32 = mybir.dt.float32
BF16 = mybir.dt.bfloat16
I32 = mybir.dt.int32
AF = mybir.ActivationFunctionType
ALU = mybir.AluOpType
AX = mybir.AxisListType

ENGS = ("pe", "act", "dve", "pool", "sp")
TWO_PI = 6.283185307179586
EPS = 1e-6
NEG = -30000.0


class Sched:
    def __init__(self, nc, es, n_dma_sems=32):
        self.nc = nc
        self.streams = {e: [] for e in ENGS}
        self.sem = {e: es.enter_context(nc.semaphore("s_" + e)) for e in ("pe", "act", "dve", "pool")}
        self.cnt = {e: 0 for e in ("pe", "act", "dve", "pool")}
        self.dsem = [es.enter_context(nc.semaphore("s_dma%d" % i)) for i in range(n_dma_sems)]
        self.dcnt = [0] * n_dma_sems
        self.dnext = 0
        self.n_sw = 4
        self.dnext_sw = 0
        self.waited = {}
        self.last_w = {}
        self.readers = {}
        self.n_ops = 0

    def _deps(self, eng, reads, writes):
        deps = []
        for k in reads:
            if k in self.last_w:
                deps.append(self.last_w[k])
        for k in writes:
            if k in self.last_w:
                deps.append(self.last_w[k])
            deps.extend(self.readers.get(k, ()))
        need = {}
        for (sk, val, peng) in deps:
            if peng == "pe" and eng == "pe":
                continue
            if self.waited.get((eng, sk), 0) >= val:
                continue
            if need.get(sk, 0) < val:
                need[sk] = val
        return need

    def _semobj(self, sk):
        return self.sem[sk] if isinstance(sk, str) else self.dsem[sk]

    def _emit_waits(self, eng, need):
        for sk, val in need.items():
            self.waited[(eng, sk)] = val
            so = self._semobj(sk)
            self.streams[eng].append(lambda e, so=so, val=val: e.wait_ge(so, val))

    def _record(self, tok, reads, writes):
        for k in writes:
            self.last_w[k] = tok
            self.readers[k] = []
        for k in reads:
            if k not in writes:
                self.readers.setdefault(k, []).append(tok)

    def op(self, eng, fn, reads=(), writes=()):
        need = self._deps(eng, reads, writes)
        self._emit_waits(eng, need)
        self.cnt[eng] += 1
        val = self.cnt[eng]
        so = self.sem[eng]
        self.streams[eng].append(lambda e, fn=fn, so=so: fn(e).then_inc(so, 1))
        self._record((eng, val, eng), reads, writes)
        self.n_ops += 1

    def dma(self, q, out, in_, reads=(), writes=(), **kw):
        nhw = len(self.dsem) - self.n_sw
        if q == "pool":
            i = nhw + self.dnext_sw
            self.dnext_sw = (self.dnext_sw + 1) % self.n_sw
        else:
            i = self.dnext
            self.dnext = (self.dnext + 1) % nhw
        need = self._deps(q, reads, writes)
        prev = 16 * self.dcnt[i]
        if prev and self.waited.get((q, i), 0) < prev:
            need[i] = max(need.get(i, 0), prev)
        self._emit_waits(q, need)
        self.dcnt[i] += 1
        val = 16 * self.dcnt[i]
        so = self.dsem[i]
        self.streams[q].append(
            lambda e, out=out, in_=in_, so=so, kw=kw: e.dma_start(out=out, in_=in_, **kw).then_inc(so, 16))
        self._record((i, val, "dma"), reads, writes)
        self.n_ops += 1

    def barrier(self):
        for eng in ENGS:
            need = {}
            for pe_ in ("pe", "act", "dve", "pool"):
                v = self.cnt[pe_]
                if v and self.waited.get((eng, pe_), 0) < v:
                    need[pe_] = v
            for i, c in enumerate(self.dcnt):
                if c and self.waited.get((eng, i), 0) < 16 * c:
                    need[i] = 16 * c
            self._emit_waits(eng, need)

    def wait_all(self, eng, keys):
        need = {}
        for k in keys:
            if k in self.last_w:
                sk, val, _ = self.last_w[k]
                if self.waited.get((eng, sk), 0) < val and need.get(sk, 0) < val:
                    need[sk] = val
        self._emit_waits(eng, need)

    def emit(self):
        if not any(self.streams[e] for e in ENGS):
            return
        streams = self.streams
        self.streams = {e: [] for e in ENGS}
        self._emit_block(streams)

    def _emit_block(self, streams):
        self_streams = streams
        with self.nc.Block() as block:
            @block.tensor
            def _(e):
                for f in self_streams["pe"]:
                    f(e)

            @block.scalar
            def _(e):
                for f in self_streams["act"]:
                    f(e)

            @block.vector
            def _(e):
                for f in self_streams["dve"]:
                    f(e)

            @block.gpsimd
            def _(e):
                for f in self_streams["pool"]:
                    f(e)

            @block.sync
            def _(e):
                for f in self_streams["sp"]:
                    f(e)


class Ctx:
    def __init__(self, nc, S):
        self.nc = nc
        self.S = S
        self.uid = 0

    def sb(self, es, name, shape, dt=F32):
        self.uid += 1
        return es.enter_context(self.nc.sbuf_tensor("%s_%d" % (name, self.uid), list(shape), dt))

    def ps(self, es, name, shape, dt=F32):
        self.uid += 1
        return es.enter_context(self.nc.psum_tensor("%s_%d" % (name, self.uid), list(shape), dt))


@contextmanager
def scope(C):
    with ExitStack() as es:
        yield es
        C.S.barrier()
        C.S.emit()


def bc(ap, shape):
    return ap.to_broadcast(list(shape))


def make_ident(C, es, name="ident"):
    S = C.S
    idf = C.sb(es, name + "f", [128, 128])
    idb = C.sb(es, name + "b", [128, 128], BF16)
    S.op("pool", lambda e: e.memset(idf[:], 1.0), writes=[name + "f"])
    S.op("pool", lambda e: e.affine_select(out=idf[:], in_=idf[:], pattern=[[-1, 128]], compare_op=ALU.is_equal,
                                           fill=0.0, base=0, channel_multiplier=1), reads=[name + "f"], writes=[name + "f"])
    S.op("dve", lambda e: e.tensor_copy(out=idb[:], in_=idf[:]), reads=[name + "f"], writes=[name + "b"])
    return idf, idb


def sincos(C, es, ang, n, tag):
    S = C.S
    outs = []
    for which, off in (("s", 64.0), ("c", 64.25)):
        k = tag + which
        y = C.sb(es, k + "y", [128, n]); yi = C.sb(es, k + "yi", [128, n], I32); yf = C.sb(es, k + "yf", [128, n])
        m = C.sb(es, k + "m", [128, n]); o = C.sb(es, k + "o", [128, n])
        S.op("dve", lambda e, y=y, off=off: e.tensor_scalar(out=y[:], in0=ang, scalar1=1.0 / TWO_PI, scalar2=off, op0=ALU.mult, op1=ALU.add),
             reads=[tag + "ang"], writes=[k + "y"])
        S.op("dve", lambda e, y=y, yi=yi: e.tensor_copy(out=yi[:], in_=y[:]), reads=[k + "y"], writes=[k + "yi"])
        S.op("dve", lambda e, yi=yi, yf=yf: e.tensor_copy(out=yf[:], in_=yi[:]), reads=[k + "yi"], writes=[k + "yf"])
        S.op("dve", lambda e, y=y, yf=yf: e.tensor_tensor(out=y[:], in0=y[:], in1=yf[:], op=ALU.subtract), reads=[k + "y", k + "yf"], writes=[k + "y"])
        S.op("dve", lambda e, y=y, m=m: e.tensor_scalar(out=m[:], in0=y[:], scalar1=0.5, scalar2=None, op0=ALU.is_gt), reads=[k + "y"], writes=[k + "m"])
        S.op("dve", lambda e, y=y, m=m: e.tensor_tensor(out=y[:], in0=y[:], in1=m[:], op=ALU.subtract), reads=[k + "y", k + "m"], writes=[k + "y"])
        S.op("act", lambda e, y=y, o=o: e.activation(out=o[:], in_=y[:], func=AF.Sin, scale=TWO_PI), reads=[k + "y"], writes=[k + "o"])
        outs.append((o, k + "o"))
    return outs


def s5_params(C, es_keep, D):
    S, nc = C.S, C.nc
    P = {}
    LT2 = C.sb(es_keep, "LT2", [128, 8, 8, 2, 128], BF16)
    WPr = C.sb(es_keep, "WPr", [128, 9, 16]); WPi = C.sb(es_keep, "WPi", [128, 9, 16]); WPn = C.sb(es_keep, "WPn", [128, 9, 16])
    P.update(LT2=LT2, WPr=WPr, WPi=WPi, WPn=WPn)
    with scope(C) as es:
        sb = lambda name, shape, dt=F32: C.sb(es, name, shape, dt)
        Mz2 = sb("Mz2", [128, 16, 8, 128], BF16)
        CA = sb("CA", [128, 32, 128], BF16); CAs = sb("CAs", [128, 32, 128], BF16)
        LR = sb("LR", [128, 32]); LI = sb("LI", [128, 32]); LS = sb("LS", [128, 32])
        SG = sb("SG", [128, 2]); NV = sb("NV", [128, 23])
        P1B = sb("P1B", [128, 32, 16]); P2B = sb("P2B", [128, 32, 16]); P1C = sb("P1C", [128, 32, 16]); P2C = sb("P2C", [128, 32, 16])
        DD = sb("DD", [128, 16, 16])
        for t, nm in ((LR, "s5_lr"), (LI, "s5_li"), (LS, "s5_ls"), (SG, "s5_sg"), (NV, "s5_nv")):
            S.dma("sp", t[:], D[nm], writes=[nm])
        S.dma("sp", DD[:].rearrange("p e c -> p (e c)"), D["s5_dd"], writes=["s5_dd"])
        for t, nm in ((P1B, "s5_p1b"), (P2B, "s5_p2b"), (P1C, "s5_p1c"), (P2C, "s5_p2c")):
            S.dma("sp", t[:].rearrange("p g c -> p (g c)"), D[nm], writes=[nm])
        idf, idb = make_ident(C, es, "pid")
        STEP = sb("STEP", [128, 32]); AA = sb("AA", [128, 32]); PH = sb("PH", [128, 32])
        S.op("act", lambda e: e.activation(out=STEP[:], in_=LS[:], func=AF.Exp), reads=["s5_ls"], writes=["STEP"])
        S.op("dve", lambda e: e.tensor_tensor(out=AA[:], in0=LR[:], in1=STEP[:], op=ALU.mult), reads=["s5_lr", "STEP"], writes=["AA"])
        S.op("dve", lambda e: e.tensor_tensor(out=PH[:], in0=LI[:], in1=STEP[:], op=ALU.mult), reads=["s5_li", "STEP"], writes=["PH"])
        EXPO = sb("EXPO", [128, 32, 23]); ANG = sb("ANG", [128, 32, 23]); MAG = sb("MAG", [128, 32, 23])
        nvb = bc(NV[:].unsqueeze(1), [128, 32, 23])
        S.op("dve", lambda e: e.tensor_tensor(out=EXPO[:], in0=bc(AA[:].unsqueeze(2), [128, 32, 23]), in1=nvb, op=ALU.mult), reads=["AA", "s5_nv"], writes=["EXPO"])
        S.op("dve", lambda e: e.tensor_tensor(out=ANG[:], in0=bc(PH[:].unsqueeze(2), [128, 32, 23]), in1=nvb, op=ALU.mult), reads=["PH", "s5_nv"], writes=["pwang"])
        S.op("act", lambda e: e.activation(out=MAG[:], in_=EXPO[:], func=AF.Exp), reads=["EXPO"], writes=["MAG"])
        (sn, snk), (cs, csk) = sincos(C, es, ANG[:].rearrange("p g n -> p (g n)"), 32 * 23, "pw")
        CR = sb("CR", [128, 32, 23]); CI = sb("CI", [128, 32, 23])
        S.op("dve", lambda e: e.tensor_tensor(out=CR[:].rearrange("p g n -> p (g n)"), in0=MAG[:].rearrange("p g n -> p (g n)"), in1=cs[:], op=ALU.mult), reads=["MAG", csk], writes=["CR"])
        S.op("dve", lambda e: e.tensor_tensor(out=CI[:].rearrange("p g n -> p (g n)"), in0=MAG[:].rearrange("p g n -> p (g n)"), in1=sn[:], op=ALU.mult), reads=["MAG", snk], writes=["CI"])
        zr = sb("zr", [128, 32]); den = sb("den", [128, 32]); t0 = sb("t0", [128, 32]); fr = sb("fr", [128, 32]); fi = sb("fi", [128, 32])
        S.op("dve", lambda e: e.tensor_scalar(out=zr[:], in0=CR[:, :, 8], scalar1=-1.0, scalar2=None, op0=ALU.add), reads=["CR"], writes=["zr"])
        S.op("dve", lambda e: e.tensor_tensor(out=den[:], in0=LR[:], in1=LR[:], op=ALU.mult), reads=["s5_lr"], writes=["den"])
        S.op("dve", lambda e: e.tensor_tensor(out=t0[:], in0=LI[:], in1=LI[:], op=ALU.mult), reads=["s5_li"], writes=["t0"])
        S.op("dve", lambda e: e.tensor_tensor(out=den[:], in0=den[:], in1=t0[:], op=ALU.add), reads=["den", "t0"], writes=["den"])
        S.op("dve", lambda e: e.reciprocal(out=den[:], in_=den[:]), reads=["den"], writes=["den"])
        S.op("dve", lambda e: e.tensor_tensor(out=fr[:], in0=zr[:], in1=LR[:], op=ALU.mult), reads=["zr", "s5_lr"], writes=["fr"])
        S.op("dve", lambda e: e.tensor_tensor(out=t0[:], in0=CI[:, :, 8], in1=LI[:], op=ALU.mult), reads=["CI", "s5_li", "den"], writes=["t0"])
        S.op("dve", lambda e: e.tensor_tensor(out=fr[:], in0=fr[:], in1=t0[:], op=ALU.add), reads=["fr", "t0"], writes=["fr"])
        S.op("dve", lambda e: e.tensor_tensor(out=fr[:], in0=fr[:], in1=den[:], op=ALU.mult), reads=["fr", "den"], writes=["fr"])
        S.op("dve", lambda e: e.tensor_tensor(out=fi[:], in0=CI[:, :, 8], in1=LR[:], op=ALU.mult), reads=["CI", "s5_lr"], writes=["fi"])
        S.op("dve", lambda e: e.tensor_tensor(out=t0[:], in0=zr[:], in1=LI[:], op=ALU.mult), reads=["zr", "s5_li", "fr"], writes=["t0"])
        S.op("dve", lambda e: e.tensor_tensor(out=fi[:], in0=fi[:], in1=t0[:], op=ALU.subtract), reads=["fi", "t0"], writes=["fi"])
        S.op("dve", lambda e: e.tensor_tensor(out=fi[:], in0=fi[:], in1=den[:], op=ALU.mult), reads=["fi", "den"], writes=["fi"])
        BB1 = sb("BB1", [128, 32, 16]); BB2 = sb("BB2", [128, 32, 16]); ta = sb("ta", [128, 32, 16]); tb = sb("tb", [128, 32, 16])
        frb = bc(fr[:].unsqueeze(2), [128, 32, 16]); fib = bc(fi[:].unsqueeze(2), [128, 32, 16])
        fl = lambda t: t[:].rearrange("p g c -> p (g c)")
        S.op("dve", lambda e: e.tensor_tensor(out=ta[:], in0=P1B[:], in1=frb, op=ALU.mult), reads=["s5_p1b", "fr"], writes=["ta"])
        S.op("dve", lambda e: e.tensor_tensor(out=tb[:], in0=P2B[:], in1=fib, op=ALU.mult), reads=["s5_p2b", "fi"], writes=["tb"])
        S.op("dve", lambda e: e.scalar_tensor_tensor(out=fl(BB1), in0=fl(tb), scalar=SG[:, 0:1], in1=fl(ta), op0=ALU.mult, op1=ALU.add), reads=["ta", "tb", "s5_sg"], writes=["BB1"])
        S.op("dve", lambda e: e.tensor_tensor(out=ta[:], in0=P2B[:], in1=frb, op=ALU.mult), reads=["s5_p2b", "fr", "BB1"], writes=["ta"])
        S.op("dve", lambda e: e.tensor_tensor(out=tb[:], in0=P1B[:], in1=fib, op=ALU.mult), reads=["s5_p1b", "fi", "BB1"], writes=["tb"])
        S.op("dve", lambda e: e.scalar_tensor_tensor(out=fl(BB2), in0=fl(tb), scalar=SG[:, 1:2], in1=fl(ta), op0=ALU.mult, op1=ALU.add), reads=["ta", "tb", "s5_sg"], writes=["BB2"])
        t5 = sb("t5", [128, 32, 16]); t6 = sb("t6", [128, 32, 16])
        Rm = sb("Rm", [128, 32, 8, 16])
        Q1 = sb("Q1", [128, 32, 16]); Q2 = sb("Q2", [128, 32, 16])
        S.op("dve", lambda e: e.tensor_scalar(out=fl(Q1), in0=fl(P1C), scalar1=SG[:, 1:2], scalar2=None, op0=ALU.mult), reads=["s5_p1c", "s5_sg"], writes=["Q1"])
        S.op("dve", lambda e: e.tensor_scalar(out=fl(Q2), in0=fl(P2C), scalar1=SG[:, 0:1], scalar2=None, op0=ALU.mult), reads=["s5_p2c", "s5_sg"], writes=["Q2"])
        CAv = CA[:].rearrange("p g (t c) -> p g t c", c=16); CAsv = CAs[:].rearrange("p g (t c) -> p g t c", c=16)
        for t in range(8):
            for (dst, dkey, a_, akey, b_, bkey, idx) in ((Rm[:, :, t, :], "Rm", Q1, "Q1", P2C, "s5_p2c", 7 + t),
                                                         (CAv[:, :, t, :], "CA", Q1, "Q1", P2C, "s5_p2c", 15 + t),
                                                         (CAsv[:, :, t, :], "CAs", Q2, "Q2", P1C, "s5_p1c", 15 + t)):
                S.op("dve", lambda e, a_=a_, idx=idx: e.tensor_tensor(out=t5[:], in0=a_[:], in1=bc(CR[:, :, idx:idx + 1], [128, 32, 16]), op=ALU.mult), reads=[akey, "CR"], writes=["t5"])
                S.op("dve", lambda e, b_=b_, idx=idx: e.tensor_tensor(out=t6[:], in0=b_[:], in1=bc(CI[:, :, idx:idx + 1], [128, 32, 16]), op=ALU.mult), reads=[bkey, "CI"], writes=["t6"])
                S.op("dve", lambda e, dst=dst: e.tensor_tensor(out=dst, in0=t5[:], in1=t6[:], op=ALU.subtract), reads=["t5", "t6"], writes=[dkey])
        Wr = sb("Wr", [128, 9, 32]); Wi = sb("Wi", [128, 9, 32]); sq = sb("sq", [128, 32])
        S.op("dve", lambda e: e.tensor_copy(out=Wr[:, 0, :], in_=CR[:, :, 15]), reads=["CR"], writes=["Wr"])
        S.op("dve", lambda e: e.tensor_copy(out=Wi[:, 0, :], in_=CI[:, :, 15]), reads=["CI"], writes=["Wi"])
        for j in range(8):
            S.op("dve", lambda e, j=j: e.tensor_tensor(out=Wr[:, j + 1, :], in0=Wr[:, j, :], in1=Wr[:, j, :], op=ALU.mult), reads=["Wr"], writes=["Wr"])
            S.op("dve", lambda e, j=j: e.tensor_tensor(out=sq[:], in0=Wi[:, j, :], in1=Wi[:, j, :], op=ALU.mult), reads=["Wi"], writes=["sq"])
            S.op("dve", lambda e, j=j: e.tensor_tensor(out=Wr[:, j + 1, :], in0=Wr[:, j + 1, :], in1=sq[:], op=ALU.subtract), reads=["Wr", "sq"], writes=["Wr"])
            S.op("dve", lambda e, j=j: e.scalar_tensor_tensor(out=Wi[:, j + 1, :], in0=Wr[:, j, :], scalar=2.0, in1=Wi[:, j, :], op0=ALU.mult, op1=ALU.mult), reads=["Wr", "Wi"], writes=["Wi"])
        Wrv = Wr[:].rearrange("p j (a r w) -> p j a r w", r=2, w=2); Wiv = Wi[:].rearrange("p j (a r w) -> p j a r w", r=2, w=2)
        for r in range(2):
            pr_ = slice(64 * r, 64 * r + 64)
            for j in range(9):
                S.op("dve", lambda e, r=r, pr_=pr_, j=j: e.tensor_copy(out=WPr[pr_, j, :].rearrange("p (a w) -> p a w", w=2), in_=Wrv[pr_, j, :, r, :]), reads=["Wr"], writes=["WPr"])
                S.op("dve", lambda e, r=r, pr_=pr_, j=j: e.tensor_copy(out=WPi[pr_, j, :].rearrange("p (a w) -> p a w", w=2), in_=Wiv[pr_, j, :, r, :]), reads=["Wi"], writes=["WPi"])
        S.op("dve", lambda e: e.tensor_scalar(out=WPn[:].rearrange("p j q -> p (j q)"), in0=WPi[:].rearrange("p j q -> p (j q)"), scalar1=-1.0, scalar2=None, op0=ALU.mult), reads=["WPi"], writes=["WPn"])
        E = [[sb("E%d%d" % (h_, r), [128, 128]) for r in range(2)] for h_ in range(2)]
        for h_ in range(2):
            for r in range(2):
                S.op("pool", lambda e, h_=h_, r=r: e.memset(E[h_][r][:], 0.0), writes=["E%d%d" % (h_, r)])
                S.op("pool", lambda e, h_=h_, r=r: e.tensor_copy(out=E[h_][r][64 * h_:64 * h_ + 64, 64 * r:64 * r + 64], in_=idf[64 * h_:64 * h_ + 64, 64 * h_:64 * h_ + 64]),
                     reads=["pidf", "E%d%d" % (h_, r)], writes=["E%d%d" % (h_, r)])
        Lz = [sb("Lz%d" % i, [128, 32, 64]) for i in range(2)]
        for i in range(2):
            S.op("pool", lambda e, i=i: e.memset(Lz[i][:].rearrange("p g c -> p (g c)"), 0.0), writes=["Lz%d" % i])
        S.op("pool", lambda e: e.memset(Mz2[:].rearrange("p a b c -> p (a b c)"), 0.0), writes=["Mz2"])
        t5v = t5[:].rearrange("p (a j) c -> p a j c", j=4); t6v = t6[:].rearrange("p (a j) c -> p a j c", j=4)
        with scope(C) as esp:
            PM = C.ps(esp, "PM", [128, 16, 128]); PL = C.ps(esp, "PL", [128, 8, 2, 128])
            for s in range(8):
                i = 7 - s; sl = s % 2; lk = "Lz%d" % sl
                Lzv = Lz[sl][:].rearrange("p (a j) c -> p a j c", j=4)
                S.op("dve", lambda e, i=i: e.tensor_tensor(out=t5[:], in0=BB1[:], in1=bc(CR[:, :, i:i + 1], [128, 32, 16]), op=ALU.mult), reads=["BB1", "CR"], writes=["t5"])
                S.op("dve", lambda e, i=i: e.tensor_tensor(out=t6[:], in0=BB2[:], in1=bc(CI[:, :, i:i + 1], [128, 32, 16]), op=ALU.mult), reads=["BB2", "CI"], writes=["t6"])
                for j4 in range(4):
                    S.op("dve", lambda e, j4=j4, Lzv=Lzv: e.scalar_tensor_tensor(out=Lzv[:, :, j4, 16 * j4:16 * j4 + 16], in0=t6v[:, :, j4, :], scalar=SG[:, 0:1], in1=t5v[:, :, j4, :], op0=ALU.mult, op1=ALU.add),
                         reads=["t5", "t6", "s5_sg"], writes=[lk])
                for g in range(32):
                    chc = g // 8; hb = (g % 8) // 4; j4 = g % 4; e_ = chc * 4 + j4
                    rows = slice(64 * hb, 64 * hb + 64)
                    S.op("pe", lambda e, g=g, sl=sl, e_=e_, rows=rows: e.matmul(PM[rows, e_, :], lhsT=Lz[sl][:, g, :], rhs=Rm[:, g, :, :].rearrange("p t c -> p (t c)"), start=True, stop=True),
                         reads=[lk, "Rm"], writes=["PM"])
                for pr in range(16):
                    a_ = pr // 2; wp = pr % 2; chc = a_ // 2; hb = a_ % 2; e2 = chc * 2 + wp
                    rows = slice(64 * hb, 64 * hb + 64)
                    for h_ in range(2):
                        for r in range(2):
                            g = 4 * a_ + 2 * r + wp
                            S.op("pe", lambda e, g=g, sl=sl, e2=e2, rows=rows, h_=h_, r=r: e.matmul(PL[rows, e2, h_, :], lhsT=Lz[sl][:, g, :], rhs=E[h_][r][:], start=(r == 0), stop=(r == 1)),
                                 reads=[lk, "E%d%d" % (h_, r)], writes=["PL"])
                S.op("dve", lambda e, s=s: e.tensor_copy(out=Mz2[:, :, s, 16 * s:128], in_=PM[:, :, 16 * s:128]), reads=["PM"], writes=["Mz2"])
                S.op("dve", lambda e, s=s: e.tensor_tensor(out=Mz2[:, :, s, 16 * s:16 * s + 16], in0=PM[:, :, 16 * s:16 * s + 16], in1=DD[:], op=ALU.add), reads=["PM", "s5_dd"], writes=["Mz2"])
                S.op("act", lambda e, s=s: e.copy(out=LT2[:, :, s, :, :], in_=PL[:]), reads=["PL"], writes=["LT2"])
        S.dma("sp", D["Mz_d"], Mz2[:].rearrange("p a b c -> p (a b c)"), reads=["Mz2"], writes=["Mz_d"])
        S.dma("sp", D["CA_d"][:, 0, :], CA[:].rearrange("p g c -> p (g c)"), reads=["CA"], writes=["CA_d"])
        S.dma("sp", D["CA_d"][:, 1, :], CAs[:].rearrange("p g c -> p (g c)"), reads=["CAs"], writes=["CA_d"])
    return P


def load_weight_bf16(C, es, es_tmp, name, src, rows_chunks, ncols, gcol=None, q="sp"):
    S = C.S
    W = C.sb(es, name, [128, rows_chunks, ncols], BF16)
    stg = [C.sb(es_tmp, name + "_stg%d" % i, [128, ncols]) for i in range(2)]
    srcv = src.rearrange("(c p) n -> c p n", p=128)
    for c in range(rows_chunks):
        st = stg[c % 2]; sk = name + "_stg%d" % (c % 2)
        S.dma(q, st[:], srcv[c], writes=[sk])
        eng = "dve" if c % 2 == 0 else "pool"
        if gcol is not None:
            S.op(eng, lambda e, st=st, c=c: e.tensor_scalar(out=W[:, c, :], in0=st[:], scalar1=gcol[0][:, c:c + 1], scalar2=None, op0=ALU.mult),
                 reads=[sk, gcol[1]], writes=[name])
        else:
            S.op(eng, lambda e, st=st, c=c: e.tensor_copy(out=W[:, c, :], in_=st[:]), reads=[sk], writes=[name])
    return W


def rms_rstd(C, x_ap, xkey, junk, jkey, ss, rs, skey, n):
    S = C.S
    S.op("act", lambda e: e.activation(out=junk, in_=x_ap, func=AF.Square, accum_out=ss), reads=[xkey], writes=[skey + "_ss"])
    S.op("act", lambda e: e.activation(out=rs, in_=ss, func=AF.Sqrt, scale=1.0 / n, bias=EPS), reads=[skey + "_ss"], writes=[skey + "_sq"])
    S.op("dve", lambda e: e.reciprocal(out=rs, in_=rs), reads=[skey + "_sq"], writes=[skey])


def transpose_chunks(C, src, skey, nch, pbank, pkey, dst, dkey, idb, evac="act"):
    S = C.S
    for c in range(nch):
        S.op("pe", lambda e, c=c: e.transpose(out=pbank[:, c, :], in_=src[:, c * 128:(c + 1) * 128], identity=idb[:]), reads=[skey, "identb"], writes=[pkey])
    if evac == "act":
        S.op("act", lambda e: e.copy(out=dst, in_=pbank), reads=[pkey], writes=[dkey])
    else:
        S.op(evac, lambda e: e.tensor_copy(out=dst, in_=pbank), reads=[pkey], writes=[dkey])


def stage_mixer(C, D, dbg=None, upto=9, prep=None):
    S, nc = C.S, C.nc
    dbg = dbg or {}
    with scope(C) as es1:
        P = s5_params(C, es1, D)
        idf = C.sb(es1, "identf", [128, 128]); idb = C.sb(es1, "identb", [128, 128], BF16)
        S.op("pool", lambda e: e.memset(idf[:], 1.0), writes=["identf"])
        S.op("pool", lambda e: e.affine_select(out=idf[:], in_=idf[:], pattern=[[-1, 128]], compare_op=ALU.is_equal, fill=0.0, base=0, channel_multiplier=1), reads=["identf"], writes=["identf"])
        S.op("dve", lambda e: e.tensor_copy(out=idb[:], in_=idf[:]), reads=["identf"], writes=["identb"])
        if "WPr" in dbg:
            for nm in ("WPr", "WPi"):
                S.dma("sp", dbg[nm], P[nm][:].rearrange("p a b -> p (a b)"), reads=[nm], writes=["o_" + nm])
            S.dma("pool", dbg["LT2"], P["LT2"][:].rearrange("p a b c d -> p (a b c d)"), reads=["LT2"], writes=["o_LT2"])
            S.dma("pool", dbg["Mz"], D["Mz_d"], reads=["Mz_d"], writes=["o_Mz"])
            S.dma("pool", dbg["CA"], D["CA_d"].rearrange("p a b -> p (a b)"), reads=["CA_d"], writes=["o_CA"])
        if upto < 1:
            return
        carry_r = C.sb(es1, "carry_r", [128, 16]); carry_i = C.sb(es1, "carry_i", [128, 16])
        S.op("pool", lambda e: e.memset(carry_r[:], 0.0), writes=["carry_r"])
        S.op("pool", lambda e: e.memset(carry_i[:], 0.0), writes=["carry_i"])
        TRE = C.sb(es1, "TRE", [128, 16, 257]); TIM = C.sb(es1, "TIM", [128, 16, 257])
        uT = C.sb(es1, "uT", [128, 4, 8, 256], BF16)
        with scope(C) as es2:
            _mixer_passes(C, es2, D, P, idb, carry_r, carry_i, TRE, TIM, uT, dbg)
        if "carry" in dbg:
            S.dma("sp", dbg["carry"][:, 0:16], carry_r[:], reads=["carry_r"], writes=["o_carry"])
            S.dma("sp", dbg["carry"][:, 16:32], carry_i[:], reads=["carry_i"], writes=["o_carry2"])
        if upto < 2:
            return
        with scope(C) as es3:
            ytm = C.sb(es3, "ytm", [128, 2, 8, 512], BF16)
            with scope(C) as es4:
                _s5_scan_out(C, es4, D, P, idb, TRE, TIM, uT, ytm, carry_r, carry_i, dbg)
            if upto < 3:
                return
            _s5_glu_out(C, es3, D, idb, ytm, dbg)
    if upto < 4:
        return
    with scope(C) as es5:
        idf = C.sb(es5, "identf", [128, 128]); idb = C.sb(es5, "identb", [128, 128], BF16)
        S.op("pool", lambda e: e.memset(idf[:], 1.0), reads=[], writes=["identf"])
        S.op("pool", lambda e: e.affine_select(out=idf[:], in_=idf[:], pattern=[[-1, 128]], compare_op=ALU.is_equal, fill=0.0, base=0, channel_multiplier=1), reads=["identf"], writes=["identf"])
        S.op("dve", lambda e: e.tensor_copy(out=idb[:], in_=idf[:]), reads=["identf"], writes=["identb"])
        _attention(C, es5, D, idb, dbg, prep)


def _mixer_passes(C, es, D, P, idb, carry_r, carry_i, TRE, TIM, uT, dbg):
    S = C.S
    sb = lambda name, shape, dt=F32: C.sb(es, name, shape, dt)
    gin = sb("gin", [128, 8])
    S.dma("sp", gin[:], D["g_mix"], writes=["gin"])
    with scope(C) as est:
        Wb = load_weight_bf16(C, es, est, "Wb", D["w_in"], 8, 2048, gcol=(gin, "gin"))
    gq = sb("gq", [128, 64]); gk = sb("gk", [128, 64]); hv = sb("hv", [128, 4])
    S.dma("sp", gq[:], D["att_q_g"].partition_broadcast(128), writes=["gq"])
    S.dma("sp", gk[:], D["att_k_g"].partition_broadcast(128), writes=["gk"])
    S.dma("sp", hv[:], D["hvalid"], writes=["hv"])
    S.op("dve", lambda e: e.tensor_scalar(out=gq[:], in0=gq[:], scalar1=0.125, scalar2=None, op0=ALU.mult), reads=["gq"], writes=["gq"])
    xt = [sb("xt%d" % i, [128, 1024]) for i in range(2)]
    junk = sb("junk", [128, 1024]); st = [sb("st%d" % i, [128, 4]) for i in range(2)]
    xn = [sb("xn%d" % i, [128, 1024], BF16) for i in range(2)]
    xnT = [sb("xnT%d" % i, [128, 8, 512], BF16) for i in range(2)]
    qkv = sb("qkv", [128, 3, 512]); sq = sb("sq2", [128, 512]); qst = sb("qst", [128, 4, 8])
    qn = sb("qn", [128, 2, 512], BF16)
    kTs = [sb("kTs%d" % i, [128, 4, 128], BF16) for i in range(2)]; qTs = [sb("qTs%d" % i, [128, 4, 128], BF16) for i in range(2)]
    Vs = [sb("Vs%d" % i, [128, 8, 65], BF16) for i in range(2)]
    tBr = sb("tBr", [128, 8, 128]); tBi = sb("tBi", [128, 8, 128]); tCr = sb("tCr", [128, 8, 64]); tCi = sb("tCi", [128, 8, 64])
    tt1 = sb("tt1", [128, 8, 128]); tt2 = sb("tt2", [128, 8, 128]); tt3 = sb("tt3", [128, 8, 128]); tt4 = sb("tt4", [128, 8, 128])
    REDr = sb("REDr", [128, 16]); REDi = sb("REDi", [128, 16]); c1 = sb("c1", [128, 16]); c2 = sb("c2", [128, 16]); c3 = sb("c3", [128, 16])
    with scope(C) as esp:
        bank = [C.ps(esp, "bk%d" % i, [128, 512]) for i in range(8)]
        xv = D["x_ext"].rearrange("(n p) d -> n p d", p=128)
        tile_ctr = 0
        for q in range(4):
            for blk in range(4):
                bslot = (q * 4 + blk) % 2
                xT = xnT[bslot]; xTk = "xnT%d" % bslot
                for tt in range(4):
                    n_tile = q * 16 + blk * 4 + tt
                    sl = tile_ctr % 2; tile_ctr += 1
                    x_ = xt[sl]; xk = "xt%d" % sl
                    S.dma("sp", x_[:], xv[n_tile], writes=[xk])
                    rms_rstd(C, x_[:], xk, junk[:], "junk", st[sl][:, 0:1], st[sl][:, 1:2], "st%d" % sl, 1024)
                    S.op("dve", lambda e, x_=x_, sl=sl: e.tensor_scalar(out=xn[sl][:], in0=x_[:], scalar1=st[sl][:, 1:2], scalar2=None, op0=ALU.mult),
                         reads=[xk, "st%d" % sl], writes=["xn%d" % sl])
                    tb_ = 0 if sl == 0 else 7
                    pb = bank[tb_][:].bitcast(BF16).rearrange("p (c n) -> p c n", n=128)
                    transpose_chunks(C, xn[sl][:], "xn%d" % sl, 8, pb, "bk%d" % tb_, xT[:, :, tt * 128:(tt + 1) * 128], xTk, idb, evac=("act" if sl == 0 else "dve"))
                for chc in range(4):
                    bi = 1 + (chc % 2); bkk = "bk%d" % bi
                    for dc in range(8):
                        S.op("pe", lambda e, chc=chc, dc=dc, bi=bi, xT=xT: e.matmul(bank[bi][:], lhsT=Wb[:, dc, 1536 + chc * 128:1536 + (chc + 1) * 128], rhs=xT[:, dc, :], start=(dc == 0), stop=(dc == 7)),
                             reads=["Wb", xTk], writes=[bkk])
                    eng = "act" if chc % 2 == 0 else "dve"
                    src = bank[bi][:].rearrange("p (k s) -> p s k", s=8)
                    dst = uT[:, chc, :, blk * 64:(blk + 1) * 64]
                    if eng == "act":
                        S.op("act", lambda e, src=src, dst=dst: e.copy(out=dst, in_=src), reads=[bkk], writes=["uT"])
                    else:
                        S.op("dve", lambda e, src=src, dst=dst: e.tensor_copy(out=dst, in_=src), reads=[bkk], writes=["uT"])
                need_kv = (q == 3) or (q == 2 and blk == 3)
                need_q = (q == 3)
                if need_kv:
                    for tt in range(4):
                        n_tile = q * 16 + blk * 4 + tt
                        kvt = n_tile - 44
                        sl = kvt % 2
                        projs = [(1, 512, 3), (2, 1024, 4)] + ([(0, 0, 5)] if need_q else [])
                        for (pi, c0, bi) in projs:
                            for dc in range(8):
                                S.op("pe", lambda e, dc=dc, bi=bi, c0=c0, tt=tt, xT=xT: e.matmul(bank[bi][:], lhsT=xT[:, dc, tt * 128:(tt + 1) * 128], rhs=Wb[:, dc, c0:c0 + 512], start=(dc == 0), stop=(dc == 7)),
                                     reads=["Wb", xTk], writes=["bk%d" % bi])
                        S.op("act", lambda e, sl=sl: e.copy(out=Vs[sl][:, :, 0:64], in_=bank[4][:].rearrange("p (h d) -> p h d", d=64)), reads=["bk4"], writes=["Vs%d" % sl])
                        if kvt < 4:
                            S.op("pool", lambda e, sl=sl, kvt=kvt: e.tensor_copy(out=Vs[sl][:, :, 64], in_=bc(hv[:, kvt:kvt + 1], [128, 8])), reads=["hv"], writes=["Vs%d" % sl])
                        else:
                            S.op("pool", lambda e, sl=sl: e.memset(Vs[sl][:, :, 64], 1.0), reads=[], writes=["Vs%d" % sl])
                        S.dma("sp", D["V_d"][kvt], Vs[sl][:].rearrange("p h d -> p (h d)"), reads=["Vs%d" % sl], writes=["V_d"])
                        for (pi, bi, gt, gkey, dstT, dkey, dram, ncol_t) in ([(1, 3, gk, "gk", kTs[sl], "kTs%d" % sl, D["kT_d"], kvt)] +
                                                                          ([(0, 5, gq, "gq", qTs[sl], "qTs%d" % sl, D["qT_d"], kvt - 4)] if need_q else [])):
                            qs = qkv[:, pi, :]; qk_ = "qkv%d" % pi
                            S.op("act", lambda e, qs=qs, bi=bi: e.copy(out=qs, in_=bank[bi][:]), reads=["bk%d" % bi], writes=[qk_])
                            S.op("pool", lambda e, qs=qs: e.tensor_tensor(out=sq[:], in0=qs, in1=qs, op=ALU.mult), reads=[qk_], writes=["sq2"])
                            S.op("dve", lambda e, pi=pi: e.tensor_reduce(out=qst[:, pi, :], in_=sq[:].rearrange("p (h d) -> p h d", d=64), axis=AX.X, op=ALU.add), reads=["sq2"], writes=["qst%d" % pi])
                            S.op("act", lambda e, pi=pi: e.activation(out=qst[:, 2 + pi, :], in_=qst[:, pi, :], func=AF.Sqrt, scale=1.0 / 64, bias=EPS), reads=["qst%d" % pi], writes=["qsq%d" % pi])
                            S.op("dve", lambda e, pi=pi: e.reciprocal(out=qst[:, 2 + pi, :], in_=qst[:, 2 + pi, :]), reads=["qsq%d" % pi], writes=["qrs%d" % pi])
                            S.op("dve", lambda e, qs=qs, pi=pi: e.tensor_tensor(out=qs.rearrange("p (h d) -> p h d", d=64), in0=qs.rearrange("p (h d) -> p h d", d=64),
                                                                        in1=bc(qst[:, 2 + pi, :].unsqueeze(2), [128, 8, 64]), op=ALU.mult), reads=[qk_, "qrs%d" % pi], writes=[qk_])
                            S.op("pool", lambda e, qs=qs, pi=pi, gt=gt: e.tensor_tensor(out=qn[:, pi, :].rearrange("p (h d) -> p h d", d=64), in0=qs.rearrange("p (h d) -> p h d", d=64),
                                                                               in1=bc(gt[:].unsqueeze(1), [128, 8, 64]), op=ALU.mult), reads=[qk_, gkey], writes=["qn%d" % pi])
                            pb = bank[6][:].bitcast(BF16).rearrange("p (c n) -> p c n", n=128)[:, 0:4, :]
                            transpose_chunks(C, qn[:, pi, :], "qn%d" % pi, 4, pb, "bk6", dstT[:], dkey, idb)
                            S.dma("sp", dram.rearrange("p (c n) -> p c n", c=4)[:, :, ncol_t * 128:(ncol_t + 1) * 128], dstT[:], reads=[dkey], writes=["qkT_d"])
            for pair in range(16):
                psl = pair % 2
                br = bank[1 + 2 * psl]; bim = bank[2 + 2 * psl]; brk = "bk%d" % (1 + 2 * psl); bik = "bk%d" % (2 + 2 * psl)
                a_ = pair // 2; wp = pair % 2; chc = a_ // 2; hb = a_ % 2; e2 = chc * 2 + wp
                rows = slice(64 * hb, 64 * hb + 64)
                for half, (bkt, bkk) in enumerate(((br, brk), (bim, bik))):
                    for s in range(8):
                        S.op("pe", lambda e, e2=e2, s=s, half=half, rows=rows, chc=chc, bkt=bkt: e.matmul(bkt[:, 0:256], lhsT=P["LT2"][rows, e2, s, half, :], rhs=uT[rows, chc, s, :], start=(s == 0), stop=(s == 7)),
                             reads=["LT2", "uT"], writes=[bkk])
                S.op("act", lambda e, pair=pair, br=br: e.copy(out=TRE[:, pair, 1:257], in_=br[:, 0:256]), reads=[brk], writes=["TRE%d" % pair])
                S.op("act", lambda e, pair=pair, bim=bim: e.copy(out=TIM[:, pair, 1:257], in_=bim[:, 0:256]), reads=[bik], writes=["TIM%d" % pair])
            if q < 3:
                for p0 in (0, 8):
                    kre = ["TRE%d" % p for p in range(p0, p0 + 8)]; kim = ["TIM%d" % p for p in range(p0, p0 + 8)]
                    src_r, src_i, srk, sik = TRE[:, p0:p0 + 8, 1:257], TIM[:, p0:p0 + 8, 1:257], kre, kim
                    bufs = [(tBr[:], tBi[:], ["tBr"], ["tBi"]), (tCr[:], tCi[:], ["tCr"], ["tCi"])]
                    for j in range(8):
                        n = 256 >> j; h = n // 2
                        wrb = bc(P["WPr"][:, j, p0:p0 + 8].unsqueeze(2), [128, 8, h]); wib = bc(P["WPi"][:, j, p0:p0 + 8].unsqueeze(2), [128, 8, h])
                        sre, sro = src_r[:, :, 0:n:2], src_r[:, :, 1:n:2]; sie, sio = src_i[:, :, 0:n:2], src_i[:, :, 1:n:2]
                        if j == 7:
                            dr, di, drk, dik = REDr[:, p0:p0 + 8].unsqueeze(2), REDi[:, p0:p0 + 8].unsqueeze(2), ["REDr%d" % p0], ["REDi%d" % p0]
                        else:
                            bb = bufs[j % 2]
                            dr, di, drk, dik = bb[0][:, :, 0:h], bb[1][:, :, 0:h], bb[2], bb[3]
                        t1v, t2v, t3v, t4v = tt1[:, :, 0:h], tt2[:, :, 0:h], tt3[:, :, 0:h], tt4[:, :, 0:h]
                        S.op("dve", lambda e, t1v=t1v, sre=sre, wrb=wrb: e.tensor_tensor(out=t1v, in0=sre, in1=wrb, op=ALU.mult), reads=srk + ["WPr"], writes=["tt1"])
                        S.op("pool", lambda e, t2v=t2v, sie=sie, wib=wib: e.tensor_tensor(out=t2v, in0=sie, in1=wib, op=ALU.mult), reads=sik + ["WPi"], writes=["tt2"])
                        S.op("pool", lambda e, t3v=t3v, sie=sie, wrb=wrb: e.tensor_tensor(out=t3v, in0=sie, in1=wrb, op=ALU.mult), reads=sik + ["WPr"], writes=["tt3"])
                        S.op("dve", lambda e, t4v=t4v, sre=sre, wib=wib: e.tensor_tensor(out=t4v, in0=sre, in1=wib, op=ALU.mult), reads=srk + ["WPi"], writes=["tt4"])
                        S.op("dve", lambda e, t1v=t1v, t2v=t2v: e.tensor_tensor(out=t1v, in0=t1v, in1=t2v, op=ALU.subtract), reads=["tt1", "tt2"], writes=["tt1"])
                        S.op("pool", lambda e, t3v=t3v, t4v=t4v: e.tensor_tensor(out=t3v, in0=t3v, in1=t4v, op=ALU.add), reads=["tt3", "tt4"], writes=["tt3"])
                        S.op("dve", lambda e, dr=dr, t1v=t1v, sro=sro: e.tensor_tensor(out=dr, in0=t1v, in1=sro, op=ALU.add), reads=["tt1"] + srk, writes=drk)
                        S.op("pool", lambda e, di=di, t3v=t3v, sio=sio: e.tensor_tensor(out=di, in0=t3v, in1=sio, op=ALU.add), reads=["tt3"] + sik, writes=dik)
                        if j < 7:
                            src_r, src_i, srk, sik = bb[0], bb[1], bb[2], bb[3]
            if q < 3:
                w8r = P["WPr"][:, 8, :]; w8i = P["WPi"][:, 8, :]
                S.op("dve", lambda e: e.tensor_tensor(out=c1[:], in0=w8r, in1=carry_r[:], op=ALU.mult), reads=["WPr", "carry_r"], writes=["c1"])
                S.op("dve", lambda e: e.tensor_tensor(out=c2[:], in0=w8i, in1=carry_i[:], op=ALU.mult), reads=["WPi", "carry_i"], writes=["c2"])
                S.op("dve", lambda e: e.tensor_tensor(out=c1[:], in0=c1[:], in1=c2[:], op=ALU.subtract), reads=["c1", "c2"], writes=["c1"])
                S.op("dve", lambda e: e.tensor_tensor(out=c1[:], in0=c1[:], in1=REDr[:], op=ALU.add), reads=["c1", "REDr0", "REDr8"], writes=["c1"])
                S.op("dve", lambda e: e.tensor_tensor(out=c2[:], in0=w8r, in1=carry_i[:], op=ALU.mult), reads=["WPr", "carry_i", "c1"], writes=["c2"])
                S.op("dve", lambda e: e.tensor_tensor(out=c3[:], in0=w8i, in1=carry_r[:], op=ALU.mult), reads=["WPi", "carry_r"], writes=["c3"])
                S.op("dve", lambda e: e.tensor_tensor(out=c2[:], in0=c2[:], in1=c3[:], op=ALU.add), reads=["c2", "c3"], writes=["c2"])
                S.op("dve", lambda e: e.tensor_tensor(out=carry_i[:], in0=c2[:], in1=REDi[:], op=ALU.add), reads=["c2", "REDi0", "REDi8"], writes=["carry_i"])
                S.op("dve", lambda e: e.tensor_copy(out=carry_r[:], in_=c1[:]), reads=["c1"], writes=["carry_r"])


def _s5_scan_out(C, es, D, P, idb, TRE, TIM, uT, ytm, carry_r, carry_i, dbg):
    S = C.S
    sb = lambda name, shape, dt=F32: C.sb(es, name, shape, dt)
    Mz2 = sb("Mz2s", [128, 16, 8, 128], BF16); CAA = sb("CAA", [128, 2, 32, 128], BF16)
    S.dma("sp", Mz2[:].rearrange("p a b c -> p (a b c)"), D["Mz_d"], reads=["Mz_d"], writes=["Mz2s"])
    S.dma("sp", CAA[:].rearrange("p a g c -> p a (g c)"), D["CA_d"], reads=["CA_d"], writes=["CAA"])
    hs1 = sb("hs1", [128, 8, 256]); hs2 = sb("hs2", [128, 8, 256]); hs3 = sb("hs3", [128, 8, 256]); hs4 = sb("hs4", [128, 8, 256])
    Tbr = [sb("Tbr%d" % i, [128, 256], BF16) for i in range(2)]; Tbi = [sb("Tbi%d" % i, [128, 256], BF16) for i in range(2)]
    Yg = [sb("Yg%d" % i, [128, 256], BF16) for i in range(2)]
    ysum = [sb("ysum%d" % i, [128, 256]) for i in range(2)]
    import os
    CUT = int(os.environ.get("SCAN_CUT", "9"))
    for p0 in (0, 8):
        kre = ["TRE%d" % p for p in range(p0, p0 + 8)]; kim = ["TIM%d" % p for p in range(p0, p0 + 8)]
        S.op("dve", lambda e, p0=p0: e.tensor_copy(out=TRE[:, p0:p0 + 8, 0:1], in_=carry_r[:, p0:p0 + 8].unsqueeze(2)), reads=["carry_r"], writes=kre)
        S.op("pool", lambda e, p0=p0: e.tensor_copy(out=TIM[:, p0:p0 + 8, 0:1], in_=carry_i[:, p0:p0 + 8].unsqueeze(2)), reads=["carry_i"], writes=kim)
        for j in range(9):
            d = 1 << j; m = 257 - d
            wrb = bc(P["WPr"][:, j, p0:p0 + 8].unsqueeze(2), [128, 8, m]); wib = bc(P["WPi"][:, j, p0:p0 + 8].unsqueeze(2), [128, 8, m])
            R0 = TRE[:, p0:p0 + 8, 0:m]; I0 = TIM[:, p0:p0 + 8, 0:m]; R1 = TRE[:, p0:p0 + 8, d:257]; I1 = TIM[:, p0:p0 + 8, d:257]
            h1, h2, h3, h4 = hs1[:, :, 0:m], hs2[:, :, 0:m], hs3[:, :, 0:m], hs4[:, :, 0:m]
            S.op("dve", lambda e, h1=h1, R0=R0, wrb=wrb: e.tensor_tensor(out=h1, in0=R0, in1=wrb, op=ALU.mult), reads=kre + ["WPr"], writes=["hs1"])
            S.op("pool", lambda e, h2=h2, I0=I0, wib=wib: e.tensor_tensor(out=h2, in0=I0, in1=wib, op=ALU.mult), reads=kim + ["WPi"], writes=["hs2"])
            S.op("pool", lambda e, h3=h3, I0=I0, wrb=wrb: e.tensor_tensor(out=h3, in0=I0, in1=wrb, op=ALU.mult), reads=kim + ["WPr"], writes=["hs3"])
            S.op("dve", lambda e, h4=h4, R0=R0, wib=wib: e.tensor_tensor(out=h4, in0=R0, in1=wib, op=ALU.mult), reads=kre + ["WPi"], writes=["hs4"])
            S.op("dve", lambda e, h1=h1, h2=h2: e.tensor_tensor(out=h1, in0=h1, in1=h2, op=ALU.subtract), reads=["hs1", "hs2"], writes=["hs1"])
            S.op("pool", lambda e, h3=h3, h4=h4: e.tensor_tensor(out=h3, in0=h3, in1=h4, op=ALU.add), reads=["hs3", "hs4"], writes=["hs3"])
            S.op("dve", lambda e, R1=R1, h1=h1: e.tensor_tensor(out=R1, in0=R1, in1=h1, op=ALU.add), reads=kre + ["hs1"], writes=kre)
            S.op("pool", lambda e, I1=I1, h3=h3: e.tensor_tensor(out=I1, in0=I1, in1=h3, op=ALU.add), reads=kim + ["hs3"], writes=kim)
    with scope(C) as esp:
        bank = [C.ps(esp, "sbk%d" % i, [128, 512]) for i in range(6)]
        for pair in range(16):
            sl = pair % 2
            kr, ki = "TRE%d" % pair, "TIM%d" % pair
            a_ = pair // 2; wp = pair % 2; chc = a_ // 2; hb = a_ % 2
            rows = slice(64 * hb, 64 * hb + 64)
            kr, ki = "TRE%d" % pair, "TIM%d" % pair
            if CUT < 2:
                continue
            S.op("act", lambda e, pair=pair, sl=sl: e.copy(out=Tbr[sl][:], in_=TRE[:, pair, 0:256]), reads=[kr], writes=["Tbr%d" % sl])
            S.op("act", lambda e, pair=pair, sl=sl: e.copy(out=Tbi[sl][:], in_=TIM[:, pair, 0:256]), reads=[ki], writes=["Tbi%d" % sl])
            for r in range(2):
                g = 4 * a_ + 2 * r + wp
                e_ = chc * 4 + (g % 4)
                pr = slice(64 * r, 64 * r + 64)
                yb = bank[r]; ybk = "sbk%d" % r
                for s in range(8):
                    S.op("pe", lambda e, e_=e_, s=s, rows=rows, chc=chc, yb=yb: e.matmul(yb[:, 0:256], lhsT=Mz2[rows, e_, s, :], rhs=uT[rows, chc, s, :], start=(s == 0), stop=(s == 7)),
                         reads=["Mz2s", "uT"], writes=[ybk])
                zb_ = bank[4 + r]; zbk = "sbk%d" % (4 + r)
                S.op("pe", lambda e, g=g, pr=pr, zb_=zb_, r=r, sl=sl: e.matmul(zb_[:, 0:256], lhsT=CAA[pr, r, g, :], rhs=Tbr[sl][pr, :], start=True, stop=False), reads=["CAA", "Tbr%d" % sl], writes=[zbk])
                S.op("pe", lambda e, g=g, pr=pr, zb_=zb_, r=r, sl=sl: e.matmul(zb_[:, 0:256], lhsT=CAA[pr, 1 - r, g, :], rhs=Tbi[sl][pr, :], start=False, stop=True), reads=["CAA", "Tbi%d" % sl], writes=[zbk])
                if CUT < 3:
                    continue
                S.op("act", lambda e, r=r, zb_=zb_: e.copy(out=ysum[r][:], in_=zb_[:, 0:256]), reads=[zbk], writes=["ysum%d" % r])
                S.op("dve", lambda e, r=r, yb=yb: e.tensor_tensor(out=ysum[r][:], in0=yb[:, 0:256], in1=ysum[r][:], op=ALU.add), reads=[ybk, "ysum%d" % r], writes=["ysum%d" % r])
                S.op("act", lambda e, r=r: e.activation(out=Yg[r][:], in_=ysum[r][:], func=AF.Gelu), reads=["ysum%d" % r], writes=["Yg%d" % r])
                if CUT < 4:
                    continue
                pT = bank[2 + r][:].bitcast(BF16).rearrange("p (c n) -> p c n", n=128)
                for kb in range(2):
                    S.op("pe", lambda e, r=r, kb=kb, pT=pT: e.transpose(out=pT[:, kb, :], in_=Yg[r][:, kb * 128:(kb + 1) * 128], identity=idb[:]), reads=["Yg%d" % r, "identb"], writes=["sbk%d" % (2 + r)])
                for kb in range(2):
                    S.op("dve", lambda e, g=g, kb=kb, pT=pT: e.tensor_copy(out=ytm[:, kb, :, 16 * g:16 * g + 16], in_=pT[:, kb, :].rearrange("p (t c) -> p t c", c=16)),
                         reads=["sbk%d" % (2 + r)], writes=["ytm"])


def _s5_glu_out(C, es, D, idb, ytm, dbg):
    S = C.S
    sb = lambda name, shape, dt=F32: C.sb(es, name, shape, dt)
    gso = sb("gso", [128, 4]); bgl = sb("bgl", [128, 512])
    S.dma("sp", gso[:], D["g_ssm_out"], writes=["gso"])
    S.dma("sp", bgl[:], D["b_glu"].partition_broadcast(128), writes=["bgl"])
    with scope(C) as est:
        Wg = load_weight_bf16(C, es, est, "Wg", D["w_glu"], 4, 512)
    with scope(C) as est:
        Wo = load_weight_bf16(C, es, est, "Wos", D["w_out"][512:1024, :], 4, 1024, gcol=(gso, "gso"))
    yT = sb("yT", [128, 4, 128], BF16); zb = sb("zb", [128, 512]); ssm = sb("ssm", [128, 512]); junk = sb("junk3", [128, 512])
    st = sb("st3", [128, 2]); sn = sb("sn", [128, 512], BF16); snT = sb("snT", [128, 4, 128], BF16)
    ho = [sb("ho%d" % i, [128, 1024]) for i in range(2)]
    hsv = D["hs_d"].rearrange("(k t) d -> t k d", t=8)
    with scope(C) as esp:
        bank = [C.ps(esp, "gbk%d" % i, [128, 512]) for i in range(5)]
        it = 0
        for kb in range(2):
            for t in range(8):
                sl = it % 2; it += 1
                y = ytm[:, kb, t, :]
                pT = bank[0][:].bitcast(BF16).rearrange("p (c n) -> p c n", n=128)[:, 0:4, :]
                transpose_chunks(C, y, "ytm", 4, pT, "gbk0", yT[:], "yT", idb)
                for c in range(4):
                    S.op("pe", lambda e, c=c: e.matmul(bank[1][:], lhsT=yT[:, c, :], rhs=Wg[:, c, :], start=(c == 0), stop=(c == 3)), reads=["yT", "Wg"], writes=["gbk1"])
                S.op("dve", lambda e: e.tensor_tensor(out=zb[:], in0=bank[1][:], in1=bgl[:], op=ALU.add), reads=["gbk1", "bgl"], writes=["zb"])
                S.op("act", lambda e: e.activation(out=zb[:], in_=zb[:], func=AF.Sigmoid), reads=["zb"], writes=["zb"])
                S.op("pool", lambda e, y=y: e.tensor_tensor(out=ssm[:], in0=y, in1=zb[:], op=ALU.mult), reads=["ytm", "zb"], writes=["ssm"])
                if "ssm" in dbg:
                    S.dma("sp", dbg["ssm"].rearrange("(k t) d -> t k d", t=8)[t, kb * 128:(kb + 1) * 128, :], ssm[:], reads=["ssm"], writes=["o_ssm"])
                rms_rstd(C, ssm[:], "ssm", junk[:], "junk3", st[:, 0:1], st[:, 1:2], "st3", 512)
                S.op("dve", lambda e: e.tensor_scalar(out=sn[:], in0=ssm[:], scalar1=st[:, 1:2], scalar2=None, op0=ALU.mult), reads=["ssm", "st3"], writes=["sn"])
                pT2 = bank[2][:].bitcast(BF16).rearrange("p (c n) -> p c n", n=128)[:, 0:4, :]
                transpose_chunks(C, sn[:], "sn", 4, pT2, "gbk2", snT[:], "snT", idb)
                for cb in range(2):
                    for c in range(4):
                        S.op("pe", lambda e, c=c, cb=cb: e.matmul(bank[3 + cb][:], lhsT=snT[:, c, :], rhs=Wo[:, c, cb * 512:(cb + 1) * 512], start=(c == 0), stop=(c == 3)), reads=["snT", "Wos"], writes=["gbk%d" % (3 + cb)])
                    S.op("act", lambda e, cb=cb, sl=sl: e.copy(out=ho[sl][:, cb * 512:(cb + 1) * 512], in_=bank[3 + cb][:]), reads=["gbk%d" % (3 + cb)], writes=["ho%d" % sl])
                S.dma("sp", hsv[t, kb * 128:(kb + 1) * 128, :], ho[sl][:], reads=["ho%d" % sl], writes=["hs_d"])


def _attention(C, es, D, idb, dbg, prep=None):
    S = C.S
    sb = lambda name, shape, dt=F32: C.sb(es, name, shape, dt)
    kT = sb("kT", [128, 4, 2560], BF16); qT = sb("qT", [128, 4, 2048], BF16); V = sb("Vall", [128, 20, 520], BF16)
    qZ = sb("qZ", [128, 8, 2048], BF16)
    S.dma("sp", kT[:].rearrange("p c n -> p (c n)"), D["kT_d"], reads=["qkT_d"], writes=["kT"])
    S.dma("sp", qT[:].rearrange("p c n -> p (c n)"), D["qT_d"], reads=["qkT_d"], writes=["qT"])
    S.dma("sp", V[:], D["V_d"].rearrange("t p n -> p t n"), reads=["V_d"], writes=["Vall"])
    S.op("pool", lambda e: e.memset(qZ[:].rearrange("p h n -> p (h n)"), 0.0), writes=["qZ"])
    for h in range(8):
        rws = slice(64 * (h % 2), 64 * (h % 2) + 64)
        S.op("dve" if h % 2 else "act", (lambda e, h=h, rws=rws: e.tensor_copy(out=qZ[rws, h, :], in_=qT[rws, h // 2, :])) if h % 2 else (lambda e, h=h, rws=rws: e.copy(out=qZ[rws, h, :], in_=qT[rws, h // 2, :])),
             reads=["qT", "qZ"], writes=["qZ"])
    BT = sb("BT", [128, 8, 5, 128], BF16)
    gao = sb("gao", [128, 4])
    S.dma("sp", gao[:], D["g_att_out"], writes=["gao"])
    with scope(C) as est:
        stg = C.sb(est, "btstg", [128, 640])
        for h in range(8):
            S.dma("sp", stg[:], D["bias_t"][:, h * 640:(h + 1) * 640], writes=["btstg"])
            S.op("dve", lambda e, h=h: e.tensor_copy(out=BT[:, h, :, :].rearrange("p j q -> p (j q)"), in_=stg[:]), reads=["btstg"], writes=["BT"])
    with scope(C) as est:
        Wo = load_weight_bf16(C, es, est, "Woa", D["w_out"][0:512, :], 4, 1024, gcol=(gao, "gao"))
    PT = [sb("PT%d" % i, [128, 5, 128], BF16) for i in range(2)]
    rd = sb("rd", [128, 8]); att = sb("att", [128, 8, 64]); junk = sb("junk4", [128, 512]); st = sb("st4", [128, 2])
    an = sb("an", [128, 512], BF16); anT = sb("anT", [128, 4, 128], BF16)
    xo = [sb("xo%d" % i, [128, 1024]) for i in range(2)]; hsl = [sb("hsl%d" % i, [128, 1024]) for i in range(2)]
    h1t = [sb("h1t%d" % i, [128, 1024]) for i in range(2)]
    xv = D["x_ext"].rearrange("(n p) d -> n p d", p=128)
    hsv = D["hs_d"].rearrange("(n p) d -> n p d", p=128)
    h1v = D["h1_d"].rearrange("(n p) d -> n p d", p=128)
    with scope(C) as esp:
        bank = [C.ps(esp, "abk%d" % i, [128, 512]) for i in range(8)]
        for qt in range(16):
            sl = qt % 2
            drip(prep, 2)
            S.dma("sp", xo[sl][:], xv[48 + qt], writes=["xo%d" % sl])
            S.dma("sp", hsl[sl][:], hsv[qt], reads=["hs_d"], writes=["hsl%d" % sl])
            for h in range(8):
                hp = h // 2; rows = slice(64 * (h % 2), 64 * (h % 2) + 64); ps_ = h % 2
                bA = bank[2 * ps_]; bB = bank[2 * ps_ + 1]; bAk = "abk%d" % (2 * ps_); bBk = "abk%d" % (2 * ps_ + 1)
                for j in range(5):
                    o = bA[:, j * 128:(j + 1) * 128] if j < 4 else bB[:, 0:128]
                    ok = bAk if j < 4 else bBk
                    S.op("pe", lambda e, o=o, h=h, hp=hp, j=j, qt=qt: e.matmul(o, lhsT=kT[:, hp, (qt + j) * 128:(qt + j + 1) * 128], rhs=qZ[:, h, qt * 128:(qt + 1) * 128], start=True, stop=False),
                         reads=["kT", "qZ"], writes=[ok])
                    S.op("pe", lambda e, o=o, h=h, j=j: e.matmul(o, lhsT=idb[:], rhs=BT[:, h, j, :], start=False, stop=True), reads=["identb", "BT"], writes=[ok])
                S.op("act", lambda e, ps_=ps_, bA=bA: e.activation(out=PT[ps_][:, 0:4, :].rearrange("p j q -> p (j q)"), in_=bA[:], func=AF.Exp), reads=[bAk], writes=["PT%d" % ps_])
                S.op("act", lambda e, ps_=ps_, bB=bB: e.activation(out=PT[ps_][:, 4, :], in_=bB[:, 0:128], func=AF.Exp), reads=[bBk], writes=["PT%d" % ps_])
                ob = bank[4 + h // 4]; obk = "abk%d" % (4 + h // 4)
                for j in range(5):
                    S.op("pe", lambda e, ob=ob, h=h, j=j, ps_=ps_, qt=qt: e.matmul(ob[:, (h % 4) * 65:(h % 4) * 65 + 65], lhsT=PT[ps_][:, j, :], rhs=V[:, qt + j, h * 65:(h + 1) * 65], start=(j == 0), stop=(j == 4)),
                         reads=["PT%d" % ps_, "Vall"], writes=[obk])
            for hb in range(2):
                ov = bank[4 + hb][:, 0:260].rearrange("p (h d) -> p h d", d=65)
                S.op("dve", lambda e, hb=hb, ov=ov: e.reciprocal(out=rd[:, hb * 4:(hb + 1) * 4], in_=ov[:, :, 64]), reads=["abk%d" % (4 + hb)], writes=["rd%d" % hb])
                S.op("dve", lambda e, hb=hb, ov=ov: e.tensor_tensor(out=att[:, hb * 4:(hb + 1) * 4, :], in0=ov[:, :, 0:64], in1=bc(rd[:, hb * 4:(hb + 1) * 4].unsqueeze(2), [128, 4, 64]), op=ALU.mult),
                     reads=["abk%d" % (4 + hb), "rd%d" % hb], writes=["att%d" % hb])
            attf = att[:].rearrange("p h d -> p (h d)")
            if "att" in dbg:
                S.dma("sp", dbg["att"].rearrange("(n p) d -> n p d", p=128)[qt], attf, reads=["att0", "att1"], writes=["o_att"])
            S.op("act", lambda e: e.activation(out=junk[:], in_=attf, func=AF.Square, accum_out=st[:, 0:1]), reads=["att0", "att1"], writes=["st4_ss"])
            S.op("act", lambda e: e.activation(out=st[:, 1:2], in_=st[:, 0:1], func=AF.Sqrt, scale=1.0 / 512, bias=EPS), reads=["st4_ss"], writes=["st4_sq"])
            S.op("dve", lambda e: e.reciprocal(out=st[:, 1:2], in_=st[:, 1:2]), reads=["st4_sq"], writes=["st4"])
            S.op("dve", lambda e: e.tensor_scalar(out=an[:], in0=attf, scalar1=st[:, 1:2], scalar2=None, op0=ALU.mult), reads=["att0", "att1", "st4"], writes=["an"])
            pT = bank[6][:].bitcast(BF16).rearrange("p (c n) -> p c n", n=128)[:, 0:4, :]
            transpose_chunks(C, an[:], "an", 4, pT, "abk6", anT[:], "anT", idb)
            for cb in range(2):
                ob_ = 7 - cb
                for c in range(4):
                    S.op("pe", lambda e, c=c, cb=cb, ob_=ob_: e.matmul(bank[ob_][:], lhsT=anT[:, c, :], rhs=Wo[:, c, cb * 512:(cb + 1) * 512], start=(c == 0), stop=(c == 3)), reads=["anT", "Woa"], writes=["abk%d" % ob_])
                S.op("dve", lambda e, cb=cb, sl=sl, ob_=ob_: e.tensor_tensor(out=h1t[sl][:, cb * 512:(cb + 1) * 512], in0=bank[ob_][:], in1=xo[sl][:, cb * 512:(cb + 1) * 512], op=ALU.add),
                     reads=["abk%d" % ob_, "xo%d" % sl], writes=["h1t%d" % sl])
            S.op("pool", lambda e, sl=sl: e.tensor_tensor(out=h1t[sl][:], in0=h1t[sl][:], in1=hsl[sl][:], op=ALU.add), reads=["h1t%d" % sl, "hsl%d" % sl], writes=["h1t%d" % sl])
            S.dma("sp", h1v[qt], h1t[sl][:], reads=["h1t%d" % sl], writes=["h1_d"])


def _col(g, n):
    return np.ascontiguousarray(np.asarray(g, np.float32).reshape(n, 128).T)


def host_shared(inp):
    f = lambda k: np.asarray(inp[k], np.float32)[0]
    sh = {}
    sh["g_mix"] = _col(f("norm_mix_g"), 8)
    sh["w_in"] = np.ascontiguousarray(f("w_in"))
    sh["att_q_g"] = f("att_q_g").reshape(1, 64)
    sh["att_k_g"] = f("att_k_g").reshape(1, 64)
    rb = f("rel_bias")
    p = np.arange(128); j = np.arange(5); q = np.arange(128)
    kidx = j[:, None] * 128 + p[None, :]
    kc = kidx // 64; ki = kidx % 64
    qc = q // 64; qi = q % 64
    jb = kc[:, :, None] - qc[None, None, :]
    allowed = (jb >= 0) & (jb <= 8)
    kj = jb * 64 + ki[:, :, None]
    dist = 512 + qi[None, None, :] - kj
    bucket = np.clip(np.clip(dist, -63, 128) + 63, 0, 191)
    bt = np.where(allowed[None], rb[:, bucket], np.float32(NEG))
    sh["bias_t"] = np.ascontiguousarray(bt.transpose(2, 0, 1, 3).reshape(128, 8 * 5 * 128).astype(np.float32))
    dup = lambda a: np.ascontiguousarray(np.concatenate([a, a], 0).astype(np.float32))
    sh["s5_lr"] = dup(f("ssm_lam_re").T)
    sh["s5_li"] = dup(f("ssm_lam_im").T)
    sh["s5_ls"] = np.ascontiguousarray(np.broadcast_to(f("ssm_log_step")[None, :], (128, 32)).astype(np.float32))
    sg = np.ones((128, 2), np.float32); sg[:64, 0] = -1.0; sg[64:, 1] = -1.0
    sh["s5_sg"] = sg
    sh["s5_nv"] = np.ascontiguousarray(np.broadcast_to(np.arange(-7, 16, dtype=np.float32)[None, :], (128, 23)))
    bre = f("ssm_b_re").transpose(1, 0, 2).reshape(64, 512); bim = f("ssm_b_im").transpose(1, 0, 2).reshape(64, 512)
    cre = f("ssm_c_re").transpose(2, 0, 1).reshape(64, 512); cim = f("ssm_c_im").transpose(2, 0, 1).reshape(64, 512)
    sh["s5_p1b"] = np.ascontiguousarray(np.concatenate([bre, bim], 0)); sh["s5_p2b"] = np.ascontiguousarray(np.concatenate([bim, bre], 0))
    sh["s5_p1c"] = np.ascontiguousarray(np.concatenate([cre, cim], 0)); sh["s5_p2c"] = np.ascontiguousarray(np.concatenate([cim, cre], 0))
    dd = np.zeros((2, 4, 16, 4, 4, 16), np.float32)
    dsk = f("ssm_d")
    for g in range(32):
        chc = g // 8; hb = (g % 8) // 4; j4 = g % 4
        for c in range(16):
            dd[hb, j4, c, chc, j4, c] = dsk[g, c]
    sh["s5_dd"] = dd.reshape(128, 256)
    sh["g_ssm_out"] = _col(f("ssm_out_g"), 4)
    sh["g_att_out"] = _col(f("att_out_g"), 4)
    sh["b_glu"] = f("ssm_b_glu").reshape(1, 512)
    sh["w_glu"] = np.ascontiguousarray(f("ssm_w_glu"))
    sh["w_out"] = np.ascontiguousarray(f("w_out"))
    return sh


def host_core(inp, c):
    b, seg = c // 4, c % 4
    x = np.asarray(inp["x"], np.float32)
    xe = np.zeros((8192, 1024), np.float32)
    n = (seg + 1) * 2048
    xe[8192 - n:] = x[b, :n]
    hv = np.full((512,), 1.0 if seg > 0 else 0.0, np.float32)
    return {"x_ext": xe, "hvalid": np.ascontiguousarray(hv.reshape(4, 128).T)}


IN_SHAPES = {
    "x_ext": [8192, 1024], "hvalid": [128, 4], "g_mix": [128, 8], "w_in": [1024, 2048], "att_q_g": [1, 64], "att_k_g": [1, 64],
    "bias_t": [128, 5120], "s5_lr": [128, 32], "s5_li": [128, 32], "s5_ls": [128, 32], "s5_sg": [128, 2], "s5_nv": [128, 23],
    "s5_p1b": [128, 512], "s5_p2b": [128, 512], "s5_p1c": [128, 512], "s5_p2c": [128, 512], "s5_dd": [128, 256],
    "g_ssm_out": [128, 4], "g_att_out": [128, 4], "b_glu": [1, 512], "w_glu": [512, 512], "w_out": [1024, 1024],
}
IN_SHAPES_MEM = {"mem": [256, 1024], "g_mem": [128, 8], "g_memkv": [128, 8], "mem_q_g": [1, 256], "mem_k_g": [1, 256],
                 "w_mem_q": [1024, 1024], "w_mem_k": [1024, 1024], "w_mem_v": [1024, 1024], "w_mem_o": [1024, 1024]}
IN_SHAPES_PEER = {"g_peer": [1, 1024], "w_peer_q": [1024, 2048], "keysT": [128, 2048], "peer_uT": [1024, 16384], "peer_v": [16384, 1024]}
SCRATCH = {"kT_d": ([128, 4 * 2560], BF16), "qT_d": ([128, 4 * 2048], BF16), "V_d": ([20, 128, 520], BF16),
           "hs_d": ([2048, 1024], F32), "Mz_d": ([128, 16 * 8 * 128], BF16), "CA_d": ([128, 2, 4096], BF16)}
SCRATCH_PEER = {"uT_b": ([1024, 16384], BF16), "v_b": ([16384, 1024], BF16), "xnT_d": ([16, 128, 1024], BF16),
                "sc_d": ([16, 128, 2048], F32), "tau_d": ([16, 128, 8], F32)}


def host_shared_rest(inp):
    f = lambda k: np.asarray(inp[k], np.float32)[0]
    sh = {}
    sh["g_mem"] = _col(f("norm_mem_g"), 8); sh["g_memkv"] = _col(f("norm_memkv_g"), 8)
    sh["mem_q_g"] = f("mem_q_g").reshape(1, 256); sh["mem_k_g"] = f("mem_k_g").reshape(1, 256)
    for k in ("w_mem_q", "w_mem_k", "w_mem_v", "w_mem_o", "w_peer_q"):
        sh[k] = np.ascontiguousarray(f(k))
    sh["g_peer"] = f("norm_peer_g").reshape(1, 1024)
    sh["keysT"] = np.ascontiguousarray(f("peer_keys").transpose(3, 0, 1, 2).reshape(128, 2048))
    sh["peer_uT"] = np.ascontiguousarray(f("peer_u").T)
    sh["peer_v"] = np.ascontiguousarray(f("peer_v"))
    return sh


def build_program(stages=("mixer", "mem", "peer"), dbg_specs=None, upto=9):
    nc = bass.Bass("TRN2", target_bir_lowering=False)
    D = {}
    shapes = {}
    if "mixer" in stages:
        shapes.update(IN_SHAPES)
    if "mem" in stages:
        shapes.update(IN_SHAPES_MEM)
    if "peer" in stages:
        shapes.update(IN_SHAPES_PEER)
    for k, shp in shapes.items():
        D[k] = nc.dram_tensor(k, shp, F32, kind="ExternalInput").ap()
    scr = {}
    if "mixer" in stages:
        scr.update(SCRATCH)
    if "peer" in stages:
        scr.update(SCRATCH_PEER)
    for k, (shp, dt) in scr.items():
        D[k] = nc.dram_tensor(k, shp, dt).ap()
    chain = ["h1_d", "h2_d", "out"]
    first = {"mixer": None, "mem": "h1_d", "peer": "h2_d"}[stages[0]]
    last = {"mixer": "h1_d", "mem": "h2_d", "peer": "out"}[stages[-1]]
    for k in chain:
        if k == first:
            D[k] = nc.dram_tensor(k, [2048, 1024], F32, kind="ExternalInput").ap()
        elif k == last:
            D[k] = nc.dram_tensor(k, [2048, 1024], F32, kind="ExternalOutput").ap()
        else:
            D[k] = nc.dram_tensor(k, [2048, 1024], F32).ap()
    dbg = {}
    for k, shp in (dbg_specs or {}).items():
        dbg[k] = nc.dram_tensor("dbg_" + k, shp, F32, kind="ExternalOutput").ap()
    with ExitStack() as es:
        S = Sched(nc, es)
        C = Ctx(nc, S)
        prep = stage_peer_prep(C, D) if "peer" in stages else None
        if "mixer" in stages:
            stage_mixer(C, D, dbg, upto, prep=prep)
        if "mem" in stages:
            stage_mem(C, D, dbg, prep=prep)
        drip(prep, 1000)
        if "peer" in stages:
            stage_peer(C, D, dbg)
        S.barrier()
        S.emit()
    return nc, S


def _headnorm(C, src_ap, skey, nh, hd, sqt, sqk, stat, stk, gt, gkey, dst_ap, dkey):
    S = C.S
    sv = src_ap.rearrange("p (h d) -> p h d", d=hd)
    S.op("pool", lambda e: e.tensor_tensor(out=sqt, in0=src_ap, in1=src_ap, op=ALU.mult), reads=[skey], writes=[sqk])
    S.op("dve", lambda e: e.tensor_reduce(out=stat[:, 0:nh], in_=sqt.rearrange("p (h d) -> p h d", d=hd), axis=AX.X, op=ALU.add), reads=[sqk], writes=[stk + "a"])
    S.op("act", lambda e: e.activation(out=stat[:, nh:2 * nh], in_=stat[:, 0:nh], func=AF.Sqrt, scale=1.0 / hd, bias=EPS), reads=[stk + "a"], writes=[stk + "b"])
    S.op("dve", lambda e: e.reciprocal(out=stat[:, nh:2 * nh], in_=stat[:, nh:2 * nh]), reads=[stk + "b"], writes=[stk])
    S.op("dve", lambda e: e.tensor_tensor(out=sv, in0=sv, in1=bc(stat[:, nh:2 * nh].unsqueeze(2), [128, nh, hd]), op=ALU.mult), reads=[skey, stk], writes=[skey])
    S.op("pool", lambda e: e.tensor_tensor(out=dst_ap.rearrange("p (h d) -> p h d", d=hd), in0=sv, in1=bc(gt.unsqueeze(1), [128, nh, hd]), op=ALU.mult), reads=[skey, gkey], writes=[dkey])


def stage_mem(C, D, dbg=None, prep=None):
    S = C.S
    dbg = dbg or {}
    with scope(C) as es:
        sb = lambda name, shape, dt=F32: C.sb(es, name, shape, dt)
        idf, idb = make_ident(C, es, "ident")
        gm = sb("gm", [128, 8]); gkv = sb("gkv", [128, 8]); gq = sb("mgq", [128, 256]); gk = sb("mgk", [128, 256])
        S.dma("sp", gm[:], D["g_mem"], writes=["gm"]); S.dma("sp", gkv[:], D["g_memkv"], writes=["gkv"])
        S.dma("sp", gq[:], D["mem_q_g"].partition_broadcast(128), writes=["mgq"]); S.dma("sp", gk[:], D["mem_k_g"].partition_broadcast(128), writes=["mgk"])
        S.op("dve", lambda e: e.tensor_scalar(out=gq[:], in0=gq[:], scalar1=1.0 / 16, scalar2=None, op0=ALU.mult), reads=["mgq"], writes=["mgq"])
        kTm = sb("kTm", [128, 8, 256], BF16); Vm = sb("Vm", [128, 2, 4, 257], BF16)
        xt = [sb("mxt%d" % i, [128, 1024]) for i in range(2)]; st = sb("mst", [128, 2]); xn = sb("mxn", [128, 1024], BF16)
        xnT = sb("mxnT", [128, 8, 128], BF16); qf = sb("mqf", [128, 1024]); sq = sb("msq", [128, 1024]); qst = sb("mqst", [128, 8])
        qn = sb("mqn", [128, 1024], BF16); mjunk = sb("mjunk", [128, 1024], BF16)
        with scope(C) as esk:
            with scope(C) as est:
                Wk = load_weight_bf16(C, esk, est, "Wmk", D["w_mem_k"], 8, 1024, gcol=(gkv, "gkv"))
            with scope(C) as est:
                Wv = load_weight_bf16(C, esk, est, "Wmv", D["w_mem_v"], 8, 1024, gcol=(gkv, "gkv"))
            with scope(C) as esp:
                bank = [C.ps(esp, "mkb%d" % i, [128, 512]) for i in range(6)]
                mv = D["mem"].rearrange("(n p) d -> n p d", p=128)
                for mt in range(2):
                    x_ = xt[mt]; xk = "mxt%d" % mt
                    S.dma("sp", x_[:], mv[mt], writes=[xk])
                    rms_rstd(C, x_[:], xk, mjunk[:], "mjunk", st[:, 0:1], st[:, 1:2], "mst", 1024)
                    S.op("dve", lambda e, x_=x_: e.tensor_scalar(out=xn[:], in0=x_[:], scalar1=st[:, 1:2], scalar2=None, op0=ALU.mult), reads=[xk, "mst"], writes=["mxn"])
                    pb = bank[0][:].bitcast(BF16).rearrange("p (c n) -> p c n", n=128)
                    transpose_chunks(C, xn[:], "mxn", 8, pb, "mkb0", xnT[:], "mxnT", idb)
                    for (W, wk, b0) in ((Wk, "Wmk", 1), (Wv, "Wmv", 3)):
                        for cb in range(2):
                            for dc in range(8):
                                S.op("pe", lambda e, W=W, cb=cb, dc=dc, b0=b0: e.matmul(bank[b0 + cb][:], lhsT=xnT[:, dc, :], rhs=W[:, dc, cb * 512:(cb + 1) * 512], start=(dc == 0), stop=(dc == 7)),
                                     reads=["mxnT", wk], writes=["mkb%d" % (b0 + cb)])
                    for cb in range(2):
                        S.op("act", lambda e, cb=cb: e.copy(out=qf[:, cb * 512:(cb + 1) * 512], in_=bank[1 + cb][:]), reads=["mkb%d" % (1 + cb)], writes=["mqf"])
                        S.op("dve", lambda e, cb=cb, mt=mt: e.tensor_copy(out=Vm[:, mt, 2 * cb:2 * cb + 2, 0:256], in_=bank[3 + cb][:].rearrange("p (h d) -> p h d", d=256)), reads=["mkb%d" % (3 + cb)], writes=["Vm"])
                    S.op("pool", lambda e, mt=mt: e.memset(Vm[:, mt, :, 256], 1.0), reads=[], writes=["Vm"])
                    _headnorm(C, qf[:], "mqf", 4, 256, sq[:], "msq", qst, "mqst", gk[:], "mgk", qn[:], "mqn")
                    pb2 = bank[5][:].bitcast(BF16).rearrange("p (c n) -> p c n", n=128)
                    transpose_chunks(C, qn[:], "mqn", 8, pb2, "mkb5", kTm[:, :, mt * 128:(mt + 1) * 128], "kTm", idb)
        with scope(C) as est:
            Wq = load_weight_bf16(C, es, est, "Wmq", D["w_mem_q"], 8, 1024, gcol=(gm, "gm"))
        with scope(C) as est:
            Wo = load_weight_bf16(C, es, est, "Wmo", D["w_mem_o"], 8, 1024)
        NM = 2
        xn2 = [sb("mxn_%d" % i, [128, 1024], BF16) for i in range(NM)]; xnT2 = [sb("mxnT_%d" % i, [128, 8, 128], BF16) for i in range(NM)]
        qf2 = [sb("mqf_%d" % i, [128, 1024]) for i in range(NM)]; sq2 = [sb("msq_%d" % i, [128, 1024]) for i in range(NM)]; qst2 = [sb("mqst_%d" % i, [128, 8]) for i in range(NM)]
        qn2 = [sb("mqn_%d" % i, [128, 1024], BF16) for i in range(NM)]; st2 = [sb("mst_%d" % i, [128, 2]) for i in range(NM)]
        qT2 = [sb("mqT%d" % i, [128, 8, 128], BF16) for i in range(NM)]; PT = [sb("mPT%d" % i, [128, 2, 128], BF16) for i in range(2)]
        rd2 = [sb("mrd%d" % i, [128, 4]) for i in range(NM)]; ob2 = [sb("mob%d" % i, [128, 1024], BF16) for i in range(NM)]; oT2 = [sb("moT%d" % i, [128, 8, 128], BF16) for i in range(NM)]
        h2t = [sb("h2t%d" % i, [128, 1024]) for i in range(2)]
        hv = D["h1_d"].rearrange("(n p) d -> n p d", p=128); ov = D["h2_d"].rearrange("(n p) d -> n p d", p=128)
        with scope(C) as esp:
            bank = [C.ps(esp, "mb%d" % i, [128, 512]) for i in range(8)]

            def mtile(tt):
                sl = tt % NM
                K = lambda nm: "%s_%d" % (nm, sl)
                xn = xn2[sl]; xnT = xnT2[sl]; qf = qf2[sl]; sq = sq2[sl]; qst = qst2[sl]; qn = qn2[sl]; st = st2[sl]; qT = qT2[sl]; rd = rd2[sl]; ob = ob2[sl]; oT = oT2[sl]
                drip(prep, 1)
                x_ = xt[sl]; xk = "mxt%d" % sl
                S.dma("sp", x_[:], hv[tt], reads=["h1_d"], writes=[xk])
                rms_rstd(C, x_[:], xk, mjunk[:], "mjunk", st[:, 0:1], st[:, 1:2], K("mst"), 1024)
                S.op("dve", lambda e: e.tensor_scalar(out=xn[:], in0=x_[:], scalar1=st[:, 1:2], scalar2=None, op0=ALU.mult), reads=[xk, K("mst")], writes=[K("mxn")])
                pb = bank[0][:].bitcast(BF16).rearrange("p (c n) -> p c n", n=128)
                transpose_chunks(C, xn[:], K("mxn"), 8, pb, "mb0", xnT[:], K("mxnT"), idb)
                yield
                for cb in range(2):
                    for dc in range(8):
                        S.op("pe", lambda e, cb=cb, dc=dc: e.matmul(bank[1 + cb][:], lhsT=xnT[:, dc, :], rhs=Wq[:, dc, cb * 512:(cb + 1) * 512], start=(dc == 0), stop=(dc == 7)),
                             reads=[K("mxnT"), "Wmq"], writes=["mb%d" % (1 + cb)])
                    S.op("act", lambda e, cb=cb: e.copy(out=qf[:, cb * 512:(cb + 1) * 512], in_=bank[1 + cb][:]), reads=["mb%d" % (1 + cb)], writes=[K("mqf")])
                yield
                _headnorm(C, qf[:], K("mqf"), 4, 256, sq[:], K("msq"), qst, K("mqst"), gq[:], "mgq", qn[:], K("mqn"))
                pb2 = bank[3][:].bitcast(BF16).rearrange("p (c n) -> p c n", n=128)
                transpose_chunks(C, qn[:], K("mqn"), 8, pb2, "mb3", qT[:], K("mqT"), idb)
                yield
                for h in range(4):
                    ps_ = h % 2
                    sbk = bank[4 + ps_]; sbkk = "mb%d" % (4 + ps_)
                    for mt in range(2):
                        for dh in range(2):
                            S.op("pe", lambda e, h=h, mt=mt, dh=dh, sbk=sbk: e.matmul(sbk[:, mt * 128:(mt + 1) * 128], lhsT=kTm[:, 2 * h + dh, mt * 128:(mt + 1) * 128], rhs=qT[:, 2 * h + dh, :], start=(dh == 0), stop=(dh == 1)),
                                 reads=["kTm", K("mqT")], writes=[sbkk])
                    S.op("act", lambda e, ps_=ps_, sbk=sbk: e.activation(out=PT[ps_][:].rearrange("p m q -> p (m q)"), in_=sbk[:, 0:256], func=AF.Exp, bias=-8.0), reads=[sbkk], writes=["mPT%d" % ps_])
                    obk = bank[6 + ps_]; obkk = "mb%d" % (6 + ps_)
                    for mt in range(2):
                        S.op("pe", lambda e, h=h, mt=mt, ps_=ps_, obk=obk: e.matmul(obk[:, 0:257], lhsT=PT[ps_][:, mt, :], rhs=Vm[:, mt, h, :], start=(mt == 0), stop=(mt == 1)), reads=["mPT%d" % ps_, "Vm"], writes=[obkk])
                    S.op("dve", lambda e, h=h, obk=obk: e.reciprocal(out=rd[:, h:h + 1], in_=obk[:, 256:257]), reads=[obkk], writes=[K("mrd") + "_%d" % h])
                    S.op("dve", lambda e, h=h, obk=obk: e.tensor_scalar(out=ob[:, h * 256:(h + 1) * 256], in0=obk[:, 0:256], scalar1=rd[:, h:h + 1], scalar2=None, op0=ALU.mult), reads=[obkk, K("mrd") + "_%d" % h], writes=[K("mob")])
                    yield
                pb3 = bank[0][:].bitcast(BF16).rearrange("p (c n) -> p c n", n=128)
                transpose_chunks(C, ob[:], K("mob"), 8, pb3, "mb0", oT[:], K("moT"), idb)
                yield
                for cb in range(2):
                    for dc in range(8):
                        S.op("pe", lambda e, cb=cb, dc=dc: e.matmul(bank[1 + cb][:], lhsT=oT[:, dc, :], rhs=Wo[:, dc, cb * 512:(cb + 1) * 512], start=(dc == 0), stop=(dc == 7)),
                             reads=[K("moT"), "Wmo"], writes=["mb%d" % (1 + cb)])
                    S.op("dve", lambda e, cb=cb: e.tensor_tensor(out=h2t[sl][:, cb * 512:(cb + 1) * 512], in0=bank[1 + cb][:], in1=x_[:, cb * 512:(cb + 1) * 512], op=ALU.add),
                         reads=["mb%d" % (1 + cb), xk], writes=["h2t%d" % sl])
                S.dma("sp", ov[tt], h2t[sl][:], reads=["h2t%d" % sl], writes=["h2_d"])

            from itertools import zip_longest
            gens = []
            for t0 in range(0, 16, NM):
                for _ in zip_longest(*[mtile(t0 + i) for i in range(NM)]):
                    pass


def stage_peer_prep(C, D):
    S = C.S
    uv = D["peer_uT"].rearrange("(c p) (a e) -> c p a e", p=128, e=2048)
    ub = D["uT_b"].rearrange("(c p) (a e) -> c p a e", p=128, e=2048)
    vv = D["peer_v"].rearrange("(c p) d -> c p d", p=512)
    vb = D["v_b"].rearrange("(c p) d -> c p d", p=512)

    def gen():
        for c in range(8):
            S.dma("pool", ub[c], uv[c], writes=["uT_b"])
            yield
        for c in range(32):
            S.dma("pool", vb[c], vv[c], writes=["v_b"])
            yield
    return gen()


def drip(g, n):
    if g is None:
        return
    for _ in range(n):
        try:
            next(g)
        except StopIteration:
            return


def _top16(C, src, skey, work, wkey, dst, dkey):
    S = C.S
    S.op("dve", lambda e: e.max(out=dst[:, 0:8], in_=src), reads=[skey], writes=[dkey])
    S.op("dve", lambda e: e.match_replace(out=work, in_to_replace=dst[:, 0:8], in_values=src, imm_value=-1e30), reads=[skey, dkey], writes=[wkey])
    S.op("dve", lambda e: e.max(out=dst[:, 8:16], in_=work), reads=[wkey], writes=[dkey])


def stage_peer(C, D, dbg=None):
    S = C.S
    dbg = dbg or {}
    hv = D["h2_d"].rearrange("(n p) d -> n p d", p=128)
    with scope(C) as es:
        sb = lambda name, shape, dt=F32: C.sb(es, name, shape, dt)
        idf, idb = make_ident(C, es, "ident")
        gp = sb("gpb", [128, 1024])
        S.dma("sp", gp[:], D["g_peer"].partition_broadcast(128), writes=["gpb"])
        with scope(C) as est:
            Wq = load_weight_bf16(C, es, est, "Wpq", D["w_peer_q"], 8, 2048)
        keyT = sb("keyT", [128, 16, 128], BF16)
        with scope(C) as est:
            kst = C.sb(est, "kst", [128, 2048])
            S.dma("sp", kst[:], D["keysT"], writes=["kst"])
            S.op("dve", lambda e: e.tensor_copy(out=keyT[:].rearrange("p a n -> p (a n)"), in_=kst[:]), reads=["kst"], writes=["keyT"])
        NS = 3
        xt = [sb("pxt%d" % i, [128, 1024]) for i in range(NS)]; st_ = [sb("pst%d" % i, [128, 2]) for i in range(NS)]; junk = sb("pjunk", [128, 1024], BF16)
        xn_ = [sb("pxn%d" % i, [128, 1024], BF16) for i in range(NS)]; xnT = [sb("pxnT%d" % i, [128, 8, 128], BF16) for i in range(NS)]
        qb_ = [sb("pqb%d" % i, [128, 2048], BF16) for i in range(NS)]; qTp_ = [sb("pqT%d" % i, [128, 16, 128], BF16) for i in range(NS)]
        sc = [sb("psc%d" % i, [128, 16, 128]) for i in range(NS)]; work_ = [sb("pwork%d" % i, [128, 256]) for i in range(NS)]
        sv_ = [sb("psv%d" % i, [128, 16, 16]) for i in range(NS)]; cand_ = [sb("pcand%d" % i, [128, 8, 256]) for i in range(NS)]
        cex_ = [sb("pcex%d" % i, [128, 8, 256]) for i in range(NS)]; ctop_ = [sb("pctop%d" % i, [128, 8, 16]) for i in range(NS)]
        Z_ = [sb("pZ%d" % i, [128, 8]) for i in range(NS)]; off_ = [sb("poff%d" % i, [128, 8]) for i in range(NS)]; tau = [sb("ptau%d" % i, [128, 8]) for i in range(NS)]
        cjunk_ = [[sb("pcj%d_%d" % (i, h), [128, 256], BF16) for h in range(8)] for i in range(NS)]; offs_ = [sb("poffs%d" % i, [128, 16]) for i in range(NS)]
        xTd = D["xnT_d"].rearrange("n p (c t) -> n p c t", t=128)
        with scope(C) as esp:
            bank = [C.ps(esp, "pab%d" % i, [128, 512]) for i in range(6)]

            def tile(tt):
                sl = tt % NS
                K = lambda nm: "%s%d" % (nm, sl)
                x_ = xt[sl]; xk = K("pxt"); st = st_[sl]; xn = xn_[sl]; qb = qb_[sl]; qTp = qTp_[sl]; work = work_[sl]
                sv = sv_[sl]; cand = cand_[sl]; cex = cex_[sl]; ctop = ctop_[sl]; Z = Z_[sl]; off = off_[sl]; cjunk = cjunk_[sl]; offs = offs_[sl]
                S.dma("sp", x_[:], hv[tt], reads=["h2_d"], writes=[xk])
                rms_rstd(C, x_[:], xk, junk[:], "pjunk", st[:, 0:1], st[:, 1:2], K("pst"), 1024)
                S.op("dve", lambda e: e.scalar_tensor_tensor(out=xn[:], in0=x_[:], scalar=st[:, 1:2], in1=gp[:], op0=ALU.mult, op1=ALU.mult), reads=[xk, K("pst"), "gpb"], writes=[K("pxn")])
                pb = bank[0][:].bitcast(BF16).rearrange("p (c n) -> p c n", n=128)
                transpose_chunks(C, xn[:], K("pxn"), 8, pb, "pab0", xnT[sl][:], K("pxnT"), idb)
                S.dma("sp", xTd[tt], xnT[sl][:], reads=[K("pxnT")], writes=["xnT_d"])
                yield
                for cb in range(4):
                    for dc in range(8):
                        S.op("pe", lambda e, cb=cb, dc=dc: e.matmul(bank[1 + cb][:], lhsT=xnT[sl][:, dc, :], rhs=Wq[:, dc, cb * 512:(cb + 1) * 512], start=(dc == 0), stop=(dc == 7)),
                             reads=[K("pxnT"), "Wpq"], writes=["pab%d" % (1 + cb)])
                    S.op("act", lambda e, cb=cb: e.copy(out=qb[:, cb * 512:(cb + 1) * 512], in_=bank[1 + cb][:]), reads=["pab%d" % (1 + cb)], writes=[K("pqb")])
                for half in range(2):
                    pbq = bank[5][:].bitcast(BF16).rearrange("p (c n) -> p c n", n=128)
                    transpose_chunks(C, qb[:, half * 1024:(half + 1) * 1024], K("pqb"), 8, pbq, "pab5", qTp[:, half * 8:(half + 1) * 8, :], K("pqT"), idb)
                for hh in range(16):
                    S.op("pe", lambda e, hh=hh: e.matmul(bank[1 + hh // 4][:, (hh % 4) * 128:(hh % 4 + 1) * 128], lhsT=qTp[:, hh, :], rhs=keyT[:, hh, :], start=True, stop=True),
                         reads=[K("pqT"), "keyT"], writes=["pab%d" % (1 + hh // 4)])
                scs = sc[sl]; sck = K("psc")
                for cb in range(4):
                    S.op("act", lambda e, cb=cb: e.copy(out=scs[:, cb * 4:(cb + 1) * 4, :].rearrange("p a n -> p (a n)"), in_=bank[1 + cb][:]), reads=["pab%d" % (1 + cb)], writes=[sck])
                yield
                for hh in range(16):
                    _top16(C, scs[:, hh, :], sck, work[:, 0:128], K("pwork"), sv[:, hh, :], K("psv"))
                    yield
                svv = sv[:].rearrange("p (h s) k -> p h s k", s=2)
                for h in range(8):
                    S.op("dve", lambda e, h=h: e.tensor_tensor(out=cand[:, h, :].rearrange("p (a b) -> p a b", b=16), in0=bc(svv[:, h, 0, :].unsqueeze(2), [128, 16, 16]),
                                                            in1=bc(svv[:, h, 1, :].unsqueeze(1), [128, 16, 16]), op=ALU.add), reads=[K("psv")], writes=[K("pcand") + "_%d" % h])
                    yield
                ck = [K("pcand") + "_%d" % h for h in range(8)]
                for h in range(8):
                    _top16(C, cand[:, h, :], ck[h], work[:], K("pwork"), ctop[:, h, :], K("pctop"))
                    yield
                S.op("dve", lambda e: e.tensor_tensor(out=cand[:], in0=cand[:], in1=bc(ctop[:, :, 0:1], [128, 8, 256]), op=ALU.subtract), reads=ck + [K("pctop")], writes=ck)
                S.op("act", lambda e: e.activation(out=cex[:].rearrange("p h n -> p (h n)"), in_=cand[:].rearrange("p h n -> p (h n)"), func=AF.Exp), reads=ck, writes=[K("pcex")])
                yield
                S.op("dve", lambda e: e.tensor_tensor(out=tau[sl][:], in0=ctop[:, :, 15], in1=ctop[:, :, 0], op=ALU.subtract), reads=[K("pctop")], writes=[K("ptau")])
                yield
                S.op("dve", lambda e: e.tensor_scalar(out=tau[sl][:], in0=tau[sl][:], scalar1=-1e-5, scalar2=None, op0=ALU.add), reads=[K("ptau")], writes=[K("ptau")])
                yield
                for h in range(8):
                    S.op("dve", lambda e, h=h: e.scalar_tensor_tensor(out=cjunk[h][:], in0=cand[:, h, :], scalar=tau[sl][:, h:h + 1], in1=cex[:, h, :], op0=ALU.is_ge, op1=ALU.mult, accum_out=Z[:, h:h + 1]),
                         reads=ck + [K("pcex"), K("ptau")], writes=[K("pZ") + "_%d" % h, K("pcj") + "_%d" % h])
                    yield
                S.op("act", lambda e: e.activation(out=off[:], in_=Z[:], func=AF.Ln), reads=[K("pZ") + "_%d" % h for h in range(8)], writes=[K("poff")])
                yield
                S.op("dve", lambda e: e.tensor_tensor(out=tau[sl][:], in0=tau[sl][:], in1=off[:], op=ALU.subtract), reads=[K("ptau"), K("poff")], writes=[K("ptau")])
                S.op("act", lambda e: e.activation(out=tau[sl][:], in_=tau[sl][:], func=AF.Exp), reads=[K("ptau")], writes=[K("ptau")])
                yield
                S.op("dve", lambda e: e.tensor_scalar(out=tau[sl][:], in0=tau[sl][:], scalar1=0.99997, scalar2=None, op0=ALU.mult), reads=[K("ptau")], writes=[K("ptau")])
                offv = offs[:].rearrange("p (h s) -> p h s", s=2)
                S.op("dve", lambda e: e.tensor_copy(out=offv[:, :, 0], in_=svv[:, :, 0, 0]), reads=[K("psv")], writes=[K("poffs") + "a"])
                yield
                S.op("dve", lambda e: e.tensor_tensor(out=offv[:, :, 1], in0=svv[:, :, 1, 0], in1=off[:], op=ALU.add), reads=[K("psv"), K("poff")], writes=[K("poffs") + "b"])
                yield
                S.op("dve", lambda e: e.tensor_tensor(out=scs[:], in0=scs[:], in1=bc(offs[:].unsqueeze(2), [128, 16, 128]), op=ALU.subtract), reads=[sck, K("poffs") + "a", K("poffs") + "b"], writes=[sck])
                S.op("act", lambda e: e.activation(out=scs[:].rearrange("p a n -> p (a n)"), in_=scs[:].rearrange("p a n -> p (a n)"), func=AF.Exp), reads=[sck], writes=[sck])
                S.dma("sp", D["sc_d"][tt], scs[:].rearrange("p a n -> p (a n)"), reads=[sck], writes=["sc_d"])
                S.dma("sp", D["tau_d"][tt], tau[sl][:], reads=[K("ptau")], writes=["tau_d"])

            from itertools import zip_longest
            for t0 in range(0, 16, NS):
                for _ in zip_longest(*[tile(t0 + i) for i in range(NS) if t0 + i < 16]):
                    pass
    with scope(C) as es:
        sb = lambda name, shape, dt=F32: C.sb(es, name, shape, dt)
        idf, idb = make_ident(C, es, "ident")
        xnT = sb("bxnT", [128, 4, 1024], BF16); sc = sb("bsc", [128, 4, 2048]); kap = sb("btau", [128, 4, 8])
        UT = [sb("UT%d" % i, [128, 8, 1024], BF16) for i in range(2)]; Vb = [sb("Vb%d" % i, [128, 8, 1024], BF16) for i in range(2)]
        acc = sb("pacc", [128, 4, 1024])
        NP = 12
        Pt = [sb("pP%d" % i, [128, 512]) for i in range(NP)]; Wh = [[sb("pWh%d_%d" % (i, h), [128, 512], BF16) for h in range(8)] for i in range(2)]
        G = [sb("pG%d" % i, [128, 512], BF16) for i in range(3)]; WA = [sb("pWA%d" % i, [128, 512], BF16) for i in range(2)]
        WAT = [sb("pWAT%d" % i, [128, 4, 128], BF16) for i in range(2)]
        h2t = [sb("ph2t%d" % i, [128, 1024]) for i in range(2)]
        uTv = D["uT_b"].rearrange("(c p) (b e) -> b p c e", p=128, e=1024)
        vbv = D["v_b"].rearrange("(b c p) d -> b p c d", p=128, c=8)
        ov = D["out"].rearrange("(n p) d -> n p d", p=128)
        with scope(C) as esp:
            bank = [C.ps(esp, "pbb%d" % i, [128, 512]) for i in range(7)]
            state = {"it": 0}

            def stage1a(u, tg, eb, sub, tt, es_):
                ub = u % 2
                xT = xnT[:, tt, :].rearrange("p (c t) -> p c t", t=128)
                scv = sc[:, tt, :].rearrange("p (h s n) -> p h s n", s=2, n=128)
                i0 = eb * 8 + sub * 4
                for dc in range(8):
                    S.op("pe", lambda e, dc=dc, xT=xT: e.matmul(bank[ub][:], lhsT=xT[:, dc, :], rhs=UT[es_][:, dc, sub * 512:(sub + 1) * 512], start=(dc == 0), stop=(dc == 7)),
                         reads=["bxnT", "UT%d" % es_], writes=["pbb%d" % ub])
                for h in range(8):
                    hs = state["it"] % NP; state["it"] += 1
                    if h >= 5:
                        S.op("pool", lambda e, h=h, hs=hs, scv=scv: e.tensor_tensor(out=Pt[hs][:].rearrange("p (i j) -> p i j", j=128), in0=bc(scv[:, h, 0, i0:i0 + 4].unsqueeze(2), [128, 4, 128]),
                                                                             in1=bc(scv[:, h, 1, :].unsqueeze(1), [128, 4, 128]), op=ALU.mult), reads=["bsc"], writes=["pP%d_%d" % (hs, il) for il in range(4)])
                    else:
                        for il in range(4):
                            S.op("act", lambda e, h=h, hs=hs, scv=scv, il=il: e.activation(out=Pt[hs][:, il * 128:(il + 1) * 128], in_=scv[:, h, 1, :], func=AF.Copy, scale=scv[:, h, 0, i0 + il:i0 + il + 1]),
                                 reads=["bsc"], writes=["pP%d_%d" % (hs, il)])
                    S.op("dve", lambda e, h=h, hs=hs: e.scalar_tensor_tensor(out=Wh[ub][h][:], in0=Pt[hs][:], scalar=kap[:, tt, h:h + 1], in1=Pt[hs][:], op0=ALU.is_ge, op1=ALU.mult),
                         reads=["pP%d_%d" % (hs, il) for il in range(4)] + ["btau"], writes=["pWh%d_%d" % (ub, h)])

            def st_gelu(u, tg, eb, sub, tt, es_):
                ub = u % 2; gb = u % 3
                S.op("act", lambda e: e.activation(out=G[gb][:], in_=bank[ub][:], func=AF.Gelu), reads=["pbb%d" % ub], writes=["pG%d" % gb])

            def st_hs(u, tg, eb, sub, tt, es_):
                ub = u % 2
                for h in range(8):
                    S.op("pe", lambda e, h=h: e.matmul(bank[2 + ub][:], lhsT=idb[:], rhs=Wh[ub][h][:], start=(h == 0), stop=(h == 7)),
                         reads=["identb", "pWh%d_%d" % (ub, h)], writes=["pbb%d" % (2 + ub)])

            def st_wa(u, tg, eb, sub, tt, es_):
                ub = u % 2; gb = u % 3
                S.op("dve", lambda e: e.tensor_tensor(out=WA[ub][:], in0=bank[2 + ub][:], in1=G[gb][:], op=ALU.mult), reads=["pbb%d" % (2 + ub), "pG%d" % gb], writes=["pWA%d" % ub])

            def st_t(u, tg, eb, sub, tt, es_):
                ub = u % 2
                pbt = bank[4][:].bitcast(BF16).rearrange("p (c n) -> p c n", n=128)[:, 0:4, :]
                for c in range(4):
                    S.op("pe", lambda e, c=c: e.transpose(out=pbt[:, c, :], in_=WA[ub][:, c * 128:(c + 1) * 128], identity=idb[:]), reads=["pWA%d" % ub, "identb"], writes=["pbb4"])

            def st_watcopy(u, tg, eb, sub, tt, es_):
                ub = u % 2
                pbt = bank[4][:].bitcast(BF16).rearrange("p (c n) -> p c n", n=128)[:, 0:4, :]
                S.op("act", lambda e: e.copy(out=WAT[ub][:], in_=pbt), reads=["pbb4"], writes=["pWAT%d" % ub])

            def st_v(u, tg, eb, sub, tt, es_):
                ub = u % 2
                for cb in range(2):
                    for ec in range(4):
                        S.op("pe", lambda e, cb=cb, ec=ec: e.matmul(bank[5 + cb][:], lhsT=WAT[ub][:, ec, :], rhs=Vb[es_][:, sub * 4 + ec, cb * 512:(cb + 1) * 512], start=(sub == 0 and ec == 0), stop=(sub == 1 and ec == 3)),
                             reads=["pWAT%d" % ub, "Vb%d" % es_], writes=["pbb%d" % (5 + cb)])

            def st_acc(u, tg, eb, sub, tt, es_):
                if sub != 1:
                    return
                for cb in range(2):
                    if eb == 0:
                        S.op("dve", lambda e, cb=cb: e.tensor_copy(out=acc[:, tt, cb * 512:(cb + 1) * 512], in_=bank[5 + cb][:]), reads=["pbb%d" % (5 + cb)], writes=["pacc%d" % tt])
                    else:
                        S.op("dve", lambda e, cb=cb: e.tensor_tensor(out=acc[:, tt, cb * 512:(cb + 1) * 512], in0=bank[5 + cb][:], in1=acc[:, tt, cb * 512:(cb + 1) * 512], op=ALU.add),
                             reads=["pbb%d" % (5 + cb), "pacc%d" % tt], writes=["pacc%d" % tt])

            u = 0
            for tg in range(4):
                S.dma("sp", xnT[:], D["xnT_d"][tg * 4:(tg + 1) * 4].rearrange("n p f -> p n f"), reads=["xnT_d"], writes=["bxnT"])
                S.dma("sp", sc[:], D["sc_d"][tg * 4:(tg + 1) * 4].rearrange("n p f -> p n f"), reads=["sc_d"], writes=["bsc"])
                S.dma("sp", kap[:], D["tau_d"][tg * 4:(tg + 1) * 4].rearrange("n p f -> p n f"), reads=["tau_d"], writes=["btau"])
                units = []
                for eb in range(16):
                    es_ = (tg * 16 + eb) % 2
                    for tt in range(4):
                        for sub in range(2):
                            units.append((u, tg, eb, sub, tt, es_)); u += 1
                n = len(units)
                U = lambda j: units[j] if 0 <= j < n else None
                for k in range(n + 3):
                    for ebk in ([0] if k == 0 else []) + ([k // 8 + 1] if (k % 8 == 3 and k // 8 + 1 < 16) else []):
                        es_ = (tg * 16 + ebk) % 2
                        S.dma("sp", UT[es_][:], uTv[ebk], reads=["uT_b"], writes=["UT%d" % es_])
                        S.dma("sp", Vb[es_][:], vbv[ebk], reads=["v_b"], writes=["Vb%d" % es_])
                    if U(k - 2): st_wa(*U(k - 2))
                    if U(k - 3): st_v(*U(k - 3))
                    if U(k - 2): st_t(*U(k - 2))
                    if U(k - 1): st_gelu(*U(k - 1))
                    if U(k - 1): st_hs(*U(k - 1))
                    if U(k): stage1a(*U(k))
                    if U(k - 2): st_watcopy(*U(k - 2))
                    if U(k - 3): st_acc(*U(k - 3))
                for tt in range(4):
                    sl = tt % 2; n = tg * 4 + tt
                    S.dma("sp", h2t[sl][:], hv[n], reads=["h2_d"], writes=["ph2t%d" % sl])
                    S.op("pool", lambda e, sl=sl, tt=tt: e.tensor_tensor(out=h2t[sl][:], in0=h2t[sl][:], in1=acc[:, tt, :], op=ALU.add), reads=["ph2t%d" % sl, "pacc%d" % tt], writes=["ph2t%d" % sl])
                    S.dma("sp", ov[n], h2t[sl][:], reads=["ph2t%d" % sl], writes=["out"])


_PROG = {}


def kernel(**inputs):
    sh = host_shared(inputs)
    sh.update(host_shared_rest(inputs))
    if "nc" not in _PROG:
        _PROG["nc"] = build_program(("mixer", "mem", "peer"))[0]
    nc = _PROG["nc"]
    names = set(IN_SHAPES) | set(IN_SHAPES_MEM) | set(IN_SHAPES_PEER)
    mem = np.asarray(inputs["mem"], np.float32)
    maps = []
    for c in range(8):
        m = {k: v for k, v in sh.items() if k in names}
        m.update(host_core(inputs, c))
        m["mem"] = np.ascontiguousarray(mem[c // 4])
        maps.append(m)
    res = run_bass_kernel_spmd(nc, maps, core_ids=list(range(8)))
    out = np.zeros((2, 8192, 1024), np.float32)
    for c in range(8):
        out[c // 4, (c % 4) * 2048:(c % 4 + 1) * 2048] = res.results[c]["out"]
    return out
```

```python
from contextlib import ExitStack, contextmanager
import numpy as np
import concourse.bass as bass
import concourse.mybir as mybir
from concourse.bass_utils import run_bass_kernel_spmd

F32 = mybir.dt.float32
BF16 = mybir.dt.bfloat16
I32 = mybir.dt.int32
AF = mybir.ActivationFunctionType
ALU = mybir.AluOpType
AX = mybir.AxisListType

ENGS = ("pe", "act", "dve", "pool", "sp")
TWO_PI = 6.283185307179586
EPS = 1e-6
NEG = -30000.0


class Sched:
    def __init__(self, nc, es, n_dma_sems=32):
        self.nc = nc
        self.streams = {e: [] for e in ENGS}
        self.sem = {e: es.enter_context(nc.semaphore("s_" + e)) for e in ("pe", "act", "dve", "pool")}
        self.cnt = {e: 0 for e in ("pe", "act", "dve", "pool")}
        self.dsem = [es.enter_context(nc.semaphore("s_dma%d" % i)) for i in range(n_dma_sems)]
        self.dcnt = [0] * n_dma_sems
        self.dnext = 0
        self.n_sw = 4
        self.dnext_sw = 0
        self.waited = {}
        self.last_w = {}
        self.readers = {}
        self.n_ops = 0

    def _deps(self, eng, reads, writes):
        deps = []
        for k in reads:
            if k in self.last_w:
                deps.append(self.last_w[k])
        for k in writes:
            if k in self.last_w:
                deps.append(self.last_w[k])
            deps.extend(self.readers.get(k, ()))
        need = {}
        for (sk, val, peng) in deps:
            if peng == "pe" and eng == "pe":
                continue
            if self.waited.get((eng, sk), 0) >= val:
                continue
            if need.get(sk, 0) < val:
                need[sk] = val
        return need

    def _semobj(self, sk):
        return self.sem[sk] if isinstance(sk, str) else self.dsem[sk]

    def _emit_waits(self, eng, need):
        for sk, val in need.items():
            self.waited[(eng, sk)] = val
            so = self._semobj(sk)
            self.streams[eng].append(lambda e, so=so, val=val: e.wait_ge(so, val))

    def _record(self, tok, reads, writes):
        for k in writes:
            self.last_w[k] = tok
            self.readers[k] = []
        for k in reads:
            if k not in writes:
                self.readers.setdefault(k, []).append(tok)

    def op(self, eng, fn, reads=(), writes=()):
        need = self._deps(eng, reads, writes)
        self._emit_waits(eng, need)
        self.cnt[eng] += 1
        val = self.cnt[eng]
        so = self.sem[eng]
        self.streams[eng].append(lambda e, fn=fn, so=so: fn(e).then_inc(so, 1))
        self._record((eng, val, eng), reads, writes)
        self.n_ops += 1

    def dma(self, q, out, in_, reads=(), writes=(), **kw):
        nhw = len(self.dsem) - self.n_sw
        if q == "pool":
            i = nhw + self.dnext_sw
            self.dnext_sw = (self.dnext_sw + 1) % self.n_sw
        else:
            i = self.dnext
            self.dnext = (self.dnext + 1) % nhw
        need = self._deps(q, reads, writes)
        prev = 16 * self.dcnt[i]
        if prev and self.waited.get((q, i), 0) < prev:
            need[i] = max(need.get(i, 0), prev)
        self._emit_waits(q, need)
        self.dcnt[i] += 1
        val = 16 * self.dcnt[i]
        so = self.dsem[i]
        self.streams[q].append(
            lambda e, out=out, in_=in_, so=so, kw=kw: e.dma_start(out=out, in_=in_, **kw).then_inc(so, 16))
        self._record((i, val, "dma"), reads, writes)
        self.n_ops += 1

    def barrier(self):
        for eng in ENGS:
            need = {}
            for pe_ in ("pe", "act", "dve", "pool"):
                v = self.cnt[pe_]
                if v and self.waited.get((eng, pe_), 0) < v:
                    need[pe_] = v
            for i, c in enumerate(self.dcnt):
                if c and self.waited.get((eng, i), 0) < 16 * c:
                    need[i] = 16 * c
            self._emit_waits(eng, need)

    def wait_all(self, eng, keys):
        need = {}
        for k in keys:
            if k in self.last_w:
                sk, val, _ = self.last_w[k]
                if self.waited.get((eng, sk), 0) < val and need.get(sk, 0) < val:
                    need[sk] = val
        self._emit_waits(eng, need)

    def emit(self):
        if not any(self.streams[e] for e in ENGS):
            return
        streams = self.streams
        self.streams = {e: [] for e in ENGS}
        self._emit_block(streams)

    def _emit_block(self, streams):
        self_streams = streams
        with self.nc.Block() as block:
            @block.tensor
            def _(e):
                for f in self_streams["pe"]:
                    f(e)

            @block.scalar
            def _(e):
                for f in self_streams["act"]:
                    f(e)

            @block.vector
            def _(e):
                for f in self_streams["dve"]:
                    f(e)

            @block.gpsimd
            def _(e):
                for f in self_streams["pool"]:
                    f(e)

            @block.sync
            def _(e):
                for f in self_streams["sp"]:
                    f(e)


class Ctx:
    def __init__(self, nc, S):
        self.nc = nc
        self.S = S
        self.uid = 0

    def sb(self, es, name, shape, dt=F32):
        self.uid += 1
        return es.enter_context(self.nc.sbuf_tensor("%s_%d" % (name, self.uid), list(shape), dt))

    def ps(self, es, name, shape, dt=F32):
        self.uid += 1
        return es.enter_context(self.nc.psum_tensor("%s_%d" % (name, self.uid), list(shape), dt))


@contextmanager
def scope(C):
    with ExitStack() as es:
        yield es
        C.S.barrier()
        C.S.emit()


def bc(ap, shape):
    return ap.to_broadcast(list(shape))


def make_ident(C, es, name="ident"):
    S = C.S
    idf = C.sb(es, name + "f", [128, 128])
    idb = C.sb(es, name + "b", [128, 128], BF16)
    S.op("pool", lambda e: e.memset(idf[:], 1.0), writes=[name + "f"])
    S.op("pool", lambda e: e.affine_select(out=idf[:], in_=idf[:], pattern=[[-1, 128]], compare_op=ALU.is_equal,
                                           fill=0.0, base=0, channel_multiplier=1), reads=[name + "f"], writes=[name + "f"])
    S.op("dve", lambda e: e.tensor_copy(out=idb[:], in_=idf[:]), reads=[name + "f"], writes=[name + "b"])
    return idf, idb


def sincos(C, es, ang, n, tag):
    S = C.S
    outs = []
    for which, off in (("s", 64.0), ("c", 64.25)):
        k = tag + which
        y = C.sb(es, k + "y", [128, n]); yi = C.sb(es, k + "yi", [128, n], I32); yf = C.sb(es, k + "yf", [128, n])
        m = C.sb(es, k + "m", [128, n]); o = C.sb(es, k + "o", [128, n])
        S.op("dve", lambda e, y=y, off=off: e.tensor_scalar(out=y[:], in0=ang, scalar1=1.0 / TWO_PI, scalar2=off, op0=ALU.mult, op1=ALU.add),
             reads=[tag + "ang"], writes=[k + "y"])
        S.op("dve", lambda e, y=y, yi=yi: e.tensor_copy(out=yi[:], in_=y[:]), reads=[k + "y"], writes=[k + "yi"])
        S.op("dve", lambda e, yi=yi, yf=yf: e.tensor_copy(out=yf[:], in_=yi[:]), reads=[k + "yi"], writes=[k + "yf"])
        S.op("dve", lambda e, y=y, yf=yf: e.tensor_tensor(out=y[:], in0=y[:], in1=yf[:], op=ALU.subtract), reads=[k + "y", k + "yf"], writes=[k + "y"])
        S.op("dve", lambda e, y=y, m=m: e.tensor_scalar(out=m[:], in0=y[:], scalar1=0.5, scalar2=None, op0=ALU.is_gt), reads=[k + "y"], writes=[k + "m"])
        S.op("dve", lambda e, y=y, m=m: e.tensor_tensor(out=y[:], in0=y[:], in1=m[:], op=ALU.subtract), reads=[k + "y", k + "m"], writes=[k + "y"])
        S.op("act", lambda e, y=y, o=o: e.activation(out=o[:], in_=y[:], func=AF.Sin, scale=TWO_PI), reads=[k + "y"], writes=[k + "o"])
        outs.append((o, k + "o"))
    return outs


def s5_params(C, es_keep, D):
    S, nc = C.S, C.nc
    P = {}
    LT2 = C.sb(es_keep, "LT2", [128, 8, 8, 2, 128], BF16)
    WPr = C.sb(es_keep, "WPr", [128, 9, 16]); WPi = C.sb(es_keep, "WPi", [128, 9, 16]); WPn = C.sb(es_keep, "WPn", [128, 9, 16])
    P.update(LT2=LT2, WPr=WPr, WPi=WPi, WPn=WPn)
    with scope(C) as es:
        sb = lambda name, shape, dt=F32: C.sb(es, name, shape, dt)
        Mz2 = sb("Mz2", [128, 16, 8, 128], BF16)
        CA = sb("CA", [128, 32, 128], BF16); CAs = sb("CAs", [128, 32, 128], BF16)
        LR = sb("LR", [128, 32]); LI = sb("LI", [128, 32]); LS = sb("LS", [128, 32])
        SG = sb("SG", [128, 2]); NV = sb("NV", [128, 23])
        P1B = sb("P1B", [128, 32, 16]); P2B = sb("P2B", [128, 32, 16]); P1C = sb("P1C", [128, 32, 16]); P2C = sb("P2C", [128, 32, 16])
        DD = sb("DD", [128, 16, 16])
        for t, nm in ((LR, "s5_lr"), (LI, "s5_li"), (LS, "s5_ls"), (SG, "s5_sg"), (NV, "s5_nv")):
            S.dma("sp", t[:], D[nm], writes=[nm])
        S.dma("sp", DD[:].rearrange("p e c -> p (e c)"), D["s5_dd"], writes=["s5_dd"])
        for t, nm in ((P1B, "s5_p1b"), (P2B, "s5_p2b"), (P1C, "s5_p1c"), (P2C, "s5_p2c")):
            S.dma("sp", t[:].rearrange("p g c -> p (g c)"), D[nm], writes=[nm])
        idf, idb = make_ident(C, es, "pid")
        STEP = sb("STEP", [128, 32]); AA = sb("AA", [128, 32]); PH = sb("PH", [128, 32])
        S.op("act", lambda e: e.activation(out=STEP[:], in_=LS[:], func=AF.Exp), reads=["s5_ls"], writes=["STEP"])
        S.op("dve", lambda e: e.tensor_tensor(out=AA[:], in0=LR[:], in1=STEP[:], op=ALU.mult), reads=["s5_lr", "STEP"], writes=["AA"])
        S.op("dve", lambda e: e.tensor_tensor(out=PH[:], in0=LI[:], in1=STEP[:], op=ALU.mult), reads=["s5_li", "STEP"], writes=["PH"])
        EXPO = sb("EXPO", [128, 32, 23]); ANG = sb("ANG", [128, 32, 23]); MAG = sb("MAG", [128, 32, 23])
        nvb = bc(NV[:].unsqueeze(1), [128, 32, 23])
        S.op("dve", lambda e: e.tensor_tensor(out=EXPO[:], in0=bc(AA[:].unsqueeze(2), [128, 32, 23]), in1=nvb, op=ALU.mult), reads=["AA", "s5_nv"], writes=["EXPO"])
        S.op("dve", lambda e: e.tensor_tensor(out=ANG[:], in0=bc(PH[:].unsqueeze(2), [128, 32, 23]), in1=nvb, op=ALU.mult), reads=["PH", "s5_nv"], writes=["pwang"])
        S.op("act", lambda e: e.activation(out=MAG[:], in_=EXPO[:], func=AF.Exp), reads=["EXPO"], writes=["MAG"])
        (sn, snk), (cs, csk) = sincos(C, es, ANG[:].rearrange("p g n -> p (g n)"), 32 * 23, "pw")
        CR = sb("CR", [128, 32, 23]); CI = sb("CI", [128, 32, 23])
        S.op("dve", lambda e: e.tensor_tensor(out=CR[:].rearrange("p g n -> p (g n)"), in0=MAG[:].rearrange("p g n -> p (g n)"), in1=cs[:], op=ALU.mult), reads=["MAG", csk], writes=["CR"])
        S.op("dve", lambda e: e.tensor_tensor(out=CI[:].rearrange("p g n -> p (g n)"), in0=MAG[:].rearrange("p g n -> p (g n)"), in1=sn[:], op=ALU.mult), reads=["MAG", snk], writes=["CI"])
        zr = sb("zr", [128, 32]); den = sb("den", [128, 32]); t0 = sb("t0", [128, 32]); fr = sb("fr", [128, 32]); fi = sb("fi", [128, 32])
        S.op("dve", lambda e: e.tensor_scalar(out=zr[:], in0=CR[:, :, 8], scalar1=-1.0, scalar2=None, op0=ALU.add), reads=["CR"], writes=["zr"])
        S.op("dve", lambda e: e.tensor_tensor(out=den[:], in0=LR[:], in1=LR[:], op=ALU.mult), reads=["s5_lr"], writes=["den"])
        S.op("dve", lambda e: e.tensor_tensor(out=t0[:], in0=LI[:], in1=LI[:], op=ALU.mult), reads=["s5_li"], writes=["t0"])
        S.op("dve", lambda e: e.tensor_tensor(out=den[:], in0=den[:], in1=t0[:], op=ALU.add), reads=["den", "t0"], writes=["den"])
        S.op("dve", lambda e: e.reciprocal(out=den[:], in_=den[:]), reads=["den"], writes=["den"])
        S.op("dve", lambda e: e.tensor_tensor(out=fr[:], in0=zr[:], in1=LR[:], op=ALU.mult), reads=["zr", "s5_lr"], writes=["fr"])
        S.op("dve", lambda e: e.tensor_tensor(out=t0[:], in0=CI[:, :, 8], in1=LI[:], op=ALU.mult), reads=["CI", "s5_li", "den"], writes=["t0"])
        S.op("dve", lambda e: e.tensor_tensor(out=fr[:], in0=fr[:], in1=t0[:], op=ALU.add), reads=["fr", "t0"], writes=["fr"])
        S.op("dve", lambda e: e.tensor_tensor(out=fr[:], in0=fr[:], in1=den[:], op=ALU.mult), reads=["fr", "den"], writes=["fr"])
        S.op("dve", lambda e: e.tensor_tensor(out=fi[:], in0=CI[:, :, 8], in1=LR[:], op=ALU.mult), reads=["CI", "s5_lr"], writes=["fi"])
        S.op("dve", lambda e: e.tensor_tensor(out=t0[:], in0=zr[:], in1=LI[:], op=ALU.mult), reads=["zr", "s5_li", "fr"], writes=["t0"])
        S.op("dve", lambda e: e.tensor_tensor(out=fi[:], in0=fi[:], in1=t0[:], op=ALU.subtract), reads=["fi", "t0"], writes=["fi"])
        S.op("dve", lambda e: e.tensor_tensor(out=fi[:], in0=fi[:], in1=den[:], op=ALU.mult), reads=["fi", "den"], writes=["fi"])
        BB1 = sb("BB1", [128, 32, 16]); BB2 = sb("BB2", [128, 32, 16]); ta = sb("ta", [128, 32, 16]); tb = sb("tb", [128, 32, 16])
        frb = bc(fr[:].unsqueeze(2), [128, 32, 16]); fib = bc(fi[:].unsqueeze(2), [128, 32, 16])
        fl = lambda t: t[:].rearrange("p g c -> p (g c)")
        S.op("dve", lambda e: e.tensor_tensor(out=ta[:], in0=P1B[:], in1=frb, op=ALU.mult), reads=["s5_p1b", "fr"], writes=["ta"])
        S.op("dve", lambda e: e.tensor_tensor(out=tb[:], in0=P2B[:], in1=fib, op=ALU.mult), reads=["s5_p2b", "fi"], writes=["tb"])
        S.op("dve", lambda e: e.scalar_tensor_tensor(out=fl(BB1), in0=fl(tb), scalar=SG[:, 0:1], in1=fl(ta), op0=ALU.mult, op1=ALU.add), reads=["ta", "tb", "s5_sg"], writes=["BB1"])
        S.op("dve", lambda e: e.tensor_tensor(out=ta[:], in0=P2B[:], in1=frb, op=ALU.mult), reads=["s5_p2b", "fr", "BB1"], writes=["ta"])
        S.op("dve", lambda e: e.tensor_tensor(out=tb[:], in0=P1B[:], in1=fib, op=ALU.mult), reads=["s5_p1b", "fi", "BB1"], writes=["tb"])
        S.op("dve", lambda e: e.scalar_tensor_tensor(out=fl(BB2), in0=fl(tb), scalar=SG[:, 1:2], in1=fl(ta), op0=ALU.mult, op1=ALU.add), reads=["ta", "tb", "s5_sg"], writes=["BB2"])
        t5 = sb("t5", [128, 32, 16]); t6 = sb("t6", [128, 32, 16])
        Rm = sb("Rm", [128, 32, 8, 16])
        Q1 = sb("Q1", [128, 32, 16]); Q2 = sb("Q2", [128, 32, 16])
        S.op("dve", lambda e: e.tensor_scalar(out=fl(Q1), in0=fl(P1C), scalar1=SG[:, 1:2], scalar2=None, op0=ALU.mult), reads=["s5_p1c", "s5_sg"], writes=["Q1"])
        S.op("dve", lambda e: e.tensor_scalar(out=fl(Q2), in0=fl(P2C), scalar1=SG[:, 0:1], scalar2=None, op0=ALU.mult), reads=["s5_p2c", "s5_sg"], writes=["Q2"])
        CAv = CA[:].rearrange("p g (t c) -> p g t c", c=16); CAsv = CAs[:].rearrange("p g (t c) -> p g t c", c=16)
        for t in range(8):
            for (dst, dkey, a_, akey, b_, bkey, idx) in ((Rm[:, :, t, :], "Rm", Q1, "Q1", P2C, "s5_p2c", 7 + t),
                                                         (CAv[:, :, t, :], "CA", Q1, "Q1", P2C, "s5_p2c", 15 + t),
                                                         (CAsv[:, :, t, :], "CAs", Q2, "Q2", P1C, "s5_p1c", 15 + t)):
                S.op("dve", lambda e, a_=a_, idx=idx: e.tensor_tensor(out=t5[:], in0=a_[:], in1=bc(CR[:, :, idx:idx + 1], [128, 32, 16]), op=ALU.mult), reads=[akey, "CR"], writes=["t5"])
                S.op("dve", lambda e, b_=b_, idx=idx: e.tensor_tensor(out=t6[:], in0=b_[:], in1=bc(CI[:, :, idx:idx + 1], [128, 32, 16]), op=ALU.mult), reads=[bkey, "CI"], writes=["t6"])
                S.op("dve", lambda e, dst=dst: e.tensor_tensor(out=dst, in0=t5[:], in1=t6[:], op=ALU.subtract), reads=["t5", "t6"], writes=[dkey])
        Wr = sb("Wr", [128, 9, 32]); Wi = sb("Wi", [128, 9, 32]); sq = sb("sq", [128, 32])
        S.op("dve", lambda e: e.tensor_copy(out=Wr[:, 0, :], in_=CR[:, :, 15]), reads=["CR"], writes=["Wr"])
        S.op("dve", lambda e: e.tensor_copy(out=Wi[:, 0, :], in_=CI[:, :, 15]), reads=["CI"], writes=["Wi"])
        for j in range(8):
            S.op("dve", lambda e, j=j: e.tensor_tensor(out=Wr[:, j + 1, :], in0=Wr[:, j, :], in1=Wr[:, j, :], op=ALU.mult), reads=["Wr"], writes=["Wr"])
            S.op("dve", lambda e, j=j: e.tensor_tensor(out=sq[:], in0=Wi[:, j, :], in1=Wi[:, j, :], op=ALU.mult), reads=["Wi"], writes=["sq"])
            S.op("dve", lambda e, j=j: e.tensor_tensor(out=Wr[:, j + 1, :], in0=Wr[:, j + 1, :], in1=sq[:], op=ALU.subtract), reads=["Wr", "sq"], writes=["Wr"])
            S.op("dve", lambda e, j=j: e.scalar_tensor_tensor(out=Wi[:, j + 1, :], in0=Wr[:, j, :], scalar=2.0, in1=Wi[:, j, :], op0=ALU.mult, op1=ALU.mult), reads=["Wr", "Wi"], writes=["Wi"])
        Wrv = Wr[:].rearrange("p j (a r w) -> p j a r w", r=2, w=2); Wiv = Wi[:].rearrange("p j (a r w) -> p j a r w", r=2, w=2)
        for r in range(2):
            pr_ = slice(64 * r, 64 * r + 64)
            for j in range(9):
                S.op("dve", lambda e, r=r, pr_=pr_, j=j: e.tensor_copy(out=WPr[pr_, j, :].rearrange("p (a w) -> p a w", w=2), in_=Wrv[pr_, j, :, r, :]), reads=["Wr"], writes=["WPr"])
                S.op("dve", lambda e, r=r, pr_=pr_, j=j: e.tensor_copy(out=WPi[pr_, j, :].rearrange("p (a w) -> p a w", w=2), in_=Wiv[pr_, j, :, r, :]), reads=["Wi"], writes=["WPi"])
        S.op("dve", lambda e: e.tensor_scalar(out=WPn[:].rearrange("p j q -> p (j q)"), in0=WPi[:].rearrange("p j q -> p (j q)"), scalar1=-1.0, scalar2=None, op0=ALU.mult), reads=["WPi"], writes=["WPn"])
        E = [[sb("E%d%d" % (h_, r), [128, 128]) for r in range(2)] for h_ in range(2)]
        for h_ in range(2):
            for r in range(2):
                S.op("pool", lambda e, h_=h_, r=r: e.memset(E[h_][r][:], 0.0), writes=["E%d%d" % (h_, r)])
                S.op("pool", lambda e, h_=h_, r=r: e.tensor_copy(out=E[h_][r][64 * h_:64 * h_ + 64, 64 * r:64 * r + 64], in_=idf[64 * h_:64 * h_ + 64, 64 * h_:64 * h_ + 64]),
                     reads=["pidf", "E%d%d" % (h_, r)], writes=["E%d%d" % (h_, r)])
        Lz = [sb("Lz%d" % i, [128, 32, 64]) for i in range(2)]
        for i in range(2):
            S.op("pool", lambda e, i=i: e.memset(Lz[i][:].rearrange("p g c -> p (g c)"), 0.0), writes=["Lz%d" % i])
        S.op("pool", lambda e: e.memset(Mz2[:].rearrange("p a b c -> p (a b c)"), 0.0), writes=["Mz2"])
        t5v = t5[:].rearrange("p (a j) c -> p a j c", j=4); t6v = t6[:].rearrange("p (a j) c -> p a j c", j=4)
        with scope(C) as esp:
            PM = C.ps(esp, "PM", [128, 16, 128]); PL = C.ps(esp, "PL", [128, 8, 2, 128])
            for s in range(8):
                i = 7 - s; sl = s % 2; lk = "Lz%d" % sl
                Lzv = Lz[sl][:].rearrange("p (a j) c -> p a j c", j=4)
                S.op("dve", lambda e, i=i: e.tensor_tensor(out=t5[:], in0=BB1[:], in1=bc(CR[:, :, i:i + 1], [128, 32, 16]), op=ALU.mult), reads=["BB1", "CR"], writes=["t5"])
                S.op("dve", lambda e, i=i: e.tensor_tensor(out=t6[:], in0=BB2[:], in1=bc(CI[:, :, i:i + 1], [128, 32, 16]), op=ALU.mult), reads=["BB2", "CI"], writes=["t6"])
                for j4 in range(4):
                    S.op("dve", lambda e, j4=j4, Lzv=Lzv: e.scalar_tensor_tensor(out=Lzv[:, :, j4, 16 * j4:16 * j4 + 16], in0=t6v[:, :, j4, :], scalar=SG[:, 0:1], in1=t5v[:, :, j4, :], op0=ALU.mult, op1=ALU.add),
                         reads=["t5", "t6", "s5_sg"], writes=[lk])
                for g in range(32):
                    chc = g // 8; hb = (g % 8) // 4; j4 = g % 4; e_ = chc * 4 + j4
                    rows = slice(64 * hb, 64 * hb + 64)
                    S.op("pe", lambda e, g=g, sl=sl, e_=e_, rows=rows: e.matmul(PM[rows, e_, :], lhsT=Lz[sl][:, g, :], rhs=Rm[:, g, :, :].rearrange("p t c -> p (t c)"), start=True, stop=True),
                         reads=[lk, "Rm"], writes=["PM"])
                for pr in range(16):
                    a_ = pr // 2; wp = pr % 2; chc = a_ // 2; hb = a_ % 2; e2 = chc * 2 + wp
                    rows = slice(64 * hb, 64 * hb + 64)
                    for h_ in range(2):
                        for r in range(2):
                            g = 4 * a_ + 2 * r + wp
                            S.op("pe", lambda e, g=g, sl=sl, e2=e2, rows=rows, h_=h_, r=r: e.matmul(PL[rows, e2, h_, :], lhsT=Lz[sl][:, g, :], rhs=E[h_][r][:], start=(r == 0), stop=(r == 1)),
                                 reads=[lk, "E%d%d" % (h_, r)], writes=["PL"])
                S.op("dve", lambda e, s=s: e.tensor_copy(out=Mz2[:, :, s, 16 * s:128], in_=PM[:, :, 16 * s:128]), reads=["PM"], writes=["Mz2"])
                S.op("dve", lambda e, s=s: e.tensor_tensor(out=Mz2[:, :, s, 16 * s:16 * s + 16], in0=PM[:, :, 16 * s:16 * s + 16], in1=DD[:], op=ALU.add), reads=["PM", "s5_dd"], writes=["Mz2"])
                S.op("act", lambda e, s=s: e.copy(out=LT2[:, :, s, :, :], in_=PL[:]), reads=["PL"], writes=["LT2"])
        S.dma("sp", D["Mz_d"], Mz2[:].rearrange("p a b c -> p (a b c)"), reads=["Mz2"], writes=["Mz_d"])
        S.dma("sp", D["CA_d"][:, 0, :], CA[:].rearrange("p g c -> p (g c)"), reads=["CA"], writes=["CA_d"])
        S.dma("sp", D["CA_d"][:, 1, :], CAs[:].rearrange("p g c -> p (g c)"), reads=["CAs"], writes=["CA_d"])
    return P


def load_weight_bf16(C, es, es_tmp, name, src, rows_chunks, ncols, gcol=None, q="sp"):
    S = C.S
    W = C.sb(es, name, [128, rows_chunks, ncols], BF16)
    stg = [C.sb(es_tmp, name + "_stg%d" % i, [128, ncols]) for i in range(2)]
    srcv = src.rearrange("(c p) n -> c p n", p=128)
    for c in range(rows_chunks):
        st = stg[c % 2]; sk = name + "_stg%d" % (c % 2)
        S.dma(q, st[:], srcv[c], writes=[sk])
        wk_ = "%s_c%d" % (name, c)
        if c % 2 == 0:
            if gcol is not None:
                S.op("dve", lambda e, st=st, c=c: e.tensor_scalar(out=W[:, c, :], in0=st[:], scalar1=gcol[0][:, c:c + 1], scalar2=None, op0=ALU.mult),
                     reads=[sk, gcol[1]], writes=[name])
            else:
                S.op("dve", lambda e, st=st, c=c: e.tensor_copy(out=W[:, c, :], in_=st[:]), reads=[sk], writes=[name])
        else:
            if gcol is not None:
                S.op("act", lambda e, st=st, c=c: e.activation(out=W[:, c, :], in_=st[:], func=AF.Copy, scale=gcol[0][:, c:c + 1]),
                     reads=[sk, gcol[1]], writes=[name])
            else:
                S.op("act", lambda e, st=st, c=c: e.copy(out=W[:, c, :], in_=st[:]), reads=[sk], writes=[name])
    return W


def rms_rstd(C, x_ap, xkey, junk, jkey, ss, rs, skey, n):
    S = C.S
    S.op("act", lambda e: e.activation(out=junk, in_=x_ap, func=AF.Square, accum_out=ss), reads=[xkey], writes=[skey + "_ss"])
    S.op("act", lambda e: e.activation(out=rs, in_=ss, func=AF.Sqrt, scale=1.0 / n, bias=EPS), reads=[skey + "_ss"], writes=[skey + "_sq"])
    S.op("dve", lambda e: e.reciprocal(out=rs, in_=rs), reads=[skey + "_sq"], writes=[skey])


def transpose_chunks(C, src, skey, nch, pbank, pkey, dst, dkey, idb, evac="act"):
    S = C.S
    for c in range(nch):
        S.op("pe", lambda e, c=c: e.transpose(out=pbank[:, c, :], in_=src[:, c * 128:(c + 1) * 128], identity=idb[:]), reads=[skey, "identb"], writes=[pkey])
    if evac == "act":
        S.op("act", lambda e: e.copy(out=dst, in_=pbank), reads=[pkey], writes=[dkey])
    else:
        S.op(evac, lambda e: e.tensor_copy(out=dst, in_=pbank), reads=[pkey], writes=[dkey])


def stage_mixer(C, D, dbg=None, upto=9, prep=None):
    S, nc = C.S, C.nc
    dbg = dbg or {}
    with scope(C) as es1:
        P = s5_params(C, es1, D)
        idf = C.sb(es1, "identf", [128, 128]); idb = C.sb(es1, "identb", [128, 128], BF16)
        S.op("pool", lambda e: e.memset(idf[:], 1.0), writes=["identf"])
        S.op("pool", lambda e: e.affine_select(out=idf[:], in_=idf[:], pattern=[[-1, 128]], compare_op=ALU.is_equal, fill=0.0, base=0, channel_multiplier=1), reads=["identf"], writes=["identf"])
        S.op("dve", lambda e: e.tensor_copy(out=idb[:], in_=idf[:]), reads=["identf"], writes=["identb"])
        if "WPr" in dbg:
            for nm in ("WPr", "WPi"):
                S.dma("sp", dbg[nm], P[nm][:].rearrange("p a b -> p (a b)"), reads=[nm], writes=["o_" + nm])
            S.dma("pool", dbg["LT2"], P["LT2"][:].rearrange("p a b c d -> p (a b c d)"), reads=["LT2"], writes=["o_LT2"])
            S.dma("pool", dbg["Mz"], D["Mz_d"], reads=["Mz_d"], writes=["o_Mz"])
            S.dma("pool", dbg["CA"], D["CA_d"].rearrange("p a b -> p (a b)"), reads=["CA_d"], writes=["o_CA"])
        if upto < 1:
            return
        carry_r = C.sb(es1, "carry_r", [128, 16]); carry_i = C.sb(es1, "carry_i", [128, 16])
        S.op("pool", lambda e: e.memset(carry_r[:], 0.0), writes=["carry_r"])
        S.op("pool", lambda e: e.memset(carry_i[:], 0.0), writes=["carry_i"])
        TRE = C.sb(es1, "TRE", [128, 16, 257]); TIM = C.sb(es1, "TIM", [128, 16, 257])
        uT = C.sb(es1, "uT", [128, 4, 8, 256], BF16)
        with scope(C) as es2:
            _mixer_passes(C, es2, D, P, idb, carry_r, carry_i, TRE, TIM, uT, dbg)
        if "carry" in dbg:
            S.dma("sp", dbg["carry"][:, 0:16], carry_r[:], reads=["carry_r"], writes=["o_carry"])
            S.dma("sp", dbg["carry"][:, 16:32], carry_i[:], reads=["carry_i"], writes=["o_carry2"])
        if upto < 2:
            return
        with scope(C) as es3:
            ytm = C.sb(es3, "ytm", [128, 2, 8, 512], BF16)
            with scope(C) as es4:
                _s5_scan_out(C, es4, D, P, idb, TRE, TIM, uT, ytm, carry_r, carry_i, dbg)
            if upto < 3:
                return
            _s5_glu_out(C, es3, D, idb, ytm, dbg)
    if upto < 4:
        return
    with scope(C) as es5:
        idf = C.sb(es5, "identf", [128, 128]); idb = C.sb(es5, "identb", [128, 128], BF16)
        S.op("pool", lambda e: e.memset(idf[:], 1.0), reads=[], writes=["identf"])
        S.op("pool", lambda e: e.affine_select(out=idf[:], in_=idf[:], pattern=[[-1, 128]], compare_op=ALU.is_equal, fill=0.0, base=0, channel_multiplier=1), reads=["identf"], writes=["identf"])
        S.op("dve", lambda e: e.tensor_copy(out=idb[:], in_=idf[:]), reads=["identf"], writes=["identb"])
        _attention(C, es5, D, idb, dbg, prep)


def _mixer_passes(C, es, D, P, idb, carry_r, carry_i, TRE, TIM, uT, dbg):
    S = C.S
    sb = lambda name, shape, dt=F32: C.sb(es, name, shape, dt)
    gin = sb("gin", [128, 8])
    S.dma("sp", gin[:], D["g_mix"], writes=["gin"])
    with scope(C) as est:
        Wb = load_weight_bf16(C, es, est, "Wb", D["w_in"], 8, 2048, gcol=(gin, "gin"))
    gq = sb("gq", [128, 64]); gk = sb("gk", [128, 64]); hv = sb("hv", [128, 4])
    S.dma("sp", gq[:], D["att_q_g"].partition_broadcast(128), writes=["gq"])
    S.dma("sp", gk[:], D["att_k_g"].partition_broadcast(128), writes=["gk"])
    S.dma("sp", hv[:], D["hvalid"], writes=["hv"])
    S.op("dve", lambda e: e.tensor_scalar(out=gq[:], in0=gq[:], scalar1=0.125, scalar2=None, op0=ALU.mult), reads=["gq"], writes=["gq"])
    xt = [sb("xt%d" % i, [128, 1024]) for i in range(2)]
    junk = sb("junk", [128, 1024]); st = [sb("st%d" % i, [128, 4]) for i in range(2)]
    xn = [sb("xn%d" % i, [128, 1024], BF16) for i in range(2)]
    xnT = [sb("xnT%d" % i, [128, 8, 512], BF16) for i in range(2)]
    qkv = sb("qkv", [128, 3, 512]); sq = sb("sq2", [128, 512]); qst = sb("qst", [128, 4, 8])
    qn = sb("qn", [128, 2, 512], BF16)
    kTs = [sb("kTs%d" % i, [128, 4, 128], BF16) for i in range(2)]; qTs = [sb("qTs%d" % i, [128, 4, 128], BF16) for i in range(2)]
    Vs = [sb("Vs%d" % i, [128, 8, 65], BF16) for i in range(2)]
    tBr = sb("tBr", [128, 8, 128]); tBi = sb("tBi", [128, 8, 128]); tCr = sb("tCr", [128, 8, 64]); tCi = sb("tCi", [128, 8, 64])
    tt1 = sb("tt1", [128, 8, 128]); tt2 = sb("tt2", [128, 8, 128]); tt3 = sb("tt3", [128, 8, 128]); tt4 = sb("tt4", [128, 8, 128])
    REDr = sb("REDr", [128, 16]); REDi = sb("REDi", [128, 16]); c1 = sb("c1", [128, 16]); c2 = sb("c2", [128, 16]); c3 = sb("c3", [128, 16])
    with scope(C) as esp:
        bank = [C.ps(esp, "bk%d" % i, [128, 512]) for i in range(8)]
        xv = D["x_ext"].rearrange("(n p) d -> n p d", p=128)
        tile_ctr = 0
        for q in range(4):
            for blk in range(4):
                bslot = (q * 4 + blk) % 2
                xT = xnT[bslot]; xTk = "xnT%d" % bslot
                for tt in range(4):
                    n_tile = q * 16 + blk * 4 + tt
                    sl = tile_ctr % 2; tile_ctr += 1
                    x_ = xt[sl]; xk = "xt%d" % sl
                    S.dma("sp", x_[:], xv[n_tile], writes=[xk])
                    rms_rstd(C, x_[:], xk, junk[:], "junk", st[sl][:, 0:1], st[sl][:, 1:2], "st%d" % sl, 1024)
                    S.op("dve", lambda e, x_=x_, sl=sl: e.tensor_scalar(out=xn[sl][:], in0=x_[:], scalar1=st[sl][:, 1:2], scalar2=None, op0=ALU.mult),
                         reads=[xk, "st%d" % sl], writes=["xn%d" % sl])
                    tb_ = 0 if sl == 0 else 7
                    pb = bank[tb_][:].bitcast(BF16).rearrange("p (c n) -> p c n", n=128)
                    transpose_chunks(C, xn[sl][:], "xn%d" % sl, 8, pb, "bk%d" % tb_, xT[:, :, tt * 128:(tt + 1) * 128], xTk, idb, evac=("act" if sl == 0 else "dve"))
                for chc in range(4):
                    bi = 1 + (chc % 2); bkk = "bk%d" % bi
                    for dc in range(8):
                        S.op("pe", lambda e, chc=chc, dc=dc, bi=bi, xT=xT: e.matmul(bank[bi][:], lhsT=Wb[:, dc, 1536 + chc * 128:1536 + (chc + 1) * 128], rhs=xT[:, dc, :], start=(dc == 0), stop=(dc == 7)),
                             reads=["Wb", xTk], writes=[bkk])
                    eng = "act" if chc % 2 == 0 else "dve"
                    src = bank[bi][:].rearrange("p (k s) -> p s k", s=8)
                    dst = uT[:, chc, :, blk * 64:(blk + 1) * 64]
                    if eng == "act":
                        S.op("act", lambda e, src=src, dst=dst: e.copy(out=dst, in_=src), reads=[bkk], writes=["uT"])
                    else:
                        S.op("dve", lambda e, src=src, dst=dst: e.tensor_copy(out=dst, in_=src), reads=[bkk], writes=["uT"])
                need_kv = (q == 3) or (q == 2 and blk == 3)
                need_q = (q == 3)
                if need_kv:
                    for tt in range(4):
                        n_tile = q * 16 + blk * 4 + tt
                        kvt = n_tile - 44
                        sl = kvt % 2
                        projs = [(1, 512, 3), (2, 1024, 4)] + ([(0, 0, 5)] if need_q else [])
                        for (pi, c0, bi) in projs:
                            for dc in range(8):
                                S.op("pe", lambda e, dc=dc, bi=bi, c0=c0, tt=tt, xT=xT: e.matmul(bank[bi][:], lhsT=xT[:, dc, tt * 128:(tt + 1) * 128], rhs=Wb[:, dc, c0:c0 + 512], start=(dc == 0), stop=(dc == 7)),
                                     reads=["Wb", xTk], writes=["bk%d" % bi])
                        S.op("act", lambda e, sl=sl: e.copy(out=Vs[sl][:, :, 0:64], in_=bank[4][:].rearrange("p (h d) -> p h d", d=64)), reads=["bk4"], writes=["Vs%d" % sl])
                        if kvt < 4:
                            S.op("pool", lambda e, sl=sl, kvt=kvt: e.tensor_copy(out=Vs[sl][:, :, 64], in_=bc(hv[:, kvt:kvt + 1], [128, 8])), reads=["hv"], writes=["Vs%d" % sl])
                        else:
                            S.op("pool", lambda e, sl=sl: e.memset(Vs[sl][:, :, 64], 1.0), reads=[], writes=["Vs%d" % sl])
                        S.dma("sp", D["V_d"][kvt], Vs[sl][:].rearrange("p h d -> p (h d)"), reads=["Vs%d" % sl], writes=["V_d"])
                        for (pi, bi, gt, gkey, dstT, dkey, dram, ncol_t) in ([(1, 3, gk, "gk", kTs[sl], "kTs%d" % sl, D["kT_d"], kvt)] +
                                                                          ([(0, 5, gq, "gq", qTs[sl], "qTs%d" % sl, D["qT_d"], kvt - 4)] if need_q else [])):
                            qs = qkv[:, pi, :]; qk_ = "qkv%d" % pi
                            S.op("act", lambda e, qs=qs, bi=bi: e.copy(out=qs, in_=bank[bi][:]), reads=["bk%d" % bi], writes=[qk_])
                            S.op("pool", lambda e, qs=qs: e.tensor_tensor(out=sq[:], in0=qs, in1=qs, op=ALU.mult), reads=[qk_], writes=["sq2"])
                            S.op("dve", lambda e, pi=pi: e.tensor_reduce(out=qst[:, pi, :], in_=sq[:].rearrange("p (h d) -> p h d", d=64), axis=AX.X, op=ALU.add), reads=["sq2"], writes=["qst%d" % pi])
                            S.op("act", lambda e, pi=pi: e.activation(out=qst[:, 2 + pi, :], in_=qst[:, pi, :], func=AF.Sqrt, scale=1.0 / 64, bias=EPS), reads=["qst%d" % pi], writes=["qsq%d" % pi])
                            S.op("dve", lambda e, pi=pi: e.reciprocal(out=qst[:, 2 + pi, :], in_=qst[:, 2 + pi, :]), reads=["qsq%d" % pi], writes=["qrs%d" % pi])
                            S.op("dve", lambda e, qs=qs, pi=pi: e.tensor_tensor(out=qs.rearrange("p (h d) -> p h d", d=64), in0=qs.rearrange("p (h d) -> p h d", d=64),
                                                                        in1=bc(qst[:, 2 + pi, :].unsqueeze(2), [128, 8, 64]), op=ALU.mult), reads=[qk_, "qrs%d" % pi], writes=[qk_])
                            S.op("pool", lambda e, qs=qs, pi=pi, gt=gt: e.tensor_tensor(out=qn[:, pi, :].rearrange("p (h d) -> p h d", d=64), in0=qs.rearrange("p (h d) -> p h d", d=64),
                                                                               in1=bc(gt[:].unsqueeze(1), [128, 8, 64]), op=ALU.mult), reads=[qk_, gkey], writes=["qn%d" % pi])
                            pb = bank[6][:].bitcast(BF16).rearrange("p (c n) -> p c n", n=128)[:, 0:4, :]
                            transpose_chunks(C, qn[:, pi, :], "qn%d" % pi, 4, pb, "bk6", dstT[:], dkey, idb)
                            S.dma("sp", dram.rearrange("p (c n) -> p c n", c=4)[:, :, ncol_t * 128:(ncol_t + 1) * 128], dstT[:], reads=[dkey], writes=["qkT_d"])
            for pair in range(16):
                psl = pair % 2
                br = bank[1 + 2 * psl]; bim = bank[2 + 2 * psl]; brk = "bk%d" % (1 + 2 * psl); bik = "bk%d" % (2 + 2 * psl)
                a_ = pair // 2; wp = pair % 2; chc = a_ // 2; hb = a_ % 2; e2 = chc * 2 + wp
                rows = slice(64 * hb, 64 * hb + 64)
                for half, (bkt, bkk) in enumerate(((br, brk), (bim, bik))):
                    for s in range(8):
                        S.op("pe", lambda e, e2=e2, s=s, half=half, rows=rows, chc=chc, bkt=bkt: e.matmul(bkt[:, 0:256], lhsT=P["LT2"][rows, e2, s, half, :], rhs=uT[rows, chc, s, :], start=(s == 0), stop=(s == 7)),
                             reads=["LT2", "uT"], writes=[bkk])
                S.op("act", lambda e, pair=pair, br=br: e.copy(out=TRE[:, pair, 1:257], in_=br[:, 0:256]), reads=[brk], writes=["TRE%d" % pair])
                S.op("act", lambda e, pair=pair, bim=bim: e.copy(out=TIM[:, pair, 1:257], in_=bim[:, 0:256]), reads=[bik], writes=["TIM%d" % pair])
            if q < 3:
                for p0 in (0, 8):
                    kre = ["TRE%d" % p for p in range(p0, p0 + 8)]; kim = ["TIM%d" % p for p in range(p0, p0 + 8)]
                    src_r, src_i, srk, sik = TRE[:, p0:p0 + 8, 1:257], TIM[:, p0:p0 + 8, 1:257], kre, kim
                    bufs = [(tBr[:], tBi[:], ["tBr"], ["tBi"]), (tCr[:], tCi[:], ["tCr"], ["tCi"])]
                    for j in range(8):
                        n = 256 >> j; h = n // 2
                        wrb = bc(P["WPr"][:, j, p0:p0 + 8].unsqueeze(2), [128, 8, h]); wib = bc(P["WPi"][:, j, p0:p0 + 8].unsqueeze(2), [128, 8, h])
                        sre, sro = src_r[:, :, 0:n:2], src_r[:, :, 1:n:2]; sie, sio = src_i[:, :, 0:n:2], src_i[:, :, 1:n:2]
                        if j == 7:
                            dr, di, drk, dik = REDr[:, p0:p0 + 8].unsqueeze(2), REDi[:, p0:p0 + 8].unsqueeze(2), ["REDr%d" % p0], ["REDi%d" % p0]
                        else:
                            bb = bufs[j % 2]
                            dr, di, drk, dik = bb[0][:, :, 0:h], bb[1][:, :, 0:h], bb[2], bb[3]
                        t1v, t2v, t3v, t4v = tt1[:, :, 0:h], tt2[:, :, 0:h], tt3[:, :, 0:h], tt4[:, :, 0:h]
                        S.op("dve", lambda e, t1v=t1v, sre=sre, wrb=wrb: e.tensor_tensor(out=t1v, in0=sre, in1=wrb, op=ALU.mult), reads=srk + ["WPr"], writes=["tt1"])
                        S.op("pool", lambda e, t2v=t2v, sie=sie, wib=wib: e.tensor_tensor(out=t2v, in0=sie, in1=wib, op=ALU.mult), reads=sik + ["WPi"], writes=["tt2"])
                        S.op("pool", lambda e, t3v=t3v, sie=sie, wrb=wrb: e.tensor_tensor(out=t3v, in0=sie, in1=wrb, op=ALU.mult), reads=sik + ["WPr"], writes=["tt3"])
                        S.op("dve", lambda e, t4v=t4v, sre=sre, wib=wib: e.tensor_tensor(out=t4v, in0=sre, in1=wib, op=ALU.mult), reads=srk + ["WPi"], writes=["tt4"])
                        S.op("dve", lambda e, t1v=t1v, t2v=t2v: e.tensor_tensor(out=t1v, in0=t1v, in1=t2v, op=ALU.subtract), reads=["tt1", "tt2"], writes=["tt1"])
                        S.op("pool", lambda e, t3v=t3v, t4v=t4v: e.tensor_tensor(out=t3v, in0=t3v, in1=t4v, op=ALU.add), reads=["tt3", "tt4"], writes=["tt3"])
                        S.op("dve", lambda e, dr=dr, t1v=t1v, sro=sro: e.tensor_tensor(out=dr, in0=t1v, in1=sro, op=ALU.add), reads=["tt1"] + srk, writes=drk)
                        S.op("pool", lambda e, di=di, t3v=t3v, sio=sio: e.tensor_tensor(out=di, in0=t3v, in1=sio, op=ALU.add), reads=["tt3"] + sik, writes=dik)
                        if j < 7:
                            src_r, src_i, srk, sik = bb[0], bb[1], bb[2], bb[3]
            if q < 3:
                w8r = P["WPr"][:, 8, :]; w8i = P["WPi"][:, 8, :]
                S.op("dve", lambda e: e.tensor_tensor(out=c1[:], in0=w8r, in1=carry_r[:], op=ALU.mult), reads=["WPr", "carry_r"], writes=["c1"])
                S.op("dve", lambda e: e.tensor_tensor(out=c2[:], in0=w8i, in1=carry_i[:], op=ALU.mult), reads=["WPi", "carry_i"], writes=["c2"])
                S.op("dve", lambda e: e.tensor_tensor(out=c1[:], in0=c1[:], in1=c2[:], op=ALU.subtract), reads=["c1", "c2"], writes=["c1"])
                S.op("dve", lambda e: e.tensor_tensor(out=c1[:], in0=c1[:], in1=REDr[:], op=ALU.add), reads=["c1", "REDr0", "REDr8"], writes=["c1"])
                S.op("dve", lambda e: e.tensor_tensor(out=c2[:], in0=w8r, in1=carry_i[:], op=ALU.mult), reads=["WPr", "carry_i", "c1"], writes=["c2"])
                S.op("dve", lambda e: e.tensor_tensor(out=c3[:], in0=w8i, in1=carry_r[:], op=ALU.mult), reads=["WPi", "carry_r"], writes=["c3"])
                S.op("dve", lambda e: e.tensor_tensor(out=c2[:], in0=c2[:], in1=c3[:], op=ALU.add), reads=["c2", "c3"], writes=["c2"])
                S.op("dve", lambda e: e.tensor_tensor(out=carry_i[:], in0=c2[:], in1=REDi[:], op=ALU.add), reads=["c2", "REDi0", "REDi8"], writes=["carry_i"])
                S.op("dve", lambda e: e.tensor_copy(out=carry_r[:], in_=c1[:]), reads=["c1"], writes=["carry_r"])


def _s5_scan_out(C, es, D, P, idb, TRE, TIM, uT, ytm, carry_r, carry_i, dbg):
    S = C.S
    sb = lambda name, shape, dt=F32: C.sb(es, name, shape, dt)
    Mz2 = sb("Mz2s", [128, 16, 8, 128], BF16); CAA = sb("CAA", [128, 2, 32, 128], BF16)
    S.dma("sp", Mz2[:].rearrange("p a b c -> p (a b c)"), D["Mz_d"], reads=["Mz_d"], writes=["Mz2s"])
    S.dma("sp", CAA[:].rearrange("p a g c -> p a (g c)"), D["CA_d"], reads=["CA_d"], writes=["CAA"])
    hs1 = sb("hs1", [128, 8, 256]); hs2 = sb("hs2", [128, 8, 256]); hs3 = sb("hs3", [128, 8, 256]); hs4 = sb("hs4", [128, 8, 256])
    Tbr = [sb("Tbr%d" % i, [128, 256], BF16) for i in range(2)]; Tbi = [sb("Tbi%d" % i, [128, 256], BF16) for i in range(2)]
    Yg = [sb("Yg%d" % i, [128, 256], BF16) for i in range(2)]
    ysum = [sb("ysum%d" % i, [128, 256]) for i in range(2)]
    import os
    CUT = int(os.environ.get("SCAN_CUT", "9"))
    for p0 in (0, 8):
        kre = ["TRE%d" % p for p in range(p0, p0 + 8)]; kim = ["TIM%d" % p for p in range(p0, p0 + 8)]
        S.op("dve", lambda e, p0=p0: e.tensor_copy(out=TRE[:, p0:p0 + 8, 0:1], in_=carry_r[:, p0:p0 + 8].unsqueeze(2)), reads=["carry_r"], writes=kre)
        S.op("pool", lambda e, p0=p0: e.tensor_copy(out=TIM[:, p0:p0 + 8, 0:1], in_=carry_i[:, p0:p0 + 8].unsqueeze(2)), reads=["carry_i"], writes=kim)
        for j in range(9):
            d = 1 << j; m = 257 - d
            wrb = bc(P["WPr"][:, j, p0:p0 + 8].unsqueeze(2), [128, 8, m]); wib = bc(P["WPi"][:, j, p0:p0 + 8].unsqueeze(2), [128, 8, m])
            R0 = TRE[:, p0:p0 + 8, 0:m]; I0 = TIM[:, p0:p0 + 8, 0:m]; R1 = TRE[:, p0:p0 + 8, d:257]; I1 = TIM[:, p0:p0 + 8, d:257]
            h1, h2, h3, h4 = hs1[:, :, 0:m], hs2[:, :, 0:m], hs3[:, :, 0:m], hs4[:, :, 0:m]
            S.op("dve", lambda e, h1=h1, R0=R0, wrb=wrb: e.tensor_tensor(out=h1, in0=R0, in1=wrb, op=ALU.mult), reads=kre + ["WPr"], writes=["hs1"])
            S.op("pool", lambda e, h2=h2, I0=I0, wib=wib: e.tensor_tensor(out=h2, in0=I0, in1=wib, op=ALU.mult), reads=kim + ["WPi"], writes=["hs2"])
            S.op("pool", lambda e, h3=h3, I0=I0, wrb=wrb: e.tensor_tensor(out=h3, in0=I0, in1=wrb, op=ALU.mult), reads=kim + ["WPr"], writes=["hs3"])
            S.op("dve", lambda e, h4=h4, R0=R0, wib=wib: e.tensor_tensor(out=h4, in0=R0, in1=wib, op=ALU.mult), reads=kre + ["WPi"], writes=["hs4"])
            S.op("dve", lambda e, h1=h1, h2=h2: e.tensor_tensor(out=h1, in0=h1, in1=h2, op=ALU.subtract), reads=["hs1", "hs2"], writes=["hs1"])
            S.op("pool", lambda e, h3=h3, h4=h4: e.tensor_tensor(out=h3, in0=h3, in1=h4, op=ALU.add), reads=["hs3", "hs4"], writes=["hs3"])
            S.op("dve", lambda e, R1=R1, h1=h1: e.tensor_tensor(out=R1, in0=R1, in1=h1, op=ALU.add), reads=kre + ["hs1"], writes=kre)
            S.op("pool", lambda e, I1=I1, h3=h3: e.tensor_tensor(out=I1, in0=I1, in1=h3, op=ALU.add), reads=kim + ["hs3"], writes=kim)
    with scope(C) as esp:
        bank = [C.ps(esp, "sbk%d" % i, [128, 512]) for i in range(6)]
        for pair in range(16):
            sl = pair % 2
            kr, ki = "TRE%d" % pair, "TIM%d" % pair
            a_ = pair // 2; wp = pair % 2; chc = a_ // 2; hb = a_ % 2
            rows = slice(64 * hb, 64 * hb + 64)
            kr, ki = "TRE%d" % pair, "TIM%d" % pair
            if CUT < 2:
                continue
            S.op("act", lambda e, pair=pair, sl=sl: e.copy(out=Tbr[sl][:], in_=TRE[:, pair, 0:256]), reads=[kr], writes=["Tbr%d" % sl])
            S.op("act", lambda e, pair=pair, sl=sl: e.copy(out=Tbi[sl][:], in_=TIM[:, pair, 0:256]), reads=[ki], writes=["Tbi%d" % sl])
            for r in range(2):
                g = 4 * a_ + 2 * r + wp
                e_ = chc * 4 + (g % 4)
                pr = slice(64 * r, 64 * r + 64)
                yb = bank[r]; ybk = "sbk%d" % r
                for s in range(8):
                    S.op("pe", lambda e, e_=e_, s=s, rows=rows, chc=chc, yb=yb: e.matmul(yb[:, 0:256], lhsT=Mz2[rows, e_, s, :], rhs=uT[rows, chc, s, :], start=(s == 0), stop=(s == 7)),
                         reads=["Mz2s", "uT"], writes=[ybk])
                zb_ = bank[4 + r]; zbk = "sbk%d" % (4 + r)
                S.op("pe", lambda e, g=g, pr=pr, zb_=zb_, r=r, sl=sl: e.matmul(zb_[:, 0:256], lhsT=CAA[pr, r, g, :], rhs=Tbr[sl][pr, :], start=True, stop=False), reads=["CAA", "Tbr%d" % sl], writes=[zbk])
                S.op("pe", lambda e, g=g, pr=pr, zb_=zb_, r=r, sl=sl: e.matmul(zb_[:, 0:256], lhsT=CAA[pr, 1 - r, g, :], rhs=Tbi[sl][pr, :], start=False, stop=True), reads=["CAA", "Tbi%d" % sl], writes=[zbk])
                if CUT < 3:
                    continue
                S.op("act", lambda e, r=r, zb_=zb_: e.copy(out=ysum[r][:], in_=zb_[:, 0:256]), reads=[zbk], writes=["ysum%d" % r])
                S.op("dve", lambda e, r=r, yb=yb: e.tensor_tensor(out=ysum[r][:], in0=yb[:, 0:256], in1=ysum[r][:], op=ALU.add), reads=[ybk, "ysum%d" % r], writes=["ysum%d" % r])
                S.op("act", lambda e, r=r: e.activation(out=Yg[r][:], in_=ysum[r][:], func=AF.Gelu), reads=["ysum%d" % r], writes=["Yg%d" % r])
                if CUT < 4:
                    continue
                pT = bank[2 + r][:].bitcast(BF16).rearrange("p (c n) -> p c n", n=128)
                for kb in range(2):
                    S.op("pe", lambda e, r=r, kb=kb, pT=pT: e.transpose(out=pT[:, kb, :], in_=Yg[r][:, kb * 128:(kb + 1) * 128], identity=idb[:]), reads=["Yg%d" % r, "identb"], writes=["sbk%d" % (2 + r)])
                for kb in range(2):
                    S.op("dve", lambda e, g=g, kb=kb, pT=pT: e.tensor_copy(out=ytm[:, kb, :, 16 * g:16 * g + 16], in_=pT[:, kb, :].rearrange("p (t c) -> p t c", c=16)),
                         reads=["sbk%d" % (2 + r)], writes=["ytm"])


def _s5_glu_out(C, es, D, idb, ytm, dbg):
    S = C.S
    sb = lambda name, shape, dt=F32: C.sb(es, name, shape, dt)
    gso = sb("gso", [128, 4]); bgl = sb("bgl", [128, 512])
    S.dma("sp", gso[:], D["g_ssm_out"], writes=["gso"])
    S.dma("sp", bgl[:], D["b_glu"].partition_broadcast(128), writes=["bgl"])
    with scope(C) as est:
        Wg = load_weight_bf16(C, es, est, "Wg", D["w_glu"], 4, 512)
    with scope(C) as est:
        Wo = load_weight_bf16(C, es, est, "Wos", D["w_out"][512:1024, :], 4, 1024, gcol=(gso, "gso"))
    yT = sb("yT", [128, 4, 128], BF16); zb = sb("zb", [128, 512]); ssm = sb("ssm", [128, 512]); junk = sb("junk3", [128, 512])
    st = sb("st3", [128, 2]); sn = sb("sn", [128, 512], BF16); snT = sb("snT", [128, 4, 128], BF16)
    ho = [sb("ho%d" % i, [128, 1024]) for i in range(2)]
    hsv = D["hs_d"].rearrange("(k t) d -> t k d", t=8)
    with scope(C) as esp:
        bank = [C.ps(esp, "gbk%d" % i, [128, 512]) for i in range(5)]
        it = 0
        for kb in range(2):
            for t in range(8):
                sl = it % 2; it += 1
                y = ytm[:, kb, t, :]
                pT = bank[0][:].bitcast(BF16).rearrange("p (c n) -> p c n", n=128)[:, 0:4, :]
                transpose_chunks(C, y, "ytm", 4, pT, "gbk0", yT[:], "yT", idb)
                for c in range(4):
                    S.op("pe", lambda e, c=c: e.matmul(bank[1][:], lhsT=yT[:, c, :], rhs=Wg[:, c, :], start=(c == 0), stop=(c == 3)), reads=["yT", "Wg"], writes=["gbk1"])
                S.op("dve", lambda e: e.tensor_tensor(out=zb[:], in0=bank[1][:], in1=bgl[:], op=ALU.add), reads=["gbk1", "bgl"], writes=["zb"])
                S.op("act", lambda e: e.activation(out=zb[:], in_=zb[:], func=AF.Sigmoid), reads=["zb"], writes=["zb"])
                S.op("pool", lambda e, y=y: e.tensor_tensor(out=ssm[:], in0=y, in1=zb[:], op=ALU.mult), reads=["ytm", "zb"], writes=["ssm"])
                if "ssm" in dbg:
                    S.dma("sp", dbg["ssm"].rearrange("(k t) d -> t k d", t=8)[t, kb * 128:(kb + 1) * 128, :], ssm[:], reads=["ssm"], writes=["o_ssm"])
                rms_rstd(C, ssm[:], "ssm", junk[:], "junk3", st[:, 0:1], st[:, 1:2], "st3", 512)
                S.op("dve", lambda e: e.tensor_scalar(out=sn[:], in0=ssm[:], scalar1=st[:, 1:2], scalar2=None, op0=ALU.mult), reads=["ssm", "st3"], writes=["sn"])
                pT2 = bank[2][:].bitcast(BF16).rearrange("p (c n) -> p c n", n=128)[:, 0:4, :]
                transpose_chunks(C, sn[:], "sn", 4, pT2, "gbk2", snT[:], "snT", idb)
                for cb in range(2):
                    for c in range(4):
                        S.op("pe", lambda e, c=c, cb=cb: e.matmul(bank[3 + cb][:], lhsT=snT[:, c, :], rhs=Wo[:, c, cb * 512:(cb + 1) * 512], start=(c == 0), stop=(c == 3)), reads=["snT", "Wos"], writes=["gbk%d" % (3 + cb)])
                    S.op("act", lambda e, cb=cb, sl=sl: e.copy(out=ho[sl][:, cb * 512:(cb + 1) * 512], in_=bank[3 + cb][:]), reads=["gbk%d" % (3 + cb)], writes=["ho%d" % sl])
                S.dma("sp", hsv[t, kb * 128:(kb + 1) * 128, :], ho[sl][:], reads=["ho%d" % sl], writes=["hs_d"])


def _attention(C, es, D, idb, dbg, prep=None):
    S = C.S
    sb = lambda name, shape, dt=F32: C.sb(es, name, shape, dt)
    kT = sb("kT", [128, 4, 2560], BF16); qT = sb("qT", [128, 4, 2048], BF16); V = sb("Vall", [128, 20, 520], BF16)
    qZ = sb("qZ", [128, 8, 2048], BF16)
    S.dma("sp", kT[:].rearrange("p c n -> p (c n)"), D["kT_d"], reads=["qkT_d"], writes=["kT"])
    S.dma("sp", qT[:].rearrange("p c n -> p (c n)"), D["qT_d"], reads=["qkT_d"], writes=["qT"])
    S.dma("sp", V[:], D["V_d"].rearrange("t p n -> p t n"), reads=["V_d"], writes=["Vall"])
    S.op("pool", lambda e: e.memset(qZ[:].rearrange("p h n -> p (h n)"), 0.0), writes=["qZ"])
    for h in range(8):
        rws = slice(64 * (h % 2), 64 * (h % 2) + 64)
        S.op("dve" if h % 2 else "act", (lambda e, h=h, rws=rws: e.tensor_copy(out=qZ[rws, h, :], in_=qT[rws, h // 2, :])) if h % 2 else (lambda e, h=h, rws=rws: e.copy(out=qZ[rws, h, :], in_=qT[rws, h // 2, :])),
             reads=["qT", "qZ"], writes=["qZ"])
    BT = sb("BT", [128, 8, 5, 128], BF16)
    gao = sb("gao", [128, 4])
    S.dma("sp", gao[:], D["g_att_out"], writes=["gao"])
    with scope(C) as est:
        stg = C.sb(est, "btstg", [128, 640])
        for h in range(8):
            S.dma("sp", stg[:], D["bias_t"][:, h * 640:(h + 1) * 640], writes=["btstg"])
            S.op("dve", lambda e, h=h: e.tensor_copy(out=BT[:, h, :, :].rearrange("p j q -> p (j q)"), in_=stg[:]), reads=["btstg"], writes=["BT"])
    with scope(C) as est:
        Wo = load_weight_bf16(C, es, est, "Woa", D["w_out"][0:512, :], 4, 1024, gcol=(gao, "gao"))
    PT = [sb("PT%d" % i, [128, 5, 128], BF16) for i in range(2)]
    rd = sb("rd", [128, 8]); att = sb("att", [128, 8, 64]); junk = sb("junk4", [128, 512]); st = sb("st4", [128, 2])
    an = sb("an", [128, 512], BF16); anT = sb("anT", [128, 4, 128], BF16)
    xo = [sb("xo%d" % i, [128, 1024]) for i in range(2)]; hsl = [sb("hsl%d" % i, [128, 1024]) for i in range(2)]
    h1t = [sb("h1t%d" % i, [128, 1024]) for i in range(2)]
    xv = D["x_ext"].rearrange("(n p) d -> n p d", p=128)
    hsv = D["hs_d"].rearrange("(n p) d -> n p d", p=128)
    h1v = D["h1_d"].rearrange("(n p) d -> n p d", p=128)
    with scope(C) as esp:
        bank = [C.ps(esp, "abk%d" % i, [128, 512]) for i in range(8)]
        for qt in range(16):
            sl = qt % 2
            drip(prep, 2)
            S.dma("sp", xo[sl][:], xv[48 + qt], writes=["xo%d" % sl])
            S.dma("sp", hsl[sl][:], hsv[qt], reads=["hs_d"], writes=["hsl%d" % sl])
            for h in range(8):
                hp = h // 2; rows = slice(64 * (h % 2), 64 * (h % 2) + 64); ps_ = h % 2
                bA = bank[2 * ps_]; bB = bank[2 * ps_ + 1]; bAk = "abk%d" % (2 * ps_); bBk = "abk%d" % (2 * ps_ + 1)
                for j in range(5):
                    o = bA[:, j * 128:(j + 1) * 128] if j < 4 else bB[:, 0:128]
                    ok = bAk if j < 4 else bBk
                    S.op("pe", lambda e, o=o, h=h, hp=hp, j=j, qt=qt: e.matmul(o, lhsT=kT[:, hp, (qt + j) * 128:(qt + j + 1) * 128], rhs=qZ[:, h, qt * 128:(qt + 1) * 128], start=True, stop=False),
                         reads=["kT", "qZ"], writes=[ok])
                    S.op("pe", lambda e, o=o, h=h, j=j: e.matmul(o, lhsT=idb[:], rhs=BT[:, h, j, :], start=False, stop=True), reads=["identb", "BT"], writes=[ok])
                S.op("act", lambda e, ps_=ps_, bA=bA: e.activation(out=PT[ps_][:, 0:4, :].rearrange("p j q -> p (j q)"), in_=bA[:], func=AF.Exp), reads=[bAk], writes=["PT%d" % ps_])
                S.op("act", lambda e, ps_=ps_, bB=bB: e.activation(out=PT[ps_][:, 4, :], in_=bB[:, 0:128], func=AF.Exp), reads=[bBk], writes=["PT%d" % ps_])
                ob = bank[4 + h // 4]; obk = "abk%d" % (4 + h // 4)
                for j in range(5):
                    S.op("pe", lambda e, ob=ob, h=h, j=j, ps_=ps_, qt=qt: e.matmul(ob[:, (h % 4) * 65:(h % 4) * 65 + 65], lhsT=PT[ps_][:, j, :], rhs=V[:, qt + j, h * 65:(h + 1) * 65], start=(j == 0), stop=(j == 4)),
                         reads=["PT%d" % ps_, "Vall"], writes=[obk])
            for hb in range(2):
                ov = bank[4 + hb][:, 0:260].rearrange("p (h d) -> p h d", d=65)
                S.op("dve", lambda e, hb=hb, ov=ov: e.reciprocal(out=rd[:, hb * 4:(hb + 1) * 4], in_=ov[:, :, 64]), reads=["abk%d" % (4 + hb)], writes=["rd%d" % hb])
                S.op("dve", lambda e, hb=hb, ov=ov: e.tensor_tensor(out=att[:, hb * 4:(hb + 1) * 4, :], in0=ov[:, :, 0:64], in1=bc(rd[:, hb * 4:(hb + 1) * 4].unsqueeze(2), [128, 4, 64]), op=ALU.mult),
                     reads=["abk%d" % (4 + hb), "rd%d" % hb], writes=["att%d" % hb])
            attf = att[:].rearrange("p h d -> p (h d)")
            if "att" in dbg:
                S.dma("sp", dbg["att"].rearrange("(n p) d -> n p d", p=128)[qt], attf, reads=["att0", "att1"], writes=["o_att"])
            S.op("act", lambda e: e.activation(out=junk[:], in_=attf, func=AF.Square, accum_out=st[:, 0:1]), reads=["att0", "att1"], writes=["st4_ss"])
            S.op("act", lambda e: e.activation(out=st[:, 1:2], in_=st[:, 0:1], func=AF.Sqrt, scale=1.0 / 512, bias=EPS), reads=["st4_ss"], writes=["st4_sq"])
            S.op("dve", lambda e: e.reciprocal(out=st[:, 1:2], in_=st[:, 1:2]), reads=["st4_sq"], writes=["st4"])
            S.op("dve", lambda e: e.tensor_scalar(out=an[:], in0=attf, scalar1=st[:, 1:2], scalar2=None, op0=ALU.mult), reads=["att0", "att1", "st4"], writes=["an"])
            pT = bank[6][:].bitcast(BF16).rearrange("p (c n) -> p c n", n=128)[:, 0:4, :]
            transpose_chunks(C, an[:], "an", 4, pT, "abk6", anT[:], "anT", idb)
            for cb in range(2):
                ob_ = 7 - cb
                for c in range(4):
                    S.op("pe", lambda e, c=c, cb=cb, ob_=ob_: e.matmul(bank[ob_][:], lhsT=anT[:, c, :], rhs=Wo[:, c, cb * 512:(cb + 1) * 512], start=(c == 0), stop=(c == 3)), reads=["anT", "Woa"], writes=["abk%d" % ob_])
                S.op("dve", lambda e, cb=cb, sl=sl, ob_=ob_: e.tensor_tensor(out=h1t[sl][:, cb * 512:(cb + 1) * 512], in0=bank[ob_][:], in1=xo[sl][:, cb * 512:(cb + 1) * 512], op=ALU.add),
                     reads=["abk%d" % ob_, "xo%d" % sl], writes=["h1t%d" % sl])
            S.op("pool", lambda e, sl=sl: e.tensor_tensor(out=h1t[sl][:], in0=h1t[sl][:], in1=hsl[sl][:], op=ALU.add), reads=["h1t%d" % sl, "hsl%d" % sl], writes=["h1t%d" % sl])
            S.dma("sp", h1v[qt], h1t[sl][:], reads=["h1t%d" % sl], writes=["h1_d"])


def _col(g, n):
    return np.ascontiguousarray(np.asarray(g, np.float32).reshape(n, 128).T)


def host_shared(inp):
    f = lambda k: np.asarray(inp[k], np.float32)[0]
    sh = {}
    sh["g_mix"] = _col(f("norm_mix_g"), 8)
    sh["w_in"] = np.ascontiguousarray(f("w_in"))
    sh["att_q_g"] = f("att_q_g").reshape(1, 64)
    sh["att_k_g"] = f("att_k_g").reshape(1, 64)
    rb = f("rel_bias")
    p = np.arange(128); j = np.arange(5); q = np.arange(128)
    kidx = j[:, None] * 128 + p[None, :]
    kc = kidx // 64; ki = kidx % 64
    qc = q // 64; qi = q % 64
    jb = kc[:, :, None] - qc[None, None, :]
    allowed = (jb >= 0) & (jb <= 8)
    kj = jb * 64 + ki[:, :, None]
    dist = 512 + qi[None, None, :] - kj
    bucket = np.clip(np.clip(dist, -63, 128) + 63, 0, 191)
    bt = np.where(allowed[None], rb[:, bucket], np.float32(NEG))
    sh["bias_t"] = np.ascontiguousarray(bt.transpose(2, 0, 1, 3).reshape(128, 8 * 5 * 128).astype(np.float32))
    dup = lambda a: np.ascontiguousarray(np.concatenate([a, a], 0).astype(np.float32))
    sh["s5_lr"] = dup(f("ssm_lam_re").T)
    sh["s5_li"] = dup(f("ssm_lam_im").T)
    sh["s5_ls"] = np.ascontiguousarray(np.broadcast_to(f("ssm_log_step")[None, :], (128, 32)).astype(np.float32))
    sg = np.ones((128, 2), np.float32); sg[:64, 0] = -1.0; sg[64:, 1] = -1.0
    sh["s5_sg"] = sg
    sh["s5_nv"] = np.ascontiguousarray(np.broadcast_to(np.arange(-7, 16, dtype=np.float32)[None, :], (128, 23)))
    bre = f("ssm_b_re").transpose(1, 0, 2).reshape(64, 512); bim = f("ssm_b_im").transpose(1, 0, 2).reshape(64, 512)
    cre = f("ssm_c_re").transpose(2, 0, 1).reshape(64, 512); cim = f("ssm_c_im").transpose(2, 0, 1).reshape(64, 512)
    sh["s5_p1b"] = np.ascontiguousarray(np.concatenate([bre, bim], 0)); sh["s5_p2b"] = np.ascontiguousarray(np.concatenate([bim, bre], 0))
    sh["s5_p1c"] = np.ascontiguousarray(np.concatenate([cre, cim], 0)); sh["s5_p2c"] = np.ascontiguousarray(np.concatenate([cim, cre], 0))
    dd = np.zeros((2, 4, 16, 4, 4, 16), np.float32)
    dsk = f("ssm_d")
    for g in range(32):
        chc = g // 8; hb = (g % 8) // 4; j4 = g % 4
        for c in range(16):
            dd[hb, j4, c, chc, j4, c] = dsk[g, c]
    sh["s5_dd"] = dd.reshape(128, 256)
    sh["g_ssm_out"] = _col(f("ssm_out_g"), 4)
    sh["g_att_out"] = _col(f("att_out_g"), 4)
    sh["b_glu"] = f("ssm_b_glu").reshape(1, 512)
    sh["w_glu"] = np.ascontiguousarray(f("ssm_w_glu"))
    sh["w_out"] = np.ascontiguousarray(f("w_out"))
    return sh


def host_core(inp, c):
    b, seg = c // 4, c % 4
    x = np.asarray(inp["x"], np.float32)
    xe = np.zeros((8192, 1024), np.float32)
    n = (seg + 1) * 2048
    xe[8192 - n:] = x[b, :n]
    hv = np.full((512,), 1.0 if seg > 0 else 0.0, np.float32)
    return {"x_ext": xe, "hvalid": np.ascontiguousarray(hv.reshape(4, 128).T)}


IN_SHAPES = {
    "x_ext": [8192, 1024], "hvalid": [128, 4], "g_mix": [128, 8], "w_in": [1024, 2048], "att_q_g": [1, 64], "att_k_g": [1, 64],
    "bias_t": [128, 5120], "s5_lr": [128, 32], "s5_li": [128, 32], "s5_ls": [128, 32], "s5_sg": [128, 2], "s5_nv": [128, 23],
    "s5_p1b": [128, 512], "s5_p2b": [128, 512], "s5_p1c": [128, 512], "s5_p2c": [128, 512], "s5_dd": [128, 256],
    "g_ssm_out": [128, 4], "g_att_out": [128, 4], "b_glu": [1, 512], "w_glu": [512, 512], "w_out": [1024, 1024],
}
IN_SHAPES_MEM = {"mem": [256, 1024], "g_mem": [128, 8], "g_memkv": [128, 8], "mem_q_g": [1, 256], "mem_k_g": [1, 256],
                 "w_mem_q": [1024, 1024], "w_mem_k": [1024, 1024], "w_mem_v": [1024, 1024], "w_mem_o": [1024, 1024]}
IN_SHAPES_PEER = {"g_peer": [1, 1024], "w_peer_q": [1024, 2048], "keysT": [128, 2048], "peer_uT": [1024, 16384], "peer_v": [16384, 1024]}
SCRATCH = {"kT_d": ([128, 4 * 2560], BF16), "qT_d": ([128, 4 * 2048], BF16), "V_d": ([20, 128, 520], BF16),
           "hs_d": ([2048, 1024], F32), "Mz_d": ([128, 16 * 8 * 128], BF16), "CA_d": ([128, 2, 4096], BF16)}
SCRATCH_PEER = {"uT_b": ([1024, 16384], BF16), "v_b": ([16384, 1024], BF16), "xnT_d": ([16, 128, 1024], BF16),
                "sc_d": ([16, 128, 2048], F32), "tau_d": ([16, 128, 8], F32)}


def host_shared_rest(inp):
    f = lambda k: np.asarray(inp[k], np.float32)[0]
    sh = {}
    sh["g_mem"] = _col(f("norm_mem_g"), 8); sh["g_memkv"] = _col(f("norm_memkv_g"), 8)
    sh["mem_q_g"] = f("mem_q_g").reshape(1, 256); sh["mem_k_g"] = f("mem_k_g").reshape(1, 256)
    for k in ("w_mem_q", "w_mem_k", "w_mem_v", "w_mem_o", "w_peer_q"):
        sh[k] = np.ascontiguousarray(f(k))
    sh["g_peer"] = f("norm_peer_g").reshape(1, 1024)
    sh["keysT"] = np.ascontiguousarray(f("peer_keys").transpose(3, 0, 1, 2).reshape(128, 2048))
    sh["peer_uT"] = np.ascontiguousarray(f("peer_u").T)
    sh["peer_v"] = np.ascontiguousarray(f("peer_v"))
    return sh


def build_program(stages=("mixer", "mem", "peer"), dbg_specs=None, upto=9):
    nc = bass.Bass("TRN2", target_bir_lowering=False)
    D = {}
    shapes = {}
    if "mixer" in stages:
        shapes.update(IN_SHAPES)
    if "mem" in stages:
        shapes.update(IN_SHAPES_MEM)
    if "peer" in stages:
        shapes.update(IN_SHAPES_PEER)
    for k, shp in shapes.items():
        D[k] = nc.dram_tensor(k, shp, F32, kind="ExternalInput").ap()
    scr = {}
    if "mixer" in stages:
        scr.update(SCRATCH)
    if "peer" in stages:
        scr.update(SCRATCH_PEER)
    for k, (shp, dt) in scr.items():
        D[k] = nc.dram_tensor(k, shp, dt).ap()
    chain = ["h1_d", "h2_d", "out"]
    first = {"mixer": None, "mem": "h1_d", "peer": "h2_d"}[stages[0]]
    last = {"mixer": "h1_d", "mem": "h2_d", "peer": "out"}[stages[-1]]
    for k in chain:
        if k == first:
            D[k] = nc.dram_tensor(k, [2048, 1024], F32, kind="ExternalInput").ap()
        elif k == last:
            D[k] = nc.dram_tensor(k, [2048, 1024], F32, kind="ExternalOutput").ap()
        else:
            D[k] = nc.dram_tensor(k, [2048, 1024], F32).ap()
    dbg = {}
    for k, shp in (dbg_specs or {}).items():
        dbg[k] = nc.dram_tensor("dbg_" + k, shp, F32, kind="ExternalOutput").ap()
    with ExitStack() as es:
        S = Sched(nc, es)
        C = Ctx(nc, S)
        prep = stage_peer_prep(C, D) if "peer" in stages else None
        if "mixer" in stages:
            stage_mixer(C, D, dbg, upto, prep=prep)
        if "mem" in stages:
            stage_mem(C, D, dbg, prep=prep)
        drip(prep, 1000)
        if "peer" in stages:
            stage_peer(C, D, dbg)
        S.barrier()
        S.emit()
    return nc, S


def _headnorm(C, src_ap, skey, nh, hd, sqt, sqk, stat, stk, gt, gkey, dst_ap, dkey):
    S = C.S
    sv = src_ap.rearrange("p (h d) -> p h d", d=hd)
    S.op("pool", lambda e: e.tensor_tensor(out=sqt, in0=src_ap, in1=src_ap, op=ALU.mult), reads=[skey], writes=[sqk])
    S.op("dve", lambda e: e.tensor_reduce(out=stat[:, 0:nh], in_=sqt.rearrange("p (h d) -> p h d", d=hd), axis=AX.X, op=ALU.add), reads=[sqk], writes=[stk + "a"])
    S.op("act", lambda e: e.activation(out=stat[:, nh:2 * nh], in_=stat[:, 0:nh], func=AF.Sqrt, scale=1.0 / hd, bias=EPS), reads=[stk + "a"], writes=[stk + "b"])
    S.op("dve", lambda e: e.reciprocal(out=stat[:, nh:2 * nh], in_=stat[:, nh:2 * nh]), reads=[stk + "b"], writes=[stk])
    S.op("dve", lambda e: e.tensor_tensor(out=sv, in0=sv, in1=bc(stat[:, nh:2 * nh].unsqueeze(2), [128, nh, hd]), op=ALU.mult), reads=[skey, stk], writes=[skey])
    S.op("pool", lambda e: e.tensor_tensor(out=dst_ap.rearrange("p (h d) -> p h d", d=hd), in0=sv, in1=bc(gt.unsqueeze(1), [128, nh, hd]), op=ALU.mult), reads=[skey, gkey], writes=[dkey])


def stage_mem(C, D, dbg=None, prep=None):
    S = C.S
    dbg = dbg or {}
    with scope(C) as es:
        sb = lambda name, shape, dt=F32: C.sb(es, name, shape, dt)
        idf, idb = make_ident(C, es, "ident")
        gm = sb("gm", [128, 8]); gkv = sb("gkv", [128, 8]); gq = sb("mgq", [128, 256]); gk = sb("mgk", [128, 256])
        S.dma("sp", gm[:], D["g_mem"], writes=["gm"]); S.dma("sp", gkv[:], D["g_memkv"], writes=["gkv"])
        S.dma("sp", gq[:], D["mem_q_g"].partition_broadcast(128), writes=["mgq"]); S.dma("sp", gk[:], D["mem_k_g"].partition_broadcast(128), writes=["mgk"])
        S.op("dve", lambda e: e.tensor_scalar(out=gq[:], in0=gq[:], scalar1=1.0 / 16, scalar2=None, op0=ALU.mult), reads=["mgq"], writes=["mgq"])
        kTm = sb("kTm", [128, 8, 256], BF16); Vm = sb("Vm", [128, 2, 4, 257], BF16)
        xt = [sb("mxt%d" % i, [128, 1024]) for i in range(2)]; st = sb("mst", [128, 2]); xn = sb("mxn", [128, 1024], BF16)
        xnT = sb("mxnT", [128, 8, 128], BF16); qf = sb("mqf", [128, 1024]); sq = sb("msq", [128, 1024]); qst = sb("mqst", [128, 8])
        qn = sb("mqn", [128, 1024], BF16); mjunk = sb("mjunk", [128, 1024], BF16)
        with scope(C) as esk:
            with scope(C) as est:
                Wk = load_weight_bf16(C, esk, est, "Wmk", D["w_mem_k"], 8, 1024, gcol=(gkv, "gkv"))
            with scope(C) as est:
                Wv = load_weight_bf16(C, esk, est, "Wmv", D["w_mem_v"], 8, 1024, gcol=(gkv, "gkv"))
            with scope(C) as esp:
                bank = [C.ps(esp, "mkb%d" % i, [128, 512]) for i in range(6)]
                mv = D["mem"].rearrange("(n p) d -> n p d", p=128)
                for mt in range(2):
                    x_ = xt[mt]; xk = "mxt%d" % mt
                    S.dma("sp", x_[:], mv[mt], writes=[xk])
                    rms_rstd(C, x_[:], xk, mjunk[:], "mjunk", st[:, 0:1], st[:, 1:2], "mst", 1024)
                    S.op("dve", lambda e, x_=x_: e.tensor_scalar(out=xn[:], in0=x_[:], scalar1=st[:, 1:2], scalar2=None, op0=ALU.mult), reads=[xk, "mst"], writes=["mxn"])
                    pb = bank[0][:].bitcast(BF16).rearrange("p (c n) -> p c n", n=128)
                    transpose_chunks(C, xn[:], "mxn", 8, pb, "mkb0", xnT[:], "mxnT", idb)
                    for (W, wk, b0) in ((Wk, "Wmk", 1), (Wv, "Wmv", 3)):
                        for cb in range(2):
                            for dc in range(8):
                                S.op("pe", lambda e, W=W, cb=cb, dc=dc, b0=b0: e.matmul(bank[b0 + cb][:], lhsT=xnT[:, dc, :], rhs=W[:, dc, cb * 512:(cb + 1) * 512], start=(dc == 0), stop=(dc == 7)),
                                     reads=["mxnT", wk], writes=["mkb%d" % (b0 + cb)])
                    for cb in range(2):
                        S.op("act", lambda e, cb=cb: e.copy(out=qf[:, cb * 512:(cb + 1) * 512], in_=bank[1 + cb][:]), reads=["mkb%d" % (1 + cb)], writes=["mqf"])
                        S.op("dve", lambda e, cb=cb, mt=mt: e.tensor_copy(out=Vm[:, mt, 2 * cb:2 * cb + 2, 0:256], in_=bank[3 + cb][:].rearrange("p (h d) -> p h d", d=256)), reads=["mkb%d" % (3 + cb)], writes=["Vm"])
                    S.op("pool", lambda e, mt=mt: e.memset(Vm[:, mt, :, 256], 1.0), reads=[], writes=["Vm"])
                    _headnorm(C, qf[:], "mqf", 4, 256, sq[:], "msq", qst, "mqst", gk[:], "mgk", qn[:], "mqn")
                    pb2 = bank[5][:].bitcast(BF16).rearrange("p (c n) -> p c n", n=128)
                    transpose_chunks(C, qn[:], "mqn", 8, pb2, "mkb5", kTm[:, :, mt * 128:(mt + 1) * 128], "kTm", idb)
        with scope(C) as est:
            Wq = load_weight_bf16(C, es, est, "Wmq", D["w_mem_q"], 8, 1024, gcol=(gm, "gm"))
        with scope(C) as est:
            Wo = load_weight_bf16(C, es, est, "Wmo", D["w_mem_o"], 8, 1024)
        NM = 2
        xn2 = [sb("mxn_%d" % i, [128, 1024], BF16) for i in range(NM)]; xnT2 = [sb("mxnT_%d" % i, [128, 8, 128], BF16) for i in range(NM)]
        qf2 = [sb("mqf_%d" % i, [128, 1024]) for i in range(NM)]; sq2 = [sb("msq_%d" % i, [128, 1024]) for i in range(NM)]; qst2 = [sb("mqst_%d" % i, [128, 8]) for i in range(NM)]
        qn2 = [sb("mqn_%d" % i, [128, 1024], BF16) for i in range(NM)]; st2 = [sb("mst_%d" % i, [128, 2]) for i in range(NM)]
        qT2 = [sb("mqT%d" % i, [128, 8, 128], BF16) for i in range(NM)]; PT = [sb("mPT%d" % i, [128, 2, 128], BF16) for i in range(2)]
        rd2 = [sb("mrd%d" % i, [128, 4]) for i in range(NM)]; ob2 = [sb("mob%d" % i, [128, 1024], BF16) for i in range(NM)]; oT2 = [sb("moT%d" % i, [128, 8, 128], BF16) for i in range(NM)]
        h2t = [sb("h2t%d" % i, [128, 1024]) for i in range(2)]
        hv = D["h1_d"].rearrange("(n p) d -> n p d", p=128); ov = D["h2_d"].rearrange("(n p) d -> n p d", p=128)
        with scope(C) as esp:
            bank = [C.ps(esp, "mb%d" % i, [128, 512]) for i in range(8)]

            def mtile(tt):
                sl = tt % NM
                K = lambda nm: "%s_%d" % (nm, sl)
                xn = xn2[sl]; xnT = xnT2[sl]; qf = qf2[sl]; sq = sq2[sl]; qst = qst2[sl]; qn = qn2[sl]; st = st2[sl]; qT = qT2[sl]; rd = rd2[sl]; ob = ob2[sl]; oT = oT2[sl]
                drip(prep, 1)
                x_ = xt[sl]; xk = "mxt%d" % sl
                S.dma("sp", x_[:], hv[tt], reads=["h1_d"], writes=[xk])
                rms_rstd(C, x_[:], xk, mjunk[:], "mjunk", st[:, 0:1], st[:, 1:2], K("mst"), 1024)
                S.op("dve", lambda e: e.tensor_scalar(out=xn[:], in0=x_[:], scalar1=st[:, 1:2], scalar2=None, op0=ALU.mult), reads=[xk, K("mst")], writes=[K("mxn")])
                pb = bank[0][:].bitcast(BF16).rearrange("p (c n) -> p c n", n=128)
                transpose_chunks(C, xn[:], K("mxn"), 8, pb, "mb0", xnT[:], K("mxnT"), idb)
                yield
                for cb in range(2):
                    for dc in range(8):
                        S.op("pe", lambda e, cb=cb, dc=dc: e.matmul(bank[1 + cb][:], lhsT=xnT[:, dc, :], rhs=Wq[:, dc, cb * 512:(cb + 1) * 512], start=(dc == 0), stop=(dc == 7)),
                             reads=[K("mxnT"), "Wmq"], writes=["mb%d" % (1 + cb)])
                    S.op("act", lambda e, cb=cb: e.copy(out=qf[:, cb * 512:(cb + 1) * 512], in_=bank[1 + cb][:]), reads=["mb%d" % (1 + cb)], writes=[K("mqf")])
                yield
                _headnorm(C, qf[:], K("mqf"), 4, 256, sq[:], K("msq"), qst, K("mqst"), gq[:], "mgq", qn[:], K("mqn"))
                pb2 = bank[3][:].bitcast(BF16).rearrange("p (c n) -> p c n", n=128)
                transpose_chunks(C, qn[:], K("mqn"), 8, pb2, "mb3", qT[:], K("mqT"), idb)
                yield
                for h in range(4):
                    ps_ = h % 2
                    sbk = bank[4 + ps_]; sbkk = "mb%d" % (4 + ps_)
                    for mt in range(2):
                        for dh in range(2):
                            S.op("pe", lambda e, h=h, mt=mt, dh=dh, sbk=sbk: e.matmul(sbk[:, mt * 128:(mt + 1) * 128], lhsT=kTm[:, 2 * h + dh, mt * 128:(mt + 1) * 128], rhs=qT[:, 2 * h + dh, :], start=(dh == 0), stop=(dh == 1)),
                                 reads=["kTm", K("mqT")], writes=[sbkk])
                    S.op("act", lambda e, ps_=ps_, sbk=sbk: e.activation(out=PT[ps_][:].rearrange("p m q -> p (m q)"), in_=sbk[:, 0:256], func=AF.Exp, bias=-8.0), reads=[sbkk], writes=["mPT%d" % ps_])
                    obk = bank[6 + ps_]; obkk = "mb%d" % (6 + ps_)
                    for mt in range(2):
                        S.op("pe", lambda e, h=h, mt=mt, ps_=ps_, obk=obk: e.matmul(obk[:, 0:257], lhsT=PT[ps_][:, mt, :], rhs=Vm[:, mt, h, :], start=(mt == 0), stop=(mt == 1)), reads=["mPT%d" % ps_, "Vm"], writes=[obkk])
                    S.op("dve", lambda e, h=h, obk=obk: e.reciprocal(out=rd[:, h:h + 1], in_=obk[:, 256:257]), reads=[obkk], writes=[K("mrd") + "_%d" % h])
                    S.op("dve", lambda e, h=h, obk=obk: e.tensor_scalar(out=ob[:, h * 256:(h + 1) * 256], in0=obk[:, 0:256], scalar1=rd[:, h:h + 1], scalar2=None, op0=ALU.mult), reads=[obkk, K("mrd") + "_%d" % h], writes=[K("mob")])
                    yield
                pb3 = bank[0][:].bitcast(BF16).rearrange("p (c n) -> p c n", n=128)
                transpose_chunks(C, ob[:], K("mob"), 8, pb3, "mb0", oT[:], K("moT"), idb)
                yield
                for cb in range(2):
                    for dc in range(8):
                        S.op("pe", lambda e, cb=cb, dc=dc: e.matmul(bank[1 + cb][:], lhsT=oT[:, dc, :], rhs=Wo[:, dc, cb * 512:(cb + 1) * 512], start=(dc == 0), stop=(dc == 7)),
                             reads=[K("moT"), "Wmo"], writes=["mb%d" % (1 + cb)])
                    S.op("dve", lambda e, cb=cb: e.tensor_tensor(out=h2t[sl][:, cb * 512:(cb + 1) * 512], in0=bank[1 + cb][:], in1=x_[:, cb * 512:(cb + 1) * 512], op=ALU.add),
                         reads=["mb%d" % (1 + cb), xk], writes=["h2t%d" % sl])
                S.dma("sp", ov[tt], h2t[sl][:], reads=["h2t%d" % sl], writes=["h2_d"])

            from itertools import zip_longest
            gens = []
            for t0 in range(0, 16, NM):
                for _ in zip_longest(*[mtile(t0 + i) for i in range(NM)]):
                    pass


def stage_peer_prep(C, D):
    S = C.S
    uv = D["peer_uT"].rearrange("(c p) (a e) -> c p a e", p=128, e=2048)
    ub = D["uT_b"].rearrange("(c p) (a e) -> c p a e", p=128, e=2048)
    vv = D["peer_v"].rearrange("(c p) d -> c p d", p=512)
    vb = D["v_b"].rearrange("(c p) d -> c p d", p=512)

    def gen():
        for c in range(8):
            S.dma("pool", ub[c], uv[c], writes=["uT_b"])
            yield
        for c in range(32):
            S.dma("pool", vb[c], vv[c], writes=["v_b"])
            yield
    return gen()


def drip(g, n):
    if g is None:
        return
    for _ in range(n):
        try:
            next(g)
        except StopIteration:
            return


def _top16(C, src, skey, work, wkey, dst, dkey):
    S = C.S
    S.op("dve", lambda e: e.max(out=dst[:, 0:8], in_=src), reads=[skey], writes=[dkey])
    S.op("dve", lambda e: e.match_replace(out=work, in_to_replace=dst[:, 0:8], in_values=src, imm_value=-1e30), reads=[skey, dkey], writes=[wkey])
    S.op("dve", lambda e: e.max(out=dst[:, 8:16], in_=work), reads=[wkey], writes=[dkey])


def stage_peer(C, D, dbg=None):
    S = C.S
    dbg = dbg or {}
    hv = D["h2_d"].rearrange("(n p) d -> n p d", p=128)
    with scope(C) as es:
        sb = lambda name, shape, dt=F32: C.sb(es, name, shape, dt)
        idf, idb = make_ident(C, es, "ident")
        gp = sb("gpb", [128, 1024])
        S.dma("sp", gp[:], D["g_peer"].partition_broadcast(128), writes=["gpb"])
        with scope(C) as est:
            Wq = load_weight_bf16(C, es, est, "Wpq", D["w_peer_q"], 8, 2048)
        keyT = sb("keyT", [128, 16, 128], BF16)
        with scope(C) as est:
            kst = C.sb(est, "kst", [128, 2048])
            S.dma("sp", kst[:], D["keysT"], writes=["kst"])
            S.op("dve", lambda e: e.tensor_copy(out=keyT[:].rearrange("p a n -> p (a n)"), in_=kst[:]), reads=["kst"], writes=["keyT"])
        NS = 3
        xt = [sb("pxt%d" % i, [128, 1024]) for i in range(NS)]; st_ = [sb("pst%d" % i, [128, 2]) for i in range(NS)]; junk = sb("pjunk", [128, 1024], BF16)
        xn_ = [sb("pxn%d" % i, [128, 1024], BF16) for i in range(NS)]; xnT = [sb("pxnT%d" % i, [128, 8, 128], BF16) for i in range(NS)]
        qb_ = [sb("pqb%d" % i, [128, 2048], BF16) for i in range(NS)]; qTp_ = [sb("pqT%d" % i, [128, 16, 128], BF16) for i in range(NS)]
        sc = [sb("psc%d" % i, [128, 16, 128]) for i in range(NS)]; work_ = [sb("pwork%d" % i, [128, 256]) for i in range(NS)]
        sv_ = [sb("psv%d" % i, [128, 16, 16]) for i in range(NS)]; cand_ = [sb("pcand%d" % i, [128, 8, 256]) for i in range(NS)]
        cex_ = [sb("pcex%d" % i, [128, 8, 256]) for i in range(NS)]; ctop_ = [sb("pctop%d" % i, [128, 8, 16]) for i in range(NS)]
        Z_ = [sb("pZ%d" % i, [128, 8]) for i in range(NS)]; off_ = [sb("poff%d" % i, [128, 8]) for i in range(NS)]; tau = [sb("ptau%d" % i, [128, 8]) for i in range(NS)]
        cjunk_ = [[sb("pcj%d_%d" % (i, h), [128, 256], BF16) for h in range(8)] for i in range(NS)]; offs_ = [sb("poffs%d" % i, [128, 16]) for i in range(NS)]
        xTd = D["xnT_d"].rearrange("n p (c t) -> n p c t", t=128)
        with scope(C) as esp:
            bank = [C.ps(esp, "pab%d" % i, [128, 512]) for i in range(6)]

            def tile(tt):
                sl = tt % NS
                K = lambda nm: "%s%d" % (nm, sl)
                x_ = xt[sl]; xk = K("pxt"); st = st_[sl]; xn = xn_[sl]; qb = qb_[sl]; qTp = qTp_[sl]; work = work_[sl]
                sv = sv_[sl]; cand = cand_[sl]; cex = cex_[sl]; ctop = ctop_[sl]; Z = Z_[sl]; off = off_[sl]; cjunk = cjunk_[sl]; offs = offs_[sl]
                S.dma("sp", x_[:], hv[tt], reads=["h2_d"], writes=[xk])
                rms_rstd(C, x_[:], xk, junk[:], "pjunk", st[:, 0:1], st[:, 1:2], K("pst"), 1024)
                S.op("dve", lambda e: e.scalar_tensor_tensor(out=xn[:], in0=x_[:], scalar=st[:, 1:2], in1=gp[:], op0=ALU.mult, op1=ALU.mult), reads=[xk, K("pst"), "gpb"], writes=[K("pxn")])
                pb = bank[0][:].bitcast(BF16).rearrange("p (c n) -> p c n", n=128)
                transpose_chunks(C, xn[:], K("pxn"), 8, pb, "pab0", xnT[sl][:], K("pxnT"), idb)
                S.dma("sp", xTd[tt], xnT[sl][:], reads=[K("pxnT")], writes=["xnT_d"])
                yield
                for cb in range(4):
                    for dc in range(8):
                        S.op("pe", lambda e, cb=cb, dc=dc: e.matmul(bank[1 + cb][:], lhsT=xnT[sl][:, dc, :], rhs=Wq[:, dc, cb * 512:(cb + 1) * 512], start=(dc == 0), stop=(dc == 7)),
                             reads=[K("pxnT"), "Wpq"], writes=["pab%d" % (1 + cb)])
                    S.op("act", lambda e, cb=cb: e.copy(out=qb[:, cb * 512:(cb + 1) * 512], in_=bank[1 + cb][:]), reads=["pab%d" % (1 + cb)], writes=[K("pqb")])
                for half in range(2):
                    pbq = bank[5][:].bitcast(BF16).rearrange("p (c n) -> p c n", n=128)
                    transpose_chunks(C, qb[:, half * 1024:(half + 1) * 1024], K("pqb"), 8, pbq, "pab5", qTp[:, half * 8:(half + 1) * 8, :], K("pqT"), idb)
                for hh in range(16):
                    S.op("pe", lambda e, hh=hh: e.matmul(bank[1 + hh // 4][:, (hh % 4) * 128:(hh % 4 + 1) * 128], lhsT=qTp[:, hh, :], rhs=keyT[:, hh, :], start=True, stop=True),
                         reads=[K("pqT"), "keyT"], writes=["pab%d" % (1 + hh // 4)])
                scs = sc[sl]; sck = K("psc")
                for cb in range(4):
                    S.op("act", lambda e, cb=cb: e.copy(out=scs[:, cb * 4:(cb + 1) * 4, :].rearrange("p a n -> p (a n)"), in_=bank[1 + cb][:]), reads=["pab%d" % (1 + cb)], writes=[sck])
                yield
                for hh in range(16):
                    _top16(C, scs[:, hh, :], sck, work[:, 0:128], K("pwork"), sv[:, hh, :], K("psv"))
                    yield
                svv = sv[:].rearrange("p (h s) k -> p h s k", s=2)
                for h in range(8):
                    S.op("dve", lambda e, h=h: e.tensor_tensor(out=cand[:, h, :].rearrange("p (a b) -> p a b", b=16), in0=bc(svv[:, h, 0, :].unsqueeze(2), [128, 16, 16]),
                                                            in1=bc(svv[:, h, 1, :].unsqueeze(1), [128, 16, 16]), op=ALU.add), reads=[K("psv")], writes=[K("pcand") + "_%d" % h])
                    yield
                ck = [K("pcand") + "_%d" % h for h in range(8)]
                for h in range(8):
                    _top16(C, cand[:, h, :], ck[h], work[:], K("pwork"), ctop[:, h, :], K("pctop"))
                    yield
                S.op("dve", lambda e: e.tensor_tensor(out=cand[:], in0=cand[:], in1=bc(ctop[:, :, 0:1], [128, 8, 256]), op=ALU.subtract), reads=ck + [K("pctop")], writes=ck)
                S.op("act", lambda e: e.activation(out=cex[:].rearrange("p h n -> p (h n)"), in_=cand[:].rearrange("p h n -> p (h n)"), func=AF.Exp), reads=ck, writes=[K("pcex")])
                yield
                S.op("dve", lambda e: e.tensor_tensor(out=tau[sl][:], in0=ctop[:, :, 15], in1=ctop[:, :, 0], op=ALU.subtract), reads=[K("pctop")], writes=[K("ptau")])
                yield
                S.op("dve", lambda e: e.tensor_scalar(out=tau[sl][:], in0=tau[sl][:], scalar1=-1e-5, scalar2=None, op0=ALU.add), reads=[K("ptau")], writes=[K("ptau")])
                yield
                for h in range(8):
                    S.op("dve", lambda e, h=h: e.scalar_tensor_tensor(out=cjunk[h][:], in0=cand[:, h, :], scalar=tau[sl][:, h:h + 1], in1=cex[:, h, :], op0=ALU.is_ge, op1=ALU.mult, accum_out=Z[:, h:h + 1]),
                         reads=ck + [K("pcex"), K("ptau")], writes=[K("pZ") + "_%d" % h, K("pcj") + "_%d" % h])
                    yield
                S.op("act", lambda e: e.activation(out=off[:], in_=Z[:], func=AF.Ln), reads=[K("pZ") + "_%d" % h for h in range(8)], writes=[K("poff")])
                yield
                S.op("dve", lambda e: e.tensor_tensor(out=tau[sl][:], in0=tau[sl][:], in1=off[:], op=ALU.subtract), reads=[K("ptau"), K("poff")], writes=[K("ptau")])
                S.op("act", lambda e: e.activation(out=tau[sl][:], in_=tau[sl][:], func=AF.Exp), reads=[K("ptau")], writes=[K("ptau")])
                yield
                S.op("dve", lambda e: e.tensor_scalar(out=tau[sl][:], in0=tau[sl][:], scalar1=0.99997, scalar2=None, op0=ALU.mult), reads=[K("ptau")], writes=[K("ptau")])
                offv = offs[:].rearrange("p (h s) -> p h s", s=2)
                S.op("dve", lambda e: e.tensor_copy(out=offv[:, :, 0], in_=svv[:, :, 0, 0]), reads=[K("psv")], writes=[K("poffs") + "a"])
                yield
                S.op("dve", lambda e: e.tensor_tensor(out=offv[:, :, 1], in0=svv[:, :, 1, 0], in1=off[:], op=ALU.add), reads=[K("psv"), K("poff")], writes=[K("poffs") + "b"])
                yield
                S.op("dve", lambda e: e.tensor_tensor(out=scs[:], in0=scs[:], in1=bc(offs[:].unsqueeze(2), [128, 16, 128]), op=ALU.subtract), reads=[sck, K("poffs") + "a", K("poffs") + "b"], writes=[sck])
                S.op("act", lambda e: e.activation(out=scs[:].rearrange("p a n -> p (a n)"), in_=scs[:].rearrange("p a n -> p (a n)"), func=AF.Exp), reads=[sck], writes=[sck])
                S.dma("sp", D["sc_d"][tt], scs[:].rearrange("p a n -> p (a n)"), reads=[sck], writes=["sc_d"])
                S.dma("sp", D["tau_d"][tt], tau[sl][:], reads=[K("ptau")], writes=["tau_d"])

            from itertools import zip_longest
            for t0 in range(0, 16, NS):
                for _ in zip_longest(*[tile(t0 + i) for i in range(NS) if t0 + i < 16]):
                    pass
    with scope(C) as es:
        sb = lambda name, shape, dt=F32: C.sb(es, name, shape, dt)
        idf, idb = make_ident(C, es, "ident")
        xnT = sb("bxnT", [128, 4, 1024], BF16); sc = sb("bsc", [128, 4, 2048]); kap = sb("btau", [128, 4, 8])
        UT = [sb("UT%d" % i, [128, 8, 1024], BF16) for i in range(2)]; Vb = [sb("Vb%d" % i, [128, 8, 1024], BF16) for i in range(2)]
        acc = sb("pacc", [128, 4, 1024])
        NP = 12
        Pt = [sb("pP%d" % i, [128, 512]) for i in range(NP)]; Wh = [[sb("pWh%d_%d" % (i, h), [128, 512], BF16) for h in range(8)] for i in range(2)]
        G = [sb("pG%d" % i, [128, 512], BF16) for i in range(3)]; WA = [sb("pWA%d" % i, [128, 512], BF16) for i in range(2)]
        WAT = [sb("pWAT%d" % i, [128, 4, 128], BF16) for i in range(2)]
        h2t = [sb("ph2t%d" % i, [128, 1024]) for i in range(2)]
        uTv = D["uT_b"].rearrange("(c p) (b e) -> b p c e", p=128, e=1024)
        vbv = D["v_b"].rearrange("(b c p) d -> b p c d", p=128, c=8)
        ov = D["out"].rearrange("(n p) d -> n p d", p=128)
        with scope(C) as esp:
            bank = [C.ps(esp, "pbb%d" % i, [128, 512]) for i in range(7)]
            state = {"it": 0}

            def stage1a(u, tg, eb, sub, tt, es_):
                ub = u % 2
                xT = xnT[:, tt, :].rearrange("p (c t) -> p c t", t=128)
                scv = sc[:, tt, :].rearrange("p (h s n) -> p h s n", s=2, n=128)
                i0 = eb * 8 + sub * 4
                for dc in range(8):
                    S.op("pe", lambda e, dc=dc, xT=xT: e.matmul(bank[ub][:], lhsT=xT[:, dc, :], rhs=UT[es_][:, dc, sub * 512:(sub + 1) * 512], start=(dc == 0), stop=(dc == 7)),
                         reads=["bxnT", "UT%d" % es_], writes=["pbb%d" % ub])
                for h in range(8):
                    hs = state["it"] % NP; state["it"] += 1
                    if h >= 5:
                        S.op("pool", lambda e, h=h, hs=hs, scv=scv: e.tensor_tensor(out=Pt[hs][:].rearrange("p (i j) -> p i j", j=128), in0=bc(scv[:, h, 0, i0:i0 + 4].unsqueeze(2), [128, 4, 128]),
                                                                             in1=bc(scv[:, h, 1, :].unsqueeze(1), [128, 4, 128]), op=ALU.mult), reads=["bsc"], writes=["pP%d_%d" % (hs, il) for il in range(4)])
                    else:
                        for il in range(4):
                            S.op("act", lambda e, h=h, hs=hs, scv=scv, il=il: e.activation(out=Pt[hs][:, il * 128:(il + 1) * 128], in_=scv[:, h, 1, :], func=AF.Copy, scale=scv[:, h, 0, i0 + il:i0 + il + 1]),
                                 reads=["bsc"], writes=["pP%d_%d" % (hs, il)])
                    S.op("dve", lambda e, h=h, hs=hs: e.scalar_tensor_tensor(out=Wh[ub][h][:], in0=Pt[hs][:], scalar=kap[:, tt, h:h + 1], in1=Pt[hs][:], op0=ALU.is_ge, op1=ALU.mult),
                         reads=["pP%d_%d" % (hs, il) for il in range(4)] + ["btau"], writes=["pWh%d_%d" % (ub, h)])

            def st_gelu(u, tg, eb, sub, tt, es_):
                ub = u % 2; gb = u % 3
                S.op("act", lambda e: e.activation(out=G[gb][:], in_=bank[ub][:], func=AF.Gelu), reads=["pbb%d" % ub], writes=["pG%d" % gb])

            def st_hs(u, tg, eb, sub, tt, es_):
                ub = u % 2
                for h in range(8):
                    S.op("pe", lambda e, h=h: e.matmul(bank[2 + ub][:], lhsT=idb[:], rhs=Wh[ub][h][:], start=(h == 0), stop=(h == 7)),
                         reads=["identb", "pWh%d_%d" % (ub, h)], writes=["pbb%d" % (2 + ub)])

            def st_wa(u, tg, eb, sub, tt, es_):
                ub = u % 2; gb = u % 3
                S.op("dve", lambda e: e.tensor_tensor(out=WA[ub][:], in0=bank[2 + ub][:], in1=G[gb][:], op=ALU.mult), reads=["pbb%d" % (2 + ub), "pG%d" % gb], writes=["pWA%d" % ub])

            def st_t(u, tg, eb, sub, tt, es_):
                ub = u % 2
                pbt = bank[4][:].bitcast(BF16).rearrange("p (c n) -> p c n", n=128)[:, 0:4, :]
                for c in range(4):
                    S.op("pe", lambda e, c=c: e.transpose(out=pbt[:, c, :], in_=WA[ub][:, c * 128:(c + 1) * 128], identity=idb[:]), reads=["pWA%d" % ub, "identb"], writes=["pbb4"])

            def st_watcopy(u, tg, eb, sub, tt, es_):
                ub = u % 2
                pbt = bank[4][:].bitcast(BF16).rearrange("p (c n) -> p c n", n=128)[:, 0:4, :]
                S.op("act", lambda e: e.copy(out=WAT[ub][:], in_=pbt), reads=["pbb4"], writes=["pWAT%d" % ub])

            def st_v(u, tg, eb, sub, tt, es_):
                ub = u % 2
                for cb in range(2):
                    for ec in range(4):
                        S.op("pe", lambda e, cb=cb, ec=ec: e.matmul(bank[5 + cb][:], lhsT=WAT[ub][:, ec, :], rhs=Vb[es_][:, sub * 4 + ec, cb * 512:(cb + 1) * 512], start=(sub == 0 and ec == 0), stop=(sub == 1 and ec == 3)),
                             reads=["pWAT%d" % ub, "Vb%d" % es_], writes=["pbb%d" % (5 + cb)])

            def st_acc(u, tg, eb, sub, tt, es_):
                if sub != 1:
                    return
                for cb in range(2):
                    if eb == 0:
                        S.op("dve", lambda e, cb=cb: e.tensor_copy(out=acc[:, tt, cb * 512:(cb + 1) * 512], in_=bank[5 + cb][:]), reads=["pbb%d" % (5 + cb)], writes=["pacc%d" % tt])
                    else:
                        S.op("dve", lambda e, cb=cb: e.tensor_tensor(out=acc[:, tt, cb * 512:(cb + 1) * 512], in0=bank[5 + cb][:], in1=acc[:, tt, cb * 512:(cb + 1) * 512], op=ALU.add),
                             reads=["pbb%d" % (5 + cb), "pacc%d" % tt], writes=["pacc%d" % tt])

            u = 0
            for tg in range(4):
                S.dma("sp", xnT[:], D["xnT_d"][tg * 4:(tg + 1) * 4].rearrange("n p f -> p n f"), reads=["xnT_d"], writes=["bxnT"])
                S.dma("sp", sc[:], D["sc_d"][tg * 4:(tg + 1) * 4].rearrange("n p f -> p n f"), reads=["sc_d"], writes=["bsc"])
                S.dma("sp", kap[:], D["tau_d"][tg * 4:(tg + 1) * 4].rearrange("n p f -> p n f"), reads=["tau_d"], writes=["btau"])
                units = []
                for eb in range(16):
                    es_ = (tg * 16 + eb) % 2
                    for tt in range(4):
                        for sub in range(2):
                            units.append((u, tg, eb, sub, tt, es_)); u += 1
                n = len(units)
                U = lambda j: units[j] if 0 <= j < n else None
                for k in range(n + 3):
                    for ebk in ([0] if k == 0 else []) + ([k // 8 + 1] if (k % 8 == 3 and k // 8 + 1 < 16) else []):
                        es_ = (tg * 16 + ebk) % 2
                        S.dma("sp", UT[es_][:], uTv[ebk], reads=["uT_b"], writes=["UT%d" % es_])
                        S.dma("sp", Vb[es_][:], vbv[ebk], reads=["v_b"], writes=["Vb%d" % es_])
                    if U(k - 2): st_wa(*U(k - 2))
                    if U(k - 3): st_v(*U(k - 3))
                    if U(k - 2): st_t(*U(k - 2))
                    if U(k - 1): st_gelu(*U(k - 1))
                    if U(k - 1): st_hs(*U(k - 1))
                    if U(k): stage1a(*U(k))
                    if U(k - 2): st_watcopy(*U(k - 2))
                    if U(k - 3): st_acc(*U(k - 3))
                for tt in range(4):
                    sl = tt % 2; n = tg * 4 + tt
                    S.dma("sp", h2t[sl][:], hv[n], reads=["h2_d"], writes=["ph2t%d" % sl])
                    S.op("pool", lambda e, sl=sl, tt=tt: e.tensor_tensor(out=h2t[sl][:], in0=h2t[sl][:], in1=acc[:, tt, :], op=ALU.add), reads=["ph2t%d" % sl, "pacc%d" % tt], writes=["ph2t%d" % sl])
                    S.dma("sp", ov[n], h2t[sl][:], reads=["ph2t%d" % sl], writes=["out"])


_PROG = {}


def kernel(**inputs):
    sh = host_shared(inputs)
    sh.update(host_shared_rest(inputs))
    if "nc" not in _PROG:
        _PROG["nc"] = build_program(("mixer", "mem", "peer"))[0]
    nc = _PROG["nc"]
    names = set(IN_SHAPES) | set(IN_SHAPES_MEM) | set(IN_SHAPES_PEER)
    mem = np.asarray(inputs["mem"], np.float32)
    maps = []
    for c in range(8):
        m = {k: v for k, v in sh.items() if k in names}
        m.update(host_core(inputs, c))
        m["mem"] = np.ascontiguousarray(mem[c // 4])
        maps.append(m)
    res = run_bass_kernel_spmd(nc, maps, core_ids=list(range(8)))
    out = np.zeros((2, 8192, 1024), np.float32)
    for c in range(8):
        out[c // 4, (c % 4) * 2048:(c % 4 + 1) * 2048] = res.results[c]["out"]
    return out
```

```python
from contextlib import ExitStack, contextmanager
import numpy as np
import concourse.bass as bass
import concourse.mybir as mybir
from concourse.bass_utils import run_bass_kernel_spmd

F32 = mybir.dt.float32
BF16 = mybir.dt.bfloat16
I32 = mybir.dt.int32
AF = mybir.ActivationFunctionType
ALU = mybir.AluOpType
AX = mybir.AxisListType

ENGS = ("pe", "act", "dve", "pool", "sp")
TWO_PI = 6.283185307179586
EPS = 1e-6
NEG = -30000.0


class Sched:
    def __init__(self, nc, es, n_dma_sems=32):
        self.nc = nc
        self.streams = {e: [] for e in ENGS}
        self.sem = {e: es.enter_context(nc.semaphore("s_" + e)) for e in ("pe", "act", "dve", "pool")}
        self.cnt = {e: 0 for e in ("pe", "act", "dve", "pool")}
        self.dsem = [es.enter_context(nc.semaphore("s_dma%d" % i)) for i in range(n_dma_sems)]
        self.dcnt = [0] * n_dma_sems
        self.dnext = 0
        self.n_sw = 4
        self.dnext_sw = 0
        self.waited = {}
        self.last_w = {}
        self.readers = {}
        self.n_ops = 0

    def _deps(self, eng, reads, writes):
        deps = []
        for k in reads:
            if k in self.last_w:
                deps.append(self.last_w[k])
        for k in writes:
            if k in self.last_w:
                deps.append(self.last_w[k])
            deps.extend(self.readers.get(k, ()))
        need = {}
        for (sk, val, peng) in deps:
            if peng == "pe" and eng == "pe":
                continue
            if self.waited.get((eng, sk), 0) >= val:
                continue
            if need.get(sk, 0) < val:
                need[sk] = val
        return need

    def _semobj(self, sk):
        return self.sem[sk] if isinstance(sk, str) else self.dsem[sk]

    def _emit_waits(self, eng, need):
        for sk, val in need.items():
            self.waited[(eng, sk)] = val
            so = self._semobj(sk)
            self.streams[eng].append(lambda e, so=so, val=val: e.wait_ge(so, val))

    def _record(self, tok, reads, writes):
        for k in writes:
            self.last_w[k] = tok
            self.readers[k] = []
        for k in reads:
            if k not in writes:
                self.readers.setdefault(k, []).append(tok)

    def op(self, eng, fn, reads=(), writes=()):
        need = self._deps(eng, reads, writes)
        self._emit_waits(eng, need)
        self.cnt[eng] += 1
        val = self.cnt[eng]
        so = self.sem[eng]
        self.streams[eng].append(lambda e, fn=fn, so=so: fn(e).then_inc(so, 1))
        self._record((eng, val, eng), reads, writes)
        self.n_ops += 1

    def dma(self, q, out, in_, reads=(), writes=(), **kw):
        nhw = len(self.dsem) - self.n_sw
        if q == "pool":
            i = nhw + self.dnext_sw
            self.dnext_sw = (self.dnext_sw + 1) % self.n_sw
        else:
            i = self.dnext
            self.dnext = (self.dnext + 1) % nhw
        need = self._deps(q, reads, writes)
        prev = 16 * self.dcnt[i]
        if prev and self.waited.get((q, i), 0) < prev:
            need[i] = max(need.get(i, 0), prev)
        self._emit_waits(q, need)
        self.dcnt[i] += 1
        val = 16 * self.dcnt[i]
        so = self.dsem[i]
        self.streams[q].append(
            lambda e, out=out, in_=in_, so=so, kw=kw: e.dma_start(out=out, in_=in_, **kw).then_inc(so, 16))
        self._record((i, val, "dma"), reads, writes)
        self.n_ops += 1

    def barrier(self):
        for eng in ENGS:
            need = {}
            for pe_ in ("pe", "act", "dve", "pool"):
                v = self.cnt[pe_]
                if v and self.waited.get((eng, pe_), 0) < v:
                    need[pe_] = v
            for i, c in enumerate(self.dcnt):
                if c and self.waited.get((eng, i), 0) < 16 * c:
                    need[i] = 16 * c
            self._emit_waits(eng, need)

    def wait_all(self, eng, keys):
        need = {}
        for k in keys:
            if k in self.last_w:
                sk, val, _ = self.last_w[k]
                if self.waited.get((eng, sk), 0) < val and need.get(sk, 0) < val:
                    need[sk] = val
        self._emit_waits(eng, need)

    def emit(self):
        if not any(self.streams[e] for e in ENGS):
            return
        streams = self.streams
        self.streams = {e: [] for e in ENGS}
        self._emit_block(streams)

    def _emit_block(self, streams):
        self_streams = streams
        with self.nc.Block() as block:
            @block.tensor
            def _(e):
                for f in self_streams["pe"]:
                    f(e)

            @block.scalar
            def _(e):
                for f in self_streams["act"]:
                    f(e)

            @block.vector
            def _(e):
                for f in self_streams["dve"]:
                    f(e)

            @block.gpsimd
            def _(e):
                for f in self_streams["pool"]:
                    f(e)

            @block.sync
            def _(e):
                for f in self_streams["sp"]:
                    f(e)


class Ctx:
    def __init__(self, nc, S):
        self.nc = nc
        self.S = S
        self.uid = 0

    def sb(self, es, name, shape, dt=F32):
        self.uid += 1
        return es.enter_context(self.nc.sbuf_tensor("%s_%d" % (name, self.uid), list(shape), dt))

    def ps(self, es, name, shape, dt=F32):
        self.uid += 1
        return es.enter_context(self.nc.psum_tensor("%s_%d" % (name, self.uid), list(shape), dt))


@contextmanager
def scope(C):
    with ExitStack() as es:
        yield es
        C.S.barrier()
        C.S.emit()


def bc(ap, shape):
    return ap.to_broadcast(list(shape))


def make_ident(C, es, name="ident"):
    S = C.S
    idf = C.sb(es, name + "f", [128, 128])
    idb = C.sb(es, name + "b", [128, 128], BF16)
    S.op("pool", lambda e: e.memset(idf[:], 1.0), writes=[name + "f"])
    S.op("pool", lambda e: e.affine_select(out=idf[:], in_=idf[:], pattern=[[-1, 128]], compare_op=ALU.is_equal,
                                           fill=0.0, base=0, channel_multiplier=1), reads=[name + "f"], writes=[name + "f"])
    S.op("dve", lambda e: e.tensor_copy(out=idb[:], in_=idf[:]), reads=[name + "f"], writes=[name + "b"])
    return idf, idb


def sincos(C, es, ang, n, tag):
    S = C.S
    outs = []
    for which, off in (("s", 64.0), ("c", 64.25)):
        k = tag + which
        y = C.sb(es, k + "y", [128, n]); yi = C.sb(es, k + "yi", [128, n], I32); yf = C.sb(es, k + "yf", [128, n])
        m = C.sb(es, k + "m", [128, n]); o = C.sb(es, k + "o", [128, n])
        S.op("dve", lambda e, y=y, off=off: e.tensor_scalar(out=y[:], in0=ang, scalar1=1.0 / TWO_PI, scalar2=off, op0=ALU.mult, op1=ALU.add),
             reads=[tag + "ang"], writes=[k + "y"])
        S.op("dve", lambda e, y=y, yi=yi: e.tensor_copy(out=yi[:], in_=y[:]), reads=[k + "y"], writes=[k + "yi"])
        S.op("dve", lambda e, yi=yi, yf=yf: e.tensor_copy(out=yf[:], in_=yi[:]), reads=[k + "yi"], writes=[k + "yf"])
        S.op("dve", lambda e, y=y, yf=yf: e.tensor_tensor(out=y[:], in0=y[:], in1=yf[:], op=ALU.subtract), reads=[k + "y", k + "yf"], writes=[k + "y"])
        S.op("dve", lambda e, y=y, m=m: e.tensor_scalar(out=m[:], in0=y[:], scalar1=0.5, scalar2=None, op0=ALU.is_gt), reads=[k + "y"], writes=[k + "m"])
        S.op("dve", lambda e, y=y, m=m: e.tensor_tensor(out=y[:], in0=y[:], in1=m[:], op=ALU.subtract), reads=[k + "y", k + "m"], writes=[k + "y"])
        S.op("act", lambda e, y=y, o=o: e.activation(out=o[:], in_=y[:], func=AF.Sin, scale=TWO_PI), reads=[k + "y"], writes=[k + "o"])
        outs.append((o, k + "o"))
    return outs


def s5_params(C, es_keep, D):
    S, nc = C.S, C.nc
    P = {}
    LT2 = C.sb(es_keep, "LT2", [128, 8, 8, 2, 128], BF16)
    WPr = C.sb(es_keep, "WPr", [128, 9, 16]); WPi = C.sb(es_keep, "WPi", [128, 9, 16]); WPn = C.sb(es_keep, "WPn", [128, 9, 16])
    P.update(LT2=LT2, WPr=WPr, WPi=WPi, WPn=WPn)
    with scope(C) as es:
        sb = lambda name, shape, dt=F32: C.sb(es, name, shape, dt)
        Mz2 = sb("Mz2", [128, 16, 8, 128], BF16)
        CA = sb("CA", [128, 32, 128], BF16); CAs = sb("CAs", [128, 32, 128], BF16)
        LR = sb("LR", [128, 32]); LI = sb("LI", [128, 32]); LS = sb("LS", [128, 32])
        SG = sb("SG", [128, 2]); NV = sb("NV", [128, 23])
        P1B = sb("P1B", [128, 32, 16]); P2B = sb("P2B", [128, 32, 16]); P1C = sb("P1C", [128, 32, 16]); P2C = sb("P2C", [128, 32, 16])
        DD = sb("DD", [128, 16, 16])
        for t, nm in ((LR, "s5_lr"), (LI, "s5_li"), (LS, "s5_ls"), (SG, "s5_sg"), (NV, "s5_nv")):
            S.dma("sp", t[:], D[nm], writes=[nm])
        S.dma("sp", DD[:].rearrange("p e c -> p (e c)"), D["s5_dd"], writes=["s5_dd"])
        for t, nm in ((P1B, "s5_p1b"), (P2B, "s5_p2b"), (P1C, "s5_p1c"), (P2C, "s5_p2c")):
            S.dma("sp", t[:].rearrange("p g c -> p (g c)"), D[nm], writes=[nm])
        idf, idb = make_ident(C, es, "pid")
        STEP = sb("STEP", [128, 32]); AA = sb("AA", [128, 32]); PH = sb("PH", [128, 32])
        S.op("act", lambda e: e.activation(out=STEP[:], in_=LS[:], func=AF.Exp), reads=["s5_ls"], writes=["STEP"])
        S.op("dve", lambda e: e.tensor_tensor(out=AA[:], in0=LR[:], in1=STEP[:], op=ALU.mult), reads=["s5_lr", "STEP"], writes=["AA"])
        S.op("dve", lambda e: e.tensor_tensor(out=PH[:], in0=LI[:], in1=STEP[:], op=ALU.mult), reads=["s5_li", "STEP"], writes=["PH"])
        EXPO = sb("EXPO", [128, 32, 23]); ANG = sb("ANG", [128, 32, 23]); MAG = sb("MAG", [128, 32, 23])
        nvb = bc(NV[:].unsqueeze(1), [128, 32, 23])
        S.op("dve", lambda e: e.tensor_tensor(out=EXPO[:], in0=bc(AA[:].unsqueeze(2), [128, 32, 23]), in1=nvb, op=ALU.mult), reads=["AA", "s5_nv"], writes=["EXPO"])
        S.op("dve", lambda e: e.tensor_tensor(out=ANG[:], in0=bc(PH[:].unsqueeze(2), [128, 32, 23]), in1=nvb, op=ALU.mult), reads=["PH", "s5_nv"], writes=["pwang"])
        S.op("act", lambda e: e.activation(out=MAG[:], in_=EXPO[:], func=AF.Exp), reads=["EXPO"], writes=["MAG"])
        (sn, snk), (cs, csk) = sincos(C, es, ANG[:].rearrange("p g n -> p (g n)"), 32 * 23, "pw")
        CR = sb("CR", [128, 32, 23]); CI = sb("CI", [128, 32, 23])
        S.op("dve", lambda e: e.tensor_tensor(out=CR[:].rearrange("p g n -> p (g n)"), in0=MAG[:].rearrange("p g n -> p (g n)"), in1=cs[:], op=ALU.mult), reads=["MAG", csk], writes=["CR"])
        S.op("dve", lambda e: e.tensor_tensor(out=CI[:].rearrange("p g n -> p (g n)"), in0=MAG[:].rearrange("p g n -> p (g n)"), in1=sn[:], op=ALU.mult), reads=["MAG", snk], writes=["CI"])
        zr = sb("zr", [128, 32]); den = sb("den", [128, 32]); t0 = sb("t0", [128, 32]); fr = sb("fr", [128, 32]); fi = sb("fi", [128, 32])
        S.op("dve", lambda e: e.tensor_scalar(out=zr[:], in0=CR[:, :, 8], scalar1=-1.0, scalar2=None, op0=ALU.add), reads=["CR"], writes=["zr"])
        S.op("dve", lambda e: e.tensor_tensor(out=den[:], in0=LR[:], in1=LR[:], op=ALU.mult), reads=["s5_lr"], writes=["den"])
        S.op("dve", lambda e: e.tensor_tensor(out=t0[:], in0=LI[:], in1=LI[:], op=ALU.mult), reads=["s5_li"], writes=["t0"])
        S.op("dve", lambda e: e.tensor_tensor(out=den[:], in0=den[:], in1=t0[:], op=ALU.add), reads=["den", "t0"], writes=["den"])
        S.op("dve", lambda e: e.reciprocal(out=den[:], in_=den[:]), reads=["den"], writes=["den"])
        S.op("dve", lambda e: e.tensor_tensor(out=fr[:], in0=zr[:], in1=LR[:], op=ALU.mult), reads=["zr", "s5_lr"], writes=["fr"])
        S.op("dve", lambda e: e.tensor_tensor(out=t0[:], in0=CI[:, :, 8], in1=LI[:], op=ALU.mult), reads=["CI", "s5_li", "den"], writes=["t0"])
        S.op("dve", lambda e: e.tensor_tensor(out=fr[:], in0=fr[:], in1=t0[:], op=ALU.add), reads=["fr", "t0"], writes=["fr"])
        S.op("dve", lambda e: e.tensor_tensor(out=fr[:], in0=fr[:], in1=den[:], op=ALU.mult), reads=["fr", "den"], writes=["fr"])
        S.op("dve", lambda e: e.tensor_tensor(out=fi[:], in0=CI[:, :, 8], in1=LR[:], op=ALU.mult), reads=["CI", "s5_lr"], writes=["fi"])
        S.op("dve", lambda e: e.tensor_tensor(out=t0[:], in0=zr[:], in1=LI[:], op=ALU.mult), reads=["zr", "s5_li", "fr"], writes=["t0"])
        S.op("dve", lambda e: e.tensor_tensor(out=fi[:], in0=fi[:], in1=t0[:], op=ALU.subtract), reads=["fi", "t0"], writes=["fi"])
        S.op("dve", lambda e: e.tensor_tensor(out=fi[:], in0=fi[:], in1=den[:], op=ALU.mult), reads=["fi", "den"], writes=["fi"])
        BB1 = sb("BB1", [128, 32, 16]); BB2 = sb("BB2", [128, 32, 16]); ta = sb("ta", [128, 32, 16]); tb = sb("tb", [128, 32, 16])
        frb = bc(fr[:].unsqueeze(2), [128, 32, 16]); fib = bc(fi[:].unsqueeze(2), [128, 32, 16])
        fl = lambda t: t[:].rearrange("p g c -> p (g c)")
        S.op("dve", lambda e: e.tensor_tensor(out=ta[:], in0=P1B[:], in1=frb, op=ALU.mult), reads=["s5_p1b", "fr"], writes=["ta"])
        S.op("dve", lambda e: e.tensor_tensor(out=tb[:], in0=P2B[:], in1=fib, op=ALU.mult), reads=["s5_p2b", "fi"], writes=["tb"])
        S.op("dve", lambda e: e.scalar_tensor_tensor(out=fl(BB1), in0=fl(tb), scalar=SG[:, 0:1], in1=fl(ta), op0=ALU.mult, op1=ALU.add), reads=["ta", "tb", "s5_sg"], writes=["BB1"])
        S.op("dve", lambda e: e.tensor_tensor(out=ta[:], in0=P2B[:], in1=frb, op=ALU.mult), reads=["s5_p2b", "fr", "BB1"], writes=["ta"])
        S.op("dve", lambda e: e.tensor_tensor(out=tb[:], in0=P1B[:], in1=fib, op=ALU.mult), reads=["s5_p1b", "fi", "BB1"], writes=["tb"])
        S.op("dve", lambda e: e.scalar_tensor_tensor(out=fl(BB2), in0=fl(tb), scalar=SG[:, 1:2], in1=fl(ta), op0=ALU.mult, op1=ALU.add), reads=["ta", "tb", "s5_sg"], writes=["BB2"])
        t5 = sb("t5", [128, 32, 16]); t6 = sb("t6", [128, 32, 16])
        Rm = sb("Rm", [128, 32, 8, 16])
        Q1 = sb("Q1", [128, 32, 16]); Q2 = sb("Q2", [128, 32, 16])
        S.op("dve", lambda e: e.tensor_scalar(out=fl(Q1), in0=fl(P1C), scalar1=SG[:, 1:2], scalar2=None, op0=ALU.mult), reads=["s5_p1c", "s5_sg"], writes=["Q1"])
        S.op("dve", lambda e: e.tensor_scalar(out=fl(Q2), in0=fl(P2C), scalar1=SG[:, 0:1], scalar2=None, op0=ALU.mult), reads=["s5_p2c", "s5_sg"], writes=["Q2"])
        CAv = CA[:].rearrange("p g (t c) -> p g t c", c=16); CAsv = CAs[:].rearrange("p g (t c) -> p g t c", c=16)
        for t in range(8):
            for (dst, dkey, a_, akey, b_, bkey, idx) in ((Rm[:, :, t, :], "Rm", Q1, "Q1", P2C, "s5_p2c", 7 + t),
                                                         (CAv[:, :, t, :], "CA", Q1, "Q1", P2C, "s5_p2c", 15 + t),
                                                         (CAsv[:, :, t, :], "CAs", Q2, "Q2", P1C, "s5_p1c", 15 + t)):
                S.op("dve", lambda e, a_=a_, idx=idx: e.tensor_tensor(out=t5[:], in0=a_[:], in1=bc(CR[:, :, idx:idx + 1], [128, 32, 16]), op=ALU.mult), reads=[akey, "CR"], writes=["t5"])
                S.op("dve", lambda e, b_=b_, idx=idx: e.tensor_tensor(out=t6[:], in0=b_[:], in1=bc(CI[:, :, idx:idx + 1], [128, 32, 16]), op=ALU.mult), reads=[bkey, "CI"], writes=["t6"])
                S.op("dve", lambda e, dst=dst: e.tensor_tensor(out=dst, in0=t5[:], in1=t6[:], op=ALU.subtract), reads=["t5", "t6"], writes=[dkey])
        Wr = sb("Wr", [128, 9, 32]); Wi = sb("Wi", [128, 9, 32]); sq = sb("sq", [128, 32])
        S.op("dve", lambda e: e.tensor_copy(out=Wr[:, 0, :], in_=CR[:, :, 15]), reads=["CR"], writes=["Wr"])
        S.op("dve", lambda e: e.tensor_copy(out=Wi[:, 0, :], in_=CI[:, :, 15]), reads=["CI"], writes=["Wi"])
        for j in range(8):
            S.op("dve", lambda e, j=j: e.tensor_tensor(out=Wr[:, j + 1, :], in0=Wr[:, j, :], in1=Wr[:, j, :], op=ALU.mult), reads=["Wr"], writes=["Wr"])
            S.op("dve", lambda e, j=j: e.tensor_tensor(out=sq[:], in0=Wi[:, j, :], in1=Wi[:, j, :], op=ALU.mult), reads=["Wi"], writes=["sq"])
            S.op("dve", lambda e, j=j: e.tensor_tensor(out=Wr[:, j + 1, :], in0=Wr[:, j + 1, :], in1=sq[:], op=ALU.subtract), reads=["Wr", "sq"], writes=["Wr"])
            S.op("dve", lambda e, j=j: e.scalar_tensor_tensor(out=Wi[:, j + 1, :], in0=Wr[:, j, :], scalar=2.0, in1=Wi[:, j, :], op0=ALU.mult, op1=ALU.mult), reads=["Wr", "Wi"], writes=["Wi"])
        Wrv = Wr[:].rearrange("p j (a r w) -> p j a r w", r=2, w=2); Wiv = Wi[:].rearrange("p j (a r w) -> p j a r w", r=2, w=2)
        for r in range(2):
            pr_ = slice(64 * r, 64 * r + 64)
            for j in range(9):
                S.op("dve", lambda e, r=r, pr_=pr_, j=j: e.tensor_copy(out=WPr[pr_, j, :].rearrange("p (a w) -> p a w", w=2), in_=Wrv[pr_, j, :, r, :]), reads=["Wr"], writes=["WPr"])
                S.op("dve", lambda e, r=r, pr_=pr_, j=j: e.tensor_copy(out=WPi[pr_, j, :].rearrange("p (a w) -> p a w", w=2), in_=Wiv[pr_, j, :, r, :]), reads=["Wi"], writes=["WPi"])
        S.op("dve", lambda e: e.tensor_scalar(out=WPn[:].rearrange("p j q -> p (j q)"), in0=WPi[:].rearrange("p j q -> p (j q)"), scalar1=-1.0, scalar2=None, op0=ALU.mult), reads=["WPi"], writes=["WPn"])
        E = [[sb("E%d%d" % (h_, r), [128, 128]) for r in range(2)] for h_ in range(2)]
        for h_ in range(2):
            for r in range(2):
                S.op("pool", lambda e, h_=h_, r=r: e.memset(E[h_][r][:], 0.0), writes=["E%d%d" % (h_, r)])
                S.op("pool", lambda e, h_=h_, r=r: e.tensor_copy(out=E[h_][r][64 * h_:64 * h_ + 64, 64 * r:64 * r + 64], in_=idf[64 * h_:64 * h_ + 64, 64 * h_:64 * h_ + 64]),
                     reads=["pidf", "E%d%d" % (h_, r)], writes=["E%d%d" % (h_, r)])
        Lz = [sb("Lz%d" % i, [128, 32, 64]) for i in range(2)]
        for i in range(2):
            S.op("pool", lambda e, i=i: e.memset(Lz[i][:].rearrange("p g c -> p (g c)"), 0.0), writes=["Lz%d" % i])
        S.op("pool", lambda e: e.memset(Mz2[:].rearrange("p a b c -> p (a b c)"), 0.0), writes=["Mz2"])
        t5v = t5[:].rearrange("p (a j) c -> p a j c", j=4); t6v = t6[:].rearrange("p (a j) c -> p a j c", j=4)
        with scope(C) as esp:
            PM = C.ps(esp, "PM", [128, 16, 128]); PL = C.ps(esp, "PL", [128, 8, 2, 128])
            for s in range(8):
                i = 7 - s; sl = s % 2; lk = "Lz%d" % sl
                Lzv = Lz[sl][:].rearrange("p (a j) c -> p a j c", j=4)
                S.op("dve", lambda e, i=i: e.tensor_tensor(out=t5[:], in0=BB1[:], in1=bc(CR[:, :, i:i + 1], [128, 32, 16]), op=ALU.mult), reads=["BB1", "CR"], writes=["t5"])
                S.op("dve", lambda e, i=i: e.tensor_tensor(out=t6[:], in0=BB2[:], in1=bc(CI[:, :, i:i + 1], [128, 32, 16]), op=ALU.mult), reads=["BB2", "CI"], writes=["t6"])
                for j4 in range(4):
                    S.op("dve", lambda e, j4=j4, Lzv=Lzv: e.scalar_tensor_tensor(out=Lzv[:, :, j4, 16 * j4:16 * j4 + 16], in0=t6v[:, :, j4, :], scalar=SG[:, 0:1], in1=t5v[:, :, j4, :], op0=ALU.mult, op1=ALU.add),
                         reads=["t5", "t6", "s5_sg"], writes=[lk])
                for g in range(32):
                    chc = g // 8; hb = (g % 8) // 4; j4 = g % 4; e_ = chc * 4 + j4
                    rows = slice(64 * hb, 64 * hb + 64)
                    S.op("pe", lambda e, g=g, sl=sl, e_=e_, rows=rows: e.matmul(PM[rows, e_, :], lhsT=Lz[sl][:, g, :], rhs=Rm[:, g, :, :].rearrange("p t c -> p (t c)"), start=True, stop=True),
                         reads=[lk, "Rm"], writes=["PM"])
                for pr in range(16):
                    a_ = pr // 2; wp = pr % 2; chc = a_ // 2; hb = a_ % 2; e2 = chc * 2 + wp
                    rows = slice(64 * hb, 64 * hb + 64)
                    for h_ in range(2):
                        for r in range(2):
                            g = 4 * a_ + 2 * r + wp
                            S.op("pe", lambda e, g=g, sl=sl, e2=e2, rows=rows, h_=h_, r=r: e.matmul(PL[rows, e2, h_, :], lhsT=Lz[sl][:, g, :], rhs=E[h_][r][:], start=(r == 0), stop=(r == 1)),
                                 reads=[lk, "E%d%d" % (h_, r)], writes=["PL"])
                S.op("dve", lambda e, s=s: e.tensor_copy(out=Mz2[:, :, s, 16 * s:128], in_=PM[:, :, 16 * s:128]), reads=["PM"], writes=["Mz2"])
                S.op("dve", lambda e, s=s: e.tensor_tensor(out=Mz2[:, :, s, 16 * s:16 * s + 16], in0=PM[:, :, 16 * s:16 * s + 16], in1=DD[:], op=ALU.add), reads=["PM", "s5_dd"], writes=["Mz2"])
                S.op("act", lambda e, s=s: e.copy(out=LT2[:, :, s, :, :], in_=PL[:]), reads=["PL"], writes=["LT2"])
        S.dma("sp", D["Mz_d"], Mz2[:].rearrange("p a b c -> p (a b c)"), reads=["Mz2"], writes=["Mz_d"])
        S.dma("sp", D["CA_d"][:, 0, :], CA[:].rearrange("p g c -> p (g c)"), reads=["CA"], writes=["CA_d"])
        S.dma("sp", D["CA_d"][:, 1, :], CAs[:].rearrange("p g c -> p (g c)"), reads=["CAs"], writes=["CA_d"])
    return P


def load_weight_bf16(C, es, es_tmp, name, src, rows_chunks, ncols, gcol=None, q="sp"):
    S = C.S
    W = C.sb(es, name, [128, rows_chunks, ncols], BF16)
    stg = [C.sb(es_tmp, name + "_stg%d" % i, [128, ncols]) for i in range(2)]
    srcv = src.rearrange("(c p) n -> c p n", p=128)
    for c in range(rows_chunks):
        st = stg[c % 2]; sk = name + "_stg%d" % (c % 2)
        S.dma(q, st[:], srcv[c], writes=[sk])
        wk_ = "%s_c%d" % (name, c)
        if c % 2 == 0:
            if gcol is not None:
                S.op("dve", lambda e, st=st, c=c: e.tensor_scalar(out=W[:, c, :], in0=st[:], scalar1=gcol[0][:, c:c + 1], scalar2=None, op0=ALU.mult),
                     reads=[sk, gcol[1]], writes=[name])
            else:
                S.op("dve", lambda e, st=st, c=c: e.tensor_copy(out=W[:, c, :], in_=st[:]), reads=[sk], writes=[name])
        else:
            if gcol is not None:
                S.op("act", lambda e, st=st, c=c: e.activation(out=W[:, c, :], in_=st[:], func=AF.Copy, scale=gcol[0][:, c:c + 1]),
                     reads=[sk, gcol[1]], writes=[name])
            else:
                S.op("act", lambda e, st=st, c=c: e.copy(out=W[:, c, :], in_=st[:]), reads=[sk], writes=[name])
    return W


def rms_rstd(C, x_ap, xkey, junk, jkey, ss, rs, skey, n):
    S = C.S
    S.op("act", lambda e: e.activation(out=junk, in_=x_ap, func=AF.Square, accum_out=ss), reads=[xkey], writes=[skey + "_ss"])
    S.op("act", lambda e: e.activation(out=rs, in_=ss, func=AF.Sqrt, scale=1.0 / n, bias=EPS), reads=[skey + "_ss"], writes=[skey + "_sq"])
    S.op("dve", lambda e: e.reciprocal(out=rs, in_=rs), reads=[skey + "_sq"], writes=[skey])


def transpose_chunks(C, src, skey, nch, pbank, pkey, dst, dkey, idb, evac="act"):
    S = C.S
    for c in range(nch):
        S.op("pe", lambda e, c=c: e.transpose(out=pbank[:, c, :], in_=src[:, c * 128:(c + 1) * 128], identity=idb[:]), reads=[skey, "identb"], writes=[pkey])
    if evac == "act":
        S.op("act", lambda e: e.copy(out=dst, in_=pbank), reads=[pkey], writes=[dkey])
    else:
        S.op(evac, lambda e: e.tensor_copy(out=dst, in_=pbank), reads=[pkey], writes=[dkey])


def stage_mixer(C, D, dbg=None, upto=9, prep=None):
    S, nc = C.S, C.nc
    dbg = dbg or {}
    with scope(C) as es1:
        P = s5_params(C, es1, D)
        idf = C.sb(es1, "identf", [128, 128]); idb = C.sb(es1, "identb", [128, 128], BF16)
        S.op("pool", lambda e: e.memset(idf[:], 1.0), writes=["identf"])
        S.op("pool", lambda e: e.affine_select(out=idf[:], in_=idf[:], pattern=[[-1, 128]], compare_op=ALU.is_equal, fill=0.0, base=0, channel_multiplier=1), reads=["identf"], writes=["identf"])
        S.op("dve", lambda e: e.tensor_copy(out=idb[:], in_=idf[:]), reads=["identf"], writes=["identb"])
        if "WPr" in dbg:
            for nm in ("WPr", "WPi"):
                S.dma("sp", dbg[nm], P[nm][:].rearrange("p a b -> p (a b)"), reads=[nm], writes=["o_" + nm])
            S.dma("pool", dbg["LT2"], P["LT2"][:].rearrange("p a b c d -> p (a b c d)"), reads=["LT2"], writes=["o_LT2"])
            S.dma("pool", dbg["Mz"], D["Mz_d"], reads=["Mz_d"], writes=["o_Mz"])
            S.dma("pool", dbg["CA"], D["CA_d"].rearrange("p a b -> p (a b)"), reads=["CA_d"], writes=["o_CA"])
        if upto < 1:
            return
        carry_r = C.sb(es1, "carry_r", [128, 16]); carry_i = C.sb(es1, "carry_i", [128, 16])
        S.op("pool", lambda e: e.memset(carry_r[:], 0.0), writes=["carry_r"])
        S.op("pool", lambda e: e.memset(carry_i[:], 0.0), writes=["carry_i"])
        TRE = C.sb(es1, "TRE", [128, 16, 257]); TIM = C.sb(es1, "TIM", [128, 16, 257])
        uT = C.sb(es1, "uT", [128, 4, 8, 256], BF16)
        with scope(C) as es2:
            _mixer_passes(C, es2, D, P, idb, carry_r, carry_i, TRE, TIM, uT, dbg)
        if "carry" in dbg:
            S.dma("sp", dbg["carry"][:, 0:16], carry_r[:], reads=["carry_r"], writes=["o_carry"])
            S.dma("sp", dbg["carry"][:, 16:32], carry_i[:], reads=["carry_i"], writes=["o_carry2"])
        if upto < 2:
            return
        with scope(C) as es3:
            ytm = C.sb(es3, "ytm", [128, 2, 8, 512], BF16)
            with scope(C) as es4:
                _s5_scan_out(C, es4, D, P, idb, TRE, TIM, uT, ytm, carry_r, carry_i, dbg)
            if upto < 3:
                return
            _s5_glu_out(C, es3, D, idb, ytm, dbg)
    if upto < 4:
        return
    with scope(C) as es5:
        idf = C.sb(es5, "identf", [128, 128]); idb = C.sb(es5, "identb", [128, 128], BF16)
        S.op("pool", lambda e: e.memset(idf[:], 1.0), reads=[], writes=["identf"])
        S.op("pool", lambda e: e.affine_select(out=idf[:], in_=idf[:], pattern=[[-1, 128]], compare_op=ALU.is_equal, fill=0.0, base=0, channel_multiplier=1), reads=["identf"], writes=["identf"])
        S.op("dve", lambda e: e.tensor_copy(out=idb[:], in_=idf[:]), reads=["identf"], writes=["identb"])
        _attention(C, es5, D, idb, dbg, prep)


def _mixer_passes(C, es, D, P, idb, carry_r, carry_i, TRE, TIM, uT, dbg):
    S = C.S
    sb = lambda name, shape, dt=F32: C.sb(es, name, shape, dt)
    gin = sb("gin", [128, 8])
    S.dma("sp", gin[:], D["g_mix"], writes=["gin"])
    with scope(C) as est:
        Wb = load_weight_bf16(C, es, est, "Wb", D["w_in"], 8, 2048, gcol=(gin, "gin"))
    gq = sb("gq", [128, 64]); gk = sb("gk", [128, 64]); hv = sb("hv", [128, 4])
    S.dma("sp", gq[:], D["att_q_g"].partition_broadcast(128), writes=["gq"])
    S.dma("sp", gk[:], D["att_k_g"].partition_broadcast(128), writes=["gk"])
    S.dma("sp", hv[:], D["hvalid"], writes=["hv"])
    S.op("dve", lambda e: e.tensor_scalar(out=gq[:], in0=gq[:], scalar1=0.125, scalar2=None, op0=ALU.mult), reads=["gq"], writes=["gq"])
    xt = [sb("xt%d" % i, [128, 1024]) for i in range(2)]
    junk = sb("junk", [128, 1024]); st = [sb("st%d" % i, [128, 4]) for i in range(2)]
    xn = [sb("xn%d" % i, [128, 1024], BF16) for i in range(2)]
    xnT = [sb("xnT%d" % i, [128, 8, 512], BF16) for i in range(2)]
    qkv = sb("qkv", [128, 3, 512]); sq = sb("sq2", [128, 512]); qst = sb("qst", [128, 4, 8])
    qn = sb("qn", [128, 2, 512], BF16)
    kTs = [sb("kTs%d" % i, [128, 4, 128], BF16) for i in range(2)]; qTs = [sb("qTs%d" % i, [128, 4, 128], BF16) for i in range(2)]
    Vs = [sb("Vs%d" % i, [128, 8, 65], BF16) for i in range(2)]
    tBr = sb("tBr", [128, 8, 128]); tBi = sb("tBi", [128, 8, 128]); tCr = sb("tCr", [128, 8, 64]); tCi = sb("tCi", [128, 8, 64])
    tt1 = sb("tt1", [128, 8, 128]); tt2 = sb("tt2", [128, 8, 128]); tt3 = sb("tt3", [128, 8, 128]); tt4 = sb("tt4", [128, 8, 128])
    REDr = sb("REDr", [128, 16]); REDi = sb("REDi", [128, 16]); c1 = sb("c1", [128, 16]); c2 = sb("c2", [128, 16]); c3 = sb("c3", [128, 16])
    with scope(C) as esp:
        bank = [C.ps(esp, "bk%d" % i, [128, 512]) for i in range(8)]
        xv = D["x_ext"].rearrange("(n p) d -> n p d", p=128)
        tile_ctr = 0
        for q in range(4):
            for blk in range(4):
                bslot = (q * 4 + blk) % 2
                xT = xnT[bslot]; xTk = "xnT%d" % bslot
                for tt in range(4):
                    n_tile = q * 16 + blk * 4 + tt
                    sl = tile_ctr % 2; tile_ctr += 1
                    x_ = xt[sl]; xk = "xt%d" % sl
                    S.dma("sp", x_[:], xv[n_tile], writes=[xk])
                    rms_rstd(C, x_[:], xk, junk[:], "junk", st[sl][:, 0:1], st[sl][:, 1:2], "st%d" % sl, 1024)
                    S.op("dve", lambda e, x_=x_, sl=sl: e.tensor_scalar(out=xn[sl][:], in0=x_[:], scalar1=st[sl][:, 1:2], scalar2=None, op0=ALU.mult),
                         reads=[xk, "st%d" % sl], writes=["xn%d" % sl])
                    tb_ = 0 if sl == 0 else 7
                    pb = bank[tb_][:].bitcast(BF16).rearrange("p (c n) -> p c n", n=128)
                    transpose_chunks(C, xn[sl][:], "xn%d" % sl, 8, pb, "bk%d" % tb_, xT[:, :, tt * 128:(tt + 1) * 128], xTk, idb, evac=("act" if sl == 0 else "dve"))
                for chc in range(4):
                    bi = 1 + (chc % 2); bkk = "bk%d" % bi
                    for dc in range(8):
                        S.op("pe", lambda e, chc=chc, dc=dc, bi=bi, xT=xT: e.matmul(bank[bi][:], lhsT=Wb[:, dc, 1536 + chc * 128:1536 + (chc + 1) * 128], rhs=xT[:, dc, :], start=(dc == 0), stop=(dc == 7)),
                             reads=["Wb", xTk], writes=[bkk])
                    eng = "act" if chc % 2 == 0 else "dve"
                    src = bank[bi][:].rearrange("p (k s) -> p s k", s=8)
                    dst = uT[:, chc, :, blk * 64:(blk + 1) * 64]
                    if eng == "act":
                        S.op("act", lambda e, src=src, dst=dst: e.copy(out=dst, in_=src), reads=[bkk], writes=["uT"])
                    else:
                        S.op("dve", lambda e, src=src, dst=dst: e.tensor_copy(out=dst, in_=src), reads=[bkk], writes=["uT"])
                need_kv = (q == 3) or (q == 2 and blk == 3)
                need_q = (q == 3)
                if need_kv:
                    for tt in range(4):
                        n_tile = q * 16 + blk * 4 + tt
                        kvt = n_tile - 44
                        sl = kvt % 2
                        projs = [(1, 512, 3), (2, 1024, 4)] + ([(0, 0, 5)] if need_q else [])
                        for (pi, c0, bi) in projs:
                            for dc in range(8):
                                S.op("pe", lambda e, dc=dc, bi=bi, c0=c0, tt=tt, xT=xT: e.matmul(bank[bi][:], lhsT=xT[:, dc, tt * 128:(tt + 1) * 128], rhs=Wb[:, dc, c0:c0 + 512], start=(dc == 0), stop=(dc == 7)),
                                     reads=["Wb", xTk], writes=["bk%d" % bi])
                        S.op("act", lambda e, sl=sl: e.copy(out=Vs[sl][:, :, 0:64], in_=bank[4][:].rearrange("p (h d) -> p h d", d=64)), reads=["bk4"], writes=["Vs%d" % sl])
                        if kvt < 4:
                            S.op("pool", lambda e, sl=sl, kvt=kvt: e.tensor_copy(out=Vs[sl][:, :, 64], in_=bc(hv[:, kvt:kvt + 1], [128, 8])), reads=["hv"], writes=["Vs%d" % sl])
                        else:
                            S.op("pool", lambda e, sl=sl: e.memset(Vs[sl][:, :, 64], 1.0), reads=[], writes=["Vs%d" % sl])
                        S.dma("sp", D["V_d"][kvt], Vs[sl][:].rearrange("p h d -> p (h d)"), reads=["Vs%d" % sl], writes=["V_d"])
                        for (pi, bi, gt, gkey, dstT, dkey, dram, ncol_t) in ([(1, 3, gk, "gk", kTs[sl], "kTs%d" % sl, D["kT_d"], kvt)] +
                                                                          ([(0, 5, gq, "gq", qTs[sl], "qTs%d" % sl, D["qT_d"], kvt - 4)] if need_q else [])):
                            qs = qkv[:, pi, :]; qk_ = "qkv%d" % pi
                            S.op("act", lambda e, qs=qs, bi=bi: e.copy(out=qs, in_=bank[bi][:]), reads=["bk%d" % bi], writes=[qk_])
                            S.op("act", lambda e, qs=qs: e.activation(out=sq[:], in_=qs, func=AF.Square), reads=[qk_], writes=["sq2"])
                            S.op("dve", lambda e, pi=pi: e.tensor_reduce(out=qst[:, pi, :], in_=sq[:].rearrange("p (h d) -> p h d", d=64), axis=AX.X, op=ALU.add), reads=["sq2"], writes=["qst%d" % pi])
                            S.op("act", lambda e, pi=pi: e.activation(out=qst[:, 2 + pi, :], in_=qst[:, pi, :], func=AF.Sqrt, scale=1.0 / 64, bias=EPS), reads=["qst%d" % pi], writes=["qsq%d" % pi])
                            S.op("dve", lambda e, pi=pi: e.reciprocal(out=qst[:, 2 + pi, :], in_=qst[:, 2 + pi, :]), reads=["qsq%d" % pi], writes=["qrs%d" % pi])
                            S.op("dve", lambda e, qs=qs, pi=pi: e.tensor_tensor(out=qs.rearrange("p (h d) -> p h d", d=64), in0=qs.rearrange("p (h d) -> p h d", d=64),
                                                                        in1=bc(qst[:, 2 + pi, :].unsqueeze(2), [128, 8, 64]), op=ALU.mult), reads=[qk_, "qrs%d" % pi], writes=[qk_])
                            S.op("pool", lambda e, qs=qs, pi=pi, gt=gt: e.tensor_tensor(out=qn[:, pi, :].rearrange("p (h d) -> p h d", d=64), in0=qs.rearrange("p (h d) -> p h d", d=64),
                                                                               in1=bc(gt[:].unsqueeze(1), [128, 8, 64]), op=ALU.mult), reads=[qk_, gkey], writes=["qn%d" % pi])
                            pb = bank[6][:].bitcast(BF16).rearrange("p (c n) -> p c n", n=128)[:, 0:4, :]
                            transpose_chunks(C, qn[:, pi, :], "qn%d" % pi, 4, pb, "bk6", dstT[:], dkey, idb)
                            S.dma("sp", dram.rearrange("p (c n) -> p c n", c=4)[:, :, ncol_t * 128:(ncol_t + 1) * 128], dstT[:], reads=[dkey], writes=["qkT_d"])
            for pair in range(16):
                psl = pair % 2
                br = bank[1 + 2 * psl]; bim = bank[2 + 2 * psl]; brk = "bk%d" % (1 + 2 * psl); bik = "bk%d" % (2 + 2 * psl)
                a_ = pair // 2; wp = pair % 2; chc = a_ // 2; hb = a_ % 2; e2 = chc * 2 + wp
                rows = slice(64 * hb, 64 * hb + 64)
                for half, (bkt, bkk) in enumerate(((br, brk), (bim, bik))):
                    for s in range(8):
                        S.op("pe", lambda e, e2=e2, s=s, half=half, rows=rows, chc=chc, bkt=bkt: e.matmul(bkt[:, 0:256], lhsT=P["LT2"][rows, e2, s, half, :], rhs=uT[rows, chc, s, :], start=(s == 0), stop=(s == 7)),
                             reads=["LT2", "uT"], writes=[bkk])
                S.op("act", lambda e, pair=pair, br=br: e.copy(out=TRE[:, pair, 1:257], in_=br[:, 0:256]), reads=[brk], writes=["TRE%d" % pair])
                S.op("act", lambda e, pair=pair, bim=bim: e.copy(out=TIM[:, pair, 1:257], in_=bim[:, 0:256]), reads=[bik], writes=["TIM%d" % pair])
            if q < 3:
                for p0 in (0, 8):
                    kre = ["TRE%d" % p for p in range(p0, p0 + 8)]; kim = ["TIM%d" % p for p in range(p0, p0 + 8)]
                    src_r, src_i, srk, sik = TRE[:, p0:p0 + 8, 1:257], TIM[:, p0:p0 + 8, 1:257], kre, kim
                    bufs = [(tBr[:], tBi[:], ["tBr"], ["tBi"]), (tCr[:], tCi[:], ["tCr"], ["tCi"])]
                    for j in range(8):
                        n = 256 >> j; h = n // 2
                        wrb = bc(P["WPr"][:, j, p0:p0 + 8].unsqueeze(2), [128, 8, h]); wib = bc(P["WPi"][:, j, p0:p0 + 8].unsqueeze(2), [128, 8, h])
                        sre, sro = src_r[:, :, 0:n:2], src_r[:, :, 1:n:2]; sie, sio = src_i[:, :, 0:n:2], src_i[:, :, 1:n:2]
                        if j == 7:
                            dr, di, drk, dik = REDr[:, p0:p0 + 8].unsqueeze(2), REDi[:, p0:p0 + 8].unsqueeze(2), ["REDr%d" % p0], ["REDi%d" % p0]
                        else:
                            bb = bufs[j % 2]
                            dr, di, drk, dik = bb[0][:, :, 0:h], bb[1][:, :, 0:h], bb[2], bb[3]
                        t1v, t2v, t3v, t4v = tt1[:, :, 0:h], tt2[:, :, 0:h], tt3[:, :, 0:h], tt4[:, :, 0:h]
                        S.op("dve", lambda e, t1v=t1v, sre=sre, wrb=wrb: e.tensor_tensor(out=t1v, in0=sre, in1=wrb, op=ALU.mult), reads=srk + ["WPr"], writes=["tt1"])
                        S.op("pool", lambda e, t2v=t2v, sie=sie, wib=wib: e.tensor_tensor(out=t2v, in0=sie, in1=wib, op=ALU.mult), reads=sik + ["WPi"], writes=["tt2"])
                        S.op("pool", lambda e, t3v=t3v, sie=sie, wrb=wrb: e.tensor_tensor(out=t3v, in0=sie, in1=wrb, op=ALU.mult), reads=sik + ["WPr"], writes=["tt3"])
                        S.op("dve", lambda e, t4v=t4v, sre=sre, wib=wib: e.tensor_tensor(out=t4v, in0=sre, in1=wib, op=ALU.mult), reads=srk + ["WPi"], writes=["tt4"])
                        S.op("dve", lambda e, t1v=t1v, t2v=t2v: e.tensor_tensor(out=t1v, in0=t1v, in1=t2v, op=ALU.subtract), reads=["tt1", "tt2"], writes=["tt1"])
                        S.op("pool", lambda e, t3v=t3v, t4v=t4v: e.tensor_tensor(out=t3v, in0=t3v, in1=t4v, op=ALU.add), reads=["tt3", "tt4"], writes=["tt3"])
                        S.op("dve", lambda e, dr=dr, t1v=t1v, sro=sro: e.tensor_tensor(out=dr, in0=t1v, in1=sro, op=ALU.add), reads=["tt1"] + srk, writes=drk)
                        S.op("pool", lambda e, di=di, t3v=t3v, sio=sio: e.tensor_tensor(out=di, in0=t3v, in1=sio, op=ALU.add), reads=["tt3"] + sik, writes=dik)
                        if j < 7:
                            src_r, src_i, srk, sik = bb[0], bb[1], bb[2], bb[3]
            if q < 3:
                w8r = P["WPr"][:, 8, :]; w8i = P["WPi"][:, 8, :]
                S.op("dve", lambda e: e.tensor_tensor(out=c1[:], in0=w8r, in1=carry_r[:], op=ALU.mult), reads=["WPr", "carry_r"], writes=["c1"])
                S.op("dve", lambda e: e.tensor_tensor(out=c2[:], in0=w8i, in1=carry_i[:], op=ALU.mult), reads=["WPi", "carry_i"], writes=["c2"])
                S.op("dve", lambda e: e.tensor_tensor(out=c1[:], in0=c1[:], in1=c2[:], op=ALU.subtract), reads=["c1", "c2"], writes=["c1"])
                S.op("dve", lambda e: e.tensor_tensor(out=c1[:], in0=c1[:], in1=REDr[:], op=ALU.add), reads=["c1", "REDr0", "REDr8"], writes=["c1"])
                S.op("dve", lambda e: e.tensor_tensor(out=c2[:], in0=w8r, in1=carry_i[:], op=ALU.mult), reads=["WPr", "carry_i", "c1"], writes=["c2"])
                S.op("dve", lambda e: e.tensor_tensor(out=c3[:], in0=w8i, in1=carry_r[:], op=ALU.mult), reads=["WPi", "carry_r"], writes=["c3"])
                S.op("dve", lambda e: e.tensor_tensor(out=c2[:], in0=c2[:], in1=c3[:], op=ALU.add), reads=["c2", "c3"], writes=["c2"])
                S.op("dve", lambda e: e.tensor_tensor(out=carry_i[:], in0=c2[:], in1=REDi[:], op=ALU.add), reads=["c2", "REDi0", "REDi8"], writes=["carry_i"])
                S.op("dve", lambda e: e.tensor_copy(out=carry_r[:], in_=c1[:]), reads=["c1"], writes=["carry_r"])


def _s5_scan_out(C, es, D, P, idb, TRE, TIM, uT, ytm, carry_r, carry_i, dbg):
    S = C.S
    sb = lambda name, shape, dt=F32: C.sb(es, name, shape, dt)
    Mz2 = sb("Mz2s", [128, 16, 8, 128], BF16); CAA = sb("CAA", [128, 2, 32, 128], BF16)
    S.dma("sp", Mz2[:].rearrange("p a b c -> p (a b c)"), D["Mz_d"], reads=["Mz_d"], writes=["Mz2s"])
    S.dma("sp", CAA[:].rearrange("p a g c -> p a (g c)"), D["CA_d"], reads=["CA_d"], writes=["CAA"])
    hs1 = sb("hs1", [128, 8, 256]); hs2 = sb("hs2", [128, 8, 256]); hs3 = sb("hs3", [128, 8, 256]); hs4 = sb("hs4", [128, 8, 256])
    Tbr = [sb("Tbr%d" % i, [128, 256], BF16) for i in range(2)]; Tbi = [sb("Tbi%d" % i, [128, 256], BF16) for i in range(2)]
    Yg = [sb("Yg%d" % i, [128, 256], BF16) for i in range(2)]
    ysum = [sb("ysum%d" % i, [128, 256]) for i in range(2)]
    import os
    CUT = int(os.environ.get("SCAN_CUT", "9"))
    for p0 in (0, 8):
        kre = ["TRE%d" % p for p in range(p0, p0 + 8)]; kim = ["TIM%d" % p for p in range(p0, p0 + 8)]
        S.op("dve", lambda e, p0=p0: e.tensor_copy(out=TRE[:, p0:p0 + 8, 0:1], in_=carry_r[:, p0:p0 + 8].unsqueeze(2)), reads=["carry_r"], writes=kre)
        S.op("pool", lambda e, p0=p0: e.tensor_copy(out=TIM[:, p0:p0 + 8, 0:1], in_=carry_i[:, p0:p0 + 8].unsqueeze(2)), reads=["carry_i"], writes=kim)
        for j in range(9):
            d = 1 << j; m = 257 - d
            wrb = bc(P["WPr"][:, j, p0:p0 + 8].unsqueeze(2), [128, 8, m]); wib = bc(P["WPi"][:, j, p0:p0 + 8].unsqueeze(2), [128, 8, m])
            R0 = TRE[:, p0:p0 + 8, 0:m]; I0 = TIM[:, p0:p0 + 8, 0:m]; R1 = TRE[:, p0:p0 + 8, d:257]; I1 = TIM[:, p0:p0 + 8, d:257]
            h1, h2, h3, h4 = hs1[:, :, 0:m], hs2[:, :, 0:m], hs3[:, :, 0:m], hs4[:, :, 0:m]
            S.op("dve", lambda e, h1=h1, R0=R0, wrb=wrb: e.tensor_tensor(out=h1, in0=R0, in1=wrb, op=ALU.mult), reads=kre + ["WPr"], writes=["hs1"])
            S.op("pool", lambda e, h2=h2, I0=I0, wib=wib: e.tensor_tensor(out=h2, in0=I0, in1=wib, op=ALU.mult), reads=kim + ["WPi"], writes=["hs2"])
            S.op("pool", lambda e, h3=h3, I0=I0, wrb=wrb: e.tensor_tensor(out=h3, in0=I0, in1=wrb, op=ALU.mult), reads=kim + ["WPr"], writes=["hs3"])
            S.op("dve", lambda e, h4=h4, R0=R0, wib=wib: e.tensor_tensor(out=h4, in0=R0, in1=wib, op=ALU.mult), reads=kre + ["WPi"], writes=["hs4"])
            S.op("dve", lambda e, h1=h1, h2=h2: e.tensor_tensor(out=h1, in0=h1, in1=h2, op=ALU.subtract), reads=["hs1", "hs2"], writes=["hs1"])
            S.op("pool", lambda e, h3=h3, h4=h4: e.tensor_tensor(out=h3, in0=h3, in1=h4, op=ALU.add), reads=["hs3", "hs4"], writes=["hs3"])
            S.op("dve", lambda e, R1=R1, h1=h1: e.tensor_tensor(out=R1, in0=R1, in1=h1, op=ALU.add), reads=kre + ["hs1"], writes=kre)
            S.op("pool", lambda e, I1=I1, h3=h3: e.tensor_tensor(out=I1, in0=I1, in1=h3, op=ALU.add), reads=kim + ["hs3"], writes=kim)
    with scope(C) as esp:
        bank = [C.ps(esp, "sbk%d" % i, [128, 512]) for i in range(6)]
        for pair in range(16):
            sl = pair % 2
            kr, ki = "TRE%d" % pair, "TIM%d" % pair
            a_ = pair // 2; wp = pair % 2; chc = a_ // 2; hb = a_ % 2
            rows = slice(64 * hb, 64 * hb + 64)
            kr, ki = "TRE%d" % pair, "TIM%d" % pair
            if CUT < 2:
                continue
            S.op("act", lambda e, pair=pair, sl=sl: e.copy(out=Tbr[sl][:], in_=TRE[:, pair, 0:256]), reads=[kr], writes=["Tbr%d" % sl])
            S.op("act", lambda e, pair=pair, sl=sl: e.copy(out=Tbi[sl][:], in_=TIM[:, pair, 0:256]), reads=[ki], writes=["Tbi%d" % sl])
            for r in range(2):
                g = 4 * a_ + 2 * r + wp
                e_ = chc * 4 + (g % 4)
                pr = slice(64 * r, 64 * r + 64)
                yb = bank[r]; ybk = "sbk%d" % r
                for s in range(8):
                    S.op("pe", lambda e, e_=e_, s=s, rows=rows, chc=chc, yb=yb: e.matmul(yb[:, 0:256], lhsT=Mz2[rows, e_, s, :], rhs=uT[rows, chc, s, :], start=(s == 0), stop=(s == 7)),
                         reads=["Mz2s", "uT"], writes=[ybk])
                zb_ = bank[4 + r]; zbk = "sbk%d" % (4 + r)
                S.op("pe", lambda e, g=g, pr=pr, zb_=zb_, r=r, sl=sl: e.matmul(zb_[:, 0:256], lhsT=CAA[pr, r, g, :], rhs=Tbr[sl][pr, :], start=True, stop=False), reads=["CAA", "Tbr%d" % sl], writes=[zbk])
                S.op("pe", lambda e, g=g, pr=pr, zb_=zb_, r=r, sl=sl: e.matmul(zb_[:, 0:256], lhsT=CAA[pr, 1 - r, g, :], rhs=Tbi[sl][pr, :], start=False, stop=True), reads=["CAA", "Tbi%d" % sl], writes=[zbk])
                if CUT < 3:
                    continue
                S.op("act", lambda e, r=r, zb_=zb_: e.copy(out=ysum[r][:], in_=zb_[:, 0:256]), reads=[zbk], writes=["ysum%d" % r])
                S.op("dve", lambda e, r=r, yb=yb: e.tensor_tensor(out=ysum[r][:], in0=yb[:, 0:256], in1=ysum[r][:], op=ALU.add), reads=[ybk, "ysum%d" % r], writes=["ysum%d" % r])
                S.op("act", lambda e, r=r: e.activation(out=Yg[r][:], in_=ysum[r][:], func=AF.Gelu), reads=["ysum%d" % r], writes=["Yg%d" % r])
                if CUT < 4:
                    continue
                pT = bank[2 + r][:].bitcast(BF16).rearrange("p (c n) -> p c n", n=128)
                for kb in range(2):
                    S.op("pe", lambda e, r=r, kb=kb, pT=pT: e.transpose(out=pT[:, kb, :], in_=Yg[r][:, kb * 128:(kb + 1) * 128], identity=idb[:]), reads=["Yg%d" % r, "identb"], writes=["sbk%d" % (2 + r)])
                for kb in range(2):
                    S.op("dve", lambda e, g=g, kb=kb, pT=pT: e.tensor_copy(out=ytm[:, kb, :, 16 * g:16 * g + 16], in_=pT[:, kb, :].rearrange("p (t c) -> p t c", c=16)),
                         reads=["sbk%d" % (2 + r)], writes=["ytm"])


def _s5_glu_out(C, es, D, idb, ytm, dbg):
    S = C.S
    sb = lambda name, shape, dt=F32: C.sb(es, name, shape, dt)
    gso = sb("gso", [128, 4]); bgl = sb("bgl", [128, 512])
    S.dma("sp", gso[:], D["g_ssm_out"], writes=["gso"])
    S.dma("sp", bgl[:], D["b_glu"].partition_broadcast(128), writes=["bgl"])
    with scope(C) as est:
        Wg = load_weight_bf16(C, es, est, "Wg", D["w_glu"], 4, 512)
    with scope(C) as est:
        Wo = load_weight_bf16(C, es, est, "Wos", D["w_out"][512:1024, :], 4, 1024, gcol=(gso, "gso"))
    yT = sb("yT", [128, 4, 128], BF16); zb = sb("zb", [128, 512]); ssm = sb("ssm", [128, 512]); junk = sb("junk3", [128, 512])
    st = sb("st3", [128, 2]); sn = sb("sn", [128, 512], BF16); snT = sb("snT", [128, 4, 128], BF16)
    ho = [sb("ho%d" % i, [128, 1024]) for i in range(2)]
    hsv = D["hs_d"].rearrange("(k t) d -> t k d", t=8)
    with scope(C) as esp:
        bank = [C.ps(esp, "gbk%d" % i, [128, 512]) for i in range(5)]
        it = 0
        for kb in range(2):
            for t in range(8):
                sl = it % 2; it += 1
                y = ytm[:, kb, t, :]
                pT = bank[0][:].bitcast(BF16).rearrange("p (c n) -> p c n", n=128)[:, 0:4, :]
                transpose_chunks(C, y, "ytm", 4, pT, "gbk0", yT[:], "yT", idb)
                for c in range(4):
                    S.op("pe", lambda e, c=c: e.matmul(bank[1][:], lhsT=yT[:, c, :], rhs=Wg[:, c, :], start=(c == 0), stop=(c == 3)), reads=["yT", "Wg"], writes=["gbk1"])
                S.op("dve", lambda e: e.tensor_tensor(out=zb[:], in0=bank[1][:], in1=bgl[:], op=ALU.add), reads=["gbk1", "bgl"], writes=["zb"])
                S.op("act", lambda e: e.activation(out=zb[:], in_=zb[:], func=AF.Sigmoid), reads=["zb"], writes=["zb"])
                S.op("pool", lambda e, y=y: e.tensor_tensor(out=ssm[:], in0=y, in1=zb[:], op=ALU.mult), reads=["ytm", "zb"], writes=["ssm"])
                if "ssm" in dbg:
                    S.dma("sp", dbg["ssm"].rearrange("(k t) d -> t k d", t=8)[t, kb * 128:(kb + 1) * 128, :], ssm[:], reads=["ssm"], writes=["o_ssm"])
                rms_rstd(C, ssm[:], "ssm", junk[:], "junk3", st[:, 0:1], st[:, 1:2], "st3", 512)
                S.op("dve", lambda e: e.tensor_scalar(out=sn[:], in0=ssm[:], scalar1=st[:, 1:2], scalar2=None, op0=ALU.mult), reads=["ssm", "st3"], writes=["sn"])
                pT2 = bank[2][:].bitcast(BF16).rearrange("p (c n) -> p c n", n=128)[:, 0:4, :]
                transpose_chunks(C, sn[:], "sn", 4, pT2, "gbk2", snT[:], "snT", idb)
                for cb in range(2):
                    for c in range(4):
                        S.op("pe", lambda e, c=c, cb=cb: e.matmul(bank[3 + cb][:], lhsT=snT[:, c, :], rhs=Wo[:, c, cb * 512:(cb + 1) * 512], start=(c == 0), stop=(c == 3)), reads=["snT", "Wos"], writes=["gbk%d" % (3 + cb)])
                    S.op("act", lambda e, cb=cb, sl=sl: e.copy(out=ho[sl][:, cb * 512:(cb + 1) * 512], in_=bank[3 + cb][:]), reads=["gbk%d" % (3 + cb)], writes=["ho%d" % sl])
                S.dma("sp", hsv[t, kb * 128:(kb + 1) * 128, :], ho[sl][:], reads=["ho%d" % sl], writes=["hs_d"])


def _attention(C, es, D, idb, dbg, prep=None):
    S = C.S
    sb = lambda name, shape, dt=F32: C.sb(es, name, shape, dt)
    kT = sb("kT", [128, 4, 2560], BF16); qT = sb("qT", [128, 4, 2048], BF16); V = sb("Vall", [128, 20, 520], BF16)
    qZ = sb("qZ", [128, 8, 2048], BF16)
    S.dma("sp", kT[:].rearrange("p c n -> p (c n)"), D["kT_d"], reads=["qkT_d"], writes=["kT"])
    S.dma("sp", qT[:].rearrange("p c n -> p (c n)"), D["qT_d"], reads=["qkT_d"], writes=["qT"])
    S.dma("sp", V[:], D["V_d"].rearrange("t p n -> p t n"), reads=["V_d"], writes=["Vall"])
    S.op("pool", lambda e: e.memset(qZ[:].rearrange("p h n -> p (h n)"), 0.0), writes=["qZ"])
    for h in range(8):
        rws = slice(64 * (h % 2), 64 * (h % 2) + 64)
        S.op("dve" if h % 2 else "act", (lambda e, h=h, rws=rws: e.tensor_copy(out=qZ[rws, h, :], in_=qT[rws, h // 2, :])) if h % 2 else (lambda e, h=h, rws=rws: e.copy(out=qZ[rws, h, :], in_=qT[rws, h // 2, :])),
             reads=["qT", "qZ"], writes=["qZ"])
    BT = sb("BT", [128, 8, 5, 128], BF16)
    gao = sb("gao", [128, 4])
    S.dma("sp", gao[:], D["g_att_out"], writes=["gao"])
    with scope(C) as est:
        stg = C.sb(est, "btstg", [128, 640])
        for h in range(8):
            S.dma("sp", stg[:], D["bias_t"][:, h * 640:(h + 1) * 640], writes=["btstg"])
            S.op("dve", lambda e, h=h: e.tensor_copy(out=BT[:, h, :, :].rearrange("p j q -> p (j q)"), in_=stg[:]), reads=["btstg"], writes=["BT"])
    with scope(C) as est:
        Wo = load_weight_bf16(C, es, est, "Woa", D["w_out"][0:512, :], 4, 1024, gcol=(gao, "gao"))
    PT = [sb("PT%d" % i, [128, 5, 128], BF16) for i in range(2)]
    rd = sb("rd", [128, 8]); att = sb("att", [128, 8, 64]); junk = sb("junk4", [128, 512]); st = sb("st4", [128, 2])
    an = sb("an", [128, 512], BF16); anT = sb("anT", [128, 4, 128], BF16)
    xo = [sb("xo%d" % i, [128, 1024]) for i in range(2)]; hsl = [sb("hsl%d" % i, [128, 1024]) for i in range(2)]
    h1t = [sb("h1t%d" % i, [128, 1024]) for i in range(2)]
    xv = D["x_ext"].rearrange("(n p) d -> n p d", p=128)
    hsv = D["hs_d"].rearrange("(n p) d -> n p d", p=128)
    h1v = D["h1_d"].rearrange("(n p) d -> n p d", p=128)
    with scope(C) as esp:
        bank = [C.ps(esp, "abk%d" % i, [128, 512]) for i in range(8)]
        for qt in range(16):
            sl = qt % 2
            drip(prep, 2)
            S.dma("sp", xo[sl][:], xv[48 + qt], writes=["xo%d" % sl])
            S.dma("sp", hsl[sl][:], hsv[qt], reads=["hs_d"], writes=["hsl%d" % sl])
            for h in range(8):
                hp = h // 2; rows = slice(64 * (h % 2), 64 * (h % 2) + 64); ps_ = h % 2
                bA = bank[2 * ps_]; bB = bank[2 * ps_ + 1]; bAk = "abk%d" % (2 * ps_); bBk = "abk%d" % (2 * ps_ + 1)
                for j in range(5):
                    o = bA[:, j * 128:(j + 1) * 128] if j < 4 else bB[:, 0:128]
                    ok = bAk if j < 4 else bBk
                    S.op("pe", lambda e, o=o, h=h, hp=hp, j=j, qt=qt: e.matmul(o, lhsT=kT[:, hp, (qt + j) * 128:(qt + j + 1) * 128], rhs=qZ[:, h, qt * 128:(qt + 1) * 128], start=True, stop=False),
                         reads=["kT", "qZ"], writes=[ok])
                    S.op("pe", lambda e, o=o, h=h, j=j: e.matmul(o, lhsT=idb[:], rhs=BT[:, h, j, :], start=False, stop=True), reads=["identb", "BT"], writes=[ok])
                S.op("act", lambda e, ps_=ps_, bA=bA: e.activation(out=PT[ps_][:, 0:4, :].rearrange("p j q -> p (j q)"), in_=bA[:], func=AF.Exp), reads=[bAk], writes=["PT%d" % ps_])
                S.op("act", lambda e, ps_=ps_, bB=bB: e.activation(out=PT[ps_][:, 4, :], in_=bB[:, 0:128], func=AF.Exp), reads=[bBk], writes=["PT%d" % ps_])
                ob = bank[4 + h // 4]; obk = "abk%d" % (4 + h // 4)
                for j in range(5):
                    S.op("pe", lambda e, ob=ob, h=h, j=j, ps_=ps_, qt=qt: e.matmul(ob[:, (h % 4) * 65:(h % 4) * 65 + 65], lhsT=PT[ps_][:, j, :], rhs=V[:, qt + j, h * 65:(h + 1) * 65], start=(j == 0), stop=(j == 4)),
                         reads=["PT%d" % ps_, "Vall"], writes=[obk])
            for hb in range(2):
                ov = bank[4 + hb][:, 0:260].rearrange("p (h d) -> p h d", d=65)
                S.op("dve", lambda e, hb=hb, ov=ov: e.reciprocal(out=rd[:, hb * 4:(hb + 1) * 4], in_=ov[:, :, 64]), reads=["abk%d" % (4 + hb)], writes=["rd%d" % hb])
                S.op("dve", lambda e, hb=hb, ov=ov: e.tensor_tensor(out=att[:, hb * 4:(hb + 1) * 4, :], in0=ov[:, :, 0:64], in1=bc(rd[:, hb * 4:(hb + 1) * 4].unsqueeze(2), [128, 4, 64]), op=ALU.mult),
                     reads=["abk%d" % (4 + hb), "rd%d" % hb], writes=["att%d" % hb])
            attf = att[:].rearrange("p h d -> p (h d)")
            if "att" in dbg:
                S.dma("sp", dbg["att"].rearrange("(n p) d -> n p d", p=128)[qt], attf, reads=["att0", "att1"], writes=["o_att"])
            S.op("act", lambda e: e.activation(out=junk[:], in_=attf, func=AF.Square, accum_out=st[:, 0:1]), reads=["att0", "att1"], writes=["st4_ss"])
            S.op("act", lambda e: e.activation(out=st[:, 1:2], in_=st[:, 0:1], func=AF.Sqrt, scale=1.0 / 512, bias=EPS), reads=["st4_ss"], writes=["st4_sq"])
            S.op("dve", lambda e: e.reciprocal(out=st[:, 1:2], in_=st[:, 1:2]), reads=["st4_sq"], writes=["st4"])
            S.op("dve", lambda e: e.tensor_scalar(out=an[:], in0=attf, scalar1=st[:, 1:2], scalar2=None, op0=ALU.mult), reads=["att0", "att1", "st4"], writes=["an"])
            pT = bank[6][:].bitcast(BF16).rearrange("p (c n) -> p c n", n=128)[:, 0:4, :]
            transpose_chunks(C, an[:], "an", 4, pT, "abk6", anT[:], "anT", idb)
            for cb in range(2):
                ob_ = 7 - cb
                for c in range(4):
                    S.op("pe", lambda e, c=c, cb=cb, ob_=ob_: e.matmul(bank[ob_][:], lhsT=anT[:, c, :], rhs=Wo[:, c, cb * 512:(cb + 1) * 512], start=(c == 0), stop=(c == 3)), reads=["anT", "Woa"], writes=["abk%d" % ob_])
                S.op("dve", lambda e, cb=cb, sl=sl, ob_=ob_: e.tensor_tensor(out=h1t[sl][:, cb * 512:(cb + 1) * 512], in0=bank[ob_][:], in1=xo[sl][:, cb * 512:(cb + 1) * 512], op=ALU.add),
                     reads=["abk%d" % ob_, "xo%d" % sl], writes=["h1t%d" % sl])
            S.op("pool", lambda e, sl=sl: e.tensor_tensor(out=h1t[sl][:], in0=h1t[sl][:], in1=hsl[sl][:], op=ALU.add), reads=["h1t%d" % sl, "hsl%d" % sl], writes=["h1t%d" % sl])
            S.dma("sp", h1v[qt], h1t[sl][:], reads=["h1t%d" % sl], writes=["h1_d"])


def _col(g, n):
    return np.ascontiguousarray(np.asarray(g, np.float32).reshape(n, 128).T)


def host_shared(inp):
    f = lambda k: np.asarray(inp[k], np.float32)[0]
    sh = {}
    sh["g_mix"] = _col(f("norm_mix_g"), 8)
    sh["w_in"] = np.ascontiguousarray(f("w_in"))
    sh["att_q_g"] = f("att_q_g").reshape(1, 64)
    sh["att_k_g"] = f("att_k_g").reshape(1, 64)
    rb = f("rel_bias")
    p = np.arange(128); j = np.arange(5); q = np.arange(128)
    kidx = j[:, None] * 128 + p[None, :]
    kc = kidx // 64; ki = kidx % 64
    qc = q // 64; qi = q % 64
    jb = kc[:, :, None] - qc[None, None, :]
    allowed = (jb >= 0) & (jb <= 8)
    kj = jb * 64 + ki[:, :, None]
    dist = 512 + qi[None, None, :] - kj
    bucket = np.clip(np.clip(dist, -63, 128) + 63, 0, 191)
    bt = np.where(allowed[None], rb[:, bucket], np.float32(NEG))
    sh["bias_t"] = np.ascontiguousarray(bt.transpose(2, 0, 1, 3).reshape(128, 8 * 5 * 128).astype(np.float32))
    dup = lambda a: np.ascontiguousarray(np.concatenate([a, a], 0).astype(np.float32))
    sh["s5_lr"] = dup(f("ssm_lam_re").T)
    sh["s5_li"] = dup(f("ssm_lam_im").T)
    sh["s5_ls"] = np.ascontiguousarray(np.broadcast_to(f("ssm_log_step")[None, :], (128, 32)).astype(np.float32))
    sg = np.ones((128, 2), np.float32); sg[:64, 0] = -1.0; sg[64:, 1] = -1.0
    sh["s5_sg"] = sg
    sh["s5_nv"] = np.ascontiguousarray(np.broadcast_to(np.arange(-7, 16, dtype=np.float32)[None, :], (128, 23)))
    bre = f("ssm_b_re").transpose(1, 0, 2).reshape(64, 512); bim = f("ssm_b_im").transpose(1, 0, 2).reshape(64, 512)
    cre = f("ssm_c_re").transpose(2, 0, 1).reshape(64, 512); cim = f("ssm_c_im").transpose(2, 0, 1).reshape(64, 512)
    sh["s5_p1b"] = np.ascontiguousarray(np.concatenate([bre, bim], 0)); sh["s5_p2b"] = np.ascontiguousarray(np.concatenate([bim, bre], 0))
    sh["s5_p1c"] = np.ascontiguousarray(np.concatenate([cre, cim], 0)); sh["s5_p2c"] = np.ascontiguousarray(np.concatenate([cim, cre], 0))
    dd = np.zeros((2, 4, 16, 4, 4, 16), np.float32)
    dsk = f("ssm_d")
    for g in range(32):
        chc = g // 8; hb = (g % 8) // 4; j4 = g % 4
        for c in range(16):
            dd[hb, j4, c, chc, j4, c] = dsk[g, c]
    sh["s5_dd"] = dd.reshape(128, 256)
    sh["g_ssm_out"] = _col(f("ssm_out_g"), 4)
    sh["g_att_out"] = _col(f("att_out_g"), 4)
    sh["b_glu"] = f("ssm_b_glu").reshape(1, 512)
    sh["w_glu"] = np.ascontiguousarray(f("ssm_w_glu"))
    sh["w_out"] = np.ascontiguousarray(f("w_out"))
    return sh


def host_core(inp, c):
    b, seg = c // 4, c % 4
    x = np.asarray(inp["x"], np.float32)
    xe = np.zeros((8192, 1024), np.float32)
    n = (seg + 1) * 2048
    xe[8192 - n:] = x[b, :n]
    hv = np.full((512,), 1.0 if seg > 0 else 0.0, np.float32)
    return {"x_ext": xe, "hvalid": np.ascontiguousarray(hv.reshape(4, 128).T)}


IN_SHAPES = {
    "x_ext": [8192, 1024], "hvalid": [128, 4], "g_mix": [128, 8], "w_in": [1024, 2048], "att_q_g": [1, 64], "att_k_g": [1, 64],
    "bias_t": [128, 5120], "s5_lr": [128, 32], "s5_li": [128, 32], "s5_ls": [128, 32], "s5_sg": [128, 2], "s5_nv": [128, 23],
    "s5_p1b": [128, 512], "s5_p2b": [128, 512], "s5_p1c": [128, 512], "s5_p2c": [128, 512], "s5_dd": [128, 256],
    "g_ssm_out": [128, 4], "g_att_out": [128, 4], "b_glu": [1, 512], "w_glu": [512, 512], "w_out": [1024, 1024],
}
IN_SHAPES_MEM = {"mem": [256, 1024], "g_mem": [128, 8], "g_memkv": [128, 8], "mem_q_g": [1, 256], "mem_k_g": [1, 256],
                 "w_mem_q": [1024, 1024], "w_mem_k": [1024, 1024], "w_mem_v": [1024, 1024], "w_mem_o": [1024, 1024]}
IN_SHAPES_PEER = {"g_peer": [1, 1024], "w_peer_q": [1024, 2048], "keysT": [128, 2048], "peer_uT": [1024, 16384], "peer_v": [16384, 1024]}
SCRATCH = {"kT_d": ([128, 4 * 2560], BF16), "qT_d": ([128, 4 * 2048], BF16), "V_d": ([20, 128, 520], BF16),
           "hs_d": ([2048, 1024], F32), "Mz_d": ([128, 16 * 8 * 128], BF16), "CA_d": ([128, 2, 4096], BF16)}
SCRATCH_PEER = {"uT_b": ([1024, 16384], BF16), "v_b": ([16384, 1024], BF16), "xnT_d": ([16, 128, 1024], BF16),
                "sc_d": ([16, 128, 2048], F32), "tau_d": ([16, 128, 8], F32)}


def host_shared_rest(inp):
    f = lambda k: np.asarray(inp[k], np.float32)[0]
    sh = {}
    sh["g_mem"] = _col(f("norm_mem_g"), 8); sh["g_memkv"] = _col(f("norm_memkv_g"), 8)
    sh["mem_q_g"] = f("mem_q_g").reshape(1, 256); sh["mem_k_g"] = f("mem_k_g").reshape(1, 256)
    for k in ("w_mem_q", "w_mem_k", "w_mem_v", "w_mem_o", "w_peer_q"):
        sh[k] = np.ascontiguousarray(f(k))
    sh["g_peer"] = f("norm_peer_g").reshape(1, 1024)
    sh["keysT"] = np.ascontiguousarray(f("peer_keys").transpose(3, 0, 1, 2).reshape(128, 2048))
    sh["peer_uT"] = np.ascontiguousarray(f("peer_u").T)
    sh["peer_v"] = np.ascontiguousarray(f("peer_v"))
    return sh


def build_program(stages=("mixer", "mem", "peer"), dbg_specs=None, upto=9):
    nc = bass.Bass("TRN2", target_bir_lowering=False)
    D = {}
    shapes = {}
    if "mixer" in stages:
        shapes.update(IN_SHAPES)
    if "mem" in stages:
        shapes.update(IN_SHAPES_MEM)
    if "peer" in stages:
        shapes.update(IN_SHAPES_PEER)
    for k, shp in shapes.items():
        D[k] = nc.dram_tensor(k, shp, F32, kind="ExternalInput").ap()
    scr = {}
    if "mixer" in stages:
        scr.update(SCRATCH)
    if "peer" in stages:
        scr.update(SCRATCH_PEER)
    for k, (shp, dt) in scr.items():
        D[k] = nc.dram_tensor(k, shp, dt).ap()
    chain = ["h1_d", "h2_d", "out"]
    first = {"mixer": None, "mem": "h1_d", "peer": "h2_d"}[stages[0]]
    last = {"mixer": "h1_d", "mem": "h2_d", "peer": "out"}[stages[-1]]
    for k in chain:
        if k == first:
            D[k] = nc.dram_tensor(k, [2048, 1024], F32, kind="ExternalInput").ap()
        elif k == last:
            D[k] = nc.dram_tensor(k, [2048, 1024], F32, kind="ExternalOutput").ap()
        else:
            D[k] = nc.dram_tensor(k, [2048, 1024], F32).ap()
    dbg = {}
    for k, shp in (dbg_specs or {}).items():
        dbg[k] = nc.dram_tensor("dbg_" + k, shp, F32, kind="ExternalOutput").ap()
    with ExitStack() as es:
        S = Sched(nc, es)
        C = Ctx(nc, S)
        prep = stage_peer_prep(C, D) if "peer" in stages else None
        if "mixer" in stages:
            stage_mixer(C, D, dbg, upto, prep=prep)
        if "mem" in stages:
            stage_mem(C, D, dbg, prep=prep)
        drip(prep, 1000)
        if "peer" in stages:
            stage_peer(C, D, dbg)
        S.barrier()
        S.emit()
    return nc, S


def _headnorm(C, src_ap, skey, nh, hd, sqt, sqk, stat, stk, gt, gkey, dst_ap, dkey):
    S = C.S
    sv = src_ap.rearrange("p (h d) -> p h d", d=hd)
    S.op("act", lambda e: e.activation(out=sqt, in_=src_ap, func=AF.Square), reads=[skey], writes=[sqk])
    S.op("dve", lambda e: e.tensor_reduce(out=stat[:, 0:nh], in_=sqt.rearrange("p (h d) -> p h d", d=hd), axis=AX.X, op=ALU.add), reads=[sqk], writes=[stk + "a"])
    S.op("act", lambda e: e.activation(out=stat[:, nh:2 * nh], in_=stat[:, 0:nh], func=AF.Sqrt, scale=1.0 / hd, bias=EPS), reads=[stk + "a"], writes=[stk + "b"])
    S.op("dve", lambda e: e.reciprocal(out=stat[:, nh:2 * nh], in_=stat[:, nh:2 * nh]), reads=[stk + "b"], writes=[stk])
    S.op("dve", lambda e: e.tensor_tensor(out=sv, in0=sv, in1=bc(stat[:, nh:2 * nh].unsqueeze(2), [128, nh, hd]), op=ALU.mult), reads=[skey, stk], writes=[skey])
    S.op("pool", lambda e: e.tensor_tensor(out=dst_ap.rearrange("p (h d) -> p h d", d=hd), in0=sv, in1=bc(gt.unsqueeze(1), [128, nh, hd]), op=ALU.mult), reads=[skey, gkey], writes=[dkey])


def stage_mem(C, D, dbg=None, prep=None):
    S = C.S
    dbg = dbg or {}
    with scope(C) as es:
        sb = lambda name, shape, dt=F32: C.sb(es, name, shape, dt)
        idf, idb = make_ident(C, es, "ident")
        gm = sb("gm", [128, 8]); gkv = sb("gkv", [128, 8]); gq = sb("mgq", [128, 256]); gk = sb("mgk", [128, 256])
        S.dma("sp", gm[:], D["g_mem"], writes=["gm"]); S.dma("sp", gkv[:], D["g_memkv"], writes=["gkv"])
        S.dma("sp", gq[:], D["mem_q_g"].partition_broadcast(128), writes=["mgq"]); S.dma("sp", gk[:], D["mem_k_g"].partition_broadcast(128), writes=["mgk"])
        S.op("dve", lambda e: e.tensor_scalar(out=gq[:], in0=gq[:], scalar1=1.0 / 16, scalar2=None, op0=ALU.mult), reads=["mgq"], writes=["mgq"])
        kTm = sb("kTm", [128, 8, 256], BF16); Vm = sb("Vm", [128, 2, 4, 257], BF16)
        xt = [sb("mxt%d" % i, [128, 1024]) for i in range(2)]; st = sb("mst", [128, 2]); xn = sb("mxn", [128, 1024], BF16)
        xnT = sb("mxnT", [128, 8, 128], BF16); qf = sb("mqf", [128, 1024]); sq = sb("msq", [128, 1024]); qst = sb("mqst", [128, 8])
        qn = sb("mqn", [128, 1024], BF16); mjunk = sb("mjunk", [128, 1024], BF16)
        with scope(C) as esk:
            with scope(C) as est:
                Wk = load_weight_bf16(C, esk, est, "Wmk", D["w_mem_k"], 8, 1024, gcol=(gkv, "gkv"))
            with scope(C) as est:
                Wv = load_weight_bf16(C, esk, est, "Wmv", D["w_mem_v"], 8, 1024, gcol=(gkv, "gkv"))
            with scope(C) as esp:
                bank = [C.ps(esp, "mkb%d" % i, [128, 512]) for i in range(6)]
                mv = D["mem"].rearrange("(n p) d -> n p d", p=128)
                for mt in range(2):
                    x_ = xt[mt]; xk = "mxt%d" % mt
                    S.dma("sp", x_[:], mv[mt], writes=[xk])
                    rms_rstd(C, x_[:], xk, mjunk[:], "mjunk", st[:, 0:1], st[:, 1:2], "mst", 1024)
                    S.op("dve", lambda e, x_=x_: e.tensor_scalar(out=xn[:], in0=x_[:], scalar1=st[:, 1:2], scalar2=None, op0=ALU.mult), reads=[xk, "mst"], writes=["mxn"])
                    pb = bank[0][:].bitcast(BF16).rearrange("p (c n) -> p c n", n=128)
                    transpose_chunks(C, xn[:], "mxn", 8, pb, "mkb0", xnT[:], "mxnT", idb)
                    for (W, wk, b0) in ((Wk, "Wmk", 1), (Wv, "Wmv", 3)):
                        for cb in range(2):
                            for dc in range(8):
                                S.op("pe", lambda e, W=W, cb=cb, dc=dc, b0=b0: e.matmul(bank[b0 + cb][:], lhsT=xnT[:, dc, :], rhs=W[:, dc, cb * 512:(cb + 1) * 512], start=(dc == 0), stop=(dc == 7)),
                                     reads=["mxnT", wk], writes=["mkb%d" % (b0 + cb)])
                    for cb in range(2):
                        S.op("act", lambda e, cb=cb: e.copy(out=qf[:, cb * 512:(cb + 1) * 512], in_=bank[1 + cb][:]), reads=["mkb%d" % (1 + cb)], writes=["mqf"])
                        S.op("dve", lambda e, cb=cb, mt=mt: e.tensor_copy(out=Vm[:, mt, 2 * cb:2 * cb + 2, 0:256], in_=bank[3 + cb][:].rearrange("p (h d) -> p h d", d=256)), reads=["mkb%d" % (3 + cb)], writes=["Vm"])
                    S.op("pool", lambda e, mt=mt: e.memset(Vm[:, mt, :, 256], 1.0), reads=[], writes=["Vm"])
                    _headnorm(C, qf[:], "mqf", 4, 256, sq[:], "msq", qst, "mqst", gk[:], "mgk", qn[:], "mqn")
                    pb2 = bank[5][:].bitcast(BF16).rearrange("p (c n) -> p c n", n=128)
                    transpose_chunks(C, qn[:], "mqn", 8, pb2, "mkb5", kTm[:, :, mt * 128:(mt + 1) * 128], "kTm", idb)
        with scope(C) as est:
            Wq = load_weight_bf16(C, es, est, "Wmq", D["w_mem_q"], 8, 1024, gcol=(gm, "gm"))
        with scope(C) as est:
            Wo = load_weight_bf16(C, es, est, "Wmo", D["w_mem_o"], 8, 1024)
        NM = 2
        xn2 = [sb("mxn_%d" % i, [128, 1024], BF16) for i in range(NM)]; xnT2 = [sb("mxnT_%d" % i, [128, 8, 128], BF16) for i in range(NM)]
        qf2 = [sb("mqf_%d" % i, [128, 1024]) for i in range(NM)]; sq2 = [sb("msq_%d" % i, [128, 1024]) for i in range(NM)]; qst2 = [sb("mqst_%d" % i, [128, 8]) for i in range(NM)]
        qn2 = [sb("mqn_%d" % i, [128, 1024], BF16) for i in range(NM)]; st2 = [sb("mst_%d" % i, [128, 2]) for i in range(NM)]
        qT2 = [sb("mqT%d" % i, [128, 8, 128], BF16) for i in range(NM)]; PT = [sb("mPT%d" % i, [128, 2, 128], BF16) for i in range(2)]
        rd2 = [sb("mrd%d" % i, [128, 4]) for i in range(NM)]; ob2 = [sb("mob%d" % i, [128, 1024], BF16) for i in range(NM)]; oT2 = [sb("moT%d" % i, [128, 8, 128], BF16) for i in range(NM)]
        h2t = [sb("h2t%d" % i, [128, 1024]) for i in range(2)]
        hv = D["h1_d"].rearrange("(n p) d -> n p d", p=128); ov = D["h2_d"].rearrange("(n p) d -> n p d", p=128)
        with scope(C) as esp:
            bank = [C.ps(esp, "mb%d" % i, [128, 512]) for i in range(8)]

            def mtile(tt):
                sl = tt % NM
                K = lambda nm: "%s_%d" % (nm, sl)
                xn = xn2[sl]; xnT = xnT2[sl]; qf = qf2[sl]; sq = sq2[sl]; qst = qst2[sl]; qn = qn2[sl]; st = st2[sl]; qT = qT2[sl]; rd = rd2[sl]; ob = ob2[sl]; oT = oT2[sl]
                drip(prep, 1)
                x_ = xt[sl]; xk = "mxt%d" % sl
                S.dma("sp", x_[:], hv[tt], reads=["h1_d"], writes=[xk])
                rms_rstd(C, x_[:], xk, mjunk[:], "mjunk", st[:, 0:1], st[:, 1:2], K("mst"), 1024)
                S.op("dve", lambda e: e.tensor_scalar(out=xn[:], in0=x_[:], scalar1=st[:, 1:2], scalar2=None, op0=ALU.mult), reads=[xk, K("mst")], writes=[K("mxn")])
                pb = bank[0][:].bitcast(BF16).rearrange("p (c n) -> p c n", n=128)
                transpose_chunks(C, xn[:], K("mxn"), 8, pb, "mb0", xnT[:], K("mxnT"), idb)
                yield
                for cb in range(2):
                    for dc in range(8):
                        S.op("pe", lambda e, cb=cb, dc=dc: e.matmul(bank[1 + cb][:], lhsT=xnT[:, dc, :], rhs=Wq[:, dc, cb * 512:(cb + 1) * 512], start=(dc == 0), stop=(dc == 7)),
                             reads=[K("mxnT"), "Wmq"], writes=["mb%d" % (1 + cb)])
                    S.op("act", lambda e, cb=cb: e.copy(out=qf[:, cb * 512:(cb + 1) * 512], in_=bank[1 + cb][:]), reads=["mb%d" % (1 + cb)], writes=[K("mqf")])
                yield
                _headnorm(C, qf[:], K("mqf"), 4, 256, sq[:], K("msq"), qst, K("mqst"), gq[:], "mgq", qn[:], K("mqn"))
                pb2 = bank[3][:].bitcast(BF16).rearrange("p (c n) -> p c n", n=128)
                transpose_chunks(C, qn[:], K("mqn"), 8, pb2, "mb3", qT[:], K("mqT"), idb)
                yield
                for h in range(4):
                    ps_ = h % 2
                    sbk = bank[4 + ps_]; sbkk = "mb%d" % (4 + ps_)
                    for mt in range(2):
                        for dh in range(2):
                            S.op("pe", lambda e, h=h, mt=mt, dh=dh, sbk=sbk: e.matmul(sbk[:, mt * 128:(mt + 1) * 128], lhsT=kTm[:, 2 * h + dh, mt * 128:(mt + 1) * 128], rhs=qT[:, 2 * h + dh, :], start=(dh == 0), stop=(dh == 1)),
                                 reads=["kTm", K("mqT")], writes=[sbkk])
                    S.op("act", lambda e, ps_=ps_, sbk=sbk: e.activation(out=PT[ps_][:].rearrange("p m q -> p (m q)"), in_=sbk[:, 0:256], func=AF.Exp, bias=-8.0), reads=[sbkk], writes=["mPT%d" % ps_])
                    obk = bank[6 + ps_]; obkk = "mb%d" % (6 + ps_)
                    for mt in range(2):
                        S.op("pe", lambda e, h=h, mt=mt, ps_=ps_, obk=obk: e.matmul(obk[:, 0:257], lhsT=PT[ps_][:, mt, :], rhs=Vm[:, mt, h, :], start=(mt == 0), stop=(mt == 1)), reads=["mPT%d" % ps_, "Vm"], writes=[obkk])
                    S.op("dve", lambda e, h=h, obk=obk: e.reciprocal(out=rd[:, h:h + 1], in_=obk[:, 256:257]), reads=[obkk], writes=[K("mrd") + "_%d" % h])
                    S.op("dve", lambda e, h=h, obk=obk: e.tensor_scalar(out=ob[:, h * 256:(h + 1) * 256], in0=obk[:, 0:256], scalar1=rd[:, h:h + 1], scalar2=None, op0=ALU.mult), reads=[obkk, K("mrd") + "_%d" % h], writes=[K("mob")])
                    yield
                pb3 = bank[0][:].bitcast(BF16).rearrange("p (c n) -> p c n", n=128)
                transpose_chunks(C, ob[:], K("mob"), 8, pb3, "mb0", oT[:], K("moT"), idb)
                yield
                for cb in range(2):
                    for dc in range(8):
                        S.op("pe", lambda e, cb=cb, dc=dc: e.matmul(bank[1 + cb][:], lhsT=oT[:, dc, :], rhs=Wo[:, dc, cb * 512:(cb + 1) * 512], start=(dc == 0), stop=(dc == 7)),
                             reads=[K("moT"), "Wmo"], writes=["mb%d" % (1 + cb)])
                    S.op("dve", lambda e, cb=cb: e.tensor_tensor(out=h2t[sl][:, cb * 512:(cb + 1) * 512], in0=bank[1 + cb][:], in1=x_[:, cb * 512:(cb + 1) * 512], op=ALU.add),
                         reads=["mb%d" % (1 + cb), xk], writes=["h2t%d" % sl])
                S.dma("sp", ov[tt], h2t[sl][:], reads=["h2t%d" % sl], writes=["h2_d"])

            from itertools import zip_longest
            gens = []
            for t0 in range(0, 16, NM):
                for _ in zip_longest(*[mtile(t0 + i) for i in range(NM)]):
                    pass


def stage_peer_prep(C, D):
    S = C.S
    uv = D["peer_uT"].rearrange("(c p) (a e) -> c p a e", p=128, e=2048)
    ub = D["uT_b"].rearrange("(c p) (a e) -> c p a e", p=128, e=2048)
    vv = D["peer_v"].rearrange("(c p) d -> c p d", p=512)
    vb = D["v_b"].rearrange("(c p) d -> c p d", p=512)

    def gen():
        for c in range(8):
            S.dma("pool", ub[c], uv[c], writes=["uT_b"])
            yield
        for c in range(32):
            S.dma("pool", vb[c], vv[c], writes=["v_b"])
            yield
    return gen()


def drip(g, n):
    if g is None:
        return
    for _ in range(n):
        try:
            next(g)
        except StopIteration:
            return


def _top16(C, src, skey, work, wkey, dst, dkey):
    S = C.S
    S.op("dve", lambda e: e.max(out=dst[:, 0:8], in_=src), reads=[skey], writes=[dkey])
    S.op("dve", lambda e: e.match_replace(out=work, in_to_replace=dst[:, 0:8], in_values=src, imm_value=-1e30), reads=[skey, dkey], writes=[wkey])
    S.op("dve", lambda e: e.max(out=dst[:, 8:16], in_=work), reads=[wkey], writes=[dkey])


def stage_peer(C, D, dbg=None):
    S = C.S
    dbg = dbg or {}
    hv = D["h2_d"].rearrange("(n p) d -> n p d", p=128)
    with scope(C) as es:
        sb = lambda name, shape, dt=F32: C.sb(es, name, shape, dt)
        idf, idb = make_ident(C, es, "ident")
        gp = sb("gpb", [128, 1024])
        S.dma("sp", gp[:], D["g_peer"].partition_broadcast(128), writes=["gpb"])
        with scope(C) as est:
            Wq = load_weight_bf16(C, es, est, "Wpq", D["w_peer_q"], 8, 2048)
        keyT = sb("keyT", [128, 16, 128], BF16)
        with scope(C) as est:
            kst = C.sb(est, "kst", [128, 2048])
            S.dma("sp", kst[:], D["keysT"], writes=["kst"])
            S.op("dve", lambda e: e.tensor_copy(out=keyT[:].rearrange("p a n -> p (a n)"), in_=kst[:]), reads=["kst"], writes=["keyT"])
        NS = 3
        xt = [sb("pxt%d" % i, [128, 1024]) for i in range(NS)]; st_ = [sb("pst%d" % i, [128, 2]) for i in range(NS)]; junk = sb("pjunk", [128, 1024], BF16)
        xn_ = [sb("pxn%d" % i, [128, 1024], BF16) for i in range(NS)]; xnT = [sb("pxnT%d" % i, [128, 8, 128], BF16) for i in range(NS)]
        qb_ = [sb("pqb%d" % i, [128, 2048], BF16) for i in range(NS)]; qTp_ = [sb("pqT%d" % i, [128, 16, 128], BF16) for i in range(NS)]
        sc = [sb("psc%d" % i, [128, 16, 128]) for i in range(NS)]; work_ = [sb("pwork%d" % i, [128, 256]) for i in range(NS)]
        sv_ = [sb("psv%d" % i, [128, 16, 16]) for i in range(NS)]; cand_ = [sb("pcand%d" % i, [128, 8, 256]) for i in range(NS)]
        cex_ = [sb("pcex%d" % i, [128, 8, 256]) for i in range(NS)]; ctop_ = [sb("pctop%d" % i, [128, 8, 16]) for i in range(NS)]
        Z_ = [sb("pZ%d" % i, [128, 8]) for i in range(NS)]; off_ = [sb("poff%d" % i, [128, 8]) for i in range(NS)]; tau = [sb("ptau%d" % i, [128, 8]) for i in range(NS)]
        cjunk_ = [[sb("pcj%d_%d" % (i, h), [128, 256], BF16) for h in range(8)] for i in range(NS)]; offs_ = [sb("poffs%d" % i, [128, 16]) for i in range(NS)]
        xTd = D["xnT_d"].rearrange("n p (c t) -> n p c t", t=128)
        with scope(C) as esp:
            bank = [C.ps(esp, "pab%d" % i, [128, 512]) for i in range(6)]

            def tile(tt):
                sl = tt % NS
                K = lambda nm: "%s%d" % (nm, sl)
                x_ = xt[sl]; xk = K("pxt"); st = st_[sl]; xn = xn_[sl]; qb = qb_[sl]; qTp = qTp_[sl]; work = work_[sl]
                sv = sv_[sl]; cand = cand_[sl]; cex = cex_[sl]; ctop = ctop_[sl]; Z = Z_[sl]; off = off_[sl]; cjunk = cjunk_[sl]; offs = offs_[sl]
                S.dma("sp", x_[:], hv[tt], reads=["h2_d"], writes=[xk])
                rms_rstd(C, x_[:], xk, junk[:], "pjunk", st[:, 0:1], st[:, 1:2], K("pst"), 1024)
                S.op("dve", lambda e: e.scalar_tensor_tensor(out=xn[:], in0=x_[:], scalar=st[:, 1:2], in1=gp[:], op0=ALU.mult, op1=ALU.mult), reads=[xk, K("pst"), "gpb"], writes=[K("pxn")])
                pb = bank[0][:].bitcast(BF16).rearrange("p (c n) -> p c n", n=128)
                transpose_chunks(C, xn[:], K("pxn"), 8, pb, "pab0", xnT[sl][:], K("pxnT"), idb)
                S.dma("sp", xTd[tt], xnT[sl][:], reads=[K("pxnT")], writes=["xnT_d"])
                yield
                for cb in range(4):
                    for dc in range(8):
                        S.op("pe", lambda e, cb=cb, dc=dc: e.matmul(bank[1 + cb][:], lhsT=xnT[sl][:, dc, :], rhs=Wq[:, dc, cb * 512:(cb + 1) * 512], start=(dc == 0), stop=(dc == 7)),
                             reads=[K("pxnT"), "Wpq"], writes=["pab%d" % (1 + cb)])
                    S.op("act", lambda e, cb=cb: e.copy(out=qb[:, cb * 512:(cb + 1) * 512], in_=bank[1 + cb][:]), reads=["pab%d" % (1 + cb)], writes=[K("pqb")])
                for half in range(2):
                    pbq = bank[5][:].bitcast(BF16).rearrange("p (c n) -> p c n", n=128)
                    transpose_chunks(C, qb[:, half * 1024:(half + 1) * 1024], K("pqb"), 8, pbq, "pab5", qTp[:, half * 8:(half + 1) * 8, :], K("pqT"), idb)
                for hh in range(16):
                    S.op("pe", lambda e, hh=hh: e.matmul(bank[1 + hh // 4][:, (hh % 4) * 128:(hh % 4 + 1) * 128], lhsT=qTp[:, hh, :], rhs=keyT[:, hh, :], start=True, stop=True),
                         reads=[K("pqT"), "keyT"], writes=["pab%d" % (1 + hh // 4)])
                scs = sc[sl]; sck = K("psc")
                for cb in range(4):
                    S.op("act", lambda e, cb=cb: e.copy(out=scs[:, cb * 4:(cb + 1) * 4, :].rearrange("p a n -> p (a n)"), in_=bank[1 + cb][:]), reads=["pab%d" % (1 + cb)], writes=[sck])
                yield
                for hh in range(16):
                    _top16(C, scs[:, hh, :], sck, work[:, 0:128], K("pwork"), sv[:, hh, :], K("psv"))
                    yield
                svv = sv[:].rearrange("p (h s) k -> p h s k", s=2)
                for h in range(8):
                    S.op("dve", lambda e, h=h: e.tensor_tensor(out=cand[:, h, :].rearrange("p (a b) -> p a b", b=16), in0=bc(svv[:, h, 0, :].unsqueeze(2), [128, 16, 16]),
                                                            in1=bc(svv[:, h, 1, :].unsqueeze(1), [128, 16, 16]), op=ALU.add), reads=[K("psv")], writes=[K("pcand") + "_%d" % h])
                    yield
                ck = [K("pcand") + "_%d" % h for h in range(8)]
                for h in range(8):
                    _top16(C, cand[:, h, :], ck[h], work[:], K("pwork"), ctop[:, h, :], K("pctop"))
                    yield
                S.op("dve", lambda e: e.tensor_tensor(out=cand[:], in0=cand[:], in1=bc(ctop[:, :, 0:1], [128, 8, 256]), op=ALU.subtract), reads=ck + [K("pctop")], writes=ck)
                S.op("act", lambda e: e.activation(out=cex[:].rearrange("p h n -> p (h n)"), in_=cand[:].rearrange("p h n -> p (h n)"), func=AF.Exp), reads=ck, writes=[K("pcex")])
                yield
                S.op("dve", lambda e: e.tensor_tensor(out=tau[sl][:], in0=ctop[:, :, 15], in1=ctop[:, :, 0], op=ALU.subtract), reads=[K("pctop")], writes=[K("ptau")])
                yield
                S.op("dve", lambda e: e.tensor_scalar(out=tau[sl][:], in0=tau[sl][:], scalar1=-1e-5, scalar2=None, op0=ALU.add), reads=[K("ptau")], writes=[K("ptau")])
                yield
                for h in range(8):
                    S.op("dve", lambda e, h=h: e.scalar_tensor_tensor(out=cjunk[h][:], in0=cand[:, h, :], scalar=tau[sl][:, h:h + 1], in1=cex[:, h, :], op0=ALU.is_ge, op1=ALU.mult, accum_out=Z[:, h:h + 1]),
                         reads=ck + [K("pcex"), K("ptau")], writes=[K("pZ") + "_%d" % h, K("pcj") + "_%d" % h])
                    yield
                S.op("act", lambda e: e.activation(out=off[:], in_=Z[:], func=AF.Ln), reads=[K("pZ") + "_%d" % h for h in range(8)], writes=[K("poff")])
                yield
                S.op("dve", lambda e: e.tensor_tensor(out=tau[sl][:], in0=tau[sl][:], in1=off[:], op=ALU.subtract), reads=[K("ptau"), K("poff")], writes=[K("ptau")])
                S.op("act", lambda e: e.activation(out=tau[sl][:], in_=tau[sl][:], func=AF.Exp), reads=[K("ptau")], writes=[K("ptau")])
                yield
                S.op("dve", lambda e: e.tensor_scalar(out=tau[sl][:], in0=tau[sl][:], scalar1=0.99997, scalar2=None, op0=ALU.mult), reads=[K("ptau")], writes=[K("ptau")])
                offv = offs[:].rearrange("p (h s) -> p h s", s=2)
                S.op("dve", lambda e: e.tensor_copy(out=offv[:, :, 0], in_=svv[:, :, 0, 0]), reads=[K("psv")], writes=[K("poffs") + "a"])
                yield
                S.op("dve", lambda e: e.tensor_tensor(out=offv[:, :, 1], in0=svv[:, :, 1, 0], in1=off[:], op=ALU.add), reads=[K("psv"), K("poff")], writes=[K("poffs") + "b"])
                yield
                S.op("dve", lambda e: e.tensor_tensor(out=scs[:], in0=scs[:], in1=bc(offs[:].unsqueeze(2), [128, 16, 128]), op=ALU.subtract), reads=[sck, K("poffs") + "a", K("poffs") + "b"], writes=[sck])
                S.op("act", lambda e: e.activation(out=scs[:].rearrange("p a n -> p (a n)"), in_=scs[:].rearrange("p a n -> p (a n)"), func=AF.Exp), reads=[sck], writes=[sck])
                S.dma("sp", D["sc_d"][tt], scs[:].rearrange("p a n -> p (a n)"), reads=[sck], writes=["sc_d"])
                S.dma("sp", D["tau_d"][tt], tau[sl][:], reads=[K("ptau")], writes=["tau_d"])

            from itertools import zip_longest
            for t0 in range(0, 16, NS):
                for _ in zip_longest(*[tile(t0 + i) for i in range(NS) if t0 + i < 16]):
                    pass
    with scope(C) as es:
        sb = lambda name, shape, dt=F32: C.sb(es, name, shape, dt)
        idf, idb = make_ident(C, es, "ident")
        xnT = sb("bxnT", [128, 4, 1024], BF16); sc = sb("bsc", [128, 4, 2048]); kap = sb("btau", [128, 4, 8])
        UT = [sb("UT%d" % i, [128, 8, 1024], BF16) for i in range(2)]; Vb = [sb("Vb%d" % i, [128, 8, 1024], BF16) for i in range(2)]
        acc = sb("pacc", [128, 4, 1024])
        NP = 12
        Pt = [sb("pP%d" % i, [128, 512]) for i in range(NP)]; Wh = [[sb("pWh%d_%d" % (i, h), [128, 512], BF16) for h in range(8)] for i in range(2)]
        G = [sb("pG%d" % i, [128, 512], BF16) for i in range(3)]; WA = [sb("pWA%d" % i, [128, 512], BF16) for i in range(2)]
        WAT = [sb("pWAT%d" % i, [128, 4, 128], BF16) for i in range(2)]
        h2t = [sb("ph2t%d" % i, [128, 1024]) for i in range(2)]
        uTv = D["uT_b"].rearrange("(c p) (b e) -> b p c e", p=128, e=1024)
        vbv = D["v_b"].rearrange("(b c p) d -> b p c d", p=128, c=8)
        ov = D["out"].rearrange("(n p) d -> n p d", p=128)
        with scope(C) as esp:
            bank = [C.ps(esp, "pbb%d" % i, [128, 512]) for i in range(7)]
            state = {"it": 0}

            def stage1a(u, tg, eb, sub, tt, es_):
                ub = u % 2
                xT = xnT[:, tt, :].rearrange("p (c t) -> p c t", t=128)
                scv = sc[:, tt, :].rearrange("p (h s n) -> p h s n", s=2, n=128)
                i0 = eb * 8 + sub * 4
                for dc in range(8):
                    S.op("pe", lambda e, dc=dc, xT=xT: e.matmul(bank[ub][:], lhsT=xT[:, dc, :], rhs=UT[es_][:, dc, sub * 512:(sub + 1) * 512], start=(dc == 0), stop=(dc == 7)),
                         reads=["bxnT", "UT%d" % es_], writes=["pbb%d" % ub])
                for h in range(8):
                    hs = state["it"] % NP; state["it"] += 1
                    if h >= 5:
                        S.op("pool", lambda e, h=h, hs=hs, scv=scv: e.tensor_tensor(out=Pt[hs][:].rearrange("p (i j) -> p i j", j=128), in0=bc(scv[:, h, 0, i0:i0 + 4].unsqueeze(2), [128, 4, 128]),
                                                                             in1=bc(scv[:, h, 1, :].unsqueeze(1), [128, 4, 128]), op=ALU.mult), reads=["bsc"], writes=["pP%d_%d" % (hs, il) for il in range(4)])
                    else:
                        for il in range(4):
                            S.op("act", lambda e, h=h, hs=hs, scv=scv, il=il: e.activation(out=Pt[hs][:, il * 128:(il + 1) * 128], in_=scv[:, h, 1, :], func=AF.Copy, scale=scv[:, h, 0, i0 + il:i0 + il + 1]),
                                 reads=["bsc"], writes=["pP%d_%d" % (hs, il)])
                    S.op("dve", lambda e, h=h, hs=hs: e.scalar_tensor_tensor(out=Wh[ub][h][:], in0=Pt[hs][:], scalar=kap[:, tt, h:h + 1], in1=Pt[hs][:], op0=ALU.is_ge, op1=ALU.mult),
                         reads=["pP%d_%d" % (hs, il) for il in range(4)] + ["btau"], writes=["pWh%d_%d" % (ub, h)])

            def st_gelu(u, tg, eb, sub, tt, es_):
                ub = u % 2; gb = u % 3
                S.op("act", lambda e: e.activation(out=G[gb][:], in_=bank[ub][:], func=AF.Gelu), reads=["pbb%d" % ub], writes=["pG%d" % gb])

            def st_hs(u, tg, eb, sub, tt, es_):
                ub = u % 2
                for h in range(8):
                    S.op("pe", lambda e, h=h: e.matmul(bank[2 + ub][:], lhsT=idb[:], rhs=Wh[ub][h][:], start=(h == 0), stop=(h == 7)),
                         reads=["identb", "pWh%d_%d" % (ub, h)], writes=["pbb%d" % (2 + ub)])

            def st_wa(u, tg, eb, sub, tt, es_):
                ub = u % 2; gb = u % 3
                S.op("dve", lambda e: e.tensor_tensor(out=WA[ub][:], in0=bank[2 + ub][:], in1=G[gb][:], op=ALU.mult), reads=["pbb%d" % (2 + ub), "pG%d" % gb], writes=["pWA%d" % ub])

            def st_t(u, tg, eb, sub, tt, es_):
                ub = u % 2
                pbt = bank[4][:].bitcast(BF16).rearrange("p (c n) -> p c n", n=128)[:, 0:4, :]
                for c in range(4):
                    S.op("pe", lambda e, c=c: e.transpose(out=pbt[:, c, :], in_=WA[ub][:, c * 128:(c + 1) * 128], identity=idb[:]), reads=["pWA%d" % ub, "identb"], writes=["pbb4"])

            def st_watcopy(u, tg, eb, sub, tt, es_):
                ub = u % 2
                pbt = bank[4][:].bitcast(BF16).rearrange("p (c n) -> p c n", n=128)[:, 0:4, :]
                S.op("act", lambda e: e.copy(out=WAT[ub][:], in_=pbt), reads=["pbb4"], writes=["pWAT%d" % ub])

            def st_v(u, tg, eb, sub, tt, es_):
                ub = u % 2
                for cb in range(2):
                    for ec in range(4):
                        S.op("pe", lambda e, cb=cb, ec=ec: e.matmul(bank[5 + cb][:], lhsT=WAT[ub][:, ec, :], rhs=Vb[es_][:, sub * 4 + ec, cb * 512:(cb + 1) * 512], start=(sub == 0 and ec == 0), stop=(sub == 1 and ec == 3)),
                             reads=["pWAT%d" % ub, "Vb%d" % es_], writes=["pbb%d" % (5 + cb)])

            def st_acc(u, tg, eb, sub, tt, es_):
                if sub != 1:
                    return
                for cb in range(2):
                    if eb == 0:
                        S.op("dve", lambda e, cb=cb: e.tensor_copy(out=acc[:, tt, cb * 512:(cb + 1) * 512], in_=bank[5 + cb][:]), reads=["pbb%d" % (5 + cb)], writes=["pacc%d" % tt])
                    else:
                        S.op("dve", lambda e, cb=cb: e.tensor_tensor(out=acc[:, tt, cb * 512:(cb + 1) * 512], in0=bank[5 + cb][:], in1=acc[:, tt, cb * 512:(cb + 1) * 512], op=ALU.add),
                             reads=["pbb%d" % (5 + cb), "pacc%d" % tt], writes=["pacc%d" % tt])

            u = 0
            for tg in range(4):
                S.dma("sp", xnT[:], D["xnT_d"][tg * 4:(tg + 1) * 4].rearrange("n p f -> p n f"), reads=["xnT_d"], writes=["bxnT"])
                S.dma("sp", sc[:], D["sc_d"][tg * 4:(tg + 1) * 4].rearrange("n p f -> p n f"), reads=["sc_d"], writes=["bsc"])
                S.dma("sp", kap[:], D["tau_d"][tg * 4:(tg + 1) * 4].rearrange("n p f -> p n f"), reads=["tau_d"], writes=["btau"])
                units = []
                for eb in range(16):
                    es_ = (tg * 16 + eb) % 2
                    for tt in range(4):
                        for sub in range(2):
                            units.append((u, tg, eb, sub, tt, es_)); u += 1
                n = len(units)
                U = lambda j: units[j] if 0 <= j < n else None
                for k in range(n + 3):
                    for ebk in ([0] if k == 0 else []) + ([k // 8 + 1] if (k % 8 == 3 and k // 8 + 1 < 16) else []):
                        es_ = (tg * 16 + ebk) % 2
                        S.dma("sp", UT[es_][:], uTv[ebk], reads=["uT_b"], writes=["UT%d" % es_])
                        S.dma("sp", Vb[es_][:], vbv[ebk], reads=["v_b"], writes=["Vb%d" % es_])
                    if U(k - 2): st_wa(*U(k - 2))
                    if U(k - 3): st_v(*U(k - 3))
                    if U(k - 2): st_t(*U(k - 2))
                    if U(k - 1): st_gelu(*U(k - 1))
                    if U(k - 1): st_hs(*U(k - 1))
                    if U(k): stage1a(*U(k))
                    if U(k - 2): st_watcopy(*U(k - 2))
                    if U(k - 3): st_acc(*U(k - 3))
                for tt in range(4):
                    sl = tt % 2; n = tg * 4 + tt
                    S.dma("sp", h2t[sl][:], hv[n], reads=["h2_d"], writes=["ph2t%d" % sl])
                    S.op("pool", lambda e, sl=sl, tt=tt: e.tensor_tensor(out=h2t[sl][:], in0=h2t[sl][:], in1=acc[:, tt, :], op=ALU.add), reads=["ph2t%d" % sl, "pacc%d" % tt], writes=["ph2t%d" % sl])
                    S.dma("sp", ov[n], h2t[sl][:], reads=["ph2t%d" % sl], writes=["out"])


_PROG = {}


def kernel(**inputs):
    sh = host_shared(inputs)
    sh.update(host_shared_rest(inputs))
    if "nc" not in _PROG:
        _PROG["nc"] = build_program(("mixer", "mem", "peer"))[0]
    nc = _PROG["nc"]
    names = set(IN_SHAPES) | set(IN_SHAPES_MEM) | set(IN_SHAPES_PEER)
    mem = np.asarray(inputs["mem"], np.float32)
    maps = []
    for c in range(8):
        m = {k: v for k, v in sh.items() if k in names}
        m.update(host_core(inputs, c))
        m["mem"] = np.ascontiguousarray(mem[c // 4])
        maps.append(m)
    res = run_bass_kernel_spmd(nc, maps, core_ids=list(range(8)))
    out = np.zeros((2, 8192, 1024), np.float32)
    for c in range(8):
        out[c // 4, (c % 4) * 2048:(c % 4 + 1) * 2048] = res.results[c]["out"]
    return out
```
